# Optimizing a Trainium2 kernel written in Bass

```python
import math
import jax
import jax.numpy as jnp
from jax import lax
import numpy as np

D_MODEL = 1024
BATCH = 8
SEQ = 8192
DEPTH = 1

GRID_W = 64
CTX_LEN = 256
ATTN_HEADS = 8
ATTN_KV_HEADS = 2
ATTN_HEAD_DIM = 64
ATTN_WINDOW = 128
ATTN_BLOCK = 128
ROPE_BASE = 10000.0
ROPE_PAIRS = ATTN_HEAD_DIM // 4
DN_HEADS = 4
DN_HEAD_DIM = 128
DN_CONV = 5
DN_CHUNK = 64
PEER_HEADS = 8
PEER_KEYS = 128
PEER_EXPERTS = PEER_KEYS * PEER_KEYS
PEER_KEY_DIM = 128
PEER_KEY_HALF = PEER_KEY_DIM // 2
PEER_TOPK = 16
PEER_BLOCK = 128
RMS_EPS = 1e-6
L2_EPS = 1e-6
ATTN_Q = ATTN_HEADS * ATTN_HEAD_DIM
ATTN_KV = ATTN_KV_HEADS * ATTN_HEAD_DIM
DN_WIDTH = DN_HEADS * DN_HEAD_DIM
IN_SPLITS = (ATTN_Q, ATTN_KV, ATTN_KV, 3 * DN_WIDTH, DN_WIDTH, DN_HEADS, DN_HEADS, DN_HEADS, DN_HEADS, 2 * D_MODEL)
IN_COLS = sum(IN_SPLITS)
F32 = jnp.float32

kernel_name = 'hybrid_diffusion_swa_deltanet_peer'


def rmsnorm(x, gain):
    x32 = x.astype(F32)
    y = x32 * lax.rsqrt(jnp.mean(x32 * x32, axis=-1, keepdims=True) + RMS_EPS)
    return (y * gain.astype(F32)).astype(x.dtype)


def l2norm(x):
    x32 = x.astype(F32)
    return x32 * lax.rsqrt(jnp.sum(x32 * x32, axis=-1, keepdims=True) + L2_EPS)


def rope_angles(pos):
    inv_freq = ROPE_BASE ** (-jnp.arange(ROPE_PAIRS, dtype=F32) / ROPE_PAIRS)
    ang = pos.astype(F32)[:, None] * inv_freq[None, :]
    return jnp.cos(ang)[:, None, :], jnp.sin(ang)[:, None, :]


def rotate_pairs(x, cos, sin):
    x1, x2 = jnp.split(x.astype(F32), 2, axis=-1)
    return jnp.concatenate([x1 * cos - x2 * sin, x1 * sin + x2 * cos], axis=-1)


def axial_rope(x, rope):
    cos_r, sin_r, cos_c, sin_c = rope
    x_row, x_col = jnp.split(x, 2, axis=-1)
    out = jnp.concatenate([rotate_pairs(x_row, cos_r, sin_r), rotate_pairs(x_col, cos_c, sin_c)], axis=-1)
    return out.astype(x.dtype)


def short_conv(u, w):
    pad = DN_CONV // 2
    return lax.conv_general_dilated(u, w[:, None, :].astype(u.dtype), window_strides=(1,), padding=[(pad, pad)], dimension_numbers=('NWC', 'WIO', 'NWC'), feature_group_count=u.shape[-1])


def project_mixer_inputs(h, w_in, b_gate, conv_w):
    B, T, _ = h.shape
    p = h @ w_in
    offsets = np.cumsum(IN_SPLITS)[:-1].tolist()
    q_a, k_a, v_a, qkv_d, z_d, a_f, a_b, b_f, b_b, gates = jnp.split(p, offsets, axis=-1)
    qkv_d = jax.nn.silu(short_conv(qkv_d, conv_w))
    q_d, k_d, v_d = jnp.split(qkv_d, 3, axis=-1)
    g_attn, g_dn = jnp.split(jax.nn.sigmoid(gates + b_gate), 2, axis=-1)
    dn_heads = lambda t: t.reshape(B, T, DN_HEADS, DN_HEAD_DIM)
    return {
        'q_attn': q_a.reshape(B, T, ATTN_HEADS, ATTN_HEAD_DIM),
        'k_attn': k_a.reshape(B, T, ATTN_KV_HEADS, ATTN_HEAD_DIM),
        'v_attn': v_a.reshape(B, T, ATTN_KV_HEADS, ATTN_HEAD_DIM),
        'q_dn': l2norm(dn_heads(q_d)) * DN_HEAD_DIM ** -0.5,
        'k_dn': l2norm(dn_heads(k_d)),
        'v_dn': dn_heads(v_d).astype(F32),
        'z_dn': dn_heads(z_d),
        'a_f': a_f, 'a_b': a_b, 'b_f': b_f, 'b_b': b_b,
        'gate_attn': g_attn, 'gate_dn': g_dn,
    }


def windowed_attention(q, k, v, k_ctx, v_ctx, sink):
    B, S, H, hd = q.shape
    KV = k.shape[2]
    G = H // KV
    L = k_ctx.shape[1]
    nb = S // ATTN_BLOCK
    span = ATTN_BLOCK + 2 * ATTN_WINDOW
    scale = hd ** -0.5
    qb = jnp.moveaxis(q.reshape(B, nb, ATTN_BLOCK, KV, G, hd), 1, 0)
    pad = ((0, 0), (ATTN_WINDOW, ATTN_WINDOW), (0, 0), (0, 0))
    kp = jnp.pad(k, pad)
    vp = jnp.pad(v, pad)
    s_sink = jnp.broadcast_to(sink.astype(F32).reshape(1, KV, G, 1, 1), (B, KV, G, ATTN_BLOCK, 1))
    s_ctx_all = None

    def one_block(args):
        i, qi = args
        start = i * ATTN_BLOCK
        kw = lax.dynamic_slice_in_dim(kp, start, span, axis=1)
        vw = lax.dynamic_slice_in_dim(vp, start, span, axis=1)
        qpos = start + jnp.arange(ATTN_BLOCK)
        kpos = start - ATTN_WINDOW + jnp.arange(span)
        valid = (kpos[None, :] >= 0) & (kpos[None, :] < S) & (jnp.abs(qpos[:, None] - kpos[None, :]) <= ATTN_WINDOW)
        s_loc = jnp.einsum('bqkgd,bjkd->bkgqj', qi, kw, preferred_element_type=F32) * scale
        s_loc = jnp.where(valid, s_loc, -jnp.inf)
        s_ctx = jnp.einsum('bqkgd,bjkd->bkgqj', qi, k_ctx, preferred_element_type=F32) * scale
        p = jax.nn.softmax(jnp.concatenate([s_loc, s_ctx, s_sink], axis=-1), axis=-1)
        p_loc = p[..., :span].astype(v.dtype)
        p_ctx = p[..., span:span + L].astype(v.dtype)
        return jnp.einsum('bkgqj,bjkd->bqkgd', p_loc, vw) + jnp.einsum('bkgqj,bjkd->bqkgd', p_ctx, v_ctx)

    out = lax.map(one_block, (jnp.arange(nb), qb))
    return jnp.moveaxis(out, 0, 1).reshape(B, S, H * hd)


def context_attention(q, k, v, sink):
    B, L, H, hd = q.shape
    KV = k.shape[2]
    G = H // KV
    qg = q.reshape(B, L, KV, G, hd)
    s = jnp.einsum('bqkgd,bjkd->bkgqj', qg, k, preferred_element_type=F32) * hd ** -0.5
    s_sink = jnp.broadcast_to(sink.astype(F32).reshape(1, KV, G, 1, 1), (B, KV, G, L, 1))
    p = jax.nn.softmax(jnp.concatenate([s, s_sink], axis=-1), axis=-1)[..., :L]
    o = jnp.einsum('bkgqj,bjkd->bqkgd', p.astype(v.dtype), v)
    return o.reshape(B, L, H * hd)


def gated_delta_chunked(q, k, v, g, beta, state0):
    B, T, H, DK = q.shape
    DV = v.shape[-1]
    C = DN_CHUNK
    N = T // C

    def chunks(a):
        return jnp.moveaxis(a.reshape((B, N, C, H) + a.shape[3:]), 3, 1)

    qc, kc, vc = chunks(q.astype(F32)), chunks(k.astype(F32)), chunks(v.astype(F32))
    gc, bc = chunks(g.astype(F32)), chunks(beta.astype(F32))
    Gcum = jnp.cumsum(gc, axis=-1)
    incl = jnp.tril(jnp.ones((C, C), bool))
    strict = jnp.tril(jnp.ones((C, C), bool), -1)
    decay = jnp.exp(jnp.where(incl, Gcum[..., :, None] - Gcum[..., None, :], -jnp.inf))
    kb = kc * bc[..., None]
    A = jnp.where(strict, jnp.einsum('bhnid,bhnjd->bhnij', kb, kc) * decay, 0.0)
    eye = jnp.eye(C, dtype=F32)
    t_inv = lax.linalg.triangular_solve(A + eye, jnp.broadcast_to(eye, A.shape), left_side=True, lower=True, unit_diagonal=True)
    W = t_inv @ (kb * jnp.exp(Gcum)[..., None])
    U = t_inv @ (vc * bc[..., None])
    attn = jnp.einsum('bhnid,bhnjd->bhnij', qc, kc) * decay
    q_dec = qc * jnp.exp(Gcum)[..., None]
    k_tail = kc * jnp.exp(Gcum[..., -1:] - Gcum)[..., None]
    g_tot = jnp.exp(Gcum[..., -1])

    def step(S, xs):
        W_n, U_n, attn_n, qd_n, kt_n, gt_n = xs
        v_new = U_n - jnp.einsum('bhcd,bhde->bhce', W_n, S)
        o = jnp.einsum('bhcd,bhde->bhce', qd_n, S) + jnp.einsum('bhij,bhje->bhie', attn_n, v_new)
        S = S * gt_n[..., None, None] + jnp.einsum('bhcd,bhce->bhde', kt_n, v_new)
        return S, o

    xs = tuple(jnp.moveaxis(a, 2, 0) for a in (W, U, attn, q_dec, k_tail, g_tot))
    S_fin, o = lax.scan(step, state0.astype(F32), xs)
    o = o.transpose(1, 0, 3, 2, 4).reshape(B, T, H, DV)
    return o, S_fin


def decay_and_beta(a_logit, b_logit, a_log, dt_bias):
    g = -jnp.exp(a_log.astype(F32)) * jax.nn.softplus(a_logit.astype(F32) + dt_bias.astype(F32))
    return g, jax.nn.sigmoid(b_logit.astype(F32))


def bidirectional_delta(m, a_log_f, dt_bias_f, a_log_b, dt_bias_b, state_f, state_b):
    g_f, beta_f = decay_and_beta(m['a_f'], m['b_f'], a_log_f, dt_bias_f)
    g_b, beta_b = decay_and_beta(m['a_b'], m['b_b'], a_log_b, dt_bias_b)
    q, k, v = m['q_dn'], m['k_dn'], m['v_dn']
    o_f, s_f = gated_delta_chunked(q, k, v, g_f, beta_f, state_f)
    flip = lambda t: jnp.flip(t, axis=1)
    o_b, s_b = gated_delta_chunked(flip(q), flip(k), flip(v), flip(g_b), flip(beta_b), state_b)
    return o_f + flip(o_b), s_f, s_b


def gated_head_norm(o, z, gain):
    B, T = o.shape[:2]
    y = o * lax.rsqrt(jnp.mean(o * o, axis=-1, keepdims=True) + RMS_EPS) * gain.astype(F32)
    y = y * jax.nn.silu(z.astype(F32))
    return y.reshape(B, T, DN_WIDTH).astype(z.dtype)


def merge_branches(o_attn, o_dn, m, w_br_attn, w_br_dn, w_out):
    y = m['gate_attn'] * (o_attn @ w_br_attn) + m['gate_dn'] * (o_dn @ w_br_dn)
    return y @ w_out


def peer_ffn(h, w_q, keys, u_table, v_table):
    shape = h.shape
    blocks = h.reshape(-1, PEER_BLOCK, shape[-1])

    def block(hb):
        q = (hb @ w_q).reshape(PEER_BLOCK, PEER_HEADS, 2, PEER_KEY_HALF)
        s = jnp.einsum('thpd,hpkd->thpk', q, keys, preferred_element_type=F32)
        half_s, half_i = lax.top_k(s, PEER_TOPK)
        cand = half_s[:, :, 0, :, None] + half_s[:, :, 1, None, :]
        best_s, best_c = lax.top_k(cand.reshape(PEER_BLOCK, PEER_HEADS, PEER_TOPK * PEER_TOPK), PEER_TOPK)
        i1 = jnp.take_along_axis(half_i[:, :, 0], best_c // PEER_TOPK, axis=-1)
        i2 = jnp.take_along_axis(half_i[:, :, 1], best_c % PEER_TOPK, axis=-1)
        experts = i1 * PEER_KEYS + i2
        gate = jax.nn.softmax(best_s, axis=-1)
        act = jax.nn.gelu(jnp.einsum('td,thkd->thk', hb, u_table[experts], preferred_element_type=F32))
        return jnp.einsum('thk,thkd->td', (gate * act).astype(hb.dtype), v_table[experts])

    return lax.map(block, blocks).reshape(shape)


def setup_inputs(seed: int = 0) -> dict:
    key = jax.random.key(seed)
    ks = jax.random.split(key, 26)
    D = D_MODEL
    L = DEPTH
    nrm = lambda k, shape, scale: jax.random.normal(k, shape, F32) * scale

    def a_log(k):
        return jnp.log(jax.random.uniform(k, (L, DN_HEADS), F32, 1.0, 16.0))

    def dt_bias(k):
        dt = jnp.exp(jax.random.uniform(k, (L, DN_HEADS), F32, math.log(1e-3), math.log(1e-1)))
        return dt + jnp.log(-jnp.expm1(-dt))

    return {
        'x': nrm(ks[0], (BATCH, SEQ, D), 1.0),
        'c': nrm(ks[1], (BATCH, D), 1.0),
        'ctx': nrm(ks[2], (BATCH, CTX_LEN, D), 1.0),
        'c_ctx': nrm(ks[3], (D,), 1.0),
        'w_ada': nrm(ks[4], (L, D, 6 * D), 0.5 * D ** -0.5),
        'b_ada': nrm(ks[5], (L, 6 * D), 0.01),
        'norm_mix': 1.0 + nrm(ks[6], (L, D), 0.01),
        'norm_ffn': 1.0 + nrm(ks[7], (L, D), 0.01),
        'w_in': nrm(ks[8], (L, D, IN_COLS), D ** -0.5),
        'b_gate': nrm(ks[9], (L, 2 * D), 0.01),
        'attn_sink': nrm(ks[10], (L, ATTN_HEADS), 0.5),
        'dn_conv': nrm(ks[11], (L, DN_CONV, 3 * DN_WIDTH), DN_CONV ** -0.5),
        'dn_a_log_f': a_log(ks[12]),
        'dn_dt_bias_f': dt_bias(ks[13]),
        'dn_a_log_b': a_log(ks[14]),
        'dn_dt_bias_b': dt_bias(ks[15]),
        'dn_norm': 1.0 + nrm(ks[16], (L, DN_HEAD_DIM), 0.01),
        'w_br_attn': nrm(ks[17], (L, ATTN_Q, D), ATTN_Q ** -0.5),
        'w_br_dn': nrm(ks[18], (L, DN_WIDTH, D), DN_WIDTH ** -0.5),
        'w_out': nrm(ks[19], (L, D, D), D ** -0.5),
        'peer_wq': nrm(ks[20], (L, D, PEER_HEADS * PEER_KEY_DIM), D ** -0.5),
        'peer_keys': nrm(ks[21], (L, PEER_HEADS, 2, PEER_KEYS, PEER_KEY_HALF), PEER_KEY_HALF ** -0.5),
        'peer_u': nrm(ks[22], (L, PEER_EXPERTS, D), D ** -0.5),
        'peer_v': nrm(ks[23], (L, PEER_EXPERTS, D), PEER_HEADS ** -0.5),
        'final_norm': 1.0 + nrm(ks[24], (D,), 0.01),
    }


def reference(x, c, ctx, c_ctx, w_ada, b_ada, norm_mix, norm_ffn, w_in, b_gate, attn_sink, dn_conv,
              dn_a_log_f, dn_dt_bias_f, dn_a_log_b, dn_dt_bias_b, dn_norm, w_br_attn, w_br_dn, w_out,
              peer_wq, peer_keys, peer_u, peer_v, final_norm):
    B, S, D = x.shape
    rows = S // GRID_W
    row_pos = jnp.repeat(jnp.arange(rows, dtype=jnp.int32), GRID_W)
    col_pos = jnp.tile(jnp.arange(GRID_W, dtype=jnp.int32), rows)
    rope = (*rope_angles(row_pos), *rope_angles(col_pos))
    zero_state = jnp.zeros((B, DN_HEADS, DN_HEAD_DIM, DN_HEAD_DIM), F32)

    for layer in range(DEPTH):
        mod = jax.nn.silu(c) @ w_ada[layer] + b_ada[layer]
        sh1, sc1, gt1, sh2, sc2, gt2 = [t[:, None, :] for t in jnp.split(mod, 6, axis=-1)]
        cmod = jax.nn.silu(c_ctx) @ w_ada[layer] + b_ada[layer]
        csh1, csc1, cgt1, csh2, csc2, cgt2 = jnp.split(cmod, 6, axis=-1)

        hc = rmsnorm(ctx, norm_mix[layer]) * (1.0 + csc1) + csh1
        mc = project_mixer_inputs(hc, w_in[layer], b_gate[layer], dn_conv[layer])
        oc_dn, st_f, st_b = bidirectional_delta(mc, dn_a_log_f[layer], dn_dt_bias_f[layer], dn_a_log_b[layer], dn_dt_bias_b[layer], zero_state, zero_state)

        h = rmsnorm(x, norm_mix[layer]) * (1.0 + sc1) + sh1
        m = project_mixer_inputs(h, w_in[layer], b_gate[layer], dn_conv[layer])
        q_lat = axial_rope(m['q_attn'], rope)
        k_lat = axial_rope(m['k_attn'], rope)
        o_attn = windowed_attention(q_lat, k_lat, m['v_attn'], mc['k_attn'], mc['v_attn'], attn_sink[layer])
        o_dn, _, _ = bidirectional_delta(m, dn_a_log_f[layer], dn_dt_bias_f[layer], dn_a_log_b[layer], dn_dt_bias_b[layer], st_f, st_b)
        o_dn = gated_head_norm(o_dn, m['z_dn'], dn_norm[layer])
        x = x + gt1 * merge_branches(o_attn, o_dn, m, w_br_attn[layer], w_br_dn[layer], w_out[layer])

        h2 = rmsnorm(x, norm_ffn[layer]) * (1.0 + sc2) + sh2
        x = x + gt2 * peer_ffn(h2, peer_wq[layer], peer_keys[layer], peer_u[layer], peer_v[layer])

        if layer + 1 < DEPTH:
            oc_attn = context_attention(mc['q_attn'], mc['k_attn'], mc['v_attn'], attn_sink[layer])
            oc_dn_g = gated_head_norm(oc_dn, mc['z_dn'], dn_norm[layer])
            ctx = ctx + cgt1 * merge_branches(oc_attn, oc_dn_g, mc, w_br_attn[layer], w_br_dn[layer], w_out[layer])
            hc2 = rmsnorm(ctx, norm_ffn[layer]) * (1.0 + csc2) + csh2
            ctx = ctx + cgt2 * peer_ffn(hc2, peer_wq[layer], peer_keys[layer], peer_u[layer], peer_v[layer])

    return rmsnorm(x, final_norm)
```

```python
import contextlib
import numpy as np
import concourse.bass as bass
import concourse.mybir as mybir
from concourse.bass_utils import run_bass_kernel_spmd

F32 = mybir.dt.float32
ALU = mybir.AluOpType
AF = mybir.ActivationFunctionType
AX = mybir.AxisListType

D = 1024
S = 8192
CTX = 256
TALL = CTX + S
NT = S // 128
IN_COLS = 4880
NEG = -30000.0


class _Ins:
    __slots__ = ("eng", "fn", "deps", "signal", "sig_no", "dma", "idx")

    def __init__(self, eng, fn, dma=None):
        self.eng = eng
        self.fn = fn
        self.deps = []
        self.signal = False
        self.sig_no = None
        self.dma = dma
        self.idx = None


class Sch:
    EPOCH = 20000
    NDMA = 24
    NEP = 6

    def __init__(self, nc, st):
        self.nc = nc
        self.engs = ("pe", "act", "dve", "pool", "sp")
        self.sems = {e: [st.enter_context(nc.semaphore(f"s_{e}_{i}")) for i in range(self.NEP)] for e in self.engs}
        self.dsems = [st.enter_context(nc.semaphore(f"s_dma_{i}")) for i in range(self.NDMA)]
        self.sigc = {e: 0 for e in self.engs}
        self.dma_rr = 0
        self.dma_cnt = [0] * self.NDMA
        self.dma_last = [None] * self.NDMA
        self._reset()

    def _reset(self):
        self.q = {e: [] for e in self.engs}
        self.lastw = {}
        self.readers = {}

    def _add(self, ins, reads, writes):
        q = self.q[ins.eng]
        ins.idx = len(q)
        deps = []
        for r in reads:
            w = self.lastw.get(r)
            if w is not None:
                deps.append((w, "raw"))
        for w_ in writes:
            w = self.lastw.get(w_)
            if w is not None:
                deps.append((w, "waw"))
            for rd in self.readers.get(w_, ()):
                deps.append((rd, "war"))
        for d, kind in deps:
            if d is ins:
                continue
            if d.dma is None and ins.dma is None and d.eng == ins.eng:
                if ins.eng == "pe":
                    continue
                if kind != "raw":
                    continue
            ins.deps.append(d)
            if d.dma is None:
                d.signal = True
        for r in reads:
            self.readers.setdefault(r, []).append(ins)
        for w_ in writes:
            self.lastw[w_] = ins
            self.readers[w_] = []
        q.append(ins)
        return ins

    PSUM_NAMES = {"pm", "pT", "pY", "pX", "pN", "pK", "pb", "pS", "pO", "pQ", "pZ", "pR", "pU", "pW"}

    def op(self, eng, fn, reads=(), writes=()):
        writes = list(writes)
        if eng != "pe":
            for r in reads:
                if isinstance(r, tuple) and r[0] in self.PSUM_NAMES and r not in writes:
                    writes.append(r)
        return self._add(_Ins(eng, fn), list(reads), writes)

    def dma(self, out, in_, reads=(), writes=(), queue="sp", **kw):
        slot = self.dma_rr
        self.dma_rr = (self.dma_rr + 1) % self.NDMA
        self.dma_cnt[slot] += 1
        n = self.dma_cnt[slot]
        ins = _Ins(queue, lambda e: e.dma_start(out=out, in_=in_, **kw), dma=(slot, n))
        prev = self.dma_last[slot]
        self._add(ins, list(reads), list(writes))
        if prev is not None:
            ins.deps.append(prev)
        self.dma_last[slot] = ins
        return ins

    def flush(self):
        nc = self.nc
        for e, q in self.q.items():
            for ins in q:
                if ins.dma is None and ins.signal:
                    ins.sig_no = self.sigc[e]
                    self.sigc[e] += 1
            assert self.sigc[e] < self.EPOCH * self.NEP, "too many signals"
        dma_final = list(self.dma_cnt)
        with nc.Block() as block:
            def run(ename):
                def body(eng):
                    seen_c = {}
                    seen_d = {}
                    for ins in self.q[ename]:
                        wc = {}
                        wd = {}
                        for d in ins.deps:
                            if d.dma is None:
                                if d.sig_no is None:
                                    continue
                                if seen_c.get(d.eng, -1) < d.sig_no:
                                    wc[d.eng] = max(wc.get(d.eng, -1), d.sig_no)
                            else:
                                s_, n = d.dma
                                if seen_d.get(s_, 0) < n:
                                    wd[s_] = max(wd.get(s_, 0), n)
                        for e2, sn in wc.items():
                            eng.wait_ge(self.sems[e2][sn // self.EPOCH], sn % self.EPOCH + 1)
                            seen_c[e2] = sn
                        for s_, n in wd.items():
                            eng.wait_ge(self.dsems[s_], 16 * n)
                            seen_d[s_] = n
                        h = ins.fn(eng)
                        if ins.dma is not None:
                            h.then_inc(self.dsems[ins.dma[0]], 16)
                        elif ins.signal:
                            h.then_inc(self.sems[ename][ins.sig_no // self.EPOCH], 1)
                    if ename == "sp":
                        for s_, n in enumerate(dma_final):
                            if n > 0:
                                eng.wait_ge(self.dsems[s_], 16 * n)
                return body

            block.sync(run("sp"))
            block.tensor(run("pe"))
            block.scalar(run("act"))
            block.vector(run("dve"))
            block.gpsimd(run("pool"))
        nc.all_engine_barrier()
        self._reset()


def _consts():
    c = {}
    ident = np.eye(128, dtype=np.float32)
    ones = np.ones((128, 128), np.float32)
    idx = np.arange(128)
    same = (idx[:, None] // 64 == idx[None, :] // 64).astype(np.float32)
    m1f = ((idx[:, None] <= idx[None, :]) * same).astype(np.float32)
    m1b = ((idx[:, None] >= idx[None, :]) * same).astype(np.float32)
    sel0 = np.zeros((128, 128), np.float32); sel0[:64, :] = 1
    sel1 = np.zeros((128, 128), np.float32); sel1[64:, :] = 1
    low_incl = ((idx[None, :] <= idx[:, None]) * same)
    up_incl = ((idx[None, :] >= idx[:, None]) * same)
    low_strict = ((idx[None, :] < idx[:, None]) * same)
    up_strict = ((idx[None, :] > idx[:, None]) * same)
    negmask = lambda m: np.where(m > 0, 0.0, NEG).astype(np.float32)
    w_prev = (idx[None, :] <= idx[:, None]).astype(np.float32)
    w_next = (idx[:, None] <= idx[None, :]).astype(np.float32)
    mats = [ident, ones, same, m1f, -m1f, m1b, -m1b, sel0, sel1,
            negmask(low_incl), negmask(up_incl), -low_strict.astype(np.float32), -up_strict.astype(np.float32),
            w_prev, w_next]
    c["cmat"] = np.ascontiguousarray(np.stack(mats, axis=1)).astype(np.float32)
    pos = np.arange(S)
    inv = (10000.0 ** (-np.arange(16, dtype=np.float32) / 16)).astype(np.float32)
    ar = (pos // 64).astype(np.float32)[:, None] * inv[None, :]
    ac = (pos % 64).astype(np.float32)[:, None] * inv[None, :]
    c["rope"] = np.concatenate([np.cos(ar), np.sin(ar), np.cos(ac), np.sin(ac)], axis=1).astype(np.float32)
    return c

(C_ID, C_ONES, C_SAME, C_M1F, C_NM1F, C_M1B, C_NM1B, C_SEL0, C_SEL1, C_NLOW, C_NUP, C_SLOW, C_SUP, C_WPREV, C_WNEXT) = range(15)


def build(upto=99, dbg=()):
    nc = bass.Bass("TRN2", target_bir_lowering=False)
    inp = lambda name, shape: nc.dram_tensor(name, list(shape), F32, kind="ExternalInput").ap()
    x_d = inp("x", [S, D]); c_d = inp("c", [D]); ctx_d = inp("ctx", [CTX, D]); cctx_d = inp("c_ctx", [D])
    wada_d = inp("w_ada", [D, 6 * D]); bada_d = inp("b_ada", [6 * D])
    nmix_d = inp("norm_mix", [D]); nffn_d = inp("norm_ffn", [D])
    win_d = inp("w_in", [D, IN_COLS]); bgate_d = inp("b_gate", [2 * D])
    sink_d = inp("attn_sink", [8]); conv_d = inp("dn_conv", [5, 1536])
    alf_d = inp("dn_a_log_f", [4]); dtf_d = inp("dn_dt_bias_f", [4]); alb_d = inp("dn_a_log_b", [4]); dtb_d = inp("dn_dt_bias_b", [4])
    dnn_d = inp("dn_norm", [128]); wba_d = inp("w_br_attn", [512, D]); wbd_d = inp("w_br_dn", [512, D]); wout_d = inp("w_out", [D, D])
    pwq_d = inp("peer_wq", [D, D]); pkeys_d = inp("peer_keysT", [8, 2, 64, 128]); pu_d = inp("peer_uT", [D, 16384]); pv_d = inp("peer_v", [16384, D])
    fnorm_d = inp("final_norm", [D]); cmat_d = inp("cmat", [128, 15, 128]); rope_d = inp("rope", [S, 64])
    out_d = nc.dram_tensor("out", [S, D], F32, kind="ExternalOutput").ap()
    scr = lambda name, shape: nc.dram_tensor(name, list(shape), F32, kind=("ExternalOutput" if name in dbg else "Internal")).ap()
    QT_s = scr("QT_s", [64, 8, S])
    KT_s = scr("KT_s", [64, 2, TALL])
    V_s = scr("V_s", [TALL, 2, 65])
    RT_s = scr("RT_s", [1536, TALL])
    Z_s = scr("Z_s", [S, 512])
    GB_s = scr("GB_s", [TALL, 16])
    GT_s = scr("GT_s", [S, 2048])
    QK_s = scr("QK_s", [1024, TALL])
    KV_s = scr("KV_s", [TALL, 1024])
    OD_s = scr("OD_s", [2, S, 512])
    OA_s = scr("OA_s", [S, 512])
    MOD_s = scr("MOD_s", [8, D])

    with contextlib.ExitStack() as gst:
        s = Sch(nc, gst)
        _uid = [0]

        def _nm(name):
            _uid[0] += 1
            return f"{name}_u{_uid[0]}"
        T = lambda st, name, shape: st.enter_context(nc.sbuf_tensor(_nm(name), list(shape), F32))
        PS = lambda st, name, shape: st.enter_context(nc.psum_tensor(_nm(name), list(shape), F32))
        cm = T(gst, "cm", [128, 15, 128])
        s.dma(cm[:], cmat_d, writes=["cm"])
        ident = cm[:, C_ID, :]
        bv = T(gst, "bv", [128, 9, D])
        BV_G1, BV_SH1, BV_GT1, BV_G2, BV_SH2, BV_GT2, BV_CG1, BV_CSH1, BV_FN = range(9)

        with contextlib.ExitStack() as st:
            cc = T(st, "cc", [128, 2, 8]); cs = T(st, "cs", [128, 2, 8]); lh = T(st, "lh", [128, 2, 8, 128])
            wa = [T(st, f"wa{i}", [128, 8, 512]) for i in range(2)]
            bb = T(st, "bb", [128, 6 * D]); nm = T(st, "nm", [128, 2, D])
            pm = [PS(st, f"pm{i}", [128, 512]) for i in range(2)]
            s.dma(cc[:, 0, :], c_d.rearrange("(kc p) -> p kc", p=128), writes=["cc"], allow_slow_non_contiguous=True)
            s.dma(cc[:, 1, :], cctx_d.rearrange("(kc p) -> p kc", p=128), writes=["cc"], allow_slow_non_contiguous=True)
            s.dma(bb[:], bada_d.partition_broadcast(128), writes=["bb"])
            s.dma(nm[:, 0, :], nmix_d.partition_broadcast(128), writes=["nm"])
            s.dma(nm[:, 1, :], nffn_d.partition_broadcast(128), writes=["nm"])
            s.dma(bv[:, BV_FN, :], fnorm_d.partition_broadcast(128), writes=["bv"])
            s.op("act", lambda e: e.activation(out=cs[:], in_=cc[:], func=AF.Silu), reads=["cc"], writes=["cs"])
            s.op("dve", lambda e: e.tensor_copy(out=lh[:], in_=cs[:].unsqueeze(3).to_broadcast([128, 2, 8, 128])), reads=["cs"], writes=["lh"])
            jobs = [(0, nb) for nb in range(12)] + [(1, nb) for nb in range(4)]
            for ji, (w, nb) in enumerate(jobs):
                wt = wa[ji % 2]; p = pm[ji % 2]
                s.dma(wt[:], wada_d[:, nb * 512:(nb + 1) * 512].rearrange("(kc p) n -> p kc n", p=128), writes=[("wa", ji % 2)], queue=("sp" if ji % 2 == 0 else "act"))
                for kc in range(8):
                    s.op("pe", lambda e, w=w, kc=kc, wt=wt, p=p: e.matmul(out=p[:], lhsT=lh[:, w, kc, :], rhs=wt[:, kc, :], start=(kc == 0), stop=(kc == 7)),
                         reads=["lh", ("wa", ji % 2)], writes=[("pm", ji % 2)])
                ch, half = nb // 2, nb % 2
                if w == 0:
                    dst = {0: BV_SH1, 1: BV_G1, 2: BV_GT1, 3: BV_SH2, 4: BV_G2, 5: BV_GT2}[ch]
                else:
                    dst = {0: BV_CSH1, 1: BV_CG1}[ch]
                o = bv[:, dst, half * 512:(half + 1) * 512]
                s.op("dve", lambda e, o=o, p=p, nb=nb: e.tensor_tensor(out=o, in0=p[:], in1=bb[:, nb * 512:(nb + 1) * 512], op=ALU.add),
                     reads=[("pm", ji % 2), "bb"], writes=["bv"])
            for dst, ni in ((BV_G1, 0), (BV_G2, 1), (BV_CG1, 0)):
                s.op("dve", lambda e, dst=dst, ni=ni: e.scalar_tensor_tensor(out=bv[:, dst, :], in0=bv[:, dst, :], scalar=1.0, in1=nm[:, ni, :], op0=ALU.add, op1=ALU.mult),
                     reads=["bv", "nm"], writes=["bv"])
            if "MOD_s" in dbg:
                s.dma(MOD_s.rearrange("(o a) d -> o a d", o=1), bv[0:1, 0:8, :], reads=["bv"])
            s.flush()
        if upto <= 0:
            return nc

        blocks = [(0, 512), (512, 256), (768, 512), (1280, 512), (1792, 512), (2304, 512), (2816, 16)] + [(2832 + 512 * i, 512) for i in range(4)]
        with contextlib.ExitStack() as st:
            xt = [T(st, f"xt{i}", [128, D]) for i in range(2)]
            junk = T(st, "junk", [128, D]); ss = T(st, "ss", [128, 1]); rstd = T(st, "rstd", [128, 1])
            h = T(st, "h", [128, D]); hT = T(st, "hT", [128, 8, 128])
            wb = [T(st, f"wb{i}", [128, 8, 512]) for i in range(3)]
            rp = [T(st, f"rp{i}", [128, 64]) for i in range(2)]
            qs = T(st, "qs", [128, 512]); qr = T(st, "qr", [128, 512]); tmp = T(st, "tmp", [128, 512])
            qT = T(st, "qT", [64, 8, 128]); kvs = T(st, "kvs", [128, 256]); kr = T(st, "kr", [128, 128]); kT = T(st, "kT", [64, 2, 128])
            va = T(st, "va", [128, 2, 65]); rw = T(st, "rw", [128, 512]); rT = T(st, "rT", [128, 4, 128])
            zz = T(st, "zz", [128, 512]); gn = T(st, "gn", [128, 128]); ab = T(st, "ab", [128, 16]); abc = T(st, "abc", [128, 2, 8])
            gbo = T(st, "gbo", [128, 16]); gg = T(st, "gg", [128, 512]); bg = T(st, "bg", [128, 2048])
            pT = [PS(st, f"pT{i}", [128, 512]) for i in range(2)]
            pY = [PS(st, f"pY{i}", [128, 512]) for i in range(3)]
            pX = [PS(st, f"pX{i}", [128, 512]) for i in range(2)]
            s.dma(bg[:], bgate_d.partition_broadcast(128), writes=["bg"])
            s.dma(gn[:], dnn_d.partition_broadcast(128), writes=["gn"])
            s.dma(abc[:, 0, 0:4], dtf_d.partition_broadcast(128), writes=["abc"])
            s.dma(abc[:, 0, 4:8], dtb_d.partition_broadcast(128), writes=["abc"])
            s.dma(abc[:, 1, 0:4], alf_d.partition_broadcast(128), writes=["abc"])
            s.dma(abc[:, 1, 4:8], alb_d.partition_broadcast(128), writes=["abc"])
            s.op("act", lambda e: e.activation(out=abc[:, 1, :], in_=abc[:, 1, :], func=AF.Exp), reads=["abc"], writes=["abc"])
            s.op("dve", lambda e: e.tensor_scalar(out=abc[:, 1, :], in0=abc[:, 1, :], scalar1=-1.0, scalar2=None, op0=ALU.mult), reads=["abc"], writes=["abc"])
            s.op("pool", lambda e: e.memset(va[:], 1.0), writes=["va"])
            wcount = [0]

            def rope_ops(src, dst, H):
                sv = src.rearrange("p (h a b c) -> p h a b c", h=H, a=2, b=2)
                dv = dst.rearrange("p (h a b c) -> p h a b c", h=H, a=2, b=2)
                tv = tmp[:, 0:H * 64].rearrange("p (h a b c) -> p h a b c", h=H, a=2, b=2)
                return sv, dv, tv

            tiles = [("c", i) for i in range(CTX // 128)] + [("l", i) for i in range(NT)]
            if upto == 1 and "small" in dbg:
                tiles = tiles[:4]
            for ti, (kind, i) in enumerate(tiles):
                lat = kind == "l"
                src = x_d if lat else ctx_d
                tg = ti
                X = xt[ti % 2]; xid = ("xt", ti % 2)
                s.dma(X[:], src[i * 128:(i + 1) * 128, :], writes=[xid])
                if lat:
                    R = rp[ti % 2]; rid = ("rp", ti % 2)
                    s.dma(R[:], rope_d[i * 128:(i + 1) * 128, :], writes=[rid], queue="act")
                s.op("act", lambda e, X=X: e.activation(out=junk[:], in_=X[:], func=AF.Square, accum_out=ss[:]), reads=[xid], writes=["junk", "ss"])
                s.op("dve", lambda e: e.tensor_scalar(out=rstd[:], in0=ss[:], scalar1=1.0 / D, scalar2=1e-6, op0=ALU.mult, op1=ALU.add), reads=["ss"], writes=["rstd"])
                s.op("act", lambda e: e.sqrt(out=rstd[:], in_=rstd[:]), reads=["rstd"], writes=["rstd"])
                s.op("dve", lambda e: e.reciprocal(out=rstd[:], in_=rstd[:]), reads=["rstd"], writes=["rstd"])
                G = BV_G1 if lat else BV_CG1
                SH = BV_SH1 if lat else BV_CSH1
                s.op("dve", lambda e, X=X, G=G: e.scalar_tensor_tensor(out=h[:], in0=X[:], scalar=rstd[:, 0:1], in1=bv[:, G, :], op0=ALU.mult, op1=ALU.mult), reads=[xid, "rstd", "bv"], writes=["h"])
                s.op("pool", lambda e, SH=SH: e.tensor_tensor(out=h[:], in0=h[:], in1=bv[:, SH, :], op=ALU.add), reads=["h", "bv"], writes=["h"])
                for hb in range(2):
                    for k4 in range(4):
                        kc = hb * 4 + k4
                        s.op("pe", lambda e, kc=kc, hb=hb, k4=k4: e.transpose(out=pT[hb][:, k4 * 128:(k4 + 1) * 128], in_=h[:, kc * 128:(kc + 1) * 128], identity=ident), reads=["h", "cm"], writes=[("pT", hb)])
                    eng = "act" if hb == 0 else "dve"
                    if eng == "act":
                        s.op("act", lambda e, hb=hb: e.copy(out=hT[:, hb * 4:(hb + 1) * 4, :].rearrange("p a b -> p (a b)"), in_=pT[hb][:]), reads=[("pT", hb)], writes=[("hT", hb)])
                    else:
                        s.op("dve", lambda e, hb=hb: e.tensor_copy(out=hT[:, hb * 4:(hb + 1) * 4, :].rearrange("p a b -> p (a b)"), in_=pT[hb][:]), reads=[("pT", hb)], writes=[("hT", hb)])
                need = range(11) if lat else (1, 2, 3, 4, 6)
                for bi in need:
                    c0, cw = blocks[bi]
                    wi = wcount[0] % 3; wcount[0] += 1
                    W = wb[wi]; P = pY[wi]
                    s.dma(W[:, :, 0:cw], win_d[:, c0:c0 + cw].rearrange("(kc p) n -> p kc n", p=128), writes=[("wb", wi)], queue=("sp", "act", "pool")[wi], allow_slow_non_contiguous=(cw < 128))
                    for kc in range(8):
                        s.op("pe", lambda e, kc=kc, W=W, P=P, cw=cw: e.matmul(out=P[:, 0:cw], lhsT=hT[:, kc, :], rhs=W[:, kc, 0:cw], start=(kc == 0), stop=(kc == 7)),
                             reads=[("hT", 0), ("hT", 1), ("wb", wi)], writes=[("pY", wi)])
                    pid = ("pY", wi)
                    if bi == 0:
                        s.op("act", lambda e, P=P: e.activation(out=qs[:], in_=P[:], func=AF.Copy, scale=0.125), reads=[pid], writes=["qs"])
                        _rope(s, qs[:], qr[:], tmp, R, rid, 8, "qs", "qr")
                        for hh in range(8):
                            s.op("pe", lambda e, hh=hh: e.transpose(out=pX[hh // 4][0:64, (hh % 4) * 128:(hh % 4 + 1) * 128], in_=qr[:, hh * 64:(hh + 1) * 64], identity=ident), reads=["qr", "cm"], writes=[("pX", hh // 4)])
                        s.op("act", lambda e: e.copy(out=qT[:, 0:4, :].rearrange("p a b -> p (a b)"), in_=pX[0][0:64, :]), reads=[("pX", 0)], writes=["qT"])
                        s.op("dve", lambda e: e.tensor_copy(out=qT[:, 4:8, :].rearrange("p a b -> p (a b)"), in_=pX[1][0:64, :]), reads=[("pX", 1)], writes=["qT"])
                        s.dma(QT_s[:, :, i * 128:(i + 1) * 128], qT[:], reads=["qT"], writes=["QT_s"], queue="pool")
                    elif bi == 1:
                        s.op("act", lambda e, P=P: e.copy(out=kvs[:], in_=P[:, 0:256]), reads=[pid], writes=["kvs"])
                        if lat:
                            _rope(s, kvs[:, 0:128], kr[:], tmp, R, rid, 2, "kvs", "kr")
                            ksrc, kid = kr, "kr"
                        else:
                            ksrc, kid = kvs, "kvs"
                        for hh in range(2):
                            s.op("pe", lambda e, hh=hh, ksrc=ksrc: e.transpose(out=pX[0][0:64, hh * 128:(hh + 1) * 128], in_=ksrc[:, hh * 64:(hh + 1) * 64], identity=ident), reads=[kid, "cm"], writes=[("pX", 0)])
                        s.op("act", lambda e: e.copy(out=kT[:].rearrange("p a b -> p (a b)"), in_=pX[0][0:64, 0:256]), reads=[("pX", 0)], writes=["kT"])
                        s.dma(KT_s[:, :, tg * 128:(tg + 1) * 128], kT[:], reads=["kT"], writes=["KT_s"], queue="pool")
                        s.op("pool", lambda e: e.tensor_copy(out=va[:, :, 0:64], in_=kvs[:, 128:256].rearrange("p (g d) -> p g d", g=2)), reads=["kvs"], writes=["va"])
                        s.dma(V_s[tg * 128:(tg + 1) * 128, :, :], va[:], reads=["va"], writes=["V_s"], queue="pool")
                    elif bi in (2, 3, 4):
                        s.op("act", lambda e, P=P: e.copy(out=rw[:], in_=P[:]), reads=[pid], writes=["rw"])
                        for k4 in range(4):
                            s.op("pe", lambda e, k4=k4: e.transpose(out=pX[1][:, k4 * 128:(k4 + 1) * 128], in_=rw[:, k4 * 128:(k4 + 1) * 128], identity=ident), reads=["rw", "cm"], writes=[("pX", 1)])
                        s.op("dve", lambda e: e.tensor_copy(out=rT[:].rearrange("p a b -> p (a b)"), in_=pX[1][:]), reads=[("pX", 1)], writes=["rT"])
                        f0 = (bi - 2) * 512
                        s.dma(RT_s[f0:f0 + 512, tg * 128:(tg + 1) * 128].rearrange("(a p) t -> p a t", p=128), rT[:], reads=["rT"], writes=["RT_s"], queue="pool")
                    elif bi == 5:
                        s.op("act", lambda e, P=P: e.activation(out=zz[:], in_=P[:], func=AF.Silu), reads=[pid], writes=["zz"])
                        s.op("pool", lambda e: e.tensor_tensor(out=zz[:].rearrange("p (h d) -> p h d", h=4), in0=zz[:].rearrange("p (h d) -> p h d", h=4), in1=gn[:].unsqueeze(1).to_broadcast([128, 4, 128]), op=ALU.mult), reads=["zz", "gn"], writes=["zz"])
                        s.dma(Z_s[i * 128:(i + 1) * 128, :], zz[:], reads=["zz"], writes=["Z_s"], queue="pool")
                    elif bi == 6:
                        s.op("dve", lambda e, P=P: e.tensor_tensor(out=ab[:, 0:8], in0=P[:, 0:8], in1=abc[:, 0, :], op=ALU.add), reads=[pid, "abc"], writes=["ab"])
                        s.op("act", lambda e: e.activation(out=ab[:, 0:8], in_=ab[:, 0:8], func=AF.Exp), reads=["ab"], writes=["ab"])
                        s.op("dve", lambda e: e.tensor_scalar(out=ab[:, 0:8], in0=ab[:, 0:8], scalar1=1.0, scalar2=None, op0=ALU.add), reads=["ab"], writes=["ab"])
                        s.op("act", lambda e: e.activation(out=ab[:, 0:8], in_=ab[:, 0:8], func=AF.Ln), reads=["ab"], writes=["ab"])
                        s.op("dve", lambda e: e.tensor_tensor(out=gbo[:, 0:8], in0=ab[:, 0:8], in1=abc[:, 1, :], op=ALU.mult), reads=["ab", "abc"], writes=["gbo"])
                        s.op("act", lambda e, P=P: e.activation(out=gbo[:, 8:16], in_=P[:, 8:16], func=AF.Sigmoid), reads=[pid], writes=["gbo"])
                        s.dma(GB_s[tg * 128:(tg + 1) * 128, :], gbo[:], reads=["gbo"], writes=["GB_s"], queue="pool")
                    else:
                        gi = bi - 7
                        s.op("dve", lambda e, P=P, gi=gi: e.tensor_tensor(out=gg[:], in0=P[:], in1=bg[:, gi * 512:(gi + 1) * 512], op=ALU.add), reads=[pid, "bg"], writes=["gg"])
                        s.op("act", lambda e: e.activation(out=gg[:], in_=gg[:], func=AF.Sigmoid), reads=["gg"], writes=["gg"])
                        s.dma(GT_s[i * 128:(i + 1) * 128, gi * 512:(gi + 1) * 512], gg[:], reads=["gg"], writes=["GT_s"], queue="pool")
            s.flush()
        if upto <= 1:
            return nc

        with contextlib.ExitStack() as st:
            cw = T(st, "cw", [128, 12, 5])
            Rt = [T(st, f"Rt{i}", [128, 516]) for i in range(3)]
            acc = [T(st, f"acc{i}", [128, 512]) for i in range(2)]
            y = [T(st, f"y{i}", [128, 512]) for i in range(2)]
            y2 = T(st, "y2", [128, 512]); rn = T(st, "rn", [128, 512]); yn = [T(st, f"yn{i}", [128, 512]) for i in range(2)]
            tok = [T(st, f"tok{i}", [128, 4, 128]) for i in range(2)]
            pN = [PS(st, f"pN{i}", [128, 512]) for i in range(2)]
            pK = [PS(st, f"pK{i}", [128, 512]) for i in range(2)]
            for j in range(5):
                s.dma(cw[:, :, j], conv_d[j, :].rearrange("(fc p) -> p fc", p=128), writes=["cw"], allow_slow_non_contiguous=True)
            it = 0
            segs = [(0, CTX), (CTX, TALL)]
            if "small" in dbg:
                segs = [(0, CTX), (CTX, CTX + 256)]
            for (g0, g1) in segs:
                for t0 in range(g0, g1, 512):
                    n = min(512, g1 - t0)
                    for fc in range(12):
                        R = Rt[it % 3]; rid = ("Rt", it % 3); A = acc[it % 2]; aid = ("acc", it % 2); Y = y[it % 2]; yid = ("y", it % 2)
                        lo = max(t0 - 2, g0); hi = min(t0 + n + 2, g1)
                        if lo > t0 - 2 or hi < t0 + n + 2:
                            s.op("pool", lambda e, R=R: e.memset(R[:], 0.0), writes=[rid])
                        s.dma(R[:, lo - (t0 - 2):hi - (t0 - 2)], RT_s[fc * 128:(fc + 1) * 128, lo:hi], reads=["RT_s"], writes=[rid], queue=("sp", "act")[it % 2])
                        s.op("dve", lambda e, R=R, A=A, fc=fc, n=n: e.tensor_scalar(out=A[:, 0:n], in0=R[:, 0:n], scalar1=cw[:, fc, 0:1], scalar2=None, op0=ALU.mult), reads=[rid, "cw"], writes=[aid])
                        for j in range(1, 5):
                            s.op("dve", lambda e, R=R, A=A, fc=fc, n=n, j=j: e.scalar_tensor_tensor(out=A[:, 0:n], in0=R[:, j:j + n], scalar=cw[:, fc, j:j + 1], in1=A[:, 0:n], op0=ALU.mult, op1=ALU.add), reads=[rid, "cw", aid], writes=[aid])
                        s.op("act", lambda e, A=A, Y=Y, n=n: e.activation(out=Y[:, 0:n], in_=A[:, 0:n], func=AF.Silu), reads=[aid], writes=[yid])
                        src, sid = Y, yid
                        if fc < 8:
                            YN = yn[it % 2]; nid = ("yn", it % 2); P = pN[it % 2]; pid = ("pN", it % 2)
                            s.op("act", lambda e, Y=Y, n=n: e.activation(out=y2[:, 0:n], in_=Y[:, 0:n], func=AF.Square), reads=[yid], writes=["y2"])
                            s.op("pe", lambda e, P=P, n=n: e.matmul(out=P[:, 0:n], lhsT=cm[:, C_ONES, :], rhs=y2[:, 0:n], start=True, stop=True), reads=["cm", "y2"], writes=[pid])
                            s.op("dve", lambda e, P=P, n=n: e.tensor_scalar(out=rn[:, 0:n], in0=P[:, 0:n], scalar1=1e-6, scalar2=None, op0=ALU.add), reads=[pid], writes=["rn"])
                            s.op("act", lambda e, n=n: e.sqrt(out=rn[:, 0:n], in_=rn[:, 0:n]), reads=["rn"], writes=["rn"])
                            s.op("dve", lambda e, n=n: e.reciprocal(out=rn[:, 0:n], in_=rn[:, 0:n]), reads=["rn"], writes=["rn"])
                            sc = float(128 ** -0.5) if fc < 4 else 1.0
                            s.op("dve", lambda e, Y=Y, YN=YN, n=n, sc=sc: e.scalar_tensor_tensor(out=YN[:, 0:n], in0=Y[:, 0:n], scalar=sc, in1=rn[:, 0:n], op0=ALU.mult, op1=ALU.mult), reads=[yid, "rn"], writes=[nid])
                            s.dma(QK_s[fc * 128:(fc + 1) * 128, t0:t0 + n], YN[:, 0:n], reads=[nid], writes=["QK_s"], queue="pool")
                            src, sid = YN, nid
                        if fc >= 4:
                            PK = pK[it % 2]; kid = ("pK", it % 2); TK = tok[it % 2]; tid = ("tok", it % 2)
                            nsb = n // 128
                            for sb in range(nsb):
                                s.op("pe", lambda e, PK=PK, src=src, sb=sb: e.transpose(out=PK[:, sb * 128:(sb + 1) * 128], in_=src[:, sb * 128:(sb + 1) * 128], identity=ident), reads=[sid, "cm"], writes=[kid])
                            s.op("act", lambda e, PK=PK, TK=TK, n=n: e.copy(out=TK[:].rearrange("p a b -> p (a b)")[:, 0:n], in_=PK[:, 0:n]), reads=[kid], writes=[tid])
                            s.dma(KV_s[t0:t0 + n, (fc - 4) * 128:(fc - 3) * 128].rearrange("(sb p) f -> p sb f", p=128), TK[:, 0:nsb, :], reads=[tid], writes=["KV_s"], queue="pool")
                        it += 1
            s.flush()
        if upto <= 2:
            return nc

        with contextlib.ExitStack() as st:
            Sst = [T(st, f"Sst{i}", [128, 4, 128]) for i in range(2)]
            qT4 = T(st, "qT4", [128, 4, 128]); kT4 = T(st, "kT4", [128, 4, 128]); ktok = T(st, "ktok", [128, 4, 128]); vtok = T(st, "vtok", [128, 4, 128])
            gb = T(st, "gb", [128, 16]); sm = T(st, "sm", [128, 16]); ex = T(st, "ex", [128, 16]); beg = T(st, "beg", [128, 4])
            G1 = T(st, "G1", [128, 4, 128]); dl = T(st, "dl", [128, 4, 128]); du = T(st, "du", [128, 4, 128])
            Bm = [T(st, f"Bm{i}", [128, 4, 128]) for i in range(2)]; Cm = [T(st, f"Cm{i}", [128, 4, 128]) for i in range(2)]; Pm = [T(st, f"Pm{i}", [128, 4, 128]) for i in range(2)]
            aT = T(st, "aT", [128, 4, 128]); kbg = T(st, "kbg", [128, 4, 128]); vb = T(st, "vb", [128, 4, 128]); ktl = T(st, "ktl", [128, 4, 128])
            WT = T(st, "WT", [128, 4, 128]); U = T(st, "U", [128, 4, 128]); vn = T(st, "vn", [128, 4, 128]); o1 = T(st, "o1", [128, 4, 128]); ot = T(st, "ot", [128, 4, 128])
            pb = [PS(st, f"pb{i}", [128, 4, 128]) for i in range(8)]
            pA, pB_, pC, pD, pE, pF, pG, pH = pb
            pid = lambda k: ("pb", k)
            H4 = [128, 4, 128]
            bc_h = lambda ap2: ap2.unsqueeze(1).to_broadcast(H4)
            bc_l = lambda ap2: ap2.unsqueeze(2).to_broadcast(H4)
            ntl = (2 if "small" in dbg else NT)
            for dr in range(2):
                M1 = cm[:, C_M1F if dr == 0 else C_M1B, :]; NM1 = cm[:, C_NM1F if dr == 0 else C_NM1B, :]
                NB = cm[:, C_NLOW if dr == 0 else C_NUP, :]; NTm = cm[:, C_NUP if dr == 0 else C_NLOW, :]
                STR = cm[:, C_SLOW if dr == 0 else C_SUP, :]
                SS = Sst[dr]; ssid = ("Sst", dr)
                s.op("pool", lambda e, SS=SS: e.memset(SS[:], 0.0), writes=[ssid])
                order = [("c", i) for i in range(CTX // 128)] + [("l", i) for i in range(ntl)]
                if dr == 1:
                    order = [("c", i) for i in reversed(range(CTX // 128))] + [("l", i) for i in reversed(range(ntl))]
                for (kind, i) in order:
                    lat = kind == "l"
                    tg = i if not lat else CTX // 128 + i
                    tsl = slice(tg * 128, (tg + 1) * 128)
                    s.dma(qT4[:], QK_s[0:512, tsl].rearrange("(h p) t -> p h t", p=128), reads=["QK_s"], writes=["qT4"])
                    s.dma(kT4[:], QK_s[512:1024, tsl].rearrange("(h p) t -> p h t", p=128), reads=["QK_s"], writes=["kT4"], queue="act")
                    s.dma(ktok[:].rearrange("p h d -> p (h d)"), KV_s[tsl, 0:512], reads=["KV_s"], writes=["ktok"])
                    s.dma(vtok[:].rearrange("p h d -> p (h d)"), KV_s[tsl, 512:1024], reads=["KV_s"], writes=["vtok"], queue="act")
                    s.dma(gb[:], GB_s[tsl, :], reads=["GB_s"], writes=["gb"])
                    g = gb[:, dr * 4:dr * 4 + 4]; beta = gb[:, 8 + dr * 4:12 + dr * 4]
                    pAf = pA[:].rearrange("p a b -> p (a b)")
                    for k, L in enumerate((M1, cm[:, C_SAME, :], cm[:, C_SEL0, :], cm[:, C_SEL1, :])):
                        s.op("pe", lambda e, k=k, L=L, g=g: e.matmul(out=pAf[:, 4 * k:4 * k + 4], lhsT=L, rhs=g, start=True, stop=True), reads=["cm", "gb"], writes=[pid(0)])
                    s.op("dve", lambda e: e.tensor_copy(out=sm[:], in_=pAf[:, 0:16]), reads=[pid(0)], writes=["sm"])
                    s.op("dve", lambda e: e.tensor_tensor(out=sm[:, 4:8], in0=sm[:, 4:8], in1=sm[:, 0:4], op=ALU.subtract), reads=["sm"], writes=["sm"])
                    s.op("act", lambda e: e.activation(out=ex[:], in_=sm[:], func=AF.Exp), reads=["sm"], writes=["ex"])
                    s.op("dve", lambda e, beta=beta: e.tensor_tensor(out=beg[:], in0=ex[:, 0:4], in1=beta, op=ALU.mult), reads=["ex", "gb"], writes=["beg"])
                    s.op("dve", lambda e, g=g: e.tensor_tensor(out=G1[:], in0=bc_h(cm[:, C_SAME, :]), in1=bc_l(g), op=ALU.mult), reads=["cm", "gb"], writes=["G1"])
                    for hh in range(4):
                        s.op("pe", lambda e, hh=hh, M1=M1: e.matmul(out=pB_[:, hh, :], lhsT=M1, rhs=G1[:, hh, :], start=True, stop=False), reads=["cm", "G1"], writes=[pid(1)])
                        s.op("pe", lambda e, hh=hh, NM1=NM1: e.matmul(out=pB_[:, hh, :], lhsT=G1[:, hh, :], rhs=NM1, start=False, stop=True), reads=["cm", "G1"], writes=[pid(1)])
                    s.op("dve", lambda e, NB=NB: e.tensor_tensor(out=dl[:], in0=pB_[:], in1=bc_h(NB), op=ALU.add), reads=[pid(1), "cm"], writes=["dl"])
                    s.op("dve", lambda e, NTm=NTm: e.scalar_tensor_tensor(out=du[:], in0=pB_[:], scalar=-1.0, in1=bc_h(NTm), op0=ALU.mult, op1=ALU.add), reads=[pid(1), "cm"], writes=["du"])
                    s.op("act", lambda e: e.activation(out=dl[:], in_=dl[:], func=AF.Exp), reads=["dl"], writes=["dl"])
                    s.op("act", lambda e: e.activation(out=du[:], in_=du[:], func=AF.Exp), reads=["du"], writes=["du"])
                    for hh in range(4):
                        s.op("pe", lambda e, hh=hh: e.matmul(out=pC[:, hh, :], lhsT=kT4[:, hh, :], rhs=kT4[:, hh, :], start=True, stop=True), reads=["kT4"], writes=[pid(2)])
                    for hh in range(4):
                        s.op("pe", lambda e, hh=hh: e.matmul(out=pD[:, hh, :], lhsT=kT4[:, hh, :], rhs=qT4[:, hh, :], start=True, stop=True), reads=["kT4", "qT4"], writes=[pid(3)])
                    B0 = Bm[0]; C0 = Cm[0]; P0 = Pm[0]
                    s.op("dve", lambda e: e.tensor_tensor(out=B0[:], in0=pC[:], in1=dl[:], op=ALU.mult), reads=[pid(2), "dl"], writes=[("Bm", 0)])
                    s.op("pool", lambda e, STR=STR: e.tensor_tensor(out=B0[:], in0=B0[:], in1=bc_h(STR), op=ALU.mult), reads=[("Bm", 0), "cm"], writes=[("Bm", 0)])
                    s.op("pool", lambda e, beta=beta: e.tensor_tensor(out=B0[:], in0=B0[:], in1=bc_l(beta), op=ALU.mult), reads=[("Bm", 0), "gb"], writes=[("Bm", 0)])
                    s.op("dve", lambda e: e.tensor_tensor(out=aT[:], in0=pD[:], in1=du[:], op=ALU.mult), reads=[pid(3), "du"], writes=["aT"])
                    for hh in range(4):
                        s.op("pe", lambda e, hh=hh: e.transpose(out=pE[:, hh, :], in_=B0[:, hh, :], identity=ident), reads=[("Bm", 0), "cm"], writes=[pid(4)])
                    s.op("act", lambda e: e.copy(out=C0[:], in_=pE[:]), reads=[pid(4)], writes=[("Cm", 0)])
                    s.op("dve", lambda e: e.tensor_tensor(out=P0[:], in0=C0[:], in1=bc_h(ident), op=ALU.add), reads=[("Cm", 0), "cm"], writes=[("Pm", 0)])
                    cur = 0
                    for lv in range(1, 6):
                        nx = 1 - cur
                        Bc, Cc, Pc = Bm[cur], Cm[cur], Pm[cur]; Bn, Cn, Pn = Bm[nx], Cm[nx], Pm[nx]
                        for hh in range(4):
                            s.op("pe", lambda e, hh=hh, Bc=Bc, Cc=Cc: e.matmul(out=pF[:, hh, :], lhsT=Cc[:, hh, :], rhs=Bc[:, hh, :], start=True, stop=True), reads=[("Bm", cur), ("Cm", cur)], writes=[pid(5)])
                        s.op("act", lambda e, Bn=Bn: e.copy(out=Bn[:], in_=pF[:]), reads=[pid(5)], writes=[("Bm", nx)])
                        if lv < 5:
                            for hh in range(4):
                                s.op("pe", lambda e, hh=hh, Bc=Bc, Cc=Cc: e.matmul(out=pG[:, hh, :], lhsT=Bc[:, hh, :], rhs=Cc[:, hh, :], start=True, stop=True), reads=[("Bm", cur), ("Cm", cur)], writes=[pid(6)])
                            s.op("dve", lambda e, Cn=Cn: e.tensor_copy(out=Cn[:], in_=pG[:]), reads=[pid(6)], writes=[("Cm", nx)])
                        for hh in range(4):
                            s.op("pe", lambda e, hh=hh, Pc=Pc: e.matmul(out=pH[:, hh, :], lhsT=ident, rhs=Pc[:, hh, :], start=True, stop=False), reads=[("Pm", cur), "cm"], writes=[pid(7)])
                            s.op("pe", lambda e, hh=hh, Pc=Pc, Bn=Bn: e.matmul(out=pH[:, hh, :], lhsT=Bn[:, hh, :], rhs=Pc[:, hh, :], start=False, stop=True), reads=[("Pm", cur), ("Bm", nx)], writes=[pid(7)])
                        s.op("dve", lambda e, Pn=Pn: e.tensor_copy(out=Pn[:], in_=pH[:]), reads=[pid(7)], writes=[("Pm", nx)])
                        cur = nx
                    TT = Pm[cur]; ttid = ("Pm", cur)
                    s.op("pool", lambda e: e.tensor_tensor(out=kbg[:], in0=ktok[:], in1=bc_l(beg[:]), op=ALU.mult), reads=["ktok", "beg"], writes=["kbg"])
                    s.op("pool", lambda e, beta=beta: e.tensor_tensor(out=vb[:], in0=vtok[:], in1=bc_l(beta), op=ALU.mult), reads=["vtok", "gb"], writes=["vb"])
                    s.op("pool", lambda e: e.tensor_tensor(out=ktl[:], in0=ktok[:], in1=bc_l(ex[:, 4:8]), op=ALU.mult), reads=["ktok", "ex"], writes=["ktl"])
                    for hh in range(4):
                        s.op("pe", lambda e, hh=hh, TT=TT: e.matmul(out=pE[:, hh, :], lhsT=kbg[:, hh, :], rhs=TT[:, hh, :], start=True, stop=True), reads=["kbg", ttid], writes=[pid(4)])
                    s.op("act", lambda e: e.copy(out=WT[:], in_=pE[:]), reads=[pid(4)], writes=["WT"])
                    for hh in range(4):
                        s.op("pe", lambda e, hh=hh, TT=TT: e.matmul(out=pF[:, hh, :], lhsT=TT[:, hh, :], rhs=vb[:, hh, :], start=True, stop=True), reads=["vb", ttid], writes=[pid(5)])
                    s.op("dve", lambda e: e.tensor_copy(out=U[:], in_=pF[:]), reads=[pid(5)], writes=["U"])
                    for c in ((0, 1) if dr == 0 else (1, 0)):
                        pr = slice(64 * c, 64 * c + 64)
                        for hh in range(4):
                            s.op("pe", lambda e, hh=hh, SS=SS: e.matmul(out=pG[:, hh, :], lhsT=WT[:, hh, :], rhs=SS[:, hh, :], start=True, stop=True), reads=["WT", ssid], writes=[pid(6)])
                        s.op("dve", lambda e, pr=pr: e.tensor_tensor(out=vn[pr], in0=U[pr], in1=pG[pr], op=ALU.subtract), reads=["U", pid(6)], writes=["vn"])
                        for hh in range(4):
                            s.op("pe", lambda e, hh=hh, SS=SS: e.matmul(out=pH[:, hh, :], lhsT=qT4[:, hh, :], rhs=SS[:, hh, :], start=True, stop=True), reads=["qT4", ssid], writes=[pid(7)])
                        for hh in range(4):
                            s.op("pe", lambda e, hh=hh, pr=pr: e.matmul(out=pC[:, hh, :], lhsT=aT[pr, hh, :], rhs=vn[pr, hh, :], start=True, stop=True), reads=["aT", "vn"], writes=[pid(2)])
                        for hh in range(4):
                            s.op("pe", lambda e, hh=hh, pr=pr: e.matmul(out=pD[:, hh, :], lhsT=ktl[pr, hh, :], rhs=vn[pr, hh, :], start=True, stop=True), reads=["ktl", "vn"], writes=[pid(3)])
                        if lat:
                            s.op("dve", lambda e, pr=pr: e.tensor_tensor(out=o1[pr], in0=pH[pr], in1=bc_l(ex[:, 0:4])[pr], op=ALU.mult), reads=[pid(7), "ex"], writes=["o1"])
                            s.op("dve", lambda e, pr=pr: e.tensor_tensor(out=ot[pr], in0=o1[pr], in1=pC[pr], op=ALU.add), reads=["o1", pid(2)], writes=["ot"])
                        s.op("pool", lambda e, c=c, SS=SS: e.tensor_tensor(out=SS[:], in0=SS[:], in1=bc_l(ex[:, 8 + 4 * c:12 + 4 * c]), op=ALU.mult), reads=[ssid, "ex", pid(6), pid(7)], writes=[ssid])
                        s.op("dve", lambda e, SS=SS: e.tensor_tensor(out=SS[:], in0=SS[:], in1=pD[:], op=ALU.add), reads=[ssid, pid(3)], writes=[ssid])
                    if lat:
                        s.dma(OD_s[dr, i * 128:(i + 1) * 128, :], ot[:].rearrange("p h d -> p (h d)"), reads=["ot"], writes=["OD_s"], queue="pool")
            s.flush()
        if upto <= 3:
            return nc

        with contextlib.ExitStack() as st:
            kt = [T(st, f"kt{i}", [64, 2, 384]) for i in range(2)]
            vt = [T(st, f"vt{i}", [128, 3, 130]) for i in range(2)]
            ktc = T(st, "ktc", [64, 2, 256]); vtc = T(st, "vtc", [128, 2, 130])
            qt = [T(st, f"qt{i}", [64, 8, 128]) for i in range(2)]
            E = [T(st, f"E{i}", [128, 5, 512]) for i in range(2)]
            esink = T(st, "esink", [128, 8]); den = T(st, "den", [128, 8]); oa = [T(st, f"oa{i}", [128, 512]) for i in range(2)]
            pS = [PS(st, f"pS{i}", [128, 512]) for i in range(3)]
            pO = [PS(st, f"pO{i}", [128, 4, 65]) for i in range(2)]
            s.dma(ktc[:], KT_s[:, :, 0:CTX], reads=["KT_s"], writes=["ktc"])
            s.dma(vtc[:], V_s[0:CTX].rearrange("(b p) g d -> p b (g d)", p=128), reads=["V_s"], writes=["vtc"])
            s.dma(esink[:], sink_d.partition_broadcast(128), writes=["esink"])
            s.op("act", lambda e: e.activation(out=esink[:], in_=esink[:], func=AF.Exp), reads=["esink"], writes=["esink"])
            ntl = (2 if "small" in dbg else NT)
            nS = 0
            for i in range(ntl):
                lo = max(i - 1, 0); hi = min(i + 1, ntl - 1); nb = hi - lo + 1
                KT_ = kt[i % 2]; VT_ = vt[i % 2]; QT_ = qt[i % 2]; OA = oa[i % 2]
                s.dma(KT_[:, :, 0:nb * 128], KT_s[:, :, CTX + lo * 128:CTX + (hi + 1) * 128], reads=["KT_s"], writes=[("kt", i % 2)])
                s.dma(VT_[:, 0:nb, :], V_s[CTX + lo * 128:CTX + (hi + 1) * 128].rearrange("(b p) g d -> p b (g d)", p=128), reads=["V_s"], writes=[("vt", i % 2)], queue="act")
                s.dma(QT_[:], QT_s[:, :, i * 128:(i + 1) * 128], reads=["QT_s"], writes=[("qt", i % 2)])
                for g in range(2):
                    Eg = E[g]; eid = ("E", g)
                    kb = [("l", j - lo, (C_WPREV if j < i else (C_WNEXT if j > i else None))) for j in range(lo, hi + 1)] + [("c", 0, None), ("c", 1, None)]
                    for bi, (kk, bl, msk) in enumerate(kb):
                        P = pS[nS % 3]; psid = ("pS", nS % 3); nS += 1
                        lhs = KT_[:, g, bl * 128:(bl + 1) * 128] if kk == "l" else ktc[:, g, bl * 128:(bl + 1) * 128]
                        s.op("pe", lambda e, P=P, lhs=lhs, QT_=QT_, g=g: e.matmul(out=P[:].rearrange("p (h q) -> p h q", h=4), lhsT=lhs, rhs=QT_[:, 4 * g:4 * g + 4, :], start=True, stop=True),
                             reads=[("kt", i % 2), "ktc", ("qt", i % 2)], writes=[psid])
                        s.op("act", lambda e, P=P, Eg=Eg, bi=bi: e.activation(out=Eg[:, bi, :], in_=P[:], func=AF.Exp), reads=[psid], writes=[eid])
                        if msk is not None:
                            s.op("dve", lambda e, Eg=Eg, bi=bi, msk=msk: e.tensor_tensor(out=Eg[:, bi, :].rearrange("p (h q) -> p h q", h=4), in0=Eg[:, bi, :].rearrange("p (h q) -> p h q", h=4),
                                                                              in1=cm[:, msk, :].unsqueeze(1).to_broadcast([128, 4, 128]), op=ALU.mult), reads=[eid, "cm"], writes=[eid])
                    for hh in range(4):
                        for bi, (kk, bl, msk) in enumerate(kb):
                            rhs = VT_[:, bl, g * 65:(g + 1) * 65] if kk == "l" else vtc[:, bl, g * 65:(g + 1) * 65]
                            s.op("pe", lambda e, Eg=Eg, bi=bi, hh=hh, rhs=rhs, g=g, last=(bi == len(kb) - 1): e.matmul(out=pO[g][:, hh, :], lhsT=Eg[:, bi, hh * 128:(hh + 1) * 128], rhs=rhs, start=(bi == 0), stop=last),
                                 reads=[eid, ("vt", i % 2), "vtc"], writes=[("pO", g)])
                    s.op("dve", lambda e, g=g: e.tensor_tensor(out=den[:, 4 * g:4 * g + 4], in0=pO[g][:, :, 64], in1=esink[:, 4 * g:4 * g + 4], op=ALU.add), reads=[("pO", g), "esink"], writes=["den"])
                    s.op("dve", lambda e, g=g: e.reciprocal(out=den[:, 4 * g:4 * g + 4], in_=den[:, 4 * g:4 * g + 4]), reads=["den"], writes=["den"])
                    s.op("dve", lambda e, g=g, OA=OA: e.tensor_tensor(out=OA[:, g * 256:(g + 1) * 256].rearrange("p (h d) -> p h d", h=4), in0=pO[g][:, :, 0:64],
                                                              in1=den[:, 4 * g:4 * g + 4].unsqueeze(2).to_broadcast([128, 4, 64]), op=ALU.mult), reads=[("pO", g), "den"], writes=[("oa", i % 2)])
                s.dma(OA_s[i * 128:(i + 1) * 128, :], OA[:], reads=[("oa", i % 2)], writes=["OA_s"], queue="pool")
            s.flush()
        if upto <= 5:
            return nc

        GI = 2
        NG = 128 // GI
        with contextlib.ExitStack() as st:
            xa = T(st, "xa", [128, D]); yb = T(st, "yb", [128, D]); tc_ = T(st, "tc_", [128, 8, 128]); gt = T(st, "gt", [128, 2048])
            od = T(st, "od", [128, 2, 512]); zt = T(st, "zt", [128, 512]); oat = T(st, "oat", [128, 512]); o2 = T(st, "o2", [128, 512])
            qsb = T(st, "qsb", [128, D]); qTs = T(st, "qTs", [64, 16, 128]); sc = T(st, "sc", [128, 16, 128])
            ssq = T(st, "ssq", [128, 4]); ss = T(st, "ss6", [128, 1]); rstd = T(st, "rstd6", [128, 1])
            t16 = T(st, "t16", [128, 2, 16]); c16 = T(st, "c16", [128, 16]); cand = T(st, "cand", [128, 16, 16]); wk = T(st, "wk", [128, 256])
            thr = T(st, "thr", [128, 8]); negm = T(st, "negm", [128, 8]); Zs = T(st, "Zs", [128, 8]); kap = T(st, "kap", [128, 8]); e16 = T(st, "e16", [128, 16])
            keysT = T(st, "keysT", [64, 16, 128])
            wS = T(st, "wS", [128, 8, D])
            UT = [T(st, f"UT{i}", [128, 8, GI * 128]) for i in range(2)]
            VG = [T(st, f"VG{i}", [128, GI, D]) for i in range(2)]
            sm_ = T(st, "sm_", [128, 8, GI, 128]); ee = T(st, "ee", [128, 8, GI, 128]); gd = T(st, "gd", [128, GI * 128])
            ga = T(st, "ga", [128, GI * 128]); gb_ = T(st, "gb_", [128, GI * 128]); Pm_ = T(st, "Pm_", [128, GI * 128]); PT = [T(st, f"PT{i}", [128, GI, 128]) for i in range(2)]
            pT = [PS(st, f"pT{i}", [128, 512]) for i in range(2)]
            pY = [PS(st, f"pY{i}", [128, 512]) for i in range(2)]
            pR = PS(st, "pR", [128, 512]); pW = PS(st, "pW", [128, 512]); pU = [PS(st, f"pU{i}", [128, 512]) for i in range(2)]
            s.dma(keysT[:], pkeys_d.rearrange("h p d k -> d (h p) k"), writes=["keysT"])

            def transpose8(src, sid, nkc, dst_off=0):
                for kc in range(nkc):
                    b = (dst_off + kc) // 4
                    s.op("pe", lambda e, kc=kc, b=b: e.transpose(out=pT[b][:, ((dst_off + kc) % 4) * 128:((dst_off + kc) % 4 + 1) * 128], in_=src[:, kc * 128:(kc + 1) * 128], identity=ident), reads=[sid, "cm"], writes=[("pT", b)])
                for b in sorted(set((dst_off + kc) // 4 for kc in range(nkc))):
                    eng = "act" if b == 0 else "dve"
                    f = (lambda e, b=b: e.copy(out=tc_[:, b * 4:(b + 1) * 4, :].rearrange("p a b -> p (a b)"), in_=pT[b][:])) if eng == "act" else (lambda e, b=b: e.tensor_copy(out=tc_[:, b * 4:(b + 1) * 4, :].rearrange("p a b -> p (a b)"), in_=pT[b][:]))
                    s.op(eng, f, reads=[("pT", b)], writes=[("tc", b)])

            def rms(src, sid):
                s.op("act", lambda e: e.activation(out=qsb[:], in_=src[:], func=AF.Square, accum_out=ss[:]), reads=[sid], writes=["qsb", "ss6"])
                s.op("dve", lambda e: e.tensor_scalar(out=rstd[:], in0=ss[:], scalar1=1.0 / D, scalar2=1e-6, op0=ALU.mult, op1=ALU.add), reads=["ss6"], writes=["rstd6"])
                s.op("act", lambda e: e.sqrt(out=rstd[:], in_=rstd[:]), reads=["rstd6"], writes=["rstd6"])
                s.op("dve", lambda e: e.reciprocal(out=rstd[:], in_=rstd[:]), reads=["rstd6"], writes=["rstd6"])

            ntl = (1 if "small" in dbg else NT)
            ngr = (2 if "small2" in dbg else NG)
            gcount = 0
            for i in range(ntl):
                tsl = slice(i * 128, (i + 1) * 128)
                s.dma(xa[:], x_d[tsl, :], writes=["xa"])
                s.dma(od[:, 0, :], OD_s[0, tsl, :], reads=["OD_s"], writes=["od"], queue="act")
                s.dma(od[:, 1, :], OD_s[1, tsl, :], reads=["OD_s"], writes=["od"], queue="act")
                s.dma(zt[:], Z_s[tsl, :], reads=["Z_s"], writes=["zt"])
                s.dma(oat[:], OA_s[tsl, :], reads=["OA_s"], writes=["oat"], queue="act")
                s.dma(gt[:], GT_s[tsl, :], reads=["GT_s"], writes=["gt"])
                s.dma(wS[:, 0:4, :], wba_d.rearrange("(kc p) n -> p kc n", p=128), writes=["wS"])
                s.dma(wS[:, 4:8, :], wbd_d.rearrange("(kc p) n -> p kc n", p=128), writes=["wS"], queue="act")
                s.op("dve", lambda e: e.tensor_tensor(out=od[:, 0, :], in0=od[:, 0, :], in1=od[:, 1, :], op=ALU.add), reads=["od"], writes=["od"])
                s.op("dve", lambda e: e.tensor_tensor(out=o2[:], in0=od[:, 0, :], in1=od[:, 0, :], op=ALU.mult), reads=["od"], writes=["o2"])
                s.op("dve", lambda e: e.tensor_reduce(out=ssq[:], in_=o2[:].rearrange("p (h d) -> p h d", h=4), axis=AX.X, op=ALU.add), reads=["o2"], writes=["ssq"])
                s.op("dve", lambda e: e.tensor_scalar(out=ssq[:], in0=ssq[:], scalar1=1.0 / 128, scalar2=1e-6, op0=ALU.mult, op1=ALU.add), reads=["ssq"], writes=["ssq"])
                s.op("act", lambda e: e.sqrt(out=ssq[:], in_=ssq[:]), reads=["ssq"], writes=["ssq"])
                s.op("dve", lambda e: e.reciprocal(out=ssq[:], in_=ssq[:]), reads=["ssq"], writes=["ssq"])
                s.op("dve", lambda e: e.tensor_tensor(out=o2[:].rearrange("p (h d) -> p h d", h=4), in0=od[:, 0, :].rearrange("p (h d) -> p h d", h=4), in1=ssq[:].unsqueeze(2).to_broadcast([128, 4, 128]), op=ALU.mult), reads=["od", "ssq"], writes=["o2"])
                s.op("dve", lambda e: e.tensor_tensor(out=o2[:], in0=o2[:], in1=zt[:], op=ALU.mult), reads=["o2", "zt"], writes=["o2"])
                transpose8(oat, "oat", 4, 0)
                transpose8(o2, "o2", 4, 4)
                for half in range(2):
                    for kc in range(4):
                        s.op("pe", lambda e, half=half, kc=kc: e.matmul(out=pY[half][:], lhsT=tc_[:, kc, :], rhs=wS[:, kc, half * 512:(half + 1) * 512], start=(kc == 0), stop=(kc == 3)), reads=[("tc", 0), "wS"], writes=[("pY", half)])
                    s.op("dve", lambda e, half=half: e.tensor_tensor(out=yb[:, half * 512:(half + 1) * 512], in0=pY[half][:], in1=gt[:, half * 512:(half + 1) * 512], op=ALU.mult), reads=[("pY", half), "gt"], writes=["yb"])
                for half in range(2):
                    for kc in range(4):
                        s.op("pe", lambda e, half=half, kc=kc: e.matmul(out=pY[half][:], lhsT=tc_[:, 4 + kc, :], rhs=wS[:, 4 + kc, half * 512:(half + 1) * 512], start=(kc == 0), stop=(kc == 3)), reads=[("tc", 1), "wS"], writes=[("pY", half)])
                    s.op("dve", lambda e, half=half: e.tensor_tensor(out=qsb[:, half * 512:(half + 1) * 512], in0=pY[half][:], in1=gt[:, 1024 + half * 512:1024 + (half + 1) * 512], op=ALU.mult), reads=[("pY", half), "gt"], writes=["qsb"])
                s.op("pool", lambda e: e.tensor_tensor(out=yb[:], in0=yb[:], in1=qsb[:], op=ALU.add), reads=["yb", "qsb"], writes=["yb"])
                s.dma(wS[:], wout_d.rearrange("(kc p) n -> p kc n", p=128), writes=["wS"])
                transpose8(yb, "yb", 8, 0)
                for half in range(2):
                    for kc in range(8):
                        s.op("pe", lambda e, half=half, kc=kc: e.matmul(out=pY[half][:], lhsT=tc_[:, kc, :], rhs=wS[:, kc, half * 512:(half + 1) * 512], start=(kc == 0), stop=(kc == 7)), reads=[("tc", 0), ("tc", 1), "wS"], writes=[("pY", half)])
                    s.op("dve", lambda e, half=half: e.tensor_tensor(out=yb[:, half * 512:(half + 1) * 512], in0=pY[half][:], in1=bv[:, BV_GT1, half * 512:(half + 1) * 512], op=ALU.mult), reads=[("pY", half), "bv"], writes=["yb"])
                s.op("pool", lambda e: e.tensor_tensor(out=xa[:], in0=xa[:], in1=yb[:], op=ALU.add), reads=["xa", "yb"], writes=["xa"])
                s.dma(wS[:], pwq_d.rearrange("(kc p) n -> p kc n", p=128), writes=["wS"])
                rms(xa, "xa")
                s.op("dve", lambda e: e.scalar_tensor_tensor(out=yb[:], in0=xa[:], scalar=rstd[:, 0:1], in1=bv[:, BV_G2, :], op0=ALU.mult, op1=ALU.mult), reads=["xa", "rstd6", "bv"], writes=["yb"])
                s.op("pool", lambda e: e.tensor_tensor(out=yb[:], in0=yb[:], in1=bv[:, BV_SH2, :], op=ALU.add), reads=["yb", "bv"], writes=["yb"])
                transpose8(yb, "yb", 8, 0)
                for half in range(2):
                    for kc in range(8):
                        s.op("pe", lambda e, half=half, kc=kc: e.matmul(out=pY[half][:], lhsT=tc_[:, kc, :], rhs=wS[:, kc, half * 512:(half + 1) * 512], start=(kc == 0), stop=(kc == 7)), reads=[("tc", 0), ("tc", 1), "wS"], writes=[("pY", half)])
                    if half == 0:
                        s.op("act", lambda e: e.copy(out=qsb[:, 0:512], in_=pY[0][:]), reads=[("pY", 0)], writes=["qsb"])
                    else:
                        s.op("dve", lambda e: e.tensor_copy(out=qsb[:, 512:1024], in_=pY[1][:]), reads=[("pY", 1)], writes=["qsb"])
                for rd in range(4):
                    b = rd % 2
                    for k4 in range(4):
                        hp = rd * 4 + k4
                        s.op("pe", lambda e, hp=hp, b=b, k4=k4: e.transpose(out=pT[b][0:64, k4 * 128:(k4 + 1) * 128], in_=qsb[:, hp * 64:(hp + 1) * 64], identity=ident), reads=["qsb", "cm"], writes=[("pT", b)])
                    if b == 0:
                        s.op("act", lambda e, rd=rd, b=b: e.copy(out=qTs[:, rd * 4:(rd + 1) * 4, :].rearrange("p a b -> p (a b)"), in_=pT[b][0:64, :]), reads=[("pT", b)], writes=["qTs"])
                    else:
                        s.op("dve", lambda e, rd=rd, b=b: e.tensor_copy(out=qTs[:, rd * 4:(rd + 1) * 4, :].rearrange("p a b -> p (a b)"), in_=pT[b][0:64, :]), reads=[("pT", b)], writes=["qTs"])
                for rd in range(4):
                    b = rd % 2
                    for k4 in range(4):
                        hp = rd * 4 + k4
                        s.op("pe", lambda e, hp=hp, b=b, k4=k4: e.matmul(out=pY[b][:, k4 * 128:(k4 + 1) * 128], lhsT=qTs[:, hp, :], rhs=keysT[:, hp, :], start=True, stop=True), reads=["qTs", "keysT"], writes=[("pY", b)])
                    if b == 0:
                        s.op("act", lambda e, rd=rd, b=b: e.copy(out=sc[:, rd * 4:(rd + 1) * 4, :].rearrange("p a b -> p (a b)"), in_=pY[b][:]), reads=[("pY", b)], writes=["sc"])
                    else:
                        s.op("dve", lambda e, rd=rd, b=b: e.tensor_copy(out=sc[:, rd * 4:(rd + 1) * 4, :].rearrange("p a b -> p (a b)"), in_=pY[b][:]), reads=[("pY", b)], writes=["sc"])
                for hh in range(8):
                    for p in range(2):
                        srow = sc[:, 2 * hh + p, :]
                        s.op("dve", lambda e, p=p, srow=srow: e.max(out=t16[:, p, 0:8], in_=srow), reads=["sc"], writes=["t16"])
                        s.op("dve", lambda e, p=p, srow=srow: e.match_replace(out=wk[:, 0:128], in_to_replace=t16[:, p, 0:8], in_values=srow, imm_value=-1e30), reads=["sc", "t16"], writes=["wk"])
                        s.op("dve", lambda e, p=p: e.max(out=t16[:, p, 8:16], in_=wk[:, 0:128]), reads=["wk"], writes=["t16"])
                    s.op("dve", lambda e: e.tensor_tensor(out=cand[:], in0=t16[:, 0, :].unsqueeze(2).to_broadcast([128, 16, 16]), in1=t16[:, 1, :].unsqueeze(1).to_broadcast([128, 16, 16]), op=ALU.add), reads=["t16"], writes=["cand"])
                    cf = cand[:].rearrange("p a b -> p (a b)")
                    s.op("dve", lambda e, cf=cf: e.max(out=c16[:, 0:8], in_=cf), reads=["cand"], writes=["c16"])
                    s.op("dve", lambda e, cf=cf: e.match_replace(out=wk[:], in_to_replace=c16[:, 0:8], in_values=cf, imm_value=-1e30), reads=["cand", "c16"], writes=["wk"])
                    s.op("dve", lambda e: e.max(out=c16[:, 8:16], in_=wk[:]), reads=["wk"], writes=["c16"])
                    s.op("dve", lambda e, hh=hh: e.tensor_copy(out=thr[:, hh:hh + 1], in_=c16[:, 15:16]), reads=["c16"], writes=["thr"])
                    s.op("dve", lambda e, hh=hh: e.tensor_scalar(out=negm[:, hh:hh + 1], in0=c16[:, 0:1], scalar1=-1.0, scalar2=None, op0=ALU.mult), reads=["c16"], writes=["negm"])
                    s.op("act", lambda e, hh=hh: e.activation(out=e16[:], in_=c16[:], func=AF.Exp, bias=negm[:, hh:hh + 1], accum_out=Zs[:, hh:hh + 1]), reads=["c16", "negm"], writes=["e16", "Zs"])
                s.op("dve", lambda e: e.tensor_tensor(out=kap[:], in0=thr[:], in1=negm[:], op=ALU.add), reads=["thr", "negm"], writes=["kap"])
                s.op("act", lambda e: e.activation(out=kap[:], in_=kap[:], func=AF.Exp), reads=["kap"], writes=["kap"])
                s.op("dve", lambda e: e.reciprocal(out=Zs[:], in_=Zs[:]), reads=["Zs"], writes=["Zs"])
                s.op("dve", lambda e: e.tensor_tensor(out=kap[:], in0=kap[:], in1=Zs[:], op=ALU.mult), reads=["kap", "Zs"], writes=["kap"])
                sc4 = sc[:].rearrange("p (h q) k -> p h q k", q=2)
                s.op("dve", lambda e: e.tensor_tensor(out=sc4[:, :, 1, :], in0=sc4[:, :, 1, :], in1=thr[:].unsqueeze(2).to_broadcast([128, 8, 128]), op=ALU.subtract), reads=["sc", "thr"], writes=["sc"])
                for g in range(ngr):
                    u = gcount % 2; gcount += 1
                    e0 = g * GI * 128
                    s.dma(UT[u][:], pu_d[:, e0:e0 + GI * 128].rearrange("(kc p) n -> p kc n", p=128), writes=[("UT", u)], queue=("sp", "act")[u])
                    s.dma(VG[u][:], pv_d[e0:e0 + GI * 128, :].rearrange("(a p) n -> p a n", p=128), writes=[("VG", u)], queue=("act", "sp")[u])
                    for kc in range(8):
                        s.op("pe", lambda e, kc=kc, u=u: e.matmul(out=pR[:, 0:GI * 128], lhsT=tc_[:, kc, :], rhs=UT[u][:, kc, :], start=(kc == 0), stop=(kc == 7)), reads=[("tc", 0), ("tc", 1), ("UT", u)], writes=["pR_"])
                    s1b = sc4[:, :, 0, g * GI:(g + 1) * GI].unsqueeze(3).to_broadcast([128, 8, GI, 128])
                    s2b = sc4[:, :, 1, :].unsqueeze(2).to_broadcast([128, 8, GI, 128])
                    s.op("pool", lambda e, s1b=s1b, s2b=s2b: e.tensor_tensor(out=sm_[:], in0=s1b, in1=s2b, op=ALU.add), reads=["sc"], writes=["sm_"])
                    s.op("act", lambda e: e.activation(out=ee[:], in_=sm_[:], func=AF.Exp), reads=["sm_"], writes=["ee"])
                    s.op("dve", lambda e: e.scalar_tensor_tensor(out=ee[:], in0=sm_[:], scalar=0.0, in1=ee[:], op0=ALU.is_ge, op1=ALU.mult), reads=["sm_", "ee"], writes=["ee"])
                    s.op("pool", lambda e: e.tensor_tensor(out=ee[:].rearrange("p h a k -> p h (a k)"), in0=ee[:].rearrange("p h a k -> p h (a k)"), in1=kap[:].unsqueeze(2).to_broadcast([128, 8, GI * 128]), op=ALU.mult), reads=["ee", "kap"], writes=["ee"])
                    s.op("dve", lambda e: e.tensor_reduce(out=gd[:], in_=ee[:].rearrange("p h a k -> p (a k) h"), axis=AX.X, op=ALU.add), reads=["ee"], writes=["gd"])
                    s.op("act", lambda e: e.activation(out=ga[:], in_=pR[:, 0:GI * 128], func=AF.Square), reads=["pR_"], writes=["ga", "pR_"])
                    s.op("dve", lambda e: e.tensor_scalar(out=ga[:], in0=ga[:], scalar1=0.044715, scalar2=1.0, op0=ALU.mult, op1=ALU.add), reads=["ga"], writes=["ga"])
                    s.op("dve", lambda e: e.tensor_tensor(out=ga[:], in0=ga[:], in1=pR[:, 0:GI * 128], op=ALU.mult), reads=["ga", "pR_"], writes=["ga", "pR_"])
                    s.op("act", lambda e: e.activation(out=ga[:], in_=ga[:], func=AF.Sigmoid, scale=1.5957691216057308), reads=["ga"], writes=["ga"])
                    s.op("dve", lambda e: e.tensor_tensor(out=gb_[:], in0=pR[:, 0:GI * 128], in1=gd[:], op=ALU.mult), reads=["pR_", "gd"], writes=["gb_", "pR_"])
                    s.op("pool", lambda e: e.tensor_tensor(out=Pm_[:], in0=gb_[:], in1=ga[:], op=ALU.mult), reads=["gb_", "ga"], writes=["Pm_"])
                    for a in range(GI):
                        s.op("pe", lambda e, a=a: e.transpose(out=pW[:, a * 128:(a + 1) * 128], in_=Pm_[:, a * 128:(a + 1) * 128], identity=ident), reads=["Pm_", "cm"], writes=["pW_"])
                    s.op("act", lambda e, u=u: e.copy(out=PT[u][:].rearrange("p a b -> p (a b)"), in_=pW[:, 0:GI * 128]), reads=["pW_"], writes=[("PT", u), "pW_"])
                    for a in range(GI):
                        for half in range(2):
                            s.op("pe", lambda e, a=a, half=half, u=u, first=(g == 0 and a == 0), last=(g == ngr - 1 and a == GI - 1): e.matmul(out=pU[half][:], lhsT=PT[u][:, a, :], rhs=VG[u][:, a, half * 512:(half + 1) * 512], start=first, stop=last),
                                 reads=[("PT", u), ("VG", u)], writes=[("pU", half)])
                for half in range(2):
                    s.op("dve", lambda e, half=half: e.tensor_tensor(out=yb[:, half * 512:(half + 1) * 512], in0=pU[half][:], in1=bv[:, BV_GT2, half * 512:(half + 1) * 512], op=ALU.mult), reads=[("pU", half), "bv"], writes=["yb"])
                s.op("pool", lambda e: e.tensor_tensor(out=xa[:], in0=xa[:], in1=yb[:], op=ALU.add), reads=["xa", "yb"], writes=["xa"])
                rms(xa, "xa")
                s.op("dve", lambda e: e.scalar_tensor_tensor(out=yb[:], in0=xa[:], scalar=rstd[:, 0:1], in1=bv[:, BV_FN, :], op0=ALU.mult, op1=ALU.mult), reads=["xa", "rstd6", "bv"], writes=["yb"])
                s.dma(out_d[tsl, :], yb[:], reads=["yb"], writes=["out"], queue="pool")
            s.flush()
        return nc


def _rope(s, src, dst, tmp, R, rid, H, sid, did):
    sv = src.rearrange("p (h a b c) -> p h a b c", h=H, a=2, b=2)
    dv = dst.rearrange("p (h a b c) -> p h a b c", h=H, a=2, b=2)
    tv = tmp[:, 0:H * 32].rearrange("p (h a c) -> p h a c", h=H, a=2)
    rv = R[:].rearrange("p (a b c) -> p a b c", a=2, b=2)
    cosb = rv[:, :, 0, :].unsqueeze(1).to_broadcast([128, H, 2, 16])
    sinb = rv[:, :, 1, :].unsqueeze(1).to_broadcast([128, H, 2, 16])
    x1 = sv[:, :, :, 0, :]; x2 = sv[:, :, :, 1, :]
    o1 = dv[:, :, :, 0, :]; o2 = dv[:, :, :, 1, :]
    s.op("dve", lambda e: e.tensor_tensor(out=o1, in0=x1, in1=cosb, op=ALU.mult), reads=[sid, rid], writes=[did])
    s.op("dve", lambda e: e.tensor_tensor(out=tv, in0=x2, in1=sinb, op=ALU.mult), reads=[sid, rid], writes=["tmp"])
    s.op("dve", lambda e: e.tensor_tensor(out=o1, in0=o1, in1=tv, op=ALU.subtract), reads=[did, "tmp"], writes=[did])
    s.op("dve", lambda e: e.tensor_tensor(out=o2, in0=x1, in1=sinb, op=ALU.mult), reads=[sid, rid, did], writes=[did])
    s.op("dve", lambda e: e.tensor_tensor(out=tv, in0=x2, in1=cosb, op=ALU.mult), reads=[sid, rid, did], writes=["tmp"])
    s.op("dve", lambda e: e.tensor_tensor(out=o2, in0=o2, in1=tv, op=ALU.add), reads=[did, "tmp"], writes=[did])


def _host_inputs(inputs, b, consts):
    g = lambda k: np.ascontiguousarray(inputs[k], dtype=np.float32)
    m = {
        "x": g("x")[b], "c": g("c")[b], "ctx": g("ctx")[b], "c_ctx": g("c_ctx"),
        "w_ada": g("w_ada")[0], "b_ada": g("b_ada")[0], "norm_mix": g("norm_mix")[0], "norm_ffn": g("norm_ffn")[0],
        "w_in": g("w_in")[0], "b_gate": g("b_gate")[0], "attn_sink": g("attn_sink")[0], "dn_conv": g("dn_conv")[0],
        "dn_a_log_f": g("dn_a_log_f")[0], "dn_dt_bias_f": g("dn_dt_bias_f")[0], "dn_a_log_b": g("dn_a_log_b")[0], "dn_dt_bias_b": g("dn_dt_bias_b")[0],
        "dn_norm": g("dn_norm")[0], "w_br_attn": g("w_br_attn")[0], "w_br_dn": g("w_br_dn")[0], "w_out": g("w_out")[0],
        "peer_wq": g("peer_wq")[0], "final_norm": g("final_norm"),
    }
    m.update(consts)
    return {k: np.ascontiguousarray(v) for k, v in m.items()}


_SHARED = {}


def kernel(**inputs):
    consts = _consts()
    nc = build()
    keysT = np.ascontiguousarray(np.transpose(np.asarray(inputs["peer_keys"], np.float32)[0], (0, 1, 3, 2)))
    uT = np.ascontiguousarray(np.asarray(inputs["peer_u"], np.float32)[0].T)
    pv = np.ascontiguousarray(np.asarray(inputs["peer_v"], np.float32)[0])
    in_maps = []
    for b in range(8):
        m = _host_inputs(inputs, b, consts)
        m["peer_keysT"] = keysT; m["peer_uT"] = uT; m["peer_v"] = pv
        in_maps.append(m)
    res = run_bass_kernel_spmd(nc, in_maps, core_ids=list(range(8)))
    return np.stack([np.asarray(r["out"], dtype=np.float32) for r in res.results], axis=0)
```

```python
import contextlib
import numpy as np
import concourse.bass as bass
import concourse.mybir as mybir
from concourse.bass_utils import run_bass_kernel_spmd

F32 = mybir.dt.float32
ALU = mybir.AluOpType
AF = mybir.ActivationFunctionType
AX = mybir.AxisListType

D = 1024
S = 8192
CTX = 256
TALL = CTX + S
NT = S // 128
IN_COLS = 4880
NEG = -30000.0


class _Ins:
    __slots__ = ("eng", "fn", "deps", "signal", "sig_no", "dma", "idx")

    def __init__(self, eng, fn, dma=None):
        self.eng = eng
        self.fn = fn
        self.deps = []
        self.signal = False
        self.sig_no = None
        self.dma = dma
        self.idx = None


class Sch:
    EPOCH = 20000
    NDMA = 24
    NEP = 6

    def __init__(self, nc, st):
        self.nc = nc
        self.engs = ("pe", "act", "dve", "pool", "sp")
        self.sems = {e: [st.enter_context(nc.semaphore(f"s_{e}_{i}")) for i in range(self.NEP)] for e in self.engs}
        self.dsems = [st.enter_context(nc.semaphore(f"s_dma_{i}")) for i in range(self.NDMA)]
        self.sigc = {e: 0 for e in self.engs}
        self.dma_rr = 0
        self.dma_cnt = [0] * self.NDMA
        self.dma_last = [None] * self.NDMA
        self._reset()

    def _reset(self):
        self.q = {e: [] for e in self.engs}
        self.lastw = {}
        self.readers = {}

    def _add(self, ins, reads, writes):
        q = self.q[ins.eng]
        ins.idx = len(q)
        deps = []
        for r in reads:
            w = self.lastw.get(r)
            if w is not None:
                deps.append((w, "raw"))
        for w_ in writes:
            w = self.lastw.get(w_)
            if w is not None:
                deps.append((w, "waw"))
            for rd in self.readers.get(w_, ()):
                deps.append((rd, "war"))
        for d, kind in deps:
            if d is ins:
                continue
            if d.dma is None and ins.dma is None and d.eng == ins.eng:
                if ins.eng == "pe":
                    continue
                if kind != "raw":
                    continue
            ins.deps.append(d)
            if d.dma is None:
                d.signal = True
        for r in reads:
            self.readers.setdefault(r, []).append(ins)
        for w_ in writes:
            self.lastw[w_] = ins
            self.readers[w_] = []
        q.append(ins)
        return ins

    PSUM_NAMES = {"pm", "pT", "pY", "pX", "pN", "pK", "pb", "pS", "pO", "pQ", "pZ", "pR", "pU", "pW"}

    def op(self, eng, fn, reads=(), writes=()):
        writes = list(writes)
        if eng != "pe":
            for r in reads:
                if isinstance(r, tuple) and r[0] in self.PSUM_NAMES and r not in writes:
                    writes.append(r)
        return self._add(_Ins(eng, fn), list(reads), writes)

    def dma(self, out, in_, reads=(), writes=(), queue="sp", **kw):
        slot = self.dma_rr
        self.dma_rr = (self.dma_rr + 1) % self.NDMA
        self.dma_cnt[slot] += 1
        n = self.dma_cnt[slot]
        ins = _Ins(queue, lambda e: e.dma_start(out=out, in_=in_, **kw), dma=(slot, n))
        prev = self.dma_last[slot]
        self._add(ins, list(reads), list(writes))
        if prev is not None:
            ins.deps.append(prev)
        self.dma_last[slot] = ins
        return ins

    def flush(self):
        nc = self.nc
        for e, q in self.q.items():
            for ins in q:
                if ins.dma is None and ins.signal:
                    ins.sig_no = self.sigc[e]
                    self.sigc[e] += 1
            assert self.sigc[e] < self.EPOCH * self.NEP, "too many signals"
        dma_final = list(self.dma_cnt)
        with nc.Block() as block:
            def run(ename):
                def body(eng):
                    seen_c = {}
                    seen_d = {}
                    for ins in self.q[ename]:
                        wc = {}
                        wd = {}
                        for d in ins.deps:
                            if d.dma is None:
                                if d.sig_no is None:
                                    continue
                                if seen_c.get(d.eng, -1) < d.sig_no:
                                    wc[d.eng] = max(wc.get(d.eng, -1), d.sig_no)
                            else:
                                s_, n = d.dma
                                if seen_d.get(s_, 0) < n:
                                    wd[s_] = max(wd.get(s_, 0), n)
                        for e2, sn in wc.items():
                            eng.wait_ge(self.sems[e2][sn // self.EPOCH], sn % self.EPOCH + 1)
                            seen_c[e2] = sn
                        for s_, n in wd.items():
                            eng.wait_ge(self.dsems[s_], 16 * n)
                            seen_d[s_] = n
                        h = ins.fn(eng)
                        if ins.dma is not None:
                            h.then_inc(self.dsems[ins.dma[0]], 16)
                        elif ins.signal:
                            h.then_inc(self.sems[ename][ins.sig_no // self.EPOCH], 1)
                    if ename == "sp":
                        for s_, n in enumerate(dma_final):
                            if n > 0:
                                eng.wait_ge(self.dsems[s_], 16 * n)
                return body

            block.sync(run("sp"))
            block.tensor(run("pe"))
            block.scalar(run("act"))
            block.vector(run("dve"))
            block.gpsimd(run("pool"))
        nc.all_engine_barrier()
        self._reset()


def _consts():
    c = {}
    ident = np.eye(128, dtype=np.float32)
    ones = np.ones((128, 128), np.float32)
    idx = np.arange(128)
    same = (idx[:, None] // 64 == idx[None, :] // 64).astype(np.float32)
    m1f = ((idx[:, None] <= idx[None, :]) * same).astype(np.float32)
    m1b = ((idx[:, None] >= idx[None, :]) * same).astype(np.float32)
    sel0 = np.zeros((128, 128), np.float32); sel0[:64, :] = 1
    sel1 = np.zeros((128, 128), np.float32); sel1[64:, :] = 1
    low_incl = ((idx[None, :] <= idx[:, None]) * same)
    up_incl = ((idx[None, :] >= idx[:, None]) * same)
    low_strict = ((idx[None, :] < idx[:, None]) * same)
    up_strict = ((idx[None, :] > idx[:, None]) * same)
    negmask = lambda m: np.where(m > 0, 0.0, NEG).astype(np.float32)
    w_prev = (idx[None, :] <= idx[:, None]).astype(np.float32)
    w_next = (idx[:, None] <= idx[None, :]).astype(np.float32)
    mats = [ident, ones, same, m1f, -m1f, m1b, -m1b, sel0, sel1,
            negmask(low_incl), negmask(up_incl), -low_strict.astype(np.float32), -up_strict.astype(np.float32),
            w_prev, w_next]
    c["cmat"] = np.ascontiguousarray(np.stack(mats, axis=1)).astype(np.float32)
    pos = np.arange(S)
    inv = (10000.0 ** (-np.arange(16, dtype=np.float32) / 16)).astype(np.float32)
    ar = (pos // 64).astype(np.float32)[:, None] * inv[None, :]
    ac = (pos % 64).astype(np.float32)[:, None] * inv[None, :]
    c["rope"] = np.concatenate([np.cos(ar), np.sin(ar), np.cos(ac), np.sin(ac)], axis=1).astype(np.float32)
    return c

(C_ID, C_ONES, C_SAME, C_M1F, C_NM1F, C_M1B, C_NM1B, C_SEL0, C_SEL1, C_NLOW, C_NUP, C_SLOW, C_SUP, C_WPREV, C_WNEXT) = range(15)


def build(upto=99, dbg=()):
    nc = bass.Bass("TRN2", target_bir_lowering=False)
    inp = lambda name, shape: nc.dram_tensor(name, list(shape), F32, kind="ExternalInput").ap()
    x_d = inp("x", [S, D]); c_d = inp("c", [D]); ctx_d = inp("ctx", [CTX, D]); cctx_d = inp("c_ctx", [D])
    wada_d = inp("w_ada", [D, 6 * D]); bada_d = inp("b_ada", [6 * D])
    nmix_d = inp("norm_mix", [D]); nffn_d = inp("norm_ffn", [D])
    win_d = inp("w_in", [D, IN_COLS]); bgate_d = inp("b_gate", [2 * D])
    sink_d = inp("attn_sink", [8]); conv_d = inp("dn_conv", [5, 1536])
    alf_d = inp("dn_a_log_f", [4]); dtf_d = inp("dn_dt_bias_f", [4]); alb_d = inp("dn_a_log_b", [4]); dtb_d = inp("dn_dt_bias_b", [4])
    dnn_d = inp("dn_norm", [128]); wba_d = inp("w_br_attn", [512, D]); wbd_d = inp("w_br_dn", [512, D]); wout_d = inp("w_out", [D, D])
    pwq_d = inp("peer_wq", [D, D]); pkeys_d = inp("peer_keysT", [8, 2, 64, 128]); pu_d = inp("peer_uT", [D, 16384]); pv_d = inp("peer_v", [16384, D])
    fnorm_d = inp("final_norm", [D]); cmat_d = inp("cmat", [128, 15, 128]); rope_d = inp("rope", [S, 64])
    out_d = nc.dram_tensor("out", [S, D], F32, kind="ExternalOutput").ap()
    scr = lambda name, shape: nc.dram_tensor(name, list(shape), F32, kind=("ExternalOutput" if name in dbg else "Internal")).ap()
    QT_s = scr("QT_s", [64, 8, S])
    KT_s = scr("KT_s", [64, 2, TALL])
    V_s = scr("V_s", [TALL, 2, 65])
    RT_s = scr("RT_s", [1536, TALL])
    Z_s = scr("Z_s", [S, 512])
    GB_s = scr("GB_s", [TALL, 16])
    GT_s = scr("GT_s", [S, 2048])
    QK_s = scr("QK_s", [1024, TALL])
    KV_s = scr("KV_s", [TALL, 1024])
    OD_s = scr("OD_s", [2, S, 512])
    OA_s = scr("OA_s", [S, 512])
    MOD_s = scr("MOD_s", [8, D])

    with contextlib.ExitStack() as gst:
        s = Sch(nc, gst)
        _uid = [0]

        def _nm(name):
            _uid[0] += 1
            return f"{name}_u{_uid[0]}"
        T = lambda st, name, shape: st.enter_context(nc.sbuf_tensor(_nm(name), list(shape), F32))
        PS = lambda st, name, shape: st.enter_context(nc.psum_tensor(_nm(name), list(shape), F32))
        cm = T(gst, "cm", [128, 15, 128])
        s.dma(cm[:], cmat_d, writes=["cm"])
        ident = cm[:, C_ID, :]
        bv = T(gst, "bv", [128, 9, D])
        BV_G1, BV_SH1, BV_GT1, BV_G2, BV_SH2, BV_GT2, BV_CG1, BV_CSH1, BV_FN = range(9)

        with contextlib.ExitStack() as st:
            cc = T(st, "cc", [128, 2, 8]); cs = T(st, "cs", [128, 2, 8]); lh = T(st, "lh", [128, 2, 8, 128])
            wa = [T(st, f"wa{i}", [128, 8, 512]) for i in range(2)]
            bb = T(st, "bb", [128, 6 * D]); nm = T(st, "nm", [128, 2, D])
            pm = [PS(st, f"pm{i}", [128, 512]) for i in range(2)]
            s.dma(cc[:, 0, :], c_d.rearrange("(kc p) -> p kc", p=128), writes=["cc"], allow_slow_non_contiguous=True)
            s.dma(cc[:, 1, :], cctx_d.rearrange("(kc p) -> p kc", p=128), writes=["cc"], allow_slow_non_contiguous=True)
            s.dma(bb[:], bada_d.partition_broadcast(128), writes=["bb"])
            s.dma(nm[:, 0, :], nmix_d.partition_broadcast(128), writes=["nm"])
            s.dma(nm[:, 1, :], nffn_d.partition_broadcast(128), writes=["nm"])
            s.dma(bv[:, BV_FN, :], fnorm_d.partition_broadcast(128), writes=["bv"])
            s.op("act", lambda e: e.activation(out=cs[:], in_=cc[:], func=AF.Silu), reads=["cc"], writes=["cs"])
            s.op("dve", lambda e: e.tensor_copy(out=lh[:], in_=cs[:].unsqueeze(3).to_broadcast([128, 2, 8, 128])), reads=["cs"], writes=["lh"])
            jobs = [(0, nb) for nb in range(12)] + [(1, nb) for nb in range(4)]
            for ji, (w, nb) in enumerate(jobs):
                wt = wa[ji % 2]; p = pm[ji % 2]
                s.dma(wt[:], wada_d[:, nb * 512:(nb + 1) * 512].rearrange("(kc p) n -> p kc n", p=128), writes=[("wa", ji % 2)], queue=("sp" if ji % 2 == 0 else "act"))
                for kc in range(8):
                    s.op("pe", lambda e, w=w, kc=kc, wt=wt, p=p: e.matmul(out=p[:], lhsT=lh[:, w, kc, :], rhs=wt[:, kc, :], start=(kc == 0), stop=(kc == 7)),
                         reads=["lh", ("wa", ji % 2)], writes=[("pm", ji % 2)])
                ch, half = nb // 2, nb % 2
                if w == 0:
                    dst = {0: BV_SH1, 1: BV_G1, 2: BV_GT1, 3: BV_SH2, 4: BV_G2, 5: BV_GT2}[ch]
                else:
                    dst = {0: BV_CSH1, 1: BV_CG1}[ch]
                o = bv[:, dst, half * 512:(half + 1) * 512]
                s.op("dve", lambda e, o=o, p=p, nb=nb: e.tensor_tensor(out=o, in0=p[:], in1=bb[:, nb * 512:(nb + 1) * 512], op=ALU.add),
                     reads=[("pm", ji % 2), "bb"], writes=["bv"])
            for dst, ni in ((BV_G1, 0), (BV_G2, 1), (BV_CG1, 0)):
                s.op("dve", lambda e, dst=dst, ni=ni: e.scalar_tensor_tensor(out=bv[:, dst, :], in0=bv[:, dst, :], scalar=1.0, in1=nm[:, ni, :], op0=ALU.add, op1=ALU.mult),
                     reads=["bv", "nm"], writes=["bv"])
            if "MOD_s" in dbg:
                s.dma(MOD_s.rearrange("(o a) d -> o a d", o=1), bv[0:1, 0:8, :], reads=["bv"])
            s.flush()
        if upto <= 0:
            return nc

        blocks = [(0, 512), (512, 256), (768, 512), (1280, 512), (1792, 512), (2304, 512), (2816, 16)] + [(2832 + 512 * i, 512) for i in range(4)]
        with contextlib.ExitStack() as st:
            xt = [T(st, f"xt{i}", [128, D]) for i in range(2)]
            junk = T(st, "junk", [128, D]); ss = T(st, "ss", [128, 1]); rstd = T(st, "rstd", [128, 1])
            h = T(st, "h", [128, D]); hT = T(st, "hT", [128, 8, 128])
            wb = [T(st, f"wb{i}", [128, 8, 512]) for i in range(3)]
            rp = [T(st, f"rp{i}", [128, 64]) for i in range(2)]
            qs = T(st, "qs", [128, 512]); qr = T(st, "qr", [128, 512]); tmp = T(st, "tmp", [128, 512])
            qT = T(st, "qT", [64, 8, 128]); kvs = T(st, "kvs", [128, 256]); kr = T(st, "kr", [128, 128]); kT = T(st, "kT", [64, 2, 128])
            va = T(st, "va", [128, 2, 65]); rw = T(st, "rw", [128, 512]); rT = T(st, "rT", [128, 4, 128])
            zz = T(st, "zz", [128, 512]); gn = T(st, "gn", [128, 128]); ab = T(st, "ab", [128, 16]); abc = T(st, "abc", [128, 2, 8])
            gbo = T(st, "gbo", [128, 16]); gg = T(st, "gg", [128, 512]); bg = T(st, "bg", [128, 2048])
            pT = [PS(st, f"pT{i}", [128, 512]) for i in range(2)]
            pY = [PS(st, f"pY{i}", [128, 512]) for i in range(3)]
            pX = [PS(st, f"pX{i}", [128, 512]) for i in range(2)]
            s.dma(bg[:], bgate_d.partition_broadcast(128), writes=["bg"])
            s.dma(gn[:], dnn_d.partition_broadcast(128), writes=["gn"])
            s.dma(abc[:, 0, 0:4], dtf_d.partition_broadcast(128), writes=["abc"])
            s.dma(abc[:, 0, 4:8], dtb_d.partition_broadcast(128), writes=["abc"])
            s.dma(abc[:, 1, 0:4], alf_d.partition_broadcast(128), writes=["abc"])
            s.dma(abc[:, 1, 4:8], alb_d.partition_broadcast(128), writes=["abc"])
            s.op("act", lambda e: e.activation(out=abc[:, 1, :], in_=abc[:, 1, :], func=AF.Exp), reads=["abc"], writes=["abc"])
            s.op("dve", lambda e: e.tensor_scalar(out=abc[:, 1, :], in0=abc[:, 1, :], scalar1=-1.0, scalar2=None, op0=ALU.mult), reads=["abc"], writes=["abc"])
            s.op("pool", lambda e: e.memset(va[:], 1.0), writes=["va"])
            wcount = [0]

            def rope_ops(src, dst, H):
                sv = src.rearrange("p (h a b c) -> p h a b c", h=H, a=2, b=2)
                dv = dst.rearrange("p (h a b c) -> p h a b c", h=H, a=2, b=2)
                tv = tmp[:, 0:H * 64].rearrange("p (h a b c) -> p h a b c", h=H, a=2, b=2)
                return sv, dv, tv

            tiles = [("c", i) for i in range(CTX // 128)] + [("l", i) for i in range(NT)]
            if upto == 1 and "small" in dbg:
                tiles = tiles[:4]
            for ti, (kind, i) in enumerate(tiles):
                lat = kind == "l"
                src = x_d if lat else ctx_d
                tg = ti
                X = xt[ti % 2]; xid = ("xt", ti % 2)
                s.dma(X[:], src[i * 128:(i + 1) * 128, :], writes=[xid])
                if lat:
                    R = rp[ti % 2]; rid = ("rp", ti % 2)
                    s.dma(R[:], rope_d[i * 128:(i + 1) * 128, :], writes=[rid], queue="act")
                s.op("act", lambda e, X=X: e.activation(out=junk[:], in_=X[:], func=AF.Square, accum_out=ss[:]), reads=[xid], writes=["junk", "ss"])
                s.op("dve", lambda e: e.tensor_scalar(out=rstd[:], in0=ss[:], scalar1=1.0 / D, scalar2=1e-6, op0=ALU.mult, op1=ALU.add), reads=["ss"], writes=["rstd"])
                s.op("act", lambda e: e.sqrt(out=rstd[:], in_=rstd[:]), reads=["rstd"], writes=["rstd"])
                s.op("dve", lambda e: e.reciprocal(out=rstd[:], in_=rstd[:]), reads=["rstd"], writes=["rstd"])
                G = BV_G1 if lat else BV_CG1
                SH = BV_SH1 if lat else BV_CSH1
                s.op("dve", lambda e, X=X, G=G: e.scalar_tensor_tensor(out=h[:], in0=X[:], scalar=rstd[:, 0:1], in1=bv[:, G, :], op0=ALU.mult, op1=ALU.mult), reads=[xid, "rstd", "bv"], writes=["h"])
                s.op("pool", lambda e, SH=SH: e.tensor_tensor(out=h[:], in0=h[:], in1=bv[:, SH, :], op=ALU.add), reads=["h", "bv"], writes=["h"])
                for hb in range(2):
                    for k4 in range(4):
                        kc = hb * 4 + k4
                        s.op("pe", lambda e, kc=kc, hb=hb, k4=k4: e.transpose(out=pT[hb][:, k4 * 128:(k4 + 1) * 128], in_=h[:, kc * 128:(kc + 1) * 128], identity=ident), reads=["h", "cm"], writes=[("pT", hb)])
                    eng = "act" if hb == 0 else "dve"
                    if eng == "act":
                        s.op("act", lambda e, hb=hb: e.copy(out=hT[:, hb * 4:(hb + 1) * 4, :].rearrange("p a b -> p (a b)"), in_=pT[hb][:]), reads=[("pT", hb)], writes=[("hT", hb)])
                    else:
                        s.op("dve", lambda e, hb=hb: e.tensor_copy(out=hT[:, hb * 4:(hb + 1) * 4, :].rearrange("p a b -> p (a b)"), in_=pT[hb][:]), reads=[("pT", hb)], writes=[("hT", hb)])
                need = range(11) if lat else (1, 2, 3, 4, 6)
                for bi in need:
                    c0, cw = blocks[bi]
                    wi = wcount[0] % 3; wcount[0] += 1
                    W = wb[wi]; P = pY[wi]
                    s.dma(W[:, :, 0:cw], win_d[:, c0:c0 + cw].rearrange("(kc p) n -> p kc n", p=128), writes=[("wb", wi)], queue=("sp", "act", "pool")[wi], allow_slow_non_contiguous=(cw < 128))
                    for kc in range(8):
                        s.op("pe", lambda e, kc=kc, W=W, P=P, cw=cw: e.matmul(out=P[:, 0:cw], lhsT=hT[:, kc, :], rhs=W[:, kc, 0:cw], start=(kc == 0), stop=(kc == 7)),
                             reads=[("hT", 0), ("hT", 1), ("wb", wi)], writes=[("pY", wi)])
                    pid = ("pY", wi)
                    if bi == 0:
                        s.op("act", lambda e, P=P: e.activation(out=qs[:], in_=P[:], func=AF.Copy, scale=0.125), reads=[pid], writes=["qs"])
                        _rope(s, qs[:], qr[:], tmp, R, rid, 8, "qs", "qr")
                        for hh in range(8):
                            s.op("pe", lambda e, hh=hh: e.transpose(out=pX[hh // 4][0:64, (hh % 4) * 128:(hh % 4 + 1) * 128], in_=qr[:, hh * 64:(hh + 1) * 64], identity=ident), reads=["qr", "cm"], writes=[("pX", hh // 4)])
                        s.op("act", lambda e: e.copy(out=qT[:, 0:4, :].rearrange("p a b -> p (a b)"), in_=pX[0][0:64, :]), reads=[("pX", 0)], writes=["qT"])
                        s.op("dve", lambda e: e.tensor_copy(out=qT[:, 4:8, :].rearrange("p a b -> p (a b)"), in_=pX[1][0:64, :]), reads=[("pX", 1)], writes=["qT"])
                        s.dma(QT_s[:, :, i * 128:(i + 1) * 128], qT[:], reads=["qT"], writes=["QT_s"], queue="pool")
                    elif bi == 1:
                        s.op("act", lambda e, P=P: e.copy(out=kvs[:], in_=P[:, 0:256]), reads=[pid], writes=["kvs"])
                        if lat:
                            _rope(s, kvs[:, 0:128], kr[:], tmp, R, rid, 2, "kvs", "kr")
                            ksrc, kid = kr, "kr"
                        else:
                            ksrc, kid = kvs, "kvs"
                        for hh in range(2):
                            s.op("pe", lambda e, hh=hh, ksrc=ksrc: e.transpose(out=pX[0][0:64, hh * 128:(hh + 1) * 128], in_=ksrc[:, hh * 64:(hh + 1) * 64], identity=ident), reads=[kid, "cm"], writes=[("pX", 0)])
                        s.op("act", lambda e: e.copy(out=kT[:].rearrange("p a b -> p (a b)"), in_=pX[0][0:64, 0:256]), reads=[("pX", 0)], writes=["kT"])
                        s.dma(KT_s[:, :, tg * 128:(tg + 1) * 128], kT[:], reads=["kT"], writes=["KT_s"], queue="pool")
                        s.op("pool", lambda e: e.tensor_copy(out=va[:, :, 0:64], in_=kvs[:, 128:256].rearrange("p (g d) -> p g d", g=2)), reads=["kvs"], writes=["va"])
                        s.dma(V_s[tg * 128:(tg + 1) * 128, :, :], va[:], reads=["va"], writes=["V_s"], queue="pool")
                    elif bi in (2, 3, 4):
                        s.op("act", lambda e, P=P: e.copy(out=rw[:], in_=P[:]), reads=[pid], writes=["rw"])
                        for k4 in range(4):
                            s.op("pe", lambda e, k4=k4: e.transpose(out=pX[1][:, k4 * 128:(k4 + 1) * 128], in_=rw[:, k4 * 128:(k4 + 1) * 128], identity=ident), reads=["rw", "cm"], writes=[("pX", 1)])
                        s.op("dve", lambda e: e.tensor_copy(out=rT[:].rearrange("p a b -> p (a b)"), in_=pX[1][:]), reads=[("pX", 1)], writes=["rT"])
                        f0 = (bi - 2) * 512
                        s.dma(RT_s[f0:f0 + 512, tg * 128:(tg + 1) * 128].rearrange("(a p) t -> p a t", p=128), rT[:], reads=["rT"], writes=["RT_s"], queue="pool")
                    elif bi == 5:
                        s.op("act", lambda e, P=P: e.activation(out=zz[:], in_=P[:], func=AF.Silu), reads=[pid], writes=["zz"])
                        s.op("pool", lambda e: e.tensor_tensor(out=zz[:].rearrange("p (h d) -> p h d", h=4), in0=zz[:].rearrange("p (h d) -> p h d", h=4), in1=gn[:].unsqueeze(1).to_broadcast([128, 4, 128]), op=ALU.mult), reads=["zz", "gn"], writes=["zz"])
                        s.dma(Z_s[i * 128:(i + 1) * 128, :], zz[:], reads=["zz"], writes=["Z_s"], queue="pool")
                    elif bi == 6:
                        s.op("dve", lambda e, P=P: e.tensor_tensor(out=ab[:, 0:8], in0=P[:, 0:8], in1=abc[:, 0, :], op=ALU.add), reads=[pid, "abc"], writes=["ab"])
                        s.op("act", lambda e: e.activation(out=ab[:, 0:8], in_=ab[:, 0:8], func=AF.Exp), reads=["ab"], writes=["ab"])
                        s.op("dve", lambda e: e.tensor_scalar(out=ab[:, 0:8], in0=ab[:, 0:8], scalar1=1.0, scalar2=None, op0=ALU.add), reads=["ab"], writes=["ab"])
                        s.op("act", lambda e: e.activation(out=ab[:, 0:8], in_=ab[:, 0:8], func=AF.Ln), reads=["ab"], writes=["ab"])
                        s.op("dve", lambda e: e.tensor_tensor(out=gbo[:, 0:8], in0=ab[:, 0:8], in1=abc[:, 1, :], op=ALU.mult), reads=["ab", "abc"], writes=["gbo"])
                        s.op("act", lambda e, P=P: e.activation(out=gbo[:, 8:16], in_=P[:, 8:16], func=AF.Sigmoid), reads=[pid], writes=["gbo"])
                        s.dma(GB_s[tg * 128:(tg + 1) * 128, :], gbo[:], reads=["gbo"], writes=["GB_s"], queue="pool")
                    else:
                        gi = bi - 7
                        s.op("dve", lambda e, P=P, gi=gi: e.tensor_tensor(out=gg[:], in0=P[:], in1=bg[:, gi * 512:(gi + 1) * 512], op=ALU.add), reads=[pid, "bg"], writes=["gg"])
                        s.op("act", lambda e: e.activation(out=gg[:], in_=gg[:], func=AF.Sigmoid), reads=["gg"], writes=["gg"])
                        s.dma(GT_s[i * 128:(i + 1) * 128, gi * 512:(gi + 1) * 512], gg[:], reads=["gg"], writes=["GT_s"], queue="pool")
            s.flush()
        if upto <= 1:
            return nc

        with contextlib.ExitStack() as st:
            cw = T(st, "cw", [128, 12, 5])
            Rt = [T(st, f"Rt{i}", [128, 516]) for i in range(3)]
            acc = [T(st, f"acc{i}", [128, 512]) for i in range(2)]
            y = [T(st, f"y{i}", [128, 512]) for i in range(2)]
            y2 = T(st, "y2", [128, 512]); rn = T(st, "rn", [128, 512]); yn = [T(st, f"yn{i}", [128, 512]) for i in range(2)]
            tok = [T(st, f"tok{i}", [128, 4, 128]) for i in range(2)]
            pN = [PS(st, f"pN{i}", [128, 512]) for i in range(2)]
            pK = [PS(st, f"pK{i}", [128, 512]) for i in range(2)]
            for j in range(5):
                s.dma(cw[:, :, j], conv_d[j, :].rearrange("(fc p) -> p fc", p=128), writes=["cw"], allow_slow_non_contiguous=True)
            it = 0
            segs = [(0, CTX), (CTX, TALL)]
            if "small" in dbg:
                segs = [(0, CTX), (CTX, CTX + 256)]
            for (g0, g1) in segs:
                for t0 in range(g0, g1, 512):
                    n = min(512, g1 - t0)
                    for fc in range(12):
                        R = Rt[it % 3]; rid = ("Rt", it % 3); A = acc[it % 2]; aid = ("acc", it % 2); Y = y[it % 2]; yid = ("y", it % 2)
                        lo = max(t0 - 2, g0); hi = min(t0 + n + 2, g1)
                        if lo > t0 - 2 or hi < t0 + n + 2:
                            s.op("pool", lambda e, R=R: e.memset(R[:], 0.0), writes=[rid])
                        s.dma(R[:, lo - (t0 - 2):hi - (t0 - 2)], RT_s[fc * 128:(fc + 1) * 128, lo:hi], reads=["RT_s"], writes=[rid], queue=("sp", "act")[it % 2])
                        s.op("dve", lambda e, R=R, A=A, fc=fc, n=n: e.tensor_scalar(out=A[:, 0:n], in0=R[:, 0:n], scalar1=cw[:, fc, 0:1], scalar2=None, op0=ALU.mult), reads=[rid, "cw"], writes=[aid])
                        for j in range(1, 5):
                            s.op("dve", lambda e, R=R, A=A, fc=fc, n=n, j=j: e.scalar_tensor_tensor(out=A[:, 0:n], in0=R[:, j:j + n], scalar=cw[:, fc, j:j + 1], in1=A[:, 0:n], op0=ALU.mult, op1=ALU.add), reads=[rid, "cw", aid], writes=[aid])
                        s.op("act", lambda e, A=A, Y=Y, n=n: e.activation(out=Y[:, 0:n], in_=A[:, 0:n], func=AF.Silu), reads=[aid], writes=[yid])
                        src, sid = Y, yid
                        if fc < 8:
                            YN = yn[it % 2]; nid = ("yn", it % 2); P = pN[it % 2]; pid = ("pN", it % 2)
                            s.op("act", lambda e, Y=Y, n=n: e.activation(out=y2[:, 0:n], in_=Y[:, 0:n], func=AF.Square), reads=[yid], writes=["y2"])
                            s.op("pe", lambda e, P=P, n=n: e.matmul(out=P[:, 0:n], lhsT=cm[:, C_ONES, :], rhs=y2[:, 0:n], start=True, stop=True), reads=["cm", "y2"], writes=[pid])
                            s.op("dve", lambda e, P=P, n=n: e.tensor_scalar(out=rn[:, 0:n], in0=P[:, 0:n], scalar1=1e-6, scalar2=None, op0=ALU.add), reads=[pid], writes=["rn"])
                            s.op("act", lambda e, n=n: e.sqrt(out=rn[:, 0:n], in_=rn[:, 0:n]), reads=["rn"], writes=["rn"])
                            s.op("dve", lambda e, n=n: e.reciprocal(out=rn[:, 0:n], in_=rn[:, 0:n]), reads=["rn"], writes=["rn"])
                            sc = float(128 ** -0.5) if fc < 4 else 1.0
                            s.op("dve", lambda e, Y=Y, YN=YN, n=n, sc=sc: e.scalar_tensor_tensor(out=YN[:, 0:n], in0=Y[:, 0:n], scalar=sc, in1=rn[:, 0:n], op0=ALU.mult, op1=ALU.mult), reads=[yid, "rn"], writes=[nid])
                            s.dma(QK_s[fc * 128:(fc + 1) * 128, t0:t0 + n], YN[:, 0:n], reads=[nid], writes=["QK_s"], queue="pool")
                            src, sid = YN, nid
                        if fc >= 4:
                            PK = pK[it % 2]; kid = ("pK", it % 2); TK = tok[it % 2]; tid = ("tok", it % 2)
                            nsb = n // 128
                            for sb in range(nsb):
                                s.op("pe", lambda e, PK=PK, src=src, sb=sb: e.transpose(out=PK[:, sb * 128:(sb + 1) * 128], in_=src[:, sb * 128:(sb + 1) * 128], identity=ident), reads=[sid, "cm"], writes=[kid])
                            s.op("act", lambda e, PK=PK, TK=TK, n=n: e.copy(out=TK[:].rearrange("p a b -> p (a b)")[:, 0:n], in_=PK[:, 0:n]), reads=[kid], writes=[tid])
                            s.dma(KV_s[t0:t0 + n, (fc - 4) * 128:(fc - 3) * 128].rearrange("(sb p) f -> p sb f", p=128), TK[:, 0:nsb, :], reads=[tid], writes=["KV_s"], queue="pool")
                        it += 1
            s.flush()
        if upto <= 2:
            return nc

        with contextlib.ExitStack() as st:
            Sst = [T(st, f"Sst{i}", [128, 4, 128]) for i in range(2)]
            qT4 = T(st, "qT4", [128, 4, 128]); kT4 = T(st, "kT4", [128, 4, 128]); ktok = T(st, "ktok", [128, 4, 128]); vtok = T(st, "vtok", [128, 4, 128])
            gb = T(st, "gb", [128, 16]); sm = T(st, "sm", [128, 16]); ex = T(st, "ex", [128, 16]); beg = T(st, "beg", [128, 4])
            G1 = T(st, "G1", [128, 4, 128]); dl = T(st, "dl", [128, 4, 128]); du = T(st, "du", [128, 4, 128])
            Bm = [T(st, f"Bm{i}", [128, 4, 128]) for i in range(2)]; Cm = [T(st, f"Cm{i}", [128, 4, 128]) for i in range(2)]; Pm = [T(st, f"Pm{i}", [128, 4, 128]) for i in range(2)]
            aT = T(st, "aT", [128, 4, 128]); kbg = T(st, "kbg", [128, 4, 128]); vb = T(st, "vb", [128, 4, 128]); ktl = T(st, "ktl", [128, 4, 128])
            WT = T(st, "WT", [128, 4, 128]); U = T(st, "U", [128, 4, 128]); vn = T(st, "vn", [128, 4, 128]); o1 = T(st, "o1", [128, 4, 128]); ot = T(st, "ot", [128, 4, 128])
            pb = [PS(st, f"pb{i}", [128, 4, 128]) for i in range(8)]
            pA, pB_, pC, pD, pE, pF, pG, pH = pb
            pid = lambda k: ("pb", k)
            H4 = [128, 4, 128]
            bc_h = lambda ap2: ap2.unsqueeze(1).to_broadcast(H4)
            bc_l = lambda ap2: ap2.unsqueeze(2).to_broadcast(H4)
            ntl = (2 if "small" in dbg else NT)
            for dr in range(2):
                M1 = cm[:, C_M1F if dr == 0 else C_M1B, :]; NM1 = cm[:, C_NM1F if dr == 0 else C_NM1B, :]
                NB = cm[:, C_NLOW if dr == 0 else C_NUP, :]; NTm = cm[:, C_NUP if dr == 0 else C_NLOW, :]
                STR = cm[:, C_SLOW if dr == 0 else C_SUP, :]
                SS = Sst[dr]; ssid = ("Sst", dr)
                s.op("pool", lambda e, SS=SS: e.memset(SS[:], 0.0), writes=[ssid])
                order = [("c", i) for i in range(CTX // 128)] + [("l", i) for i in range(ntl)]
                if dr == 1:
                    order = [("c", i) for i in reversed(range(CTX // 128))] + [("l", i) for i in reversed(range(ntl))]
                for (kind, i) in order:
                    lat = kind == "l"
                    tg = i if not lat else CTX // 128 + i
                    tsl = slice(tg * 128, (tg + 1) * 128)
                    s.dma(qT4[:], QK_s[0:512, tsl].rearrange("(h p) t -> p h t", p=128), reads=["QK_s"], writes=["qT4"])
                    s.dma(kT4[:], QK_s[512:1024, tsl].rearrange("(h p) t -> p h t", p=128), reads=["QK_s"], writes=["kT4"], queue="act")
                    s.dma(ktok[:].rearrange("p h d -> p (h d)"), KV_s[tsl, 0:512], reads=["KV_s"], writes=["ktok"])
                    s.dma(vtok[:].rearrange("p h d -> p (h d)"), KV_s[tsl, 512:1024], reads=["KV_s"], writes=["vtok"], queue="act")
                    s.dma(gb[:], GB_s[tsl, :], reads=["GB_s"], writes=["gb"])
                    g = gb[:, dr * 4:dr * 4 + 4]; beta = gb[:, 8 + dr * 4:12 + dr * 4]
                    pAf = pA[:].rearrange("p a b -> p (a b)")
                    for k, L in enumerate((M1, cm[:, C_SAME, :], cm[:, C_SEL0, :], cm[:, C_SEL1, :])):
                        s.op("pe", lambda e, k=k, L=L, g=g: e.matmul(out=pAf[:, 4 * k:4 * k + 4], lhsT=L, rhs=g, start=True, stop=True), reads=["cm", "gb"], writes=[pid(0)])
                    s.op("dve", lambda e: e.tensor_copy(out=sm[:], in_=pAf[:, 0:16]), reads=[pid(0)], writes=["sm"])
                    s.op("dve", lambda e: e.tensor_tensor(out=sm[:, 4:8], in0=sm[:, 4:8], in1=sm[:, 0:4], op=ALU.subtract), reads=["sm"], writes=["sm"])
                    s.op("act", lambda e: e.activation(out=ex[:], in_=sm[:], func=AF.Exp), reads=["sm"], writes=["ex"])
                    s.op("dve", lambda e, beta=beta: e.tensor_tensor(out=beg[:], in0=ex[:, 0:4], in1=beta, op=ALU.mult), reads=["ex", "gb"], writes=["beg"])
                    s.op("dve", lambda e, g=g: e.tensor_tensor(out=G1[:], in0=bc_h(cm[:, C_SAME, :]), in1=bc_l(g), op=ALU.mult), reads=["cm", "gb"], writes=["G1"])
                    for hh in range(4):
                        s.op("pe", lambda e, hh=hh, M1=M1: e.matmul(out=pB_[:, hh, :], lhsT=M1, rhs=G1[:, hh, :], start=True, stop=False), reads=["cm", "G1"], writes=[pid(1)])
                        s.op("pe", lambda e, hh=hh, NM1=NM1: e.matmul(out=pB_[:, hh, :], lhsT=G1[:, hh, :], rhs=NM1, start=False, stop=True), reads=["cm", "G1"], writes=[pid(1)])
                    s.op("dve", lambda e, NB=NB: e.tensor_tensor(out=dl[:], in0=pB_[:], in1=bc_h(NB), op=ALU.add), reads=[pid(1), "cm"], writes=["dl"])
                    s.op("dve", lambda e, NTm=NTm: e.scalar_tensor_tensor(out=du[:], in0=pB_[:], scalar=-1.0, in1=bc_h(NTm), op0=ALU.mult, op1=ALU.add), reads=[pid(1), "cm"], writes=["du"])
                    s.op("act", lambda e: e.activation(out=dl[:], in_=dl[:], func=AF.Exp), reads=["dl"], writes=["dl"])
                    s.op("act", lambda e: e.activation(out=du[:], in_=du[:], func=AF.Exp), reads=["du"], writes=["du"])
                    for hh in range(4):
                        s.op("pe", lambda e, hh=hh: e.matmul(out=pC[:, hh, :], lhsT=kT4[:, hh, :], rhs=kT4[:, hh, :], start=True, stop=True), reads=["kT4"], writes=[pid(2)])
                    for hh in range(4):
                        s.op("pe", lambda e, hh=hh: e.matmul(out=pD[:, hh, :], lhsT=kT4[:, hh, :], rhs=qT4[:, hh, :], start=True, stop=True), reads=["kT4", "qT4"], writes=[pid(3)])
                    B0 = Bm[0]; C0 = Cm[0]; P0 = Pm[0]
                    s.op("dve", lambda e: e.tensor_tensor(out=B0[:], in0=pC[:], in1=dl[:], op=ALU.mult), reads=[pid(2), "dl"], writes=[("Bm", 0)])
                    s.op("pool", lambda e, STR=STR: e.tensor_tensor(out=B0[:], in0=B0[:], in1=bc_h(STR), op=ALU.mult), reads=[("Bm", 0), "cm"], writes=[("Bm", 0)])
                    s.op("pool", lambda e, beta=beta: e.tensor_tensor(out=B0[:], in0=B0[:], in1=bc_l(beta), op=ALU.mult), reads=[("Bm", 0), "gb"], writes=[("Bm", 0)])
                    s.op("dve", lambda e: e.tensor_tensor(out=aT[:], in0=pD[:], in1=du[:], op=ALU.mult), reads=[pid(3), "du"], writes=["aT"])
                    for hh in range(4):
                        s.op("pe", lambda e, hh=hh: e.transpose(out=pE[:, hh, :], in_=B0[:, hh, :], identity=ident), reads=[("Bm", 0), "cm"], writes=[pid(4)])
                    s.op("act", lambda e: e.copy(out=C0[:], in_=pE[:]), reads=[pid(4)], writes=[("Cm", 0)])
                    s.op("dve", lambda e: e.tensor_tensor(out=P0[:], in0=C0[:], in1=bc_h(ident), op=ALU.add), reads=[("Cm", 0), "cm"], writes=[("Pm", 0)])
                    cur = 0
                    for lv in range(1, 6):
                        nx = 1 - cur
                        Bc, Cc, Pc = Bm[cur], Cm[cur], Pm[cur]; Bn, Cn, Pn = Bm[nx], Cm[nx], Pm[nx]
                        for hh in range(4):
                            s.op("pe", lambda e, hh=hh, Bc=Bc, Cc=Cc: e.matmul(out=pF[:, hh, :], lhsT=Cc[:, hh, :], rhs=Bc[:, hh, :], start=True, stop=True), reads=[("Bm", cur), ("Cm", cur)], writes=[pid(5)])
                        s.op("act", lambda e, Bn=Bn: e.copy(out=Bn[:], in_=pF[:]), reads=[pid(5)], writes=[("Bm", nx)])
                        if lv < 5:
                            for hh in range(4):
                                s.op("pe", lambda e, hh=hh, Bc=Bc, Cc=Cc: e.matmul(out=pG[:, hh, :], lhsT=Bc[:, hh, :], rhs=Cc[:, hh, :], start=True, stop=True), reads=[("Bm", cur), ("Cm", cur)], writes=[pid(6)])
                            s.op("dve", lambda e, Cn=Cn: e.tensor_copy(out=Cn[:], in_=pG[:]), reads=[pid(6)], writes=[("Cm", nx)])
                        for hh in range(4):
                            s.op("pe", lambda e, hh=hh, Pc=Pc: e.matmul(out=pH[:, hh, :], lhsT=ident, rhs=Pc[:, hh, :], start=True, stop=False), reads=[("Pm", cur), "cm"], writes=[pid(7)])
                            s.op("pe", lambda e, hh=hh, Pc=Pc, Bn=Bn: e.matmul(out=pH[:, hh, :], lhsT=Bn[:, hh, :], rhs=Pc[:, hh, :], start=False, stop=True), reads=[("Pm", cur), ("Bm", nx)], writes=[pid(7)])
                        s.op("dve", lambda e, Pn=Pn: e.tensor_copy(out=Pn[:], in_=pH[:]), reads=[pid(7)], writes=[("Pm", nx)])
                        cur = nx
                    TT = Pm[cur]; ttid = ("Pm", cur)
                    s.op("pool", lambda e: e.tensor_tensor(out=kbg[:], in0=ktok[:], in1=bc_l(beg[:]), op=ALU.mult), reads=["ktok", "beg"], writes=["kbg"])
                    s.op("pool", lambda e, beta=beta: e.tensor_tensor(out=vb[:], in0=vtok[:], in1=bc_l(beta), op=ALU.mult), reads=["vtok", "gb"], writes=["vb"])
                    s.op("pool", lambda e: e.tensor_tensor(out=ktl[:], in0=ktok[:], in1=bc_l(ex[:, 4:8]), op=ALU.mult), reads=["ktok", "ex"], writes=["ktl"])
                    for hh in range(4):
                        s.op("pe", lambda e, hh=hh, TT=TT: e.matmul(out=pE[:, hh, :], lhsT=kbg[:, hh, :], rhs=TT[:, hh, :], start=True, stop=True), reads=["kbg", ttid], writes=[pid(4)])
                    s.op("act", lambda e: e.copy(out=WT[:], in_=pE[:]), reads=[pid(4)], writes=["WT"])
                    for hh in range(4):
                        s.op("pe", lambda e, hh=hh, TT=TT: e.matmul(out=pF[:, hh, :], lhsT=TT[:, hh, :], rhs=vb[:, hh, :], start=True, stop=True), reads=["vb", ttid], writes=[pid(5)])
                    s.op("dve", lambda e: e.tensor_copy(out=U[:], in_=pF[:]), reads=[pid(5)], writes=["U"])
                    for c in ((0, 1) if dr == 0 else (1, 0)):
                        pr = slice(64 * c, 64 * c + 64)
                        for hh in range(4):
                            s.op("pe", lambda e, hh=hh, SS=SS: e.matmul(out=pG[:, hh, :], lhsT=WT[:, hh, :], rhs=SS[:, hh, :], start=True, stop=True), reads=["WT", ssid], writes=[pid(6)])
                        s.op("dve", lambda e, pr=pr: e.tensor_tensor(out=vn[pr], in0=U[pr], in1=pG[pr], op=ALU.subtract), reads=["U", pid(6)], writes=["vn"])
                        for hh in range(4):
                            s.op("pe", lambda e, hh=hh, SS=SS: e.matmul(out=pH[:, hh, :], lhsT=qT4[:, hh, :], rhs=SS[:, hh, :], start=True, stop=True), reads=["qT4", ssid], writes=[pid(7)])
                        for hh in range(4):
                            s.op("pe", lambda e, hh=hh, pr=pr: e.matmul(out=pC[:, hh, :], lhsT=aT[pr, hh, :], rhs=vn[pr, hh, :], start=True, stop=True), reads=["aT", "vn"], writes=[pid(2)])
                        for hh in range(4):
                            s.op("pe", lambda e, hh=hh, pr=pr: e.matmul(out=pD[:, hh, :], lhsT=ktl[pr, hh, :], rhs=vn[pr, hh, :], start=True, stop=True), reads=["ktl", "vn"], writes=[pid(3)])
                        if lat:
                            s.op("dve", lambda e, pr=pr: e.tensor_tensor(out=o1[pr], in0=pH[pr], in1=bc_l(ex[:, 0:4])[pr], op=ALU.mult), reads=[pid(7), "ex"], writes=["o1"])
                            s.op("dve", lambda e, pr=pr: e.tensor_tensor(out=ot[pr], in0=o1[pr], in1=pC[pr], op=ALU.add), reads=["o1", pid(2)], writes=["ot"])
                        s.op("pool", lambda e, c=c, SS=SS: e.tensor_tensor(out=SS[:], in0=SS[:], in1=bc_l(ex[:, 8 + 4 * c:12 + 4 * c]), op=ALU.mult), reads=[ssid, "ex", pid(6), pid(7)], writes=[ssid])
                        s.op("dve", lambda e, SS=SS: e.tensor_tensor(out=SS[:], in0=SS[:], in1=pD[:], op=ALU.add), reads=[ssid, pid(3)], writes=[ssid])
                    if lat:
                        s.dma(OD_s[dr, i * 128:(i + 1) * 128, :], ot[:].rearrange("p h d -> p (h d)"), reads=["ot"], writes=["OD_s"], queue="pool")
            s.flush()
        if upto <= 3:
            return nc

        with contextlib.ExitStack() as st:
            kt = [T(st, f"kt{i}", [64, 2, 384]) for i in range(2)]
            vt = [T(st, f"vt{i}", [128, 3, 130]) for i in range(2)]
            ktc = T(st, "ktc", [64, 2, 256]); vtc = T(st, "vtc", [128, 2, 130])
            qt = [T(st, f"qt{i}", [64, 8, 128]) for i in range(2)]
            E = [T(st, f"E{i}", [128, 5, 512]) for i in range(2)]
            esink = T(st, "esink", [128, 8]); den = T(st, "den", [128, 8]); oa = [T(st, f"oa{i}", [128, 512]) for i in range(2)]
            pS = [PS(st, f"pS{i}", [128, 512]) for i in range(3)]
            pO = [PS(st, f"pO{i}", [128, 4, 65]) for i in range(2)]
            s.dma(ktc[:], KT_s[:, :, 0:CTX], reads=["KT_s"], writes=["ktc"])
            s.dma(vtc[:], V_s[0:CTX].rearrange("(b p) g d -> p b (g d)", p=128), reads=["V_s"], writes=["vtc"])
            s.dma(esink[:], sink_d.partition_broadcast(128), writes=["esink"])
            s.op("act", lambda e: e.activation(out=esink[:], in_=esink[:], func=AF.Exp), reads=["esink"], writes=["esink"])
            ntl = (2 if "small" in dbg else NT)
            nS = 0
            for i in range(ntl):
                lo = max(i - 1, 0); hi = min(i + 1, ntl - 1); nb = hi - lo + 1
                KT_ = kt[i % 2]; VT_ = vt[i % 2]; QT_ = qt[i % 2]; OA = oa[i % 2]
                s.dma(KT_[:, :, 0:nb * 128], KT_s[:, :, CTX + lo * 128:CTX + (hi + 1) * 128], reads=["KT_s"], writes=[("kt", i % 2)])
                s.dma(VT_[:, 0:nb, :], V_s[CTX + lo * 128:CTX + (hi + 1) * 128].rearrange("(b p) g d -> p b (g d)", p=128), reads=["V_s"], writes=[("vt", i % 2)], queue="act")
                s.dma(QT_[:], QT_s[:, :, i * 128:(i + 1) * 128], reads=["QT_s"], writes=[("qt", i % 2)])
                for g in range(2):
                    Eg = E[g]; eid = ("E", g)
                    kb = [("l", j - lo, (C_WPREV if j < i else (C_WNEXT if j > i else None))) for j in range(lo, hi + 1)] + [("c", 0, None), ("c", 1, None)]
                    for bi, (kk, bl, msk) in enumerate(kb):
                        P = pS[nS % 3]; psid = ("pS", nS % 3); nS += 1
                        lhs = KT_[:, g, bl * 128:(bl + 1) * 128] if kk == "l" else ktc[:, g, bl * 128:(bl + 1) * 128]
                        s.op("pe", lambda e, P=P, lhs=lhs, QT_=QT_, g=g: e.matmul(out=P[:].rearrange("p (h q) -> p h q", h=4), lhsT=lhs, rhs=QT_[:, 4 * g:4 * g + 4, :], start=True, stop=True),
                             reads=[("kt", i % 2), "ktc", ("qt", i % 2)], writes=[psid])
                        s.op("act", lambda e, P=P, Eg=Eg, bi=bi: e.activation(out=Eg[:, bi, :], in_=P[:], func=AF.Exp), reads=[psid], writes=[eid])
                        if msk is not None:
                            s.op("dve", lambda e, Eg=Eg, bi=bi, msk=msk: e.tensor_tensor(out=Eg[:, bi, :].rearrange("p (h q) -> p h q", h=4), in0=Eg[:, bi, :].rearrange("p (h q) -> p h q", h=4),
                                                                              in1=cm[:, msk, :].unsqueeze(1).to_broadcast([128, 4, 128]), op=ALU.mult), reads=[eid, "cm"], writes=[eid])
                    for hh in range(4):
                        for bi, (kk, bl, msk) in enumerate(kb):
                            rhs = VT_[:, bl, g * 65:(g + 1) * 65] if kk == "l" else vtc[:, bl, g * 65:(g + 1) * 65]
                            s.op("pe", lambda e, Eg=Eg, bi=bi, hh=hh, rhs=rhs, g=g, last=(bi == len(kb) - 1): e.matmul(out=pO[g][:, hh, :], lhsT=Eg[:, bi, hh * 128:(hh + 1) * 128], rhs=rhs, start=(bi == 0), stop=last),
                                 reads=[eid, ("vt", i % 2), "vtc"], writes=[("pO", g)])
                    s.op("dve", lambda e, g=g: e.tensor_tensor(out=den[:, 4 * g:4 * g + 4], in0=pO[g][:, :, 64], in1=esink[:, 4 * g:4 * g + 4], op=ALU.add), reads=[("pO", g), "esink"], writes=["den"])
                    s.op("dve", lambda e, g=g: e.reciprocal(out=den[:, 4 * g:4 * g + 4], in_=den[:, 4 * g:4 * g + 4]), reads=["den"], writes=["den"])
                    s.op("dve", lambda e, g=g, OA=OA: e.tensor_tensor(out=OA[:, g * 256:(g + 1) * 256].rearrange("p (h d) -> p h d", h=4), in0=pO[g][:, :, 0:64],
                                                              in1=den[:, 4 * g:4 * g + 4].unsqueeze(2).to_broadcast([128, 4, 64]), op=ALU.mult), reads=[("pO", g), "den"], writes=[("oa", i % 2)])
                s.dma(OA_s[i * 128:(i + 1) * 128, :], OA[:], reads=[("oa", i % 2)], writes=["OA_s"], queue="pool")
            s.flush()
        if upto <= 5:
            return nc

        GI = 2
        NG = 128 // GI
        with contextlib.ExitStack() as st:
            xa = T(st, "xa", [128, D]); yb = T(st, "yb", [128, D]); tc_ = T(st, "tc_", [128, 8, 128]); gt = T(st, "gt", [128, 2048])
            od = T(st, "od", [128, 2, 512]); zt = T(st, "zt", [128, 512]); oat = T(st, "oat", [128, 512]); o2 = T(st, "o2", [128, 512])
            qsb = T(st, "qsb", [128, D]); qTs = T(st, "qTs", [64, 16, 128]); sc = T(st, "sc", [128, 16, 128])
            ssq = T(st, "ssq", [128, 4]); ss = T(st, "ss6", [128, 1]); rstd = T(st, "rstd6", [128, 1])
            t16 = T(st, "t16", [128, 2, 16]); c16 = T(st, "c16", [128, 16]); cand = T(st, "cand", [128, 16, 16]); wk = T(st, "wk", [128, 256])
            thr = T(st, "thr", [128, 8]); negm = T(st, "negm", [128, 8]); Zs = T(st, "Zs", [128, 8]); kap = T(st, "kap", [128, 8]); e16 = T(st, "e16", [128, 16])
            keysT = T(st, "keysT", [64, 16, 128])
            wS = T(st, "wS", [128, 8, D])
            UT = [T(st, f"UT{i}", [128, 8, GI * 128]) for i in range(2)]
            VG = [T(st, f"VG{i}", [128, GI, D]) for i in range(2)]
            sm_ = T(st, "sm_", [128, 8, GI, 128]); ee = T(st, "ee", [128, 8, GI, 128]); gd = T(st, "gd", [128, GI * 128])
            ga = T(st, "ga", [128, GI * 128]); gb_ = T(st, "gb_", [128, GI * 128]); Pm_ = T(st, "Pm_", [128, GI * 128]); PT = [T(st, f"PT{i}", [128, GI, 128]) for i in range(2)]
            pT = [PS(st, f"pT{i}", [128, 512]) for i in range(2)]
            pY = [PS(st, f"pY{i}", [128, 512]) for i in range(2)]
            pR = PS(st, "pR", [128, 512]); pW = PS(st, "pW", [128, 512]); pU = [PS(st, f"pU{i}", [128, 512]) for i in range(2)]
            s.dma(keysT[:], pkeys_d.rearrange("h p d k -> d (h p) k"), writes=["keysT"])

            def transpose8(src, sid, nkc, dst_off=0):
                for kc in range(nkc):
                    b = (dst_off + kc) // 4
                    s.op("pe", lambda e, kc=kc, b=b: e.transpose(out=pT[b][:, ((dst_off + kc) % 4) * 128:((dst_off + kc) % 4 + 1) * 128], in_=src[:, kc * 128:(kc + 1) * 128], identity=ident), reads=[sid, "cm"], writes=[("pT", b)])
                for b in sorted(set((dst_off + kc) // 4 for kc in range(nkc))):
                    eng = "act" if b == 0 else "dve"
                    f = (lambda e, b=b: e.copy(out=tc_[:, b * 4:(b + 1) * 4, :].rearrange("p a b -> p (a b)"), in_=pT[b][:])) if eng == "act" else (lambda e, b=b: e.tensor_copy(out=tc_[:, b * 4:(b + 1) * 4, :].rearrange("p a b -> p (a b)"), in_=pT[b][:]))
                    s.op(eng, f, reads=[("pT", b)], writes=[("tc", b)])

            def rms(src, sid):
                s.op("act", lambda e: e.activation(out=qsb[:], in_=src[:], func=AF.Square, accum_out=ss[:]), reads=[sid], writes=["qsb", "ss6"])
                s.op("dve", lambda e: e.tensor_scalar(out=rstd[:], in0=ss[:], scalar1=1.0 / D, scalar2=1e-6, op0=ALU.mult, op1=ALU.add), reads=["ss6"], writes=["rstd6"])
                s.op("act", lambda e: e.sqrt(out=rstd[:], in_=rstd[:]), reads=["rstd6"], writes=["rstd6"])
                s.op("dve", lambda e: e.reciprocal(out=rstd[:], in_=rstd[:]), reads=["rstd6"], writes=["rstd6"])

            ntl = (1 if "small" in dbg else NT)
            ngr = (2 if "small2" in dbg else NG)
            gcount = 0
            for i in range(ntl):
                tsl = slice(i * 128, (i + 1) * 128)
                s.dma(xa[:], x_d[tsl, :], writes=["xa"])
                s.dma(od[:, 0, :], OD_s[0, tsl, :], reads=["OD_s"], writes=["od"], queue="act")
                s.dma(od[:, 1, :], OD_s[1, tsl, :], reads=["OD_s"], writes=["od"], queue="act")
                s.dma(zt[:], Z_s[tsl, :], reads=["Z_s"], writes=["zt"])
                s.dma(oat[:], OA_s[tsl, :], reads=["OA_s"], writes=["oat"], queue="act")
                s.dma(gt[:], GT_s[tsl, :], reads=["GT_s"], writes=["gt"])
                s.dma(wS[:, 0:4, :], wba_d.rearrange("(kc p) n -> p kc n", p=128), writes=["wS"])
                s.dma(wS[:, 4:8, :], wbd_d.rearrange("(kc p) n -> p kc n", p=128), writes=["wS"], queue="act")
                s.op("dve", lambda e: e.tensor_tensor(out=od[:, 0, :], in0=od[:, 0, :], in1=od[:, 1, :], op=ALU.add), reads=["od"], writes=["od"])
                s.op("dve", lambda e: e.tensor_tensor(out=o2[:], in0=od[:, 0, :], in1=od[:, 0, :], op=ALU.mult), reads=["od"], writes=["o2"])
                s.op("dve", lambda e: e.tensor_reduce(out=ssq[:], in_=o2[:].rearrange("p (h d) -> p h d", h=4), axis=AX.X, op=ALU.add), reads=["o2"], writes=["ssq"])
                s.op("dve", lambda e: e.tensor_scalar(out=ssq[:], in0=ssq[:], scalar1=1.0 / 128, scalar2=1e-6, op0=ALU.mult, op1=ALU.add), reads=["ssq"], writes=["ssq"])
                s.op("act", lambda e: e.sqrt(out=ssq[:], in_=ssq[:]), reads=["ssq"], writes=["ssq"])
                s.op("dve", lambda e: e.reciprocal(out=ssq[:], in_=ssq[:]), reads=["ssq"], writes=["ssq"])
                s.op("dve", lambda e: e.tensor_tensor(out=o2[:].rearrange("p (h d) -> p h d", h=4), in0=od[:, 0, :].rearrange("p (h d) -> p h d", h=4), in1=ssq[:].unsqueeze(2).to_broadcast([128, 4, 128]), op=ALU.mult), reads=["od", "ssq"], writes=["o2"])
                s.op("dve", lambda e: e.tensor_tensor(out=o2[:], in0=o2[:], in1=zt[:], op=ALU.mult), reads=["o2", "zt"], writes=["o2"])
                transpose8(oat, "oat", 4, 0)
                transpose8(o2, "o2", 4, 4)
                for half in range(2):
                    for kc in range(4):
                        s.op("pe", lambda e, half=half, kc=kc: e.matmul(out=pY[half][:], lhsT=tc_[:, kc, :], rhs=wS[:, kc, half * 512:(half + 1) * 512], start=(kc == 0), stop=(kc == 3)), reads=[("tc", 0), "wS"], writes=[("pY", half)])
                    s.op("dve", lambda e, half=half: e.tensor_tensor(out=yb[:, half * 512:(half + 1) * 512], in0=pY[half][:], in1=gt[:, half * 512:(half + 1) * 512], op=ALU.mult), reads=[("pY", half), "gt"], writes=["yb"])
                for half in range(2):
                    for kc in range(4):
                        s.op("pe", lambda e, half=half, kc=kc: e.matmul(out=pY[half][:], lhsT=tc_[:, 4 + kc, :], rhs=wS[:, 4 + kc, half * 512:(half + 1) * 512], start=(kc == 0), stop=(kc == 3)), reads=[("tc", 1), "wS"], writes=[("pY", half)])
                    s.op("dve", lambda e, half=half: e.tensor_tensor(out=qsb[:, half * 512:(half + 1) * 512], in0=pY[half][:], in1=gt[:, 1024 + half * 512:1024 + (half + 1) * 512], op=ALU.mult), reads=[("pY", half), "gt"], writes=["qsb"])
                s.op("pool", lambda e: e.tensor_tensor(out=yb[:], in0=yb[:], in1=qsb[:], op=ALU.add), reads=["yb", "qsb"], writes=["yb"])
                s.dma(wS[:], wout_d.rearrange("(kc p) n -> p kc n", p=128), writes=["wS"])
                transpose8(yb, "yb", 8, 0)
                for half in range(2):
                    for kc in range(8):
                        s.op("pe", lambda e, half=half, kc=kc: e.matmul(out=pY[half][:], lhsT=tc_[:, kc, :], rhs=wS[:, kc, half * 512:(half + 1) * 512], start=(kc == 0), stop=(kc == 7)), reads=[("tc", 0), ("tc", 1), "wS"], writes=[("pY", half)])
                    s.op("dve", lambda e, half=half: e.tensor_tensor(out=yb[:, half * 512:(half + 1) * 512], in0=pY[half][:], in1=bv[:, BV_GT1, half * 512:(half + 1) * 512], op=ALU.mult), reads=[("pY", half), "bv"], writes=["yb"])
                s.op("pool", lambda e: e.tensor_tensor(out=xa[:], in0=xa[:], in1=yb[:], op=ALU.add), reads=["xa", "yb"], writes=["xa"])
                s.dma(wS[:], pwq_d.rearrange("(kc p) n -> p kc n", p=128), writes=["wS"])
                rms(xa, "xa")
                s.op("dve", lambda e: e.scalar_tensor_tensor(out=yb[:], in0=xa[:], scalar=rstd[:, 0:1], in1=bv[:, BV_G2, :], op0=ALU.mult, op1=ALU.mult), reads=["xa", "rstd6", "bv"], writes=["yb"])
                s.op("pool", lambda e: e.tensor_tensor(out=yb[:], in0=yb[:], in1=bv[:, BV_SH2, :], op=ALU.add), reads=["yb", "bv"], writes=["yb"])
                transpose8(yb, "yb", 8, 0)
                for half in range(2):
                    for kc in range(8):
                        s.op("pe", lambda e, half=half, kc=kc: e.matmul(out=pY[half][:], lhsT=tc_[:, kc, :], rhs=wS[:, kc, half * 512:(half + 1) * 512], start=(kc == 0), stop=(kc == 7)), reads=[("tc", 0), ("tc", 1), "wS"], writes=[("pY", half)])
                    if half == 0:
                        s.op("act", lambda e: e.copy(out=qsb[:, 0:512], in_=pY[0][:]), reads=[("pY", 0)], writes=["qsb"])
                    else:
                        s.op("dve", lambda e: e.tensor_copy(out=qsb[:, 512:1024], in_=pY[1][:]), reads=[("pY", 1)], writes=["qsb"])
                for rd in range(4):
                    b = rd % 2
                    for k4 in range(4):
                        hp = rd * 4 + k4
                        s.op("pe", lambda e, hp=hp, b=b, k4=k4: e.transpose(out=pT[b][0:64, k4 * 128:(k4 + 1) * 128], in_=qsb[:, hp * 64:(hp + 1) * 64], identity=ident), reads=["qsb", "cm"], writes=[("pT", b)])
                    if b == 0:
                        s.op("act", lambda e, rd=rd, b=b: e.copy(out=qTs[:, rd * 4:(rd + 1) * 4, :].rearrange("p a b -> p (a b)"), in_=pT[b][0:64, :]), reads=[("pT", b)], writes=["qTs"])
                    else:
                        s.op("dve", lambda e, rd=rd, b=b: e.tensor_copy(out=qTs[:, rd * 4:(rd + 1) * 4, :].rearrange("p a b -> p (a b)"), in_=pT[b][0:64, :]), reads=[("pT", b)], writes=["qTs"])
                for rd in range(4):
                    b = rd % 2
                    for k4 in range(4):
                        hp = rd * 4 + k4
                        s.op("pe", lambda e, hp=hp, b=b, k4=k4: e.matmul(out=pY[b][:, k4 * 128:(k4 + 1) * 128], lhsT=qTs[:, hp, :], rhs=keysT[:, hp, :], start=True, stop=True), reads=["qTs", "keysT"], writes=[("pY", b)])
                    if b == 0:
                        s.op("act", lambda e, rd=rd, b=b: e.copy(out=sc[:, rd * 4:(rd + 1) * 4, :].rearrange("p a b -> p (a b)"), in_=pY[b][:]), reads=[("pY", b)], writes=["sc"])
                    else:
                        s.op("dve", lambda e, rd=rd, b=b: e.tensor_copy(out=sc[:, rd * 4:(rd + 1) * 4, :].rearrange("p a b -> p (a b)"), in_=pY[b][:]), reads=[("pY", b)], writes=["sc"])
                for hh in range(8):
                    for p in range(2):
                        srow = sc[:, 2 * hh + p, :]
                        s.op("dve", lambda e, p=p, srow=srow: e.max(out=t16[:, p, 0:8], in_=srow), reads=["sc"], writes=["t16"])
                        s.op("dve", lambda e, p=p, srow=srow: e.match_replace(out=wk[:, 0:128], in_to_replace=t16[:, p, 0:8], in_values=srow, imm_value=-1e30), reads=["sc", "t16"], writes=["wk"])
                        s.op("dve", lambda e, p=p: e.max(out=t16[:, p, 8:16], in_=wk[:, 0:128]), reads=["wk"], writes=["t16"])
                    s.op("dve", lambda e: e.tensor_tensor(out=cand[:], in0=t16[:, 0, :].unsqueeze(2).to_broadcast([128, 16, 16]), in1=t16[:, 1, :].unsqueeze(1).to_broadcast([128, 16, 16]), op=ALU.add), reads=["t16"], writes=["cand"])
                    cf = cand[:].rearrange("p a b -> p (a b)")
                    s.op("dve", lambda e, cf=cf: e.max(out=c16[:, 0:8], in_=cf), reads=["cand"], writes=["c16"])
                    s.op("dve", lambda e, cf=cf: e.match_replace(out=wk[:], in_to_replace=c16[:, 0:8], in_values=cf, imm_value=-1e30), reads=["cand", "c16"], writes=["wk"])
                    s.op("dve", lambda e: e.max(out=c16[:, 8:16], in_=wk[:]), reads=["wk"], writes=["c16"])
                    s.op("dve", lambda e, hh=hh: e.tensor_scalar(out=thr[:, hh:hh + 1], in0=c16[:, 15:16], scalar1=-1e-4, scalar2=None, op0=ALU.add), reads=["c16"], writes=["thr"])
                    s.op("dve", lambda e, hh=hh: e.tensor_scalar(out=negm[:, hh:hh + 1], in0=c16[:, 0:1], scalar1=-1.0, scalar2=None, op0=ALU.mult), reads=["c16"], writes=["negm"])
                    s.op("act", lambda e, hh=hh: e.activation(out=e16[:], in_=c16[:], func=AF.Exp, bias=negm[:, hh:hh + 1], accum_out=Zs[:, hh:hh + 1]), reads=["c16", "negm"], writes=["e16", "Zs"])
                s.op("dve", lambda e: e.tensor_tensor(out=kap[:], in0=thr[:], in1=negm[:], op=ALU.add), reads=["thr", "negm"], writes=["kap"])
                s.op("act", lambda e: e.activation(out=kap[:], in_=kap[:], func=AF.Exp), reads=["kap"], writes=["kap"])
                s.op("dve", lambda e: e.reciprocal(out=Zs[:], in_=Zs[:]), reads=["Zs"], writes=["Zs"])
                s.op("dve", lambda e: e.tensor_tensor(out=kap[:], in0=kap[:], in1=Zs[:], op=ALU.mult), reads=["kap", "Zs"], writes=["kap"])
                sc4 = sc[:].rearrange("p (h q) k -> p h q k", q=2)
                s.op("dve", lambda e: e.tensor_tensor(out=sc4[:, :, 1, :], in0=sc4[:, :, 1, :], in1=thr[:].unsqueeze(2).to_broadcast([128, 8, 128]), op=ALU.subtract), reads=["sc", "thr"], writes=["sc"])
                for g in range(ngr):
                    u = gcount % 2; gcount += 1
                    e0 = g * GI * 128
                    s.dma(UT[u][:], pu_d[:, e0:e0 + GI * 128].rearrange("(kc p) n -> p kc n", p=128), writes=[("UT", u)], queue=("sp", "act")[u])
                    s.dma(VG[u][:], pv_d[e0:e0 + GI * 128, :].rearrange("(a p) n -> p a n", p=128), writes=[("VG", u)], queue=("act", "sp")[u])
                    for kc in range(8):
                        s.op("pe", lambda e, kc=kc, u=u: e.matmul(out=pR[:, 0:GI * 128], lhsT=tc_[:, kc, :], rhs=UT[u][:, kc, :], start=(kc == 0), stop=(kc == 7)), reads=[("tc", 0), ("tc", 1), ("UT", u)], writes=["pR_"])
                    s1b = sc4[:, :, 0, g * GI:(g + 1) * GI].unsqueeze(3).to_broadcast([128, 8, GI, 128])
                    s2b = sc4[:, :, 1, :].unsqueeze(2).to_broadcast([128, 8, GI, 128])
                    s.op("pool", lambda e, s1b=s1b, s2b=s2b: e.tensor_tensor(out=sm_[:], in0=s1b, in1=s2b, op=ALU.add), reads=["sc"], writes=["sm_"])
                    s.op("act", lambda e: e.activation(out=ee[:], in_=sm_[:], func=AF.Exp), reads=["sm_"], writes=["ee"])
                    s.op("dve", lambda e: e.scalar_tensor_tensor(out=ee[:], in0=sm_[:], scalar=0.0, in1=ee[:], op0=ALU.is_ge, op1=ALU.mult), reads=["sm_", "ee"], writes=["ee"])
                    s.op("pool", lambda e: e.tensor_tensor(out=ee[:].rearrange("p h a k -> p h (a k)"), in0=ee[:].rearrange("p h a k -> p h (a k)"), in1=kap[:].unsqueeze(2).to_broadcast([128, 8, GI * 128]), op=ALU.mult), reads=["ee", "kap"], writes=["ee"])
                    s.op("dve", lambda e: e.tensor_reduce(out=gd[:], in_=ee[:].rearrange("p h a k -> p (a k) h"), axis=AX.X, op=ALU.add), reads=["ee"], writes=["gd"])
                    s.op("act", lambda e: e.activation(out=ga[:], in_=pR[:, 0:GI * 128], func=AF.Square), reads=["pR_"], writes=["ga", "pR_"])
                    s.op("dve", lambda e: e.tensor_scalar(out=ga[:], in0=ga[:], scalar1=0.044715, scalar2=1.0, op0=ALU.mult, op1=ALU.add), reads=["ga"], writes=["ga"])
                    s.op("dve", lambda e: e.tensor_tensor(out=ga[:], in0=ga[:], in1=pR[:, 0:GI * 128], op=ALU.mult), reads=["ga", "pR_"], writes=["ga", "pR_"])
                    s.op("act", lambda e: e.activation(out=ga[:], in_=ga[:], func=AF.Sigmoid, scale=1.5957691216057308), reads=["ga"], writes=["ga"])
                    s.op("dve", lambda e: e.tensor_tensor(out=gb_[:], in0=pR[:, 0:GI * 128], in1=gd[:], op=ALU.mult), reads=["pR_", "gd"], writes=["gb_", "pR_"])
                    s.op("pool", lambda e: e.tensor_tensor(out=Pm_[:], in0=gb_[:], in1=ga[:], op=ALU.mult), reads=["gb_", "ga"], writes=["Pm_"])
                    for a in range(GI):
                        s.op("pe", lambda e, a=a: e.transpose(out=pW[:, a * 128:(a + 1) * 128], in_=Pm_[:, a * 128:(a + 1) * 128], identity=ident), reads=["Pm_", "cm"], writes=["pW_"])
                    s.op("act", lambda e, u=u: e.copy(out=PT[u][:].rearrange("p a b -> p (a b)"), in_=pW[:, 0:GI * 128]), reads=["pW_"], writes=[("PT", u), "pW_"])
                    for a in range(GI):
                        for half in range(2):
                            s.op("pe", lambda e, a=a, half=half, u=u, first=(g == 0 and a == 0), last=(g == ngr - 1 and a == GI - 1): e.matmul(out=pU[half][:], lhsT=PT[u][:, a, :], rhs=VG[u][:, a, half * 512:(half + 1) * 512], start=first, stop=last),
                                 reads=[("PT", u), ("VG", u)], writes=[("pU", half)])
                for half in range(2):
                    s.op("dve", lambda e, half=half: e.tensor_tensor(out=yb[:, half * 512:(half + 1) * 512], in0=pU[half][:], in1=bv[:, BV_GT2, half * 512:(half + 1) * 512], op=ALU.mult), reads=[("pU", half), "bv"], writes=["yb"])
                s.op("pool", lambda e: e.tensor_tensor(out=xa[:], in0=xa[:], in1=yb[:], op=ALU.add), reads=["xa", "yb"], writes=["xa"])
                rms(xa, "xa")
                s.op("dve", lambda e: e.scalar_tensor_tensor(out=yb[:], in0=xa[:], scalar=rstd[:, 0:1], in1=bv[:, BV_FN, :], op0=ALU.mult, op1=ALU.mult), reads=["xa", "rstd6", "bv"], writes=["yb"])
                s.dma(out_d[tsl, :], yb[:], reads=["yb"], writes=["out"], queue="pool")
            s.flush()
        return nc


def _rope(s, src, dst, tmp, R, rid, H, sid, did):
    sv = src.rearrange("p (h a b c) -> p h a b c", h=H, a=2, b=2)
    dv = dst.rearrange("p (h a b c) -> p h a b c", h=H, a=2, b=2)
    tv = tmp[:, 0:H * 32].rearrange("p (h a c) -> p h a c", h=H, a=2)
    rv = R[:].rearrange("p (a b c) -> p a b c", a=2, b=2)
    cosb = rv[:, :, 0, :].unsqueeze(1).to_broadcast([128, H, 2, 16])
    sinb = rv[:, :, 1, :].unsqueeze(1).to_broadcast([128, H, 2, 16])
    x1 = sv[:, :, :, 0, :]; x2 = sv[:, :, :, 1, :]
    o1 = dv[:, :, :, 0, :]; o2 = dv[:, :, :, 1, :]
    s.op("dve", lambda e: e.tensor_tensor(out=o1, in0=x1, in1=cosb, op=ALU.mult), reads=[sid, rid], writes=[did])
    s.op("dve", lambda e: e.tensor_tensor(out=tv, in0=x2, in1=sinb, op=ALU.mult), reads=[sid, rid], writes=["tmp"])
    s.op("dve", lambda e: e.tensor_tensor(out=o1, in0=o1, in1=tv, op=ALU.subtract), reads=[did, "tmp"], writes=[did])
    s.op("dve", lambda e: e.tensor_tensor(out=o2, in0=x1, in1=sinb, op=ALU.mult), reads=[sid, rid, did], writes=[did])
    s.op("dve", lambda e: e.tensor_tensor(out=tv, in0=x2, in1=cosb, op=ALU.mult), reads=[sid, rid, did], writes=["tmp"])
    s.op("dve", lambda e: e.tensor_tensor(out=o2, in0=o2, in1=tv, op=ALU.add), reads=[did, "tmp"], writes=[did])


def _host_inputs(inputs, b, consts):
    g = lambda k: np.ascontiguousarray(inputs[k], dtype=np.float32)
    m = {
        "x": g("x")[b], "c": g("c")[b], "ctx": g("ctx")[b], "c_ctx": g("c_ctx"),
        "w_ada": g("w_ada")[0], "b_ada": g("b_ada")[0], "norm_mix": g("norm_mix")[0], "norm_ffn": g("norm_ffn")[0],
        "w_in": g("w_in")[0], "b_gate": g("b_gate")[0], "attn_sink": g("attn_sink")[0], "dn_conv": g("dn_conv")[0],
        "dn_a_log_f": g("dn_a_log_f")[0], "dn_dt_bias_f": g("dn_dt_bias_f")[0], "dn_a_log_b": g("dn_a_log_b")[0], "dn_dt_bias_b": g("dn_dt_bias_b")[0],
        "dn_norm": g("dn_norm")[0], "w_br_attn": g("w_br_attn")[0], "w_br_dn": g("w_br_dn")[0], "w_out": g("w_out")[0],
        "peer_wq": g("peer_wq")[0], "final_norm": g("final_norm"),
    }
    m.update(consts)
    return {k: np.ascontiguousarray(v) for k, v in m.items()}


_SHARED = {}


def kernel(**inputs):
    consts = _consts()
    nc = build()
    keysT = np.ascontiguousarray(np.transpose(np.asarray(inputs["peer_keys"], np.float32)[0], (0, 1, 3, 2)))
    uT = np.ascontiguousarray(np.asarray(inputs["peer_u"], np.float32)[0].T)
    pv = np.ascontiguousarray(np.asarray(inputs["peer_v"], np.float32)[0])
    in_maps = []
    for b in range(8):
        m = _host_inputs(inputs, b, consts)
        m["peer_keysT"] = keysT; m["peer_uT"] = uT; m["peer_v"] = pv
        in_maps.append(m)
    res = run_bass_kernel_spmd(nc, in_maps, core_ids=list(range(8)))
    return np.stack([np.asarray(r["out"], dtype=np.float32) for r in res.results], axis=0)
```

```python
import contextlib
import numpy as np
import concourse.bass as bass
import concourse.mybir as mybir
from concourse.bass_utils import run_bass_kernel_spmd

F32 = mybir.dt.float32
F32R = mybir.dt.float32r
ALU = mybir.AluOpType
AF = mybir.ActivationFunctionType
AX = mybir.AxisListType

D = 1024
S = 8192
CTX = 256
TALL = CTX + S
NT = S // 128
IN_COLS = 4880
NEG = -30000.0


class _Ins:
    __slots__ = ("eng", "fn", "deps", "signal", "sig_no", "dma", "idx")

    def __init__(self, eng, fn, dma=None):
        self.eng = eng
        self.fn = fn
        self.deps = []
        self.signal = False
        self.sig_no = None
        self.dma = dma
        self.idx = None


class Sch:
    EPOCH = 20000
    NDMA = 24
    NEP = 16

    def __init__(self, nc, st):
        self.nc = nc
        self.engs = ("pe", "act", "dve", "pool", "sp")
        self.nep = {"pe": 12, "act": 4, "dve": 6, "pool": 3, "sp": 1}
        self.sems = {e: [st.enter_context(nc.semaphore(f"s_{e}_{i}")) for i in range(self.nep[e])] for e in self.engs}
        self.dsems = [st.enter_context(nc.semaphore(f"s_dma_{i}")) for i in range(self.NDMA)]
        self.sigc = {e: 0 for e in self.engs}
        self.dma_rr = 0
        self.dma_cnt = [0] * self.NDMA
        self.dma_last = [None] * self.NDMA
        self._reset()

    def _reset(self):
        self.q = {e: [] for e in self.engs}
        self.lastw = {}
        self.readers = {}

    def _add(self, ins, reads, writes):
        q = self.q[ins.eng]
        ins.idx = len(q)
        deps = []
        for r in reads:
            w = self.lastw.get(r)
            if w is not None:
                deps.append((w, "raw"))
        for w_ in writes:
            w = self.lastw.get(w_)
            if w is not None:
                deps.append((w, "waw"))
            for rd in self.readers.get(w_, ()):
                deps.append((rd, "war"))
        for d, kind in deps:
            if d is ins:
                continue
            if d.dma is None and ins.dma is None and d.eng == ins.eng:
                if ins.eng == "pe":
                    continue
                if kind != "raw":
                    continue
            ins.deps.append(d)
            if d.dma is None:
                d.signal = True
        for r in reads:
            self.readers.setdefault(r, []).append(ins)
        for w_ in writes:
            self.lastw[w_] = ins
            self.readers[w_] = []
        q.append(ins)
        return ins

    PSUM_NAMES = {"bk", "pm", "pT", "pY", "pX", "pN", "pK", "pb", "pS", "pO", "pQ", "pZ", "pR", "pU", "pW"}

    def op(self, eng, fn, reads=(), writes=()):
        writes = list(writes)
        if eng != "pe":
            for r in reads:
                if isinstance(r, tuple) and r[0] in self.PSUM_NAMES and r not in writes:
                    writes.append(r)
        return self._add(_Ins(eng, fn), list(reads), writes)

    def dma(self, out, in_, reads=(), writes=(), queue="sp", **kw):
        slot = self.dma_rr
        self.dma_rr = (self.dma_rr + 1) % self.NDMA
        self.dma_cnt[slot] += 1
        n = self.dma_cnt[slot]
        ins = _Ins(queue, lambda e: e.dma_start(out=out, in_=in_, **kw), dma=(slot, n))
        prev = self.dma_last[slot]
        self._add(ins, list(reads), list(writes))
        if prev is not None:
            ins.deps.append(prev)
        self.dma_last[slot] = ins
        return ins

    def flush(self):
        nc = self.nc
        for e, q in self.q.items():
            for ins in q:
                if ins.dma is None and ins.signal:
                    ins.sig_no = self.sigc[e]
                    self.sigc[e] += 1
            assert self.sigc[e] < self.EPOCH * self.nep[e], f"too many signals on {e}: {self.sigc[e]}"
        dma_final = list(self.dma_cnt)
        with nc.Block() as block:
            def run(ename):
                def body(eng):
                    seen_c = {}
                    seen_d = {}
                    for ins in self.q[ename]:
                        wc = {}
                        wd = {}
                        for d in ins.deps:
                            if d.dma is None:
                                if d.sig_no is None:
                                    continue
                                if seen_c.get(d.eng, -1) < d.sig_no:
                                    wc[d.eng] = max(wc.get(d.eng, -1), d.sig_no)
                            else:
                                s_, n = d.dma
                                if seen_d.get(s_, 0) < n:
                                    wd[s_] = max(wd.get(s_, 0), n)
                        for e2, sn in wc.items():
                            eng.wait_ge(self.sems[e2][sn // self.EPOCH], sn % self.EPOCH + 1)
                            seen_c[e2] = sn
                        for s_, n in wd.items():
                            eng.wait_ge(self.dsems[s_], 16 * n)
                            seen_d[s_] = n
                        h = ins.fn(eng)
                        if ins.dma is not None:
                            h.then_inc(self.dsems[ins.dma[0]], 16)
                        elif ins.signal:
                            h.then_inc(self.sems[ename][ins.sig_no // self.EPOCH], 1)
                    if ename == "sp":
                        for s_, n in enumerate(dma_final):
                            if n > 0:
                                eng.wait_ge(self.dsems[s_], 16 * n)
                return body

            block.sync(run("sp"))
            block.tensor(run("pe"))
            block.scalar(run("act"))
            block.vector(run("dve"))
            block.gpsimd(run("pool"))
        nc.all_engine_barrier()
        self._reset()


def _consts():
    c = {}
    ident = np.eye(128, dtype=np.float32)
    ones = np.ones((128, 128), np.float32)
    idx = np.arange(128)
    same = (idx[:, None] // 64 == idx[None, :] // 64).astype(np.float32)
    m1f = ((idx[:, None] <= idx[None, :]) * same).astype(np.float32)
    m1b = ((idx[:, None] >= idx[None, :]) * same).astype(np.float32)
    sel0 = np.zeros((128, 128), np.float32); sel0[:64, :] = 1
    sel1 = np.zeros((128, 128), np.float32); sel1[64:, :] = 1
    low_incl = ((idx[None, :] <= idx[:, None]) * same)
    up_incl = ((idx[None, :] >= idx[:, None]) * same)
    low_strict = ((idx[None, :] < idx[:, None]) * same)
    up_strict = ((idx[None, :] > idx[:, None]) * same)
    negmask = lambda m: np.where(m > 0, 0.0, NEG).astype(np.float32)
    w_prev = (idx[None, :] <= idx[:, None]).astype(np.float32)
    w_next = (idx[:, None] <= idx[None, :]).astype(np.float32)
    mats = [ident, ones, same, m1f, -m1f, m1b, -m1b, sel0, sel1,
            negmask(low_incl), negmask(up_incl), -low_strict.astype(np.float32), -up_strict.astype(np.float32),
            w_prev, w_next]
    c["cmat"] = np.ascontiguousarray(np.stack(mats, axis=1)).astype(np.float32)
    pos = np.arange(S)
    inv = (10000.0 ** (-np.arange(16, dtype=np.float32) / 16)).astype(np.float32)
    ar = (pos // 64).astype(np.float32)[:, None] * inv[None, :]
    ac = (pos % 64).astype(np.float32)[:, None] * inv[None, :]
    c["rope"] = np.concatenate([np.cos(ar), np.sin(ar), np.cos(ac), np.sin(ac)], axis=1).astype(np.float32)
    return c

(C_ID, C_ONES, C_SAME, C_M1F, C_NM1F, C_M1B, C_NM1B, C_SEL0, C_SEL1, C_NLOW, C_NUP, C_SLOW, C_SUP, C_WPREV, C_WNEXT) = range(15)


def build(upto=99, dbg=()):
    nc = bass.Bass("TRN2", target_bir_lowering=False)
    nc.dge_precook = False
    inp = lambda name, shape: nc.dram_tensor(name, list(shape), F32, kind="ExternalInput").ap()
    x_d = inp("x", [S, D]); c_d = inp("c", [D]); ctx_d = inp("ctx", [CTX, D]); cctx_d = inp("c_ctx", [D])
    wada_d = inp("w_ada", [D, 6 * D]); bada_d = inp("b_ada", [6 * D])
    nmix_d = inp("norm_mix", [D]); nffn_d = inp("norm_ffn", [D])
    win_d = inp("w_in", [D, IN_COLS]); bgate_d = inp("b_gate", [2 * D])
    sink_d = inp("attn_sink", [8]); conv_d = inp("dn_conv", [5, 1536])
    alf_d = inp("dn_a_log_f", [4]); dtf_d = inp("dn_dt_bias_f", [4]); alb_d = inp("dn_a_log_b", [4]); dtb_d = inp("dn_dt_bias_b", [4])
    dnn_d = inp("dn_norm", [128]); wba_d = inp("w_br_attn", [512, D]); wbd_d = inp("w_br_dn", [512, D]); wout_d = inp("w_out", [D, D])
    pwq_d = inp("peer_wq", [D, D]); pkeys_d = inp("peer_keysT", [8, 2, 64, 128]); pu_d = inp("peer_uT", [D, 16384]); pv_d = inp("peer_v", [16384, D])
    fnorm_d = inp("final_norm", [D]); cmat_d = inp("cmat", [128, 15, 128]); rope_d = inp("rope", [S, 64])
    out_d = nc.dram_tensor("out", [S, D], F32, kind="ExternalOutput").ap()
    scr = lambda name, shape: nc.dram_tensor(name, list(shape), F32, kind=("ExternalOutput" if name in dbg else "Internal")).ap()
    QT_s = scr("QT_s", [64, 8, S])
    KT_s = scr("KT_s", [64, 2, TALL])
    V_s = scr("V_s", [TALL, 2, 65])
    RT_s = scr("RT_s", [1536, TALL])
    Z_s = scr("Z_s", [S, 512])
    GB_s = scr("GB_s", [TALL, 16])
    GT_s = scr("GT_s", [S, 2048])
    QK_s = scr("QK_s", [1024, TALL])
    KV_s = scr("KV_s", [TALL, 1024])
    OD_s = scr("OD_s", [2, S, 512])
    OA_s = scr("OA_s", [S, 512])
    MOD_s = scr("MOD_s", [8, D])

    with contextlib.ExitStack() as gst:
        s = Sch(nc, gst)
        _uid = [0]

        def _nm(name):
            _uid[0] += 1
            return f"{name}_u{_uid[0]}"
        T = lambda st, name, shape: st.enter_context(nc.sbuf_tensor(_nm(name), list(shape), F32))
        PS = lambda st, name, shape: st.enter_context(nc.psum_tensor(_nm(name), list(shape), F32))
        cm = T(gst, "cm", [128, 15, 128])
        s.dma(cm[:], cmat_d, writes=["cm"])
        ident = cm[:, C_ID, :]
        BV_G1, BV_SH1, BV_GT1, BV_G2, BV_SH2, BV_GT2, BV_CG1, BV_CSH1, BV_FN = range(9)
        bvB = T(gst, "bvB", [128, 5, D])
        stA = contextlib.ExitStack()
        bvA = T(stA, "bvA", [128, 4, D])
        _amap = {BV_G1: 0, BV_SH1: 1, BV_CG1: 2, BV_CSH1: 3}
        _bmap = {BV_GT1: 0, BV_G2: 1, BV_SH2: 2, BV_GT2: 3, BV_FN: 4}

        def bvv(k):
            return bvA[:, _amap[k], :] if k in _amap else bvB[:, _bmap[k], :]

        with contextlib.ExitStack() as st:
            cc = T(st, "cc", [128, 2, 8]); cs = T(st, "cs", [128, 2, 8]); lh = T(st, "lh", [128, 2, 8, 128])
            wa = [T(st, f"wa{i}", [128, 8, 512]) for i in range(2)]
            bb = T(st, "bb", [128, 6 * D]); nm = T(st, "nm", [128, 2, D])
            pm = [PS(st, f"pm{i}", [128, 512]) for i in range(2)]
            s.dma(cc[:, 0, :], c_d.rearrange("(kc p) -> p kc", p=128), writes=["cc"], allow_slow_non_contiguous=True)
            s.dma(cc[:, 1, :], cctx_d.rearrange("(kc p) -> p kc", p=128), writes=["cc"], allow_slow_non_contiguous=True)
            s.dma(bb[:], bada_d.partition_broadcast(128), writes=["bb"])
            s.dma(nm[:, 0, :], nmix_d.partition_broadcast(128), writes=["nm"])
            s.dma(nm[:, 1, :], nffn_d.partition_broadcast(128), writes=["nm"])
            s.dma(bvv(BV_FN)[:, :], fnorm_d.partition_broadcast(128), writes=["bv"])
            s.op("act", lambda e: e.activation(out=cs[:], in_=cc[:], func=AF.Silu), reads=["cc"], writes=["cs"])
            s.op("dve", lambda e: e.tensor_copy(out=lh[:], in_=cs[:].unsqueeze(3).to_broadcast([128, 2, 8, 128])), reads=["cs"], writes=["lh"])
            jobs = [(0, nb) for nb in range(12)] + [(1, nb) for nb in range(4)]
            for ji, (w, nb) in enumerate(jobs):
                wt = wa[ji % 2]; p = pm[ji % 2]
                s.dma(wt[:], wada_d[:, nb * 512:(nb + 1) * 512].rearrange("(kc p) n -> p kc n", p=128), writes=[("wa", ji % 2)], queue=("sp" if ji % 2 == 0 else "act"))
                for kc in range(8):
                    s.op("pe", lambda e, w=w, kc=kc, wt=wt, p=p: e.matmul(out=p[:], lhsT=lh[:, w, kc, :], rhs=wt[:, kc, :], start=(kc == 0), stop=(kc == 7)),
                         reads=["lh", ("wa", ji % 2)], writes=[("pm", ji % 2)])
                ch, half = nb // 2, nb % 2
                if w == 0:
                    dst = {0: BV_SH1, 1: BV_G1, 2: BV_GT1, 3: BV_SH2, 4: BV_G2, 5: BV_GT2}[ch]
                else:
                    dst = {0: BV_CSH1, 1: BV_CG1}[ch]
                o = bvv(dst)[:, half * 512:(half + 1) * 512]
                s.op("dve", lambda e, o=o, p=p, nb=nb: e.tensor_tensor(out=o, in0=p[:], in1=bb[:, nb * 512:(nb + 1) * 512], op=ALU.add),
                     reads=[("pm", ji % 2), "bb"], writes=["bv"])
            for dst, ni in ((BV_G1, 0), (BV_G2, 1), (BV_CG1, 0)):
                s.op("dve", lambda e, dst=dst, ni=ni: e.scalar_tensor_tensor(out=bvv(dst)[:, :], in0=bvv(dst)[:, :], scalar=1.0, in1=nm[:, ni, :], op0=ALU.add, op1=ALU.mult),
                     reads=["bv", "nm"], writes=["bv"])
            s.flush()
        if upto <= 0:
            stA.close()
            return nc

        blocks = [(0, 512), (512, 256), (768, 512), (1280, 512), (1792, 512), (2304, 512), (2816, 16)] + [(2832 + 512 * i, 512) for i in range(4)]
        with contextlib.ExitStack() as st:
            xt = [T(st, f"xt{i}", [128, D]) for i in range(2)]
            junk = T(st, "junk", [128, D]); ss = T(st, "ss", [128, 1]); rstd = T(st, "rstd", [128, 1])
            h = T(st, "h", [128, D]); hT = T(st, "hT", [128, 8, 128])
            wb = [T(st, f"wb{i}", [128, 8, 512]) for i in range(3)]
            rp = [T(st, f"rp{i}", [128, 64]) for i in range(2)]
            qs = T(st, "qs", [128, 512]); qr = T(st, "qr", [128, 512]); tmp = T(st, "tmp", [128, 512])
            qT = T(st, "qT", [64, 8, 128]); kvs = T(st, "kvs", [128, 256]); kr = T(st, "kr", [128, 128]); kT = T(st, "kT", [64, 2, 128])
            va = T(st, "va", [128, 2, 65]); rw = T(st, "rw", [128, 512]); rT = T(st, "rT", [128, 4, 128])
            zz = T(st, "zz", [128, 512]); gn = T(st, "gn", [128, 128]); ab = T(st, "ab", [128, 16]); abc = T(st, "abc", [128, 2, 8])
            gbo = T(st, "gbo", [128, 16]); gg = T(st, "gg", [128, 512]); bg = T(st, "bg", [128, 2048])
            pT = [PS(st, f"pT{i}", [128, 512]) for i in range(2)]
            pY = [PS(st, f"pY{i}", [128, 512]) for i in range(3)]
            pX = [PS(st, f"pX{i}", [128, 512]) for i in range(2)]
            s.dma(bg[:], bgate_d.partition_broadcast(128), writes=["bg"])
            s.dma(gn[:], dnn_d.partition_broadcast(128), writes=["gn"])
            s.dma(abc[:, 0, 0:4], dtf_d.partition_broadcast(128), writes=["abc"])
            s.dma(abc[:, 0, 4:8], dtb_d.partition_broadcast(128), writes=["abc"])
            s.dma(abc[:, 1, 0:4], alf_d.partition_broadcast(128), writes=["abc"])
            s.dma(abc[:, 1, 4:8], alb_d.partition_broadcast(128), writes=["abc"])
            s.op("act", lambda e: e.activation(out=abc[:, 1, :], in_=abc[:, 1, :], func=AF.Exp), reads=["abc"], writes=["abc"])
            s.op("dve", lambda e: e.tensor_scalar(out=abc[:, 1, :], in0=abc[:, 1, :], scalar1=-1.0, scalar2=None, op0=ALU.mult), reads=["abc"], writes=["abc"])
            s.op("pool", lambda e: e.memset(va[:], 1.0), writes=["va"])
            wcount = [0]

            def rope_ops(src, dst, H):
                sv = src.rearrange("p (h a b c) -> p h a b c", h=H, a=2, b=2)
                dv = dst.rearrange("p (h a b c) -> p h a b c", h=H, a=2, b=2)
                tv = tmp[:, 0:H * 64].rearrange("p (h a b c) -> p h a b c", h=H, a=2, b=2)
                return sv, dv, tv

            tiles = [("c", i) for i in range(CTX // 128)] + [("l", i) for i in range(NT)]
            if upto == 1 and "small" in dbg:
                tiles = tiles[:4]
            for ti, (kind, i) in enumerate(tiles):
                lat = kind == "l"
                src = x_d if lat else ctx_d
                tg = ti
                X = xt[ti % 2]; xid = ("xt", ti % 2)
                s.dma(X[:], src[i * 128:(i + 1) * 128, :], writes=[xid])
                if lat:
                    R = rp[ti % 2]; rid = ("rp", ti % 2)
                    s.dma(R[:], rope_d[i * 128:(i + 1) * 128, :], writes=[rid], queue="act")
                s.op("act", lambda e, X=X: e.activation(out=junk[:], in_=X[:], func=AF.Square, accum_out=ss[:]), reads=[xid], writes=["junk", "ss"])
                s.op("dve", lambda e: e.tensor_scalar(out=rstd[:], in0=ss[:], scalar1=1.0 / D, scalar2=1e-6, op0=ALU.mult, op1=ALU.add), reads=["ss"], writes=["rstd"])
                s.op("act", lambda e: e.sqrt(out=rstd[:], in_=rstd[:]), reads=["rstd"], writes=["rstd"])
                s.op("dve", lambda e: e.reciprocal(out=rstd[:], in_=rstd[:]), reads=["rstd"], writes=["rstd"])
                G = BV_G1 if lat else BV_CG1
                SH = BV_SH1 if lat else BV_CSH1
                s.op("dve", lambda e, X=X, G=G: e.scalar_tensor_tensor(out=h[:], in0=X[:], scalar=rstd[:, 0:1], in1=bvv(G)[:, :], op0=ALU.mult, op1=ALU.mult), reads=[xid, "rstd", "bv"], writes=["h"])
                s.op("pool", lambda e, SH=SH: e.tensor_tensor(out=h[:], in0=h[:], in1=bvv(SH)[:, :], op=ALU.add), reads=["h", "bv"], writes=["h"])
                for hb in range(2):
                    for k4 in range(4):
                        kc = hb * 4 + k4
                        s.op("pe", lambda e, kc=kc, hb=hb, k4=k4: e.transpose(out=pT[hb][:, k4 * 128:(k4 + 1) * 128], in_=h[:, kc * 128:(kc + 1) * 128], identity=ident), reads=["h", "cm"], writes=[("pT", hb)])
                    eng = "act" if hb == 0 else "dve"
                    if eng == "act":
                        s.op("act", lambda e, hb=hb: e.copy(out=hT[:, hb * 4:(hb + 1) * 4, :].rearrange("p a b -> p (a b)"), in_=pT[hb][:]), reads=[("pT", hb)], writes=[("hT", hb)])
                    else:
                        s.op("dve", lambda e, hb=hb: e.tensor_copy(out=hT[:, hb * 4:(hb + 1) * 4, :].rearrange("p a b -> p (a b)"), in_=pT[hb][:]), reads=[("pT", hb)], writes=[("hT", hb)])
                need = range(11) if lat else (1, 2, 3, 4, 6)
                for bi in need:
                    c0, cw = blocks[bi]
                    wi = wcount[0] % 3; wcount[0] += 1
                    W = wb[wi]; P = pY[wi]
                    s.dma(W[:, :, 0:cw], win_d[:, c0:c0 + cw].rearrange("(kc p) n -> p kc n", p=128), writes=[("wb", wi)], queue=("sp", "act", "pool")[wi], allow_slow_non_contiguous=(cw < 128))
                    for kc in range(8):
                        s.op("pe", lambda e, kc=kc, W=W, P=P, cw=cw: e.matmul(out=P[:, 0:cw], lhsT=hT[:, kc, :], rhs=W[:, kc, 0:cw], start=(kc == 0), stop=(kc == 7)),
                             reads=[("hT", 0), ("hT", 1), ("wb", wi)], writes=[("pY", wi)])
                    pid = ("pY", wi)
                    if bi == 0:
                        s.op("act", lambda e, P=P: e.activation(out=qs[:], in_=P[:], func=AF.Copy, scale=0.125), reads=[pid], writes=["qs"])
                        _rope(s, qs[:], qr[:], tmp, R, rid, 8, "qs", "qr")
                        for hh in range(8):
                            s.op("pe", lambda e, hh=hh: e.transpose(out=pX[hh // 4][0:64, (hh % 4) * 128:(hh % 4 + 1) * 128], in_=qr[:, hh * 64:(hh + 1) * 64], identity=ident), reads=["qr", "cm"], writes=[("pX", hh // 4)])
                        s.op("act", lambda e: e.copy(out=qT[:, 0:4, :].rearrange("p a b -> p (a b)"), in_=pX[0][0:64, :]), reads=[("pX", 0)], writes=["qT"])
                        s.op("dve", lambda e: e.tensor_copy(out=qT[:, 4:8, :].rearrange("p a b -> p (a b)"), in_=pX[1][0:64, :]), reads=[("pX", 1)], writes=["qT"])
                        s.dma(QT_s[:, :, i * 128:(i + 1) * 128], qT[:], reads=["qT"], writes=["QT_s"], queue="pool")
                    elif bi == 1:
                        s.op("act", lambda e, P=P: e.copy(out=kvs[:], in_=P[:, 0:256]), reads=[pid], writes=["kvs"])
                        if lat:
                            _rope(s, kvs[:, 0:128], kr[:], tmp, R, rid, 2, "kvs", "kr")
                            ksrc, kid = kr, "kr"
                        else:
                            ksrc, kid = kvs, "kvs"
                        for hh in range(2):
                            s.op("pe", lambda e, hh=hh, ksrc=ksrc: e.transpose(out=pX[0][0:64, hh * 128:(hh + 1) * 128], in_=ksrc[:, hh * 64:(hh + 1) * 64], identity=ident), reads=[kid, "cm"], writes=[("pX", 0)])
                        s.op("act", lambda e: e.copy(out=kT[:].rearrange("p a b -> p (a b)"), in_=pX[0][0:64, 0:256]), reads=[("pX", 0)], writes=["kT"])
                        s.dma(KT_s[:, :, tg * 128:(tg + 1) * 128], kT[:], reads=["kT"], writes=["KT_s"], queue="pool")
                        s.op("pool", lambda e: e.tensor_copy(out=va[:, :, 0:64], in_=kvs[:, 128:256].rearrange("p (g d) -> p g d", g=2)), reads=["kvs"], writes=["va"])
                        s.dma(V_s[tg * 128:(tg + 1) * 128, :, :], va[:], reads=["va"], writes=["V_s"], queue="pool")
                    elif bi in (2, 3, 4):
                        s.op("act", lambda e, P=P: e.copy(out=rw[:], in_=P[:]), reads=[pid], writes=["rw"])
                        for k4 in range(4):
                            s.op("pe", lambda e, k4=k4: e.transpose(out=pX[1][:, k4 * 128:(k4 + 1) * 128], in_=rw[:, k4 * 128:(k4 + 1) * 128], identity=ident), reads=["rw", "cm"], writes=[("pX", 1)])
                        s.op("dve", lambda e: e.tensor_copy(out=rT[:].rearrange("p a b -> p (a b)"), in_=pX[1][:]), reads=[("pX", 1)], writes=["rT"])
                        f0 = (bi - 2) * 512
                        s.dma(RT_s[f0:f0 + 512, tg * 128:(tg + 1) * 128].rearrange("(a p) t -> p a t", p=128), rT[:], reads=["rT"], writes=["RT_s"], queue="pool")
                    elif bi == 5:
                        s.op("act", lambda e, P=P: e.activation(out=zz[:], in_=P[:], func=AF.Silu), reads=[pid], writes=["zz"])
                        s.op("pool", lambda e: e.tensor_tensor(out=zz[:].rearrange("p (h d) -> p h d", h=4), in0=zz[:].rearrange("p (h d) -> p h d", h=4), in1=gn[:].unsqueeze(1).to_broadcast([128, 4, 128]), op=ALU.mult), reads=["zz", "gn"], writes=["zz"])
                        s.dma(Z_s[i * 128:(i + 1) * 128, :], zz[:], reads=["zz"], writes=["Z_s"], queue="pool")
                    elif bi == 6:
                        s.op("dve", lambda e, P=P: e.tensor_tensor(out=ab[:, 0:8], in0=P[:, 0:8], in1=abc[:, 0, :], op=ALU.add), reads=[pid, "abc"], writes=["ab"])
                        s.op("act", lambda e: e.activation(out=ab[:, 0:8], in_=ab[:, 0:8], func=AF.Exp), reads=["ab"], writes=["ab"])
                        s.op("dve", lambda e: e.tensor_scalar(out=ab[:, 0:8], in0=ab[:, 0:8], scalar1=1.0, scalar2=None, op0=ALU.add), reads=["ab"], writes=["ab"])
                        s.op("act", lambda e: e.activation(out=ab[:, 0:8], in_=ab[:, 0:8], func=AF.Ln), reads=["ab"], writes=["ab"])
                        s.op("dve", lambda e: e.tensor_tensor(out=gbo[:, 0:8], in0=ab[:, 0:8], in1=abc[:, 1, :], op=ALU.mult), reads=["ab", "abc"], writes=["gbo"])
                        s.op("act", lambda e, P=P: e.activation(out=gbo[:, 8:16], in_=P[:, 8:16], func=AF.Sigmoid), reads=[pid], writes=["gbo"])
                        s.dma(GB_s[tg * 128:(tg + 1) * 128, :], gbo[:], reads=["gbo"], writes=["GB_s"], queue="pool")
                    else:
                        gi = bi - 7
                        s.op("dve", lambda e, P=P, gi=gi: e.tensor_tensor(out=gg[:], in0=P[:], in1=bg[:, gi * 512:(gi + 1) * 512], op=ALU.add), reads=[pid, "bg"], writes=["gg"])
                        s.op("act", lambda e: e.activation(out=gg[:], in_=gg[:], func=AF.Sigmoid), reads=["gg"], writes=["gg"])
                        s.dma(GT_s[i * 128:(i + 1) * 128, gi * 512:(gi + 1) * 512], gg[:], reads=["gg"], writes=["GT_s"], queue="pool")
            s.flush()
        stA.close()
        if upto <= 1:
            return nc

        with contextlib.ExitStack() as st:
            cw = T(st, "cw", [128, 12, 5])
            Rt = [T(st, f"Rt{i}", [128, 516]) for i in range(3)]
            acc = [T(st, f"acc{i}", [128, 512]) for i in range(2)]
            y = [T(st, f"y{i}", [128, 512]) for i in range(2)]
            y2 = T(st, "y2", [128, 512]); rn = T(st, "rn", [128, 512]); yn = [T(st, f"yn{i}", [128, 512]) for i in range(2)]
            tok = [T(st, f"tok{i}", [128, 4, 128]) for i in range(2)]
            pN = [PS(st, f"pN{i}", [128, 512]) for i in range(2)]
            pK = [PS(st, f"pK{i}", [128, 512]) for i in range(2)]
            for j in range(5):
                s.dma(cw[:, :, j], conv_d[j, :].rearrange("(fc p) -> p fc", p=128), writes=["cw"], allow_slow_non_contiguous=True)
            it = 0
            segs = [(0, CTX), (CTX, TALL)]
            if "small" in dbg:
                segs = [(0, CTX), (CTX, CTX + 256)]
            for (g0, g1) in segs:
                for t0 in range(g0, g1, 512):
                    n = min(512, g1 - t0)
                    for fc in range(12):
                        R = Rt[it % 3]; rid = ("Rt", it % 3); A = acc[it % 2]; aid = ("acc", it % 2); Y = y[it % 2]; yid = ("y", it % 2)
                        lo = max(t0 - 2, g0); hi = min(t0 + n + 2, g1)
                        if lo > t0 - 2 or hi < t0 + n + 2:
                            s.op("pool", lambda e, R=R: e.memset(R[:], 0.0), writes=[rid])
                        s.dma(R[:, lo - (t0 - 2):hi - (t0 - 2)], RT_s[fc * 128:(fc + 1) * 128, lo:hi], reads=["RT_s"], writes=[rid], queue=("sp", "act")[it % 2])
                        s.op("dve", lambda e, R=R, A=A, fc=fc, n=n: e.tensor_scalar(out=A[:, 0:n], in0=R[:, 0:n], scalar1=cw[:, fc, 0:1], scalar2=None, op0=ALU.mult), reads=[rid, "cw"], writes=[aid])
                        for j in range(1, 5):
                            s.op("dve", lambda e, R=R, A=A, fc=fc, n=n, j=j: e.scalar_tensor_tensor(out=A[:, 0:n], in0=R[:, j:j + n], scalar=cw[:, fc, j:j + 1], in1=A[:, 0:n], op0=ALU.mult, op1=ALU.add), reads=[rid, "cw", aid], writes=[aid])
                        s.op("act", lambda e, A=A, Y=Y, n=n: e.activation(out=Y[:, 0:n], in_=A[:, 0:n], func=AF.Silu), reads=[aid], writes=[yid])
                        src, sid = Y, yid
                        if fc < 8:
                            YN = yn[it % 2]; nid = ("yn", it % 2); P = pN[it % 2]; pid = ("pN", it % 2)
                            s.op("act", lambda e, Y=Y, n=n: e.activation(out=y2[:, 0:n], in_=Y[:, 0:n], func=AF.Square), reads=[yid], writes=["y2"])
                            s.op("pe", lambda e, P=P, n=n: e.matmul(out=P[:, 0:n], lhsT=cm[:, C_ONES, :], rhs=y2[:, 0:n], start=True, stop=True), reads=["cm", "y2"], writes=[pid])
                            s.op("dve", lambda e, P=P, n=n: e.tensor_scalar(out=rn[:, 0:n], in0=P[:, 0:n], scalar1=1e-6, scalar2=None, op0=ALU.add), reads=[pid], writes=["rn"])
                            s.op("act", lambda e, n=n: e.sqrt(out=rn[:, 0:n], in_=rn[:, 0:n]), reads=["rn"], writes=["rn"])
                            s.op("dve", lambda e, n=n: e.reciprocal(out=rn[:, 0:n], in_=rn[:, 0:n]), reads=["rn"], writes=["rn"])
                            sc = float(128 ** -0.5) if fc < 4 else 1.0
                            s.op("dve", lambda e, Y=Y, YN=YN, n=n, sc=sc: e.scalar_tensor_tensor(out=YN[:, 0:n], in0=Y[:, 0:n], scalar=sc, in1=rn[:, 0:n], op0=ALU.mult, op1=ALU.mult), reads=[yid, "rn"], writes=[nid])
                            s.dma(QK_s[fc * 128:(fc + 1) * 128, t0:t0 + n], YN[:, 0:n], reads=[nid], writes=["QK_s"], queue="pool")
                            src, sid = YN, nid
                        if fc >= 4:
                            PK = pK[it % 2]; kid = ("pK", it % 2); TK = tok[it % 2]; tid = ("tok", it % 2)
                            nsb = n // 128
                            for sb in range(nsb):
                                s.op("pe", lambda e, PK=PK, src=src, sb=sb: e.transpose(out=PK[:, sb * 128:(sb + 1) * 128], in_=src[:, sb * 128:(sb + 1) * 128], identity=ident), reads=[sid, "cm"], writes=[kid])
                            s.op("act", lambda e, PK=PK, TK=TK, n=n: e.copy(out=TK[:].rearrange("p a b -> p (a b)")[:, 0:n], in_=PK[:, 0:n]), reads=[kid], writes=[tid])
                            s.dma(KV_s[t0:t0 + n, (fc - 4) * 128:(fc - 3) * 128].rearrange("(sb p) f -> p sb f", p=128), TK[:, 0:nsb, :], reads=[tid], writes=["KV_s"], queue="pool")
                        it += 1
            s.flush()
        if upto <= 2:
            return nc

        with contextlib.ExitStack() as st:
            Sst = [T(st, f"Sst{i}", [128, 4, 128]) for i in range(2)]
            qT4 = T(st, "qT4", [128, 4, 128]); kT4 = T(st, "kT4", [128, 4, 128]); ktok = T(st, "ktok", [128, 4, 128]); vtok = T(st, "vtok", [128, 4, 128])
            gb = T(st, "gb", [128, 16]); sm = T(st, "sm", [128, 16]); ex = T(st, "ex", [128, 16]); beg = T(st, "beg", [128, 4])
            G1 = T(st, "G1", [128, 4, 128]); dl = T(st, "dl", [128, 4, 128]); du = T(st, "du", [128, 4, 128])
            Bm = [T(st, f"Bm{i}", [128, 4, 128]) for i in range(2)]; Cm = [T(st, f"Cm{i}", [128, 4, 128]) for i in range(2)]; Pm = [T(st, f"Pm{i}", [128, 4, 128]) for i in range(2)]
            aT = T(st, "aT", [128, 4, 128]); kbg = T(st, "kbg", [128, 4, 128]); vb = T(st, "vb", [128, 4, 128]); ktl = T(st, "ktl", [128, 4, 128])
            WT = T(st, "WT", [128, 4, 128]); U = T(st, "U", [128, 4, 128]); vn = T(st, "vn", [128, 4, 128]); o1 = T(st, "o1", [128, 4, 128]); ot = T(st, "ot", [128, 4, 128])
            pb = [PS(st, f"pb{i}", [128, 4, 128]) for i in range(8)]
            pA, pB_, pC, pD, pE, pF, pG, pH = pb
            pid = lambda k: ("pb", k)
            H4 = [128, 4, 128]
            bc_h = lambda ap2: ap2.unsqueeze(1).to_broadcast(H4)
            bc_l = lambda ap2: ap2.unsqueeze(2).to_broadcast(H4)
            ntl = (2 if "small" in dbg else NT)
            for dr in range(2):
                M1 = cm[:, C_M1F if dr == 0 else C_M1B, :]; NM1 = cm[:, C_NM1F if dr == 0 else C_NM1B, :]
                NB = cm[:, C_NLOW if dr == 0 else C_NUP, :]; NTm = cm[:, C_NUP if dr == 0 else C_NLOW, :]
                STR = cm[:, C_SLOW if dr == 0 else C_SUP, :]
                SS = Sst[dr]; ssid = ("Sst", dr)
                s.op("pool", lambda e, SS=SS: e.memset(SS[:], 0.0), writes=[ssid])
                order = [("c", i) for i in range(CTX // 128)] + [("l", i) for i in range(ntl)]
                if dr == 1:
                    order = [("c", i) for i in reversed(range(CTX // 128))] + [("l", i) for i in reversed(range(ntl))]
                for (kind, i) in order:
                    lat = kind == "l"
                    tg = i if not lat else CTX // 128 + i
                    tsl = slice(tg * 128, (tg + 1) * 128)
                    s.dma(qT4[:], QK_s[0:512, tsl].rearrange("(h p) t -> p h t", p=128), reads=["QK_s"], writes=["qT4"])
                    s.dma(kT4[:], QK_s[512:1024, tsl].rearrange("(h p) t -> p h t", p=128), reads=["QK_s"], writes=["kT4"], queue="act")
                    s.dma(ktok[:].rearrange("p h d -> p (h d)"), KV_s[tsl, 0:512], reads=["KV_s"], writes=["ktok"])
                    s.dma(vtok[:].rearrange("p h d -> p (h d)"), KV_s[tsl, 512:1024], reads=["KV_s"], writes=["vtok"], queue="act")
                    s.dma(gb[:], GB_s[tsl, :], reads=["GB_s"], writes=["gb"])
                    g = gb[:, dr * 4:dr * 4 + 4]; beta = gb[:, 8 + dr * 4:12 + dr * 4]
                    pAf = pA[:].rearrange("p a b -> p (a b)")
                    for k, L in enumerate((M1, cm[:, C_SAME, :], cm[:, C_SEL0, :], cm[:, C_SEL1, :])):
                        s.op("pe", lambda e, k=k, L=L, g=g: e.matmul(out=pAf[:, 4 * k:4 * k + 4], lhsT=L, rhs=g, start=True, stop=True), reads=["cm", "gb"], writes=[pid(0)])
                    s.op("dve", lambda e: e.tensor_copy(out=sm[:], in_=pAf[:, 0:16]), reads=[pid(0)], writes=["sm"])
                    s.op("dve", lambda e: e.tensor_tensor(out=sm[:, 4:8], in0=sm[:, 4:8], in1=sm[:, 0:4], op=ALU.subtract), reads=["sm"], writes=["sm"])
                    s.op("act", lambda e: e.activation(out=ex[:], in_=sm[:], func=AF.Exp), reads=["sm"], writes=["ex"])
                    s.op("dve", lambda e, beta=beta: e.tensor_tensor(out=beg[:], in0=ex[:, 0:4], in1=beta, op=ALU.mult), reads=["ex", "gb"], writes=["beg"])
                    s.op("dve", lambda e, g=g: e.tensor_tensor(out=G1[:], in0=bc_h(cm[:, C_SAME, :]), in1=bc_l(g), op=ALU.mult), reads=["cm", "gb"], writes=["G1"])
                    for hh in range(4):
                        s.op("pe", lambda e, hh=hh, M1=M1: e.matmul(out=pB_[:, hh, :], lhsT=M1, rhs=G1[:, hh, :], start=True, stop=False), reads=["cm", "G1"], writes=[pid(1)])
                        s.op("pe", lambda e, hh=hh, NM1=NM1: e.matmul(out=pB_[:, hh, :], lhsT=G1[:, hh, :], rhs=NM1, start=False, stop=True), reads=["cm", "G1"], writes=[pid(1)])
                    s.op("dve", lambda e, NB=NB: e.tensor_tensor(out=dl[:], in0=pB_[:], in1=bc_h(NB), op=ALU.add), reads=[pid(1), "cm"], writes=["dl"])
                    s.op("dve", lambda e, NTm=NTm: e.scalar_tensor_tensor(out=du[:], in0=pB_[:], scalar=-1.0, in1=bc_h(NTm), op0=ALU.mult, op1=ALU.add), reads=[pid(1), "cm"], writes=["du"])
                    s.op("act", lambda e: e.activation(out=dl[:], in_=dl[:], func=AF.Exp), reads=["dl"], writes=["dl"])
                    s.op("act", lambda e: e.activation(out=du[:], in_=du[:], func=AF.Exp), reads=["du"], writes=["du"])
                    for hh in range(4):
                        s.op("pe", lambda e, hh=hh: e.matmul(out=pC[:, hh, :], lhsT=kT4[:, hh, :], rhs=kT4[:, hh, :], start=True, stop=True), reads=["kT4"], writes=[pid(2)])
                    for hh in range(4):
                        s.op("pe", lambda e, hh=hh: e.matmul(out=pD[:, hh, :], lhsT=kT4[:, hh, :], rhs=qT4[:, hh, :], start=True, stop=True), reads=["kT4", "qT4"], writes=[pid(3)])
                    B0 = Bm[0]; C0 = Cm[0]; P0 = Pm[0]
                    s.op("dve", lambda e: e.tensor_tensor(out=B0[:], in0=pC[:], in1=dl[:], op=ALU.mult), reads=[pid(2), "dl"], writes=[("Bm", 0)])
                    s.op("pool", lambda e, STR=STR: e.tensor_tensor(out=B0[:], in0=B0[:], in1=bc_h(STR), op=ALU.mult), reads=[("Bm", 0), "cm"], writes=[("Bm", 0)])
                    s.op("pool", lambda e, beta=beta: e.tensor_tensor(out=B0[:], in0=B0[:], in1=bc_l(beta), op=ALU.mult), reads=[("Bm", 0), "gb"], writes=[("Bm", 0)])
                    s.op("dve", lambda e: e.tensor_tensor(out=aT[:], in0=pD[:], in1=du[:], op=ALU.mult), reads=[pid(3), "du"], writes=["aT"])
                    for hh in range(4):
                        s.op("pe", lambda e, hh=hh: e.transpose(out=pE[:, hh, :], in_=B0[:, hh, :], identity=ident), reads=[("Bm", 0), "cm"], writes=[pid(4)])
                    s.op("act", lambda e: e.copy(out=C0[:], in_=pE[:]), reads=[pid(4)], writes=[("Cm", 0)])
                    s.op("dve", lambda e: e.tensor_tensor(out=P0[:], in0=C0[:], in1=bc_h(ident), op=ALU.add), reads=[("Cm", 0), "cm"], writes=[("Pm", 0)])
                    cur = 0
                    for lv in range(1, 6):
                        nx = 1 - cur
                        Bc, Cc, Pc = Bm[cur], Cm[cur], Pm[cur]; Bn, Cn, Pn = Bm[nx], Cm[nx], Pm[nx]
                        for hh in range(4):
                            s.op("pe", lambda e, hh=hh, Bc=Bc, Cc=Cc: e.matmul(out=pF[:, hh, :], lhsT=Cc[:, hh, :], rhs=Bc[:, hh, :], start=True, stop=True), reads=[("Bm", cur), ("Cm", cur)], writes=[pid(5)])
                        s.op("act", lambda e, Bn=Bn: e.copy(out=Bn[:], in_=pF[:]), reads=[pid(5)], writes=[("Bm", nx)])
                        if lv < 5:
                            for hh in range(4):
                                s.op("pe", lambda e, hh=hh, Bc=Bc, Cc=Cc: e.matmul(out=pG[:, hh, :], lhsT=Bc[:, hh, :], rhs=Cc[:, hh, :], start=True, stop=True), reads=[("Bm", cur), ("Cm", cur)], writes=[pid(6)])
                            s.op("dve", lambda e, Cn=Cn: e.tensor_copy(out=Cn[:], in_=pG[:]), reads=[pid(6)], writes=[("Cm", nx)])
                        for hh in range(4):
                            s.op("pe", lambda e, hh=hh, Pc=Pc: e.matmul(out=pH[:, hh, :], lhsT=ident, rhs=Pc[:, hh, :], start=True, stop=False), reads=[("Pm", cur), "cm"], writes=[pid(7)])
                            s.op("pe", lambda e, hh=hh, Pc=Pc, Bn=Bn: e.matmul(out=pH[:, hh, :], lhsT=Bn[:, hh, :], rhs=Pc[:, hh, :], start=False, stop=True), reads=[("Pm", cur), ("Bm", nx)], writes=[pid(7)])
                        s.op("dve", lambda e, Pn=Pn: e.tensor_copy(out=Pn[:], in_=pH[:]), reads=[pid(7)], writes=[("Pm", nx)])
                        cur = nx
                    TT = Pm[cur]; ttid = ("Pm", cur)
                    s.op("pool", lambda e: e.tensor_tensor(out=kbg[:], in0=ktok[:], in1=bc_l(beg[:]), op=ALU.mult), reads=["ktok", "beg"], writes=["kbg"])
                    s.op("pool", lambda e, beta=beta: e.tensor_tensor(out=vb[:], in0=vtok[:], in1=bc_l(beta), op=ALU.mult), reads=["vtok", "gb"], writes=["vb"])
                    s.op("pool", lambda e: e.tensor_tensor(out=ktl[:], in0=ktok[:], in1=bc_l(ex[:, 4:8]), op=ALU.mult), reads=["ktok", "ex"], writes=["ktl"])
                    for hh in range(4):
                        s.op("pe", lambda e, hh=hh, TT=TT: e.matmul(out=pE[:, hh, :], lhsT=kbg[:, hh, :], rhs=TT[:, hh, :], start=True, stop=True), reads=["kbg", ttid], writes=[pid(4)])
                    s.op("act", lambda e: e.copy(out=WT[:], in_=pE[:]), reads=[pid(4)], writes=["WT"])
                    for hh in range(4):
                        s.op("pe", lambda e, hh=hh, TT=TT: e.matmul(out=pF[:, hh, :], lhsT=TT[:, hh, :], rhs=vb[:, hh, :], start=True, stop=True), reads=["vb", ttid], writes=[pid(5)])
                    s.op("dve", lambda e: e.tensor_copy(out=U[:], in_=pF[:]), reads=[pid(5)], writes=["U"])
                    for c in ((0, 1) if dr == 0 else (1, 0)):
                        pr = slice(64 * c, 64 * c + 64)
                        for hh in range(4):
                            s.op("pe", lambda e, hh=hh, SS=SS: e.matmul(out=pG[:, hh, :], lhsT=WT[:, hh, :], rhs=SS[:, hh, :], start=True, stop=True), reads=["WT", ssid], writes=[pid(6)])
                        s.op("dve", lambda e, pr=pr: e.tensor_tensor(out=vn[pr], in0=U[pr], in1=pG[pr], op=ALU.subtract), reads=["U", pid(6)], writes=["vn"])
                        for hh in range(4):
                            s.op("pe", lambda e, hh=hh, SS=SS: e.matmul(out=pH[:, hh, :], lhsT=qT4[:, hh, :], rhs=SS[:, hh, :], start=True, stop=True), reads=["qT4", ssid], writes=[pid(7)])
                        for hh in range(4):
                            s.op("pe", lambda e, hh=hh, pr=pr: e.matmul(out=pC[:, hh, :], lhsT=aT[pr, hh, :], rhs=vn[pr, hh, :], start=True, stop=True), reads=["aT", "vn"], writes=[pid(2)])
                        for hh in range(4):
                            s.op("pe", lambda e, hh=hh, pr=pr: e.matmul(out=pD[:, hh, :], lhsT=ktl[pr, hh, :], rhs=vn[pr, hh, :], start=True, stop=True), reads=["ktl", "vn"], writes=[pid(3)])
                        if lat:
                            s.op("dve", lambda e, pr=pr: e.tensor_tensor(out=o1[pr], in0=pH[pr], in1=bc_l(ex[:, 0:4])[pr], op=ALU.mult), reads=[pid(7), "ex"], writes=["o1"])
                            s.op("dve", lambda e, pr=pr: e.tensor_tensor(out=ot[pr], in0=o1[pr], in1=pC[pr], op=ALU.add), reads=["o1", pid(2)], writes=["ot"])
                        s.op("pool", lambda e, c=c, SS=SS: e.tensor_tensor(out=SS[:], in0=SS[:], in1=bc_l(ex[:, 8 + 4 * c:12 + 4 * c]), op=ALU.mult), reads=[ssid, "ex", pid(6), pid(7)], writes=[ssid])
                        s.op("dve", lambda e, SS=SS: e.tensor_tensor(out=SS[:], in0=SS[:], in1=pD[:], op=ALU.add), reads=[ssid, pid(3)], writes=[ssid])
                    if lat:
                        s.dma(OD_s[dr, i * 128:(i + 1) * 128, :], ot[:].rearrange("p h d -> p (h d)"), reads=["ot"], writes=["OD_s"], queue="pool")
            s.flush()
        if upto <= 3:
            return nc

        with contextlib.ExitStack() as st:
            kt = [T(st, f"kt{i}", [64, 2, 384]) for i in range(2)]
            vt = [T(st, f"vt{i}", [128, 3, 130]) for i in range(2)]
            ktc = T(st, "ktc", [64, 2, 256]); vtc = T(st, "vtc", [128, 2, 130])
            qt = [T(st, f"qt{i}", [64, 8, 128]) for i in range(2)]
            E = [T(st, f"E{i}", [128, 5, 512]) for i in range(2)]
            esink = T(st, "esink", [128, 8]); den = T(st, "den", [128, 8]); oa = [T(st, f"oa{i}", [128, 512]) for i in range(2)]
            pS = [PS(st, f"pS{i}", [128, 512]) for i in range(3)]
            pO = [PS(st, f"pO{i}", [128, 4, 65]) for i in range(2)]
            s.dma(ktc[:], KT_s[:, :, 0:CTX], reads=["KT_s"], writes=["ktc"])
            s.dma(vtc[:], V_s[0:CTX].rearrange("(b p) g d -> p b (g d)", p=128), reads=["V_s"], writes=["vtc"])
            s.dma(esink[:], sink_d.partition_broadcast(128), writes=["esink"])
            s.op("act", lambda e: e.activation(out=esink[:], in_=esink[:], func=AF.Exp), reads=["esink"], writes=["esink"])
            ntl = (2 if "small" in dbg else NT)
            nS = 0
            for i in range(ntl):
                lo = max(i - 1, 0); hi = min(i + 1, ntl - 1); nb = hi - lo + 1
                KT_ = kt[i % 2]; VT_ = vt[i % 2]; QT_ = qt[i % 2]; OA = oa[i % 2]
                s.dma(KT_[:, :, 0:nb * 128], KT_s[:, :, CTX + lo * 128:CTX + (hi + 1) * 128], reads=["KT_s"], writes=[("kt", i % 2)])
                s.dma(VT_[:, 0:nb, :], V_s[CTX + lo * 128:CTX + (hi + 1) * 128].rearrange("(b p) g d -> p b (g d)", p=128), reads=["V_s"], writes=[("vt", i % 2)], queue="act")
                s.dma(QT_[:], QT_s[:, :, i * 128:(i + 1) * 128], reads=["QT_s"], writes=[("qt", i % 2)])
                for g in range(2):
                    Eg = E[g]; eid = ("E", g)
                    kb = [("l", j - lo, (C_WPREV if j < i else (C_WNEXT if j > i else None))) for j in range(lo, hi + 1)] + [("c", 0, None), ("c", 1, None)]
                    for bi, (kk, bl, msk) in enumerate(kb):
                        P = pS[nS % 3]; psid = ("pS", nS % 3); nS += 1
                        lhs = KT_[:, g, bl * 128:(bl + 1) * 128] if kk == "l" else ktc[:, g, bl * 128:(bl + 1) * 128]
                        s.op("pe", lambda e, P=P, lhs=lhs, QT_=QT_, g=g: e.matmul(out=P[:].rearrange("p (h q) -> p h q", h=4), lhsT=lhs, rhs=QT_[:, 4 * g:4 * g + 4, :], start=True, stop=True),
                             reads=[("kt", i % 2), "ktc", ("qt", i % 2)], writes=[psid])
                        s.op("act", lambda e, P=P, Eg=Eg, bi=bi: e.activation(out=Eg[:, bi, :], in_=P[:], func=AF.Exp), reads=[psid], writes=[eid])
                        if msk is not None:
                            s.op("dve", lambda e, Eg=Eg, bi=bi, msk=msk: e.tensor_tensor(out=Eg[:, bi, :].rearrange("p (h q) -> p h q", h=4), in0=Eg[:, bi, :].rearrange("p (h q) -> p h q", h=4),
                                                                              in1=cm[:, msk, :].unsqueeze(1).to_broadcast([128, 4, 128]), op=ALU.mult), reads=[eid, "cm"], writes=[eid])
                    for hh in range(4):
                        for bi, (kk, bl, msk) in enumerate(kb):
                            rhs = VT_[:, bl, g * 65:(g + 1) * 65] if kk == "l" else vtc[:, bl, g * 65:(g + 1) * 65]
                            s.op("pe", lambda e, Eg=Eg, bi=bi, hh=hh, rhs=rhs, g=g, last=(bi == len(kb) - 1): e.matmul(out=pO[g][:, hh, :], lhsT=Eg[:, bi, hh * 128:(hh + 1) * 128], rhs=rhs, start=(bi == 0), stop=last),
                                 reads=[eid, ("vt", i % 2), "vtc"], writes=[("pO", g)])
                    s.op("dve", lambda e, g=g: e.tensor_tensor(out=den[:, 4 * g:4 * g + 4], in0=pO[g][:, :, 64], in1=esink[:, 4 * g:4 * g + 4], op=ALU.add), reads=[("pO", g), "esink"], writes=["den"])
                    s.op("dve", lambda e, g=g: e.reciprocal(out=den[:, 4 * g:4 * g + 4], in_=den[:, 4 * g:4 * g + 4]), reads=["den"], writes=["den"])
                    s.op("dve", lambda e, g=g, OA=OA: e.tensor_tensor(out=OA[:, g * 256:(g + 1) * 256].rearrange("p (h d) -> p h d", h=4), in0=pO[g][:, :, 0:64],
                                                              in1=den[:, 4 * g:4 * g + 4].unsqueeze(2).to_broadcast([128, 4, 64]), op=ALU.mult), reads=[("pO", g), "den"], writes=[("oa", i % 2)])
                s.dma(OA_s[i * 128:(i + 1) * 128, :], OA[:], reads=[("oa", i % 2)], writes=["OA_s"], queue="pool")
            s.flush()
        if upto <= 5:
            return nc

        UTr_s = nc.dram_tensor("UTr_s", [D, 16384], F32R, kind="Internal").ap()
        Vr_s = nc.dram_tensor("Vr_s", [16384, D], F32R, kind="Internal").ap()
        with contextlib.ExitStack() as st:
            cvb = [st.enter_context(nc.sbuf_tensor(_nm("cvb"), [128, 4096], F32R)) for _ in range(3)]
            ci = 0
            for r0 in range(0, D, 128):
                for c0 in range(0, 16384, 4096):
                    k = ci % 3; ci += 1
                    s.dma(cvb[k][:], pu_d[r0:r0 + 128, c0:c0 + 4096], writes=[("cvb", k)], queue="pool")
                    s.dma(UTr_s[r0:r0 + 128, c0:c0 + 4096], cvb[k][:], reads=[("cvb", k)], writes=["UTr_s"], queue=("sp", "act")[ci % 2])
            for r0 in range(0, 16384, 512):
                k = ci % 3; ci += 1
                s.dma(cvb[k][:], pv_d[r0:r0 + 512, :].rearrange("(p a) n -> p (a n)", p=128), writes=[("cvb", k)], queue="pool")
                s.dma(Vr_s[r0:r0 + 512, :].rearrange("(p a) n -> p (a n)", p=128), cvb[k][:], reads=[("cvb", k)], writes=["Vr_s"], queue=("sp", "act")[ci % 2])
            s.flush()
        if "stopconv" in dbg:
            return nc
        GI = 2
        NG = 128 // GI
        NB = 2
        with contextlib.ExitStack() as st:
            TR = lambda name, shape: st.enter_context(nc.sbuf_tensor(_nm(name), list(shape), F32R))
            xa = [T(st, f"xa{t}", [128, D]) for t in range(NB)]
            yb = T(st, "yb", [128, D]); tc_ = T(st, "tc_", [128, 8, 128]); gt = T(st, "gt", [128, 2048])
            h2r = [TR(f"h2r{t}", [128, 8, 128]) for t in range(NB)]
            od = T(st, "od", [128, 2, 512]); zt = T(st, "zt", [128, 512]); oat = T(st, "oat", [128, 512]); o2 = T(st, "o2", [128, 512])
            qsb = T(st, "qsb", [128, D]); qTs = T(st, "qTs", [64, 16, 128])
            sc = [T(st, f"sc{t}", [128, 16, 128]) for t in range(NB)]
            ssq = T(st, "ssq", [128, 4]); ss = T(st, "ss6", [128, 1]); rstd = T(st, "rstd6", [128, 1])
            t16 = T(st, "t16", [128, 2, 16]); c16 = T(st, "c16", [128, 16]); cand = T(st, "cand", [128, 16, 16]); wk = T(st, "wk", [128, 256])
            thr = T(st, "thr", [128, 8]); negm = T(st, "negm", [128, 8]); Zs = T(st, "Zs", [128, 8]); kap = T(st, "kap", [128, 8]); e16 = T(st, "e16", [128, 16])
            m1 = T(st, "m1", [128, 8]); th2 = T(st, "th2", [128, 8])
            dg = [TR(f"dg{t}", [128, 8, 128]) for t in range(NB)]
            keysT = T(st, "keysT", [64, 16, 128])
            big = T(st, "big", [128, 8 * D])
            wS = big[:].rearrange("p (k n) -> p k n", k=8)
            UT = [TR(f"UT{i}", [128, 8, GI * 128]) for i in range(2)]
            VG = [TR(f"VG{i}", [128, GI, D]) for i in range(2)]
            pe_ = [big[:, i * 4096:(i + 1) * 4096].rearrange("p (t h k) -> p t h k", t=NB, h=8) for i in range(2)]
            _Mr = TR("Mr", [128, NB, 8, GI * 128])
            Mr = [_Mr, _Mr]
            W5 = NB * GI * 128
            assert W5 == 512
            xs = gt[:, 0:512]; ga = gt[:, 512:1024]; g1 = gt[:, 1024:1536]; Pm_ = gt[:, 1536:2048]
            PT = [TR(f"PT{i}", [128, NB * GI, 128]) for i in range(2)]
            bk = [PS(st, f"bk{i}", [128, 512]) for i in range(8)]
            B = lambda k: ("bk", k)
            pT = [bk[0], bk[1]]; pY = [bk[2], bk[3]]
            s.dma(keysT[:], pkeys_d.rearrange("h p d k -> d (h p) k"), writes=["keysT"])

            def transpose8(src, sid, nkc, dst_off=0, extra=None):
                for kc in range(nkc):
                    b = (dst_off + kc) // 4
                    s.op("pe", lambda e, kc=kc, b=b: e.transpose(out=pT[b][:, ((dst_off + kc) % 4) * 128:((dst_off + kc) % 4 + 1) * 128], in_=src[:, kc * 128:(kc + 1) * 128], identity=ident), reads=[sid, "cm"], writes=[B(b)])
                for b in sorted(set((dst_off + kc) // 4 for kc in range(nkc))):
                    if b == 0:
                        s.op("act", lambda e, b=b: e.copy(out=tc_[:, b * 4:(b + 1) * 4, :].rearrange("p a b -> p (a b)"), in_=pT[b][:]), reads=[B(b)], writes=[("tc", b)])
                    else:
                        s.op("dve", lambda e, b=b: e.tensor_copy(out=tc_[:, b * 4:(b + 1) * 4, :].rearrange("p a b -> p (a b)"), in_=pT[b][:]), reads=[B(b)], writes=[("tc", b)])
                    if extra is not None:
                        dst, did = extra
                        if b == 0:
                            s.op("dve", lambda e, b=b, dst=dst: e.tensor_copy(out=dst[:, b * 4:(b + 1) * 4, :].rearrange("p a b -> p (a b)"), in_=pT[b][:]), reads=[B(b)], writes=[did])
                        else:
                            s.op("act", lambda e, b=b, dst=dst: e.copy(out=dst[:, b * 4:(b + 1) * 4, :].rearrange("p a b -> p (a b)"), in_=pT[b][:]), reads=[B(b)], writes=[did])

            def rms(src, sid):
                s.op("act", lambda e: e.activation(out=qsb[:], in_=src[:], func=AF.Square, accum_out=ss[:]), reads=[sid], writes=["qsb", "ss6"])
                s.op("dve", lambda e: e.tensor_scalar(out=rstd[:], in0=ss[:], scalar1=1.0 / D, scalar2=1e-6, op0=ALU.mult, op1=ALU.add), reads=["ss6"], writes=["rstd6"])
                s.op("act", lambda e: e.sqrt(out=rstd[:], in_=rstd[:]), reads=["rstd6"], writes=["rstd6"])
                s.op("dve", lambda e: e.reciprocal(out=rstd[:], in_=rstd[:]), reads=["rstd6"], writes=["rstd6"])

            nblk = (1 if "small" in dbg else NT // NB)
            _c = [int(x[3:]) for x in dbg if x.startswith("cut")]
            cut = _c[0] if _c else 99
            ngr = (2 if "small2" in dbg else NG)
            gcount = 0
            for blk in range(nblk):
              for tau in range(NB):
                i = blk * NB + tau
                XA = xa[tau]; xid = ("xa", tau); SC = sc[tau]; scid = ("sc", tau)
                tsl = slice(i * 128, (i + 1) * 128)
                s.dma(XA[:], x_d[tsl, :], writes=[xid])
                s.dma(od[:, 0, :], OD_s[0, tsl, :], reads=["OD_s"], writes=["od"], queue="act")
                s.dma(od[:, 1, :], OD_s[1, tsl, :], reads=["OD_s"], writes=["od"], queue="act")
                s.dma(zt[:], Z_s[tsl, :], reads=["Z_s"], writes=["zt"])
                s.dma(oat[:], OA_s[tsl, :], reads=["OA_s"], writes=["oat"], queue="act")
                s.dma(gt[:], GT_s[tsl, :], reads=["GT_s"], writes=[("gtq", 0), ("gtq", 1), ("gtq", 2), ("gtq", 3)])
                s.dma(wS[:, 0:4, :], wba_d.rearrange("(kc p) n -> p kc n", p=128), writes=[("pe", 0), ("pe", 1)])
                s.dma(wS[:, 4:8, :], wbd_d.rearrange("(kc p) n -> p kc n", p=128), writes=[("pe", 0), ("pe", 1)], queue="act")
                s.op("dve", lambda e: e.tensor_tensor(out=od[:, 0, :], in0=od[:, 0, :], in1=od[:, 1, :], op=ALU.add), reads=["od"], writes=["od"])
                s.op("dve", lambda e: e.tensor_tensor(out=o2[:], in0=od[:, 0, :], in1=od[:, 0, :], op=ALU.mult), reads=["od"], writes=["o2"])
                s.op("dve", lambda e: e.tensor_reduce(out=ssq[:], in_=o2[:].rearrange("p (h d) -> p h d", h=4), axis=AX.X, op=ALU.add), reads=["o2"], writes=["ssq"])
                s.op("dve", lambda e: e.tensor_scalar(out=ssq[:], in0=ssq[:], scalar1=1.0 / 128, scalar2=1e-6, op0=ALU.mult, op1=ALU.add), reads=["ssq"], writes=["ssq"])
                s.op("act", lambda e: e.sqrt(out=ssq[:], in_=ssq[:]), reads=["ssq"], writes=["ssq"])
                s.op("dve", lambda e: e.reciprocal(out=ssq[:], in_=ssq[:]), reads=["ssq"], writes=["ssq"])
                s.op("dve", lambda e: e.tensor_tensor(out=o2[:].rearrange("p (h d) -> p h d", h=4), in0=od[:, 0, :].rearrange("p (h d) -> p h d", h=4), in1=ssq[:].unsqueeze(2).to_broadcast([128, 4, 128]), op=ALU.mult), reads=["od", "ssq"], writes=["o2"])
                s.op("dve", lambda e: e.tensor_tensor(out=o2[:], in0=o2[:], in1=zt[:], op=ALU.mult), reads=["o2", "zt"], writes=["o2"])
                transpose8(oat, "oat", 4, 0)
                transpose8(o2, "o2", 4, 4)
                for half in range(2):
                    for kc in range(4):
                        s.op("pe", lambda e, half=half, kc=kc: e.matmul(out=pY[half][:], lhsT=tc_[:, kc, :], rhs=wS[:, kc, half * 512:(half + 1) * 512], start=(kc == 0), stop=(kc == 3)), reads=[("tc", 0), ("pe", 0), ("pe", 1)], writes=[B(2 + half)])
                    s.op("dve", lambda e, half=half: e.tensor_tensor(out=yb[:, half * 512:(half + 1) * 512], in0=pY[half][:], in1=gt[:, half * 512:(half + 1) * 512], op=ALU.mult), reads=[B(2 + half), ("gtq", half)], writes=["yb"])
                for half in range(2):
                    for kc in range(4):
                        s.op("pe", lambda e, half=half, kc=kc: e.matmul(out=pY[half][:], lhsT=tc_[:, 4 + kc, :], rhs=wS[:, 4 + kc, half * 512:(half + 1) * 512], start=(kc == 0), stop=(kc == 3)), reads=[("tc", 1), ("pe", 0), ("pe", 1)], writes=[B(2 + half)])
                    s.op("dve", lambda e, half=half: e.tensor_tensor(out=qsb[:, half * 512:(half + 1) * 512], in0=pY[half][:], in1=gt[:, 1024 + half * 512:1024 + (half + 1) * 512], op=ALU.mult), reads=[B(2 + half), ("gtq", 2 + half)], writes=["qsb"])
                s.op("pool", lambda e: e.tensor_tensor(out=yb[:], in0=yb[:], in1=qsb[:], op=ALU.add), reads=["yb", "qsb"], writes=["yb"])
                s.dma(wS[:], wout_d.rearrange("(kc p) n -> p kc n", p=128), writes=[("pe", 0), ("pe", 1)])
                transpose8(yb, "yb", 8, 0)
                for half in range(2):
                    for kc in range(8):
                        s.op("pe", lambda e, half=half, kc=kc: e.matmul(out=pY[half][:], lhsT=tc_[:, kc, :], rhs=wS[:, kc, half * 512:(half + 1) * 512], start=(kc == 0), stop=(kc == 7)), reads=[("tc", 0), ("tc", 1), ("pe", 0), ("pe", 1)], writes=[B(2 + half)])
                    s.op("dve", lambda e, half=half: e.tensor_tensor(out=yb[:, half * 512:(half + 1) * 512], in0=pY[half][:], in1=bvv(BV_GT1)[:, half * 512:(half + 1) * 512], op=ALU.mult), reads=[B(2 + half), "bv"], writes=["yb"])
                s.op("pool", lambda e, XA=XA: e.tensor_tensor(out=XA[:], in0=XA[:], in1=yb[:], op=ALU.add), reads=[xid, "yb"], writes=[xid])
                s.dma(wS[:], pwq_d.rearrange("(kc p) n -> p kc n", p=128), writes=[("pe", 0), ("pe", 1)])
                rms(XA, xid)
                s.op("dve", lambda e, XA=XA: e.scalar_tensor_tensor(out=yb[:], in0=XA[:], scalar=rstd[:, 0:1], in1=bvv(BV_G2)[:, :], op0=ALU.mult, op1=ALU.mult), reads=[xid, "rstd6", "bv"], writes=["yb"])
                s.op("pool", lambda e: e.tensor_tensor(out=yb[:], in0=yb[:], in1=bvv(BV_SH2)[:, :], op=ALU.add), reads=["yb", "bv"], writes=["yb"])
                transpose8(yb, "yb", 8, 0, extra=(h2r[tau], ("h2r", tau)))
                for half in range(2):
                    for kc in range(8):
                        s.op("pe", lambda e, half=half, kc=kc: e.matmul(out=pY[half][:], lhsT=tc_[:, kc, :], rhs=wS[:, kc, half * 512:(half + 1) * 512], start=(kc == 0), stop=(kc == 7)), reads=[("tc", 0), ("tc", 1), ("pe", 0), ("pe", 1)], writes=[B(2 + half)])
                    if half == 0:
                        s.op("act", lambda e: e.copy(out=qsb[:, 0:512], in_=pY[0][:]), reads=[B(2)], writes=["qsb"])
                    else:
                        s.op("dve", lambda e: e.tensor_copy(out=qsb[:, 512:1024], in_=pY[1][:]), reads=[B(3)], writes=["qsb"])
                for rd in range(4):
                    b = rd % 2
                    for k4 in range(4):
                        hp = rd * 4 + k4
                        s.op("pe", lambda e, hp=hp, b=b, k4=k4: e.transpose(out=pT[b][0:64, k4 * 128:(k4 + 1) * 128], in_=qsb[:, hp * 64:(hp + 1) * 64], identity=ident), reads=["qsb", "cm"], writes=[B(b)])
                    if b == 0:
                        s.op("act", lambda e, rd=rd, b=b: e.copy(out=qTs[:, rd * 4:(rd + 1) * 4, :].rearrange("p a b -> p (a b)"), in_=pT[b][0:64, :]), reads=[B(b)], writes=["qTs"])
                    else:
                        s.op("dve", lambda e, rd=rd, b=b: e.tensor_copy(out=qTs[:, rd * 4:(rd + 1) * 4, :].rearrange("p a b -> p (a b)"), in_=pT[b][0:64, :]), reads=[B(b)], writes=["qTs"])
                for rd in range(4):
                    b = rd % 2
                    for k4 in range(4):
                        hp = rd * 4 + k4
                        s.op("pe", lambda e, hp=hp, b=b, k4=k4: e.matmul(out=pY[b][:, k4 * 128:(k4 + 1) * 128], lhsT=qTs[:, hp, :], rhs=keysT[:, hp, :], start=True, stop=True), reads=["qTs", "keysT"], writes=[B(2 + b)])
                    if b == 0:
                        s.op("act", lambda e, rd=rd, b=b, SC=SC: e.copy(out=SC[:, rd * 4:(rd + 1) * 4, :].rearrange("p a b -> p (a b)"), in_=pY[b][:]), reads=[B(2 + b)], writes=[scid])
                    else:
                        s.op("dve", lambda e, rd=rd, b=b, SC=SC: e.tensor_copy(out=SC[:, rd * 4:(rd + 1) * 4, :].rearrange("p a b -> p (a b)"), in_=pY[b][:]), reads=[B(2 + b)], writes=[scid])
                for hh in range(8):
                    for p in range(2):
                        srow = SC[:, 2 * hh + p, :]
                        s.op("dve", lambda e, p=p, srow=srow: e.max(out=t16[:, p, 0:8], in_=srow), reads=[scid], writes=["t16"])
                        s.op("dve", lambda e, p=p, srow=srow: e.match_replace(out=wk[:, 0:128], in_to_replace=t16[:, p, 0:8], in_values=srow, imm_value=-1e30), reads=[scid, "t16"], writes=["wk"])
                        s.op("dve", lambda e, p=p: e.max(out=t16[:, p, 8:16], in_=wk[:, 0:128]), reads=["wk"], writes=["t16"])
                    s.op("dve", lambda e: e.tensor_tensor(out=cand[:], in0=t16[:, 0, :].unsqueeze(2).to_broadcast([128, 16, 16]), in1=t16[:, 1, :].unsqueeze(1).to_broadcast([128, 16, 16]), op=ALU.add), reads=["t16"], writes=["cand"])
                    cf = cand[:].rearrange("p a b -> p (a b)")
                    s.op("dve", lambda e, cf=cf: e.max(out=c16[:, 0:8], in_=cf), reads=["cand"], writes=["c16"])
                    s.op("dve", lambda e, cf=cf: e.match_replace(out=wk[:], in_to_replace=c16[:, 0:8], in_values=cf, imm_value=-1e30), reads=["cand", "c16"], writes=["wk"])
                    s.op("dve", lambda e: e.max(out=c16[:, 8:16], in_=wk[:]), reads=["wk"], writes=["c16"])
                    s.op("dve", lambda e, hh=hh: e.tensor_scalar(out=thr[:, hh:hh + 1], in0=c16[:, 15:16], scalar1=-1e-4, scalar2=None, op0=ALU.add), reads=["c16"], writes=["thr"])
                    s.op("dve", lambda e, hh=hh: e.tensor_scalar(out=negm[:, hh:hh + 1], in0=c16[:, 0:1], scalar1=-1.0, scalar2=None, op0=ALU.mult), reads=["c16"], writes=["negm"])
                    s.op("dve", lambda e, hh=hh: e.tensor_copy(out=m1[:, hh:hh + 1], in_=t16[:, 0, 0:1]), reads=["t16"], writes=["m1"])
                    s.op("act", lambda e, hh=hh: e.activation(out=e16[:], in_=c16[:], func=AF.Exp, bias=negm[:, hh:hh + 1], accum_out=Zs[:, hh:hh + 1]), reads=["c16", "negm"], writes=["e16", "Zs"])
                s.op("dve", lambda e: e.tensor_tensor(out=kap[:], in0=thr[:], in1=negm[:], op=ALU.add), reads=["thr", "negm"], writes=["kap"])
                s.op("act", lambda e: e.activation(out=kap[:], in_=kap[:], func=AF.Exp), reads=["kap"], writes=["kap"])
                s.op("dve", lambda e: e.reciprocal(out=Zs[:], in_=Zs[:]), reads=["Zs"], writes=["Zs"])
                s.op("dve", lambda e: e.tensor_tensor(out=kap[:], in0=kap[:], in1=Zs[:], op=ALU.mult), reads=["kap", "Zs"], writes=["kap"])
                s.op("dve", lambda e: e.tensor_tensor(out=th2[:], in0=thr[:], in1=m1[:], op=ALU.subtract), reads=["thr", "m1"], writes=["th2"])
                sc4 = SC[:].rearrange("p (h q) k -> p h q k", q=2)
                s.op("dve", lambda e, sc4=sc4: e.tensor_tensor(out=sc4[:, :, 0, :], in0=sc4[:, :, 0, :], in1=m1[:].unsqueeze(2).to_broadcast([128, 8, 128]), op=ALU.subtract), reads=[scid, "m1"], writes=[scid])
                s.op("dve", lambda e, sc4=sc4: e.tensor_tensor(out=sc4[:, :, 1, :], in0=sc4[:, :, 1, :], in1=th2[:].unsqueeze(2).to_broadcast([128, 8, 128]), op=ALU.subtract), reads=[scid, "th2"], writes=[scid])
                s.op("act", lambda e, SC=SC: e.activation(out=SC[:], in_=SC[:], func=AF.Exp), reads=[scid], writes=[scid])
                for hh in range(8):
                    s.op("dve", lambda e, hh=hh, tau=tau: e.tensor_scalar(out=dg[tau][:, hh, :], in0=ident, scalar1=kap[:, hh:hh + 1], scalar2=None, op0=ALU.mult), reads=["cm", "kap"], writes=[("dg", tau)])
              pU = [[bk[4 + 2 * t + hf] for hf in range(2)] for t in range(NB)]
              for g in range(ngr if cut >= 1 else 0):
                u = gcount % 2; gcount += 1
                e0 = g * GI * 128
                pR = bk[u]; pG = bk[2]; pW = bk[3]
                s.dma(UT[u][:], UTr_s[:, e0:e0 + GI * 128].rearrange("(kc p) n -> p kc n", p=128), reads=["UTr_s"], writes=[("UT", u)], queue=("sp", "act")[u])
                s.dma(VG[u][:], Vr_s[e0:e0 + GI * 128, :].rearrange("(a p) n -> p a n", p=128), reads=["Vr_s"], writes=[("VG", u)], queue=("act", "sp")[u])
                for tau in range(NB):
                    for kc in range(8):
                        s.op("pe", lambda e, kc=kc, u=u, tau=tau, pR=pR: e.matmul(out=pR[:, tau * GI * 128:(tau + 1) * GI * 128], lhsT=h2r[tau][:, kc, :], rhs=UT[u][:, kc, :], start=(kc == 0), stop=(kc == 7)), reads=[("h2r", tau), ("UT", u)], writes=[B(u)])
                for tau in range(NB if cut >= 2 else 0):
                    sc4 = sc[tau][:].rearrange("p (h q) k -> p h q k", q=2)
                    e1b = sc4[:, :, 0, g * GI:(g + 1) * GI].unsqueeze(3).to_broadcast([128, 8, GI, 128])
                    e2b = sc4[:, :, 1, :].unsqueeze(2).to_broadcast([128, 8, GI, 128])
                    s.op("pool", lambda e, e1b=e1b, e2b=e2b, tau=tau, u=u: e.tensor_tensor(out=pe_[u][:, tau, :, :].rearrange("p h (a k) -> p h a k", a=GI), in0=e1b, in1=e2b, op=ALU.mult), reads=[("sc", tau)], writes=[("pe", u)])
                if cut >= 2:
                  s.op("dve", lambda e, u=u: e.scalar_tensor_tensor(out=Mr[u][:], in0=pe_[u], scalar=1.0, in1=pe_[u], op0=ALU.is_ge, op1=ALU.mult), reads=[("pe", u)], writes=["Mr"])
                for tau in range(NB if cut >= 3 else 0):
                    for hh in range(8):
                        s.op("pe", lambda e, hh=hh, tau=tau, u=u: e.matmul(out=pG[:, tau * GI * 128:(tau + 1) * GI * 128], lhsT=dg[tau][:, hh, :], rhs=Mr[u][:, tau, hh, :], start=(hh == 0), stop=(hh == 7)), reads=[("dg", tau), "Mr"], writes=[B(2)])
                if cut < 4:
                    continue
                s.op("act", lambda e, pR=pR: e.copy(out=xs, in_=pR[:, 0:W5]), reads=[B(u)], writes=[("gtq", 0)])
                s.op("act", lambda e, pR=pR: e.activation(out=ga, in_=pR[:, 0:W5], func=AF.Square), reads=[B(u)], writes=[("gtq", 1)])
                s.op("pool", lambda e: e.tensor_scalar(out=ga, in0=ga, scalar1=0.044715, scalar2=1.0, op0=ALU.mult, op1=ALU.add), reads=[("gtq", 1)], writes=[("gtq", 1)])
                s.op("dve", lambda e: e.tensor_tensor(out=ga, in0=ga, in1=xs, op=ALU.mult), reads=[("gtq", 1), ("gtq", 0)], writes=[("gtq", 1)])
                s.op("act", lambda e: e.activation(out=ga, in_=ga, func=AF.Sigmoid, scale=1.5957691216057308), reads=[("gtq", 1)], writes=[("gtq", 1)])
                s.op("pool", lambda e: e.tensor_tensor(out=g1, in0=xs, in1=ga, op=ALU.mult), reads=[("gtq", 0), ("gtq", 1)], writes=[("gtq", 2)])
                s.op("dve", lambda e: e.tensor_tensor(out=Pm_, in0=g1, in1=pG[:, 0:W5], op=ALU.mult), reads=[("gtq", 2), B(2)], writes=[("gtq", 3)])
                if cut < 5:
                    continue
                for tau in range(NB):
                    for a in range(GI):
                        k = tau * GI + a
                        s.op("pe", lambda e, k=k: e.transpose(out=pW[:, k * 128:(k + 1) * 128], in_=Pm_[:, k * 128:(k + 1) * 128], identity=ident), reads=[("gtq", 3), "cm"], writes=[B(3)])
                s.op("act", lambda e, u=u: e.copy(out=PT[u][:].rearrange("p a b -> p (a b)"), in_=pW[:, 0:W5]), reads=[B(3)], writes=[("PT", u)])
                if cut < 6:
                    continue
                for tau in range(NB):
                    for a in range(GI):
                        for half in range(2):
                            s.op("pe", lambda e, a=a, half=half, u=u, tau=tau, first=(g == 0 and a == 0), last=(g == ngr - 1 and a == GI - 1): e.matmul(out=pU[tau][half][:], lhsT=PT[u][:, tau * GI + a, :], rhs=VG[u][:, a, half * 512:(half + 1) * 512], start=first, stop=last),
                                 reads=[("PT", u), ("VG", u)], writes=[B(4 + 2 * tau + half)])
              for tau in range(NB if cut >= 7 else 0):
                i = blk * NB + tau
                XA = xa[tau]; xid = ("xa", tau)
                for half in range(2):
                    s.op("dve", lambda e, half=half, tau=tau: e.tensor_tensor(out=yb[:, half * 512:(half + 1) * 512], in0=pU[tau][half][:], in1=bvv(BV_GT2)[:, half * 512:(half + 1) * 512], op=ALU.mult), reads=[B(4 + 2 * tau + half), "bv"], writes=["yb"])
                s.op("pool", lambda e, XA=XA: e.tensor_tensor(out=XA[:], in0=XA[:], in1=yb[:], op=ALU.add), reads=[xid, "yb"], writes=[xid])
                rms(XA, xid)
                s.op("dve", lambda e, XA=XA: e.scalar_tensor_tensor(out=yb[:], in0=XA[:], scalar=rstd[:, 0:1], in1=bvv(BV_FN)[:, :], op0=ALU.mult, op1=ALU.mult), reads=[xid, "rstd6", "bv"], writes=["yb"])
                s.dma(out_d[i * 128:(i + 1) * 128, :], yb[:], reads=["yb"], writes=["out"], queue="pool")
            s.flush()
        return nc


def _rope(s, src, dst, tmp, R, rid, H, sid, did):
    sv = src.rearrange("p (h a b c) -> p h a b c", h=H, a=2, b=2)
    dv = dst.rearrange("p (h a b c) -> p h a b c", h=H, a=2, b=2)
    tv = tmp[:, 0:H * 32].rearrange("p (h a c) -> p h a c", h=H, a=2)
    rv = R[:].rearrange("p (a b c) -> p a b c", a=2, b=2)
    cosb = rv[:, :, 0, :].unsqueeze(1).to_broadcast([128, H, 2, 16])
    sinb = rv[:, :, 1, :].unsqueeze(1).to_broadcast([128, H, 2, 16])
    x1 = sv[:, :, :, 0, :]; x2 = sv[:, :, :, 1, :]
    o1 = dv[:, :, :, 0, :]; o2 = dv[:, :, :, 1, :]
    s.op("dve", lambda e: e.tensor_tensor(out=o1, in0=x1, in1=cosb, op=ALU.mult), reads=[sid, rid], writes=[did])
    s.op("dve", lambda e: e.tensor_tensor(out=tv, in0=x2, in1=sinb, op=ALU.mult), reads=[sid, rid], writes=["tmp"])
    s.op("dve", lambda e: e.tensor_tensor(out=o1, in0=o1, in1=tv, op=ALU.subtract), reads=[did, "tmp"], writes=[did])
    s.op("dve", lambda e: e.tensor_tensor(out=o2, in0=x1, in1=sinb, op=ALU.mult), reads=[sid, rid, did], writes=[did])
    s.op("dve", lambda e: e.tensor_tensor(out=tv, in0=x2, in1=cosb, op=ALU.mult), reads=[sid, rid, did], writes=["tmp"])
    s.op("dve", lambda e: e.tensor_tensor(out=o2, in0=o2, in1=tv, op=ALU.add), reads=[did, "tmp"], writes=[did])


def _host_inputs(inputs, b, consts):
    g = lambda k: np.ascontiguousarray(inputs[k], dtype=np.float32)
    m = {
        "x": g("x")[b], "c": g("c")[b], "ctx": g("ctx")[b], "c_ctx": g("c_ctx"),
        "w_ada": g("w_ada")[0], "b_ada": g("b_ada")[0], "norm_mix": g("norm_mix")[0], "norm_ffn": g("norm_ffn")[0],
        "w_in": g("w_in")[0], "b_gate": g("b_gate")[0], "attn_sink": g("attn_sink")[0], "dn_conv": g("dn_conv")[0],
        "dn_a_log_f": g("dn_a_log_f")[0], "dn_dt_bias_f": g("dn_dt_bias_f")[0], "dn_a_log_b": g("dn_a_log_b")[0], "dn_dt_bias_b": g("dn_dt_bias_b")[0],
        "dn_norm": g("dn_norm")[0], "w_br_attn": g("w_br_attn")[0], "w_br_dn": g("w_br_dn")[0], "w_out": g("w_out")[0],
        "peer_wq": g("peer_wq")[0], "final_norm": g("final_norm"),
    }
    m.update(consts)
    return {k: np.ascontiguousarray(v) for k, v in m.items()}


_SHARED = {}


def kernel(**inputs):
    consts = _consts()
    nc = build()
    keysT = np.ascontiguousarray(np.transpose(np.asarray(inputs["peer_keys"], np.float32)[0], (0, 1, 3, 2)))
    uT = np.ascontiguousarray(np.asarray(inputs["peer_u"], np.float32)[0].T)
    pv = np.ascontiguousarray(np.asarray(inputs["peer_v"], np.float32)[0])
    in_maps = []
    for b in range(8):
        m = _host_inputs(inputs, b, consts)
        m["peer_keysT"] = keysT; m["peer_uT"] = uT; m["peer_v"] = pv
        in_maps.append(m)
    res = run_bass_kernel_spmd(nc, in_maps, core_ids=list(range(8)))
    return np.stack([np.asarray(r["out"], dtype=np.float32) for r in res.results], axis=0)
```

```python
import contextlib
import numpy as np
import concourse.bass as bass
import concourse.mybir as mybir
from concourse.bass_utils import run_bass_kernel_spmd

F32 = mybir.dt.float32
F32R = mybir.dt.float32r
ALU = mybir.AluOpType
AF = mybir.ActivationFunctionType
AX = mybir.AxisListType

D = 1024
S = 8192
CTX = 256
TALL = CTX + S
NT = S // 128
IN_COLS = 4880
NEG = -30000.0


class _Ins:
    __slots__ = ("eng", "fn", "deps", "signal", "sig_no", "dma", "idx")

    def __init__(self, eng, fn, dma=None):
        self.eng = eng
        self.fn = fn
        self.deps = []
        self.signal = False
        self.sig_no = None
        self.dma = dma
        self.idx = None


class Sch:
    EPOCH = 20000
    NDMA = 24
    NEP = 16

    def __init__(self, nc, st):
        self.nc = nc
        self.engs = ("pe", "act", "dve", "pool", "sp")
        self.nep = {"pe": 12, "act": 4, "dve": 6, "pool": 3, "sp": 1}
        self.sems = {e: [st.enter_context(nc.semaphore(f"s_{e}_{i}")) for i in range(self.nep[e])] for e in self.engs}
        self.dsems = [st.enter_context(nc.semaphore(f"s_dma_{i}")) for i in range(self.NDMA)]
        self.sigc = {e: 0 for e in self.engs}
        self.dma_rr = 0
        self.dma_cnt = [0] * self.NDMA
        self.dma_last = [None] * self.NDMA
        self._reset()

    def _reset(self):
        self.q = {e: [] for e in self.engs}
        self.lastw = {}
        self.readers = {}

    def _add(self, ins, reads, writes):
        q = self.q[ins.eng]
        ins.idx = len(q)
        deps = []
        for r in reads:
            w = self.lastw.get(r)
            if w is not None:
                deps.append((w, "raw"))
        for w_ in writes:
            w = self.lastw.get(w_)
            if w is not None:
                deps.append((w, "waw"))
            for rd in self.readers.get(w_, ()):
                deps.append((rd, "war"))
        for d, kind in deps:
            if d is ins:
                continue
            if d.dma is None and ins.dma is None and d.eng == ins.eng:
                if ins.eng == "pe":
                    continue
                if kind != "raw":
                    continue
            ins.deps.append(d)
            if d.dma is None:
                d.signal = True
        for r in reads:
            self.readers.setdefault(r, []).append(ins)
        for w_ in writes:
            self.lastw[w_] = ins
            self.readers[w_] = []
        q.append(ins)
        return ins

    PSUM_NAMES = {"bk", "pm", "pT", "pY", "pX", "pN", "pK", "pb", "pS", "pO", "pQ", "pZ", "pR", "pU", "pW"}

    def op(self, eng, fn, reads=(), writes=()):
        writes = list(writes)
        if eng != "pe":
            for r in reads:
                if isinstance(r, tuple) and r[0] in self.PSUM_NAMES and r not in writes:
                    writes.append(r)
        return self._add(_Ins(eng, fn), list(reads), writes)

    def dma(self, out, in_, reads=(), writes=(), queue="sp", **kw):
        slot = self.dma_rr
        self.dma_rr = (self.dma_rr + 1) % self.NDMA
        self.dma_cnt[slot] += 1
        n = self.dma_cnt[slot]
        ins = _Ins(queue, lambda e: e.dma_start(out=out, in_=in_, **kw), dma=(slot, n))
        prev = self.dma_last[slot]
        self._add(ins, list(reads), list(writes))
        if prev is not None:
            ins.deps.append(prev)
        self.dma_last[slot] = ins
        return ins

    def flush(self):
        nc = self.nc
        for e, q in self.q.items():
            for ins in q:
                if ins.dma is None and ins.signal:
                    ins.sig_no = self.sigc[e]
                    self.sigc[e] += 1
            assert self.sigc[e] < self.EPOCH * self.nep[e], f"too many signals on {e}: {self.sigc[e]}"
        dma_final = list(self.dma_cnt)
        with nc.Block() as block:
            def run(ename):
                def body(eng):
                    seen_c = {}
                    seen_d = {}
                    for ins in self.q[ename]:
                        wc = {}
                        wd = {}
                        for d in ins.deps:
                            if d.dma is None:
                                if d.sig_no is None:
                                    continue
                                if seen_c.get(d.eng, -1) < d.sig_no:
                                    wc[d.eng] = max(wc.get(d.eng, -1), d.sig_no)
                            else:
                                s_, n = d.dma
                                if seen_d.get(s_, 0) < n:
                                    wd[s_] = max(wd.get(s_, 0), n)
                        for e2, sn in wc.items():
                            eng.wait_ge(self.sems[e2][sn // self.EPOCH], sn % self.EPOCH + 1)
                            seen_c[e2] = sn
                        for s_, n in wd.items():
                            eng.wait_ge(self.dsems[s_], 16 * n)
                            seen_d[s_] = n
                        h = ins.fn(eng)
                        if ins.dma is not None:
                            h.then_inc(self.dsems[ins.dma[0]], 16)
                        elif ins.signal:
                            h.then_inc(self.sems[ename][ins.sig_no // self.EPOCH], 1)
                    if ename == "sp":
                        for s_, n in enumerate(dma_final):
                            if n > 0:
                                eng.wait_ge(self.dsems[s_], 16 * n)
                return body

            block.sync(run("sp"))
            block.tensor(run("pe"))
            block.scalar(run("act"))
            block.vector(run("dve"))
            block.gpsimd(run("pool"))
        nc.all_engine_barrier()
        self._reset()


def _consts():
    c = {}
    ident = np.eye(128, dtype=np.float32)
    ones = np.ones((128, 128), np.float32)
    idx = np.arange(128)
    same = (idx[:, None] // 64 == idx[None, :] // 64).astype(np.float32)
    m1f = ((idx[:, None] <= idx[None, :]) * same).astype(np.float32)
    m1b = ((idx[:, None] >= idx[None, :]) * same).astype(np.float32)
    sel0 = np.zeros((128, 128), np.float32); sel0[:64, :] = 1
    sel1 = np.zeros((128, 128), np.float32); sel1[64:, :] = 1
    low_incl = ((idx[None, :] <= idx[:, None]) * same)
    up_incl = ((idx[None, :] >= idx[:, None]) * same)
    low_strict = ((idx[None, :] < idx[:, None]) * same)
    up_strict = ((idx[None, :] > idx[:, None]) * same)
    negmask = lambda m: np.where(m > 0, 0.0, NEG).astype(np.float32)
    w_prev = (idx[None, :] <= idx[:, None]).astype(np.float32)
    w_next = (idx[:, None] <= idx[None, :]).astype(np.float32)
    mats = [ident, ones, same, m1f, -m1f, m1b, -m1b, sel0, sel1,
            negmask(low_incl), negmask(up_incl), -low_strict.astype(np.float32), -up_strict.astype(np.float32),
            w_prev, w_next]
    c["cmat"] = np.ascontiguousarray(np.stack(mats, axis=1)).astype(np.float32)
    pos = np.arange(S)
    inv = (10000.0 ** (-np.arange(16, dtype=np.float32) / 16)).astype(np.float32)
    ar = (pos // 64).astype(np.float32)[:, None] * inv[None, :]
    ac = (pos % 64).astype(np.float32)[:, None] * inv[None, :]
    c["rope"] = np.concatenate([np.cos(ar), np.sin(ar), np.cos(ac), np.sin(ac)], axis=1).astype(np.float32)
    return c

(C_ID, C_ONES, C_SAME, C_M1F, C_NM1F, C_M1B, C_NM1B, C_SEL0, C_SEL1, C_NLOW, C_NUP, C_SLOW, C_SUP, C_WPREV, C_WNEXT) = range(15)


def build(upto=99, dbg=()):
    nc = bass.Bass("TRN2", target_bir_lowering=False)
    nc.dge_precook = False
    inp = lambda name, shape: nc.dram_tensor(name, list(shape), F32, kind="ExternalInput").ap()
    x_d = inp("x", [S, D]); c_d = inp("c", [D]); ctx_d = inp("ctx", [CTX, D]); cctx_d = inp("c_ctx", [D])
    wada_d = inp("w_ada", [D, 6 * D]); bada_d = inp("b_ada", [6 * D])
    nmix_d = inp("norm_mix", [D]); nffn_d = inp("norm_ffn", [D])
    win_d = inp("w_in", [D, IN_COLS]); bgate_d = inp("b_gate", [2 * D])
    sink_d = inp("attn_sink", [8]); conv_d = inp("dn_conv", [5, 1536])
    alf_d = inp("dn_a_log_f", [4]); dtf_d = inp("dn_dt_bias_f", [4]); alb_d = inp("dn_a_log_b", [4]); dtb_d = inp("dn_dt_bias_b", [4])
    dnn_d = inp("dn_norm", [128]); wba_d = inp("w_br_attn", [512, D]); wbd_d = inp("w_br_dn", [512, D]); wout_d = inp("w_out", [D, D])
    pwq_d = inp("peer_wq", [D, D]); pkeys_d = inp("peer_keysT", [8, 2, 64, 128]); pu_d = inp("peer_uT", [D, 16384]); pv_d = inp("peer_v", [16384, D])
    fnorm_d = inp("final_norm", [D]); cmat_d = inp("cmat", [128, 15, 128]); rope_d = inp("rope", [S, 64])
    out_d = nc.dram_tensor("out", [S, D], F32, kind="ExternalOutput").ap()
    scr = lambda name, shape: nc.dram_tensor(name, list(shape), F32, kind=("ExternalOutput" if name in dbg else "Internal")).ap()
    QT_s = scr("QT_s", [64, 8, S])
    KT_s = scr("KT_s", [64, 2, TALL])
    V_s = scr("V_s", [TALL, 2, 65])
    RT_s = scr("RT_s", [1536, TALL])
    Z_s = scr("Z_s", [S, 512])
    GB_s = scr("GB_s", [TALL, 16])
    GT_s = scr("GT_s", [S, 2048])
    QK_s = scr("QK_s", [1024, TALL])
    KV_s = scr("KV_s", [TALL, 1024])
    OD_s = scr("OD_s", [2, S, 512])
    OA_s = scr("OA_s", [S, 512])
    MOD_s = scr("MOD_s", [8, D])

    with contextlib.ExitStack() as gst:
        s = Sch(nc, gst)
        _uid = [0]

        def _nm(name):
            _uid[0] += 1
            return f"{name}_u{_uid[0]}"
        T = lambda st, name, shape: st.enter_context(nc.sbuf_tensor(_nm(name), list(shape), F32))
        PS = lambda st, name, shape: st.enter_context(nc.psum_tensor(_nm(name), list(shape), F32))
        cm = T(gst, "cm", [128, 15, 128])
        s.dma(cm[:], cmat_d, writes=["cm"])
        ident = cm[:, C_ID, :]
        BV_G1, BV_SH1, BV_GT1, BV_G2, BV_SH2, BV_GT2, BV_CG1, BV_CSH1, BV_FN = range(9)
        bvB = T(gst, "bvB", [128, 5, D])
        stA = contextlib.ExitStack()
        bvA = T(stA, "bvA", [128, 4, D])
        _amap = {BV_G1: 0, BV_SH1: 1, BV_CG1: 2, BV_CSH1: 3}
        _bmap = {BV_GT1: 0, BV_G2: 1, BV_SH2: 2, BV_GT2: 3, BV_FN: 4}

        def bvv(k):
            return bvA[:, _amap[k], :] if k in _amap else bvB[:, _bmap[k], :]

        with contextlib.ExitStack() as st:
            cc = T(st, "cc", [128, 2, 8]); cs = T(st, "cs", [128, 2, 8]); lh = T(st, "lh", [128, 2, 8, 128])
            wa = [T(st, f"wa{i}", [128, 8, 512]) for i in range(2)]
            bb = T(st, "bb", [128, 6 * D]); nm = T(st, "nm", [128, 2, D])
            pm = [PS(st, f"pm{i}", [128, 512]) for i in range(2)]
            s.dma(cc[:, 0, :], c_d.rearrange("(kc p) -> p kc", p=128), writes=["cc"], allow_slow_non_contiguous=True)
            s.dma(cc[:, 1, :], cctx_d.rearrange("(kc p) -> p kc", p=128), writes=["cc"], allow_slow_non_contiguous=True)
            s.dma(bb[:], bada_d.partition_broadcast(128), writes=["bb"])
            s.dma(nm[:, 0, :], nmix_d.partition_broadcast(128), writes=["nm"])
            s.dma(nm[:, 1, :], nffn_d.partition_broadcast(128), writes=["nm"])
            s.dma(bvv(BV_FN)[:, :], fnorm_d.partition_broadcast(128), writes=["bv"])
            s.op("act", lambda e: e.activation(out=cs[:], in_=cc[:], func=AF.Silu), reads=["cc"], writes=["cs"])
            s.op("dve", lambda e: e.tensor_copy(out=lh[:], in_=cs[:].unsqueeze(3).to_broadcast([128, 2, 8, 128])), reads=["cs"], writes=["lh"])
            jobs = [(0, nb) for nb in range(12)] + [(1, nb) for nb in range(4)]
            for ji, (w, nb) in enumerate(jobs):
                wt = wa[ji % 2]; p = pm[ji % 2]
                s.dma(wt[:], wada_d[:, nb * 512:(nb + 1) * 512].rearrange("(kc p) n -> p kc n", p=128), writes=[("wa", ji % 2)], queue=("sp" if ji % 2 == 0 else "act"))
                for kc in range(8):
                    s.op("pe", lambda e, w=w, kc=kc, wt=wt, p=p: e.matmul(out=p[:], lhsT=lh[:, w, kc, :], rhs=wt[:, kc, :], start=(kc == 0), stop=(kc == 7)),
                         reads=["lh", ("wa", ji % 2)], writes=[("pm", ji % 2)])
                ch, half = nb // 2, nb % 2
                if w == 0:
                    dst = {0: BV_SH1, 1: BV_G1, 2: BV_GT1, 3: BV_SH2, 4: BV_G2, 5: BV_GT2}[ch]
                else:
                    dst = {0: BV_CSH1, 1: BV_CG1}[ch]
                o = bvv(dst)[:, half * 512:(half + 1) * 512]
                s.op("dve", lambda e, o=o, p=p, nb=nb: e.tensor_tensor(out=o, in0=p[:], in1=bb[:, nb * 512:(nb + 1) * 512], op=ALU.add),
                     reads=[("pm", ji % 2), "bb"], writes=["bv"])
            for dst, ni in ((BV_G1, 0), (BV_G2, 1), (BV_CG1, 0)):
                s.op("dve", lambda e, dst=dst, ni=ni: e.scalar_tensor_tensor(out=bvv(dst)[:, :], in0=bvv(dst)[:, :], scalar=1.0, in1=nm[:, ni, :], op0=ALU.add, op1=ALU.mult),
                     reads=["bv", "nm"], writes=["bv"])
            s.flush()
        if upto <= 0:
            stA.close()
            return nc

        blocks = [(0, 512), (512, 256), (768, 512), (1280, 512), (1792, 512), (2304, 512), (2816, 16)] + [(2832 + 512 * i, 512) for i in range(4)]
        with contextlib.ExitStack() as st:
            xt = [T(st, f"xt{i}", [128, D]) for i in range(2)]
            junk = T(st, "junk", [128, D]); ss = T(st, "ss", [128, 1]); rstd = T(st, "rstd", [128, 1])
            h = T(st, "h", [128, D]); hT = T(st, "hT", [128, 8, 128])
            wb = [T(st, f"wb{i}", [128, 8, 512]) for i in range(3)]
            rp = [T(st, f"rp{i}", [128, 64]) for i in range(2)]
            qs = T(st, "qs", [128, 512]); qr = T(st, "qr", [128, 512]); tmp = T(st, "tmp", [128, 512])
            qT = T(st, "qT", [64, 8, 128]); kvs = T(st, "kvs", [128, 256]); kr = T(st, "kr", [128, 128]); kT = T(st, "kT", [64, 2, 128])
            va = T(st, "va", [128, 2, 65]); rw = T(st, "rw", [128, 512]); rT = T(st, "rT", [128, 4, 128])
            zz = T(st, "zz", [128, 512]); gn = T(st, "gn", [128, 128]); ab = T(st, "ab", [128, 16]); abc = T(st, "abc", [128, 2, 8])
            gbo = T(st, "gbo", [128, 16]); gg = T(st, "gg", [128, 512]); bg = T(st, "bg", [128, 2048])
            pT = [PS(st, f"pT{i}", [128, 512]) for i in range(2)]
            pY = [PS(st, f"pY{i}", [128, 512]) for i in range(3)]
            pX = [PS(st, f"pX{i}", [128, 512]) for i in range(2)]
            s.dma(bg[:], bgate_d.partition_broadcast(128), writes=["bg"])
            s.dma(gn[:], dnn_d.partition_broadcast(128), writes=["gn"])
            s.dma(abc[:, 0, 0:4], dtf_d.partition_broadcast(128), writes=["abc"])
            s.dma(abc[:, 0, 4:8], dtb_d.partition_broadcast(128), writes=["abc"])
            s.dma(abc[:, 1, 0:4], alf_d.partition_broadcast(128), writes=["abc"])
            s.dma(abc[:, 1, 4:8], alb_d.partition_broadcast(128), writes=["abc"])
            s.op("act", lambda e: e.activation(out=abc[:, 1, :], in_=abc[:, 1, :], func=AF.Exp), reads=["abc"], writes=["abc"])
            s.op("dve", lambda e: e.tensor_scalar(out=abc[:, 1, :], in0=abc[:, 1, :], scalar1=-1.0, scalar2=None, op0=ALU.mult), reads=["abc"], writes=["abc"])
            s.op("pool", lambda e: e.memset(va[:], 1.0), writes=["va"])
            wcount = [0]

            def rope_ops(src, dst, H):
                sv = src.rearrange("p (h a b c) -> p h a b c", h=H, a=2, b=2)
                dv = dst.rearrange("p (h a b c) -> p h a b c", h=H, a=2, b=2)
                tv = tmp[:, 0:H * 64].rearrange("p (h a b c) -> p h a b c", h=H, a=2, b=2)
                return sv, dv, tv

            tiles = [("c", i) for i in range(CTX // 128)] + [("l", i) for i in range(NT)]
            if upto == 1 and "small" in dbg:
                tiles = tiles[:4]
            for ti, (kind, i) in enumerate(tiles):
                lat = kind == "l"
                src = x_d if lat else ctx_d
                tg = ti
                X = xt[ti % 2]; xid = ("xt", ti % 2)
                s.dma(X[:], src[i * 128:(i + 1) * 128, :], writes=[xid])
                if lat:
                    R = rp[ti % 2]; rid = ("rp", ti % 2)
                    s.dma(R[:], rope_d[i * 128:(i + 1) * 128, :], writes=[rid], queue="act")
                s.op("act", lambda e, X=X: e.activation(out=junk[:], in_=X[:], func=AF.Square, accum_out=ss[:]), reads=[xid], writes=["junk", "ss"])
                s.op("dve", lambda e: e.tensor_scalar(out=rstd[:], in0=ss[:], scalar1=1.0 / D, scalar2=1e-6, op0=ALU.mult, op1=ALU.add), reads=["ss"], writes=["rstd"])
                s.op("act", lambda e: e.sqrt(out=rstd[:], in_=rstd[:]), reads=["rstd"], writes=["rstd"])
                s.op("dve", lambda e: e.reciprocal(out=rstd[:], in_=rstd[:]), reads=["rstd"], writes=["rstd"])
                G = BV_G1 if lat else BV_CG1
                SH = BV_SH1 if lat else BV_CSH1
                s.op("dve", lambda e, X=X, G=G: e.scalar_tensor_tensor(out=h[:], in0=X[:], scalar=rstd[:, 0:1], in1=bvv(G)[:, :], op0=ALU.mult, op1=ALU.mult), reads=[xid, "rstd", "bv"], writes=["h"])
                s.op("pool", lambda e, SH=SH: e.tensor_tensor(out=h[:], in0=h[:], in1=bvv(SH)[:, :], op=ALU.add), reads=["h", "bv"], writes=["h"])
                for hb in range(2):
                    for k4 in range(4):
                        kc = hb * 4 + k4
                        s.op("pe", lambda e, kc=kc, hb=hb, k4=k4: e.transpose(out=pT[hb][:, k4 * 128:(k4 + 1) * 128], in_=h[:, kc * 128:(kc + 1) * 128], identity=ident), reads=["h", "cm"], writes=[("pT", hb)])
                    eng = "act" if hb == 0 else "dve"
                    if eng == "act":
                        s.op("act", lambda e, hb=hb: e.copy(out=hT[:, hb * 4:(hb + 1) * 4, :].rearrange("p a b -> p (a b)"), in_=pT[hb][:]), reads=[("pT", hb)], writes=[("hT", hb)])
                    else:
                        s.op("dve", lambda e, hb=hb: e.tensor_copy(out=hT[:, hb * 4:(hb + 1) * 4, :].rearrange("p a b -> p (a b)"), in_=pT[hb][:]), reads=[("pT", hb)], writes=[("hT", hb)])
                need = range(11) if lat else (1, 2, 3, 4, 6)
                for bi in need:
                    c0, cw = blocks[bi]
                    wi = wcount[0] % 3; wcount[0] += 1
                    W = wb[wi]; P = pY[wi]
                    s.dma(W[:, :, 0:cw], win_d[:, c0:c0 + cw].rearrange("(kc p) n -> p kc n", p=128), writes=[("wb", wi)], queue=("sp", "act", "pool")[wi], allow_slow_non_contiguous=(cw < 128))
                    for kc in range(8):
                        s.op("pe", lambda e, kc=kc, W=W, P=P, cw=cw: e.matmul(out=P[:, 0:cw], lhsT=hT[:, kc, :], rhs=W[:, kc, 0:cw], start=(kc == 0), stop=(kc == 7)),
                             reads=[("hT", 0), ("hT", 1), ("wb", wi)], writes=[("pY", wi)])
                    pid = ("pY", wi)
                    if bi == 0:
                        s.op("act", lambda e, P=P: e.activation(out=qs[:], in_=P[:], func=AF.Copy, scale=0.125), reads=[pid], writes=["qs"])
                        _rope(s, qs[:], qr[:], tmp, R, rid, 8, "qs", "qr")
                        for hh in range(8):
                            s.op("pe", lambda e, hh=hh: e.transpose(out=pX[hh // 4][0:64, (hh % 4) * 128:(hh % 4 + 1) * 128], in_=qr[:, hh * 64:(hh + 1) * 64], identity=ident), reads=["qr", "cm"], writes=[("pX", hh // 4)])
                        s.op("act", lambda e: e.copy(out=qT[:, 0:4, :].rearrange("p a b -> p (a b)"), in_=pX[0][0:64, :]), reads=[("pX", 0)], writes=["qT"])
                        s.op("dve", lambda e: e.tensor_copy(out=qT[:, 4:8, :].rearrange("p a b -> p (a b)"), in_=pX[1][0:64, :]), reads=[("pX", 1)], writes=["qT"])
                        s.dma(QT_s[:, :, i * 128:(i + 1) * 128], qT[:], reads=["qT"], writes=["QT_s"], queue="pool")
                    elif bi == 1:
                        s.op("act", lambda e, P=P: e.copy(out=kvs[:], in_=P[:, 0:256]), reads=[pid], writes=["kvs"])
                        if lat:
                            _rope(s, kvs[:, 0:128], kr[:], tmp, R, rid, 2, "kvs", "kr")
                            ksrc, kid = kr, "kr"
                        else:
                            ksrc, kid = kvs, "kvs"
                        for hh in range(2):
                            s.op("pe", lambda e, hh=hh, ksrc=ksrc: e.transpose(out=pX[0][0:64, hh * 128:(hh + 1) * 128], in_=ksrc[:, hh * 64:(hh + 1) * 64], identity=ident), reads=[kid, "cm"], writes=[("pX", 0)])
                        s.op("act", lambda e: e.copy(out=kT[:].rearrange("p a b -> p (a b)"), in_=pX[0][0:64, 0:256]), reads=[("pX", 0)], writes=["kT"])
                        s.dma(KT_s[:, :, tg * 128:(tg + 1) * 128], kT[:], reads=["kT"], writes=["KT_s"], queue="pool")
                        s.op("pool", lambda e: e.tensor_copy(out=va[:, :, 0:64], in_=kvs[:, 128:256].rearrange("p (g d) -> p g d", g=2)), reads=["kvs"], writes=["va"])
                        s.dma(V_s[tg * 128:(tg + 1) * 128, :, :], va[:], reads=["va"], writes=["V_s"], queue="pool")
                    elif bi in (2, 3, 4):
                        s.op("act", lambda e, P=P: e.copy(out=rw[:], in_=P[:]), reads=[pid], writes=["rw"])
                        for k4 in range(4):
                            s.op("pe", lambda e, k4=k4: e.transpose(out=pX[1][:, k4 * 128:(k4 + 1) * 128], in_=rw[:, k4 * 128:(k4 + 1) * 128], identity=ident), reads=["rw", "cm"], writes=[("pX", 1)])
                        s.op("dve", lambda e: e.tensor_copy(out=rT[:].rearrange("p a b -> p (a b)"), in_=pX[1][:]), reads=[("pX", 1)], writes=["rT"])
                        f0 = (bi - 2) * 512
                        s.dma(RT_s[f0:f0 + 512, tg * 128:(tg + 1) * 128].rearrange("(a p) t -> p a t", p=128), rT[:], reads=["rT"], writes=["RT_s"], queue="pool")
                    elif bi == 5:
                        s.op("act", lambda e, P=P: e.activation(out=zz[:], in_=P[:], func=AF.Silu), reads=[pid], writes=["zz"])
                        s.op("pool", lambda e: e.tensor_tensor(out=zz[:].rearrange("p (h d) -> p h d", h=4), in0=zz[:].rearrange("p (h d) -> p h d", h=4), in1=gn[:].unsqueeze(1).to_broadcast([128, 4, 128]), op=ALU.mult), reads=["zz", "gn"], writes=["zz"])
                        s.dma(Z_s[i * 128:(i + 1) * 128, :], zz[:], reads=["zz"], writes=["Z_s"], queue="pool")
                    elif bi == 6:
                        s.op("dve", lambda e, P=P: e.tensor_tensor(out=ab[:, 0:8], in0=P[:, 0:8], in1=abc[:, 0, :], op=ALU.add), reads=[pid, "abc"], writes=["ab"])
                        s.op("act", lambda e: e.activation(out=ab[:, 0:8], in_=ab[:, 0:8], func=AF.Exp), reads=["ab"], writes=["ab"])
                        s.op("dve", lambda e: e.tensor_scalar(out=ab[:, 0:8], in0=ab[:, 0:8], scalar1=1.0, scalar2=None, op0=ALU.add), reads=["ab"], writes=["ab"])
                        s.op("act", lambda e: e.activation(out=ab[:, 0:8], in_=ab[:, 0:8], func=AF.Ln), reads=["ab"], writes=["ab"])
                        s.op("dve", lambda e: e.tensor_tensor(out=gbo[:, 0:8], in0=ab[:, 0:8], in1=abc[:, 1, :], op=ALU.mult), reads=["ab", "abc"], writes=["gbo"])
                        s.op("act", lambda e, P=P: e.activation(out=gbo[:, 8:16], in_=P[:, 8:16], func=AF.Sigmoid), reads=[pid], writes=["gbo"])
                        s.dma(GB_s[tg * 128:(tg + 1) * 128, :], gbo[:], reads=["gbo"], writes=["GB_s"], queue="pool")
                    else:
                        gi = bi - 7
                        s.op("dve", lambda e, P=P, gi=gi: e.tensor_tensor(out=gg[:], in0=P[:], in1=bg[:, gi * 512:(gi + 1) * 512], op=ALU.add), reads=[pid, "bg"], writes=["gg"])
                        s.op("act", lambda e: e.activation(out=gg[:], in_=gg[:], func=AF.Sigmoid), reads=["gg"], writes=["gg"])
                        s.dma(GT_s[i * 128:(i + 1) * 128, gi * 512:(gi + 1) * 512], gg[:], reads=["gg"], writes=["GT_s"], queue="pool")
            s.flush()
        stA.close()
        if upto <= 1:
            return nc

        with contextlib.ExitStack() as st:
            cw = T(st, "cw", [128, 12, 5])
            Rt = [T(st, f"Rt{i}", [128, 516]) for i in range(3)]
            acc = [T(st, f"acc{i}", [128, 512]) for i in range(2)]
            y = [T(st, f"y{i}", [128, 512]) for i in range(2)]
            y2 = T(st, "y2", [128, 512]); rn = T(st, "rn", [128, 512]); yn = [T(st, f"yn{i}", [128, 512]) for i in range(2)]
            tok = [T(st, f"tok{i}", [128, 4, 128]) for i in range(2)]
            pN = [PS(st, f"pN{i}", [128, 512]) for i in range(2)]
            pK = [PS(st, f"pK{i}", [128, 512]) for i in range(2)]
            for j in range(5):
                s.dma(cw[:, :, j], conv_d[j, :].rearrange("(fc p) -> p fc", p=128), writes=["cw"], allow_slow_non_contiguous=True)
            it = 0
            segs = [(0, CTX), (CTX, TALL)]
            if "small" in dbg:
                segs = [(0, CTX), (CTX, CTX + 256)]
            for (g0, g1) in segs:
                for t0 in range(g0, g1, 512):
                    n = min(512, g1 - t0)
                    for fc in range(12):
                        R = Rt[it % 3]; rid = ("Rt", it % 3); A = acc[it % 2]; aid = ("acc", it % 2); Y = y[it % 2]; yid = ("y", it % 2)
                        lo = max(t0 - 2, g0); hi = min(t0 + n + 2, g1)
                        if lo > t0 - 2 or hi < t0 + n + 2:
                            s.op("pool", lambda e, R=R: e.memset(R[:], 0.0), writes=[rid])
                        s.dma(R[:, lo - (t0 - 2):hi - (t0 - 2)], RT_s[fc * 128:(fc + 1) * 128, lo:hi], reads=["RT_s"], writes=[rid], queue=("sp", "act")[it % 2])
                        s.op("dve", lambda e, R=R, A=A, fc=fc, n=n: e.tensor_scalar(out=A[:, 0:n], in0=R[:, 0:n], scalar1=cw[:, fc, 0:1], scalar2=None, op0=ALU.mult), reads=[rid, "cw"], writes=[aid])
                        for j in range(1, 5):
                            s.op("dve", lambda e, R=R, A=A, fc=fc, n=n, j=j: e.scalar_tensor_tensor(out=A[:, 0:n], in0=R[:, j:j + n], scalar=cw[:, fc, j:j + 1], in1=A[:, 0:n], op0=ALU.mult, op1=ALU.add), reads=[rid, "cw", aid], writes=[aid])
                        s.op("act", lambda e, A=A, Y=Y, n=n: e.activation(out=Y[:, 0:n], in_=A[:, 0:n], func=AF.Silu), reads=[aid], writes=[yid])
                        src, sid = Y, yid
                        if fc < 8:
                            YN = yn[it % 2]; nid = ("yn", it % 2); P = pN[it % 2]; pid = ("pN", it % 2)
                            s.op("act", lambda e, Y=Y, n=n: e.activation(out=y2[:, 0:n], in_=Y[:, 0:n], func=AF.Square), reads=[yid], writes=["y2"])
                            s.op("pe", lambda e, P=P, n=n: e.matmul(out=P[:, 0:n], lhsT=cm[:, C_ONES, :], rhs=y2[:, 0:n], start=True, stop=True), reads=["cm", "y2"], writes=[pid])
                            s.op("dve", lambda e, P=P, n=n: e.tensor_scalar(out=rn[:, 0:n], in0=P[:, 0:n], scalar1=1e-6, scalar2=None, op0=ALU.add), reads=[pid], writes=["rn"])
                            s.op("act", lambda e, n=n: e.sqrt(out=rn[:, 0:n], in_=rn[:, 0:n]), reads=["rn"], writes=["rn"])
                            s.op("dve", lambda e, n=n: e.reciprocal(out=rn[:, 0:n], in_=rn[:, 0:n]), reads=["rn"], writes=["rn"])
                            sc = float(128 ** -0.5) if fc < 4 else 1.0
                            s.op("dve", lambda e, Y=Y, YN=YN, n=n, sc=sc: e.scalar_tensor_tensor(out=YN[:, 0:n], in0=Y[:, 0:n], scalar=sc, in1=rn[:, 0:n], op0=ALU.mult, op1=ALU.mult), reads=[yid, "rn"], writes=[nid])
                            s.dma(QK_s[fc * 128:(fc + 1) * 128, t0:t0 + n], YN[:, 0:n], reads=[nid], writes=["QK_s"], queue="pool")
                            src, sid = YN, nid
                        if fc >= 4:
                            PK = pK[it % 2]; kid = ("pK", it % 2); TK = tok[it % 2]; tid = ("tok", it % 2)
                            nsb = n // 128
                            for sb in range(nsb):
                                s.op("pe", lambda e, PK=PK, src=src, sb=sb: e.transpose(out=PK[:, sb * 128:(sb + 1) * 128], in_=src[:, sb * 128:(sb + 1) * 128], identity=ident), reads=[sid, "cm"], writes=[kid])
                            s.op("act", lambda e, PK=PK, TK=TK, n=n: e.copy(out=TK[:].rearrange("p a b -> p (a b)")[:, 0:n], in_=PK[:, 0:n]), reads=[kid], writes=[tid])
                            s.dma(KV_s[t0:t0 + n, (fc - 4) * 128:(fc - 3) * 128].rearrange("(sb p) f -> p sb f", p=128), TK[:, 0:nsb, :], reads=[tid], writes=["KV_s"], queue="pool")
                        it += 1
            s.flush()
        if upto <= 2:
            return nc

        with contextlib.ExitStack() as st:
            Sst = [T(st, f"Sst{i}", [128, 4, 128]) for i in range(2)]
            qT4 = T(st, "qT4", [128, 4, 128]); kT4 = T(st, "kT4", [128, 4, 128]); ktok = T(st, "ktok", [128, 4, 128]); vtok = T(st, "vtok", [128, 4, 128])
            gb = T(st, "gb", [128, 16]); sm = T(st, "sm", [128, 16]); ex = T(st, "ex", [128, 16]); beg = T(st, "beg", [128, 4])
            G1 = T(st, "G1", [128, 4, 128]); dl = T(st, "dl", [128, 4, 128]); du = T(st, "du", [128, 4, 128])
            Bm = [T(st, f"Bm{i}", [128, 4, 128]) for i in range(2)]; Cm = [T(st, f"Cm{i}", [128, 4, 128]) for i in range(2)]; Pm = [T(st, f"Pm{i}", [128, 4, 128]) for i in range(2)]
            aT = T(st, "aT", [128, 4, 128]); kbg = T(st, "kbg", [128, 4, 128]); vb = T(st, "vb", [128, 4, 128]); ktl = T(st, "ktl", [128, 4, 128])
            WT = T(st, "WT", [128, 4, 128]); U = T(st, "U", [128, 4, 128]); vn = T(st, "vn", [128, 4, 128]); o1 = T(st, "o1", [128, 4, 128]); ot = T(st, "ot", [128, 4, 128])
            pb = [PS(st, f"pb{i}", [128, 4, 128]) for i in range(8)]
            pA, pB_, pC, pD, pE, pF, pG, pH = pb
            pid = lambda k: ("pb", k)
            H4 = [128, 4, 128]
            bc_h = lambda ap2: ap2.unsqueeze(1).to_broadcast(H4)
            bc_l = lambda ap2: ap2.unsqueeze(2).to_broadcast(H4)
            ntl = (2 if "small" in dbg else NT)
            for dr in range(2):
                M1 = cm[:, C_M1F if dr == 0 else C_M1B, :]; NM1 = cm[:, C_NM1F if dr == 0 else C_NM1B, :]
                NB = cm[:, C_NLOW if dr == 0 else C_NUP, :]; NTm = cm[:, C_NUP if dr == 0 else C_NLOW, :]
                STR = cm[:, C_SLOW if dr == 0 else C_SUP, :]
                SS = Sst[dr]; ssid = ("Sst", dr)
                s.op("pool", lambda e, SS=SS: e.memset(SS[:], 0.0), writes=[ssid])
                order = [("c", i) for i in range(CTX // 128)] + [("l", i) for i in range(ntl)]
                if dr == 1:
                    order = [("c", i) for i in reversed(range(CTX // 128))] + [("l", i) for i in reversed(range(ntl))]
                for (kind, i) in order:
                    lat = kind == "l"
                    tg = i if not lat else CTX // 128 + i
                    tsl = slice(tg * 128, (tg + 1) * 128)
                    s.dma(qT4[:], QK_s[0:512, tsl].rearrange("(h p) t -> p h t", p=128), reads=["QK_s"], writes=["qT4"])
                    s.dma(kT4[:], QK_s[512:1024, tsl].rearrange("(h p) t -> p h t", p=128), reads=["QK_s"], writes=["kT4"], queue="act")
                    s.dma(ktok[:].rearrange("p h d -> p (h d)"), KV_s[tsl, 0:512], reads=["KV_s"], writes=["ktok"])
                    s.dma(vtok[:].rearrange("p h d -> p (h d)"), KV_s[tsl, 512:1024], reads=["KV_s"], writes=["vtok"], queue="act")
                    s.dma(gb[:], GB_s[tsl, :], reads=["GB_s"], writes=["gb"])
                    g = gb[:, dr * 4:dr * 4 + 4]; beta = gb[:, 8 + dr * 4:12 + dr * 4]
                    pAf = pA[:].rearrange("p a b -> p (a b)")
                    for k, L in enumerate((M1, cm[:, C_SAME, :], cm[:, C_SEL0, :], cm[:, C_SEL1, :])):
                        s.op("pe", lambda e, k=k, L=L, g=g: e.matmul(out=pAf[:, 4 * k:4 * k + 4], lhsT=L, rhs=g, start=True, stop=True), reads=["cm", "gb"], writes=[pid(0)])
                    s.op("dve", lambda e: e.tensor_copy(out=sm[:], in_=pAf[:, 0:16]), reads=[pid(0)], writes=["sm"])
                    s.op("dve", lambda e: e.tensor_tensor(out=sm[:, 4:8], in0=sm[:, 4:8], in1=sm[:, 0:4], op=ALU.subtract), reads=["sm"], writes=["sm"])
                    s.op("act", lambda e: e.activation(out=ex[:], in_=sm[:], func=AF.Exp), reads=["sm"], writes=["ex"])
                    s.op("dve", lambda e, beta=beta: e.tensor_tensor(out=beg[:], in0=ex[:, 0:4], in1=beta, op=ALU.mult), reads=["ex", "gb"], writes=["beg"])
                    s.op("dve", lambda e, g=g: e.tensor_tensor(out=G1[:], in0=bc_h(cm[:, C_SAME, :]), in1=bc_l(g), op=ALU.mult), reads=["cm", "gb"], writes=["G1"])
                    for hh in range(4):
                        s.op("pe", lambda e, hh=hh, M1=M1: e.matmul(out=pB_[:, hh, :], lhsT=M1, rhs=G1[:, hh, :], start=True, stop=False), reads=["cm", "G1"], writes=[pid(1)])
                        s.op("pe", lambda e, hh=hh, NM1=NM1: e.matmul(out=pB_[:, hh, :], lhsT=G1[:, hh, :], rhs=NM1, start=False, stop=True), reads=["cm", "G1"], writes=[pid(1)])
                    s.op("dve", lambda e, NB=NB: e.tensor_tensor(out=dl[:], in0=pB_[:], in1=bc_h(NB), op=ALU.add), reads=[pid(1), "cm"], writes=["dl"])
                    s.op("dve", lambda e, NTm=NTm: e.scalar_tensor_tensor(out=du[:], in0=pB_[:], scalar=-1.0, in1=bc_h(NTm), op0=ALU.mult, op1=ALU.add), reads=[pid(1), "cm"], writes=["du"])
                    s.op("act", lambda e: e.activation(out=dl[:], in_=dl[:], func=AF.Exp), reads=["dl"], writes=["dl"])
                    s.op("act", lambda e: e.activation(out=du[:], in_=du[:], func=AF.Exp), reads=["du"], writes=["du"])
                    for hh in range(4):
                        s.op("pe", lambda e, hh=hh: e.matmul(out=pC[:, hh, :], lhsT=kT4[:, hh, :], rhs=kT4[:, hh, :], start=True, stop=True), reads=["kT4"], writes=[pid(2)])
                    for hh in range(4):
                        s.op("pe", lambda e, hh=hh: e.matmul(out=pD[:, hh, :], lhsT=kT4[:, hh, :], rhs=qT4[:, hh, :], start=True, stop=True), reads=["kT4", "qT4"], writes=[pid(3)])
                    B0 = Bm[0]; C0 = Cm[0]; P0 = Pm[0]
                    s.op("dve", lambda e: e.tensor_tensor(out=B0[:], in0=pC[:], in1=dl[:], op=ALU.mult), reads=[pid(2), "dl"], writes=[("Bm", 0)])
                    s.op("pool", lambda e, STR=STR: e.tensor_tensor(out=B0[:], in0=B0[:], in1=bc_h(STR), op=ALU.mult), reads=[("Bm", 0), "cm"], writes=[("Bm", 0)])
                    s.op("pool", lambda e, beta=beta: e.tensor_tensor(out=B0[:], in0=B0[:], in1=bc_l(beta), op=ALU.mult), reads=[("Bm", 0), "gb"], writes=[("Bm", 0)])
                    s.op("dve", lambda e: e.tensor_tensor(out=aT[:], in0=pD[:], in1=du[:], op=ALU.mult), reads=[pid(3), "du"], writes=["aT"])
                    for hh in range(4):
                        s.op("pe", lambda e, hh=hh: e.transpose(out=pE[:, hh, :], in_=B0[:, hh, :], identity=ident), reads=[("Bm", 0), "cm"], writes=[pid(4)])
                    s.op("act", lambda e: e.copy(out=C0[:], in_=pE[:]), reads=[pid(4)], writes=[("Cm", 0)])
                    s.op("dve", lambda e: e.tensor_tensor(out=P0[:], in0=C0[:], in1=bc_h(ident), op=ALU.add), reads=[("Cm", 0), "cm"], writes=[("Pm", 0)])
                    cur = 0
                    for lv in range(1, 6):
                        nx = 1 - cur
                        Bc, Cc, Pc = Bm[cur], Cm[cur], Pm[cur]; Bn, Cn, Pn = Bm[nx], Cm[nx], Pm[nx]
                        for hh in range(4):
                            s.op("pe", lambda e, hh=hh, Bc=Bc, Cc=Cc: e.matmul(out=pF[:, hh, :], lhsT=Cc[:, hh, :], rhs=Bc[:, hh, :], start=True, stop=True), reads=[("Bm", cur), ("Cm", cur)], writes=[pid(5)])
                        s.op("act", lambda e, Bn=Bn: e.copy(out=Bn[:], in_=pF[:]), reads=[pid(5)], writes=[("Bm", nx)])
                        if lv < 5:
                            for hh in range(4):
                                s.op("pe", lambda e, hh=hh, Bc=Bc, Cc=Cc: e.matmul(out=pG[:, hh, :], lhsT=Bc[:, hh, :], rhs=Cc[:, hh, :], start=True, stop=True), reads=[("Bm", cur), ("Cm", cur)], writes=[pid(6)])
                            s.op("dve", lambda e, Cn=Cn: e.tensor_copy(out=Cn[:], in_=pG[:]), reads=[pid(6)], writes=[("Cm", nx)])
                        for hh in range(4):
                            s.op("pe", lambda e, hh=hh, Pc=Pc: e.matmul(out=pH[:, hh, :], lhsT=ident, rhs=Pc[:, hh, :], start=True, stop=False), reads=[("Pm", cur), "cm"], writes=[pid(7)])
                            s.op("pe", lambda e, hh=hh, Pc=Pc, Bn=Bn: e.matmul(out=pH[:, hh, :], lhsT=Bn[:, hh, :], rhs=Pc[:, hh, :], start=False, stop=True), reads=[("Pm", cur), ("Bm", nx)], writes=[pid(7)])
                        s.op("dve", lambda e, Pn=Pn: e.tensor_copy(out=Pn[:], in_=pH[:]), reads=[pid(7)], writes=[("Pm", nx)])
                        cur = nx
                    TT = Pm[cur]; ttid = ("Pm", cur)
                    s.op("pool", lambda e: e.tensor_tensor(out=kbg[:], in0=ktok[:], in1=bc_l(beg[:]), op=ALU.mult), reads=["ktok", "beg"], writes=["kbg"])
                    s.op("pool", lambda e, beta=beta: e.tensor_tensor(out=vb[:], in0=vtok[:], in1=bc_l(beta), op=ALU.mult), reads=["vtok", "gb"], writes=["vb"])
                    s.op("pool", lambda e: e.tensor_tensor(out=ktl[:], in0=ktok[:], in1=bc_l(ex[:, 4:8]), op=ALU.mult), reads=["ktok", "ex"], writes=["ktl"])
                    for hh in range(4):
                        s.op("pe", lambda e, hh=hh, TT=TT: e.matmul(out=pE[:, hh, :], lhsT=kbg[:, hh, :], rhs=TT[:, hh, :], start=True, stop=True), reads=["kbg", ttid], writes=[pid(4)])
                    s.op("act", lambda e: e.copy(out=WT[:], in_=pE[:]), reads=[pid(4)], writes=["WT"])
                    for hh in range(4):
                        s.op("pe", lambda e, hh=hh, TT=TT: e.matmul(out=pF[:, hh, :], lhsT=TT[:, hh, :], rhs=vb[:, hh, :], start=True, stop=True), reads=["vb", ttid], writes=[pid(5)])
                    s.op("dve", lambda e: e.tensor_copy(out=U[:], in_=pF[:]), reads=[pid(5)], writes=["U"])
                    for c in ((0, 1) if dr == 0 else (1, 0)):
                        pr = slice(64 * c, 64 * c + 64)
                        for hh in range(4):
                            s.op("pe", lambda e, hh=hh, SS=SS: e.matmul(out=pG[:, hh, :], lhsT=WT[:, hh, :], rhs=SS[:, hh, :], start=True, stop=True), reads=["WT", ssid], writes=[pid(6)])
                        s.op("dve", lambda e, pr=pr: e.tensor_tensor(out=vn[pr], in0=U[pr], in1=pG[pr], op=ALU.subtract), reads=["U", pid(6)], writes=["vn"])
                        for hh in range(4):
                            s.op("pe", lambda e, hh=hh, SS=SS: e.matmul(out=pH[:, hh, :], lhsT=qT4[:, hh, :], rhs=SS[:, hh, :], start=True, stop=True), reads=["qT4", ssid], writes=[pid(7)])
                        for hh in range(4):
                            s.op("pe", lambda e, hh=hh, pr=pr: e.matmul(out=pC[:, hh, :], lhsT=aT[pr, hh, :], rhs=vn[pr, hh, :], start=True, stop=True), reads=["aT", "vn"], writes=[pid(2)])
                        for hh in range(4):
                            s.op("pe", lambda e, hh=hh, pr=pr: e.matmul(out=pD[:, hh, :], lhsT=ktl[pr, hh, :], rhs=vn[pr, hh, :], start=True, stop=True), reads=["ktl", "vn"], writes=[pid(3)])
                        if lat:
                            s.op("dve", lambda e, pr=pr: e.tensor_tensor(out=o1[pr], in0=pH[pr], in1=bc_l(ex[:, 0:4])[pr], op=ALU.mult), reads=[pid(7), "ex"], writes=["o1"])
                            s.op("dve", lambda e, pr=pr: e.tensor_tensor(out=ot[pr], in0=o1[pr], in1=pC[pr], op=ALU.add), reads=["o1", pid(2)], writes=["ot"])
                        s.op("pool", lambda e, c=c, SS=SS: e.tensor_tensor(out=SS[:], in0=SS[:], in1=bc_l(ex[:, 8 + 4 * c:12 + 4 * c]), op=ALU.mult), reads=[ssid, "ex", pid(6), pid(7)], writes=[ssid])
                        s.op("dve", lambda e, SS=SS: e.tensor_tensor(out=SS[:], in0=SS[:], in1=pD[:], op=ALU.add), reads=[ssid, pid(3)], writes=[ssid])
                    if lat:
                        s.dma(OD_s[dr, i * 128:(i + 1) * 128, :], ot[:].rearrange("p h d -> p (h d)"), reads=["ot"], writes=["OD_s"], queue="pool")
            s.flush()
        if upto <= 3:
            return nc

        with contextlib.ExitStack() as st:
            kt = [T(st, f"kt{i}", [64, 2, 384]) for i in range(2)]
            vt = [T(st, f"vt{i}", [128, 3, 130]) for i in range(2)]
            ktc = T(st, "ktc", [64, 2, 256]); vtc = T(st, "vtc", [128, 2, 130])
            qt = [T(st, f"qt{i}", [64, 8, 128]) for i in range(2)]
            E = [T(st, f"E{i}", [128, 5, 512]) for i in range(2)]
            esink = T(st, "esink", [128, 8]); den = T(st, "den", [128, 8]); oa = [T(st, f"oa{i}", [128, 512]) for i in range(2)]
            pS = [PS(st, f"pS{i}", [128, 512]) for i in range(3)]
            pO = [PS(st, f"pO{i}", [128, 4, 65]) for i in range(2)]
            s.dma(ktc[:], KT_s[:, :, 0:CTX], reads=["KT_s"], writes=["ktc"])
            s.dma(vtc[:], V_s[0:CTX].rearrange("(b p) g d -> p b (g d)", p=128), reads=["V_s"], writes=["vtc"])
            s.dma(esink[:], sink_d.partition_broadcast(128), writes=["esink"])
            s.op("act", lambda e: e.activation(out=esink[:], in_=esink[:], func=AF.Exp), reads=["esink"], writes=["esink"])
            ntl = (2 if "small" in dbg else NT)
            nS = 0
            for i in range(ntl):
                lo = max(i - 1, 0); hi = min(i + 1, ntl - 1); nb = hi - lo + 1
                KT_ = kt[i % 2]; VT_ = vt[i % 2]; QT_ = qt[i % 2]; OA = oa[i % 2]
                s.dma(KT_[:, :, 0:nb * 128], KT_s[:, :, CTX + lo * 128:CTX + (hi + 1) * 128], reads=["KT_s"], writes=[("kt", i % 2)])
                s.dma(VT_[:, 0:nb, :], V_s[CTX + lo * 128:CTX + (hi + 1) * 128].rearrange("(b p) g d -> p b (g d)", p=128), reads=["V_s"], writes=[("vt", i % 2)], queue="act")
                s.dma(QT_[:], QT_s[:, :, i * 128:(i + 1) * 128], reads=["QT_s"], writes=[("qt", i % 2)])
                for g in range(2):
                    Eg = E[g]; eid = ("E", g)
                    kb = [("l", j - lo, (C_WPREV if j < i else (C_WNEXT if j > i else None))) for j in range(lo, hi + 1)] + [("c", 0, None), ("c", 1, None)]
                    for bi, (kk, bl, msk) in enumerate(kb):
                        P = pS[nS % 3]; psid = ("pS", nS % 3); nS += 1
                        lhs = KT_[:, g, bl * 128:(bl + 1) * 128] if kk == "l" else ktc[:, g, bl * 128:(bl + 1) * 128]
                        s.op("pe", lambda e, P=P, lhs=lhs, QT_=QT_, g=g: e.matmul(out=P[:].rearrange("p (h q) -> p h q", h=4), lhsT=lhs, rhs=QT_[:, 4 * g:4 * g + 4, :], start=True, stop=True),
                             reads=[("kt", i % 2), "ktc", ("qt", i % 2)], writes=[psid])
                        s.op("act", lambda e, P=P, Eg=Eg, bi=bi: e.activation(out=Eg[:, bi, :], in_=P[:], func=AF.Exp), reads=[psid], writes=[eid])
                        if msk is not None:
                            s.op("dve", lambda e, Eg=Eg, bi=bi, msk=msk: e.tensor_tensor(out=Eg[:, bi, :].rearrange("p (h q) -> p h q", h=4), in0=Eg[:, bi, :].rearrange("p (h q) -> p h q", h=4),
                                                                              in1=cm[:, msk, :].unsqueeze(1).to_broadcast([128, 4, 128]), op=ALU.mult), reads=[eid, "cm"], writes=[eid])
                    for hh in range(4):
                        for bi, (kk, bl, msk) in enumerate(kb):
                            rhs = VT_[:, bl, g * 65:(g + 1) * 65] if kk == "l" else vtc[:, bl, g * 65:(g + 1) * 65]
                            s.op("pe", lambda e, Eg=Eg, bi=bi, hh=hh, rhs=rhs, g=g, last=(bi == len(kb) - 1): e.matmul(out=pO[g][:, hh, :], lhsT=Eg[:, bi, hh * 128:(hh + 1) * 128], rhs=rhs, start=(bi == 0), stop=last),
                                 reads=[eid, ("vt", i % 2), "vtc"], writes=[("pO", g)])
                    s.op("dve", lambda e, g=g: e.tensor_tensor(out=den[:, 4 * g:4 * g + 4], in0=pO[g][:, :, 64], in1=esink[:, 4 * g:4 * g + 4], op=ALU.add), reads=[("pO", g), "esink"], writes=["den"])
                    s.op("dve", lambda e, g=g: e.reciprocal(out=den[:, 4 * g:4 * g + 4], in_=den[:, 4 * g:4 * g + 4]), reads=["den"], writes=["den"])
                    s.op("dve", lambda e, g=g, OA=OA: e.tensor_tensor(out=OA[:, g * 256:(g + 1) * 256].rearrange("p (h d) -> p h d", h=4), in0=pO[g][:, :, 0:64],
                                                              in1=den[:, 4 * g:4 * g + 4].unsqueeze(2).to_broadcast([128, 4, 64]), op=ALU.mult), reads=[("pO", g), "den"], writes=[("oa", i % 2)])
                s.dma(OA_s[i * 128:(i + 1) * 128, :], OA[:], reads=[("oa", i % 2)], writes=["OA_s"], queue="pool")
            s.flush()
        if upto <= 5:
            return nc

        UTr_s = nc.dram_tensor("UTr_s", [D, 16384], F32R, kind="Internal").ap()
        Vr_s = nc.dram_tensor("Vr_s", [16384, D], F32R, kind="Internal").ap()
        with contextlib.ExitStack() as st:
            cvb = [st.enter_context(nc.sbuf_tensor(_nm("cvb"), [128, 4096], F32R)) for _ in range(3)]
            ci = 0
            for r0 in range(0, D, 128):
                for c0 in range(0, 16384, 4096):
                    k = ci % 3; ci += 1
                    s.dma(cvb[k][:], pu_d[r0:r0 + 128, c0:c0 + 4096], writes=[("cvb", k)], queue="pool")
                    s.dma(UTr_s[r0:r0 + 128, c0:c0 + 4096], cvb[k][:], reads=[("cvb", k)], writes=["UTr_s"], queue=("sp", "act")[ci % 2])
            for r0 in range(0, 16384, 512):
                k = ci % 3; ci += 1
                s.dma(cvb[k][:], pv_d[r0:r0 + 512, :].rearrange("(p a) n -> p (a n)", p=128), writes=[("cvb", k)], queue="pool")
                s.dma(Vr_s[r0:r0 + 512, :].rearrange("(p a) n -> p (a n)", p=128), cvb[k][:], reads=[("cvb", k)], writes=["Vr_s"], queue=("sp", "act")[ci % 2])
            s.flush()
        if "stopconv" in dbg:
            return nc
        GI = 2
        NG = 128 // GI
        NB = 2
        with contextlib.ExitStack() as st:
            TR = lambda name, shape: st.enter_context(nc.sbuf_tensor(_nm(name), list(shape), F32R))
            xa = [T(st, f"xa{t}", [128, D]) for t in range(NB)]
            yb = T(st, "yb", [128, D]); tc_ = T(st, "tc_", [128, 8, 128]); gt = T(st, "gt", [128, 2048])
            h2r = [TR(f"h2r{t}", [128, 8, 128]) for t in range(NB)]
            od = T(st, "od", [128, 2, 512]); zt = T(st, "zt", [128, 512]); oat = T(st, "oat", [128, 512]); o2 = T(st, "o2", [128, 512])
            qsb = T(st, "qsb", [128, D]); qTs = T(st, "qTs", [64, 16, 128])
            sc = [T(st, f"sc{t}", [128, 16, 128]) for t in range(NB)]
            ssq = T(st, "ssq", [128, 4]); ss = T(st, "ss6", [128, 1]); rstd = T(st, "rstd6", [128, 1])
            t16 = T(st, "t16", [128, 2, 16]); c16 = T(st, "c16", [128, 16]); cand = T(st, "cand", [128, 16, 16]); wk = T(st, "wk", [128, 256])
            thr = T(st, "thr", [128, 8]); negm = T(st, "negm", [128, 8]); Zs = T(st, "Zs", [128, 8]); kap = T(st, "kap", [128, 8]); e16 = T(st, "e16", [128, 16])
            m1 = T(st, "m1", [128, 8]); th2 = T(st, "th2", [128, 8])
            dg = [TR(f"dg{t}", [128, 8, 128]) for t in range(NB)]
            keysT = T(st, "keysT", [64, 16, 128])
            big = T(st, "big", [128, 8 * D])
            wS = big[:].rearrange("p (k n) -> p k n", k=8)
            UT = [TR(f"UT{i}", [128, 8, GI * 128]) for i in range(2)]
            VG = [TR(f"VG{i}", [128, GI, D]) for i in range(2)]
            pe_ = [big[:, i * 4096:(i + 1) * 4096].rearrange("p (t h k) -> p t h k", t=NB, h=8) for i in range(2)]
            _Mr = TR("Mr", [128, NB, 8, GI * 128])
            Mr = [_Mr, _Mr]
            W5 = NB * GI * 128
            assert W5 == 512
            g1 = [gt[:, 0:512], gt[:, 512:1024]]; Pm_ = [gt[:, 1024:1536], gt[:, 1536:2048]]
            PT = [TR(f"PT{i}", [128, NB * GI, 128]) for i in range(2)]
            bk = [PS(st, f"bk{i}", [128, 512]) for i in range(8)]
            B = lambda k: ("bk", k)
            pT = [bk[0], bk[1]]; pY = [bk[2], bk[3]]
            s.dma(keysT[:], pkeys_d.rearrange("h p d k -> d (h p) k"), writes=["keysT"])

            def transpose8(src, sid, nkc, dst_off=0, extra=None):
                for kc in range(nkc):
                    b = (dst_off + kc) // 4
                    s.op("pe", lambda e, kc=kc, b=b: e.transpose(out=pT[b][:, ((dst_off + kc) % 4) * 128:((dst_off + kc) % 4 + 1) * 128], in_=src[:, kc * 128:(kc + 1) * 128], identity=ident), reads=[sid, "cm"], writes=[B(b)])
                for b in sorted(set((dst_off + kc) // 4 for kc in range(nkc))):
                    if b == 0:
                        s.op("act", lambda e, b=b: e.copy(out=tc_[:, b * 4:(b + 1) * 4, :].rearrange("p a b -> p (a b)"), in_=pT[b][:]), reads=[B(b)], writes=[("tc", b)])
                    else:
                        s.op("dve", lambda e, b=b: e.tensor_copy(out=tc_[:, b * 4:(b + 1) * 4, :].rearrange("p a b -> p (a b)"), in_=pT[b][:]), reads=[B(b)], writes=[("tc", b)])
                    if extra is not None:
                        dst, did = extra
                        if b == 0:
                            s.op("dve", lambda e, b=b, dst=dst: e.tensor_copy(out=dst[:, b * 4:(b + 1) * 4, :].rearrange("p a b -> p (a b)"), in_=pT[b][:]), reads=[B(b)], writes=[did])
                        else:
                            s.op("act", lambda e, b=b, dst=dst: e.copy(out=dst[:, b * 4:(b + 1) * 4, :].rearrange("p a b -> p (a b)"), in_=pT[b][:]), reads=[B(b)], writes=[did])

            def rms(src, sid):
                s.op("act", lambda e: e.activation(out=qsb[:], in_=src[:], func=AF.Square, accum_out=ss[:]), reads=[sid], writes=["qsb", "ss6"])
                s.op("dve", lambda e: e.tensor_scalar(out=rstd[:], in0=ss[:], scalar1=1.0 / D, scalar2=1e-6, op0=ALU.mult, op1=ALU.add), reads=["ss6"], writes=["rstd6"])
                s.op("act", lambda e: e.sqrt(out=rstd[:], in_=rstd[:]), reads=["rstd6"], writes=["rstd6"])
                s.op("dve", lambda e: e.reciprocal(out=rstd[:], in_=rstd[:]), reads=["rstd6"], writes=["rstd6"])

            nblk = (1 if "small" in dbg else NT // NB)
            _c = [int(x[3:]) for x in dbg if x.startswith("cut")]
            cut = _c[0] if _c else 99
            ngr = (2 if "small2" in dbg else NG)
            gcount = 0
            for blk in range(nblk):
              for tau in range(NB):
                i = blk * NB + tau
                XA = xa[tau]; xid = ("xa", tau); SC = sc[tau]; scid = ("sc", tau)
                tsl = slice(i * 128, (i + 1) * 128)
                s.dma(XA[:], x_d[tsl, :], writes=[xid])
                s.dma(od[:, 0, :], OD_s[0, tsl, :], reads=["OD_s"], writes=["od"], queue="act")
                s.dma(od[:, 1, :], OD_s[1, tsl, :], reads=["OD_s"], writes=["od"], queue="act")
                s.dma(zt[:], Z_s[tsl, :], reads=["Z_s"], writes=["zt"])
                s.dma(oat[:], OA_s[tsl, :], reads=["OA_s"], writes=["oat"], queue="act")
                s.dma(gt[:], GT_s[tsl, :], reads=["GT_s"], writes=[("gtq", 0), ("gtq", 1), ("gtq", 2), ("gtq", 3)])
                s.dma(wS[:, 0:4, :], wba_d.rearrange("(kc p) n -> p kc n", p=128), writes=[("pe", 0, 0), ("pe", 0, 1), ("pe", 1, 0), ("pe", 1, 1)])
                s.dma(wS[:, 4:8, :], wbd_d.rearrange("(kc p) n -> p kc n", p=128), writes=[("pe", 0, 0), ("pe", 0, 1), ("pe", 1, 0), ("pe", 1, 1)], queue="act")
                s.op("dve", lambda e: e.tensor_tensor(out=od[:, 0, :], in0=od[:, 0, :], in1=od[:, 1, :], op=ALU.add), reads=["od"], writes=["od"])
                s.op("dve", lambda e: e.tensor_tensor(out=o2[:], in0=od[:, 0, :], in1=od[:, 0, :], op=ALU.mult), reads=["od"], writes=["o2"])
                s.op("dve", lambda e: e.tensor_reduce(out=ssq[:], in_=o2[:].rearrange("p (h d) -> p h d", h=4), axis=AX.X, op=ALU.add), reads=["o2"], writes=["ssq"])
                s.op("dve", lambda e: e.tensor_scalar(out=ssq[:], in0=ssq[:], scalar1=1.0 / 128, scalar2=1e-6, op0=ALU.mult, op1=ALU.add), reads=["ssq"], writes=["ssq"])
                s.op("act", lambda e: e.sqrt(out=ssq[:], in_=ssq[:]), reads=["ssq"], writes=["ssq"])
                s.op("dve", lambda e: e.reciprocal(out=ssq[:], in_=ssq[:]), reads=["ssq"], writes=["ssq"])
                s.op("dve", lambda e: e.tensor_tensor(out=o2[:].rearrange("p (h d) -> p h d", h=4), in0=od[:, 0, :].rearrange("p (h d) -> p h d", h=4), in1=ssq[:].unsqueeze(2).to_broadcast([128, 4, 128]), op=ALU.mult), reads=["od", "ssq"], writes=["o2"])
                s.op("dve", lambda e: e.tensor_tensor(out=o2[:], in0=o2[:], in1=zt[:], op=ALU.mult), reads=["o2", "zt"], writes=["o2"])
                transpose8(oat, "oat", 4, 0)
                transpose8(o2, "o2", 4, 4)
                for half in range(2):
                    for kc in range(4):
                        s.op("pe", lambda e, half=half, kc=kc: e.matmul(out=pY[half][:], lhsT=tc_[:, kc, :], rhs=wS[:, kc, half * 512:(half + 1) * 512], start=(kc == 0), stop=(kc == 3)), reads=[("tc", 0), ("pe", 0, 0), ("pe", 0, 1), ("pe", 1, 0), ("pe", 1, 1)], writes=[B(2 + half)])
                    s.op("dve", lambda e, half=half: e.tensor_tensor(out=yb[:, half * 512:(half + 1) * 512], in0=pY[half][:], in1=gt[:, half * 512:(half + 1) * 512], op=ALU.mult), reads=[B(2 + half), ("gtq", half)], writes=["yb"])
                for half in range(2):
                    for kc in range(4):
                        s.op("pe", lambda e, half=half, kc=kc: e.matmul(out=pY[half][:], lhsT=tc_[:, 4 + kc, :], rhs=wS[:, 4 + kc, half * 512:(half + 1) * 512], start=(kc == 0), stop=(kc == 3)), reads=[("tc", 1), ("pe", 0, 0), ("pe", 0, 1), ("pe", 1, 0), ("pe", 1, 1)], writes=[B(2 + half)])
                    s.op("dve", lambda e, half=half: e.tensor_tensor(out=qsb[:, half * 512:(half + 1) * 512], in0=pY[half][:], in1=gt[:, 1024 + half * 512:1024 + (half + 1) * 512], op=ALU.mult), reads=[B(2 + half), ("gtq", 2 + half)], writes=["qsb"])
                s.op("pool", lambda e: e.tensor_tensor(out=yb[:], in0=yb[:], in1=qsb[:], op=ALU.add), reads=["yb", "qsb"], writes=["yb"])
                s.dma(wS[:], wout_d.rearrange("(kc p) n -> p kc n", p=128), writes=[("pe", 0, 0), ("pe", 0, 1), ("pe", 1, 0), ("pe", 1, 1)])
                transpose8(yb, "yb", 8, 0)
                for half in range(2):
                    for kc in range(8):
                        s.op("pe", lambda e, half=half, kc=kc: e.matmul(out=pY[half][:], lhsT=tc_[:, kc, :], rhs=wS[:, kc, half * 512:(half + 1) * 512], start=(kc == 0), stop=(kc == 7)), reads=[("tc", 0), ("tc", 1), ("pe", 0, 0), ("pe", 0, 1), ("pe", 1, 0), ("pe", 1, 1)], writes=[B(2 + half)])
                    s.op("dve", lambda e, half=half: e.tensor_tensor(out=yb[:, half * 512:(half + 1) * 512], in0=pY[half][:], in1=bvv(BV_GT1)[:, half * 512:(half + 1) * 512], op=ALU.mult), reads=[B(2 + half), "bv"], writes=["yb"])
                s.op("pool", lambda e, XA=XA: e.tensor_tensor(out=XA[:], in0=XA[:], in1=yb[:], op=ALU.add), reads=[xid, "yb"], writes=[xid])
                s.dma(wS[:], pwq_d.rearrange("(kc p) n -> p kc n", p=128), writes=[("pe", 0, 0), ("pe", 0, 1), ("pe", 1, 0), ("pe", 1, 1)])
                rms(XA, xid)
                s.op("dve", lambda e, XA=XA: e.scalar_tensor_tensor(out=yb[:], in0=XA[:], scalar=rstd[:, 0:1], in1=bvv(BV_G2)[:, :], op0=ALU.mult, op1=ALU.mult), reads=[xid, "rstd6", "bv"], writes=["yb"])
                s.op("pool", lambda e: e.tensor_tensor(out=yb[:], in0=yb[:], in1=bvv(BV_SH2)[:, :], op=ALU.add), reads=["yb", "bv"], writes=["yb"])
                transpose8(yb, "yb", 8, 0, extra=(h2r[tau], ("h2r", tau)))
                for half in range(2):
                    for kc in range(8):
                        s.op("pe", lambda e, half=half, kc=kc: e.matmul(out=pY[half][:], lhsT=tc_[:, kc, :], rhs=wS[:, kc, half * 512:(half + 1) * 512], start=(kc == 0), stop=(kc == 7)), reads=[("tc", 0), ("tc", 1), ("pe", 0, 0), ("pe", 0, 1), ("pe", 1, 0), ("pe", 1, 1)], writes=[B(2 + half)])
                    if half == 0:
                        s.op("act", lambda e: e.copy(out=qsb[:, 0:512], in_=pY[0][:]), reads=[B(2)], writes=["qsb"])
                    else:
                        s.op("dve", lambda e: e.tensor_copy(out=qsb[:, 512:1024], in_=pY[1][:]), reads=[B(3)], writes=["qsb"])
                for rd in range(4):
                    b = rd % 2
                    for k4 in range(4):
                        hp = rd * 4 + k4
                        s.op("pe", lambda e, hp=hp, b=b, k4=k4: e.transpose(out=pT[b][0:64, k4 * 128:(k4 + 1) * 128], in_=qsb[:, hp * 64:(hp + 1) * 64], identity=ident), reads=["qsb", "cm"], writes=[B(b)])
                    if b == 0:
                        s.op("act", lambda e, rd=rd, b=b: e.copy(out=qTs[:, rd * 4:(rd + 1) * 4, :].rearrange("p a b -> p (a b)"), in_=pT[b][0:64, :]), reads=[B(b)], writes=["qTs"])
                    else:
                        s.op("dve", lambda e, rd=rd, b=b: e.tensor_copy(out=qTs[:, rd * 4:(rd + 1) * 4, :].rearrange("p a b -> p (a b)"), in_=pT[b][0:64, :]), reads=[B(b)], writes=["qTs"])
                for rd in range(4):
                    b = rd % 2
                    for k4 in range(4):
                        hp = rd * 4 + k4
                        s.op("pe", lambda e, hp=hp, b=b, k4=k4: e.matmul(out=pY[b][:, k4 * 128:(k4 + 1) * 128], lhsT=qTs[:, hp, :], rhs=keysT[:, hp, :], start=True, stop=True), reads=["qTs", "keysT"], writes=[B(2 + b)])
                    if b == 0:
                        s.op("act", lambda e, rd=rd, b=b, SC=SC: e.copy(out=SC[:, rd * 4:(rd + 1) * 4, :].rearrange("p a b -> p (a b)"), in_=pY[b][:]), reads=[B(2 + b)], writes=[scid])
                    else:
                        s.op("dve", lambda e, rd=rd, b=b, SC=SC: e.tensor_copy(out=SC[:, rd * 4:(rd + 1) * 4, :].rearrange("p a b -> p (a b)"), in_=pY[b][:]), reads=[B(2 + b)], writes=[scid])
                for hh in range(8):
                    for p in range(2):
                        srow = SC[:, 2 * hh + p, :]
                        s.op("dve", lambda e, p=p, srow=srow: e.max(out=t16[:, p, 0:8], in_=srow), reads=[scid], writes=["t16"])
                        s.op("dve", lambda e, p=p, srow=srow: e.match_replace(out=wk[:, 0:128], in_to_replace=t16[:, p, 0:8], in_values=srow, imm_value=-1e30), reads=[scid, "t16"], writes=["wk"])
                        s.op("dve", lambda e, p=p: e.max(out=t16[:, p, 8:16], in_=wk[:, 0:128]), reads=["wk"], writes=["t16"])
                    s.op("dve", lambda e: e.tensor_tensor(out=cand[:], in0=t16[:, 0, :].unsqueeze(2).to_broadcast([128, 16, 16]), in1=t16[:, 1, :].unsqueeze(1).to_broadcast([128, 16, 16]), op=ALU.add), reads=["t16"], writes=["cand"])
                    cf = cand[:].rearrange("p a b -> p (a b)")
                    s.op("dve", lambda e, cf=cf: e.max(out=c16[:, 0:8], in_=cf), reads=["cand"], writes=["c16"])
                    s.op("dve", lambda e, cf=cf: e.match_replace(out=wk[:], in_to_replace=c16[:, 0:8], in_values=cf, imm_value=-1e30), reads=["cand", "c16"], writes=["wk"])
                    s.op("dve", lambda e: e.max(out=c16[:, 8:16], in_=wk[:]), reads=["wk"], writes=["c16"])
                    s.op("dve", lambda e, hh=hh: e.tensor_scalar(out=thr[:, hh:hh + 1], in0=c16[:, 15:16], scalar1=-1e-4, scalar2=None, op0=ALU.add), reads=["c16"], writes=["thr"])
                    s.op("dve", lambda e, hh=hh: e.tensor_scalar(out=negm[:, hh:hh + 1], in0=c16[:, 0:1], scalar1=-1.0, scalar2=None, op0=ALU.mult), reads=["c16"], writes=["negm"])
                    s.op("dve", lambda e, hh=hh: e.tensor_copy(out=m1[:, hh:hh + 1], in_=t16[:, 0, 0:1]), reads=["t16"], writes=["m1"])
                    s.op("act", lambda e, hh=hh: e.activation(out=e16[:], in_=c16[:], func=AF.Exp, bias=negm[:, hh:hh + 1], accum_out=Zs[:, hh:hh + 1]), reads=["c16", "negm"], writes=["e16", "Zs"])
                s.op("dve", lambda e: e.tensor_tensor(out=kap[:], in0=thr[:], in1=negm[:], op=ALU.add), reads=["thr", "negm"], writes=["kap"])
                s.op("act", lambda e: e.activation(out=kap[:], in_=kap[:], func=AF.Exp), reads=["kap"], writes=["kap"])
                s.op("dve", lambda e: e.reciprocal(out=Zs[:], in_=Zs[:]), reads=["Zs"], writes=["Zs"])
                s.op("dve", lambda e: e.tensor_tensor(out=kap[:], in0=kap[:], in1=Zs[:], op=ALU.mult), reads=["kap", "Zs"], writes=["kap"])
                s.op("dve", lambda e: e.tensor_tensor(out=th2[:], in0=thr[:], in1=m1[:], op=ALU.subtract), reads=["thr", "m1"], writes=["th2"])
                sc4 = SC[:].rearrange("p (h q) k -> p h q k", q=2)
                s.op("dve", lambda e, sc4=sc4: e.tensor_tensor(out=sc4[:, :, 0, :], in0=sc4[:, :, 0, :], in1=m1[:].unsqueeze(2).to_broadcast([128, 8, 128]), op=ALU.subtract), reads=[scid, "m1"], writes=[scid])
                s.op("dve", lambda e, sc4=sc4: e.tensor_tensor(out=sc4[:, :, 1, :], in0=sc4[:, :, 1, :], in1=th2[:].unsqueeze(2).to_broadcast([128, 8, 128]), op=ALU.subtract), reads=[scid, "th2"], writes=[scid])
                s.op("act", lambda e, SC=SC: e.activation(out=SC[:], in_=SC[:], func=AF.Exp), reads=[scid], writes=[scid])
                for hh in range(8):
                    s.op("dve", lambda e, hh=hh, tau=tau: e.tensor_scalar(out=dg[tau][:, hh, :], in0=ident, scalar1=kap[:, hh:hh + 1], scalar2=None, op0=ALU.mult), reads=["cm", "kap"], writes=[("dg", tau)])
              pU = [[bk[4 + 2 * t + hf] for hf in range(2)] for t in range(NB)]
              ub = [None] * (ngr + 1)

              def g_load(g):
                  nonlocal gcount
                  u = gcount % 2; gcount += 1
                  ub[g] = u
                  e0 = g * GI * 128
                  s.dma(UT[u][:], UTr_s[:, e0:e0 + GI * 128].rearrange("(kc p) n -> p kc n", p=128), reads=["UTr_s"], writes=[("UT", u)], queue=("sp", "act")[u])
                  s.dma(VG[u][:], Vr_s[e0:e0 + GI * 128, :].rearrange("(a p) n -> p a n", p=128), reads=["Vr_s"], writes=[("VG", u)], queue=("act", "sp")[u])

              def g_prod(g):
                  u = ub[g]
                  for tau in range(NB):
                      sc4 = sc[tau][:].rearrange("p (h q) k -> p h q k", q=2)
                      e1b = sc4[:, :, 0, g * GI:(g + 1) * GI].unsqueeze(3).to_broadcast([128, 8, GI, 128])
                      e2b = sc4[:, :, 1, :].unsqueeze(2).to_broadcast([128, 8, GI, 128])
                      s.op("pool", lambda e, e1b=e1b, e2b=e2b, tau=tau, u=u: e.tensor_tensor(out=pe_[u][:, tau, :, :].rearrange("p h (a k) -> p h a k", a=GI), in0=e1b, in1=e2b, op=ALU.mult), reads=[("sc", tau)], writes=[("pe", u, tau)])

              def g_act(g):
                  u = ub[g]; pR = bk[u]
                  for tau in range(NB):
                      for kc in range(8):
                          s.op("pe", lambda e, kc=kc, u=u, tau=tau, pR=pR: e.matmul(out=pR[:, tau * GI * 128:(tau + 1) * GI * 128], lhsT=h2r[tau][:, kc, :], rhs=UT[u][:, kc, :], start=(kc == 0), stop=(kc == 7)), reads=[("h2r", tau), ("UT", u)], writes=[B(u)])

              def g_gelu(g):
                  u = ub[g]; pR = bk[u]
                  s.op("act", lambda e, pR=pR, u=u: e.activation(out=g1[u], in_=pR[:, 0:W5], func=AF.Gelu_apprx_tanh), reads=[B(u)], writes=[("gtq", u)])

              def g_mask(g):
                  u = ub[g]
                  for tau in range(NB):
                      s.op("dve", lambda e, u=u, tau=tau: e.scalar_tensor_tensor(out=_Mr[:, tau], in0=pe_[u][:, tau], scalar=1.0, in1=pe_[u][:, tau], op0=ALU.is_ge, op1=ALU.mult), reads=[("pe", u, tau)], writes=[("Mr", tau)])

              def g_gd(g):
                  u = ub[g]; pG = bk[2 + u]
                  for tau in range(NB):
                      for hh in range(8):
                          s.op("pe", lambda e, hh=hh, tau=tau, pG=pG: e.matmul(out=pG[:, tau * GI * 128:(tau + 1) * GI * 128], lhsT=dg[tau][:, hh, :], rhs=_Mr[:, tau, hh, :], start=(hh == 0), stop=(hh == 7)), reads=[("dg", tau), ("Mr", tau)], writes=[B(2 + u)])

              def g_pm(g):
                  u = ub[g]; pG = bk[2 + u]
                  s.op("dve", lambda e, u=u, pG=pG: e.tensor_tensor(out=Pm_[u], in0=g1[u], in1=pG[:, 0:W5], op=ALU.mult), reads=[("gtq", u), B(2 + u)], writes=[("gtq", 2 + u)])

              def g_tr(g):
                  u = ub[g]; pW = bk[2 + u]
                  for k in range(NB * GI):
                      s.op("pe", lambda e, k=k, u=u, pW=pW: e.transpose(out=pW[:, k * 128:(k + 1) * 128], in_=Pm_[u][:, k * 128:(k + 1) * 128], identity=ident), reads=[("gtq", 2 + u), "cm"], writes=[B(2 + u)])
                  s.op("act", lambda e, u=u, pW=pW: e.copy(out=PT[u][:].rearrange("p a b -> p (a b)"), in_=pW[:, 0:W5]), reads=[B(2 + u)], writes=[("PT", u)])

              def g_out(g):
                  u = ub[g]
                  for tau in range(NB):
                      for a in range(GI):
                          for half in range(2):
                              s.op("pe", lambda e, a=a, half=half, u=u, tau=tau, first=(g == 0 and a == 0), last=(g == ngr - 1 and a == GI - 1): e.matmul(out=pU[tau][half][:], lhsT=PT[u][:, tau * GI + a, :], rhs=VG[u][:, a, half * 512:(half + 1) * 512], start=first, stop=last),
                                   reads=[("PT", u), ("VG", u)], writes=[B(4 + 2 * tau + half)])

              g_load(0); g_prod(0); g_act(0); g_gelu(0); g_mask(0)
              for g in range(ngr):
                  nxt = g + 1 < ngr
                  if nxt:
                      g_load(g + 1); g_prod(g + 1)
                  g_gd(g)
                  if nxt:
                      g_act(g + 1); g_gelu(g + 1)
                  g_pm(g)
                  if nxt:
                      g_mask(g + 1)
                  g_tr(g)
                  g_out(g)
              for tau in range(NB if cut >= 7 else 0):
                i = blk * NB + tau
                XA = xa[tau]; xid = ("xa", tau)
                for half in range(2):
                    s.op("dve", lambda e, half=half, tau=tau: e.tensor_tensor(out=yb[:, half * 512:(half + 1) * 512], in0=pU[tau][half][:], in1=bvv(BV_GT2)[:, half * 512:(half + 1) * 512], op=ALU.mult), reads=[B(4 + 2 * tau + half), "bv"], writes=["yb"])
                s.op("pool", lambda e, XA=XA: e.tensor_tensor(out=XA[:], in0=XA[:], in1=yb[:], op=ALU.add), reads=[xid, "yb"], writes=[xid])
                rms(XA, xid)
                s.op("dve", lambda e, XA=XA: e.scalar_tensor_tensor(out=yb[:], in0=XA[:], scalar=rstd[:, 0:1], in1=bvv(BV_FN)[:, :], op0=ALU.mult, op1=ALU.mult), reads=[xid, "rstd6", "bv"], writes=["yb"])
                s.dma(out_d[i * 128:(i + 1) * 128, :], yb[:], reads=["yb"], writes=["out"], queue="pool")
            s.flush()
        return nc


def _rope(s, src, dst, tmp, R, rid, H, sid, did):
    sv = src.rearrange("p (h a b c) -> p h a b c", h=H, a=2, b=2)
    dv = dst.rearrange("p (h a b c) -> p h a b c", h=H, a=2, b=2)
    tv = tmp[:, 0:H * 32].rearrange("p (h a c) -> p h a c", h=H, a=2)
    rv = R[:].rearrange("p (a b c) -> p a b c", a=2, b=2)
    cosb = rv[:, :, 0, :].unsqueeze(1).to_broadcast([128, H, 2, 16])
    sinb = rv[:, :, 1, :].unsqueeze(1).to_broadcast([128, H, 2, 16])
    x1 = sv[:, :, :, 0, :]; x2 = sv[:, :, :, 1, :]
    o1 = dv[:, :, :, 0, :]; o2 = dv[:, :, :, 1, :]
    s.op("dve", lambda e: e.tensor_tensor(out=o1, in0=x1, in1=cosb, op=ALU.mult), reads=[sid, rid], writes=[did])
    s.op("dve", lambda e: e.tensor_tensor(out=tv, in0=x2, in1=sinb, op=ALU.mult), reads=[sid, rid], writes=["tmp"])
    s.op("dve", lambda e: e.tensor_tensor(out=o1, in0=o1, in1=tv, op=ALU.subtract), reads=[did, "tmp"], writes=[did])
    s.op("dve", lambda e: e.tensor_tensor(out=o2, in0=x1, in1=sinb, op=ALU.mult), reads=[sid, rid, did], writes=[did])
    s.op("dve", lambda e: e.tensor_tensor(out=tv, in0=x2, in1=cosb, op=ALU.mult), reads=[sid, rid, did], writes=["tmp"])
    s.op("dve", lambda e: e.tensor_tensor(out=o2, in0=o2, in1=tv, op=ALU.add), reads=[did, "tmp"], writes=[did])


def _host_inputs(inputs, b, consts):
    g = lambda k: np.ascontiguousarray(inputs[k], dtype=np.float32)
    m = {
        "x": g("x")[b], "c": g("c")[b], "ctx": g("ctx")[b], "c_ctx": g("c_ctx"),
        "w_ada": g("w_ada")[0], "b_ada": g("b_ada")[0], "norm_mix": g("norm_mix")[0], "norm_ffn": g("norm_ffn")[0],
        "w_in": g("w_in")[0], "b_gate": g("b_gate")[0], "attn_sink": g("attn_sink")[0], "dn_conv": g("dn_conv")[0],
        "dn_a_log_f": g("dn_a_log_f")[0], "dn_dt_bias_f": g("dn_dt_bias_f")[0], "dn_a_log_b": g("dn_a_log_b")[0], "dn_dt_bias_b": g("dn_dt_bias_b")[0],
        "dn_norm": g("dn_norm")[0], "w_br_attn": g("w_br_attn")[0], "w_br_dn": g("w_br_dn")[0], "w_out": g("w_out")[0],
        "peer_wq": g("peer_wq")[0], "final_norm": g("final_norm"),
    }
    m.update(consts)
    return {k: np.ascontiguousarray(v) for k, v in m.items()}


_SHARED = {}


def kernel(**inputs):
    consts = _consts()
    nc = build()
    keysT = np.ascontiguousarray(np.transpose(np.asarray(inputs["peer_keys"], np.float32)[0], (0, 1, 3, 2)))
    uT = np.ascontiguousarray(np.asarray(inputs["peer_u"], np.float32)[0].T)
    pv = np.ascontiguousarray(np.asarray(inputs["peer_v"], np.float32)[0])
    in_maps = []
    for b in range(8):
        m = _host_inputs(inputs, b, consts)
        m["peer_keysT"] = keysT; m["peer_uT"] = uT; m["peer_v"] = pv
        in_maps.append(m)
    res = run_bass_kernel_spmd(nc, in_maps, core_ids=list(range(8)))
    return np.stack([np.asarray(r["out"], dtype=np.float32) for r in res.results], axis=0)
```

```python
import contextlib
import numpy as np
import concourse.bass as bass
import concourse.mybir as mybir
from concourse.bass_utils import run_bass_kernel_spmd

F32 = mybir.dt.float32
F32R = mybir.dt.float32r
ALU = mybir.AluOpType
AF = mybir.ActivationFunctionType
AX = mybir.AxisListType

D = 1024
S = 8192
CTX = 256
TALL = CTX + S
NT = S // 128
IN_COLS = 4880
NEG = -30000.0


class _Ins:
    __slots__ = ("eng", "fn", "deps", "signal", "sig_no", "dma", "idx")

    def __init__(self, eng, fn, dma=None):
        self.eng = eng
        self.fn = fn
        self.deps = []
        self.signal = False
        self.sig_no = None
        self.dma = dma
        self.idx = None


class Sch:
    EPOCH = 20000
    NDMA = 24
    NEP = 16

    def __init__(self, nc, st):
        self.nc = nc
        self.engs = ("pe", "act", "dve", "pool", "sp")
        self.nep = {"pe": 12, "act": 4, "dve": 6, "pool": 3, "sp": 1}
        self.sems = {e: [st.enter_context(nc.semaphore(f"s_{e}_{i}")) for i in range(self.nep[e])] for e in self.engs}
        self.dsems = [st.enter_context(nc.semaphore(f"s_dma_{i}")) for i in range(self.NDMA)]
        self.sigc = {e: 0 for e in self.engs}
        self.dma_rr = 0
        self.dma_cnt = [0] * self.NDMA
        self.dma_last = [None] * self.NDMA
        self._reset()

    def _reset(self):
        self.q = {e: [] for e in self.engs}
        self.lastw = {}
        self.readers = {}

    def _add(self, ins, reads, writes):
        q = self.q[ins.eng]
        ins.idx = len(q)
        deps = []
        for r in reads:
            w = self.lastw.get(r)
            if w is not None:
                deps.append((w, "raw"))
        for w_ in writes:
            w = self.lastw.get(w_)
            if w is not None:
                deps.append((w, "waw"))
            for rd in self.readers.get(w_, ()):
                deps.append((rd, "war"))
        for d, kind in deps:
            if d is ins:
                continue
            if d.dma is None and ins.dma is None and d.eng == ins.eng:
                if ins.eng == "pe":
                    continue
                if kind != "raw":
                    continue
            ins.deps.append(d)
            if d.dma is None:
                d.signal = True
        for r in reads:
            self.readers.setdefault(r, []).append(ins)
        for w_ in writes:
            self.lastw[w_] = ins
            self.readers[w_] = []
        q.append(ins)
        return ins

    PSUM_NAMES = {"bk", "pm", "pT", "pY", "pX", "pN", "pK", "pb", "pS", "pO", "pQ", "pZ", "pR", "pU", "pW"}

    def op(self, eng, fn, reads=(), writes=()):
        writes = list(writes)
        if eng != "pe":
            for r in reads:
                if isinstance(r, tuple) and r[0] in self.PSUM_NAMES and r not in writes:
                    writes.append(r)
        return self._add(_Ins(eng, fn), list(reads), writes)

    def dma(self, out, in_, reads=(), writes=(), queue="sp", **kw):
        slot = self.dma_rr
        self.dma_rr = (self.dma_rr + 1) % self.NDMA
        self.dma_cnt[slot] += 1
        n = self.dma_cnt[slot]
        ins = _Ins(queue, lambda e: e.dma_start(out=out, in_=in_, **kw), dma=(slot, n))
        prev = self.dma_last[slot]
        self._add(ins, list(reads), list(writes))
        if prev is not None:
            ins.deps.append(prev)
        self.dma_last[slot] = ins
        return ins

    def flush(self):
        nc = self.nc
        for e, q in self.q.items():
            for ins in q:
                if ins.dma is None and ins.signal:
                    ins.sig_no = self.sigc[e]
                    self.sigc[e] += 1
            assert self.sigc[e] < self.EPOCH * self.nep[e], f"too many signals on {e}: {self.sigc[e]}"
        dma_final = list(self.dma_cnt)
        with nc.Block() as block:
            def run(ename):
                def body(eng):
                    seen_c = {}
                    seen_d = {}
                    for ins in self.q[ename]:
                        wc = {}
                        wd = {}
                        for d in ins.deps:
                            if d.dma is None:
                                if d.sig_no is None:
                                    continue
                                if seen_c.get(d.eng, -1) < d.sig_no:
                                    wc[d.eng] = max(wc.get(d.eng, -1), d.sig_no)
                            else:
                                s_, n = d.dma
                                if seen_d.get(s_, 0) < n:
                                    wd[s_] = max(wd.get(s_, 0), n)
                        for e2, sn in wc.items():
                            eng.wait_ge(self.sems[e2][sn // self.EPOCH], sn % self.EPOCH + 1)
                            seen_c[e2] = sn
                        for s_, n in wd.items():
                            eng.wait_ge(self.dsems[s_], 16 * n)
                            seen_d[s_] = n
                        h = ins.fn(eng)
                        if ins.dma is not None:
                            h.then_inc(self.dsems[ins.dma[0]], 16)
                        elif ins.signal:
                            h.then_inc(self.sems[ename][ins.sig_no // self.EPOCH], 1)
                    if ename == "sp":
                        for s_, n in enumerate(dma_final):
                            if n > 0:
                                eng.wait_ge(self.dsems[s_], 16 * n)
                return body

            block.sync(run("sp"))
            block.tensor(run("pe"))
            block.scalar(run("act"))
            block.vector(run("dve"))
            block.gpsimd(run("pool"))
        nc.all_engine_barrier()
        self._reset()


def _consts():
    c = {}
    ident = np.eye(128, dtype=np.float32)
    ones = np.ones((128, 128), np.float32)
    idx = np.arange(128)
    same = (idx[:, None] // 64 == idx[None, :] // 64).astype(np.float32)
    m1f = ((idx[:, None] <= idx[None, :]) * same).astype(np.float32)
    m1b = ((idx[:, None] >= idx[None, :]) * same).astype(np.float32)
    sel0 = np.zeros((128, 128), np.float32); sel0[:64, :] = 1
    sel1 = np.zeros((128, 128), np.float32); sel1[64:, :] = 1
    low_incl = ((idx[None, :] <= idx[:, None]) * same)
    up_incl = ((idx[None, :] >= idx[:, None]) * same)
    low_strict = ((idx[None, :] < idx[:, None]) * same)
    up_strict = ((idx[None, :] > idx[:, None]) * same)
    negmask = lambda m: np.where(m > 0, 0.0, NEG).astype(np.float32)
    w_prev = (idx[None, :] <= idx[:, None]).astype(np.float32)
    w_next = (idx[:, None] <= idx[None, :]).astype(np.float32)
    mats = [ident, ones, same, m1f, -m1f, m1b, -m1b, sel0, sel1,
            negmask(low_incl), negmask(up_incl), -low_strict.astype(np.float32), -up_strict.astype(np.float32),
            w_prev, w_next]
    c["cmat"] = np.ascontiguousarray(np.stack(mats, axis=1)).astype(np.float32)
    pos = np.arange(S)
    inv = (10000.0 ** (-np.arange(16, dtype=np.float32) / 16)).astype(np.float32)
    ar = (pos // 64).astype(np.float32)[:, None] * inv[None, :]
    ac = (pos % 64).astype(np.float32)[:, None] * inv[None, :]
    c["rope"] = np.concatenate([np.cos(ar), np.sin(ar), np.cos(ac), np.sin(ac)], axis=1).astype(np.float32)
    return c

(C_ID, C_ONES, C_SAME, C_M1F, C_NM1F, C_M1B, C_NM1B, C_SEL0, C_SEL1, C_NLOW, C_NUP, C_SLOW, C_SUP, C_WPREV, C_WNEXT) = range(15)


def build(upto=99, dbg=()):
    nc = bass.Bass("TRN2", target_bir_lowering=False)
    nc.dge_precook = False
    inp = lambda name, shape: nc.dram_tensor(name, list(shape), F32, kind="ExternalInput").ap()
    x_d = inp("x", [S, D]); c_d = inp("c", [D]); ctx_d = inp("ctx", [CTX, D]); cctx_d = inp("c_ctx", [D])
    wada_d = inp("w_ada", [D, 6 * D]); bada_d = inp("b_ada", [6 * D])
    nmix_d = inp("norm_mix", [D]); nffn_d = inp("norm_ffn", [D])
    win_d = inp("w_in", [D, IN_COLS]); bgate_d = inp("b_gate", [2 * D])
    sink_d = inp("attn_sink", [8]); conv_d = inp("dn_conv", [5, 1536])
    alf_d = inp("dn_a_log_f", [4]); dtf_d = inp("dn_dt_bias_f", [4]); alb_d = inp("dn_a_log_b", [4]); dtb_d = inp("dn_dt_bias_b", [4])
    dnn_d = inp("dn_norm", [128]); wba_d = inp("w_br_attn", [512, D]); wbd_d = inp("w_br_dn", [512, D]); wout_d = inp("w_out", [D, D])
    pwq_d = inp("peer_wq", [D, D]); pkeys_d = inp("peer_keysT", [8, 2, 64, 128]); pu_d = inp("peer_uT", [D, 16384]); pv_d = inp("peer_v", [16384, D])
    fnorm_d = inp("final_norm", [D]); cmat_d = inp("cmat", [128, 15, 128]); rope_d = inp("rope", [S, 64])
    out_d = nc.dram_tensor("out", [S, D], F32, kind="ExternalOutput").ap()
    scr = lambda name, shape: nc.dram_tensor(name, list(shape), F32, kind=("ExternalOutput" if name in dbg else "Internal")).ap()
    QT_s = scr("QT_s", [64, 8, S])
    KT_s = scr("KT_s", [64, 2, TALL])
    V_s = scr("V_s", [TALL, 2, 65])
    RT_s = scr("RT_s", [1536, TALL])
    Z_s = scr("Z_s", [S, 512])
    GB_s = scr("GB_s", [TALL, 16])
    GT_s = scr("GT_s", [S, 2048])
    QK_s = scr("QK_s", [1024, TALL])
    KV_s = scr("KV_s", [TALL, 1024])
    OD_s = scr("OD_s", [2, S, 512])
    OA_s = scr("OA_s", [S, 512])
    MOD_s = scr("MOD_s", [8, D])

    with contextlib.ExitStack() as gst:
        s = Sch(nc, gst)
        _uid = [0]

        def _nm(name):
            _uid[0] += 1
            return f"{name}_u{_uid[0]}"
        T = lambda st, name, shape: st.enter_context(nc.sbuf_tensor(_nm(name), list(shape), F32))
        PS = lambda st, name, shape: st.enter_context(nc.psum_tensor(_nm(name), list(shape), F32))
        cm = T(gst, "cm", [128, 15, 128])
        s.dma(cm[:], cmat_d, writes=["cm"])
        ident = cm[:, C_ID, :]
        BV_G1, BV_SH1, BV_GT1, BV_G2, BV_SH2, BV_GT2, BV_CG1, BV_CSH1, BV_FN = range(9)
        bvB = T(gst, "bvB", [128, 5, D])
        stA = contextlib.ExitStack()
        bvA = T(stA, "bvA", [128, 4, D])
        _amap = {BV_G1: 0, BV_SH1: 1, BV_CG1: 2, BV_CSH1: 3}
        _bmap = {BV_GT1: 0, BV_G2: 1, BV_SH2: 2, BV_GT2: 3, BV_FN: 4}

        def bvv(k):
            return bvA[:, _amap[k], :] if k in _amap else bvB[:, _bmap[k], :]

        with contextlib.ExitStack() as st:
            cc = T(st, "cc", [128, 2, 8]); cs = T(st, "cs", [128, 2, 8]); lh = T(st, "lh", [128, 2, 8, 128])
            wa = [T(st, f"wa{i}", [128, 8, 512]) for i in range(2)]
            bb = T(st, "bb", [128, 6 * D]); nm = T(st, "nm", [128, 2, D])
            pm = [PS(st, f"pm{i}", [128, 512]) for i in range(2)]
            s.dma(cc[:, 0, :], c_d.rearrange("(kc p) -> p kc", p=128), writes=["cc"], allow_slow_non_contiguous=True)
            s.dma(cc[:, 1, :], cctx_d.rearrange("(kc p) -> p kc", p=128), writes=["cc"], allow_slow_non_contiguous=True)
            s.dma(bb[:], bada_d.partition_broadcast(128), writes=["bb"])
            s.dma(nm[:, 0, :], nmix_d.partition_broadcast(128), writes=["nm"])
            s.dma(nm[:, 1, :], nffn_d.partition_broadcast(128), writes=["nm"])
            s.dma(bvv(BV_FN)[:, :], fnorm_d.partition_broadcast(128), writes=["bv"])
            s.op("act", lambda e: e.activation(out=cs[:], in_=cc[:], func=AF.Silu), reads=["cc"], writes=["cs"])
            s.op("dve", lambda e: e.tensor_copy(out=lh[:], in_=cs[:].unsqueeze(3).to_broadcast([128, 2, 8, 128])), reads=["cs"], writes=["lh"])
            jobs = [(0, nb) for nb in range(12)] + [(1, nb) for nb in range(4)]
            for ji, (w, nb) in enumerate(jobs):
                wt = wa[ji % 2]; p = pm[ji % 2]
                s.dma(wt[:], wada_d[:, nb * 512:(nb + 1) * 512].rearrange("(kc p) n -> p kc n", p=128), writes=[("wa", ji % 2)], queue=("sp" if ji % 2 == 0 else "act"))
                for kc in range(8):
                    s.op("pe", lambda e, w=w, kc=kc, wt=wt, p=p: e.matmul(out=p[:], lhsT=lh[:, w, kc, :], rhs=wt[:, kc, :], start=(kc == 0), stop=(kc == 7)),
                         reads=["lh", ("wa", ji % 2)], writes=[("pm", ji % 2)])
                ch, half = nb // 2, nb % 2
                if w == 0:
                    dst = {0: BV_SH1, 1: BV_G1, 2: BV_GT1, 3: BV_SH2, 4: BV_G2, 5: BV_GT2}[ch]
                else:
                    dst = {0: BV_CSH1, 1: BV_CG1}[ch]
                o = bvv(dst)[:, half * 512:(half + 1) * 512]
                s.op("dve", lambda e, o=o, p=p, nb=nb: e.tensor_tensor(out=o, in0=p[:], in1=bb[:, nb * 512:(nb + 1) * 512], op=ALU.add),
                     reads=[("pm", ji % 2), "bb"], writes=["bv"])
            for dst, ni in ((BV_G1, 0), (BV_G2, 1), (BV_CG1, 0)):
                s.op("dve", lambda e, dst=dst, ni=ni: e.scalar_tensor_tensor(out=bvv(dst)[:, :], in0=bvv(dst)[:, :], scalar=1.0, in1=nm[:, ni, :], op0=ALU.add, op1=ALU.mult),
                     reads=["bv", "nm"], writes=["bv"])
            s.flush()
        if upto <= 0:
            stA.close()
            return nc

        blocks = [(0, 512), (512, 256), (768, 512), (1280, 512), (1792, 512), (2304, 512), (2816, 16)] + [(2832 + 512 * i, 512) for i in range(4)]
        with contextlib.ExitStack() as st:
            xt = [T(st, f"xt{i}", [128, D]) for i in range(2)]
            junk = T(st, "junk", [128, D]); ss = T(st, "ss", [128, 1]); rstd = T(st, "rstd", [128, 1])
            h = T(st, "h", [128, D]); hT = T(st, "hT", [128, 8, 128])
            wb = [T(st, f"wb{i}", [128, 8, 512]) for i in range(3)]
            rp = [T(st, f"rp{i}", [128, 64]) for i in range(2)]
            qs = T(st, "qs", [128, 512]); qr = T(st, "qr", [128, 512]); tmp = T(st, "tmp", [128, 512])
            qT = T(st, "qT", [64, 8, 128]); kvs = T(st, "kvs", [128, 256]); kr = T(st, "kr", [128, 128]); kT = T(st, "kT", [64, 2, 128])
            va = T(st, "va", [128, 2, 65]); rw = T(st, "rw", [128, 512]); rT = T(st, "rT", [128, 4, 128])
            zz = T(st, "zz", [128, 512]); gn = T(st, "gn", [128, 128]); ab = T(st, "ab", [128, 16]); abc = T(st, "abc", [128, 2, 8])
            gbo = T(st, "gbo", [128, 16]); gg = T(st, "gg", [128, 512]); bg = T(st, "bg", [128, 2048])
            pT = [PS(st, f"pT{i}", [128, 512]) for i in range(2)]
            pY = [PS(st, f"pY{i}", [128, 512]) for i in range(3)]
            pX = [PS(st, f"pX{i}", [128, 512]) for i in range(2)]
            s.dma(bg[:], bgate_d.partition_broadcast(128), writes=["bg"])
            s.dma(gn[:], dnn_d.partition_broadcast(128), writes=["gn"])
            s.dma(abc[:, 0, 0:4], dtf_d.partition_broadcast(128), writes=["abc"])
            s.dma(abc[:, 0, 4:8], dtb_d.partition_broadcast(128), writes=["abc"])
            s.dma(abc[:, 1, 0:4], alf_d.partition_broadcast(128), writes=["abc"])
            s.dma(abc[:, 1, 4:8], alb_d.partition_broadcast(128), writes=["abc"])
            s.op("act", lambda e: e.activation(out=abc[:, 1, :], in_=abc[:, 1, :], func=AF.Exp), reads=["abc"], writes=["abc"])
            s.op("dve", lambda e: e.tensor_scalar(out=abc[:, 1, :], in0=abc[:, 1, :], scalar1=-1.0, scalar2=None, op0=ALU.mult), reads=["abc"], writes=["abc"])
            s.op("pool", lambda e: e.memset(va[:], 1.0), writes=["va"])
            wcount = [0]

            def rope_ops(src, dst, H):
                sv = src.rearrange("p (h a b c) -> p h a b c", h=H, a=2, b=2)
                dv = dst.rearrange("p (h a b c) -> p h a b c", h=H, a=2, b=2)
                tv = tmp[:, 0:H * 64].rearrange("p (h a b c) -> p h a b c", h=H, a=2, b=2)
                return sv, dv, tv

            tiles = [("c", i) for i in range(CTX // 128)] + [("l", i) for i in range(NT)]
            if upto == 1 and "small" in dbg:
                tiles = tiles[:4]
            for ti, (kind, i) in enumerate(tiles):
                lat = kind == "l"
                src = x_d if lat else ctx_d
                tg = ti
                X = xt[ti % 2]; xid = ("xt", ti % 2)
                s.dma(X[:], src[i * 128:(i + 1) * 128, :], writes=[xid])
                if lat:
                    R = rp[ti % 2]; rid = ("rp", ti % 2)
                    s.dma(R[:], rope_d[i * 128:(i + 1) * 128, :], writes=[rid], queue="act")
                s.op("act", lambda e, X=X: e.activation(out=junk[:], in_=X[:], func=AF.Square, accum_out=ss[:]), reads=[xid], writes=["junk", "ss"])
                s.op("dve", lambda e: e.tensor_scalar(out=rstd[:], in0=ss[:], scalar1=1.0 / D, scalar2=1e-6, op0=ALU.mult, op1=ALU.add), reads=["ss"], writes=["rstd"])
                s.op("act", lambda e: e.sqrt(out=rstd[:], in_=rstd[:]), reads=["rstd"], writes=["rstd"])
                s.op("dve", lambda e: e.reciprocal(out=rstd[:], in_=rstd[:]), reads=["rstd"], writes=["rstd"])
                G = BV_G1 if lat else BV_CG1
                SH = BV_SH1 if lat else BV_CSH1
                s.op("dve", lambda e, X=X, G=G: e.scalar_tensor_tensor(out=h[:], in0=X[:], scalar=rstd[:, 0:1], in1=bvv(G)[:, :], op0=ALU.mult, op1=ALU.mult), reads=[xid, "rstd", "bv"], writes=["h"])
                s.op("pool", lambda e, SH=SH: e.tensor_tensor(out=h[:], in0=h[:], in1=bvv(SH)[:, :], op=ALU.add), reads=["h", "bv"], writes=["h"])
                for hb in range(2):
                    for k4 in range(4):
                        kc = hb * 4 + k4
                        s.op("pe", lambda e, kc=kc, hb=hb, k4=k4: e.transpose(out=pT[hb][:, k4 * 128:(k4 + 1) * 128], in_=h[:, kc * 128:(kc + 1) * 128], identity=ident), reads=["h", "cm"], writes=[("pT", hb)])
                    eng = "act" if hb == 0 else "dve"
                    if eng == "act":
                        s.op("act", lambda e, hb=hb: e.copy(out=hT[:, hb * 4:(hb + 1) * 4, :].rearrange("p a b -> p (a b)"), in_=pT[hb][:]), reads=[("pT", hb)], writes=[("hT", hb)])
                    else:
                        s.op("dve", lambda e, hb=hb: e.tensor_copy(out=hT[:, hb * 4:(hb + 1) * 4, :].rearrange("p a b -> p (a b)"), in_=pT[hb][:]), reads=[("pT", hb)], writes=[("hT", hb)])
                need = range(11) if lat else (1, 2, 3, 4, 6)
                for bi in need:
                    c0, cw = blocks[bi]
                    wi = wcount[0] % 3; wcount[0] += 1
                    W = wb[wi]; P = pY[wi]
                    s.dma(W[:, :, 0:cw], win_d[:, c0:c0 + cw].rearrange("(kc p) n -> p kc n", p=128), writes=[("wb", wi)], queue=("sp", "act", "pool")[wi], allow_slow_non_contiguous=(cw < 128))
                    for kc in range(8):
                        s.op("pe", lambda e, kc=kc, W=W, P=P, cw=cw: e.matmul(out=P[:, 0:cw], lhsT=hT[:, kc, :], rhs=W[:, kc, 0:cw], start=(kc == 0), stop=(kc == 7)),
                             reads=[("hT", 0), ("hT", 1), ("wb", wi)], writes=[("pY", wi)])
                    pid = ("pY", wi)
                    if bi == 0:
                        s.op("act", lambda e, P=P: e.activation(out=qs[:], in_=P[:], func=AF.Copy, scale=0.125), reads=[pid], writes=["qs"])
                        _rope(s, qs[:], qr[:], tmp, R, rid, 8, "qs", "qr")
                        for hh in range(8):
                            s.op("pe", lambda e, hh=hh: e.transpose(out=pX[hh // 4][0:64, (hh % 4) * 128:(hh % 4 + 1) * 128], in_=qr[:, hh * 64:(hh + 1) * 64], identity=ident), reads=["qr", "cm"], writes=[("pX", hh // 4)])
                        s.op("act", lambda e: e.copy(out=qT[:, 0:4, :].rearrange("p a b -> p (a b)"), in_=pX[0][0:64, :]), reads=[("pX", 0)], writes=["qT"])
                        s.op("dve", lambda e: e.tensor_copy(out=qT[:, 4:8, :].rearrange("p a b -> p (a b)"), in_=pX[1][0:64, :]), reads=[("pX", 1)], writes=["qT"])
                        s.dma(QT_s[:, :, i * 128:(i + 1) * 128], qT[:], reads=["qT"], writes=["QT_s"], queue="pool")
                    elif bi == 1:
                        s.op("act", lambda e, P=P: e.copy(out=kvs[:], in_=P[:, 0:256]), reads=[pid], writes=["kvs"])
                        if lat:
                            _rope(s, kvs[:, 0:128], kr[:], tmp, R, rid, 2, "kvs", "kr")
                            ksrc, kid = kr, "kr"
                        else:
                            ksrc, kid = kvs, "kvs"
                        for hh in range(2):
                            s.op("pe", lambda e, hh=hh, ksrc=ksrc: e.transpose(out=pX[0][0:64, hh * 128:(hh + 1) * 128], in_=ksrc[:, hh * 64:(hh + 1) * 64], identity=ident), reads=[kid, "cm"], writes=[("pX", 0)])
                        s.op("act", lambda e: e.copy(out=kT[:].rearrange("p a b -> p (a b)"), in_=pX[0][0:64, 0:256]), reads=[("pX", 0)], writes=["kT"])
                        s.dma(KT_s[:, :, tg * 128:(tg + 1) * 128], kT[:], reads=["kT"], writes=["KT_s"], queue="pool")
                        s.op("pool", lambda e: e.tensor_copy(out=va[:, :, 0:64], in_=kvs[:, 128:256].rearrange("p (g d) -> p g d", g=2)), reads=["kvs"], writes=["va"])
                        s.dma(V_s[tg * 128:(tg + 1) * 128, :, :], va[:], reads=["va"], writes=["V_s"], queue="pool")
                    elif bi in (2, 3, 4):
                        s.op("act", lambda e, P=P: e.copy(out=rw[:], in_=P[:]), reads=[pid], writes=["rw"])
                        for k4 in range(4):
                            s.op("pe", lambda e, k4=k4: e.transpose(out=pX[1][:, k4 * 128:(k4 + 1) * 128], in_=rw[:, k4 * 128:(k4 + 1) * 128], identity=ident), reads=["rw", "cm"], writes=[("pX", 1)])
                        s.op("dve", lambda e: e.tensor_copy(out=rT[:].rearrange("p a b -> p (a b)"), in_=pX[1][:]), reads=[("pX", 1)], writes=["rT"])
                        f0 = (bi - 2) * 512
                        s.dma(RT_s[f0:f0 + 512, tg * 128:(tg + 1) * 128].rearrange("(a p) t -> p a t", p=128), rT[:], reads=["rT"], writes=["RT_s"], queue="pool")
                    elif bi == 5:
                        s.op("act", lambda e, P=P: e.activation(out=zz[:], in_=P[:], func=AF.Silu), reads=[pid], writes=["zz"])
                        s.op("pool", lambda e: e.tensor_tensor(out=zz[:].rearrange("p (h d) -> p h d", h=4), in0=zz[:].rearrange("p (h d) -> p h d", h=4), in1=gn[:].unsqueeze(1).to_broadcast([128, 4, 128]), op=ALU.mult), reads=["zz", "gn"], writes=["zz"])
                        s.dma(Z_s[i * 128:(i + 1) * 128, :], zz[:], reads=["zz"], writes=["Z_s"], queue="pool")
                    elif bi == 6:
                        s.op("dve", lambda e, P=P: e.tensor_tensor(out=ab[:, 0:8], in0=P[:, 0:8], in1=abc[:, 0, :], op=ALU.add), reads=[pid, "abc"], writes=["ab"])
                        s.op("act", lambda e: e.activation(out=ab[:, 0:8], in_=ab[:, 0:8], func=AF.Exp), reads=["ab"], writes=["ab"])
                        s.op("dve", lambda e: e.tensor_scalar(out=ab[:, 0:8], in0=ab[:, 0:8], scalar1=1.0, scalar2=None, op0=ALU.add), reads=["ab"], writes=["ab"])
                        s.op("act", lambda e: e.activation(out=ab[:, 0:8], in_=ab[:, 0:8], func=AF.Ln), reads=["ab"], writes=["ab"])
                        s.op("dve", lambda e: e.tensor_tensor(out=gbo[:, 0:8], in0=ab[:, 0:8], in1=abc[:, 1, :], op=ALU.mult), reads=["ab", "abc"], writes=["gbo"])
                        s.op("act", lambda e, P=P: e.activation(out=gbo[:, 8:16], in_=P[:, 8:16], func=AF.Sigmoid), reads=[pid], writes=["gbo"])
                        s.dma(GB_s[tg * 128:(tg + 1) * 128, :], gbo[:], reads=["gbo"], writes=["GB_s"], queue="pool")
                    else:
                        gi = bi - 7
                        s.op("dve", lambda e, P=P, gi=gi: e.tensor_tensor(out=gg[:], in0=P[:], in1=bg[:, gi * 512:(gi + 1) * 512], op=ALU.add), reads=[pid, "bg"], writes=["gg"])
                        s.op("act", lambda e: e.activation(out=gg[:], in_=gg[:], func=AF.Sigmoid), reads=["gg"], writes=["gg"])
                        s.dma(GT_s[i * 128:(i + 1) * 128, gi * 512:(gi + 1) * 512], gg[:], reads=["gg"], writes=["GT_s"], queue="pool")
            s.flush()
        stA.close()
        if upto <= 1:
            return nc

        with contextlib.ExitStack() as st:
            cw = T(st, "cw", [128, 12, 5])
            Rt = [T(st, f"Rt{i}", [128, 516]) for i in range(3)]
            acc = [T(st, f"acc{i}", [128, 512]) for i in range(2)]
            y = [T(st, f"y{i}", [128, 512]) for i in range(2)]
            y2 = T(st, "y2", [128, 512]); rn = T(st, "rn", [128, 512]); yn = [T(st, f"yn{i}", [128, 512]) for i in range(2)]
            tok = [T(st, f"tok{i}", [128, 4, 128]) for i in range(2)]
            pN = [PS(st, f"pN{i}", [128, 512]) for i in range(2)]
            pK = [PS(st, f"pK{i}", [128, 512]) for i in range(2)]
            for j in range(5):
                s.dma(cw[:, :, j], conv_d[j, :].rearrange("(fc p) -> p fc", p=128), writes=["cw"], allow_slow_non_contiguous=True)
            it = 0
            segs = [(0, CTX), (CTX, TALL)]
            if "small" in dbg:
                segs = [(0, CTX), (CTX, CTX + 256)]
            for (g0, g1) in segs:
                for t0 in range(g0, g1, 512):
                    n = min(512, g1 - t0)
                    for fc in range(12):
                        R = Rt[it % 3]; rid = ("Rt", it % 3); A = acc[it % 2]; aid = ("acc", it % 2); Y = y[it % 2]; yid = ("y", it % 2)
                        lo = max(t0 - 2, g0); hi = min(t0 + n + 2, g1)
                        if lo > t0 - 2 or hi < t0 + n + 2:
                            s.op("pool", lambda e, R=R: e.memset(R[:], 0.0), writes=[rid])
                        s.dma(R[:, lo - (t0 - 2):hi - (t0 - 2)], RT_s[fc * 128:(fc + 1) * 128, lo:hi], reads=["RT_s"], writes=[rid], queue=("sp", "act")[it % 2])
                        s.op("dve", lambda e, R=R, A=A, fc=fc, n=n: e.tensor_scalar(out=A[:, 0:n], in0=R[:, 0:n], scalar1=cw[:, fc, 0:1], scalar2=None, op0=ALU.mult), reads=[rid, "cw"], writes=[aid])
                        for j in range(1, 5):
                            s.op("dve", lambda e, R=R, A=A, fc=fc, n=n, j=j: e.scalar_tensor_tensor(out=A[:, 0:n], in0=R[:, j:j + n], scalar=cw[:, fc, j:j + 1], in1=A[:, 0:n], op0=ALU.mult, op1=ALU.add), reads=[rid, "cw", aid], writes=[aid])
                        s.op("act", lambda e, A=A, Y=Y, n=n: e.activation(out=Y[:, 0:n], in_=A[:, 0:n], func=AF.Silu), reads=[aid], writes=[yid])
                        src, sid = Y, yid
                        if fc < 8:
                            YN = yn[it % 2]; nid = ("yn", it % 2); P = pN[it % 2]; pid = ("pN", it % 2)
                            s.op("act", lambda e, Y=Y, n=n: e.activation(out=y2[:, 0:n], in_=Y[:, 0:n], func=AF.Square), reads=[yid], writes=["y2"])
                            s.op("pe", lambda e, P=P, n=n: e.matmul(out=P[:, 0:n], lhsT=cm[:, C_ONES, :], rhs=y2[:, 0:n], start=True, stop=True), reads=["cm", "y2"], writes=[pid])
                            s.op("dve", lambda e, P=P, n=n: e.tensor_scalar(out=rn[:, 0:n], in0=P[:, 0:n], scalar1=1e-6, scalar2=None, op0=ALU.add), reads=[pid], writes=["rn"])
                            s.op("act", lambda e, n=n: e.sqrt(out=rn[:, 0:n], in_=rn[:, 0:n]), reads=["rn"], writes=["rn"])
                            s.op("dve", lambda e, n=n: e.reciprocal(out=rn[:, 0:n], in_=rn[:, 0:n]), reads=["rn"], writes=["rn"])
                            sc = float(128 ** -0.5) if fc < 4 else 1.0
                            s.op("dve", lambda e, Y=Y, YN=YN, n=n, sc=sc: e.scalar_tensor_tensor(out=YN[:, 0:n], in0=Y[:, 0:n], scalar=sc, in1=rn[:, 0:n], op0=ALU.mult, op1=ALU.mult), reads=[yid, "rn"], writes=[nid])
                            s.dma(QK_s[fc * 128:(fc + 1) * 128, t0:t0 + n], YN[:, 0:n], reads=[nid], writes=["QK_s"], queue="pool")
                            src, sid = YN, nid
                        if fc >= 4:
                            PK = pK[it % 2]; kid = ("pK", it % 2); TK = tok[it % 2]; tid = ("tok", it % 2)
                            nsb = n // 128
                            for sb in range(nsb):
                                s.op("pe", lambda e, PK=PK, src=src, sb=sb: e.transpose(out=PK[:, sb * 128:(sb + 1) * 128], in_=src[:, sb * 128:(sb + 1) * 128], identity=ident), reads=[sid, "cm"], writes=[kid])
                            s.op("act", lambda e, PK=PK, TK=TK, n=n: e.copy(out=TK[:].rearrange("p a b -> p (a b)")[:, 0:n], in_=PK[:, 0:n]), reads=[kid], writes=[tid])
                            s.dma(KV_s[t0:t0 + n, (fc - 4) * 128:(fc - 3) * 128].rearrange("(sb p) f -> p sb f", p=128), TK[:, 0:nsb, :], reads=[tid], writes=["KV_s"], queue="pool")
                        it += 1
            s.flush()
        if upto <= 2:
            return nc

        with contextlib.ExitStack() as st:
            Sst = [T(st, f"Sst{i}", [128, 4, 128]) for i in range(2)]
            qT4 = T(st, "qT4", [128, 4, 128]); kT4 = T(st, "kT4", [128, 4, 128]); ktok = T(st, "ktok", [128, 4, 128]); vtok = T(st, "vtok", [128, 4, 128])
            gb = T(st, "gb", [128, 16]); sm = T(st, "sm", [128, 16]); ex = T(st, "ex", [128, 16]); beg = T(st, "beg", [128, 4])
            G1 = T(st, "G1", [128, 4, 128]); dl = T(st, "dl", [128, 4, 128]); du = T(st, "du", [128, 4, 128])
            Bm = [T(st, f"Bm{i}", [128, 4, 128]) for i in range(2)]; Cm = [T(st, f"Cm{i}", [128, 4, 128]) for i in range(2)]; Pm = [T(st, f"Pm{i}", [128, 4, 128]) for i in range(2)]
            aT = T(st, "aT", [128, 4, 128]); kbg = T(st, "kbg", [128, 4, 128]); vb = T(st, "vb", [128, 4, 128]); ktl = T(st, "ktl", [128, 4, 128])
            WT = T(st, "WT", [128, 4, 128]); U = T(st, "U", [128, 4, 128]); vn = T(st, "vn", [128, 4, 128]); o1 = T(st, "o1", [128, 4, 128]); ot = T(st, "ot", [128, 4, 128])
            pb = [PS(st, f"pb{i}", [128, 4, 128]) for i in range(8)]
            pA, pB_, pC, pD, pE, pF, pG, pH = pb
            pid = lambda k: ("pb", k)
            H4 = [128, 4, 128]
            bc_h = lambda ap2: ap2.unsqueeze(1).to_broadcast(H4)
            bc_l = lambda ap2: ap2.unsqueeze(2).to_broadcast(H4)
            ntl = (2 if "small" in dbg else NT)
            for dr in range(2):
                M1 = cm[:, C_M1F if dr == 0 else C_M1B, :]; NM1 = cm[:, C_NM1F if dr == 0 else C_NM1B, :]
                NB = cm[:, C_NLOW if dr == 0 else C_NUP, :]; NTm = cm[:, C_NUP if dr == 0 else C_NLOW, :]
                STR = cm[:, C_SLOW if dr == 0 else C_SUP, :]
                SS = Sst[dr]; ssid = ("Sst", dr)
                s.op("pool", lambda e, SS=SS: e.memset(SS[:], 0.0), writes=[ssid])
                order = [("c", i) for i in range(CTX // 128)] + [("l", i) for i in range(ntl)]
                if dr == 1:
                    order = [("c", i) for i in reversed(range(CTX // 128))] + [("l", i) for i in reversed(range(ntl))]
                for (kind, i) in order:
                    lat = kind == "l"
                    tg = i if not lat else CTX // 128 + i
                    tsl = slice(tg * 128, (tg + 1) * 128)
                    s.dma(qT4[:], QK_s[0:512, tsl].rearrange("(h p) t -> p h t", p=128), reads=["QK_s"], writes=["qT4"])
                    s.dma(kT4[:], QK_s[512:1024, tsl].rearrange("(h p) t -> p h t", p=128), reads=["QK_s"], writes=["kT4"], queue="act")
                    s.dma(ktok[:].rearrange("p h d -> p (h d)"), KV_s[tsl, 0:512], reads=["KV_s"], writes=["ktok"])
                    s.dma(vtok[:].rearrange("p h d -> p (h d)"), KV_s[tsl, 512:1024], reads=["KV_s"], writes=["vtok"], queue="act")
                    s.dma(gb[:], GB_s[tsl, :], reads=["GB_s"], writes=["gb"])
                    g = gb[:, dr * 4:dr * 4 + 4]; beta = gb[:, 8 + dr * 4:12 + dr * 4]
                    pAf = pA[:].rearrange("p a b -> p (a b)")
                    for k, L in enumerate((M1, cm[:, C_SAME, :], cm[:, C_SEL0, :], cm[:, C_SEL1, :])):
                        s.op("pe", lambda e, k=k, L=L, g=g: e.matmul(out=pAf[:, 4 * k:4 * k + 4], lhsT=L, rhs=g, start=True, stop=True), reads=["cm", "gb"], writes=[pid(0)])
                    s.op("dve", lambda e: e.tensor_copy(out=sm[:], in_=pAf[:, 0:16]), reads=[pid(0)], writes=["sm"])
                    s.op("dve", lambda e: e.tensor_tensor(out=sm[:, 4:8], in0=sm[:, 4:8], in1=sm[:, 0:4], op=ALU.subtract), reads=["sm"], writes=["sm"])
                    s.op("act", lambda e: e.activation(out=ex[:], in_=sm[:], func=AF.Exp), reads=["sm"], writes=["ex"])
                    s.op("dve", lambda e, beta=beta: e.tensor_tensor(out=beg[:], in0=ex[:, 0:4], in1=beta, op=ALU.mult), reads=["ex", "gb"], writes=["beg"])
                    s.op("dve", lambda e, g=g: e.tensor_tensor(out=G1[:], in0=bc_h(cm[:, C_SAME, :]), in1=bc_l(g), op=ALU.mult), reads=["cm", "gb"], writes=["G1"])
                    for hh in range(4):
                        s.op("pe", lambda e, hh=hh, M1=M1: e.matmul(out=pB_[:, hh, :], lhsT=M1, rhs=G1[:, hh, :], start=True, stop=False), reads=["cm", "G1"], writes=[pid(1)])
                        s.op("pe", lambda e, hh=hh, NM1=NM1: e.matmul(out=pB_[:, hh, :], lhsT=G1[:, hh, :], rhs=NM1, start=False, stop=True), reads=["cm", "G1"], writes=[pid(1)])
                    s.op("dve", lambda e, NB=NB: e.tensor_tensor(out=dl[:], in0=pB_[:], in1=bc_h(NB), op=ALU.add), reads=[pid(1), "cm"], writes=["dl"])
                    s.op("dve", lambda e, NTm=NTm: e.scalar_tensor_tensor(out=du[:], in0=pB_[:], scalar=-1.0, in1=bc_h(NTm), op0=ALU.mult, op1=ALU.add), reads=[pid(1), "cm"], writes=["du"])
                    s.op("act", lambda e: e.activation(out=dl[:], in_=dl[:], func=AF.Exp), reads=["dl"], writes=["dl"])
                    s.op("act", lambda e: e.activation(out=du[:], in_=du[:], func=AF.Exp), reads=["du"], writes=["du"])
                    for hh in range(4):
                        s.op("pe", lambda e, hh=hh: e.matmul(out=pC[:, hh, :], lhsT=kT4[:, hh, :], rhs=kT4[:, hh, :], start=True, stop=True), reads=["kT4"], writes=[pid(2)])
                    for hh in range(4):
                        s.op("pe", lambda e, hh=hh: e.matmul(out=pD[:, hh, :], lhsT=kT4[:, hh, :], rhs=qT4[:, hh, :], start=True, stop=True), reads=["kT4", "qT4"], writes=[pid(3)])
                    B0 = Bm[0]; C0 = Cm[0]; P0 = Pm[0]
                    s.op("dve", lambda e: e.tensor_tensor(out=B0[:], in0=pC[:], in1=dl[:], op=ALU.mult), reads=[pid(2), "dl"], writes=[("Bm", 0)])
                    s.op("pool", lambda e, STR=STR: e.tensor_tensor(out=B0[:], in0=B0[:], in1=bc_h(STR), op=ALU.mult), reads=[("Bm", 0), "cm"], writes=[("Bm", 0)])
                    s.op("pool", lambda e, beta=beta: e.tensor_tensor(out=B0[:], in0=B0[:], in1=bc_l(beta), op=ALU.mult), reads=[("Bm", 0), "gb"], writes=[("Bm", 0)])
                    s.op("dve", lambda e: e.tensor_tensor(out=aT[:], in0=pD[:], in1=du[:], op=ALU.mult), reads=[pid(3), "du"], writes=["aT"])
                    for hh in range(4):
                        s.op("pe", lambda e, hh=hh: e.transpose(out=pE[:, hh, :], in_=B0[:, hh, :], identity=ident), reads=[("Bm", 0), "cm"], writes=[pid(4)])
                    s.op("act", lambda e: e.copy(out=C0[:], in_=pE[:]), reads=[pid(4)], writes=[("Cm", 0)])
                    s.op("dve", lambda e: e.tensor_tensor(out=P0[:], in0=C0[:], in1=bc_h(ident), op=ALU.add), reads=[("Cm", 0), "cm"], writes=[("Pm", 0)])
                    cur = 0
                    for lv in range(1, 6):
                        nx = 1 - cur
                        Bc, Cc, Pc = Bm[cur], Cm[cur], Pm[cur]; Bn, Cn, Pn = Bm[nx], Cm[nx], Pm[nx]
                        for hh in range(4):
                            s.op("pe", lambda e, hh=hh, Bc=Bc, Cc=Cc: e.matmul(out=pF[:, hh, :], lhsT=Cc[:, hh, :], rhs=Bc[:, hh, :], start=True, stop=True), reads=[("Bm", cur), ("Cm", cur)], writes=[pid(5)])
                        s.op("act", lambda e, Bn=Bn: e.copy(out=Bn[:], in_=pF[:]), reads=[pid(5)], writes=[("Bm", nx)])
                        if lv < 5:
                            for hh in range(4):
                                s.op("pe", lambda e, hh=hh, Bc=Bc, Cc=Cc: e.matmul(out=pG[:, hh, :], lhsT=Bc[:, hh, :], rhs=Cc[:, hh, :], start=True, stop=True), reads=[("Bm", cur), ("Cm", cur)], writes=[pid(6)])
                            s.op("dve", lambda e, Cn=Cn: e.tensor_copy(out=Cn[:], in_=pG[:]), reads=[pid(6)], writes=[("Cm", nx)])
                        for hh in range(4):
                            s.op("pe", lambda e, hh=hh, Pc=Pc: e.matmul(out=pH[:, hh, :], lhsT=ident, rhs=Pc[:, hh, :], start=True, stop=False), reads=[("Pm", cur), "cm"], writes=[pid(7)])
                            s.op("pe", lambda e, hh=hh, Pc=Pc, Bn=Bn: e.matmul(out=pH[:, hh, :], lhsT=Bn[:, hh, :], rhs=Pc[:, hh, :], start=False, stop=True), reads=[("Pm", cur), ("Bm", nx)], writes=[pid(7)])
                        s.op("dve", lambda e, Pn=Pn: e.tensor_copy(out=Pn[:], in_=pH[:]), reads=[pid(7)], writes=[("Pm", nx)])
                        cur = nx
                    TT = Pm[cur]; ttid = ("Pm", cur)
                    s.op("pool", lambda e: e.tensor_tensor(out=kbg[:], in0=ktok[:], in1=bc_l(beg[:]), op=ALU.mult), reads=["ktok", "beg"], writes=["kbg"])
                    s.op("pool", lambda e, beta=beta: e.tensor_tensor(out=vb[:], in0=vtok[:], in1=bc_l(beta), op=ALU.mult), reads=["vtok", "gb"], writes=["vb"])
                    s.op("pool", lambda e: e.tensor_tensor(out=ktl[:], in0=ktok[:], in1=bc_l(ex[:, 4:8]), op=ALU.mult), reads=["ktok", "ex"], writes=["ktl"])
                    for hh in range(4):
                        s.op("pe", lambda e, hh=hh, TT=TT: e.matmul(out=pE[:, hh, :], lhsT=kbg[:, hh, :], rhs=TT[:, hh, :], start=True, stop=True), reads=["kbg", ttid], writes=[pid(4)])
                    s.op("act", lambda e: e.copy(out=WT[:], in_=pE[:]), reads=[pid(4)], writes=["WT"])
                    for hh in range(4):
                        s.op("pe", lambda e, hh=hh, TT=TT: e.matmul(out=pF[:, hh, :], lhsT=TT[:, hh, :], rhs=vb[:, hh, :], start=True, stop=True), reads=["vb", ttid], writes=[pid(5)])
                    s.op("dve", lambda e: e.tensor_copy(out=U[:], in_=pF[:]), reads=[pid(5)], writes=["U"])
                    for c in ((0, 1) if dr == 0 else (1, 0)):
                        pr = slice(64 * c, 64 * c + 64)
                        for hh in range(4):
                            s.op("pe", lambda e, hh=hh, SS=SS: e.matmul(out=pG[:, hh, :], lhsT=WT[:, hh, :], rhs=SS[:, hh, :], start=True, stop=True), reads=["WT", ssid], writes=[pid(6)])
                        s.op("dve", lambda e, pr=pr: e.tensor_tensor(out=vn[pr], in0=U[pr], in1=pG[pr], op=ALU.subtract), reads=["U", pid(6)], writes=["vn"])
                        for hh in range(4):
                            s.op("pe", lambda e, hh=hh, SS=SS: e.matmul(out=pH[:, hh, :], lhsT=qT4[:, hh, :], rhs=SS[:, hh, :], start=True, stop=True), reads=["qT4", ssid], writes=[pid(7)])
                        for hh in range(4):
                            s.op("pe", lambda e, hh=hh, pr=pr: e.matmul(out=pC[:, hh, :], lhsT=aT[pr, hh, :], rhs=vn[pr, hh, :], start=True, stop=True), reads=["aT", "vn"], writes=[pid(2)])
                        for hh in range(4):
                            s.op("pe", lambda e, hh=hh, pr=pr: e.matmul(out=pD[:, hh, :], lhsT=ktl[pr, hh, :], rhs=vn[pr, hh, :], start=True, stop=True), reads=["ktl", "vn"], writes=[pid(3)])
                        if lat:
                            s.op("dve", lambda e, pr=pr: e.tensor_tensor(out=o1[pr], in0=pH[pr], in1=bc_l(ex[:, 0:4])[pr], op=ALU.mult), reads=[pid(7), "ex"], writes=["o1"])
                            s.op("dve", lambda e, pr=pr: e.tensor_tensor(out=ot[pr], in0=o1[pr], in1=pC[pr], op=ALU.add), reads=["o1", pid(2)], writes=["ot"])
                        s.op("pool", lambda e, c=c, SS=SS: e.tensor_tensor(out=SS[:], in0=SS[:], in1=bc_l(ex[:, 8 + 4 * c:12 + 4 * c]), op=ALU.mult), reads=[ssid, "ex", pid(6), pid(7)], writes=[ssid])
                        s.op("dve", lambda e, SS=SS: e.tensor_tensor(out=SS[:], in0=SS[:], in1=pD[:], op=ALU.add), reads=[ssid, pid(3)], writes=[ssid])
                    if lat:
                        s.dma(OD_s[dr, i * 128:(i + 1) * 128, :], ot[:].rearrange("p h d -> p (h d)"), reads=["ot"], writes=["OD_s"], queue="pool")
            s.flush()
        if upto <= 3:
            return nc

        with contextlib.ExitStack() as st:
            kt = [T(st, f"kt{i}", [64, 2, 384]) for i in range(2)]
            vt = [T(st, f"vt{i}", [128, 3, 130]) for i in range(2)]
            ktc = T(st, "ktc", [64, 2, 256]); vtc = T(st, "vtc", [128, 2, 130])
            qt = [T(st, f"qt{i}", [64, 8, 128]) for i in range(2)]
            E = [T(st, f"E{i}", [128, 5, 512]) for i in range(2)]
            esink = T(st, "esink", [128, 8]); den = T(st, "den", [128, 8]); oa = [T(st, f"oa{i}", [128, 512]) for i in range(2)]
            pS = [PS(st, f"pS{i}", [128, 512]) for i in range(3)]
            pO = [PS(st, f"pO{i}", [128, 4, 65]) for i in range(2)]
            s.dma(ktc[:], KT_s[:, :, 0:CTX], reads=["KT_s"], writes=["ktc"])
            s.dma(vtc[:], V_s[0:CTX].rearrange("(b p) g d -> p b (g d)", p=128), reads=["V_s"], writes=["vtc"])
            s.dma(esink[:], sink_d.partition_broadcast(128), writes=["esink"])
            s.op("act", lambda e: e.activation(out=esink[:], in_=esink[:], func=AF.Exp), reads=["esink"], writes=["esink"])
            ntl = (2 if "small" in dbg else NT)
            nS = 0
            for i in range(ntl):
                lo = max(i - 1, 0); hi = min(i + 1, ntl - 1); nb = hi - lo + 1
                KT_ = kt[i % 2]; VT_ = vt[i % 2]; QT_ = qt[i % 2]; OA = oa[i % 2]
                s.dma(KT_[:, :, 0:nb * 128], KT_s[:, :, CTX + lo * 128:CTX + (hi + 1) * 128], reads=["KT_s"], writes=[("kt", i % 2)])
                s.dma(VT_[:, 0:nb, :], V_s[CTX + lo * 128:CTX + (hi + 1) * 128].rearrange("(b p) g d -> p b (g d)", p=128), reads=["V_s"], writes=[("vt", i % 2)], queue="act")
                s.dma(QT_[:], QT_s[:, :, i * 128:(i + 1) * 128], reads=["QT_s"], writes=[("qt", i % 2)])
                for g in range(2):
                    Eg = E[g]; eid = ("E", g)
                    kb = [("l", j - lo, (C_WPREV if j < i else (C_WNEXT if j > i else None))) for j in range(lo, hi + 1)] + [("c", 0, None), ("c", 1, None)]
                    for bi, (kk, bl, msk) in enumerate(kb):
                        P = pS[nS % 3]; psid = ("pS", nS % 3); nS += 1
                        lhs = KT_[:, g, bl * 128:(bl + 1) * 128] if kk == "l" else ktc[:, g, bl * 128:(bl + 1) * 128]
                        s.op("pe", lambda e, P=P, lhs=lhs, QT_=QT_, g=g: e.matmul(out=P[:].rearrange("p (h q) -> p h q", h=4), lhsT=lhs, rhs=QT_[:, 4 * g:4 * g + 4, :], start=True, stop=True),
                             reads=[("kt", i % 2), "ktc", ("qt", i % 2)], writes=[psid])
                        s.op("act", lambda e, P=P, Eg=Eg, bi=bi: e.activation(out=Eg[:, bi, :], in_=P[:], func=AF.Exp), reads=[psid], writes=[eid])
                        if msk is not None:
                            s.op("dve", lambda e, Eg=Eg, bi=bi, msk=msk: e.tensor_tensor(out=Eg[:, bi, :].rearrange("p (h q) -> p h q", h=4), in0=Eg[:, bi, :].rearrange("p (h q) -> p h q", h=4),
                                                                              in1=cm[:, msk, :].unsqueeze(1).to_broadcast([128, 4, 128]), op=ALU.mult), reads=[eid, "cm"], writes=[eid])
                    for hh in range(4):
                        for bi, (kk, bl, msk) in enumerate(kb):
                            rhs = VT_[:, bl, g * 65:(g + 1) * 65] if kk == "l" else vtc[:, bl, g * 65:(g + 1) * 65]
                            s.op("pe", lambda e, Eg=Eg, bi=bi, hh=hh, rhs=rhs, g=g, last=(bi == len(kb) - 1): e.matmul(out=pO[g][:, hh, :], lhsT=Eg[:, bi, hh * 128:(hh + 1) * 128], rhs=rhs, start=(bi == 0), stop=last),
                                 reads=[eid, ("vt", i % 2), "vtc"], writes=[("pO", g)])
                    s.op("dve", lambda e, g=g: e.tensor_tensor(out=den[:, 4 * g:4 * g + 4], in0=pO[g][:, :, 64], in1=esink[:, 4 * g:4 * g + 4], op=ALU.add), reads=[("pO", g), "esink"], writes=["den"])
                    s.op("dve", lambda e, g=g: e.reciprocal(out=den[:, 4 * g:4 * g + 4], in_=den[:, 4 * g:4 * g + 4]), reads=["den"], writes=["den"])
                    s.op("dve", lambda e, g=g, OA=OA: e.tensor_tensor(out=OA[:, g * 256:(g + 1) * 256].rearrange("p (h d) -> p h d", h=4), in0=pO[g][:, :, 0:64],
                                                              in1=den[:, 4 * g:4 * g + 4].unsqueeze(2).to_broadcast([128, 4, 64]), op=ALU.mult), reads=[("pO", g), "den"], writes=[("oa", i % 2)])
                s.dma(OA_s[i * 128:(i + 1) * 128, :], OA[:], reads=[("oa", i % 2)], writes=["OA_s"], queue="pool")
            s.flush()
        if upto <= 5:
            return nc

        UTr_s = nc.dram_tensor("UTr_s", [D, 16384], F32R, kind="Internal").ap()
        Vr_s = nc.dram_tensor("Vr_s", [16384, D], F32R, kind="Internal").ap()
        with contextlib.ExitStack() as st:
            cvb = [st.enter_context(nc.sbuf_tensor(_nm("cvb"), [128, 4096], F32R)) for _ in range(3)]
            ci = 0
            for r0 in range(0, D, 128):
                for c0 in range(0, 16384, 4096):
                    k = ci % 3; ci += 1
                    s.dma(cvb[k][:], pu_d[r0:r0 + 128, c0:c0 + 4096], writes=[("cvb", k)], queue="pool")
                    s.dma(UTr_s[r0:r0 + 128, c0:c0 + 4096], cvb[k][:], reads=[("cvb", k)], writes=["UTr_s"], queue=("sp", "act")[ci % 2])
            for r0 in range(0, 16384, 512):
                k = ci % 3; ci += 1
                s.dma(cvb[k][:], pv_d[r0:r0 + 512, :].rearrange("(p a) n -> p (a n)", p=128), writes=[("cvb", k)], queue="pool")
                s.dma(Vr_s[r0:r0 + 512, :].rearrange("(p a) n -> p (a n)", p=128), cvb[k][:], reads=[("cvb", k)], writes=["Vr_s"], queue=("sp", "act")[ci % 2])
            s.flush()
        if "stopconv" in dbg:
            return nc
        GI = 2
        NG = 128 // GI
        NB = 2
        with contextlib.ExitStack() as st:
            TR = lambda name, shape: st.enter_context(nc.sbuf_tensor(_nm(name), list(shape), F32R))
            xa = [T(st, f"xa{t}", [128, D]) for t in range(NB)]
            yb = T(st, "yb", [128, D]); tc_ = T(st, "tc_", [128, 8, 128]); gt = T(st, "gt", [128, 2048])
            h2r = [TR(f"h2r{t}", [128, 8, 128]) for t in range(NB)]
            od = T(st, "od", [128, 2, 512]); zt = T(st, "zt", [128, 512]); oat = T(st, "oat", [128, 512]); o2 = T(st, "o2", [128, 512])
            qsb = T(st, "qsb", [128, D]); qTs = T(st, "qTs", [64, 16, 128])
            sc = [T(st, f"sc{t}", [128, 16, 128]) for t in range(NB)]
            ssq = T(st, "ssq", [128, 4]); ss = T(st, "ss6", [128, 1]); rstd = T(st, "rstd6", [128, 1])
            t16 = T(st, "t16", [128, 2, 16]); c16 = T(st, "c16", [128, 16]); cand = T(st, "cand", [128, 16, 16]); wk = T(st, "wk", [128, 256])
            thr = T(st, "thr", [128, 8]); negm = T(st, "negm", [128, 8]); Zs = T(st, "Zs", [128, 8]); kap = T(st, "kap", [128, 8]); e16 = T(st, "e16", [128, 16])
            m1 = T(st, "m1", [128, 8]); th2 = T(st, "th2", [128, 8])
            dg = [TR(f"dg{t}", [128, 8, 128]) for t in range(NB)]
            keysT = T(st, "keysT", [64, 16, 128])
            big = T(st, "big", [128, 8 * D])
            wS = big[:].rearrange("p (k n) -> p k n", k=8)
            UT = [TR(f"UT{i}", [128, 8, GI * 128]) for i in range(2)]
            VG = [TR(f"VG{i}", [128, GI, D]) for i in range(2)]
            pe_ = [big[:, i * 4096:(i + 1) * 4096].rearrange("p (t h k) -> p t h k", t=NB, h=8) for i in range(2)]
            _Mr = TR("Mr", [128, NB, 8, GI * 128])
            Mr = [_Mr, _Mr]
            W5 = NB * GI * 128
            assert W5 == 512
            g1 = [gt[:, 0:512], gt[:, 512:1024]]; Pm_ = [gt[:, 1024:1536], gt[:, 1536:2048]]
            PT = [TR(f"PT{i}", [128, NB * GI, 128]) for i in range(2)]
            bk = [PS(st, f"bk{i}", [128, 512]) for i in range(8)]
            B = lambda k: ("bk", k)
            pT = [bk[0], bk[1]]; pY = [bk[2], bk[3]]
            s.dma(keysT[:], pkeys_d.rearrange("h p d k -> d (h p) k"), writes=["keysT"])

            def transpose8(src, sid, nkc, dst_off=0, extra=None):
                for kc in range(nkc):
                    b = (dst_off + kc) // 4
                    s.op("pe", lambda e, kc=kc, b=b: e.transpose(out=pT[b][:, ((dst_off + kc) % 4) * 128:((dst_off + kc) % 4 + 1) * 128], in_=src[:, kc * 128:(kc + 1) * 128], identity=ident), reads=[sid, "cm"], writes=[B(b)])
                for b in sorted(set((dst_off + kc) // 4 for kc in range(nkc))):
                    if b == 0:
                        s.op("act", lambda e, b=b: e.copy(out=tc_[:, b * 4:(b + 1) * 4, :].rearrange("p a b -> p (a b)"), in_=pT[b][:]), reads=[B(b)], writes=[("tc", b)])
                    else:
                        s.op("dve", lambda e, b=b: e.tensor_copy(out=tc_[:, b * 4:(b + 1) * 4, :].rearrange("p a b -> p (a b)"), in_=pT[b][:]), reads=[B(b)], writes=[("tc", b)])
                    if extra is not None:
                        dst, did = extra
                        if b == 0:
                            s.op("dve", lambda e, b=b, dst=dst: e.tensor_copy(out=dst[:, b * 4:(b + 1) * 4, :].rearrange("p a b -> p (a b)"), in_=pT[b][:]), reads=[B(b)], writes=[did])
                        else:
                            s.op("act", lambda e, b=b, dst=dst: e.copy(out=dst[:, b * 4:(b + 1) * 4, :].rearrange("p a b -> p (a b)"), in_=pT[b][:]), reads=[B(b)], writes=[did])

            def rms(src, sid):
                s.op("act", lambda e: e.activation(out=qsb[:], in_=src[:], func=AF.Square, accum_out=ss[:]), reads=[sid], writes=["qsb", "ss6"])
                s.op("dve", lambda e: e.tensor_scalar(out=rstd[:], in0=ss[:], scalar1=1.0 / D, scalar2=1e-6, op0=ALU.mult, op1=ALU.add), reads=["ss6"], writes=["rstd6"])
                s.op("act", lambda e: e.sqrt(out=rstd[:], in_=rstd[:]), reads=["rstd6"], writes=["rstd6"])
                s.op("dve", lambda e: e.reciprocal(out=rstd[:], in_=rstd[:]), reads=["rstd6"], writes=["rstd6"])

            nblk = (1 if "small" in dbg else NT // NB)
            _c = [int(x[3:]) for x in dbg if x.startswith("cut")]
            cut = _c[0] if _c else 99
            ngr = (2 if "small2" in dbg else NG)
            gcount = 0
            for blk in range(nblk):
              for tau in range(NB):
                i = blk * NB + tau
                XA = xa[tau]; xid = ("xa", tau); SC = sc[tau]; scid = ("sc", tau)
                tsl = slice(i * 128, (i + 1) * 128)
                s.dma(XA[:], x_d[tsl, :], writes=[xid])
                s.dma(od[:, 0, :], OD_s[0, tsl, :], reads=["OD_s"], writes=["od"], queue="act")
                s.dma(od[:, 1, :], OD_s[1, tsl, :], reads=["OD_s"], writes=["od"], queue="act")
                s.dma(zt[:], Z_s[tsl, :], reads=["Z_s"], writes=["zt"])
                s.dma(oat[:], OA_s[tsl, :], reads=["OA_s"], writes=["oat"], queue="act")
                s.dma(gt[:], GT_s[tsl, :], reads=["GT_s"], writes=[("gtq", 0), ("gtq", 1), ("gtq", 2), ("gtq", 3)])
                s.dma(wS[:, 0:4, :], wba_d.rearrange("(kc p) n -> p kc n", p=128), writes=[("pe", 0, 0), ("pe", 0, 1), ("pe", 1, 0), ("pe", 1, 1)])
                s.dma(wS[:, 4:8, :], wbd_d.rearrange("(kc p) n -> p kc n", p=128), writes=[("pe", 0, 0), ("pe", 0, 1), ("pe", 1, 0), ("pe", 1, 1)], queue="act")
                s.op("dve", lambda e: e.tensor_tensor(out=od[:, 0, :], in0=od[:, 0, :], in1=od[:, 1, :], op=ALU.add), reads=["od"], writes=["od"])
                s.op("dve", lambda e: e.tensor_tensor(out=o2[:], in0=od[:, 0, :], in1=od[:, 0, :], op=ALU.mult), reads=["od"], writes=["o2"])
                s.op("dve", lambda e: e.tensor_reduce(out=ssq[:], in_=o2[:].rearrange("p (h d) -> p h d", h=4), axis=AX.X, op=ALU.add), reads=["o2"], writes=["ssq"])
                s.op("dve", lambda e: e.tensor_scalar(out=ssq[:], in0=ssq[:], scalar1=1.0 / 128, scalar2=1e-6, op0=ALU.mult, op1=ALU.add), reads=["ssq"], writes=["ssq"])
                s.op("act", lambda e: e.sqrt(out=ssq[:], in_=ssq[:]), reads=["ssq"], writes=["ssq"])
                s.op("dve", lambda e: e.reciprocal(out=ssq[:], in_=ssq[:]), reads=["ssq"], writes=["ssq"])
                s.op("dve", lambda e: e.tensor_tensor(out=o2[:].rearrange("p (h d) -> p h d", h=4), in0=od[:, 0, :].rearrange("p (h d) -> p h d", h=4), in1=ssq[:].unsqueeze(2).to_broadcast([128, 4, 128]), op=ALU.mult), reads=["od", "ssq"], writes=["o2"])
                s.op("dve", lambda e: e.tensor_tensor(out=o2[:], in0=o2[:], in1=zt[:], op=ALU.mult), reads=["o2", "zt"], writes=["o2"])
                transpose8(oat, "oat", 4, 0)
                transpose8(o2, "o2", 4, 4)
                for half in range(2):
                    for kc in range(4):
                        s.op("pe", lambda e, half=half, kc=kc: e.matmul(out=pY[half][:], lhsT=tc_[:, kc, :], rhs=wS[:, kc, half * 512:(half + 1) * 512], start=(kc == 0), stop=(kc == 3)), reads=[("tc", 0), ("pe", 0, 0), ("pe", 0, 1), ("pe", 1, 0), ("pe", 1, 1)], writes=[B(2 + half)])
                    s.op("dve", lambda e, half=half: e.tensor_tensor(out=yb[:, half * 512:(half + 1) * 512], in0=pY[half][:], in1=gt[:, half * 512:(half + 1) * 512], op=ALU.mult), reads=[B(2 + half), ("gtq", half)], writes=["yb"])
                for half in range(2):
                    for kc in range(4):
                        s.op("pe", lambda e, half=half, kc=kc: e.matmul(out=pY[half][:], lhsT=tc_[:, 4 + kc, :], rhs=wS[:, 4 + kc, half * 512:(half + 1) * 512], start=(kc == 0), stop=(kc == 3)), reads=[("tc", 1), ("pe", 0, 0), ("pe", 0, 1), ("pe", 1, 0), ("pe", 1, 1)], writes=[B(2 + half)])
                    s.op("dve", lambda e, half=half: e.tensor_tensor(out=qsb[:, half * 512:(half + 1) * 512], in0=pY[half][:], in1=gt[:, 1024 + half * 512:1024 + (half + 1) * 512], op=ALU.mult), reads=[B(2 + half), ("gtq", 2 + half)], writes=["qsb"])
                s.op("pool", lambda e: e.tensor_tensor(out=yb[:], in0=yb[:], in1=qsb[:], op=ALU.add), reads=["yb", "qsb"], writes=["yb"])
                s.dma(wS[:], wout_d.rearrange("(kc p) n -> p kc n", p=128), writes=[("pe", 0, 0), ("pe", 0, 1), ("pe", 1, 0), ("pe", 1, 1)])
                transpose8(yb, "yb", 8, 0)
                for half in range(2):
                    for kc in range(8):
                        s.op("pe", lambda e, half=half, kc=kc: e.matmul(out=pY[half][:], lhsT=tc_[:, kc, :], rhs=wS[:, kc, half * 512:(half + 1) * 512], start=(kc == 0), stop=(kc == 7)), reads=[("tc", 0), ("tc", 1), ("pe", 0, 0), ("pe", 0, 1), ("pe", 1, 0), ("pe", 1, 1)], writes=[B(2 + half)])
                    s.op("dve", lambda e, half=half: e.tensor_tensor(out=yb[:, half * 512:(half + 1) * 512], in0=pY[half][:], in1=bvv(BV_GT1)[:, half * 512:(half + 1) * 512], op=ALU.mult), reads=[B(2 + half), "bv"], writes=["yb"])
                s.op("pool", lambda e, XA=XA: e.tensor_tensor(out=XA[:], in0=XA[:], in1=yb[:], op=ALU.add), reads=[xid, "yb"], writes=[xid])
                s.dma(wS[:], pwq_d.rearrange("(kc p) n -> p kc n", p=128), writes=[("pe", 0, 0), ("pe", 0, 1), ("pe", 1, 0), ("pe", 1, 1)])
                rms(XA, xid)
                s.op("dve", lambda e, XA=XA: e.scalar_tensor_tensor(out=yb[:], in0=XA[:], scalar=rstd[:, 0:1], in1=bvv(BV_G2)[:, :], op0=ALU.mult, op1=ALU.mult), reads=[xid, "rstd6", "bv"], writes=["yb"])
                s.op("pool", lambda e: e.tensor_tensor(out=yb[:], in0=yb[:], in1=bvv(BV_SH2)[:, :], op=ALU.add), reads=["yb", "bv"], writes=["yb"])
                transpose8(yb, "yb", 8, 0, extra=(h2r[tau], ("h2r", tau)))
                for half in range(2):
                    for kc in range(8):
                        s.op("pe", lambda e, half=half, kc=kc: e.matmul(out=pY[half][:], lhsT=tc_[:, kc, :], rhs=wS[:, kc, half * 512:(half + 1) * 512], start=(kc == 0), stop=(kc == 7)), reads=[("tc", 0), ("tc", 1), ("pe", 0, 0), ("pe", 0, 1), ("pe", 1, 0), ("pe", 1, 1)], writes=[B(2 + half)])
                    if half == 0:
                        s.op("act", lambda e: e.copy(out=qsb[:, 0:512], in_=pY[0][:]), reads=[B(2)], writes=["qsb"])
                    else:
                        s.op("dve", lambda e: e.tensor_copy(out=qsb[:, 512:1024], in_=pY[1][:]), reads=[B(3)], writes=["qsb"])
                for rd in range(4):
                    b = rd % 2
                    for k4 in range(4):
                        hp = rd * 4 + k4
                        s.op("pe", lambda e, hp=hp, b=b, k4=k4: e.transpose(out=pT[b][0:64, k4 * 128:(k4 + 1) * 128], in_=qsb[:, hp * 64:(hp + 1) * 64], identity=ident), reads=["qsb", "cm"], writes=[B(b)])
                    if b == 0:
                        s.op("act", lambda e, rd=rd, b=b: e.copy(out=qTs[:, rd * 4:(rd + 1) * 4, :].rearrange("p a b -> p (a b)"), in_=pT[b][0:64, :]), reads=[B(b)], writes=["qTs"])
                    else:
                        s.op("dve", lambda e, rd=rd, b=b: e.tensor_copy(out=qTs[:, rd * 4:(rd + 1) * 4, :].rearrange("p a b -> p (a b)"), in_=pT[b][0:64, :]), reads=[B(b)], writes=["qTs"])
                for rd in range(4):
                    b = rd % 2
                    for k4 in range(4):
                        hp = rd * 4 + k4
                        s.op("pe", lambda e, hp=hp, b=b, k4=k4: e.matmul(out=pY[b][:, k4 * 128:(k4 + 1) * 128], lhsT=qTs[:, hp, :], rhs=keysT[:, hp, :], start=True, stop=True), reads=["qTs", "keysT"], writes=[B(2 + b)])
                    if b == 0:
                        s.op("act", lambda e, rd=rd, b=b, SC=SC: e.copy(out=SC[:, rd * 4:(rd + 1) * 4, :].rearrange("p a b -> p (a b)"), in_=pY[b][:]), reads=[B(2 + b)], writes=[scid])
                    else:
                        s.op("dve", lambda e, rd=rd, b=b, SC=SC: e.tensor_copy(out=SC[:, rd * 4:(rd + 1) * 4, :].rearrange("p a b -> p (a b)"), in_=pY[b][:]), reads=[B(2 + b)], writes=[scid])
                for hh in range(8):
                    for p in range(2):
                        srow = SC[:, 2 * hh + p, :]
                        s.op("dve", lambda e, p=p, srow=srow: e.max(out=t16[:, p, 0:8], in_=srow), reads=[scid], writes=["t16"])
                        s.op("dve", lambda e, p=p, srow=srow: e.match_replace(out=wk[:, 0:128], in_to_replace=t16[:, p, 0:8], in_values=srow, imm_value=-1e30), reads=[scid, "t16"], writes=["wk"])
                        s.op("dve", lambda e, p=p: e.max(out=t16[:, p, 8:16], in_=wk[:, 0:128]), reads=["wk"], writes=["t16"])
                    s.op("dve", lambda e: e.tensor_tensor(out=cand[:], in0=t16[:, 0, :].unsqueeze(2).to_broadcast([128, 16, 16]), in1=t16[:, 1, :].unsqueeze(1).to_broadcast([128, 16, 16]), op=ALU.add), reads=["t16"], writes=["cand"])
                    cf = cand[:].rearrange("p a b -> p (a b)")
                    s.op("dve", lambda e, cf=cf: e.max(out=c16[:, 0:8], in_=cf), reads=["cand"], writes=["c16"])
                    s.op("dve", lambda e, cf=cf: e.match_replace(out=wk[:], in_to_replace=c16[:, 0:8], in_values=cf, imm_value=-1e30), reads=["cand", "c16"], writes=["wk"])
                    s.op("dve", lambda e: e.max(out=c16[:, 8:16], in_=wk[:]), reads=["wk"], writes=["c16"])
                    s.op("dve", lambda e, hh=hh: e.tensor_scalar(out=thr[:, hh:hh + 1], in0=c16[:, 15:16], scalar1=-1e-4, scalar2=None, op0=ALU.add), reads=["c16"], writes=["thr"])
                    s.op("dve", lambda e, hh=hh: e.tensor_scalar(out=negm[:, hh:hh + 1], in0=c16[:, 0:1], scalar1=-1.0, scalar2=None, op0=ALU.mult), reads=["c16"], writes=["negm"])
                    s.op("dve", lambda e, hh=hh: e.tensor_copy(out=m1[:, hh:hh + 1], in_=t16[:, 0, 0:1]), reads=["t16"], writes=["m1"])
                    s.op("act", lambda e, hh=hh: e.activation(out=e16[:], in_=c16[:], func=AF.Exp, bias=negm[:, hh:hh + 1], accum_out=Zs[:, hh:hh + 1]), reads=["c16", "negm"], writes=["e16", "Zs"])
                s.op("dve", lambda e: e.tensor_tensor(out=kap[:], in0=thr[:], in1=negm[:], op=ALU.add), reads=["thr", "negm"], writes=["kap"])
                s.op("act", lambda e: e.activation(out=kap[:], in_=kap[:], func=AF.Exp), reads=["kap"], writes=["kap"])
                s.op("dve", lambda e: e.reciprocal(out=Zs[:], in_=Zs[:]), reads=["Zs"], writes=["Zs"])
                s.op("dve", lambda e: e.tensor_tensor(out=kap[:], in0=kap[:], in1=Zs[:], op=ALU.mult), reads=["kap", "Zs"], writes=["kap"])
                s.op("dve", lambda e: e.tensor_tensor(out=th2[:], in0=thr[:], in1=m1[:], op=ALU.subtract), reads=["thr", "m1"], writes=["th2"])
                sc4 = SC[:].rearrange("p (h q) k -> p h q k", q=2)
                s.op("dve", lambda e, sc4=sc4: e.tensor_tensor(out=sc4[:, :, 0, :], in0=sc4[:, :, 0, :], in1=m1[:].unsqueeze(2).to_broadcast([128, 8, 128]), op=ALU.subtract), reads=[scid, "m1"], writes=[scid])
                s.op("dve", lambda e, sc4=sc4: e.tensor_tensor(out=sc4[:, :, 1, :], in0=sc4[:, :, 1, :], in1=th2[:].unsqueeze(2).to_broadcast([128, 8, 128]), op=ALU.subtract), reads=[scid, "th2"], writes=[scid])
                s.op("act", lambda e, SC=SC: e.activation(out=SC[:], in_=SC[:], func=AF.Exp), reads=[scid], writes=[scid])
                for hh in range(8):
                    s.op("dve", lambda e, hh=hh, tau=tau: e.tensor_scalar(out=dg[tau][:, hh, :], in0=ident, scalar1=kap[:, hh:hh + 1], scalar2=None, op0=ALU.mult), reads=["cm", "kap"], writes=[("dg", tau)])
              pU = [[bk[4 + 2 * t + hf] for hf in range(2)] for t in range(NB)]
              ub = [None] * (ngr + 1)

              def g_load(g):
                  nonlocal gcount
                  u = gcount % 2; gcount += 1
                  ub[g] = u
                  e0 = g * GI * 128
                  s.dma(UT[u][:], UTr_s[:, e0:e0 + GI * 128].rearrange("(kc p) n -> p kc n", p=128), reads=["UTr_s"], writes=[("UT", u)], queue=("sp", "act")[u])
                  s.dma(VG[u][:], Vr_s[e0:e0 + GI * 128, :].rearrange("(a p) n -> p a n", p=128), reads=["Vr_s"], writes=[("VG", u)], queue=("act", "sp")[u])

              def g_prod(g):
                  u = ub[g]
                  for tau in range(NB):
                      sc4 = sc[tau][:].rearrange("p (h q) k -> p h q k", q=2)
                      e1b = sc4[:, :, 0, g * GI:(g + 1) * GI].unsqueeze(3).to_broadcast([128, 8, GI, 128])
                      e2b = sc4[:, :, 1, :].unsqueeze(2).to_broadcast([128, 8, GI, 128])
                      s.op("dve", lambda e, e1b=e1b, e2b=e2b, tau=tau, u=u: e.tensor_tensor(out=pe_[u][:, tau, :, :].rearrange("p h (a k) -> p h a k", a=GI), in0=e1b, in1=e2b, op=ALU.mult), reads=[("sc", tau)], writes=[("pe", u, tau)])

              def g_act(g):
                  u = ub[g]; pR = bk[u]
                  for tau in range(NB):
                      for kc in range(8):
                          s.op("pe", lambda e, kc=kc, u=u, tau=tau, pR=pR: e.matmul(out=pR[:, tau * GI * 128:(tau + 1) * GI * 128], lhsT=h2r[tau][:, kc, :], rhs=UT[u][:, kc, :], start=(kc == 0), stop=(kc == 7)), reads=[("h2r", tau), ("UT", u)], writes=[B(u)])

              def g_gelu(g):
                  u = ub[g]; pR = bk[u]
                  s.op("act", lambda e, pR=pR, u=u: e.activation(out=g1[u], in_=pR[:, 0:W5], func=AF.Gelu_apprx_tanh), reads=[B(u)], writes=[("gtq", u)])

              def g_mask(g):
                  u = ub[g]
                  for tau in range(NB):
                      s.op("dve", lambda e, u=u, tau=tau: e.scalar_tensor_tensor(out=_Mr[:, tau], in0=pe_[u][:, tau], scalar=1.0, in1=pe_[u][:, tau], op0=ALU.is_ge, op1=ALU.mult), reads=[("pe", u, tau)], writes=[("Mr", tau)])

              def g_gd(g):
                  u = ub[g]; pG = bk[2 + u]
                  for tau in range(NB):
                      for hh in range(8):
                          s.op("pe", lambda e, hh=hh, tau=tau, pG=pG: e.matmul(out=pG[:, tau * GI * 128:(tau + 1) * GI * 128], lhsT=dg[tau][:, hh, :], rhs=_Mr[:, tau, hh, :], start=(hh == 0), stop=(hh == 7)), reads=[("dg", tau), ("Mr", tau)], writes=[B(2 + u)])

              def g_pm(g):
                  u = ub[g]; pG = bk[2 + u]
                  s.op("dve", lambda e, u=u, pG=pG: e.tensor_tensor(out=Pm_[u], in0=g1[u], in1=pG[:, 0:W5], op=ALU.mult), reads=[("gtq", u), B(2 + u)], writes=[("gtq", 2 + u)])

              def g_tr(g):
                  u = ub[g]; pW = bk[2 + u]
                  for k in range(NB * GI):
                      s.op("pe", lambda e, k=k, u=u, pW=pW: e.transpose(out=pW[:, k * 128:(k + 1) * 128], in_=Pm_[u][:, k * 128:(k + 1) * 128], identity=ident), reads=[("gtq", 2 + u), "cm"], writes=[B(2 + u)])
                  s.op("act", lambda e, u=u, pW=pW: e.copy(out=PT[u][:].rearrange("p a b -> p (a b)"), in_=pW[:, 0:W5]), reads=[B(2 + u)], writes=[("PT", u)])

              def g_out(g):
                  u = ub[g]
                  for tau in range(NB):
                      for a in range(GI):
                          for half in range(2):
                              s.op("pe", lambda e, a=a, half=half, u=u, tau=tau, first=(g == 0 and a == 0), last=(g == ngr - 1 and a == GI - 1): e.matmul(out=pU[tau][half][:], lhsT=PT[u][:, tau * GI + a, :], rhs=VG[u][:, a, half * 512:(half + 1) * 512], start=first, stop=last),
                                   reads=[("PT", u), ("VG", u)], writes=[B(4 + 2 * tau + half)])

              g_load(0); g_prod(0); g_act(0); g_gelu(0); g_mask(0)
              for g in range(ngr):
                  nxt = g + 1 < ngr
                  if nxt:
                      g_load(g + 1); g_prod(g + 1)
                  g_gd(g)
                  if nxt:
                      g_act(g + 1); g_gelu(g + 1)
                  g_pm(g)
                  if nxt:
                      g_mask(g + 1)
                  g_tr(g)
                  g_out(g)
              for tau in range(NB if cut >= 7 else 0):
                i = blk * NB + tau
                XA = xa[tau]; xid = ("xa", tau)
                for half in range(2):
                    s.op("dve", lambda e, half=half, tau=tau: e.tensor_tensor(out=yb[:, half * 512:(half + 1) * 512], in0=pU[tau][half][:], in1=bvv(BV_GT2)[:, half * 512:(half + 1) * 512], op=ALU.mult), reads=[B(4 + 2 * tau + half), "bv"], writes=["yb"])
                s.op("pool", lambda e, XA=XA: e.tensor_tensor(out=XA[:], in0=XA[:], in1=yb[:], op=ALU.add), reads=[xid, "yb"], writes=[xid])
                rms(XA, xid)
                s.op("dve", lambda e, XA=XA: e.scalar_tensor_tensor(out=yb[:], in0=XA[:], scalar=rstd[:, 0:1], in1=bvv(BV_FN)[:, :], op0=ALU.mult, op1=ALU.mult), reads=[xid, "rstd6", "bv"], writes=["yb"])
                s.dma(out_d[i * 128:(i + 1) * 128, :], yb[:], reads=["yb"], writes=["out"], queue="pool")
            s.flush()
        return nc


def _rope(s, src, dst, tmp, R, rid, H, sid, did):
    sv = src.rearrange("p (h a b c) -> p h a b c", h=H, a=2, b=2)
    dv = dst.rearrange("p (h a b c) -> p h a b c", h=H, a=2, b=2)
    tv = tmp[:, 0:H * 32].rearrange("p (h a c) -> p h a c", h=H, a=2)
    rv = R[:].rearrange("p (a b c) -> p a b c", a=2, b=2)
    cosb = rv[:, :, 0, :].unsqueeze(1).to_broadcast([128, H, 2, 16])
    sinb = rv[:, :, 1, :].unsqueeze(1).to_broadcast([128, H, 2, 16])
    x1 = sv[:, :, :, 0, :]; x2 = sv[:, :, :, 1, :]
    o1 = dv[:, :, :, 0, :]; o2 = dv[:, :, :, 1, :]
    s.op("dve", lambda e: e.tensor_tensor(out=o1, in0=x1, in1=cosb, op=ALU.mult), reads=[sid, rid], writes=[did])
    s.op("dve", lambda e: e.tensor_tensor(out=tv, in0=x2, in1=sinb, op=ALU.mult), reads=[sid, rid], writes=["tmp"])
    s.op("dve", lambda e: e.tensor_tensor(out=o1, in0=o1, in1=tv, op=ALU.subtract), reads=[did, "tmp"], writes=[did])
    s.op("dve", lambda e: e.tensor_tensor(out=o2, in0=x1, in1=sinb, op=ALU.mult), reads=[sid, rid, did], writes=[did])
    s.op("dve", lambda e: e.tensor_tensor(out=tv, in0=x2, in1=cosb, op=ALU.mult), reads=[sid, rid, did], writes=["tmp"])
    s.op("dve", lambda e: e.tensor_tensor(out=o2, in0=o2, in1=tv, op=ALU.add), reads=[did, "tmp"], writes=[did])


def _host_inputs(inputs, b, consts):
    g = lambda k: np.ascontiguousarray(inputs[k], dtype=np.float32)
    m = {
        "x": g("x")[b], "c": g("c")[b], "ctx": g("ctx")[b], "c_ctx": g("c_ctx"),
        "w_ada": g("w_ada")[0], "b_ada": g("b_ada")[0], "norm_mix": g("norm_mix")[0], "norm_ffn": g("norm_ffn")[0],
        "w_in": g("w_in")[0], "b_gate": g("b_gate")[0], "attn_sink": g("attn_sink")[0], "dn_conv": g("dn_conv")[0],
        "dn_a_log_f": g("dn_a_log_f")[0], "dn_dt_bias_f": g("dn_dt_bias_f")[0], "dn_a_log_b": g("dn_a_log_b")[0], "dn_dt_bias_b": g("dn_dt_bias_b")[0],
        "dn_norm": g("dn_norm")[0], "w_br_attn": g("w_br_attn")[0], "w_br_dn": g("w_br_dn")[0], "w_out": g("w_out")[0],
        "peer_wq": g("peer_wq")[0], "final_norm": g("final_norm"),
    }
    m.update(consts)
    return {k: np.ascontiguousarray(v) for k, v in m.items()}


_SHARED = {}


def kernel(**inputs):
    consts = _consts()
    nc = build()
    keysT = np.ascontiguousarray(np.transpose(np.asarray(inputs["peer_keys"], np.float32)[0], (0, 1, 3, 2)))
    uT = np.ascontiguousarray(np.asarray(inputs["peer_u"], np.float32)[0].T)
    pv = np.ascontiguousarray(np.asarray(inputs["peer_v"], np.float32)[0])
    in_maps = []
    for b in range(8):
        m = _host_inputs(inputs, b, consts)
        m["peer_keysT"] = keysT; m["peer_uT"] = uT; m["peer_v"] = pv
        in_maps.append(m)
    res = run_bass_kernel_spmd(nc, in_maps, core_ids=list(range(8)))
    return np.stack([np.asarray(r["out"], dtype=np.float32) for r in res.results], axis=0)
```

```python
import contextlib
import numpy as np
import concourse.bass as bass
import concourse.mybir as mybir
from concourse.bass_utils import run_bass_kernel_spmd

F32 = mybir.dt.float32
F32R = mybir.dt.float32r
ALU = mybir.AluOpType
AF = mybir.ActivationFunctionType
AX = mybir.AxisListType

D = 1024
S = 8192
CTX = 256
TALL = CTX + S
NT = S // 128
IN_COLS = 4880
NEG = -30000.0


class _Ins:
    __slots__ = ("eng", "fn", "deps", "signal", "sig_no", "dma", "idx")

    def __init__(self, eng, fn, dma=None):
        self.eng = eng
        self.fn = fn
        self.deps = []
        self.signal = False
        self.sig_no = None
        self.dma = dma
        self.idx = None


class Sch:
    EPOCH = 20000
    NDMA = 24
    NEP = 16

    def __init__(self, nc, st):
        self.nc = nc
        self.engs = ("pe", "act", "dve", "pool", "sp")
        self.nep = {"pe": 12, "act": 4, "dve": 6, "pool": 3, "sp": 1}
        self.sems = {e: [st.enter_context(nc.semaphore(f"s_{e}_{i}")) for i in range(self.nep[e])] for e in self.engs}
        self.dsems = [st.enter_context(nc.semaphore(f"s_dma_{i}")) for i in range(self.NDMA)]
        self.sigc = {e: 0 for e in self.engs}
        self.dma_rr = 0
        self.dma_cnt = [0] * self.NDMA
        self.dma_last = [None] * self.NDMA
        self._reset()

    def _reset(self):
        self.q = {e: [] for e in self.engs}
        self.lastw = {}
        self.readers = {}

    def _add(self, ins, reads, writes):
        q = self.q[ins.eng]
        ins.idx = len(q)
        deps = []
        for r in reads:
            w = self.lastw.get(r)
            if w is not None:
                deps.append((w, "raw"))
        for w_ in writes:
            w = self.lastw.get(w_)
            if w is not None:
                deps.append((w, "waw"))
            for rd in self.readers.get(w_, ()):
                deps.append((rd, "war"))
        for d, kind in deps:
            if d is ins:
                continue
            if d.dma is None and ins.dma is None and d.eng == ins.eng:
                if ins.eng == "pe":
                    continue
                if kind != "raw":
                    continue
            ins.deps.append(d)
            if d.dma is None:
                d.signal = True
        for r in reads:
            self.readers.setdefault(r, []).append(ins)
        for w_ in writes:
            self.lastw[w_] = ins
            self.readers[w_] = []
        q.append(ins)
        return ins

    PSUM_NAMES = {"bk", "pm", "pT", "pY", "pX", "pN", "pK", "pb", "pS", "pO", "pQ", "pZ", "pR", "pU", "pW"}

    def op(self, eng, fn, reads=(), writes=()):
        writes = list(writes)
        if eng != "pe":
            for r in reads:
                if isinstance(r, tuple) and r[0] in self.PSUM_NAMES and r not in writes:
                    writes.append(r)
        return self._add(_Ins(eng, fn), list(reads), writes)

    def dma(self, out, in_, reads=(), writes=(), queue="sp", **kw):
        slot = self.dma_rr
        self.dma_rr = (self.dma_rr + 1) % self.NDMA
        self.dma_cnt[slot] += 1
        n = self.dma_cnt[slot]
        ins = _Ins(queue, lambda e: e.dma_start(out=out, in_=in_, **kw), dma=(slot, n))
        prev = self.dma_last[slot]
        self._add(ins, list(reads), list(writes))
        if prev is not None:
            ins.deps.append(prev)
        self.dma_last[slot] = ins
        return ins

    def flush(self):
        nc = self.nc
        for e, q in self.q.items():
            for ins in q:
                if ins.dma is None and ins.signal:
                    ins.sig_no = self.sigc[e]
                    self.sigc[e] += 1
            assert self.sigc[e] < self.EPOCH * self.nep[e], f"too many signals on {e}: {self.sigc[e]}"
        dma_final = list(self.dma_cnt)
        with nc.Block() as block:
            def run(ename):
                def body(eng):
                    seen_c = {}
                    seen_d = {}
                    for ins in self.q[ename]:
                        wc = {}
                        wd = {}
                        for d in ins.deps:
                            if d.dma is None:
                                if d.sig_no is None:
                                    continue
                                if seen_c.get(d.eng, -1) < d.sig_no:
                                    wc[d.eng] = max(wc.get(d.eng, -1), d.sig_no)
                            else:
                                s_, n = d.dma
                                if seen_d.get(s_, 0) < n:
                                    wd[s_] = max(wd.get(s_, 0), n)
                        for e2, sn in wc.items():
                            eng.wait_ge(self.sems[e2][sn // self.EPOCH], sn % self.EPOCH + 1)
                            seen_c[e2] = sn
                        for s_, n in wd.items():
                            eng.wait_ge(self.dsems[s_], 16 * n)
                            seen_d[s_] = n
                        h = ins.fn(eng)
                        if ins.dma is not None:
                            h.then_inc(self.dsems[ins.dma[0]], 16)
                        elif ins.signal:
                            h.then_inc(self.sems[ename][ins.sig_no // self.EPOCH], 1)
                    if ename == "sp":
                        for s_, n in enumerate(dma_final):
                            if n > 0:
                                eng.wait_ge(self.dsems[s_], 16 * n)
                return body

            block.sync(run("sp"))
            block.tensor(run("pe"))
            block.scalar(run("act"))
            block.vector(run("dve"))
            block.gpsimd(run("pool"))
        nc.all_engine_barrier()
        self._reset()


def _consts():
    c = {}
    ident = np.eye(128, dtype=np.float32)
    ones = np.ones((128, 128), np.float32)
    idx = np.arange(128)
    same = (idx[:, None] // 64 == idx[None, :] // 64).astype(np.float32)
    m1f = ((idx[:, None] <= idx[None, :]) * same).astype(np.float32)
    m1b = ((idx[:, None] >= idx[None, :]) * same).astype(np.float32)
    sel0 = np.zeros((128, 128), np.float32); sel0[:64, :] = 1
    sel1 = np.zeros((128, 128), np.float32); sel1[64:, :] = 1
    low_incl = ((idx[None, :] <= idx[:, None]) * same)
    up_incl = ((idx[None, :] >= idx[:, None]) * same)
    low_strict = ((idx[None, :] < idx[:, None]) * same)
    up_strict = ((idx[None, :] > idx[:, None]) * same)
    negmask = lambda m: np.where(m > 0, 0.0, NEG).astype(np.float32)
    w_prev = (idx[None, :] <= idx[:, None]).astype(np.float32)
    w_next = (idx[:, None] <= idx[None, :]).astype(np.float32)
    mats = [ident, ones, same, m1f, -m1f, m1b, -m1b, sel0, sel1,
            negmask(low_incl), negmask(up_incl), -low_strict.astype(np.float32), -up_strict.astype(np.float32),
            w_prev, w_next]
    c["cmat"] = np.ascontiguousarray(np.stack(mats, axis=1)).astype(np.float32)
    pos = np.arange(S)
    inv = (10000.0 ** (-np.arange(16, dtype=np.float32) / 16)).astype(np.float32)
    ar = (pos // 64).astype(np.float32)[:, None] * inv[None, :]
    ac = (pos % 64).astype(np.float32)[:, None] * inv[None, :]
    c["rope"] = np.concatenate([np.cos(ar), np.sin(ar), np.cos(ac), np.sin(ac)], axis=1).astype(np.float32)
    return c

(C_ID, C_ONES, C_SAME, C_M1F, C_NM1F, C_M1B, C_NM1B, C_SEL0, C_SEL1, C_NLOW, C_NUP, C_SLOW, C_SUP, C_WPREV, C_WNEXT) = range(15)


def build(upto=99, dbg=()):
    nc = bass.Bass("TRN2", target_bir_lowering=False)
    nc.dge_precook = False
    inp = lambda name, shape: nc.dram_tensor(name, list(shape), F32, kind="ExternalInput").ap()
    x_d = inp("x", [S, D]); c_d = inp("c", [D]); ctx_d = inp("ctx", [CTX, D]); cctx_d = inp("c_ctx", [D])
    wada_d = inp("w_ada", [D, 6 * D]); bada_d = inp("b_ada", [6 * D])
    nmix_d = inp("norm_mix", [D]); nffn_d = inp("norm_ffn", [D])
    win_d = inp("w_in", [D, IN_COLS]); bgate_d = inp("b_gate", [2 * D])
    sink_d = inp("attn_sink", [8]); conv_d = inp("dn_conv", [5, 1536])
    alf_d = inp("dn_a_log_f", [4]); dtf_d = inp("dn_dt_bias_f", [4]); alb_d = inp("dn_a_log_b", [4]); dtb_d = inp("dn_dt_bias_b", [4])
    dnn_d = inp("dn_norm", [128]); wba_d = inp("w_br_attn", [512, D]); wbd_d = inp("w_br_dn", [512, D]); wout_d = inp("w_out", [D, D])
    pwq_d = inp("peer_wq", [D, D]); pkeys_d = inp("peer_keysT", [128, 8, 128]); pu_d = inp("peer_uT", [D, 16384]); pv_d = inp("peer_v", [16384, D])
    fnorm_d = inp("final_norm", [D]); cmat_d = inp("cmat", [128, 15, 128]); rope_d = inp("rope", [S, 64])
    out_d = nc.dram_tensor("out", [S, D], F32, kind="ExternalOutput").ap()
    scr = lambda name, shape: nc.dram_tensor(name, list(shape), F32, kind=("ExternalOutput" if name in dbg else "Internal")).ap()
    QT_s = scr("QT_s", [64, 8, S])
    KT_s = scr("KT_s", [64, 2, TALL])
    V_s = scr("V_s", [TALL, 2, 65])
    RT_s = scr("RT_s", [1536, TALL])
    Z_s = scr("Z_s", [S, 512])
    GB_s = scr("GB_s", [TALL, 16])
    GT_s = scr("GT_s", [S, 2048])
    QK_s = scr("QK_s", [1024, TALL])
    KV_s = scr("KV_s", [TALL, 1024])
    OD_s = scr("OD_s", [2, S, 512])
    OA_s = scr("OA_s", [S, 512])
    MOD_s = scr("MOD_s", [8, D])

    with contextlib.ExitStack() as gst:
        s = Sch(nc, gst)
        _uid = [0]

        def _nm(name):
            _uid[0] += 1
            return f"{name}_u{_uid[0]}"
        T = lambda st, name, shape: st.enter_context(nc.sbuf_tensor(_nm(name), list(shape), F32))
        PS = lambda st, name, shape: st.enter_context(nc.psum_tensor(_nm(name), list(shape), F32))
        cm = T(gst, "cm", [128, 15, 128])
        s.dma(cm[:], cmat_d, writes=["cm"])
        ident = cm[:, C_ID, :]
        BV_G1, BV_SH1, BV_GT1, BV_G2, BV_SH2, BV_GT2, BV_CG1, BV_CSH1, BV_FN = range(9)
        bvB = T(gst, "bvB", [128, 5, D])
        stA = contextlib.ExitStack()
        bvA = T(stA, "bvA", [128, 4, D])
        _amap = {BV_G1: 0, BV_SH1: 1, BV_CG1: 2, BV_CSH1: 3}
        _bmap = {BV_GT1: 0, BV_G2: 1, BV_SH2: 2, BV_GT2: 3, BV_FN: 4}

        def bvv(k):
            return bvA[:, _amap[k], :] if k in _amap else bvB[:, _bmap[k], :]

        with contextlib.ExitStack() as st:
            cc = T(st, "cc", [128, 2, 8]); cs = T(st, "cs", [128, 2, 8]); lh = T(st, "lh", [128, 2, 8, 128])
            wa = [T(st, f"wa{i}", [128, 8, 512]) for i in range(2)]
            bb = T(st, "bb", [128, 6 * D]); nm = T(st, "nm", [128, 2, D])
            pm = [PS(st, f"pm{i}", [128, 512]) for i in range(2)]
            s.dma(cc[:, 0, :], c_d.rearrange("(kc p) -> p kc", p=128), writes=["cc"], allow_slow_non_contiguous=True)
            s.dma(cc[:, 1, :], cctx_d.rearrange("(kc p) -> p kc", p=128), writes=["cc"], allow_slow_non_contiguous=True)
            s.dma(bb[:], bada_d.partition_broadcast(128), writes=["bb"])
            s.dma(nm[:, 0, :], nmix_d.partition_broadcast(128), writes=["nm"])
            s.dma(nm[:, 1, :], nffn_d.partition_broadcast(128), writes=["nm"])
            s.dma(bvv(BV_FN)[:, :], fnorm_d.partition_broadcast(128), writes=["bv"])
            s.op("act", lambda e: e.activation(out=cs[:], in_=cc[:], func=AF.Silu), reads=["cc"], writes=["cs"])
            s.op("dve", lambda e: e.tensor_copy(out=lh[:], in_=cs[:].unsqueeze(3).to_broadcast([128, 2, 8, 128])), reads=["cs"], writes=["lh"])
            jobs = [(0, nb) for nb in range(12)] + [(1, nb) for nb in range(4)]
            for ji, (w, nb) in enumerate(jobs):
                wt = wa[ji % 2]; p = pm[ji % 2]
                s.dma(wt[:], wada_d[:, nb * 512:(nb + 1) * 512].rearrange("(kc p) n -> p kc n", p=128), writes=[("wa", ji % 2)], queue=("sp" if ji % 2 == 0 else "act"))
                for kc in range(8):
                    s.op("pe", lambda e, w=w, kc=kc, wt=wt, p=p: e.matmul(out=p[:], lhsT=lh[:, w, kc, :], rhs=wt[:, kc, :], start=(kc == 0), stop=(kc == 7)),
                         reads=["lh", ("wa", ji % 2)], writes=[("pm", ji % 2)])
                ch, half = nb // 2, nb % 2
                if w == 0:
                    dst = {0: BV_SH1, 1: BV_G1, 2: BV_GT1, 3: BV_SH2, 4: BV_G2, 5: BV_GT2}[ch]
                else:
                    dst = {0: BV_CSH1, 1: BV_CG1}[ch]
                o = bvv(dst)[:, half * 512:(half + 1) * 512]
                s.op("dve", lambda e, o=o, p=p, nb=nb: e.tensor_tensor(out=o, in0=p[:], in1=bb[:, nb * 512:(nb + 1) * 512], op=ALU.add),
                     reads=[("pm", ji % 2), "bb"], writes=["bv"])
            for dst, ni in ((BV_G1, 0), (BV_G2, 1), (BV_CG1, 0)):
                s.op("dve", lambda e, dst=dst, ni=ni: e.scalar_tensor_tensor(out=bvv(dst)[:, :], in0=bvv(dst)[:, :], scalar=1.0, in1=nm[:, ni, :], op0=ALU.add, op1=ALU.mult),
                     reads=["bv", "nm"], writes=["bv"])
            s.flush()
        if upto <= 0:
            stA.close()
            return nc

        blocks = [(0, 512), (512, 256), (768, 512), (1280, 512), (1792, 512), (2304, 512), (2816, 16)] + [(2832 + 512 * i, 512) for i in range(4)]
        with contextlib.ExitStack() as st:
            xt = [T(st, f"xt{i}", [128, D]) for i in range(2)]
            junk = T(st, "junk", [128, D]); ss = T(st, "ss", [128, 1]); rstd = T(st, "rstd", [128, 1])
            h = T(st, "h", [128, D]); hT = T(st, "hT", [128, 8, 128])
            wb = [T(st, f"wb{i}", [128, 8, 512]) for i in range(3)]
            rp = [T(st, f"rp{i}", [128, 64]) for i in range(2)]
            qs = T(st, "qs", [128, 512]); qr = T(st, "qr", [128, 512]); tmp = T(st, "tmp", [128, 512])
            qT = T(st, "qT", [64, 8, 128]); kvs = T(st, "kvs", [128, 256]); kr = T(st, "kr", [128, 128]); kT = T(st, "kT", [64, 2, 128])
            va = T(st, "va", [128, 2, 65]); rw = T(st, "rw", [128, 512]); rT = T(st, "rT", [128, 4, 128])
            zz = T(st, "zz", [128, 512]); gn = T(st, "gn", [128, 128]); ab = T(st, "ab", [128, 16]); abc = T(st, "abc", [128, 2, 8])
            gbo = T(st, "gbo", [128, 16]); gg = T(st, "gg", [128, 512]); bg = T(st, "bg", [128, 2048])
            pT = [PS(st, f"pT{i}", [128, 512]) for i in range(2)]
            pY = [PS(st, f"pY{i}", [128, 512]) for i in range(3)]
            pX = [PS(st, f"pX{i}", [128, 512]) for i in range(2)]
            s.dma(bg[:], bgate_d.partition_broadcast(128), writes=["bg"])
            s.dma(gn[:], dnn_d.partition_broadcast(128), writes=["gn"])
            s.dma(abc[:, 0, 0:4], dtf_d.partition_broadcast(128), writes=["abc"])
            s.dma(abc[:, 0, 4:8], dtb_d.partition_broadcast(128), writes=["abc"])
            s.dma(abc[:, 1, 0:4], alf_d.partition_broadcast(128), writes=["abc"])
            s.dma(abc[:, 1, 4:8], alb_d.partition_broadcast(128), writes=["abc"])
            s.op("act", lambda e: e.activation(out=abc[:, 1, :], in_=abc[:, 1, :], func=AF.Exp), reads=["abc"], writes=["abc"])
            s.op("dve", lambda e: e.tensor_scalar(out=abc[:, 1, :], in0=abc[:, 1, :], scalar1=-1.0, scalar2=None, op0=ALU.mult), reads=["abc"], writes=["abc"])
            s.op("pool", lambda e: e.memset(va[:], 1.0), writes=["va"])
            wcount = [0]

            def rope_ops(src, dst, H):
                sv = src.rearrange("p (h a b c) -> p h a b c", h=H, a=2, b=2)
                dv = dst.rearrange("p (h a b c) -> p h a b c", h=H, a=2, b=2)
                tv = tmp[:, 0:H * 64].rearrange("p (h a b c) -> p h a b c", h=H, a=2, b=2)
                return sv, dv, tv

            tiles = [("c", i) for i in range(CTX // 128)] + [("l", i) for i in range(NT)]
            if upto == 1 and "small" in dbg:
                tiles = tiles[:4]
            hT4 = [T(st, f"hT4_{j}", [128, 8, 128]) for j in range(4)]
            rp4 = [T(st, f"rp4_{j}", [128, 64]) for j in range(4)]
            pcount = [0]

            def prep(ti, kind, i, j):
                lat = kind == "l"
                src = x_d if lat else ctx_d
                tg = ti
                X = xt[ti % 2]; xid = ("xt", ti % 2)
                s.dma(X[:], src[i * 128:(i + 1) * 128, :], writes=[xid])
                if lat:
                    R = rp4[j]; rid = ("rp", j)
                    s.dma(R[:], rope_d[i * 128:(i + 1) * 128, :], writes=[rid], queue="act")
                s.op("act", lambda e, X=X: e.activation(out=junk[:], in_=X[:], func=AF.Square, accum_out=ss[:]), reads=[xid], writes=["junk", "ss"])
                s.op("dve", lambda e: e.tensor_scalar(out=rstd[:], in0=ss[:], scalar1=1.0 / D, scalar2=1e-6, op0=ALU.mult, op1=ALU.add), reads=["ss"], writes=["rstd"])
                s.op("act", lambda e: e.sqrt(out=rstd[:], in_=rstd[:]), reads=["rstd"], writes=["rstd"])
                s.op("dve", lambda e: e.reciprocal(out=rstd[:], in_=rstd[:]), reads=["rstd"], writes=["rstd"])
                G = BV_G1 if lat else BV_CG1
                SH = BV_SH1 if lat else BV_CSH1
                s.op("dve", lambda e, X=X, G=G: e.scalar_tensor_tensor(out=h[:], in0=X[:], scalar=rstd[:, 0:1], in1=bvv(G)[:, :], op0=ALU.mult, op1=ALU.mult), reads=[xid, "rstd", "bv"], writes=["h"])
                s.op("pool", lambda e, SH=SH: e.tensor_tensor(out=h[:], in0=h[:], in1=bvv(SH)[:, :], op=ALU.add), reads=["h", "bv"], writes=["h"])
                for hb in range(2):
                    for k4 in range(4):
                        kc = hb * 4 + k4
                        s.op("pe", lambda e, kc=kc, hb=hb, k4=k4: e.transpose(out=pT[hb][:, k4 * 128:(k4 + 1) * 128], in_=h[:, kc * 128:(kc + 1) * 128], identity=ident), reads=["h", "cm"], writes=[("pT", hb)])
                    eng = "act" if hb == 0 else "dve"
                    if eng == "act":
                        s.op("act", lambda e, hb=hb: e.copy(out=hT4[j][:, hb * 4:(hb + 1) * 4, :].rearrange("p a b -> p (a b)"), in_=pT[hb][:]), reads=[("pT", hb)], writes=[("hT", j, hb)])
                    else:
                        s.op("dve", lambda e, hb=hb: e.tensor_copy(out=hT4[j][:, hb * 4:(hb + 1) * 4, :].rearrange("p a b -> p (a b)"), in_=pT[hb][:]), reads=[("pT", hb)], writes=[("hT", j, hb)])

            def proj(ti, kind, i, j, bi, W, wi):
                lat = kind == "l"; tg = ti; R = rp4[j]; rid = ("rp", j)
                c0, cw = blocks[bi]
                pi_ = pcount[0] % 3; pcount[0] += 1
                P = pY[pi_]
                for kc in range(8):
                    s.op("pe", lambda e, kc=kc, W=W, P=P, cw=cw: e.matmul(out=P[:, 0:cw], lhsT=hT4[j][:, kc, :], rhs=W[:, kc, 0:cw], start=(kc == 0), stop=(kc == 7)),
                         reads=[("hT", j, 0), ("hT", j, 1), ("wb", wi)], writes=[("pY", pi_)])
                pid = ("pY", pi_)
                if bi == 0:
                    s.op("act", lambda e, P=P: e.activation(out=qs[:], in_=P[:], func=AF.Copy, scale=0.125), reads=[pid], writes=["qs"])
                    _rope(s, qs[:], qr[:], tmp, R, rid, 8, "qs", "qr")
                    for hh in range(8):
                        s.op("pe", lambda e, hh=hh: e.transpose(out=pX[hh // 4][0:64, (hh % 4) * 128:(hh % 4 + 1) * 128], in_=qr[:, hh * 64:(hh + 1) * 64], identity=ident), reads=["qr", "cm"], writes=[("pX", hh // 4)])
                    s.op("act", lambda e: e.copy(out=qT[:, 0:4, :].rearrange("p a b -> p (a b)"), in_=pX[0][0:64, :]), reads=[("pX", 0)], writes=["qT"])
                    s.op("dve", lambda e: e.tensor_copy(out=qT[:, 4:8, :].rearrange("p a b -> p (a b)"), in_=pX[1][0:64, :]), reads=[("pX", 1)], writes=["qT"])
                    s.dma(QT_s[:, :, i * 128:(i + 1) * 128], qT[:], reads=["qT"], writes=["QT_s"], queue="pool")
                elif bi == 1:
                    s.op("act", lambda e, P=P: e.copy(out=kvs[:], in_=P[:, 0:256]), reads=[pid], writes=["kvs"])
                    if lat:
                        _rope(s, kvs[:, 0:128], kr[:], tmp, R, rid, 2, "kvs", "kr")
                        ksrc, kid = kr, "kr"
                    else:
                        ksrc, kid = kvs, "kvs"
                    for hh in range(2):
                        s.op("pe", lambda e, hh=hh, ksrc=ksrc: e.transpose(out=pX[0][0:64, hh * 128:(hh + 1) * 128], in_=ksrc[:, hh * 64:(hh + 1) * 64], identity=ident), reads=[kid, "cm"], writes=[("pX", 0)])
                    s.op("act", lambda e: e.copy(out=kT[:].rearrange("p a b -> p (a b)"), in_=pX[0][0:64, 0:256]), reads=[("pX", 0)], writes=["kT"])
                    s.dma(KT_s[:, :, tg * 128:(tg + 1) * 128], kT[:], reads=["kT"], writes=["KT_s"], queue="pool")
                    s.op("pool", lambda e: e.tensor_copy(out=va[:, :, 0:64], in_=kvs[:, 128:256].rearrange("p (g d) -> p g d", g=2)), reads=["kvs"], writes=["va"])
                    s.dma(V_s[tg * 128:(tg + 1) * 128, :, :], va[:], reads=["va"], writes=["V_s"], queue="pool")
                elif bi in (2, 3, 4):
                    s.op("act", lambda e, P=P: e.copy(out=rw[:], in_=P[:]), reads=[pid], writes=["rw"])
                    for k4 in range(4):
                        s.op("pe", lambda e, k4=k4: e.transpose(out=pX[1][:, k4 * 128:(k4 + 1) * 128], in_=rw[:, k4 * 128:(k4 + 1) * 128], identity=ident), reads=["rw", "cm"], writes=[("pX", 1)])
                    s.op("dve", lambda e: e.tensor_copy(out=rT[:].rearrange("p a b -> p (a b)"), in_=pX[1][:]), reads=[("pX", 1)], writes=["rT"])
                    f0 = (bi - 2) * 512
                    s.dma(RT_s[f0:f0 + 512, tg * 128:(tg + 1) * 128].rearrange("(a p) t -> p a t", p=128), rT[:], reads=["rT"], writes=["RT_s"], queue="pool")
                elif bi == 5:
                    s.op("act", lambda e, P=P: e.activation(out=zz[:], in_=P[:], func=AF.Silu), reads=[pid], writes=["zz"])
                    s.op("pool", lambda e: e.tensor_tensor(out=zz[:].rearrange("p (h d) -> p h d", h=4), in0=zz[:].rearrange("p (h d) -> p h d", h=4), in1=gn[:].unsqueeze(1).to_broadcast([128, 4, 128]), op=ALU.mult), reads=["zz", "gn"], writes=["zz"])
                    s.dma(Z_s[i * 128:(i + 1) * 128, :], zz[:], reads=["zz"], writes=["Z_s"], queue="pool")
                elif bi == 6:
                    s.op("dve", lambda e, P=P: e.tensor_tensor(out=ab[:, 0:8], in0=P[:, 0:8], in1=abc[:, 0, :], op=ALU.add), reads=[pid, "abc"], writes=["ab"])
                    s.op("act", lambda e: e.activation(out=ab[:, 0:8], in_=ab[:, 0:8], func=AF.Exp), reads=["ab"], writes=["ab"])
                    s.op("dve", lambda e: e.tensor_scalar(out=ab[:, 0:8], in0=ab[:, 0:8], scalar1=1.0, scalar2=None, op0=ALU.add), reads=["ab"], writes=["ab"])
                    s.op("act", lambda e: e.activation(out=ab[:, 0:8], in_=ab[:, 0:8], func=AF.Ln), reads=["ab"], writes=["ab"])
                    s.op("dve", lambda e: e.tensor_tensor(out=gbo[:, 0:8], in0=ab[:, 0:8], in1=abc[:, 1, :], op=ALU.mult), reads=["ab", "abc"], writes=["gbo"])
                    s.op("act", lambda e, P=P: e.activation(out=gbo[:, 8:16], in_=P[:, 8:16], func=AF.Sigmoid), reads=[pid], writes=["gbo"])
                    s.dma(GB_s[tg * 128:(tg + 1) * 128, :], gbo[:], reads=["gbo"], writes=["GB_s"], queue="pool")
                else:
                    gi = bi - 7
                    s.op("dve", lambda e, P=P, gi=gi: e.tensor_tensor(out=gg[:], in0=P[:], in1=bg[:, gi * 512:(gi + 1) * 512], op=ALU.add), reads=[pid, "bg"], writes=["gg"])
                    s.op("act", lambda e: e.activation(out=gg[:], in_=gg[:], func=AF.Sigmoid), reads=["gg"], writes=["gg"])
                    s.dma(GT_s[i * 128:(i + 1) * 128, gi * 512:(gi + 1) * 512], gg[:], reads=["gg"], writes=["GT_s"], queue="pool")

            groups = [tiles[0:2]] + [tiles[k:k + 4] for k in range(2, len(tiles), 4)]
            tbase = 0
            for grp in groups:
                for j, (kind, i) in enumerate(grp):
                    prep(tbase + j, kind, i, j)
                need = range(11) if grp[0][0] == "l" else (1, 2, 3, 4, 6)
                for bi in need:
                    c0, cw = blocks[bi]
                    wi = wcount[0] % 3; wcount[0] += 1
                    W = wb[wi]
                    s.dma(W[:, :, 0:cw], win_d[:, c0:c0 + cw].rearrange("(kc p) n -> p kc n", p=128), writes=[("wb", wi)], queue=("sp", "act", "pool")[wi], allow_slow_non_contiguous=(cw < 128))
                    for j, (kind, i) in enumerate(grp):
                        proj(tbase + j, kind, i, j, bi, W, wi)
                tbase += len(grp)
            s.flush()
        stA.close()
        if upto <= 1:
            return nc

        with contextlib.ExitStack() as st:
            cw = T(st, "cw", [128, 12, 5])
            Rt = [T(st, f"Rt{i}", [128, 516]) for i in range(3)]
            acc = [T(st, f"acc{i}", [128, 512]) for i in range(2)]
            y = [T(st, f"y{i}", [128, 512]) for i in range(2)]
            y2 = T(st, "y2", [128, 512]); rn = T(st, "rn", [128, 512]); yn = [T(st, f"yn{i}", [128, 512]) for i in range(2)]
            tok = [T(st, f"tok{i}", [128, 4, 128]) for i in range(2)]
            pN = [PS(st, f"pN{i}", [128, 512]) for i in range(2)]
            pK = [PS(st, f"pK{i}", [128, 512]) for i in range(2)]
            for j in range(5):
                s.dma(cw[:, :, j], conv_d[j, :].rearrange("(fc p) -> p fc", p=128), writes=["cw"], allow_slow_non_contiguous=True)
            it = 0
            segs = [(0, CTX), (CTX, TALL)]
            if "small" in dbg:
                segs = [(0, CTX), (CTX, CTX + 256)]
            for (g0, g1) in segs:
                for t0 in range(g0, g1, 512):
                    n = min(512, g1 - t0)
                    for fc in range(12):
                        R = Rt[it % 3]; rid = ("Rt", it % 3); A = acc[it % 2]; aid = ("acc", it % 2); Y = y[it % 2]; yid = ("y", it % 2)
                        lo = max(t0 - 2, g0); hi = min(t0 + n + 2, g1)
                        if lo > t0 - 2 or hi < t0 + n + 2:
                            s.op("pool", lambda e, R=R: e.memset(R[:], 0.0), writes=[rid])
                        s.dma(R[:, lo - (t0 - 2):hi - (t0 - 2)], RT_s[fc * 128:(fc + 1) * 128, lo:hi], reads=["RT_s"], writes=[rid], queue=("sp", "act")[it % 2])
                        s.op("dve", lambda e, R=R, A=A, fc=fc, n=n: e.tensor_scalar(out=A[:, 0:n], in0=R[:, 0:n], scalar1=cw[:, fc, 0:1], scalar2=None, op0=ALU.mult), reads=[rid, "cw"], writes=[aid])
                        for j in range(1, 5):
                            s.op("dve", lambda e, R=R, A=A, fc=fc, n=n, j=j: e.scalar_tensor_tensor(out=A[:, 0:n], in0=R[:, j:j + n], scalar=cw[:, fc, j:j + 1], in1=A[:, 0:n], op0=ALU.mult, op1=ALU.add), reads=[rid, "cw", aid], writes=[aid])
                        s.op("act", lambda e, A=A, Y=Y, n=n: e.activation(out=Y[:, 0:n], in_=A[:, 0:n], func=AF.Silu), reads=[aid], writes=[yid])
                        src, sid = Y, yid
                        if fc < 8:
                            YN = yn[it % 2]; nid = ("yn", it % 2); P = pN[it % 2]; pid = ("pN", it % 2)
                            s.op("act", lambda e, Y=Y, n=n: e.activation(out=y2[:, 0:n], in_=Y[:, 0:n], func=AF.Square), reads=[yid], writes=["y2"])
                            s.op("pe", lambda e, P=P, n=n: e.matmul(out=P[:, 0:n], lhsT=cm[:, C_ONES, :], rhs=y2[:, 0:n], start=True, stop=True), reads=["cm", "y2"], writes=[pid])
                            s.op("dve", lambda e, P=P, n=n: e.tensor_scalar(out=rn[:, 0:n], in0=P[:, 0:n], scalar1=1e-6, scalar2=None, op0=ALU.add), reads=[pid], writes=["rn"])
                            s.op("act", lambda e, n=n: e.sqrt(out=rn[:, 0:n], in_=rn[:, 0:n]), reads=["rn"], writes=["rn"])
                            s.op("dve", lambda e, n=n: e.reciprocal(out=rn[:, 0:n], in_=rn[:, 0:n]), reads=["rn"], writes=["rn"])
                            sc = float(128 ** -0.5) if fc < 4 else 1.0
                            s.op("dve", lambda e, Y=Y, YN=YN, n=n, sc=sc: e.scalar_tensor_tensor(out=YN[:, 0:n], in0=Y[:, 0:n], scalar=sc, in1=rn[:, 0:n], op0=ALU.mult, op1=ALU.mult), reads=[yid, "rn"], writes=[nid])
                            s.dma(QK_s[fc * 128:(fc + 1) * 128, t0:t0 + n], YN[:, 0:n], reads=[nid], writes=["QK_s"], queue="pool")
                            src, sid = YN, nid
                        if fc >= 4:
                            PK = pK[it % 2]; kid = ("pK", it % 2); TK = tok[it % 2]; tid = ("tok", it % 2)
                            nsb = n // 128
                            for sb in range(nsb):
                                s.op("pe", lambda e, PK=PK, src=src, sb=sb: e.transpose(out=PK[:, sb * 128:(sb + 1) * 128], in_=src[:, sb * 128:(sb + 1) * 128], identity=ident), reads=[sid, "cm"], writes=[kid])
                            s.op("act", lambda e, PK=PK, TK=TK, n=n: e.copy(out=TK[:].rearrange("p a b -> p (a b)")[:, 0:n], in_=PK[:, 0:n]), reads=[kid], writes=[tid])
                            s.dma(KV_s[t0:t0 + n, (fc - 4) * 128:(fc - 3) * 128].rearrange("(sb p) f -> p sb f", p=128), TK[:, 0:nsb, :], reads=[tid], writes=["KV_s"], queue="pool")
                        it += 1
            s.flush()
        if upto <= 2:
            return nc

        with contextlib.ExitStack() as st:
            Sst = [T(st, f"Sst{i}", [128, 4, 128]) for i in range(2)]
            qT4 = T(st, "qT4", [128, 4, 128]); kT4 = T(st, "kT4", [128, 4, 128]); ktok = T(st, "ktok", [128, 4, 128]); vtok = T(st, "vtok", [128, 4, 128])
            gb = T(st, "gb", [128, 16]); sm = T(st, "sm", [128, 16]); ex = T(st, "ex", [128, 16]); beg = T(st, "beg", [128, 4])
            G1 = T(st, "G1", [128, 4, 128]); dl = T(st, "dl", [128, 4, 128]); du = T(st, "du", [128, 4, 128])
            Bm = [T(st, f"Bm{i}", [128, 4, 128]) for i in range(2)]; Cm = [T(st, f"Cm{i}", [128, 4, 128]) for i in range(2)]; Pm = [T(st, f"Pm{i}", [128, 4, 128]) for i in range(2)]
            aT = T(st, "aT", [128, 4, 128]); kbg = T(st, "kbg", [128, 4, 128]); vb = T(st, "vb", [128, 4, 128]); ktl = T(st, "ktl", [128, 4, 128])
            WT = T(st, "WT", [128, 4, 128]); U = T(st, "U", [128, 4, 128]); vn = T(st, "vn", [128, 4, 128]); o1 = T(st, "o1", [128, 4, 128]); ot = T(st, "ot", [128, 4, 128])
            pb = [PS(st, f"pb{i}", [128, 4, 128]) for i in range(8)]
            pA, pB_, pC, pD, pE, pF, pG, pH = pb
            pid = lambda k: ("pb", k)
            H4 = [128, 4, 128]
            bc_h = lambda ap2: ap2.unsqueeze(1).to_broadcast(H4)
            bc_l = lambda ap2: ap2.unsqueeze(2).to_broadcast(H4)
            ntl = (2 if "small" in dbg else NT)
            for dr in range(2):
                M1 = cm[:, C_M1F if dr == 0 else C_M1B, :]; NM1 = cm[:, C_NM1F if dr == 0 else C_NM1B, :]
                NB = cm[:, C_NLOW if dr == 0 else C_NUP, :]; NTm = cm[:, C_NUP if dr == 0 else C_NLOW, :]
                STR = cm[:, C_SLOW if dr == 0 else C_SUP, :]
                SS = Sst[dr]; ssid = ("Sst", dr)
                s.op("pool", lambda e, SS=SS: e.memset(SS[:], 0.0), writes=[ssid])
                order = [("c", i) for i in range(CTX // 128)] + [("l", i) for i in range(ntl)]
                if dr == 1:
                    order = [("c", i) for i in reversed(range(CTX // 128))] + [("l", i) for i in reversed(range(ntl))]
                for (kind, i) in order:
                    lat = kind == "l"
                    tg = i if not lat else CTX // 128 + i
                    tsl = slice(tg * 128, (tg + 1) * 128)
                    s.dma(qT4[:], QK_s[0:512, tsl].rearrange("(h p) t -> p h t", p=128), reads=["QK_s"], writes=["qT4"])
                    s.dma(kT4[:], QK_s[512:1024, tsl].rearrange("(h p) t -> p h t", p=128), reads=["QK_s"], writes=["kT4"], queue="act")
                    s.dma(ktok[:].rearrange("p h d -> p (h d)"), KV_s[tsl, 0:512], reads=["KV_s"], writes=["ktok"])
                    s.dma(vtok[:].rearrange("p h d -> p (h d)"), KV_s[tsl, 512:1024], reads=["KV_s"], writes=["vtok"], queue="act")
                    s.dma(gb[:], GB_s[tsl, :], reads=["GB_s"], writes=["gb"])
                    g = gb[:, dr * 4:dr * 4 + 4]; beta = gb[:, 8 + dr * 4:12 + dr * 4]
                    pAf = pA[:].rearrange("p a b -> p (a b)")
                    for k, L in enumerate((M1, cm[:, C_SAME, :], cm[:, C_SEL0, :], cm[:, C_SEL1, :])):
                        s.op("pe", lambda e, k=k, L=L, g=g: e.matmul(out=pAf[:, 4 * k:4 * k + 4], lhsT=L, rhs=g, start=True, stop=True), reads=["cm", "gb"], writes=[pid(0)])
                    s.op("dve", lambda e: e.tensor_copy(out=sm[:], in_=pAf[:, 0:16]), reads=[pid(0)], writes=["sm"])
                    s.op("dve", lambda e: e.tensor_tensor(out=sm[:, 4:8], in0=sm[:, 4:8], in1=sm[:, 0:4], op=ALU.subtract), reads=["sm"], writes=["sm"])
                    s.op("act", lambda e: e.activation(out=ex[:], in_=sm[:], func=AF.Exp), reads=["sm"], writes=["ex"])
                    s.op("dve", lambda e, beta=beta: e.tensor_tensor(out=beg[:], in0=ex[:, 0:4], in1=beta, op=ALU.mult), reads=["ex", "gb"], writes=["beg"])
                    s.op("dve", lambda e, g=g: e.tensor_tensor(out=G1[:], in0=bc_h(cm[:, C_SAME, :]), in1=bc_l(g), op=ALU.mult), reads=["cm", "gb"], writes=["G1"])
                    for hh in range(4):
                        s.op("pe", lambda e, hh=hh, M1=M1: e.matmul(out=pB_[:, hh, :], lhsT=M1, rhs=G1[:, hh, :], start=True, stop=False), reads=["cm", "G1"], writes=[pid(1)])
                        s.op("pe", lambda e, hh=hh, NM1=NM1: e.matmul(out=pB_[:, hh, :], lhsT=G1[:, hh, :], rhs=NM1, start=False, stop=True), reads=["cm", "G1"], writes=[pid(1)])
                    s.op("dve", lambda e, NB=NB: e.tensor_tensor(out=dl[:], in0=pB_[:], in1=bc_h(NB), op=ALU.add), reads=[pid(1), "cm"], writes=["dl"])
                    s.op("dve", lambda e, NTm=NTm: e.scalar_tensor_tensor(out=du[:], in0=pB_[:], scalar=-1.0, in1=bc_h(NTm), op0=ALU.mult, op1=ALU.add), reads=[pid(1), "cm"], writes=["du"])
                    s.op("act", lambda e: e.activation(out=dl[:], in_=dl[:], func=AF.Exp), reads=["dl"], writes=["dl"])
                    s.op("act", lambda e: e.activation(out=du[:], in_=du[:], func=AF.Exp), reads=["du"], writes=["du"])
                    for hh in range(4):
                        s.op("pe", lambda e, hh=hh: e.matmul(out=pC[:, hh, :], lhsT=kT4[:, hh, :], rhs=kT4[:, hh, :], start=True, stop=True), reads=["kT4"], writes=[pid(2)])
                    for hh in range(4):
                        s.op("pe", lambda e, hh=hh: e.matmul(out=pD[:, hh, :], lhsT=kT4[:, hh, :], rhs=qT4[:, hh, :], start=True, stop=True), reads=["kT4", "qT4"], writes=[pid(3)])
                    B0 = Bm[0]; C0 = Cm[0]; P0 = Pm[0]
                    s.op("dve", lambda e: e.tensor_tensor(out=B0[:], in0=pC[:], in1=dl[:], op=ALU.mult), reads=[pid(2), "dl"], writes=[("Bm", 0)])
                    s.op("pool", lambda e, STR=STR: e.tensor_tensor(out=B0[:], in0=B0[:], in1=bc_h(STR), op=ALU.mult), reads=[("Bm", 0), "cm"], writes=[("Bm", 0)])
                    s.op("pool", lambda e, beta=beta: e.tensor_tensor(out=B0[:], in0=B0[:], in1=bc_l(beta), op=ALU.mult), reads=[("Bm", 0), "gb"], writes=[("Bm", 0)])
                    s.op("dve", lambda e: e.tensor_tensor(out=aT[:], in0=pD[:], in1=du[:], op=ALU.mult), reads=[pid(3), "du"], writes=["aT"])
                    for hh in range(4):
                        s.op("pe", lambda e, hh=hh: e.transpose(out=pE[:, hh, :], in_=B0[:, hh, :], identity=ident), reads=[("Bm", 0), "cm"], writes=[pid(4)])
                    s.op("act", lambda e: e.copy(out=C0[:], in_=pE[:]), reads=[pid(4)], writes=[("Cm", 0)])
                    s.op("dve", lambda e: e.tensor_tensor(out=P0[:], in0=C0[:], in1=bc_h(ident), op=ALU.add), reads=[("Cm", 0), "cm"], writes=[("Pm", 0)])
                    cur = 0
                    for lv in range(1, 6):
                        nx = 1 - cur
                        Bc, Cc, Pc = Bm[cur], Cm[cur], Pm[cur]; Bn, Cn, Pn = Bm[nx], Cm[nx], Pm[nx]
                        for hh in range(4):
                            s.op("pe", lambda e, hh=hh, Bc=Bc, Cc=Cc: e.matmul(out=pF[:, hh, :], lhsT=Cc[:, hh, :], rhs=Bc[:, hh, :], start=True, stop=True), reads=[("Bm", cur), ("Cm", cur)], writes=[pid(5)])
                        s.op("act", lambda e, Bn=Bn: e.copy(out=Bn[:], in_=pF[:]), reads=[pid(5)], writes=[("Bm", nx)])
                        if lv < 5:
                            for hh in range(4):
                                s.op("pe", lambda e, hh=hh, Bc=Bc, Cc=Cc: e.matmul(out=pG[:, hh, :], lhsT=Bc[:, hh, :], rhs=Cc[:, hh, :], start=True, stop=True), reads=[("Bm", cur), ("Cm", cur)], writes=[pid(6)])
                            s.op("dve", lambda e, Cn=Cn: e.tensor_copy(out=Cn[:], in_=pG[:]), reads=[pid(6)], writes=[("Cm", nx)])
                        for hh in range(4):
                            s.op("pe", lambda e, hh=hh, Pc=Pc: e.matmul(out=pH[:, hh, :], lhsT=ident, rhs=Pc[:, hh, :], start=True, stop=False), reads=[("Pm", cur), "cm"], writes=[pid(7)])
                            s.op("pe", lambda e, hh=hh, Pc=Pc, Bn=Bn: e.matmul(out=pH[:, hh, :], lhsT=Bn[:, hh, :], rhs=Pc[:, hh, :], start=False, stop=True), reads=[("Pm", cur), ("Bm", nx)], writes=[pid(7)])
                        s.op("dve", lambda e, Pn=Pn: e.tensor_copy(out=Pn[:], in_=pH[:]), reads=[pid(7)], writes=[("Pm", nx)])
                        cur = nx
                    TT = Pm[cur]; ttid = ("Pm", cur)
                    s.op("pool", lambda e: e.tensor_tensor(out=kbg[:], in0=ktok[:], in1=bc_l(beg[:]), op=ALU.mult), reads=["ktok", "beg"], writes=["kbg"])
                    s.op("pool", lambda e, beta=beta: e.tensor_tensor(out=vb[:], in0=vtok[:], in1=bc_l(beta), op=ALU.mult), reads=["vtok", "gb"], writes=["vb"])
                    s.op("pool", lambda e: e.tensor_tensor(out=ktl[:], in0=ktok[:], in1=bc_l(ex[:, 4:8]), op=ALU.mult), reads=["ktok", "ex"], writes=["ktl"])
                    for hh in range(4):
                        s.op("pe", lambda e, hh=hh, TT=TT: e.matmul(out=pE[:, hh, :], lhsT=kbg[:, hh, :], rhs=TT[:, hh, :], start=True, stop=True), reads=["kbg", ttid], writes=[pid(4)])
                    s.op("act", lambda e: e.copy(out=WT[:], in_=pE[:]), reads=[pid(4)], writes=["WT"])
                    for hh in range(4):
                        s.op("pe", lambda e, hh=hh, TT=TT: e.matmul(out=pF[:, hh, :], lhsT=TT[:, hh, :], rhs=vb[:, hh, :], start=True, stop=True), reads=["vb", ttid], writes=[pid(5)])
                    s.op("dve", lambda e: e.tensor_copy(out=U[:], in_=pF[:]), reads=[pid(5)], writes=["U"])
                    for c in ((0, 1) if dr == 0 else (1, 0)):
                        pr = slice(64 * c, 64 * c + 64)
                        for hh in range(4):
                            s.op("pe", lambda e, hh=hh, SS=SS: e.matmul(out=pG[:, hh, :], lhsT=WT[:, hh, :], rhs=SS[:, hh, :], start=True, stop=True), reads=["WT", ssid], writes=[pid(6)])
                        s.op("dve", lambda e, pr=pr: e.tensor_tensor(out=vn[pr], in0=U[pr], in1=pG[pr], op=ALU.subtract), reads=["U", pid(6)], writes=["vn"])
                        for hh in range(4):
                            s.op("pe", lambda e, hh=hh, SS=SS: e.matmul(out=pH[:, hh, :], lhsT=qT4[:, hh, :], rhs=SS[:, hh, :], start=True, stop=True), reads=["qT4", ssid], writes=[pid(7)])
                        for hh in range(4):
                            s.op("pe", lambda e, hh=hh, pr=pr: e.matmul(out=pC[:, hh, :], lhsT=aT[pr, hh, :], rhs=vn[pr, hh, :], start=True, stop=True), reads=["aT", "vn"], writes=[pid(2)])
                        for hh in range(4):
                            s.op("pe", lambda e, hh=hh, pr=pr: e.matmul(out=pD[:, hh, :], lhsT=ktl[pr, hh, :], rhs=vn[pr, hh, :], start=True, stop=True), reads=["ktl", "vn"], writes=[pid(3)])
                        if lat:
                            s.op("dve", lambda e, pr=pr: e.tensor_tensor(out=o1[pr], in0=pH[pr], in1=bc_l(ex[:, 0:4])[pr], op=ALU.mult), reads=[pid(7), "ex"], writes=["o1"])
                            s.op("dve", lambda e, pr=pr: e.tensor_tensor(out=ot[pr], in0=o1[pr], in1=pC[pr], op=ALU.add), reads=["o1", pid(2)], writes=["ot"])
                        s.op("pool", lambda e, c=c, SS=SS: e.tensor_tensor(out=SS[:], in0=SS[:], in1=bc_l(ex[:, 8 + 4 * c:12 + 4 * c]), op=ALU.mult), reads=[ssid, "ex", pid(6), pid(7)], writes=[ssid])
                        s.op("dve", lambda e, SS=SS: e.tensor_tensor(out=SS[:], in0=SS[:], in1=pD[:], op=ALU.add), reads=[ssid, pid(3)], writes=[ssid])
                    if lat:
                        s.dma(OD_s[dr, i * 128:(i + 1) * 128, :], ot[:].rearrange("p h d -> p (h d)"), reads=["ot"], writes=["OD_s"], queue="pool")
            s.flush()
        if upto <= 3:
            return nc

        with contextlib.ExitStack() as st:
            kt = [T(st, f"kt{i}", [64, 2, 384]) for i in range(2)]
            vt = [T(st, f"vt{i}", [128, 3, 130]) for i in range(2)]
            ktc = T(st, "ktc", [64, 2, 256]); vtc = T(st, "vtc", [128, 2, 130])
            qt = [T(st, f"qt{i}", [64, 8, 128]) for i in range(2)]
            E = [T(st, f"E{i}", [128, 5, 512]) for i in range(2)]
            esink = T(st, "esink", [128, 8]); den = T(st, "den", [128, 8]); oa = [T(st, f"oa{i}", [128, 512]) for i in range(2)]
            pS = [PS(st, f"pS{i}", [128, 512]) for i in range(3)]
            pO = [PS(st, f"pO{i}", [128, 4, 65]) for i in range(2)]
            s.dma(ktc[:], KT_s[:, :, 0:CTX], reads=["KT_s"], writes=["ktc"])
            s.dma(vtc[:], V_s[0:CTX].rearrange("(b p) g d -> p b (g d)", p=128), reads=["V_s"], writes=["vtc"])
            s.dma(esink[:], sink_d.partition_broadcast(128), writes=["esink"])
            s.op("act", lambda e: e.activation(out=esink[:], in_=esink[:], func=AF.Exp), reads=["esink"], writes=["esink"])
            ntl = (2 if "small" in dbg else NT)
            nS = 0
            for i in range(ntl):
                lo = max(i - 1, 0); hi = min(i + 1, ntl - 1); nb = hi - lo + 1
                KT_ = kt[i % 2]; VT_ = vt[i % 2]; QT_ = qt[i % 2]; OA = oa[i % 2]
                s.dma(KT_[:, :, 0:nb * 128], KT_s[:, :, CTX + lo * 128:CTX + (hi + 1) * 128], reads=["KT_s"], writes=[("kt", i % 2)])
                s.dma(VT_[:, 0:nb, :], V_s[CTX + lo * 128:CTX + (hi + 1) * 128].rearrange("(b p) g d -> p b (g d)", p=128), reads=["V_s"], writes=[("vt", i % 2)], queue="act")
                s.dma(QT_[:], QT_s[:, :, i * 128:(i + 1) * 128], reads=["QT_s"], writes=[("qt", i % 2)])
                for g in range(2):
                    Eg = E[g]; eid = ("E", g)
                    kb = [("l", j - lo, (C_WPREV if j < i else (C_WNEXT if j > i else None))) for j in range(lo, hi + 1)] + [("c", 0, None), ("c", 1, None)]
                    for bi, (kk, bl, msk) in enumerate(kb):
                        P = pS[nS % 3]; psid = ("pS", nS % 3); nS += 1
                        lhs = KT_[:, g, bl * 128:(bl + 1) * 128] if kk == "l" else ktc[:, g, bl * 128:(bl + 1) * 128]
                        s.op("pe", lambda e, P=P, lhs=lhs, QT_=QT_, g=g: e.matmul(out=P[:].rearrange("p (h q) -> p h q", h=4), lhsT=lhs, rhs=QT_[:, 4 * g:4 * g + 4, :], start=True, stop=True),
                             reads=[("kt", i % 2), "ktc", ("qt", i % 2)], writes=[psid])
                        s.op("act", lambda e, P=P, Eg=Eg, bi=bi: e.activation(out=Eg[:, bi, :], in_=P[:], func=AF.Exp), reads=[psid], writes=[eid])
                        if msk is not None:
                            s.op("dve", lambda e, Eg=Eg, bi=bi, msk=msk: e.tensor_tensor(out=Eg[:, bi, :].rearrange("p (h q) -> p h q", h=4), in0=Eg[:, bi, :].rearrange("p (h q) -> p h q", h=4),
                                                                              in1=cm[:, msk, :].unsqueeze(1).to_broadcast([128, 4, 128]), op=ALU.mult), reads=[eid, "cm"], writes=[eid])
                    for hh in range(4):
                        for bi, (kk, bl, msk) in enumerate(kb):
                            rhs = VT_[:, bl, g * 65:(g + 1) * 65] if kk == "l" else vtc[:, bl, g * 65:(g + 1) * 65]
                            s.op("pe", lambda e, Eg=Eg, bi=bi, hh=hh, rhs=rhs, g=g, last=(bi == len(kb) - 1): e.matmul(out=pO[g][:, hh, :], lhsT=Eg[:, bi, hh * 128:(hh + 1) * 128], rhs=rhs, start=(bi == 0), stop=last),
                                 reads=[eid, ("vt", i % 2), "vtc"], writes=[("pO", g)])
                    s.op("dve", lambda e, g=g: e.tensor_tensor(out=den[:, 4 * g:4 * g + 4], in0=pO[g][:, :, 64], in1=esink[:, 4 * g:4 * g + 4], op=ALU.add), reads=[("pO", g), "esink"], writes=["den"])
                    s.op("dve", lambda e, g=g: e.reciprocal(out=den[:, 4 * g:4 * g + 4], in_=den[:, 4 * g:4 * g + 4]), reads=["den"], writes=["den"])
                    s.op("dve", lambda e, g=g, OA=OA: e.tensor_tensor(out=OA[:, g * 256:(g + 1) * 256].rearrange("p (h d) -> p h d", h=4), in0=pO[g][:, :, 0:64],
                                                              in1=den[:, 4 * g:4 * g + 4].unsqueeze(2).to_broadcast([128, 4, 64]), op=ALU.mult), reads=[("pO", g), "den"], writes=[("oa", i % 2)])
                s.dma(OA_s[i * 128:(i + 1) * 128, :], OA[:], reads=[("oa", i % 2)], writes=["OA_s"], queue="pool")
            s.flush()
        if upto <= 5:
            return nc

        UTr_s = nc.dram_tensor("UTr_s", [D, 16384], F32R, kind="Internal").ap()
        Vr_s = nc.dram_tensor("Vr_s", [16384, D], F32R, kind="Internal").ap()
        X1_s = scr("X1_s", [S, D])
        H2T_s = scr("H2T_s", [D, S])
        H2R_s = nc.dram_tensor("H2R_s", [D, S], F32R, kind="Internal").ap()
        SC_s = scr("SC_s", [S, 2048])
        KAP_s = scr("KAP_s", [S, 8])
        TR = lambda st, name, shape: st.enter_context(nc.sbuf_tensor(_nm(name), list(shape), F32R))
        B = lambda k: ("bk", k)
        ntl6 = (2 if "small" in dbg else NT)

        with contextlib.ExitStack() as st:
            cvb = [TR(st, "cvb", [128, 4096]) for _ in range(2)]
            wbr = T(st, "wbr", [128, 8, D]); wo = T(st, "wo", [128, 8, D])
            xa = [T(st, f"xa{b}", [128, D]) for b in range(2)]; yb = [T(st, f"yb{b}", [128, D]) for b in range(2)]
            tcA = [T(st, f"tcA{b}", [128, 8, 128]) for b in range(2)]; h2r = [TR(st, f"h2r{b}", [128, 8, 128]) for b in range(2)]
            gt = [T(st, f"gt{b}", [128, 2048]) for b in range(2)]
            od = [T(st, f"od{b}", [128, 2, 512]) for b in range(2)]; zt = [T(st, f"zt{b}", [128, 512]) for b in range(2)]
            oat = [T(st, f"oat{b}", [128, 512]) for b in range(2)]; o2 = [T(st, f"o2{b}", [128, 512]) for b in range(2)]
            qsb = [T(st, f"qsb{b}", [128, D]) for b in range(2)]
            ssq = [T(st, f"ssq{b}", [128, 4]) for b in range(2)]; ss = [T(st, f"ss{b}", [128, 1]) for b in range(2)]; rstd = [T(st, f"rstd{b}", [128, 1]) for b in range(2)]
            bk = [PS(st, f"bkA{i}", [128, 512]) for i in range(8)]
            cjobs = []
            for r0 in range(0, D, 128):
                for c0 in range(0, 16384, 4096):
                    cjobs.append((pu_d[r0:r0 + 128, c0:c0 + 4096], UTr_s[r0:r0 + 128, c0:c0 + 4096], "UTr_s"))
            for r0 in range(0, 16384, 512):
                cjobs.append((pv_d[r0:r0 + 512, :].rearrange("(p a) n -> p (a n)", p=128), Vr_s[r0:r0 + 512, :].rearrange("(p a) n -> p (a n)", p=128), "Vr_s"))
            cstate = [0]

            def conv_some(n):
                for _ in range(n):
                    if not cjobs:
                        return
                    src, dst, did = cjobs.pop(0)
                    k = cstate[0] % 2; cstate[0] += 1
                    s.dma(cvb[k][:], src, writes=[("cvb", k)], queue="pool")
                    s.dma(dst, cvb[k][:], reads=[("cvb", k)], writes=[did], queue="pool")
            s.dma(wbr[:, 0:4, :], wba_d.rearrange("(kc p) n -> p kc n", p=128), writes=["wbr"])
            s.dma(wbr[:, 4:8, :], wbd_d.rearrange("(kc p) n -> p kc n", p=128), writes=["wbr"], queue="act")
            s.dma(wo[:], wout_d.rearrange("(kc p) n -> p kc n", p=128), writes=["wo"])

            def tr8(b, src, sid, nkc, dst_off, dst, did, extra=None):
                pT = [bk[4 * b], bk[4 * b + 1]]
                for kc in range(nkc):
                    q_ = (dst_off + kc) // 4
                    s.op("pe", lambda e, kc=kc, q_=q_: e.transpose(out=pT[q_][:, ((dst_off + kc) % 4) * 128:((dst_off + kc) % 4 + 1) * 128], in_=src[:, kc * 128:(kc + 1) * 128], identity=ident), reads=[sid, "cm"], writes=[B(4 * b + q_)])
                for q_ in sorted(set((dst_off + kc) // 4 for kc in range(nkc))):
                    if q_ == 0:
                        s.op("act", lambda e, q_=q_: e.copy(out=dst[:, q_ * 4:(q_ + 1) * 4, :].rearrange("p a b -> p (a b)"), in_=pT[q_][:]), reads=[B(4 * b + q_)], writes=[(did, q_)])
                    else:
                        s.op("dve", lambda e, q_=q_: e.tensor_copy(out=dst[:, q_ * 4:(q_ + 1) * 4, :].rearrange("p a b -> p (a b)"), in_=pT[q_][:]), reads=[B(4 * b + q_)], writes=[(did, q_)])
                    if extra is not None:
                        d2, d2id = extra
                        if q_ == 0:
                            s.op("dve", lambda e, q_=q_: e.tensor_copy(out=d2[:, q_ * 4:(q_ + 1) * 4, :].rearrange("p a b -> p (a b)"), in_=pT[q_][:]), reads=[B(4 * b + q_)], writes=[(d2id, q_)])
                        else:
                            s.op("act", lambda e, q_=q_: e.copy(out=d2[:, q_ * 4:(q_ + 1) * 4, :].rearrange("p a b -> p (a b)"), in_=pT[q_][:]), reads=[B(4 * b + q_)], writes=[(d2id, q_)])

            def rms6(b, src, sid):
                s.op("act", lambda e: e.activation(out=qsb[b][:], in_=src[:], func=AF.Square, accum_out=ss[b][:]), reads=[sid], writes=[("qsb", b), ("ss", b)])
                s.op("dve", lambda e: e.tensor_scalar(out=rstd[b][:], in0=ss[b][:], scalar1=1.0 / D, scalar2=1e-6, op0=ALU.mult, op1=ALU.add), reads=[("ss", b)], writes=[("rstd", b)])
                s.op("act", lambda e: e.sqrt(out=rstd[b][:], in_=rstd[b][:]), reads=[("rstd", b)], writes=[("rstd", b)])
                s.op("dve", lambda e: e.reciprocal(out=rstd[b][:], in_=rstd[b][:]), reads=[("rstd", b)], writes=[("rstd", b)])

            for i in range(ntl6):
                conv_some(4)
                b = i % 2
                tsl = slice(i * 128, (i + 1) * 128)
                XA = xa[b]; YB = yb[b]; GT = gt[b]; OD = od[b]; ZT = zt[b]; OAT = oat[b]; O2 = o2[b]; QSB = qsb[b]; SSQ = ssq[b]; TC = tcA[b]
                pY = [bk[4 * b + 2], bk[4 * b + 3]]
                s.dma(XA[:], x_d[tsl, :], writes=[("xa", b)])
                s.dma(OD[:, 0, :], OD_s[0, tsl, :], reads=["OD_s"], writes=[("od", b)], queue="act")
                s.dma(OD[:, 1, :], OD_s[1, tsl, :], reads=["OD_s"], writes=[("od", b)], queue="act")
                s.dma(ZT[:], Z_s[tsl, :], reads=["Z_s"], writes=[("zt", b)])
                s.dma(OAT[:], OA_s[tsl, :], reads=["OA_s"], writes=[("oat", b)], queue="act")
                s.dma(GT[:], GT_s[tsl, :], reads=["GT_s"], writes=[("gt", b)])
                s.op("dve", lambda e, OD=OD: e.tensor_tensor(out=OD[:, 0, :], in0=OD[:, 0, :], in1=OD[:, 1, :], op=ALU.add), reads=[("od", b)], writes=[("od", b)])
                s.op("pool", lambda e, OD=OD, O2=O2: e.tensor_tensor(out=O2[:], in0=OD[:, 0, :], in1=OD[:, 0, :], op=ALU.mult), reads=[("od", b)], writes=[("o2", b)])
                s.op("dve", lambda e, O2=O2, SSQ=SSQ: e.tensor_reduce(out=SSQ[:], in_=O2[:].rearrange("p (h d) -> p h d", h=4), axis=AX.X, op=ALU.add), reads=[("o2", b)], writes=[("ssq", b)])
                s.op("dve", lambda e, SSQ=SSQ: e.tensor_scalar(out=SSQ[:], in0=SSQ[:], scalar1=1.0 / 128, scalar2=1e-6, op0=ALU.mult, op1=ALU.add), reads=[("ssq", b)], writes=[("ssq", b)])
                s.op("act", lambda e, SSQ=SSQ: e.sqrt(out=SSQ[:], in_=SSQ[:]), reads=[("ssq", b)], writes=[("ssq", b)])
                s.op("dve", lambda e, SSQ=SSQ: e.reciprocal(out=SSQ[:], in_=SSQ[:]), reads=[("ssq", b)], writes=[("ssq", b)])
                s.op("dve", lambda e, O2=O2, OD=OD, SSQ=SSQ: e.tensor_tensor(out=O2[:].rearrange("p (h d) -> p h d", h=4), in0=OD[:, 0, :].rearrange("p (h d) -> p h d", h=4), in1=SSQ[:].unsqueeze(2).to_broadcast([128, 4, 128]), op=ALU.mult), reads=[("od", b), ("ssq", b)], writes=[("o2", b)])
                s.op("pool", lambda e, O2=O2, ZT=ZT: e.tensor_tensor(out=O2[:], in0=O2[:], in1=ZT[:], op=ALU.mult), reads=[("o2", b), ("zt", b)], writes=[("o2", b)])
                tr8(b, OAT, ("oat", b), 4, 0, TC, ("tcA", b))
                tr8(b, O2, ("o2", b), 4, 4, TC, ("tcA", b))
                for half in range(2):
                    for kc in range(4):
                        s.op("pe", lambda e, half=half, kc=kc, pY=pY, TC=TC: e.matmul(out=pY[half][:], lhsT=TC[:, kc, :], rhs=wbr[:, kc, half * 512:(half + 1) * 512], start=(kc == 0), stop=(kc == 3)), reads=[(("tcA", b), 0), "wbr"], writes=[B(4 * b + 2 + half)])
                    s.op("dve", lambda e, half=half, pY=pY, YB=YB, GT=GT: e.tensor_tensor(out=YB[:, half * 512:(half + 1) * 512], in0=pY[half][:], in1=GT[:, half * 512:(half + 1) * 512], op=ALU.mult), reads=[B(4 * b + 2 + half), ("gt", b)], writes=[("yb", b)])
                for half in range(2):
                    for kc in range(4):
                        s.op("pe", lambda e, half=half, kc=kc, pY=pY, TC=TC: e.matmul(out=pY[half][:], lhsT=TC[:, 4 + kc, :], rhs=wbr[:, 4 + kc, half * 512:(half + 1) * 512], start=(kc == 0), stop=(kc == 3)), reads=[(("tcA", b), 1), "wbr"], writes=[B(4 * b + 2 + half)])
                    s.op("dve", lambda e, half=half, pY=pY, QSB=QSB, GT=GT: e.tensor_tensor(out=QSB[:, half * 512:(half + 1) * 512], in0=pY[half][:], in1=GT[:, 1024 + half * 512:1024 + (half + 1) * 512], op=ALU.mult), reads=[B(4 * b + 2 + half), ("gt", b)], writes=[("qsb", b)])
                s.op("pool", lambda e, YB=YB, QSB=QSB: e.tensor_tensor(out=YB[:], in0=YB[:], in1=QSB[:], op=ALU.add), reads=[("yb", b), ("qsb", b)], writes=[("yb", b)])
                tr8(b, YB, ("yb", b), 8, 0, TC, ("tcA", b))
                for half in range(2):
                    for kc in range(8):
                        s.op("pe", lambda e, half=half, kc=kc, pY=pY, TC=TC: e.matmul(out=pY[half][:], lhsT=TC[:, kc, :], rhs=wo[:, kc, half * 512:(half + 1) * 512], start=(kc == 0), stop=(kc == 7)), reads=[(("tcA", b), 0), (("tcA", b), 1), "wo"], writes=[B(4 * b + 2 + half)])
                    s.op("dve", lambda e, half=half, pY=pY, YB=YB: e.tensor_tensor(out=YB[:, half * 512:(half + 1) * 512], in0=pY[half][:], in1=bvv(BV_GT1)[:, half * 512:(half + 1) * 512], op=ALU.mult), reads=[B(4 * b + 2 + half), "bv"], writes=[("yb", b)])
                s.op("pool", lambda e, XA=XA, YB=YB: e.tensor_tensor(out=XA[:], in0=XA[:], in1=YB[:], op=ALU.add), reads=[("xa", b), ("yb", b)], writes=[("xa", b)])
                s.dma(X1_s[tsl, :], XA[:], reads=[("xa", b)], writes=["X1_s"], queue="act")
                rms6(b, XA, ("xa", b))
                s.op("dve", lambda e, XA=XA, YB=YB, RS=rstd[b]: e.scalar_tensor_tensor(out=YB[:], in0=XA[:], scalar=RS[:, 0:1], in1=bvv(BV_G2), op0=ALU.mult, op1=ALU.mult), reads=[("xa", b), ("rstd", b), "bv"], writes=[("yb", b)])
                s.op("pool", lambda e, YB=YB: e.tensor_tensor(out=YB[:], in0=YB[:], in1=bvv(BV_SH2), op=ALU.add), reads=[("yb", b), "bv"], writes=[("yb", b)])
                tr8(b, YB, ("yb", b), 8, 0, TC, ("tcA", b), extra=(h2r[b], ("h2r", b)))
                s.dma(H2T_s[:, tsl].rearrange("(kc p) t -> p kc t", p=128), TC[:], reads=[(("tcA", b), 0), (("tcA", b), 1)], writes=["H2T_s"])
                s.dma(H2R_s[:, tsl].rearrange("(kc p) t -> p kc t", p=128), h2r[b][:], reads=[(("h2r", b), 0), (("h2r", b), 1)], writes=["H2R_s"], queue="act")
            conv_some(10 ** 6)
            s.flush()
        if upto <= 6:
            return nc

        with contextlib.ExitStack() as st:
            wq = T(st, "wq", [128, 8, D]); keys2 = T(st, "keys2", [128, 8, 128])
            tcB = [T(st, f"tcB{b}", [128, 8, 128]) for b in range(2)]
            qsb = [T(st, f"qsbB{b}", [128, D]) for b in range(2)]; qTs = [T(st, f"qTs{b}", [128, 8, 128]) for b in range(2)]
            sc = [T(st, f"scB{b}", [128, 16, 128]) for b in range(2)]
            t16 = [T(st, f"t16{b}", [128, 8, 2, 16]) for b in range(2)]; c16 = [T(st, f"c16{b}", [128, 8, 16]) for b in range(2)]
            wk4 = T(st, "wk4", [128, 4, 256]); cand4 = T(st, "cand4", [128, 4, 256])
            thr = [T(st, f"thr{b}", [128, 8]) for b in range(2)]; negm = [T(st, f"negm{b}", [128, 8]) for b in range(2)]; Zs = [T(st, f"Zs{b}", [128, 8]) for b in range(2)]
            kap = [T(st, f"kap{b}", [128, 8]) for b in range(2)]; m1 = [T(st, f"m1{b}", [128, 8]) for b in range(2)]; th2 = [T(st, f"th2{b}", [128, 8]) for b in range(2)]
            e16 = [T(st, f"e16{b}", [128, 8, 16]) for b in range(2)]
            bk = [PS(st, f"bkB{i}", [128, 512]) for i in range(8)]
            s.dma(wq[:], pwq_d.rearrange("(kc p) n -> p kc n", p=128), writes=["wq"])
            s.dma(keys2[:], pkeys_d, writes=["keys2"])
            _cb = [int(x[4:]) for x in dbg if x.startswith("cutB")]
            cutB = _cb[0] if _cb else 99
            for i in range(ntl6):
                b = i % 2
                tsl = slice(i * 128, (i + 1) * 128)
                TC = tcB[b]; QSB = qsb[b]; QT = qTs[b]; SC = sc[b]; T16 = t16[b]; C16 = c16[b]
                pT = [bk[4 * b], bk[4 * b + 1]]; pY = [bk[4 * b + 2], bk[4 * b + 3]]
                s.dma(TC[:], H2T_s[:, tsl].rearrange("(kc p) t -> p kc t", p=128), reads=["H2T_s"], writes=[("tcB", b)], queue=("sp", "act")[b])
                for half in range(2):
                    for kc in range(8):
                        s.op("pe", lambda e, half=half, kc=kc, pY=pY, TC=TC: e.matmul(out=pY[half][:], lhsT=TC[:, kc, :], rhs=wq[:, kc, half * 512:(half + 1) * 512], start=(kc == 0), stop=(kc == 7)), reads=[("tcB", b), "wq"], writes=[B(4 * b + 2 + half)])
                    if half == 0:
                        s.op("act", lambda e, pY=pY, QSB=QSB: e.copy(out=QSB[:, 0:512], in_=pY[0][:]), reads=[B(4 * b + 2)], writes=[("qsbB", b)])
                    else:
                        s.op("dve", lambda e, pY=pY, QSB=QSB: e.tensor_copy(out=QSB[:, 512:1024], in_=pY[1][:]), reads=[B(4 * b + 3)], writes=[("qsbB", b)])
                if cutB < 2:
                    continue
                for hh in range(8):
                    s.op("pe", lambda e, hh=hh, pT=pT, QSB=QSB: e.transpose(out=pT[hh // 4][:, (hh % 4) * 128:(hh % 4 + 1) * 128], in_=QSB[:, hh * 128:(hh + 1) * 128], identity=ident), reads=[("qsbB", b), "cm"], writes=[B(4 * b + hh // 4)])
                s.op("act", lambda e, pT=pT, QT=QT: e.copy(out=QT[:, 0:4, :].rearrange("p a b -> p (a b)"), in_=pT[0][:]), reads=[B(4 * b)], writes=[("qTs", b)])
                s.op("dve", lambda e, pT=pT, QT=QT: e.tensor_copy(out=QT[:, 4:8, :].rearrange("p a b -> p (a b)"), in_=pT[1][:]), reads=[B(4 * b + 1)], writes=[("qTs", b)])
                if cutB < 3:
                    continue
                banks = [pY[0], pY[1], pT[0], pT[1]]; bids = [B(4 * b + 2), B(4 * b + 3), B(4 * b), B(4 * b + 1)]
                for p in range(2):
                    for hh in range(8):
                        bi_ = 2 * p + hh // 4
                        s.op("pe", lambda e, hh=hh, p=p, bi_=bi_, QT=QT, banks=banks: e.matmul(out=banks[bi_][:, (hh % 4) * 128:(hh % 4 + 1) * 128], lhsT=QT[64 * p:64 * p + 64, hh, :], rhs=keys2[64 * p:64 * p + 64, hh, :], start=True, stop=True), reads=[("qTs", b), "keys2"], writes=[bids[bi_]])
                SC4 = SC[:].rearrange("p (h q) k -> p h q k", q=2)
                for bi_ in range(4):
                    p, hq = bi_ // 2, bi_ % 2
                    dst = SC4[:, hq * 4:(hq + 1) * 4, p, :]
                    src = banks[bi_][:].rearrange("p (a k) -> p a k", a=4)
                    allq = [("scB", b, q4) for q4 in range(4)]
                    if bi_ % 2 == 0:
                        s.op("act", lambda e, dst=dst, src=src: e.copy(out=dst, in_=src), reads=[bids[bi_]], writes=[("scB", b, 2 * hq), ("scB", b, 2 * hq + 1)])
                    else:
                        s.op("dve", lambda e, dst=dst, src=src: e.tensor_copy(out=dst, in_=src), reads=[bids[bi_]], writes=[("scB", b, 2 * hq), ("scB", b, 2 * hq + 1)])
                if cutB < 4:
                    continue
                for hb in range(2):
                    hs = range(hb * 4, hb * 4 + 4)
                    scid = lambda hh: ("scB", b, hh // 2)
                    for hh in hs:
                        for p in range(2):
                            s.op("dve", lambda e, hh=hh, p=p, SC=SC, T16=T16: e.max(out=T16[:, hh, p, 0:8], in_=SC[:, 2 * hh + p, :]), reads=[scid(hh)], writes=[("t16", b, hh, p)])
                    for hh in hs:
                        for p in range(2):
                            s.op("dve", lambda e, hh=hh, p=p, SC=SC, T16=T16: e.match_replace(out=wk4[:, hh % 4, p * 128:(p + 1) * 128], in_to_replace=T16[:, hh, p, 0:8], in_values=SC[:, 2 * hh + p, :], imm_value=-1e30), reads=[scid(hh), ("t16", b, hh, p)], writes=[("wk4", hh % 4, p)])
                    for hh in hs:
                        for p in range(2):
                            s.op("dve", lambda e, hh=hh, p=p, T16=T16: e.max(out=T16[:, hh, p, 8:16], in_=wk4[:, hh % 4, p * 128:(p + 1) * 128]), reads=[("wk4", hh % 4, p)], writes=[("t16", b, hh, p)])
                    for hh in hs:
                        s.op("dve", lambda e, hh=hh, T16=T16: e.tensor_tensor(out=cand4[:, hh % 4, :].rearrange("p (a c) -> p a c", a=16), in0=T16[:, hh, 0, :].unsqueeze(2).to_broadcast([128, 16, 16]), in1=T16[:, hh, 1, :].unsqueeze(1).to_broadcast([128, 16, 16]), op=ALU.add), reads=[("t16", b, hh, 0), ("t16", b, hh, 1)], writes=[("cand4", hh % 4)])
                    for hh in hs:
                        s.op("dve", lambda e, hh=hh, C16=C16: e.max(out=C16[:, hh, 0:8], in_=cand4[:, hh % 4, :]), reads=[("cand4", hh % 4)], writes=[("c16", b, hh)])
                    for hh in hs:
                        s.op("dve", lambda e, hh=hh, C16=C16: e.match_replace(out=wk4[:, hh % 4, :], in_to_replace=C16[:, hh, 0:8], in_values=cand4[:, hh % 4, :], imm_value=-1e30), reads=[("cand4", hh % 4), ("c16", b, hh)], writes=[("wk4", hh % 4, 0), ("wk4", hh % 4, 1)])
                    for hh in hs:
                        s.op("dve", lambda e, hh=hh, C16=C16: e.max(out=C16[:, hh, 8:16], in_=wk4[:, hh % 4, :]), reads=[("wk4", hh % 4, 0), ("wk4", hh % 4, 1)], writes=[("c16", b, hh)])
                if cutB < 5:
                    continue
                allc = [("c16", b, hh) for hh in range(8)]; allt = [("t16", b, hh, 0) for hh in range(8)]
                THR = thr[b]; NEGM = negm[b]; M1 = m1[b]; ZS = Zs[b]; KAP = kap[b]; TH2 = th2[b]; E16 = e16[b]
                s.op("dve", lambda e, C16=C16, THR=THR: e.tensor_scalar(out=THR[:], in0=C16[:, :, 15], scalar1=-1e-4, scalar2=None, op0=ALU.add), reads=allc, writes=[("thr", b)])
                s.op("dve", lambda e, C16=C16, NEGM=NEGM: e.tensor_scalar(out=NEGM[:], in0=C16[:, :, 0], scalar1=-1.0, scalar2=None, op0=ALU.mult), reads=allc, writes=[("negm", b)])
                s.op("dve", lambda e, T16=T16, M1=M1: e.tensor_copy(out=M1[:], in_=T16[:, :, 0, 0]), reads=allt, writes=[("m1", b)])
                s.op("dve", lambda e, C16=C16, NEGM=NEGM, E16=E16: e.tensor_tensor(out=E16[:], in0=C16[:], in1=NEGM[:].unsqueeze(2).to_broadcast([128, 8, 16]), op=ALU.add), reads=allc + [("negm", b)], writes=[("e16", b)])
                s.op("act", lambda e, E16=E16: e.activation(out=E16[:], in_=E16[:], func=AF.Exp), reads=[("e16", b)], writes=[("e16", b)])
                s.op("dve", lambda e, E16=E16, ZS=ZS: e.tensor_reduce(out=ZS[:], in_=E16[:], axis=AX.X, op=ALU.add), reads=[("e16", b)], writes=[("Zs", b)])
                s.op("dve", lambda e, KAP=KAP, THR=THR, NEGM=NEGM: e.tensor_tensor(out=KAP[:], in0=THR[:], in1=NEGM[:], op=ALU.add), reads=[("thr", b), ("negm", b)], writes=[("kap", b)])
                s.op("act", lambda e, KAP=KAP: e.activation(out=KAP[:], in_=KAP[:], func=AF.Exp), reads=[("kap", b)], writes=[("kap", b)])
                s.op("dve", lambda e, ZS=ZS: e.reciprocal(out=ZS[:], in_=ZS[:]), reads=[("Zs", b)], writes=[("Zs", b)])
                s.op("dve", lambda e, KAP=KAP, ZS=ZS: e.tensor_tensor(out=KAP[:], in0=KAP[:], in1=ZS[:], op=ALU.mult), reads=[("kap", b), ("Zs", b)], writes=[("kap", b)])
                s.op("dve", lambda e, TH2=TH2, THR=THR, M1=M1: e.tensor_tensor(out=TH2[:], in0=THR[:], in1=M1[:], op=ALU.subtract), reads=[("thr", b), ("m1", b)], writes=[("th2", b)])
                sc4 = SC[:].rearrange("p (h q) k -> p h q k", q=2)
                allsc = [("scB", b, q4) for q4 in range(4)]
                s.op("dve", lambda e, sc4=sc4, M1=M1: e.tensor_tensor(out=sc4[:, :, 0, :], in0=sc4[:, :, 0, :], in1=M1[:].unsqueeze(2).to_broadcast([128, 8, 128]), op=ALU.subtract), reads=allsc + [("m1", b)], writes=allsc)
                s.op("pool", lambda e, sc4=sc4, TH2=TH2: e.tensor_tensor(out=sc4[:, :, 1, :], in0=sc4[:, :, 1, :], in1=TH2[:].unsqueeze(2).to_broadcast([128, 8, 128]), op=ALU.subtract), reads=allsc + [("th2", b)], writes=allsc)
                s.op("act", lambda e, SC=SC: e.activation(out=SC[:], in_=SC[:], func=AF.Exp), reads=allsc, writes=allsc)
                s.dma(SC_s[tsl, :], SC[:].rearrange("p a b -> p (a b)"), reads=allsc, writes=["SC_s"], queue="pool")
                s.dma(KAP_s[tsl, :], KAP[:], reads=[("kap", b)], writes=["KAP_s"], queue="pool")
            s.flush()
        if upto <= 7:
            return nc

        GI = 2
        NG = 128 // GI
        NB = 2
        with contextlib.ExitStack() as st:
            xa = [T(st, f"xc{t}", [128, D]) for t in range(NB)]; yb = T(st, "ybc", [128, D]); qsb = T(st, "qsbc", [128, D])
            ss = T(st, "ssc", [128, 1]); rstd = T(st, "rstdc", [128, 1])
            h2r = [[TR(st, f"h2c{k}{t}", [128, 8, 128]) for t in range(NB)] for k in range(2)]
            sc = [[T(st, f"scc{k}{t}", [128, 16, 128]) for t in range(NB)] for k in range(2)]
            kap = [[T(st, f"kapc{k}{t}", [128, 8]) for t in range(NB)] for k in range(2)]
            dg = [[TR(st, f"dgc{k}{t}", [128, 8, 128]) for t in range(NB)] for k in range(2)]
            UT = [TR(st, f"UT{i}", [128, 8, GI * 128]) for i in range(2)]
            VG = [TR(st, f"VG{i}", [128, GI, D]) for i in range(2)]
            pe_ = [T(st, f"pec{i}", [128, NB, 8, GI * 128]) for i in range(2)]
            _Mr = TR(st, "Mr", [128, NB, 8, GI * 128])
            W5 = NB * GI * 128
            g1 = [T(st, f"g1c{i}", [128, W5]) for i in range(2)]; Pm_ = [T(st, f"Pmc{i}", [128, W5]) for i in range(2)]
            PT = [TR(st, f"PT{i}", [128, NB * GI, 128]) for i in range(2)]
            bk = [PS(st, f"bkC{i}", [128, 512]) for i in range(8)]
            nblk = ntl6 // NB
            ngr = (2 if "small2" in dbg else NG)
            gcount = 0

            def blk_load(blk):
                k = blk % 2
                for tau in range(NB):
                    i = blk * NB + tau
                    tsl = slice(i * 128, (i + 1) * 128)
                    s.dma(h2r[k][tau][:], H2R_s[:, tsl].rearrange("(kc p) t -> p kc t", p=128), reads=["H2R_s"], writes=[("h2c", k, tau)], queue="pool")
                    s.dma(sc[k][tau][:].rearrange("p a b -> p (a b)"), SC_s[tsl, :], reads=["SC_s"], writes=[("scc", k, tau)], queue="pool")
                    s.dma(kap[k][tau][:], KAP_s[tsl, :], reads=["KAP_s"], writes=[("kapc", k, tau)], queue="pool")
                    for hh in range(8):
                        s.op("dve", lambda e, hh=hh, tau=tau, k=k: e.tensor_scalar(out=dg[k][tau][:, hh, :], in0=ident, scalar1=kap[k][tau][:, hh:hh + 1], scalar2=None, op0=ALU.mult), reads=["cm", ("kapc", k, tau)], writes=[("dgc", k, tau)])

            blk_load(0)
            for blk in range(nblk):
              kb = blk % 2
              pU = [[bk[4 + 2 * t + hf] for hf in range(2)] for t in range(NB)]
              ub = [None] * (ngr + 1)

              def g_load(g):
                  nonlocal gcount
                  u = gcount % 2; gcount += 1
                  ub[g] = u
                  e0 = g * GI * 128
                  s.dma(UT[u][:], UTr_s[:, e0:e0 + GI * 128].rearrange("(kc p) n -> p kc n", p=128), reads=["UTr_s"], writes=[("UT", u)], queue=("sp", "act")[u])
                  s.dma(VG[u][:], Vr_s[e0:e0 + GI * 128, :].rearrange("(a p) n -> p a n", p=128), reads=["Vr_s"], writes=[("VG", u)], queue=("act", "sp")[u])

              def g_prod(g):
                  u = ub[g]
                  for tau in range(NB):
                      sc4 = sc[kb][tau][:].rearrange("p (h q) k -> p h q k", q=2)
                      e1b = sc4[:, :, 0, g * GI:(g + 1) * GI].unsqueeze(3).to_broadcast([128, 8, GI, 128])
                      e2b = sc4[:, :, 1, :].unsqueeze(2).to_broadcast([128, 8, GI, 128])
                      s.op("dve", lambda e, e1b=e1b, e2b=e2b, tau=tau, u=u: e.tensor_tensor(out=pe_[u][:, tau, :, :].rearrange("p h (a k) -> p h a k", a=GI), in0=e1b, in1=e2b, op=ALU.mult), reads=[("scc", kb, tau)], writes=[("pe", u, tau)])

              def g_act(g):
                  u = ub[g]; pR = bk[u]
                  for tau in range(NB):
                      for kc in range(8):
                          s.op("pe", lambda e, kc=kc, u=u, tau=tau, pR=pR, H=h2r[kb][tau]: e.matmul(out=pR[:, tau * GI * 128:(tau + 1) * GI * 128], lhsT=H[:, kc, :], rhs=UT[u][:, kc, :], start=(kc == 0), stop=(kc == 7)), reads=[("h2c", kb, tau), ("UT", u)], writes=[B(u)])

              def g_gelu(g):
                  u = ub[g]; pR = bk[u]
                  s.op("act", lambda e, pR=pR, u=u: e.activation(out=g1[u][:], in_=pR[:, 0:W5], func=AF.Gelu_apprx_tanh), reads=[B(u)], writes=[("g1", u)])

              def g_mask(g):
                  u = ub[g]
                  for tau in range(NB):
                      s.op("dve", lambda e, u=u, tau=tau: e.scalar_tensor_tensor(out=_Mr[:, tau], in0=pe_[u][:, tau], scalar=1.0, in1=pe_[u][:, tau], op0=ALU.is_ge, op1=ALU.mult), reads=[("pe", u, tau)], writes=[("Mr", tau)])

              def g_gd(g):
                  u = ub[g]; pG = bk[2 + u]
                  for tau in range(NB):
                      for hh in range(8):
                          s.op("pe", lambda e, hh=hh, tau=tau, pG=pG, DG=dg[kb][tau]: e.matmul(out=pG[:, tau * GI * 128:(tau + 1) * GI * 128], lhsT=DG[:, hh, :], rhs=_Mr[:, tau, hh, :], start=(hh == 0), stop=(hh == 7)), reads=[("dgc", kb, tau), ("Mr", tau)], writes=[B(2 + u)])

              def g_pm(g):
                  u = ub[g]; pG = bk[2 + u]
                  s.op("dve", lambda e, u=u, pG=pG: e.tensor_tensor(out=Pm_[u][:], in0=g1[u][:], in1=pG[:, 0:W5], op=ALU.mult), reads=[("g1", u), B(2 + u)], writes=[("Pm", u)])

              def g_tr(g):
                  u = ub[g]; pW = bk[2 + u]
                  for k in range(NB * GI):
                      s.op("pe", lambda e, k=k, u=u, pW=pW: e.transpose(out=pW[:, k * 128:(k + 1) * 128], in_=Pm_[u][:, k * 128:(k + 1) * 128], identity=ident), reads=[("Pm", u), "cm"], writes=[B(2 + u)])
                  s.op("act", lambda e, u=u, pW=pW: e.copy(out=PT[u][:].rearrange("p a b -> p (a b)"), in_=pW[:, 0:W5]), reads=[B(2 + u)], writes=[("PT", u)])

              def g_out(g):
                  u = ub[g]
                  for tau in range(NB):
                      for a in range(GI):
                          for half in range(2):
                              s.op("pe", lambda e, a=a, half=half, u=u, tau=tau, first=(g == 0 and a == 0), last=(g == ngr - 1 and a == GI - 1): e.matmul(out=pU[tau][half][:], lhsT=PT[u][:, tau * GI + a, :], rhs=VG[u][:, a, half * 512:(half + 1) * 512], start=first, stop=last),
                                   reads=[("PT", u), ("VG", u)], writes=[B(4 + 2 * tau + half)])

              g_load(0); g_prod(0); g_act(0); g_gelu(0); g_mask(0)
              for g in range(ngr):
                  nxt = g + 1 < ngr
                  if nxt:
                      g_load(g + 1); g_prod(g + 1)
                  g_gd(g)
                  if nxt:
                      g_act(g + 1); g_gelu(g + 1)
                  g_pm(g)
                  if nxt:
                      g_mask(g + 1)
                  g_tr(g)
                  g_out(g)
                  if g == 4 and blk + 1 < nblk:
                      blk_load(blk + 1)
              for tau in range(NB):
                i = blk * NB + tau
                XA = xa[tau]; xid = ("xc", tau)
                s.dma(XA[:], X1_s[i * 128:(i + 1) * 128, :], reads=["X1_s"], writes=[xid], queue="pool")
                for half in range(2):
                    s.op("dve", lambda e, half=half, tau=tau: e.tensor_tensor(out=yb[:, half * 512:(half + 1) * 512], in0=pU[tau][half][:], in1=bvv(BV_GT2)[:, half * 512:(half + 1) * 512], op=ALU.mult), reads=[B(4 + 2 * tau + half), "bv"], writes=["ybc"])
                s.op("pool", lambda e, XA=XA: e.tensor_tensor(out=XA[:], in0=XA[:], in1=yb[:], op=ALU.add), reads=[xid, "ybc"], writes=[xid])
                s.op("act", lambda e, XA=XA: e.activation(out=qsb[:], in_=XA[:], func=AF.Square, accum_out=ss[:]), reads=[xid], writes=["qsbc", "ssc"])
                s.op("dve", lambda e: e.tensor_scalar(out=rstd[:], in0=ss[:], scalar1=1.0 / D, scalar2=1e-6, op0=ALU.mult, op1=ALU.add), reads=["ssc"], writes=["rstdc"])
                s.op("act", lambda e: e.sqrt(out=rstd[:], in_=rstd[:]), reads=["rstdc"], writes=["rstdc"])
                s.op("dve", lambda e: e.reciprocal(out=rstd[:], in_=rstd[:]), reads=["rstdc"], writes=["rstdc"])
                s.op("dve", lambda e, XA=XA: e.scalar_tensor_tensor(out=yb[:], in0=XA[:], scalar=rstd[:, 0:1], in1=bvv(BV_FN), op0=ALU.mult, op1=ALU.mult), reads=[xid, "rstdc", "bv"], writes=["ybc"])
                s.dma(out_d[i * 128:(i + 1) * 128, :], yb[:], reads=["ybc"], writes=["out"], queue="pool")
            s.flush()
        return nc


def _rope(s, src, dst, tmp, R, rid, H, sid, did):
    sv = src.rearrange("p (h a b c) -> p h a b c", h=H, a=2, b=2)
    dv = dst.rearrange("p (h a b c) -> p h a b c", h=H, a=2, b=2)
    tv = tmp[:, 0:H * 32].rearrange("p (h a c) -> p h a c", h=H, a=2)
    rv = R[:].rearrange("p (a b c) -> p a b c", a=2, b=2)
    cosb = rv[:, :, 0, :].unsqueeze(1).to_broadcast([128, H, 2, 16])
    sinb = rv[:, :, 1, :].unsqueeze(1).to_broadcast([128, H, 2, 16])
    x1 = sv[:, :, :, 0, :]; x2 = sv[:, :, :, 1, :]
    o1 = dv[:, :, :, 0, :]; o2 = dv[:, :, :, 1, :]
    s.op("dve", lambda e: e.tensor_tensor(out=o1, in0=x1, in1=cosb, op=ALU.mult), reads=[sid, rid], writes=[did])
    s.op("dve", lambda e: e.tensor_tensor(out=tv, in0=x2, in1=sinb, op=ALU.mult), reads=[sid, rid], writes=["tmp"])
    s.op("dve", lambda e: e.tensor_tensor(out=o1, in0=o1, in1=tv, op=ALU.subtract), reads=[did, "tmp"], writes=[did])
    s.op("dve", lambda e: e.tensor_tensor(out=o2, in0=x1, in1=sinb, op=ALU.mult), reads=[sid, rid, did], writes=[did])
    s.op("dve", lambda e: e.tensor_tensor(out=tv, in0=x2, in1=cosb, op=ALU.mult), reads=[sid, rid, did], writes=["tmp"])
    s.op("dve", lambda e: e.tensor_tensor(out=o2, in0=o2, in1=tv, op=ALU.add), reads=[did, "tmp"], writes=[did])


def _host_inputs(inputs, b, consts):
    g = lambda k: np.ascontiguousarray(inputs[k], dtype=np.float32)
    m = {
        "x": g("x")[b], "c": g("c")[b], "ctx": g("ctx")[b], "c_ctx": g("c_ctx"),
        "w_ada": g("w_ada")[0], "b_ada": g("b_ada")[0], "norm_mix": g("norm_mix")[0], "norm_ffn": g("norm_ffn")[0],
        "w_in": g("w_in")[0], "b_gate": g("b_gate")[0], "attn_sink": g("attn_sink")[0], "dn_conv": g("dn_conv")[0],
        "dn_a_log_f": g("dn_a_log_f")[0], "dn_dt_bias_f": g("dn_dt_bias_f")[0], "dn_a_log_b": g("dn_a_log_b")[0], "dn_dt_bias_b": g("dn_dt_bias_b")[0],
        "dn_norm": g("dn_norm")[0], "w_br_attn": g("w_br_attn")[0], "w_br_dn": g("w_br_dn")[0], "w_out": g("w_out")[0],
        "peer_wq": g("peer_wq")[0], "final_norm": g("final_norm"),
    }
    m.update(consts)
    return {k: np.ascontiguousarray(v) for k, v in m.items()}


_SHARED = {}


def kernel(**inputs):
    consts = _consts()
    nc = build()
    keysT = np.ascontiguousarray(np.transpose(np.asarray(inputs["peer_keys"], np.float32)[0], (1, 3, 0, 2)).reshape(128, 8, 128))
    uT = np.ascontiguousarray(np.asarray(inputs["peer_u"], np.float32)[0].T)
    pv = np.ascontiguousarray(np.asarray(inputs["peer_v"], np.float32)[0])
    in_maps = []
    for b in range(8):
        m = _host_inputs(inputs, b, consts)
        m["peer_keysT"] = keysT; m["peer_uT"] = uT; m["peer_v"] = pv
        in_maps.append(m)
    res = run_bass_kernel_spmd(nc, in_maps, core_ids=list(range(8)))
    return np.stack([np.asarray(r["out"], dtype=np.float32) for r in res.results], axis=0)
```

```python
import contextlib
import numpy as np
import concourse.bass as bass
import concourse.mybir as mybir
from concourse.bass_utils import run_bass_kernel_spmd

F32 = mybir.dt.float32
F32R = mybir.dt.float32r
ALU = mybir.AluOpType
AF = mybir.ActivationFunctionType
AX = mybir.AxisListType

D = 1024
S = 8192
CTX = 256
TALL = CTX + S
NT = S // 128
IN_COLS = 4880
NEG = -30000.0


class _Ins:
    __slots__ = ("eng", "fn", "deps", "signal", "sig_no", "dma", "idx")

    def __init__(self, eng, fn, dma=None):
        self.eng = eng
        self.fn = fn
        self.deps = []
        self.signal = False
        self.sig_no = None
        self.dma = dma
        self.idx = None


class Sch:
    EPOCH = 20000
    NDMA = 24
    NEP = 16

    def __init__(self, nc, st):
        self.nc = nc
        self.engs = ("pe", "act", "dve", "pool", "sp")
        self.nep = {"pe": 12, "act": 4, "dve": 6, "pool": 3, "sp": 1}
        self.sems = {e: [st.enter_context(nc.semaphore(f"s_{e}_{i}")) for i in range(self.nep[e])] for e in self.engs}
        self.dsems = [st.enter_context(nc.semaphore(f"s_dma_{i}")) for i in range(self.NDMA)]
        self.sigc = {e: 0 for e in self.engs}
        self.dma_rr = 0
        self.dma_cnt = [0] * self.NDMA
        self.dma_last = [None] * self.NDMA
        self._reset()

    def _reset(self):
        self.q = {e: [] for e in self.engs}
        self.lastw = {}
        self.readers = {}

    def _add(self, ins, reads, writes):
        q = self.q[ins.eng]
        ins.idx = len(q)
        deps = []
        for r in reads:
            w = self.lastw.get(r)
            if w is not None:
                deps.append((w, "raw"))
        for w_ in writes:
            w = self.lastw.get(w_)
            if w is not None:
                deps.append((w, "waw"))
            for rd in self.readers.get(w_, ()):
                deps.append((rd, "war"))
        for d, kind in deps:
            if d is ins:
                continue
            if d.dma is None and ins.dma is None and d.eng == ins.eng:
                if ins.eng == "pe":
                    continue
                if kind != "raw":
                    continue
            ins.deps.append(d)
            if d.dma is None:
                d.signal = True
        for r in reads:
            self.readers.setdefault(r, []).append(ins)
        for w_ in writes:
            self.lastw[w_] = ins
            self.readers[w_] = []
        q.append(ins)
        return ins

    PSUM_NAMES = {"bk", "pm", "pT", "pY", "pX", "pN", "pK", "pb", "pS", "pO", "pQ", "pZ", "pR", "pU", "pW"}

    def op(self, eng, fn, reads=(), writes=()):
        writes = list(writes)
        if eng != "pe":
            for r in reads:
                if isinstance(r, tuple) and r[0] in self.PSUM_NAMES and r not in writes:
                    writes.append(r)
        return self._add(_Ins(eng, fn), list(reads), writes)

    def dma(self, out, in_, reads=(), writes=(), queue="sp", **kw):
        slot = self.dma_rr
        self.dma_rr = (self.dma_rr + 1) % self.NDMA
        self.dma_cnt[slot] += 1
        n = self.dma_cnt[slot]
        ins = _Ins(queue, lambda e: e.dma_start(out=out, in_=in_, **kw), dma=(slot, n))
        prev = self.dma_last[slot]
        self._add(ins, list(reads), list(writes))
        if prev is not None:
            ins.deps.append(prev)
        self.dma_last[slot] = ins
        return ins

    def flush(self):
        nc = self.nc
        for e, q in self.q.items():
            for ins in q:
                if ins.dma is None and ins.signal:
                    ins.sig_no = self.sigc[e]
                    self.sigc[e] += 1
            assert self.sigc[e] < self.EPOCH * self.nep[e], f"too many signals on {e}: {self.sigc[e]}"
        dma_final = list(self.dma_cnt)
        with nc.Block() as block:
            def run(ename):
                def body(eng):
                    seen_c = {}
                    seen_d = {}
                    for ins in self.q[ename]:
                        wc = {}
                        wd = {}
                        for d in ins.deps:
                            if d.dma is None:
                                if d.sig_no is None:
                                    continue
                                if seen_c.get(d.eng, -1) < d.sig_no:
                                    wc[d.eng] = max(wc.get(d.eng, -1), d.sig_no)
                            else:
                                s_, n = d.dma
                                if seen_d.get(s_, 0) < n:
                                    wd[s_] = max(wd.get(s_, 0), n)
                        for e2, sn in wc.items():
                            eng.wait_ge(self.sems[e2][sn // self.EPOCH], sn % self.EPOCH + 1)
                            seen_c[e2] = sn
                        for s_, n in wd.items():
                            eng.wait_ge(self.dsems[s_], 16 * n)
                            seen_d[s_] = n
                        h = ins.fn(eng)
                        if ins.dma is not None:
                            h.then_inc(self.dsems[ins.dma[0]], 16)
                        elif ins.signal:
                            h.then_inc(self.sems[ename][ins.sig_no // self.EPOCH], 1)
                    if ename == "sp":
                        for s_, n in enumerate(dma_final):
                            if n > 0:
                                eng.wait_ge(self.dsems[s_], 16 * n)
                return body

            block.sync(run("sp"))
            block.tensor(run("pe"))
            block.scalar(run("act"))
            block.vector(run("dve"))
            block.gpsimd(run("pool"))
        nc.all_engine_barrier()
        self._reset()


def _consts():
    c = {}
    ident = np.eye(128, dtype=np.float32)
    ones = np.ones((128, 128), np.float32)
    idx = np.arange(128)
    same = (idx[:, None] // 64 == idx[None, :] // 64).astype(np.float32)
    m1f = ((idx[:, None] <= idx[None, :]) * same).astype(np.float32)
    m1b = ((idx[:, None] >= idx[None, :]) * same).astype(np.float32)
    sel0 = np.zeros((128, 128), np.float32); sel0[:64, :] = 1
    sel1 = np.zeros((128, 128), np.float32); sel1[64:, :] = 1
    low_incl = ((idx[None, :] <= idx[:, None]) * same)
    up_incl = ((idx[None, :] >= idx[:, None]) * same)
    low_strict = ((idx[None, :] < idx[:, None]) * same)
    up_strict = ((idx[None, :] > idx[:, None]) * same)
    negmask = lambda m: np.where(m > 0, 0.0, NEG).astype(np.float32)
    w_prev = (idx[None, :] <= idx[:, None]).astype(np.float32)
    w_next = (idx[:, None] <= idx[None, :]).astype(np.float32)
    mats = [ident, ones, same, m1f, -m1f, m1b, -m1b, sel0, sel1,
            negmask(low_incl), negmask(up_incl), -low_strict.astype(np.float32), -up_strict.astype(np.float32),
            w_prev, w_next]
    c["cmat"] = np.ascontiguousarray(np.stack(mats, axis=1)).astype(np.float32)
    pos = np.arange(S)
    inv = (10000.0 ** (-np.arange(16, dtype=np.float32) / 16)).astype(np.float32)
    ar = (pos // 64).astype(np.float32)[:, None] * inv[None, :]
    ac = (pos % 64).astype(np.float32)[:, None] * inv[None, :]
    c["rope"] = np.concatenate([np.cos(ar), np.sin(ar), np.cos(ac), np.sin(ac)], axis=1).astype(np.float32)
    return c

(C_ID, C_ONES, C_SAME, C_M1F, C_NM1F, C_M1B, C_NM1B, C_SEL0, C_SEL1, C_NLOW, C_NUP, C_SLOW, C_SUP, C_WPREV, C_WNEXT) = range(15)


def build(upto=99, dbg=()):
    nc = bass.Bass("TRN2", target_bir_lowering=False)
    nc.dge_precook = False
    inp = lambda name, shape: nc.dram_tensor(name, list(shape), F32, kind="ExternalInput").ap()
    x_d = inp("x", [S, D]); c_d = inp("c", [D]); ctx_d = inp("ctx", [CTX, D]); cctx_d = inp("c_ctx", [D])
    wada_d = inp("w_ada", [D, 6 * D]); bada_d = inp("b_ada", [6 * D])
    nmix_d = inp("norm_mix", [D]); nffn_d = inp("norm_ffn", [D])
    win_d = inp("w_in", [D, IN_COLS]); bgate_d = inp("b_gate", [2 * D])
    sink_d = inp("attn_sink", [8]); conv_d = inp("dn_conv", [5, 1536])
    alf_d = inp("dn_a_log_f", [4]); dtf_d = inp("dn_dt_bias_f", [4]); alb_d = inp("dn_a_log_b", [4]); dtb_d = inp("dn_dt_bias_b", [4])
    dnn_d = inp("dn_norm", [128]); wba_d = inp("w_br_attn", [512, D]); wbd_d = inp("w_br_dn", [512, D]); wout_d = inp("w_out", [D, D])
    pwq_d = inp("peer_wq", [D, D]); pkeys_d = inp("peer_keysT", [128, 8, 128]); pu_d = inp("peer_uT", [D, 16384]); pv_d = inp("peer_v", [16384, D])
    fnorm_d = inp("final_norm", [D]); cmat_d = inp("cmat", [128, 15, 128]); rope_d = inp("rope", [S, 64])
    out_d = nc.dram_tensor("out", [S, D], F32, kind="ExternalOutput").ap()
    scr = lambda name, shape: nc.dram_tensor(name, list(shape), F32, kind=("ExternalOutput" if name in dbg else "Internal")).ap()
    QT_s = scr("QT_s", [64, 8, S])
    KT_s = scr("KT_s", [64, 2, TALL])
    V_s = scr("V_s", [TALL, 2, 65])
    RT_s = scr("RT_s", [1536, TALL])
    Z_s = scr("Z_s", [S, 512])
    GB_s = scr("GB_s", [TALL, 16])
    GT_s = scr("GT_s", [S, 2048])
    QK_s = scr("QK_s", [1024, TALL])
    KV_s = scr("KV_s", [TALL, 1024])
    OD_s = scr("OD_s", [2, S, 512])
    OA_s = scr("OA_s", [S, 512])
    MOD_s = scr("MOD_s", [8, D])

    with contextlib.ExitStack() as gst:
        s = Sch(nc, gst)
        _uid = [0]

        def _nm(name):
            _uid[0] += 1
            return f"{name}_u{_uid[0]}"
        T = lambda st, name, shape: st.enter_context(nc.sbuf_tensor(_nm(name), list(shape), F32))
        PS = lambda st, name, shape: st.enter_context(nc.psum_tensor(_nm(name), list(shape), F32))
        cm = T(gst, "cm", [128, 15, 128])
        s.dma(cm[:], cmat_d, writes=["cm"])
        ident = cm[:, C_ID, :]
        BV_G1, BV_SH1, BV_GT1, BV_G2, BV_SH2, BV_GT2, BV_CG1, BV_CSH1, BV_FN = range(9)
        bvB = T(gst, "bvB", [128, 5, D])
        stA = contextlib.ExitStack()
        bvA = T(stA, "bvA", [128, 4, D])
        _amap = {BV_G1: 0, BV_SH1: 1, BV_CG1: 2, BV_CSH1: 3}
        _bmap = {BV_GT1: 0, BV_G2: 1, BV_SH2: 2, BV_GT2: 3, BV_FN: 4}

        def bvv(k):
            return bvA[:, _amap[k], :] if k in _amap else bvB[:, _bmap[k], :]

        with contextlib.ExitStack() as st:
            cc = T(st, "cc", [128, 2, 8]); cs = T(st, "cs", [128, 2, 8]); lh = T(st, "lh", [128, 2, 8, 128])
            wa = [T(st, f"wa{i}", [128, 8, 512]) for i in range(2)]
            bb = T(st, "bb", [128, 6 * D]); nm = T(st, "nm", [128, 2, D])
            pm = [PS(st, f"pm{i}", [128, 512]) for i in range(2)]
            s.dma(cc[:, 0, :], c_d.rearrange("(kc p) -> p kc", p=128), writes=["cc"], allow_slow_non_contiguous=True)
            s.dma(cc[:, 1, :], cctx_d.rearrange("(kc p) -> p kc", p=128), writes=["cc"], allow_slow_non_contiguous=True)
            s.dma(bb[:], bada_d.partition_broadcast(128), writes=["bb"])
            s.dma(nm[:, 0, :], nmix_d.partition_broadcast(128), writes=["nm"])
            s.dma(nm[:, 1, :], nffn_d.partition_broadcast(128), writes=["nm"])
            s.dma(bvv(BV_FN)[:, :], fnorm_d.partition_broadcast(128), writes=["bv"])
            s.op("act", lambda e: e.activation(out=cs[:], in_=cc[:], func=AF.Silu), reads=["cc"], writes=["cs"])
            s.op("dve", lambda e: e.tensor_copy(out=lh[:], in_=cs[:].unsqueeze(3).to_broadcast([128, 2, 8, 128])), reads=["cs"], writes=["lh"])
            jobs = [(0, nb) for nb in range(12)] + [(1, nb) for nb in range(4)]
            for ji, (w, nb) in enumerate(jobs):
                wt = wa[ji % 2]; p = pm[ji % 2]
                s.dma(wt[:], wada_d[:, nb * 512:(nb + 1) * 512].rearrange("(kc p) n -> p kc n", p=128), writes=[("wa", ji % 2)], queue=("sp" if ji % 2 == 0 else "act"))
                for kc in range(8):
                    s.op("pe", lambda e, w=w, kc=kc, wt=wt, p=p: e.matmul(out=p[:], lhsT=lh[:, w, kc, :], rhs=wt[:, kc, :], start=(kc == 0), stop=(kc == 7)),
                         reads=["lh", ("wa", ji % 2)], writes=[("pm", ji % 2)])
                ch, half = nb // 2, nb % 2
                if w == 0:
                    dst = {0: BV_SH1, 1: BV_G1, 2: BV_GT1, 3: BV_SH2, 4: BV_G2, 5: BV_GT2}[ch]
                else:
                    dst = {0: BV_CSH1, 1: BV_CG1}[ch]
                o = bvv(dst)[:, half * 512:(half + 1) * 512]
                s.op("dve", lambda e, o=o, p=p, nb=nb: e.tensor_tensor(out=o, in0=p[:], in1=bb[:, nb * 512:(nb + 1) * 512], op=ALU.add),
                     reads=[("pm", ji % 2), "bb"], writes=["bv"])
            for dst, ni in ((BV_G1, 0), (BV_G2, 1), (BV_CG1, 0)):
                s.op("dve", lambda e, dst=dst, ni=ni: e.scalar_tensor_tensor(out=bvv(dst)[:, :], in0=bvv(dst)[:, :], scalar=1.0, in1=nm[:, ni, :], op0=ALU.add, op1=ALU.mult),
                     reads=["bv", "nm"], writes=["bv"])
            s.flush()
        if upto <= 0:
            stA.close()
            return nc

        blocks = [(0, 512), (512, 256), (768, 512), (1280, 512), (1792, 512), (2304, 512), (2816, 16)] + [(2832 + 512 * i, 512) for i in range(4)]
        with contextlib.ExitStack() as st:
            xt = [T(st, f"xt{i}", [128, D]) for i in range(2)]
            junk = T(st, "junk", [128, D]); ss = T(st, "ss", [128, 1]); rstd = T(st, "rstd", [128, 1])
            h = T(st, "h", [128, D]); hT = T(st, "hT", [128, 8, 128])
            wb = [st.enter_context(nc.sbuf_tensor(_nm(f"wb{i}"), [128, 8, 512], F32R)) for i in range(3)]
            rp = [T(st, f"rp{i}", [128, 64]) for i in range(2)]
            qs = T(st, "qs", [128, 512]); qr = T(st, "qr", [128, 512]); tmp = T(st, "tmp", [128, 512])
            qT = T(st, "qT", [64, 8, 128]); kvs = T(st, "kvs", [128, 256]); kr = T(st, "kr", [128, 128]); kT = T(st, "kT", [64, 2, 128])
            va = T(st, "va", [128, 2, 65]); rw = T(st, "rw", [128, 512]); rT = T(st, "rT", [128, 4, 128])
            zz = T(st, "zz", [128, 512]); gn = T(st, "gn", [128, 128]); ab = T(st, "ab", [128, 16]); abc = T(st, "abc", [128, 2, 8])
            gbo = T(st, "gbo", [128, 16]); gg = T(st, "gg", [128, 512]); bg = T(st, "bg", [128, 2048])
            pT = [PS(st, f"pT{i}", [128, 512]) for i in range(2)]
            pY = [PS(st, f"pY{i}", [128, 512]) for i in range(3)]
            pX = [PS(st, f"pX{i}", [128, 512]) for i in range(2)]
            s.dma(bg[:], bgate_d.partition_broadcast(128), writes=["bg"])
            s.dma(gn[:], dnn_d.partition_broadcast(128), writes=["gn"])
            s.dma(abc[:, 0, 0:4], dtf_d.partition_broadcast(128), writes=["abc"])
            s.dma(abc[:, 0, 4:8], dtb_d.partition_broadcast(128), writes=["abc"])
            s.dma(abc[:, 1, 0:4], alf_d.partition_broadcast(128), writes=["abc"])
            s.dma(abc[:, 1, 4:8], alb_d.partition_broadcast(128), writes=["abc"])
            s.op("act", lambda e: e.activation(out=abc[:, 1, :], in_=abc[:, 1, :], func=AF.Exp), reads=["abc"], writes=["abc"])
            s.op("dve", lambda e: e.tensor_scalar(out=abc[:, 1, :], in0=abc[:, 1, :], scalar1=-1.0, scalar2=None, op0=ALU.mult), reads=["abc"], writes=["abc"])
            s.op("pool", lambda e: e.memset(va[:], 1.0), writes=[("va", 0)])
            wcount = [0]

            def rope_ops(src, dst, H):
                sv = src.rearrange("p (h a b c) -> p h a b c", h=H, a=2, b=2)
                dv = dst.rearrange("p (h a b c) -> p h a b c", h=H, a=2, b=2)
                tv = tmp[:, 0:H * 64].rearrange("p (h a b c) -> p h a b c", h=H, a=2, b=2)
                return sv, dv, tv

            tiles = [("c", i) for i in range(CTX // 128)] + [("l", i) for i in range(NT)]
            if upto == 1 and "small" in dbg:
                tiles = tiles[:4]
            qs2 = [qs, T(st, "qsb_", [128, 512])]; qr2 = [qr, T(st, "qrb_", [128, 512])]; tmp2 = [tmp, T(st, "tmpb_", [128, 512])]
            qT2 = [qT, T(st, "qTb_", [64, 8, 128])]; kvs2 = [kvs, T(st, "kvsb_", [128, 256])]; kr2 = [kr, T(st, "krb_", [128, 128])]; kT2 = [kT, T(st, "kTb_", [64, 2, 128])]
            va2 = [va, T(st, "vab_", [128, 2, 65])]; rw2 = [rw, T(st, "rwb_", [128, 512])]; rT2 = [rT, T(st, "rTb_", [128, 4, 128])]; zz2 = [zz, T(st, "zzb_", [128, 512])]
            ab2 = [ab, T(st, "abb_", [128, 16])]; gbo2 = [gbo, T(st, "gbob_", [128, 16])]; gg2 = [gg, T(st, "ggb_", [128, 512])]
            s.op("pool", lambda e: e.memset(va2[1][:], 1.0), writes=[("va", 1)])
            hT4 = [st.enter_context(nc.sbuf_tensor(_nm(f"hT4_{j}"), [128, 8, 128], F32R)) for j in range(4)]
            rp4 = [T(st, f"rp4_{j}", [128, 64]) for j in range(4)]
            pcount = [0]

            def prep(ti, kind, i, j):
                lat = kind == "l"
                src = x_d if lat else ctx_d
                tg = ti
                X = xt[ti % 2]; xid = ("xt", ti % 2)
                s.dma(X[:], src[i * 128:(i + 1) * 128, :], writes=[xid])
                if lat:
                    R = rp4[j]; rid = ("rp", j)
                    s.dma(R[:], rope_d[i * 128:(i + 1) * 128, :], writes=[rid], queue="act")
                s.op("act", lambda e, X=X: e.activation(out=junk[:], in_=X[:], func=AF.Square, accum_out=ss[:]), reads=[xid], writes=["junk", "ss"])
                s.op("dve", lambda e: e.tensor_scalar(out=rstd[:], in0=ss[:], scalar1=1.0 / D, scalar2=1e-6, op0=ALU.mult, op1=ALU.add), reads=["ss"], writes=["rstd"])
                s.op("act", lambda e: e.sqrt(out=rstd[:], in_=rstd[:]), reads=["rstd"], writes=["rstd"])
                s.op("dve", lambda e: e.reciprocal(out=rstd[:], in_=rstd[:]), reads=["rstd"], writes=["rstd"])
                G = BV_G1 if lat else BV_CG1
                SH = BV_SH1 if lat else BV_CSH1
                s.op("dve", lambda e, X=X, G=G: e.scalar_tensor_tensor(out=h[:], in0=X[:], scalar=rstd[:, 0:1], in1=bvv(G)[:, :], op0=ALU.mult, op1=ALU.mult), reads=[xid, "rstd", "bv"], writes=["h"])
                s.op("pool", lambda e, SH=SH: e.tensor_tensor(out=h[:], in0=h[:], in1=bvv(SH)[:, :], op=ALU.add), reads=["h", "bv"], writes=["h"])
                for hb in range(2):
                    for k4 in range(4):
                        kc = hb * 4 + k4
                        s.op("pe", lambda e, kc=kc, hb=hb, k4=k4: e.transpose(out=pT[hb][:, k4 * 128:(k4 + 1) * 128], in_=h[:, kc * 128:(kc + 1) * 128], identity=ident), reads=["h", "cm"], writes=[("pT", hb)])
                    eng = "act" if hb == 0 else "dve"
                    if eng == "act":
                        s.op("act", lambda e, hb=hb: e.copy(out=hT4[j][:, hb * 4:(hb + 1) * 4, :].rearrange("p a b -> p (a b)"), in_=pT[hb][:]), reads=[("pT", hb)], writes=[("hT", j, hb)])
                    else:
                        s.op("dve", lambda e, hb=hb: e.tensor_copy(out=hT4[j][:, hb * 4:(hb + 1) * 4, :].rearrange("p a b -> p (a b)"), in_=pT[hb][:]), reads=[("pT", hb)], writes=[("hT", j, hb)])

            def proj(ti, kind, i, j, bi, W, wi):
                lat = kind == "l"; tg = ti; R = rp4[j]; rid = ("rp", j)
                c0, cw = blocks[bi]
                pi_ = pcount[0] % 3; pp = pcount[0] % 2; pcount[0] += 1
                P = pY[pi_]
                for kc in range(8):
                    s.op("pe", lambda e, kc=kc, W=W, P=P, cw=cw: e.matmul(out=P[:, 0:cw], lhsT=hT4[j][:, kc, :], rhs=W[:, kc, 0:cw], start=(kc == 0), stop=(kc == 7)),
                         reads=[("hT", j, 0), ("hT", j, 1), ("wb", wi)], writes=[("pY", pi_)])
                pid = ("pY", pi_)
                if bi == 0:
                    s.op("act", lambda e, P=P: e.activation(out=qs2[pp][:], in_=P[:], func=AF.Copy, scale=0.125), reads=[pid], writes=[("qs", pp)])
                    _rope(s, qs2[pp][:], qr2[pp][:], tmp2[pp], R, rid, 8, ("qs", pp), ("qr", pp), ("tmp", pp))
                    for hh in range(8):
                        s.op("pe", lambda e, hh=hh: e.transpose(out=pX[hh // 4][0:64, (hh % 4) * 128:(hh % 4 + 1) * 128], in_=qr2[pp][:, hh * 64:(hh + 1) * 64], identity=ident), reads=[("qr", pp), "cm"], writes=[("pX", hh // 4)])
                    s.op("act", lambda e: e.copy(out=qT2[pp][:, 0:4, :].rearrange("p a b -> p (a b)"), in_=pX[0][0:64, :]), reads=[("pX", 0)], writes=[("qT", pp)])
                    s.op("dve", lambda e: e.tensor_copy(out=qT2[pp][:, 4:8, :].rearrange("p a b -> p (a b)"), in_=pX[1][0:64, :]), reads=[("pX", 1)], writes=[("qT", pp)])
                    s.dma(QT_s[:, :, i * 128:(i + 1) * 128], qT2[pp][:], reads=[("qT", pp)], writes=["QT_s"], queue="pool")
                elif bi == 1:
                    s.op("act", lambda e, P=P: e.copy(out=kvs2[pp][:], in_=P[:, 0:256]), reads=[pid], writes=[("kvs", pp)])
                    if lat:
                        _rope(s, kvs2[pp][:, 0:128], kr2[pp][:], tmp2[pp], R, rid, 2, ("kvs", pp), ("kr", pp), ("tmp", pp))
                        ksrc, kid = kr2[pp], ("kr", pp)
                    else:
                        ksrc, kid = kvs2[pp], ("kvs", pp)
                    for hh in range(2):
                        s.op("pe", lambda e, hh=hh, ksrc=ksrc: e.transpose(out=pX[0][0:64, hh * 128:(hh + 1) * 128], in_=ksrc[:, hh * 64:(hh + 1) * 64], identity=ident), reads=[kid, "cm"], writes=[("pX", 0)])
                    s.op("act", lambda e: e.copy(out=kT2[pp][:].rearrange("p a b -> p (a b)"), in_=pX[0][0:64, 0:256]), reads=[("pX", 0)], writes=[("kT", pp)])
                    s.dma(KT_s[:, :, tg * 128:(tg + 1) * 128], kT2[pp][:], reads=[("kT", pp)], writes=["KT_s"], queue="pool")
                    s.op("pool", lambda e: e.tensor_copy(out=va2[pp][:, :, 0:64], in_=kvs2[pp][:, 128:256].rearrange("p (g d) -> p g d", g=2)), reads=[("kvs", pp)], writes=[("va", pp)])
                    s.dma(V_s[tg * 128:(tg + 1) * 128, :, :], va2[pp][:], reads=[("va", pp)], writes=["V_s"], queue="pool")
                elif bi in (2, 3, 4):
                    s.op("act", lambda e, P=P: e.copy(out=rw2[pp][:], in_=P[:]), reads=[pid], writes=[("rw", pp)])
                    for k4 in range(4):
                        s.op("pe", lambda e, k4=k4: e.transpose(out=pX[1][:, k4 * 128:(k4 + 1) * 128], in_=rw2[pp][:, k4 * 128:(k4 + 1) * 128], identity=ident), reads=[("rw", pp), "cm"], writes=[("pX", 1)])
                    s.op("dve", lambda e: e.tensor_copy(out=rT2[pp][:].rearrange("p a b -> p (a b)"), in_=pX[1][:]), reads=[("pX", 1)], writes=[("rT", pp)])
                    f0 = (bi - 2) * 512
                    s.dma(RT_s[f0:f0 + 512, tg * 128:(tg + 1) * 128].rearrange("(a p) t -> p a t", p=128), rT2[pp][:], reads=[("rT", pp)], writes=["RT_s"], queue="pool")
                elif bi == 5:
                    s.op("act", lambda e, P=P: e.activation(out=zz2[pp][:], in_=P[:], func=AF.Silu), reads=[pid], writes=[("zz", pp)])
                    s.op("pool", lambda e: e.tensor_tensor(out=zz2[pp][:].rearrange("p (h d) -> p h d", h=4), in0=zz2[pp][:].rearrange("p (h d) -> p h d", h=4), in1=gn[:].unsqueeze(1).to_broadcast([128, 4, 128]), op=ALU.mult), reads=[("zz", pp), "gn"], writes=[("zz", pp)])
                    s.dma(Z_s[i * 128:(i + 1) * 128, :], zz2[pp][:], reads=[("zz", pp)], writes=["Z_s"], queue="pool")
                elif bi == 6:
                    s.op("dve", lambda e, P=P: e.tensor_tensor(out=ab2[pp][:, 0:8], in0=P[:, 0:8], in1=abc[:, 0, :], op=ALU.add), reads=[pid, "abc"], writes=[("ab", pp)])
                    s.op("act", lambda e: e.activation(out=ab2[pp][:, 0:8], in_=ab2[pp][:, 0:8], func=AF.Exp), reads=[("ab", pp)], writes=[("ab", pp)])
                    s.op("dve", lambda e: e.tensor_scalar(out=ab2[pp][:, 0:8], in0=ab2[pp][:, 0:8], scalar1=1.0, scalar2=None, op0=ALU.add), reads=[("ab", pp)], writes=[("ab", pp)])
                    s.op("act", lambda e: e.activation(out=ab2[pp][:, 0:8], in_=ab2[pp][:, 0:8], func=AF.Ln), reads=[("ab", pp)], writes=[("ab", pp)])
                    s.op("dve", lambda e: e.tensor_tensor(out=gbo2[pp][:, 0:8], in0=ab2[pp][:, 0:8], in1=abc[:, 1, :], op=ALU.mult), reads=[("ab", pp), "abc"], writes=[("gbo", pp)])
                    s.op("act", lambda e, P=P: e.activation(out=gbo2[pp][:, 8:16], in_=P[:, 8:16], func=AF.Sigmoid), reads=[pid], writes=[("gbo", pp)])
                    s.dma(GB_s[tg * 128:(tg + 1) * 128, :], gbo2[pp][:], reads=[("gbo", pp)], writes=["GB_s"], queue="pool")
                else:
                    gi = bi - 7
                    s.op("dve", lambda e, P=P, gi=gi: e.tensor_tensor(out=gg2[pp][:], in0=P[:], in1=bg[:, gi * 512:(gi + 1) * 512], op=ALU.add), reads=[pid, "bg"], writes=[("gg", pp)])
                    s.op("act", lambda e: e.activation(out=gg2[pp][:], in_=gg2[pp][:], func=AF.Sigmoid), reads=[("gg", pp)], writes=[("gg", pp)])
                    s.dma(GT_s[i * 128:(i + 1) * 128, gi * 512:(gi + 1) * 512], gg2[pp][:], reads=[("gg", pp)], writes=["GT_s"], queue="pool")

            groups = [tiles[0:2]] + [tiles[k:k + 4] for k in range(2, len(tiles), 4)]
            tbase = 0
            for grp in groups:
                for j, (kind, i) in enumerate(grp):
                    prep(tbase + j, kind, i, j)
                need = range(11) if grp[0][0] == "l" else (1, 2, 3, 4, 6)
                for bi in need:
                    c0, cw = blocks[bi]
                    wi = wcount[0] % 3; wcount[0] += 1
                    W = wb[wi]
                    s.dma(W[:, :, 0:cw], win_d[:, c0:c0 + cw].rearrange("(kc p) n -> p kc n", p=128), writes=[("wb", wi)], queue="pool", allow_slow_non_contiguous=(cw < 128))
                    for j, (kind, i) in enumerate(grp):
                        proj(tbase + j, kind, i, j, bi, W, wi)
                tbase += len(grp)
            s.flush()
        stA.close()
        if upto <= 1:
            return nc

        with contextlib.ExitStack() as st:
            cw = T(st, "cw", [128, 12, 5])
            Rt = [T(st, f"Rt{i}", [128, 516]) for i in range(3)]
            acc = [T(st, f"acc{i}", [128, 512]) for i in range(2)]
            y = [T(st, f"y{i}", [128, 512]) for i in range(2)]
            y2 = T(st, "y2", [128, 512]); rn = T(st, "rn", [128, 512]); yn = [T(st, f"yn{i}", [128, 512]) for i in range(2)]
            tok = [T(st, f"tok{i}", [128, 4, 128]) for i in range(2)]
            pN = [PS(st, f"pN{i}", [128, 512]) for i in range(2)]
            pK = [PS(st, f"pK{i}", [128, 512]) for i in range(2)]
            for j in range(5):
                s.dma(cw[:, :, j], conv_d[j, :].rearrange("(fc p) -> p fc", p=128), writes=["cw"], allow_slow_non_contiguous=True)
            it = 0
            segs = [(0, CTX), (CTX, TALL)]
            if "small" in dbg:
                segs = [(0, CTX), (CTX, CTX + 256)]
            for (g0, g1) in segs:
                for t0 in range(g0, g1, 512):
                    n = min(512, g1 - t0)
                    for fc in range(12):
                        R = Rt[it % 3]; rid = ("Rt", it % 3); A = acc[it % 2]; aid = ("acc", it % 2); Y = y[it % 2]; yid = ("y", it % 2)
                        lo = max(t0 - 2, g0); hi = min(t0 + n + 2, g1)
                        if lo > t0 - 2 or hi < t0 + n + 2:
                            s.op("pool", lambda e, R=R: e.memset(R[:], 0.0), writes=[rid])
                        s.dma(R[:, lo - (t0 - 2):hi - (t0 - 2)], RT_s[fc * 128:(fc + 1) * 128, lo:hi], reads=["RT_s"], writes=[rid], queue=("sp", "act")[it % 2])
                        s.op("dve", lambda e, R=R, A=A, fc=fc, n=n: e.tensor_scalar(out=A[:, 0:n], in0=R[:, 0:n], scalar1=cw[:, fc, 0:1], scalar2=None, op0=ALU.mult), reads=[rid, "cw"], writes=[aid])
                        for j in range(1, 5):
                            s.op("dve", lambda e, R=R, A=A, fc=fc, n=n, j=j: e.scalar_tensor_tensor(out=A[:, 0:n], in0=R[:, j:j + n], scalar=cw[:, fc, j:j + 1], in1=A[:, 0:n], op0=ALU.mult, op1=ALU.add), reads=[rid, "cw", aid], writes=[aid])
                        s.op("act", lambda e, A=A, Y=Y, n=n: e.activation(out=Y[:, 0:n], in_=A[:, 0:n], func=AF.Silu), reads=[aid], writes=[yid])
                        src, sid = Y, yid
                        if fc < 8:
                            YN = yn[it % 2]; nid = ("yn", it % 2); P = pN[it % 2]; pid = ("pN", it % 2)
                            s.op("act", lambda e, Y=Y, n=n: e.activation(out=y2[:, 0:n], in_=Y[:, 0:n], func=AF.Square), reads=[yid], writes=["y2"])
                            s.op("pe", lambda e, P=P, n=n: e.matmul(out=P[:, 0:n], lhsT=cm[:, C_ONES, :], rhs=y2[:, 0:n], start=True, stop=True), reads=["cm", "y2"], writes=[pid])
                            s.op("dve", lambda e, P=P, n=n: e.tensor_scalar(out=rn[:, 0:n], in0=P[:, 0:n], scalar1=1e-6, scalar2=None, op0=ALU.add), reads=[pid], writes=["rn"])
                            s.op("act", lambda e, n=n: e.sqrt(out=rn[:, 0:n], in_=rn[:, 0:n]), reads=["rn"], writes=["rn"])
                            s.op("dve", lambda e, n=n: e.reciprocal(out=rn[:, 0:n], in_=rn[:, 0:n]), reads=["rn"], writes=["rn"])
                            sc = float(128 ** -0.5) if fc < 4 else 1.0
                            s.op("dve", lambda e, Y=Y, YN=YN, n=n, sc=sc: e.scalar_tensor_tensor(out=YN[:, 0:n], in0=Y[:, 0:n], scalar=sc, in1=rn[:, 0:n], op0=ALU.mult, op1=ALU.mult), reads=[yid, "rn"], writes=[nid])
                            s.dma(QK_s[fc * 128:(fc + 1) * 128, t0:t0 + n], YN[:, 0:n], reads=[nid], writes=["QK_s"], queue="pool")
                            src, sid = YN, nid
                        if fc >= 4:
                            PK = pK[it % 2]; kid = ("pK", it % 2); TK = tok[it % 2]; tid = ("tok", it % 2)
                            nsb = n // 128
                            for sb in range(nsb):
                                s.op("pe", lambda e, PK=PK, src=src, sb=sb: e.transpose(out=PK[:, sb * 128:(sb + 1) * 128], in_=src[:, sb * 128:(sb + 1) * 128], identity=ident), reads=[sid, "cm"], writes=[kid])
                            s.op("act", lambda e, PK=PK, TK=TK, n=n: e.copy(out=TK[:].rearrange("p a b -> p (a b)")[:, 0:n], in_=PK[:, 0:n]), reads=[kid], writes=[tid])
                            s.dma(KV_s[t0:t0 + n, (fc - 4) * 128:(fc - 3) * 128].rearrange("(sb p) f -> p sb f", p=128), TK[:, 0:nsb, :], reads=[tid], writes=["KV_s"], queue="pool")
                        it += 1
            s.flush()
        if upto <= 2:
            return nc

        with contextlib.ExitStack() as st:
            Sst = [T(st, f"Sst{i}", [128, 4, 128]) for i in range(2)]
            qT4 = T(st, "qT4", [128, 4, 128]); kT4 = T(st, "kT4", [128, 4, 128]); ktok = T(st, "ktok", [128, 4, 128]); vtok = T(st, "vtok", [128, 4, 128])
            gb = T(st, "gb", [128, 16]); sm = T(st, "sm", [128, 16]); ex = T(st, "ex", [128, 16]); beg = T(st, "beg", [128, 4])
            G1 = T(st, "G1", [128, 4, 128]); dl = T(st, "dl", [128, 4, 128]); du = T(st, "du", [128, 4, 128])
            Bm = [T(st, f"Bm{i}", [128, 4, 128]) for i in range(2)]; Cm = [T(st, f"Cm{i}", [128, 4, 128]) for i in range(2)]; Pm = [T(st, f"Pm{i}", [128, 4, 128]) for i in range(2)]
            aT = T(st, "aT", [128, 4, 128]); kbg = T(st, "kbg", [128, 4, 128]); vb = T(st, "vb", [128, 4, 128]); ktl = T(st, "ktl", [128, 4, 128])
            WT = T(st, "WT", [128, 4, 128]); U = T(st, "U", [128, 4, 128]); vn = T(st, "vn", [128, 4, 128]); o1 = T(st, "o1", [128, 4, 128]); ot = T(st, "ot", [128, 4, 128])
            pb = [PS(st, f"pb{i}", [128, 4, 128]) for i in range(8)]
            pA, pB_, pC, pD, pE, pF, pG, pH = pb
            pid = lambda k: ("pb", k)
            H4 = [128, 4, 128]
            bc_h = lambda ap2: ap2.unsqueeze(1).to_broadcast(H4)
            bc_l = lambda ap2: ap2.unsqueeze(2).to_broadcast(H4)
            ntl = (2 if "small" in dbg else NT)
            for dr in range(2):
                M1 = cm[:, C_M1F if dr == 0 else C_M1B, :]; NM1 = cm[:, C_NM1F if dr == 0 else C_NM1B, :]
                NB = cm[:, C_NLOW if dr == 0 else C_NUP, :]; NTm = cm[:, C_NUP if dr == 0 else C_NLOW, :]
                STR = cm[:, C_SLOW if dr == 0 else C_SUP, :]
                SS = Sst[dr]; ssid = ("Sst", dr)
                s.op("pool", lambda e, SS=SS: e.memset(SS[:], 0.0), writes=[ssid])
                order = [("c", i) for i in range(CTX // 128)] + [("l", i) for i in range(ntl)]
                if dr == 1:
                    order = [("c", i) for i in reversed(range(CTX // 128))] + [("l", i) for i in reversed(range(ntl))]
                for (kind, i) in order:
                    lat = kind == "l"
                    tg = i if not lat else CTX // 128 + i
                    tsl = slice(tg * 128, (tg + 1) * 128)
                    s.dma(qT4[:], QK_s[0:512, tsl].rearrange("(h p) t -> p h t", p=128), reads=["QK_s"], writes=["qT4"])
                    s.dma(kT4[:], QK_s[512:1024, tsl].rearrange("(h p) t -> p h t", p=128), reads=["QK_s"], writes=["kT4"], queue="act")
                    s.dma(ktok[:].rearrange("p h d -> p (h d)"), KV_s[tsl, 0:512], reads=["KV_s"], writes=["ktok"])
                    s.dma(vtok[:].rearrange("p h d -> p (h d)"), KV_s[tsl, 512:1024], reads=["KV_s"], writes=["vtok"], queue="act")
                    s.dma(gb[:], GB_s[tsl, :], reads=["GB_s"], writes=["gb"])
                    g = gb[:, dr * 4:dr * 4 + 4]; beta = gb[:, 8 + dr * 4:12 + dr * 4]
                    pAf = pA[:].rearrange("p a b -> p (a b)")
                    for k, L in enumerate((M1, cm[:, C_SAME, :], cm[:, C_SEL0, :], cm[:, C_SEL1, :])):
                        s.op("pe", lambda e, k=k, L=L, g=g: e.matmul(out=pAf[:, 4 * k:4 * k + 4], lhsT=L, rhs=g, start=True, stop=True), reads=["cm", "gb"], writes=[pid(0)])
                    s.op("dve", lambda e: e.tensor_copy(out=sm[:], in_=pAf[:, 0:16]), reads=[pid(0)], writes=["sm"])
                    s.op("dve", lambda e: e.tensor_tensor(out=sm[:, 4:8], in0=sm[:, 4:8], in1=sm[:, 0:4], op=ALU.subtract), reads=["sm"], writes=["sm"])
                    s.op("act", lambda e: e.activation(out=ex[:], in_=sm[:], func=AF.Exp), reads=["sm"], writes=["ex"])
                    s.op("dve", lambda e, beta=beta: e.tensor_tensor(out=beg[:], in0=ex[:, 0:4], in1=beta, op=ALU.mult), reads=["ex", "gb"], writes=["beg"])
                    s.op("dve", lambda e, g=g: e.tensor_tensor(out=G1[:], in0=bc_h(cm[:, C_SAME, :]), in1=bc_l(g), op=ALU.mult), reads=["cm", "gb"], writes=["G1"])
                    for hh in range(4):
                        s.op("pe", lambda e, hh=hh, M1=M1: e.matmul(out=pB_[:, hh, :], lhsT=M1, rhs=G1[:, hh, :], start=True, stop=False), reads=["cm", "G1"], writes=[pid(1)])
                        s.op("pe", lambda e, hh=hh, NM1=NM1: e.matmul(out=pB_[:, hh, :], lhsT=G1[:, hh, :], rhs=NM1, start=False, stop=True), reads=["cm", "G1"], writes=[pid(1)])
                    s.op("dve", lambda e, NB=NB: e.tensor_tensor(out=dl[:], in0=pB_[:], in1=bc_h(NB), op=ALU.add), reads=[pid(1), "cm"], writes=["dl"])
                    s.op("dve", lambda e, NTm=NTm: e.scalar_tensor_tensor(out=du[:], in0=pB_[:], scalar=-1.0, in1=bc_h(NTm), op0=ALU.mult, op1=ALU.add), reads=[pid(1), "cm"], writes=["du"])
                    s.op("act", lambda e: e.activation(out=dl[:], in_=dl[:], func=AF.Exp), reads=["dl"], writes=["dl"])
                    s.op("act", lambda e: e.activation(out=du[:], in_=du[:], func=AF.Exp), reads=["du"], writes=["du"])
                    for hh in range(4):
                        s.op("pe", lambda e, hh=hh: e.matmul(out=pC[:, hh, :], lhsT=kT4[:, hh, :], rhs=kT4[:, hh, :], start=True, stop=True), reads=["kT4"], writes=[pid(2)])
                    for hh in range(4):
                        s.op("pe", lambda e, hh=hh: e.matmul(out=pD[:, hh, :], lhsT=kT4[:, hh, :], rhs=qT4[:, hh, :], start=True, stop=True), reads=["kT4", "qT4"], writes=[pid(3)])
                    B0 = Bm[0]; C0 = Cm[0]; P0 = Pm[0]
                    s.op("dve", lambda e: e.tensor_tensor(out=B0[:], in0=pC[:], in1=dl[:], op=ALU.mult), reads=[pid(2), "dl"], writes=[("Bm", 0)])
                    s.op("pool", lambda e, STR=STR: e.tensor_tensor(out=B0[:], in0=B0[:], in1=bc_h(STR), op=ALU.mult), reads=[("Bm", 0), "cm"], writes=[("Bm", 0)])
                    s.op("pool", lambda e, beta=beta: e.tensor_tensor(out=B0[:], in0=B0[:], in1=bc_l(beta), op=ALU.mult), reads=[("Bm", 0), "gb"], writes=[("Bm", 0)])
                    s.op("dve", lambda e: e.tensor_tensor(out=aT[:], in0=pD[:], in1=du[:], op=ALU.mult), reads=[pid(3), "du"], writes=["aT"])
                    for hh in range(4):
                        s.op("pe", lambda e, hh=hh: e.transpose(out=pE[:, hh, :], in_=B0[:, hh, :], identity=ident), reads=[("Bm", 0), "cm"], writes=[pid(4)])
                    s.op("act", lambda e: e.copy(out=C0[:], in_=pE[:]), reads=[pid(4)], writes=[("Cm", 0)])
                    s.op("dve", lambda e: e.tensor_tensor(out=P0[:], in0=C0[:], in1=bc_h(ident), op=ALU.add), reads=[("Cm", 0), "cm"], writes=[("Pm", 0)])
                    cur = 0
                    for lv in range(1, 6):
                        nx = 1 - cur
                        Bc, Cc, Pc = Bm[cur], Cm[cur], Pm[cur]; Bn, Cn, Pn = Bm[nx], Cm[nx], Pm[nx]
                        for hh in range(4):
                            s.op("pe", lambda e, hh=hh, Bc=Bc, Cc=Cc: e.matmul(out=pF[:, hh, :], lhsT=Cc[:, hh, :], rhs=Bc[:, hh, :], start=True, stop=True), reads=[("Bm", cur), ("Cm", cur)], writes=[pid(5)])
                        s.op("act", lambda e, Bn=Bn: e.copy(out=Bn[:], in_=pF[:]), reads=[pid(5)], writes=[("Bm", nx)])
                        if lv < 5:
                            for hh in range(4):
                                s.op("pe", lambda e, hh=hh, Bc=Bc, Cc=Cc: e.matmul(out=pG[:, hh, :], lhsT=Bc[:, hh, :], rhs=Cc[:, hh, :], start=True, stop=True), reads=[("Bm", cur), ("Cm", cur)], writes=[pid(6)])
                            s.op("dve", lambda e, Cn=Cn: e.tensor_copy(out=Cn[:], in_=pG[:]), reads=[pid(6)], writes=[("Cm", nx)])
                        for hh in range(4):
                            s.op("pe", lambda e, hh=hh, Pc=Pc: e.matmul(out=pH[:, hh, :], lhsT=ident, rhs=Pc[:, hh, :], start=True, stop=False), reads=[("Pm", cur), "cm"], writes=[pid(7)])
                            s.op("pe", lambda e, hh=hh, Pc=Pc, Bn=Bn: e.matmul(out=pH[:, hh, :], lhsT=Bn[:, hh, :], rhs=Pc[:, hh, :], start=False, stop=True), reads=[("Pm", cur), ("Bm", nx)], writes=[pid(7)])
                        s.op("dve", lambda e, Pn=Pn: e.tensor_copy(out=Pn[:], in_=pH[:]), reads=[pid(7)], writes=[("Pm", nx)])
                        cur = nx
                    TT = Pm[cur]; ttid = ("Pm", cur)
                    s.op("pool", lambda e: e.tensor_tensor(out=kbg[:], in0=ktok[:], in1=bc_l(beg[:]), op=ALU.mult), reads=["ktok", "beg"], writes=["kbg"])
                    s.op("pool", lambda e, beta=beta: e.tensor_tensor(out=vb[:], in0=vtok[:], in1=bc_l(beta), op=ALU.mult), reads=["vtok", "gb"], writes=["vb"])
                    s.op("pool", lambda e: e.tensor_tensor(out=ktl[:], in0=ktok[:], in1=bc_l(ex[:, 4:8]), op=ALU.mult), reads=["ktok", "ex"], writes=["ktl"])
                    for hh in range(4):
                        s.op("pe", lambda e, hh=hh, TT=TT: e.matmul(out=pE[:, hh, :], lhsT=kbg[:, hh, :], rhs=TT[:, hh, :], start=True, stop=True), reads=["kbg", ttid], writes=[pid(4)])
                    s.op("act", lambda e: e.copy(out=WT[:], in_=pE[:]), reads=[pid(4)], writes=["WT"])
                    for hh in range(4):
                        s.op("pe", lambda e, hh=hh, TT=TT: e.matmul(out=pF[:, hh, :], lhsT=TT[:, hh, :], rhs=vb[:, hh, :], start=True, stop=True), reads=["vb", ttid], writes=[pid(5)])
                    s.op("dve", lambda e: e.tensor_copy(out=U[:], in_=pF[:]), reads=[pid(5)], writes=["U"])
                    for c in ((0, 1) if dr == 0 else (1, 0)):
                        pr = slice(64 * c, 64 * c + 64)
                        for hh in range(4):
                            s.op("pe", lambda e, hh=hh, SS=SS: e.matmul(out=pG[:, hh, :], lhsT=WT[:, hh, :], rhs=SS[:, hh, :], start=True, stop=True), reads=["WT", ssid], writes=[pid(6)])
                        s.op("dve", lambda e, pr=pr: e.tensor_tensor(out=vn[pr], in0=U[pr], in1=pG[pr], op=ALU.subtract), reads=["U", pid(6)], writes=["vn"])
                        for hh in range(4):
                            s.op("pe", lambda e, hh=hh, SS=SS: e.matmul(out=pH[:, hh, :], lhsT=qT4[:, hh, :], rhs=SS[:, hh, :], start=True, stop=True), reads=["qT4", ssid], writes=[pid(7)])
                        for hh in range(4):
                            s.op("pe", lambda e, hh=hh, pr=pr: e.matmul(out=pC[:, hh, :], lhsT=aT[pr, hh, :], rhs=vn[pr, hh, :], start=True, stop=True), reads=["aT", "vn"], writes=[pid(2)])
                        for hh in range(4):
                            s.op("pe", lambda e, hh=hh, pr=pr: e.matmul(out=pD[:, hh, :], lhsT=ktl[pr, hh, :], rhs=vn[pr, hh, :], start=True, stop=True), reads=["ktl", "vn"], writes=[pid(3)])
                        if lat:
                            s.op("dve", lambda e, pr=pr: e.tensor_tensor(out=o1[pr], in0=pH[pr], in1=bc_l(ex[:, 0:4])[pr], op=ALU.mult), reads=[pid(7), "ex"], writes=["o1"])
                            s.op("dve", lambda e, pr=pr: e.tensor_tensor(out=ot[pr], in0=o1[pr], in1=pC[pr], op=ALU.add), reads=["o1", pid(2)], writes=["ot"])
                        s.op("pool", lambda e, c=c, SS=SS: e.tensor_tensor(out=SS[:], in0=SS[:], in1=bc_l(ex[:, 8 + 4 * c:12 + 4 * c]), op=ALU.mult), reads=[ssid, "ex", pid(6), pid(7)], writes=[ssid])
                        s.op("dve", lambda e, SS=SS: e.tensor_tensor(out=SS[:], in0=SS[:], in1=pD[:], op=ALU.add), reads=[ssid, pid(3)], writes=[ssid])
                    if lat:
                        s.dma(OD_s[dr, i * 128:(i + 1) * 128, :], ot[:].rearrange("p h d -> p (h d)"), reads=["ot"], writes=["OD_s"], queue="pool")
            s.flush()
        if upto <= 3:
            return nc

        with contextlib.ExitStack() as st:
            kt = [T(st, f"kt{i}", [64, 2, 384]) for i in range(2)]
            vt = [T(st, f"vt{i}", [128, 3, 130]) for i in range(2)]
            ktc = T(st, "ktc", [64, 2, 256]); vtc = T(st, "vtc", [128, 2, 130])
            qt = [T(st, f"qt{i}", [64, 8, 128]) for i in range(2)]
            E = [T(st, f"E{i}", [128, 5, 512]) for i in range(2)]
            esink = T(st, "esink", [128, 8]); den = T(st, "den", [128, 8]); oa = [T(st, f"oa{i}", [128, 512]) for i in range(2)]
            pS = [PS(st, f"pS{i}", [128, 512]) for i in range(3)]
            pO = [PS(st, f"pO{i}", [128, 4, 65]) for i in range(2)]
            s.dma(ktc[:], KT_s[:, :, 0:CTX], reads=["KT_s"], writes=["ktc"])
            s.dma(vtc[:], V_s[0:CTX].rearrange("(b p) g d -> p b (g d)", p=128), reads=["V_s"], writes=["vtc"])
            s.dma(esink[:], sink_d.partition_broadcast(128), writes=["esink"])
            s.op("act", lambda e: e.activation(out=esink[:], in_=esink[:], func=AF.Exp), reads=["esink"], writes=["esink"])
            ntl = (2 if "small" in dbg else NT)
            nS = 0
            for i in range(ntl):
                lo = max(i - 1, 0); hi = min(i + 1, ntl - 1); nb = hi - lo + 1
                KT_ = kt[i % 2]; VT_ = vt[i % 2]; QT_ = qt[i % 2]; OA = oa[i % 2]
                s.dma(KT_[:, :, 0:nb * 128], KT_s[:, :, CTX + lo * 128:CTX + (hi + 1) * 128], reads=["KT_s"], writes=[("kt", i % 2)])
                s.dma(VT_[:, 0:nb, :], V_s[CTX + lo * 128:CTX + (hi + 1) * 128].rearrange("(b p) g d -> p b (g d)", p=128), reads=["V_s"], writes=[("vt", i % 2)], queue="act")
                s.dma(QT_[:], QT_s[:, :, i * 128:(i + 1) * 128], reads=["QT_s"], writes=[("qt", i % 2)])
                for g in range(2):
                    Eg = E[g]; eid = ("E", g)
                    kb = [("l", j - lo, (C_WPREV if j < i else (C_WNEXT if j > i else None))) for j in range(lo, hi + 1)] + [("c", 0, None), ("c", 1, None)]
                    for bi, (kk, bl, msk) in enumerate(kb):
                        P = pS[nS % 3]; psid = ("pS", nS % 3); nS += 1
                        lhs = KT_[:, g, bl * 128:(bl + 1) * 128] if kk == "l" else ktc[:, g, bl * 128:(bl + 1) * 128]
                        s.op("pe", lambda e, P=P, lhs=lhs, QT_=QT_, g=g: e.matmul(out=P[:].rearrange("p (h q) -> p h q", h=4), lhsT=lhs, rhs=QT_[:, 4 * g:4 * g + 4, :], start=True, stop=True),
                             reads=[("kt", i % 2), "ktc", ("qt", i % 2)], writes=[psid])
                        s.op("act", lambda e, P=P, Eg=Eg, bi=bi: e.activation(out=Eg[:, bi, :], in_=P[:], func=AF.Exp), reads=[psid], writes=[eid])
                        if msk is not None:
                            s.op("dve", lambda e, Eg=Eg, bi=bi, msk=msk: e.tensor_tensor(out=Eg[:, bi, :].rearrange("p (h q) -> p h q", h=4), in0=Eg[:, bi, :].rearrange("p (h q) -> p h q", h=4),
                                                                              in1=cm[:, msk, :].unsqueeze(1).to_broadcast([128, 4, 128]), op=ALU.mult), reads=[eid, "cm"], writes=[eid])
                    for hh in range(4):
                        for bi, (kk, bl, msk) in enumerate(kb):
                            rhs = VT_[:, bl, g * 65:(g + 1) * 65] if kk == "l" else vtc[:, bl, g * 65:(g + 1) * 65]
                            s.op("pe", lambda e, Eg=Eg, bi=bi, hh=hh, rhs=rhs, g=g, last=(bi == len(kb) - 1): e.matmul(out=pO[g][:, hh, :], lhsT=Eg[:, bi, hh * 128:(hh + 1) * 128], rhs=rhs, start=(bi == 0), stop=last),
                                 reads=[eid, ("vt", i % 2), "vtc"], writes=[("pO", g)])
                    s.op("dve", lambda e, g=g: e.tensor_tensor(out=den[:, 4 * g:4 * g + 4], in0=pO[g][:, :, 64], in1=esink[:, 4 * g:4 * g + 4], op=ALU.add), reads=[("pO", g), "esink"], writes=["den"])
                    s.op("dve", lambda e, g=g: e.reciprocal(out=den[:, 4 * g:4 * g + 4], in_=den[:, 4 * g:4 * g + 4]), reads=["den"], writes=["den"])
                    s.op("dve", lambda e, g=g, OA=OA: e.tensor_tensor(out=OA[:, g * 256:(g + 1) * 256].rearrange("p (h d) -> p h d", h=4), in0=pO[g][:, :, 0:64],
                                                              in1=den[:, 4 * g:4 * g + 4].unsqueeze(2).to_broadcast([128, 4, 64]), op=ALU.mult), reads=[("pO", g), "den"], writes=[("oa", i % 2)])
                s.dma(OA_s[i * 128:(i + 1) * 128, :], OA[:], reads=[("oa", i % 2)], writes=["OA_s"], queue="pool")
            s.flush()
        if upto <= 5:
            return nc

        UTr_s = nc.dram_tensor("UTr_s", [D, 16384], F32R, kind="Internal").ap()
        Vr_s = nc.dram_tensor("Vr_s", [16384, D], F32R, kind="Internal").ap()
        X1_s = scr("X1_s", [S, D])
        H2T_s = scr("H2T_s", [D, S])
        H2R_s = nc.dram_tensor("H2R_s", [D, S], F32R, kind="Internal").ap()
        SC_s = scr("SC_s", [S, 2048])
        KAP_s = scr("KAP_s", [S, 8])
        TR = lambda st, name, shape: st.enter_context(nc.sbuf_tensor(_nm(name), list(shape), F32R))
        B = lambda k: ("bk", k)
        ntl6 = (2 if "small" in dbg else NT)

        with contextlib.ExitStack() as st:
            cvb = [TR(st, "cvb", [128, 4096]) for _ in range(2)]
            wbr = T(st, "wbr", [128, 8, D]); wo = T(st, "wo", [128, 8, D])
            xa = [T(st, f"xa{b}", [128, D]) for b in range(2)]; yb = [T(st, f"yb{b}", [128, D]) for b in range(2)]
            tcA = [T(st, f"tcA{b}", [128, 8, 128]) for b in range(2)]; h2r = [TR(st, f"h2r{b}", [128, 8, 128]) for b in range(2)]
            gt = [T(st, f"gt{b}", [128, 2048]) for b in range(2)]
            od = [T(st, f"od{b}", [128, 2, 512]) for b in range(2)]; zt = [T(st, f"zt{b}", [128, 512]) for b in range(2)]
            oat = [T(st, f"oat{b}", [128, 512]) for b in range(2)]; o2 = [T(st, f"o2{b}", [128, 512]) for b in range(2)]
            qsb = [T(st, f"qsb{b}", [128, D]) for b in range(2)]
            ssq = [T(st, f"ssq{b}", [128, 4]) for b in range(2)]; ss = [T(st, f"ss{b}", [128, 1]) for b in range(2)]; rstd = [T(st, f"rstd{b}", [128, 1]) for b in range(2)]
            bk = [PS(st, f"bkA{i}", [128, 512]) for i in range(8)]
            cjobs = []
            for r0 in range(0, D, 128):
                for c0 in range(0, 16384, 4096):
                    cjobs.append((pu_d[r0:r0 + 128, c0:c0 + 4096], UTr_s[r0:r0 + 128, c0:c0 + 4096], "UTr_s"))
            for r0 in range(0, 16384, 512):
                cjobs.append((pv_d[r0:r0 + 512, :].rearrange("(p a) n -> p (a n)", p=128), Vr_s[r0:r0 + 512, :].rearrange("(p a) n -> p (a n)", p=128), "Vr_s"))
            cstate = [0]

            def conv_some(n):
                for _ in range(n):
                    if not cjobs:
                        return
                    src, dst, did = cjobs.pop(0)
                    k = cstate[0] % 2; cstate[0] += 1
                    s.dma(cvb[k][:], src, writes=[("cvb", k)], queue="pool")
                    s.dma(dst, cvb[k][:], reads=[("cvb", k)], writes=[did], queue="pool")
            s.dma(wbr[:, 0:4, :], wba_d.rearrange("(kc p) n -> p kc n", p=128), writes=["wbr"])
            s.dma(wbr[:, 4:8, :], wbd_d.rearrange("(kc p) n -> p kc n", p=128), writes=["wbr"], queue="act")
            s.dma(wo[:], wout_d.rearrange("(kc p) n -> p kc n", p=128), writes=["wo"])

            def tr8(b, src, sid, nkc, dst_off, dst, did, extra=None):
                pT = [bk[4 * b], bk[4 * b + 1]]
                for kc in range(nkc):
                    q_ = (dst_off + kc) // 4
                    s.op("pe", lambda e, kc=kc, q_=q_: e.transpose(out=pT[q_][:, ((dst_off + kc) % 4) * 128:((dst_off + kc) % 4 + 1) * 128], in_=src[:, kc * 128:(kc + 1) * 128], identity=ident), reads=[sid, "cm"], writes=[B(4 * b + q_)])
                for q_ in sorted(set((dst_off + kc) // 4 for kc in range(nkc))):
                    if q_ == 0:
                        s.op("act", lambda e, q_=q_: e.copy(out=dst[:, q_ * 4:(q_ + 1) * 4, :].rearrange("p a b -> p (a b)"), in_=pT[q_][:]), reads=[B(4 * b + q_)], writes=[(did, q_)])
                    else:
                        s.op("dve", lambda e, q_=q_: e.tensor_copy(out=dst[:, q_ * 4:(q_ + 1) * 4, :].rearrange("p a b -> p (a b)"), in_=pT[q_][:]), reads=[B(4 * b + q_)], writes=[(did, q_)])
                    if extra is not None:
                        d2, d2id = extra
                        if q_ == 0:
                            s.op("dve", lambda e, q_=q_: e.tensor_copy(out=d2[:, q_ * 4:(q_ + 1) * 4, :].rearrange("p a b -> p (a b)"), in_=pT[q_][:]), reads=[B(4 * b + q_)], writes=[(d2id, q_)])
                        else:
                            s.op("act", lambda e, q_=q_: e.copy(out=d2[:, q_ * 4:(q_ + 1) * 4, :].rearrange("p a b -> p (a b)"), in_=pT[q_][:]), reads=[B(4 * b + q_)], writes=[(d2id, q_)])

            def rms6(b, src, sid):
                s.op("act", lambda e: e.activation(out=qsb[b][:], in_=src[:], func=AF.Square, accum_out=ss[b][:]), reads=[sid], writes=[("qsb", b), ("ss", b)])
                s.op("dve", lambda e: e.tensor_scalar(out=rstd[b][:], in0=ss[b][:], scalar1=1.0 / D, scalar2=1e-6, op0=ALU.mult, op1=ALU.add), reads=[("ss", b)], writes=[("rstd", b)])
                s.op("act", lambda e: e.sqrt(out=rstd[b][:], in_=rstd[b][:]), reads=[("rstd", b)], writes=[("rstd", b)])
                s.op("dve", lambda e: e.reciprocal(out=rstd[b][:], in_=rstd[b][:]), reads=[("rstd", b)], writes=[("rstd", b)])

            for i in range(ntl6):
                conv_some(4)
                b = i % 2
                tsl = slice(i * 128, (i + 1) * 128)
                XA = xa[b]; YB = yb[b]; GT = gt[b]; OD = od[b]; ZT = zt[b]; OAT = oat[b]; O2 = o2[b]; QSB = qsb[b]; SSQ = ssq[b]; TC = tcA[b]
                pY = [bk[4 * b + 2], bk[4 * b + 3]]
                s.dma(XA[:], x_d[tsl, :], writes=[("xa", b)])
                s.dma(OD[:, 0, :], OD_s[0, tsl, :], reads=["OD_s"], writes=[("od", b)], queue="act")
                s.dma(OD[:, 1, :], OD_s[1, tsl, :], reads=["OD_s"], writes=[("od", b)], queue="act")
                s.dma(ZT[:], Z_s[tsl, :], reads=["Z_s"], writes=[("zt", b)])
                s.dma(OAT[:], OA_s[tsl, :], reads=["OA_s"], writes=[("oat", b)], queue="act")
                s.dma(GT[:], GT_s[tsl, :], reads=["GT_s"], writes=[("gt", b)])
                s.op("dve", lambda e, OD=OD: e.tensor_tensor(out=OD[:, 0, :], in0=OD[:, 0, :], in1=OD[:, 1, :], op=ALU.add), reads=[("od", b)], writes=[("od", b)])
                s.op("pool", lambda e, OD=OD, O2=O2: e.tensor_tensor(out=O2[:], in0=OD[:, 0, :], in1=OD[:, 0, :], op=ALU.mult), reads=[("od", b)], writes=[("o2", b)])
                s.op("dve", lambda e, O2=O2, SSQ=SSQ: e.tensor_reduce(out=SSQ[:], in_=O2[:].rearrange("p (h d) -> p h d", h=4), axis=AX.X, op=ALU.add), reads=[("o2", b)], writes=[("ssq", b)])
                s.op("dve", lambda e, SSQ=SSQ: e.tensor_scalar(out=SSQ[:], in0=SSQ[:], scalar1=1.0 / 128, scalar2=1e-6, op0=ALU.mult, op1=ALU.add), reads=[("ssq", b)], writes=[("ssq", b)])
                s.op("act", lambda e, SSQ=SSQ: e.sqrt(out=SSQ[:], in_=SSQ[:]), reads=[("ssq", b)], writes=[("ssq", b)])
                s.op("dve", lambda e, SSQ=SSQ: e.reciprocal(out=SSQ[:], in_=SSQ[:]), reads=[("ssq", b)], writes=[("ssq", b)])
                s.op("dve", lambda e, O2=O2, OD=OD, SSQ=SSQ: e.tensor_tensor(out=O2[:].rearrange("p (h d) -> p h d", h=4), in0=OD[:, 0, :].rearrange("p (h d) -> p h d", h=4), in1=SSQ[:].unsqueeze(2).to_broadcast([128, 4, 128]), op=ALU.mult), reads=[("od", b), ("ssq", b)], writes=[("o2", b)])
                s.op("pool", lambda e, O2=O2, ZT=ZT: e.tensor_tensor(out=O2[:], in0=O2[:], in1=ZT[:], op=ALU.mult), reads=[("o2", b), ("zt", b)], writes=[("o2", b)])
                tr8(b, OAT, ("oat", b), 4, 0, TC, ("tcA", b))
                tr8(b, O2, ("o2", b), 4, 4, TC, ("tcA", b))
                for half in range(2):
                    for kc in range(4):
                        s.op("pe", lambda e, half=half, kc=kc, pY=pY, TC=TC: e.matmul(out=pY[half][:], lhsT=TC[:, kc, :], rhs=wbr[:, kc, half * 512:(half + 1) * 512], start=(kc == 0), stop=(kc == 3)), reads=[(("tcA", b), 0), "wbr"], writes=[B(4 * b + 2 + half)])
                    s.op("dve", lambda e, half=half, pY=pY, YB=YB, GT=GT: e.tensor_tensor(out=YB[:, half * 512:(half + 1) * 512], in0=pY[half][:], in1=GT[:, half * 512:(half + 1) * 512], op=ALU.mult), reads=[B(4 * b + 2 + half), ("gt", b)], writes=[("yb", b)])
                for half in range(2):
                    for kc in range(4):
                        s.op("pe", lambda e, half=half, kc=kc, pY=pY, TC=TC: e.matmul(out=pY[half][:], lhsT=TC[:, 4 + kc, :], rhs=wbr[:, 4 + kc, half * 512:(half + 1) * 512], start=(kc == 0), stop=(kc == 3)), reads=[(("tcA", b), 1), "wbr"], writes=[B(4 * b + 2 + half)])
                    s.op("dve", lambda e, half=half, pY=pY, QSB=QSB, GT=GT: e.tensor_tensor(out=QSB[:, half * 512:(half + 1) * 512], in0=pY[half][:], in1=GT[:, 1024 + half * 512:1024 + (half + 1) * 512], op=ALU.mult), reads=[B(4 * b + 2 + half), ("gt", b)], writes=[("qsb", b)])
                s.op("pool", lambda e, YB=YB, QSB=QSB: e.tensor_tensor(out=YB[:], in0=YB[:], in1=QSB[:], op=ALU.add), reads=[("yb", b), ("qsb", b)], writes=[("yb", b)])
                tr8(b, YB, ("yb", b), 8, 0, TC, ("tcA", b))
                for half in range(2):
                    for kc in range(8):
                        s.op("pe", lambda e, half=half, kc=kc, pY=pY, TC=TC: e.matmul(out=pY[half][:], lhsT=TC[:, kc, :], rhs=wo[:, kc, half * 512:(half + 1) * 512], start=(kc == 0), stop=(kc == 7)), reads=[(("tcA", b), 0), (("tcA", b), 1), "wo"], writes=[B(4 * b + 2 + half)])
                    s.op("dve", lambda e, half=half, pY=pY, YB=YB: e.tensor_tensor(out=YB[:, half * 512:(half + 1) * 512], in0=pY[half][:], in1=bvv(BV_GT1)[:, half * 512:(half + 1) * 512], op=ALU.mult), reads=[B(4 * b + 2 + half), "bv"], writes=[("yb", b)])
                s.op("pool", lambda e, XA=XA, YB=YB: e.tensor_tensor(out=XA[:], in0=XA[:], in1=YB[:], op=ALU.add), reads=[("xa", b), ("yb", b)], writes=[("xa", b)])
                s.dma(X1_s[tsl, :], XA[:], reads=[("xa", b)], writes=["X1_s"], queue="act")
                rms6(b, XA, ("xa", b))
                s.op("dve", lambda e, XA=XA, YB=YB, RS=rstd[b]: e.scalar_tensor_tensor(out=YB[:], in0=XA[:], scalar=RS[:, 0:1], in1=bvv(BV_G2), op0=ALU.mult, op1=ALU.mult), reads=[("xa", b), ("rstd", b), "bv"], writes=[("yb", b)])
                s.op("pool", lambda e, YB=YB: e.tensor_tensor(out=YB[:], in0=YB[:], in1=bvv(BV_SH2), op=ALU.add), reads=[("yb", b), "bv"], writes=[("yb", b)])
                tr8(b, YB, ("yb", b), 8, 0, TC, ("tcA", b), extra=(h2r[b], ("h2r", b)))
                s.dma(H2T_s[:, tsl].rearrange("(kc p) t -> p kc t", p=128), TC[:], reads=[(("tcA", b), 0), (("tcA", b), 1)], writes=["H2T_s"])
                s.dma(H2R_s[:, tsl].rearrange("(kc p) t -> p kc t", p=128), h2r[b][:], reads=[(("h2r", b), 0), (("h2r", b), 1)], writes=["H2R_s"], queue="act")
            conv_some(10 ** 6)
            s.flush()
        if upto <= 6:
            return nc

        with contextlib.ExitStack() as st:
            wq = T(st, "wq", [128, 8, D]); keys2 = T(st, "keys2", [128, 8, 128])
            tcB = [T(st, f"tcB{b}", [128, 8, 128]) for b in range(2)]
            qsb = [T(st, f"qsbB{b}", [128, D]) for b in range(2)]; qTs = [T(st, f"qTs{b}", [128, 8, 128]) for b in range(2)]
            sc = [T(st, f"scB{b}", [128, 16, 128]) for b in range(2)]
            t16 = [T(st, f"t16{b}", [128, 8, 2, 16]) for b in range(2)]; c16 = [T(st, f"c16{b}", [128, 8, 16]) for b in range(2)]
            wk4 = T(st, "wk4", [128, 4, 256]); cand4 = T(st, "cand4", [128, 4, 256])
            thr = [T(st, f"thr{b}", [128, 8]) for b in range(2)]; negm = [T(st, f"negm{b}", [128, 8]) for b in range(2)]; Zs = [T(st, f"Zs{b}", [128, 8]) for b in range(2)]
            kap = [T(st, f"kap{b}", [128, 8]) for b in range(2)]; m1 = [T(st, f"m1{b}", [128, 8]) for b in range(2)]; th2 = [T(st, f"th2{b}", [128, 8]) for b in range(2)]
            e16 = [T(st, f"e16{b}", [128, 8, 16]) for b in range(2)]
            bk = [PS(st, f"bkB{i}", [128, 512]) for i in range(8)]
            s.dma(wq[:], pwq_d.rearrange("(kc p) n -> p kc n", p=128), writes=["wq"])
            s.dma(keys2[:], pkeys_d, writes=["keys2"])
            _cb = [int(x[4:]) for x in dbg if x.startswith("cutB")]
            cutB = _cb[0] if _cb else 99
            for i in range(ntl6):
                b = i % 2
                tsl = slice(i * 128, (i + 1) * 128)
                TC = tcB[b]; QSB = qsb[b]; QT = qTs[b]; SC = sc[b]; T16 = t16[b]; C16 = c16[b]
                pT = [bk[4 * b], bk[4 * b + 1]]; pY = [bk[4 * b + 2], bk[4 * b + 3]]
                s.dma(TC[:], H2T_s[:, tsl].rearrange("(kc p) t -> p kc t", p=128), reads=["H2T_s"], writes=[("tcB", b)], queue=("sp", "act")[b])
                for half in range(2):
                    for kc in range(8):
                        s.op("pe", lambda e, half=half, kc=kc, pY=pY, TC=TC: e.matmul(out=pY[half][:], lhsT=TC[:, kc, :], rhs=wq[:, kc, half * 512:(half + 1) * 512], start=(kc == 0), stop=(kc == 7)), reads=[("tcB", b), "wq"], writes=[B(4 * b + 2 + half)])
                    if half == 0:
                        s.op("act", lambda e, pY=pY, QSB=QSB: e.copy(out=QSB[:, 0:512], in_=pY[0][:]), reads=[B(4 * b + 2)], writes=[("qsbB", b)])
                    else:
                        s.op("dve", lambda e, pY=pY, QSB=QSB: e.tensor_copy(out=QSB[:, 512:1024], in_=pY[1][:]), reads=[B(4 * b + 3)], writes=[("qsbB", b)])
                if cutB < 2:
                    continue
                for hh in range(8):
                    s.op("pe", lambda e, hh=hh, pT=pT, QSB=QSB: e.transpose(out=pT[hh // 4][:, (hh % 4) * 128:(hh % 4 + 1) * 128], in_=QSB[:, hh * 128:(hh + 1) * 128], identity=ident), reads=[("qsbB", b), "cm"], writes=[B(4 * b + hh // 4)])
                s.op("act", lambda e, pT=pT, QT=QT: e.copy(out=QT[:, 0:4, :].rearrange("p a b -> p (a b)"), in_=pT[0][:]), reads=[B(4 * b)], writes=[("qTs", b)])
                s.op("dve", lambda e, pT=pT, QT=QT: e.tensor_copy(out=QT[:, 4:8, :].rearrange("p a b -> p (a b)"), in_=pT[1][:]), reads=[B(4 * b + 1)], writes=[("qTs", b)])
                if cutB < 3:
                    continue
                banks = [pY[0], pY[1], pT[0], pT[1]]; bids = [B(4 * b + 2), B(4 * b + 3), B(4 * b), B(4 * b + 1)]
                for p in range(2):
                    for hh in range(8):
                        bi_ = 2 * p + hh // 4
                        s.op("pe", lambda e, hh=hh, p=p, bi_=bi_, QT=QT, banks=banks: e.matmul(out=banks[bi_][:, (hh % 4) * 128:(hh % 4 + 1) * 128], lhsT=QT[64 * p:64 * p + 64, hh, :], rhs=keys2[64 * p:64 * p + 64, hh, :], start=True, stop=True), reads=[("qTs", b), "keys2"], writes=[bids[bi_]])
                SC4 = SC[:].rearrange("p (h q) k -> p h q k", q=2)
                for bi_ in range(4):
                    p, hq = bi_ // 2, bi_ % 2
                    dst = SC4[:, hq * 4:(hq + 1) * 4, p, :]
                    src = banks[bi_][:].rearrange("p (a k) -> p a k", a=4)
                    allq = [("scB", b, q4) for q4 in range(4)]
                    if bi_ % 2 == 0:
                        s.op("act", lambda e, dst=dst, src=src: e.copy(out=dst, in_=src), reads=[bids[bi_]], writes=[("scB", b, 2 * hq), ("scB", b, 2 * hq + 1)])
                    else:
                        s.op("dve", lambda e, dst=dst, src=src: e.tensor_copy(out=dst, in_=src), reads=[bids[bi_]], writes=[("scB", b, 2 * hq), ("scB", b, 2 * hq + 1)])
                if cutB < 4:
                    continue
                for hb in range(2):
                    hs = range(hb * 4, hb * 4 + 4)
                    scid = lambda hh: ("scB", b, hh // 2)
                    for hh in hs:
                        for p in range(2):
                            s.op("dve", lambda e, hh=hh, p=p, SC=SC, T16=T16: e.max(out=T16[:, hh, p, 0:8], in_=SC[:, 2 * hh + p, :]), reads=[scid(hh)], writes=[("t16", b, hh, p)])
                    for hh in hs:
                        for p in range(2):
                            s.op("dve", lambda e, hh=hh, p=p, SC=SC, T16=T16: e.match_replace(out=wk4[:, hh % 4, p * 128:(p + 1) * 128], in_to_replace=T16[:, hh, p, 0:8], in_values=SC[:, 2 * hh + p, :], imm_value=-1e30), reads=[scid(hh), ("t16", b, hh, p)], writes=[("wk4", hh % 4, p)])
                    for hh in hs:
                        for p in range(2):
                            s.op("dve", lambda e, hh=hh, p=p, T16=T16: e.max(out=T16[:, hh, p, 8:16], in_=wk4[:, hh % 4, p * 128:(p + 1) * 128]), reads=[("wk4", hh % 4, p)], writes=[("t16", b, hh, p)])
                    for hh in hs:
                        s.op("dve", lambda e, hh=hh, T16=T16: e.tensor_tensor(out=cand4[:, hh % 4, :].rearrange("p (a c) -> p a c", a=16), in0=T16[:, hh, 0, :].unsqueeze(2).to_broadcast([128, 16, 16]), in1=T16[:, hh, 1, :].unsqueeze(1).to_broadcast([128, 16, 16]), op=ALU.add), reads=[("t16", b, hh, 0), ("t16", b, hh, 1)], writes=[("cand4", hh % 4)])
                    for hh in hs:
                        s.op("dve", lambda e, hh=hh, C16=C16: e.max(out=C16[:, hh, 0:8], in_=cand4[:, hh % 4, :]), reads=[("cand4", hh % 4)], writes=[("c16", b, hh)])
                    for hh in hs:
                        s.op("dve", lambda e, hh=hh, C16=C16: e.match_replace(out=wk4[:, hh % 4, :], in_to_replace=C16[:, hh, 0:8], in_values=cand4[:, hh % 4, :], imm_value=-1e30), reads=[("cand4", hh % 4), ("c16", b, hh)], writes=[("wk4", hh % 4, 0), ("wk4", hh % 4, 1)])
                    for hh in hs:
                        s.op("dve", lambda e, hh=hh, C16=C16: e.max(out=C16[:, hh, 8:16], in_=wk4[:, hh % 4, :]), reads=[("wk4", hh % 4, 0), ("wk4", hh % 4, 1)], writes=[("c16", b, hh)])
                if cutB < 5:
                    continue
                allc = [("c16", b, hh) for hh in range(8)]; allt = [("t16", b, hh, 0) for hh in range(8)]
                THR = thr[b]; NEGM = negm[b]; M1 = m1[b]; ZS = Zs[b]; KAP = kap[b]; TH2 = th2[b]; E16 = e16[b]
                s.op("dve", lambda e, C16=C16, THR=THR: e.tensor_scalar(out=THR[:], in0=C16[:, :, 15], scalar1=-1e-4, scalar2=None, op0=ALU.add), reads=allc, writes=[("thr", b)])
                s.op("dve", lambda e, C16=C16, NEGM=NEGM: e.tensor_scalar(out=NEGM[:], in0=C16[:, :, 0], scalar1=-1.0, scalar2=None, op0=ALU.mult), reads=allc, writes=[("negm", b)])
                s.op("dve", lambda e, T16=T16, M1=M1: e.tensor_copy(out=M1[:], in_=T16[:, :, 0, 0]), reads=allt, writes=[("m1", b)])
                s.op("dve", lambda e, C16=C16, NEGM=NEGM, E16=E16: e.tensor_tensor(out=E16[:], in0=C16[:], in1=NEGM[:].unsqueeze(2).to_broadcast([128, 8, 16]), op=ALU.add), reads=allc + [("negm", b)], writes=[("e16", b)])
                s.op("act", lambda e, E16=E16: e.activation(out=E16[:], in_=E16[:], func=AF.Exp), reads=[("e16", b)], writes=[("e16", b)])
                s.op("dve", lambda e, E16=E16, ZS=ZS: e.tensor_reduce(out=ZS[:], in_=E16[:], axis=AX.X, op=ALU.add), reads=[("e16", b)], writes=[("Zs", b)])
                s.op("dve", lambda e, KAP=KAP, THR=THR, NEGM=NEGM: e.tensor_tensor(out=KAP[:], in0=THR[:], in1=NEGM[:], op=ALU.add), reads=[("thr", b), ("negm", b)], writes=[("kap", b)])
                s.op("act", lambda e, KAP=KAP: e.activation(out=KAP[:], in_=KAP[:], func=AF.Exp), reads=[("kap", b)], writes=[("kap", b)])
                s.op("dve", lambda e, ZS=ZS: e.reciprocal(out=ZS[:], in_=ZS[:]), reads=[("Zs", b)], writes=[("Zs", b)])
                s.op("dve", lambda e, KAP=KAP, ZS=ZS: e.tensor_tensor(out=KAP[:], in0=KAP[:], in1=ZS[:], op=ALU.mult), reads=[("kap", b), ("Zs", b)], writes=[("kap", b)])
                s.op("dve", lambda e, TH2=TH2, THR=THR, M1=M1: e.tensor_tensor(out=TH2[:], in0=THR[:], in1=M1[:], op=ALU.subtract), reads=[("thr", b), ("m1", b)], writes=[("th2", b)])
                sc4 = SC[:].rearrange("p (h q) k -> p h q k", q=2)
                allsc = [("scB", b, q4) for q4 in range(4)]
                s.op("dve", lambda e, sc4=sc4, M1=M1: e.tensor_tensor(out=sc4[:, :, 0, :], in0=sc4[:, :, 0, :], in1=M1[:].unsqueeze(2).to_broadcast([128, 8, 128]), op=ALU.subtract), reads=allsc + [("m1", b)], writes=allsc)
                s.op("pool", lambda e, sc4=sc4, TH2=TH2: e.tensor_tensor(out=sc4[:, :, 1, :], in0=sc4[:, :, 1, :], in1=TH2[:].unsqueeze(2).to_broadcast([128, 8, 128]), op=ALU.subtract), reads=allsc + [("th2", b)], writes=allsc)
                s.op("act", lambda e, SC=SC: e.activation(out=SC[:], in_=SC[:], func=AF.Exp), reads=allsc, writes=allsc)
                s.dma(SC_s[tsl, :], SC[:].rearrange("p a b -> p (a b)"), reads=allsc, writes=["SC_s"], queue="pool")
                s.dma(KAP_s[tsl, :], KAP[:], reads=[("kap", b)], writes=["KAP_s"], queue="pool")
            s.flush()
        if upto <= 7:
            return nc

        GI = 2
        NG = 128 // GI
        NB = 2
        with contextlib.ExitStack() as st:
            _xa = T(st, "xc", [128, D]); xa = [_xa, _xa]; yb = T(st, "ybc", [128, D]); qsb = yb
            ss = T(st, "ssc", [128, 1]); rstd = T(st, "rstdc", [128, 1])
            h2r = [[TR(st, f"h2c{k}{t}", [128, 8, 128]) for t in range(NB)] for k in range(2)]
            sc = [[T(st, f"scc{k}{t}", [128, 16, 128]) for t in range(NB)] for k in range(2)]
            kap = [[T(st, f"kapc{k}{t}", [128, 8]) for t in range(NB)] for k in range(2)]
            dg = [[TR(st, f"dgc{k}{t}", [128, 8, 128]) for t in range(NB)] for k in range(2)]
            UT = [TR(st, f"UT{i}", [128, 8, GI * 128]) for i in range(2)]
            VG = [TR(st, f"VG{i}", [128, GI, D]) for i in range(2)]
            pe_t = [T(st, f"pec{t}", [128, 8, GI * 128]) for t in range(NB)]
            Mr2 = [[TR(st, f"Mr{k}{t}", [128, 8, GI * 128]) for t in range(NB)] for k in range(2)]
            Sm2 = [TR(st, f"Sm{k}", [128, 8, GI * 128]) for k in range(2)]
            negone = T(st, "negone", [128, 1])
            s.op("pool", lambda e: e.memset(negone[:], -1.0), writes=["negone"])
            W5 = NB * GI * 128
            g1 = [T(st, f"g1c{i}", [128, W5]) for i in range(2)]; Pm_ = [T(st, f"Pmc{i}", [128, W5]) for i in range(2)]
            PT = [TR(st, f"PT{i}", [128, NB * GI, 128]) for i in range(2)]
            bk = [PS(st, f"bkC{i}", [128, 512]) for i in range(8)]
            nblk = ntl6 // NB
            ngr = (2 if "small2" in dbg else NG)
            gcount = 0

            def blk_load(blk):
                k = blk % 2
                for tau in range(NB):
                    i = blk * NB + tau
                    tsl = slice(i * 128, (i + 1) * 128)
                    s.dma(h2r[k][tau][:], H2R_s[:, tsl].rearrange("(kc p) t -> p kc t", p=128), reads=["H2R_s"], writes=[("h2c", k, tau)], queue="pool")
                    s.dma(sc[k][tau][:].rearrange("p a b -> p (a b)"), SC_s[tsl, :], reads=["SC_s"], writes=[("scc", k, tau)], queue="pool")
                    s.dma(kap[k][tau][:], KAP_s[tsl, :], reads=["KAP_s"], writes=[("kapc", k, tau)], queue="pool")
                    for hh in range(8):
                        s.op("dve", lambda e, hh=hh, tau=tau, k=k: e.tensor_scalar(out=dg[k][tau][:, hh, :], in0=ident, scalar1=kap[k][tau][:, hh:hh + 1], scalar2=None, op0=ALU.mult), reads=["cm", ("kapc", k, tau)], writes=[("dgc", k, tau)])

            blk_load(0)
            for blk in range(nblk):
              kb = blk % 2
              pU = [[bk[4 + 2 * t + hf] for hf in range(2)] for t in range(NB)]
              ub = [k % 2 for k in range(ngr + 4)]

              def g_utload(g):
                  u = g % 2; ub[g] = u
                  e0 = g * GI * 128
                  s.dma(UT[u][:], UTr_s[:, e0:e0 + GI * 128].rearrange("(kc p) n -> p kc n", p=128), reads=["UTr_s"], writes=[("UT", u)], queue="sp")

              def g_vgload(g):
                  u = g % 2
                  e0 = g * GI * 128
                  s.dma(VG[u][:], Vr_s[e0:e0 + GI * 128, :].rearrange("(a p) n -> p a n", p=128), reads=["Vr_s"], writes=[("VG", u)], queue="act")

              def g_prodmask(g):
                  mk = g % 2
                  for tau in range(NB):
                      sc4 = sc[kb][tau][:].rearrange("p (h q) k -> p h q k", q=2)
                      e1b = sc4[:, :, 0, g * GI:(g + 1) * GI].unsqueeze(3).to_broadcast([128, 8, GI, 128])
                      e2b = sc4[:, :, 1, :].unsqueeze(2).to_broadcast([128, 8, GI, 128])
                      s.op("dve", lambda e, e1b=e1b, e2b=e2b, tau=tau: e.tensor_tensor(out=pe_t[tau][:].rearrange("p h (a k) -> p h a k", a=GI), in0=e1b, in1=e2b, op=ALU.mult), reads=[("scc", kb, tau)], writes=[("pe", tau)])
                      if tau == 0:
                          s.op("dve", lambda e, mk=mk: e.scalar_tensor_tensor(out=Mr2[mk][0][:], in0=pe_t[0][:], scalar=1.0, in1=pe_t[0][:], op0=ALU.is_ge, op1=ALU.mult), reads=[("pe", 0)], writes=[("Mr", mk, 0)])
                      else:
                          s.op("act", lambda e, mk=mk: e.activation(out=Mr2[mk][1][:], in_=pe_t[1][:], func=AF.Relu, bias=negone[:, 0:1], scale=1.0), reads=[("pe", 1), "negone"], writes=[("Mr", mk, 1)])
                          s.op("act", lambda e, mk=mk: e.activation(out=Sm2[mk][:], in_=Mr2[mk][1][:].bitcast(F32), func=AF.Sign), reads=[("Mr", mk, 1)], writes=[("Sm", mk)])

              def g_act(g):
                  u = ub[g]; pR = bk[u]
                  for tau in range(NB):
                      for kc in range(8):
                          s.op("pe", lambda e, kc=kc, u=u, tau=tau, pR=pR, H=h2r[kb][tau]: e.matmul(out=pR[:, tau * GI * 128:(tau + 1) * GI * 128], lhsT=H[:, kc, :], rhs=UT[u][:, kc, :], start=(kc == 0), stop=(kc == 7)), reads=[("h2c", kb, tau), ("UT", u)], writes=[B(u)])

              def g_gelu(g):
                  u = ub[g]; pR = bk[u]
                  s.op("act", lambda e, pR=pR, u=u: e.activation(out=g1[u][:], in_=pR[:, 0:W5], func=AF.Gelu_apprx_tanh), reads=[B(u)], writes=[("g1", u)])

              def g_gd(g):
                  u = ub[g]; pG = bk[2 + u]; mk = g % 2
                  for hh in range(8):
                      s.op("pe", lambda e, hh=hh, pG=pG, DG=dg[kb][0], M=Mr2[mk][0]: e.matmul(out=pG[:, 0:GI * 128], lhsT=DG[:, hh, :], rhs=M[:, hh, :], start=(hh == 0), stop=(hh == 7)), reads=[("dgc", kb, 0), ("Mr", mk, 0)], writes=[B(2 + u)])
                  for hh in range(8):
                      s.op("pe", lambda e, hh=hh, pG=pG, DG=dg[kb][1], M=Mr2[mk][1]: e.matmul(out=pG[:, GI * 128:2 * GI * 128], lhsT=DG[:, hh, :], rhs=M[:, hh, :], start=(hh == 0), stop=False), reads=[("dgc", kb, 1), ("Mr", mk, 1)], writes=[B(2 + u)])
                  for hh in range(8):
                      s.op("pe", lambda e, hh=hh, pG=pG, DG=dg[kb][1], M=Sm2[mk]: e.matmul(out=pG[:, GI * 128:2 * GI * 128], lhsT=DG[:, hh, :], rhs=M[:, hh, :], start=False, stop=(hh == 7)), reads=[("dgc", kb, 1), ("Sm", mk)], writes=[B(2 + u)])

              def g_pm(g):
                  u = ub[g]; pG = bk[2 + u]
                  s.op("dve", lambda e, u=u, pG=pG: e.tensor_tensor(out=Pm_[u][:], in0=g1[u][:], in1=pG[:, 0:W5], op=ALU.mult), reads=[("g1", u), B(2 + u)], writes=[("Pm", u)])

              def g_tr(g):
                  u = ub[g]; pW = bk[2 + u]
                  for k in range(NB * GI):
                      s.op("pe", lambda e, k=k, u=u, pW=pW: e.transpose(out=pW[:, k * 128:(k + 1) * 128], in_=Pm_[u][:, k * 128:(k + 1) * 128], identity=ident), reads=[("Pm", u), "cm"], writes=[B(2 + u)])
                  s.op("act", lambda e, u=u, pW=pW: e.copy(out=PT[u][:].rearrange("p a b -> p (a b)"), in_=pW[:, 0:W5]), reads=[B(2 + u)], writes=[("PT", u)])

              def g_out(g):
                  u = ub[g]
                  for tau in range(NB):
                      for a in range(GI):
                          for half in range(2):
                              s.op("pe", lambda e, a=a, half=half, u=u, tau=tau, first=(g == 0 and a == 0), last=(g == ngr - 1 and a == GI - 1): e.matmul(out=pU[tau][half][:], lhsT=PT[u][:, tau * GI + a, :], rhs=VG[u][:, a, half * 512:(half + 1) * 512], start=first, stop=last),
                                   reads=[("PT", u), ("VG", u)], writes=[B(4 + 2 * tau + half)])

              ok = lambda k: 0 <= k < ngr
              for g in range(-3, ngr + 1):
                  if ok(g + 3):
                      g_utload(g + 3)
                  if ok(g - 1):
                      g_out(g - 1)
                  if ok(g + 1):
                      g_vgload(g + 1)
                      g_gd(g + 1)
                  if ok(g):
                      g_tr(g)
                  if ok(g + 2):
                      g_act(g + 2); g_gelu(g + 2)
                  if ok(g + 3):
                      g_prodmask(g + 3)
                  if ok(g + 1):
                      g_pm(g + 1)
                  if g == 4 and blk + 1 < nblk:
                      blk_load(blk + 1)
              for tau in range(NB):
                i = blk * NB + tau
                XA = xa[tau]; xid = "xc"
                s.dma(XA[:], X1_s[i * 128:(i + 1) * 128, :], reads=["X1_s"], writes=[xid], queue="pool")
                for half in range(2):
                    s.op("dve", lambda e, half=half, tau=tau: e.tensor_tensor(out=yb[:, half * 512:(half + 1) * 512], in0=pU[tau][half][:], in1=bvv(BV_GT2)[:, half * 512:(half + 1) * 512], op=ALU.mult), reads=[B(4 + 2 * tau + half), "bv"], writes=["ybc"])
                s.op("pool", lambda e, XA=XA: e.tensor_tensor(out=XA[:], in0=XA[:], in1=yb[:], op=ALU.add), reads=[xid, "ybc"], writes=[xid])
                s.op("act", lambda e, XA=XA: e.activation(out=qsb[:], in_=XA[:], func=AF.Square, accum_out=ss[:]), reads=[xid, "ybc"], writes=["ybc", "ssc"])
                s.op("dve", lambda e: e.tensor_scalar(out=rstd[:], in0=ss[:], scalar1=1.0 / D, scalar2=1e-6, op0=ALU.mult, op1=ALU.add), reads=["ssc"], writes=["rstdc"])
                s.op("act", lambda e: e.sqrt(out=rstd[:], in_=rstd[:]), reads=["rstdc"], writes=["rstdc"])
                s.op("dve", lambda e: e.reciprocal(out=rstd[:], in_=rstd[:]), reads=["rstdc"], writes=["rstdc"])
                s.op("dve", lambda e, XA=XA: e.scalar_tensor_tensor(out=yb[:], in0=XA[:], scalar=rstd[:, 0:1], in1=bvv(BV_FN), op0=ALU.mult, op1=ALU.mult), reads=[xid, "rstdc", "bv"], writes=["ybc"])
                s.dma(out_d[i * 128:(i + 1) * 128, :], yb[:], reads=["ybc"], writes=["out"], queue="pool")
            s.flush()
        return nc


def _rope(s, src, dst, tmp, R, rid, H, sid, did, tid="tmp"):
    sv = src.rearrange("p (h a b c) -> p h a b c", h=H, a=2, b=2)
    dv = dst.rearrange("p (h a b c) -> p h a b c", h=H, a=2, b=2)
    tv = tmp[:, 0:H * 32].rearrange("p (h a c) -> p h a c", h=H, a=2)
    rv = R[:].rearrange("p (a b c) -> p a b c", a=2, b=2)
    cosb = rv[:, :, 0, :].unsqueeze(1).to_broadcast([128, H, 2, 16])
    sinb = rv[:, :, 1, :].unsqueeze(1).to_broadcast([128, H, 2, 16])
    x1 = sv[:, :, :, 0, :]; x2 = sv[:, :, :, 1, :]
    o1 = dv[:, :, :, 0, :]; o2 = dv[:, :, :, 1, :]
    s.op("dve", lambda e: e.tensor_tensor(out=o1, in0=x1, in1=cosb, op=ALU.mult), reads=[sid, rid], writes=[did])
    s.op("dve", lambda e: e.tensor_tensor(out=tv, in0=x2, in1=sinb, op=ALU.mult), reads=[sid, rid], writes=[tid])
    s.op("dve", lambda e: e.tensor_tensor(out=o1, in0=o1, in1=tv, op=ALU.subtract), reads=[did, tid], writes=[did])
    s.op("dve", lambda e: e.tensor_tensor(out=o2, in0=x1, in1=sinb, op=ALU.mult), reads=[sid, rid, did], writes=[did])
    s.op("dve", lambda e: e.tensor_tensor(out=tv, in0=x2, in1=cosb, op=ALU.mult), reads=[sid, rid, did], writes=[tid])
    s.op("dve", lambda e: e.tensor_tensor(out=o2, in0=o2, in1=tv, op=ALU.add), reads=[did, tid], writes=[did])


def _host_inputs(inputs, b, consts):
    g = lambda k: np.ascontiguousarray(inputs[k], dtype=np.float32)
    m = {
        "x": g("x")[b], "c": g("c")[b], "ctx": g("ctx")[b], "c_ctx": g("c_ctx"),
        "w_ada": g("w_ada")[0], "b_ada": g("b_ada")[0], "norm_mix": g("norm_mix")[0], "norm_ffn": g("norm_ffn")[0],
        "w_in": g("w_in")[0], "b_gate": g("b_gate")[0], "attn_sink": g("attn_sink")[0], "dn_conv": g("dn_conv")[0],
        "dn_a_log_f": g("dn_a_log_f")[0], "dn_dt_bias_f": g("dn_dt_bias_f")[0], "dn_a_log_b": g("dn_a_log_b")[0], "dn_dt_bias_b": g("dn_dt_bias_b")[0],
        "dn_norm": g("dn_norm")[0], "w_br_attn": g("w_br_attn")[0], "w_br_dn": g("w_br_dn")[0], "w_out": g("w_out")[0],
        "peer_wq": g("peer_wq")[0], "final_norm": g("final_norm"),
    }
    m.update(consts)
    return {k: np.ascontiguousarray(v) for k, v in m.items()}


_SHARED = {}


def kernel(**inputs):
    consts = _consts()
    nc = build()
    keysT = np.ascontiguousarray(np.transpose(np.asarray(inputs["peer_keys"], np.float32)[0], (1, 3, 0, 2)).reshape(128, 8, 128))
    uT = np.ascontiguousarray(np.asarray(inputs["peer_u"], np.float32)[0].T)
    pv = np.ascontiguousarray(np.asarray(inputs["peer_v"], np.float32)[0])
    in_maps = []
    for b in range(8):
        m = _host_inputs(inputs, b, consts)
        m["peer_keysT"] = keysT; m["peer_uT"] = uT; m["peer_v"] = pv
        in_maps.append(m)
    res = run_bass_kernel_spmd(nc, in_maps, core_ids=list(range(8)))
    return np.stack([np.asarray(r["out"], dtype=np.float32) for r in res.results], axis=0)
```

```python
import contextlib
import numpy as np
import concourse.bass as bass
import concourse.mybir as mybir
from concourse.bass_utils import run_bass_kernel_spmd

F32 = mybir.dt.float32
F32R = mybir.dt.float32r
ALU = mybir.AluOpType
AF = mybir.ActivationFunctionType
AX = mybir.AxisListType

D = 1024
S = 8192
CTX = 256
TALL = CTX + S
NT = S // 128
IN_COLS = 4880
NEG = -30000.0


class _Ins:
    __slots__ = ("eng", "fn", "deps", "signal", "sig_no", "dma", "idx")

    def __init__(self, eng, fn, dma=None):
        self.eng = eng
        self.fn = fn
        self.deps = []
        self.signal = False
        self.sig_no = None
        self.dma = dma
        self.idx = None


class Sch:
    EPOCH = 20000
    NDMA = 24
    NEP = 16

    def __init__(self, nc, st):
        self.nc = nc
        self.engs = ("pe", "act", "dve", "pool", "sp")
        self.nep = {"pe": 12, "act": 4, "dve": 6, "pool": 3, "sp": 1}
        self.sems = {e: [st.enter_context(nc.semaphore(f"s_{e}_{i}")) for i in range(self.nep[e])] for e in self.engs}
        self.dsems = [st.enter_context(nc.semaphore(f"s_dma_{i}")) for i in range(self.NDMA)]
        self.sigc = {e: 0 for e in self.engs}
        self.dma_rr = 0
        self.dma_cnt = [0] * self.NDMA
        self.dma_last = [None] * self.NDMA
        self._reset()

    def _reset(self):
        self.q = {e: [] for e in self.engs}
        self.lastw = {}
        self.readers = {}

    def _add(self, ins, reads, writes):
        q = self.q[ins.eng]
        ins.idx = len(q)
        deps = []
        for r in reads:
            w = self.lastw.get(r)
            if w is not None:
                deps.append((w, "raw"))
        for w_ in writes:
            w = self.lastw.get(w_)
            if w is not None:
                deps.append((w, "waw"))
            for rd in self.readers.get(w_, ()):
                deps.append((rd, "war"))
        for d, kind in deps:
            if d is ins:
                continue
            if d.dma is None and ins.dma is None and d.eng == ins.eng:
                if ins.eng == "pe":
                    continue
                if kind != "raw":
                    continue
            ins.deps.append(d)
            if d.dma is None:
                d.signal = True
        for r in reads:
            self.readers.setdefault(r, []).append(ins)
        for w_ in writes:
            self.lastw[w_] = ins
            self.readers[w_] = []
        q.append(ins)
        return ins

    PSUM_NAMES = {"bk", "pm", "pT", "pY", "pX", "pN", "pK", "pb", "pS", "pO", "pQ", "pZ", "pR", "pU", "pW"}

    def op(self, eng, fn, reads=(), writes=()):
        writes = list(writes)
        if eng != "pe":
            for r in reads:
                if isinstance(r, tuple) and r[0] in self.PSUM_NAMES and r not in writes:
                    writes.append(r)
        return self._add(_Ins(eng, fn), list(reads), writes)

    def dma(self, out, in_, reads=(), writes=(), queue="sp", **kw):
        slot = self.dma_rr
        self.dma_rr = (self.dma_rr + 1) % self.NDMA
        self.dma_cnt[slot] += 1
        n = self.dma_cnt[slot]
        ins = _Ins(queue, lambda e: e.dma_start(out=out, in_=in_, **kw), dma=(slot, n))
        prev = self.dma_last[slot]
        self._add(ins, list(reads), list(writes))
        if prev is not None:
            ins.deps.append(prev)
        self.dma_last[slot] = ins
        return ins

    def flush(self):
        nc = self.nc
        for e, q in self.q.items():
            for ins in q:
                if ins.dma is None and ins.signal:
                    ins.sig_no = self.sigc[e]
                    self.sigc[e] += 1
            assert self.sigc[e] < self.EPOCH * self.nep[e], f"too many signals on {e}: {self.sigc[e]}"
        dma_final = list(self.dma_cnt)
        with nc.Block() as block:
            def run(ename):
                def body(eng):
                    seen_c = {}
                    seen_d = {}
                    for ins in self.q[ename]:
                        wc = {}
                        wd = {}
                        for d in ins.deps:
                            if d.dma is None:
                                if d.sig_no is None:
                                    continue
                                if seen_c.get(d.eng, -1) < d.sig_no:
                                    wc[d.eng] = max(wc.get(d.eng, -1), d.sig_no)
                            else:
                                s_, n = d.dma
                                if seen_d.get(s_, 0) < n:
                                    wd[s_] = max(wd.get(s_, 0), n)
                        for e2, sn in wc.items():
                            eng.wait_ge(self.sems[e2][sn // self.EPOCH], sn % self.EPOCH + 1)
                            seen_c[e2] = sn
                        for s_, n in wd.items():
                            eng.wait_ge(self.dsems[s_], 16 * n)
                            seen_d[s_] = n
                        h = ins.fn(eng)
                        if ins.dma is not None:
                            h.then_inc(self.dsems[ins.dma[0]], 16)
                        elif ins.signal:
                            h.then_inc(self.sems[ename][ins.sig_no // self.EPOCH], 1)
                    if ename == "sp":
                        for s_, n in enumerate(dma_final):
                            if n > 0:
                                eng.wait_ge(self.dsems[s_], 16 * n)
                return body

            block.sync(run("sp"))
            block.tensor(run("pe"))
            block.scalar(run("act"))
            block.vector(run("dve"))
            block.gpsimd(run("pool"))
        nc.all_engine_barrier()
        self._reset()


def _consts():
    c = {}
    ident = np.eye(128, dtype=np.float32)
    ones = np.ones((128, 128), np.float32)
    idx = np.arange(128)
    same = (idx[:, None] // 64 == idx[None, :] // 64).astype(np.float32)
    m1f = ((idx[:, None] <= idx[None, :]) * same).astype(np.float32)
    m1b = ((idx[:, None] >= idx[None, :]) * same).astype(np.float32)
    sel0 = np.zeros((128, 128), np.float32); sel0[:64, :] = 1
    sel1 = np.zeros((128, 128), np.float32); sel1[64:, :] = 1
    low_incl = ((idx[None, :] <= idx[:, None]) * same)
    up_incl = ((idx[None, :] >= idx[:, None]) * same)
    low_strict = ((idx[None, :] < idx[:, None]) * same)
    up_strict = ((idx[None, :] > idx[:, None]) * same)
    negmask = lambda m: np.where(m > 0, 0.0, NEG).astype(np.float32)
    w_prev = (idx[None, :] <= idx[:, None]).astype(np.float32)
    w_next = (idx[:, None] <= idx[None, :]).astype(np.float32)
    mats = [ident, ones, same, m1f, -m1f, m1b, -m1b, sel0, sel1,
            negmask(low_incl), negmask(up_incl), -low_strict.astype(np.float32), -up_strict.astype(np.float32),
            w_prev, w_next]
    c["cmat"] = np.ascontiguousarray(np.stack(mats, axis=1)).astype(np.float32)
    pos = np.arange(S)
    inv = (10000.0 ** (-np.arange(16, dtype=np.float32) / 16)).astype(np.float32)
    ar = (pos // 64).astype(np.float32)[:, None] * inv[None, :]
    ac = (pos % 64).astype(np.float32)[:, None] * inv[None, :]
    c["rope"] = np.concatenate([np.cos(ar), np.sin(ar), np.cos(ac), np.sin(ac)], axis=1).astype(np.float32)
    return c

(C_ID, C_ONES, C_SAME, C_M1F, C_NM1F, C_M1B, C_NM1B, C_SEL0, C_SEL1, C_NLOW, C_NUP, C_SLOW, C_SUP, C_WPREV, C_WNEXT) = range(15)


def build(upto=99, dbg=()):
    nc = bass.Bass("TRN2", target_bir_lowering=False)
    nc.dge_precook = False
    inp = lambda name, shape: nc.dram_tensor(name, list(shape), F32, kind="ExternalInput").ap()
    x_d = inp("x", [S, D]); c_d = inp("c", [D]); ctx_d = inp("ctx", [CTX, D]); cctx_d = inp("c_ctx", [D])
    wada_d = inp("w_ada", [D, 6 * D]); bada_d = inp("b_ada", [6 * D])
    nmix_d = inp("norm_mix", [D]); nffn_d = inp("norm_ffn", [D])
    win_d = inp("w_in", [D, IN_COLS]); bgate_d = inp("b_gate", [2 * D])
    sink_d = inp("attn_sink", [8]); conv_d = inp("dn_conv", [5, 1536])
    alf_d = inp("dn_a_log_f", [4]); dtf_d = inp("dn_dt_bias_f", [4]); alb_d = inp("dn_a_log_b", [4]); dtb_d = inp("dn_dt_bias_b", [4])
    dnn_d = inp("dn_norm", [128]); wba_d = inp("w_br_attn", [512, D]); wbd_d = inp("w_br_dn", [512, D]); wout_d = inp("w_out", [D, D])
    pwq_d = inp("peer_wq", [D, D]); pkeys_d = inp("peer_keysT", [128, 8, 128]); pu_d = inp("peer_uT", [D, 16384]); pv_d = inp("peer_v", [16384, D])
    fnorm_d = inp("final_norm", [D]); cmat_d = inp("cmat", [128, 15, 128]); rope_d = inp("rope", [S, 64])
    out_d = nc.dram_tensor("out", [S, D], F32, kind="ExternalOutput").ap()
    scr = lambda name, shape: nc.dram_tensor(name, list(shape), F32, kind=("ExternalOutput" if name in dbg else "Internal")).ap()
    QT_s = scr("QT_s", [64, 8, S])
    KT_s = scr("KT_s", [64, 2, TALL])
    V_s = scr("V_s", [TALL, 2, 65])
    RT_s = scr("RT_s", [1536, TALL])
    Z_s = scr("Z_s", [S, 512])
    GB_s = scr("GB_s", [TALL, 16])
    GT_s = scr("GT_s", [S, 2048])
    QK_s = scr("QK_s", [1024, TALL])
    KV_s = scr("KV_s", [TALL, 1024])
    OD_s = scr("OD_s", [2, S, 512])
    OA_s = scr("OA_s", [S, 512])
    MOD_s = scr("MOD_s", [8, D])

    with contextlib.ExitStack() as gst:
        s = Sch(nc, gst)
        _uid = [0]

        def _nm(name):
            _uid[0] += 1
            return f"{name}_u{_uid[0]}"
        T = lambda st, name, shape: st.enter_context(nc.sbuf_tensor(_nm(name), list(shape), F32))
        PS = lambda st, name, shape: st.enter_context(nc.psum_tensor(_nm(name), list(shape), F32))
        cm = T(gst, "cm", [128, 15, 128])
        s.dma(cm[:], cmat_d, writes=["cm"])
        ident = cm[:, C_ID, :]
        BV_G1, BV_SH1, BV_GT1, BV_G2, BV_SH2, BV_GT2, BV_CG1, BV_CSH1, BV_FN = range(9)
        bvB = T(gst, "bvB", [128, 5, D])
        stA = contextlib.ExitStack()
        bvA = T(stA, "bvA", [128, 4, D])
        _amap = {BV_G1: 0, BV_SH1: 1, BV_CG1: 2, BV_CSH1: 3}
        _bmap = {BV_GT1: 0, BV_G2: 1, BV_SH2: 2, BV_GT2: 3, BV_FN: 4}

        def bvv(k):
            return bvA[:, _amap[k], :] if k in _amap else bvB[:, _bmap[k], :]

        with contextlib.ExitStack() as st:
            cc = T(st, "cc", [128, 2, 8]); cs = T(st, "cs", [128, 2, 8]); lh = T(st, "lh", [128, 2, 8, 128])
            wa = [T(st, f"wa{i}", [128, 8, 512]) for i in range(2)]
            bb = T(st, "bb", [128, 6 * D]); nm = T(st, "nm", [128, 2, D])
            pm = [PS(st, f"pm{i}", [128, 512]) for i in range(2)]
            s.dma(cc[:, 0, :], c_d.rearrange("(kc p) -> p kc", p=128), writes=["cc"], allow_slow_non_contiguous=True)
            s.dma(cc[:, 1, :], cctx_d.rearrange("(kc p) -> p kc", p=128), writes=["cc"], allow_slow_non_contiguous=True)
            s.dma(bb[:], bada_d.partition_broadcast(128), writes=["bb"])
            s.dma(nm[:, 0, :], nmix_d.partition_broadcast(128), writes=["nm"])
            s.dma(nm[:, 1, :], nffn_d.partition_broadcast(128), writes=["nm"])
            s.dma(bvv(BV_FN)[:, :], fnorm_d.partition_broadcast(128), writes=["bv"])
            s.op("act", lambda e: e.activation(out=cs[:], in_=cc[:], func=AF.Silu), reads=["cc"], writes=["cs"])
            s.op("dve", lambda e: e.tensor_copy(out=lh[:], in_=cs[:].unsqueeze(3).to_broadcast([128, 2, 8, 128])), reads=["cs"], writes=["lh"])
            jobs = [(0, nb) for nb in range(12)] + [(1, nb) for nb in range(4)]
            for ji, (w, nb) in enumerate(jobs):
                wt = wa[ji % 2]; p = pm[ji % 2]
                s.dma(wt[:], wada_d[:, nb * 512:(nb + 1) * 512].rearrange("(kc p) n -> p kc n", p=128), writes=[("wa", ji % 2)], queue=("sp" if ji % 2 == 0 else "act"))
                for kc in range(8):
                    s.op("pe", lambda e, w=w, kc=kc, wt=wt, p=p: e.matmul(out=p[:], lhsT=lh[:, w, kc, :], rhs=wt[:, kc, :], start=(kc == 0), stop=(kc == 7)),
                         reads=["lh", ("wa", ji % 2)], writes=[("pm", ji % 2)])
                ch, half = nb // 2, nb % 2
                if w == 0:
                    dst = {0: BV_SH1, 1: BV_G1, 2: BV_GT1, 3: BV_SH2, 4: BV_G2, 5: BV_GT2}[ch]
                else:
                    dst = {0: BV_CSH1, 1: BV_CG1}[ch]
                o = bvv(dst)[:, half * 512:(half + 1) * 512]
                s.op("dve", lambda e, o=o, p=p, nb=nb: e.tensor_tensor(out=o, in0=p[:], in1=bb[:, nb * 512:(nb + 1) * 512], op=ALU.add),
                     reads=[("pm", ji % 2), "bb"], writes=["bv"])
            for dst, ni in ((BV_G1, 0), (BV_G2, 1), (BV_CG1, 0)):
                s.op("dve", lambda e, dst=dst, ni=ni: e.scalar_tensor_tensor(out=bvv(dst)[:, :], in0=bvv(dst)[:, :], scalar=1.0, in1=nm[:, ni, :], op0=ALU.add, op1=ALU.mult),
                     reads=["bv", "nm"], writes=["bv"])
            s.flush()
        if upto <= 0:
            stA.close()
            return nc

        blocks = [(0, 512), (512, 256), (768, 512), (1280, 512), (1792, 512), (2304, 512), (2816, 16)] + [(2832 + 512 * i, 512) for i in range(4)]
        with contextlib.ExitStack() as st:
            xt = [T(st, f"xt{i}", [128, D]) for i in range(2)]
            junk = T(st, "junk", [128, D]); ss = T(st, "ss", [128, 1]); rstd = T(st, "rstd", [128, 1])
            h = T(st, "h", [128, D]); hT = T(st, "hT", [128, 8, 128])
            wb = [st.enter_context(nc.sbuf_tensor(_nm(f"wb{i}"), [128, 8, 512], F32R)) for i in range(3)]
            rp = [T(st, f"rp{i}", [128, 64]) for i in range(2)]
            qs = T(st, "qs", [128, 512]); qr = T(st, "qr", [128, 512]); tmp = T(st, "tmp", [128, 512])
            qT = T(st, "qT", [64, 8, 128]); kvs = T(st, "kvs", [128, 256]); kr = T(st, "kr", [128, 128]); kT = T(st, "kT", [64, 2, 128])
            va = T(st, "va", [128, 2, 65]); rw = T(st, "rw", [128, 512]); rT = T(st, "rT", [128, 4, 128])
            zz = T(st, "zz", [128, 512]); gn = T(st, "gn", [128, 128]); ab = T(st, "ab", [128, 16]); abc = T(st, "abc", [128, 2, 8])
            gbo = T(st, "gbo", [128, 16]); gg = T(st, "gg", [128, 512]); bg = T(st, "bg", [128, 2048])
            pT = [PS(st, f"pT{i}", [128, 512]) for i in range(2)]
            pY = [PS(st, f"pY{i}", [128, 512]) for i in range(3)]
            pX = [PS(st, f"pX{i}", [128, 512]) for i in range(2)]
            s.dma(bg[:], bgate_d.partition_broadcast(128), writes=["bg"])
            s.dma(gn[:], dnn_d.partition_broadcast(128), writes=["gn"])
            s.dma(abc[:, 0, 0:4], dtf_d.partition_broadcast(128), writes=["abc"])
            s.dma(abc[:, 0, 4:8], dtb_d.partition_broadcast(128), writes=["abc"])
            s.dma(abc[:, 1, 0:4], alf_d.partition_broadcast(128), writes=["abc"])
            s.dma(abc[:, 1, 4:8], alb_d.partition_broadcast(128), writes=["abc"])
            s.op("act", lambda e: e.activation(out=abc[:, 1, :], in_=abc[:, 1, :], func=AF.Exp), reads=["abc"], writes=["abc"])
            s.op("dve", lambda e: e.tensor_scalar(out=abc[:, 1, :], in0=abc[:, 1, :], scalar1=-1.0, scalar2=None, op0=ALU.mult), reads=["abc"], writes=["abc"])
            s.op("pool", lambda e: e.memset(va[:], 1.0), writes=[("va", 0)])
            wcount = [0]

            def rope_ops(src, dst, H):
                sv = src.rearrange("p (h a b c) -> p h a b c", h=H, a=2, b=2)
                dv = dst.rearrange("p (h a b c) -> p h a b c", h=H, a=2, b=2)
                tv = tmp[:, 0:H * 64].rearrange("p (h a b c) -> p h a b c", h=H, a=2, b=2)
                return sv, dv, tv

            tiles = [("c", i) for i in range(CTX // 128)] + [("l", i) for i in range(NT)]
            if upto == 1 and "small" in dbg:
                tiles = tiles[:4]
            qs2 = [qs, T(st, "qsb_", [128, 512])]; qr2 = [qr, T(st, "qrb_", [128, 512])]; tmp2 = [tmp, T(st, "tmpb_", [128, 512])]
            qT2 = [qT, T(st, "qTb_", [64, 8, 128])]; kvs2 = [kvs, T(st, "kvsb_", [128, 256])]; kr2 = [kr, T(st, "krb_", [128, 128])]; kT2 = [kT, T(st, "kTb_", [64, 2, 128])]
            va2 = [va, T(st, "vab_", [128, 2, 65])]; rw2 = [rw, T(st, "rwb_", [128, 512])]; rT2 = [rT, T(st, "rTb_", [128, 4, 128])]; zz2 = [zz, T(st, "zzb_", [128, 512])]
            ab2 = [ab, T(st, "abb_", [128, 16])]; gbo2 = [gbo, T(st, "gbob_", [128, 16])]; gg2 = [gg, T(st, "ggb_", [128, 512])]
            s.op("pool", lambda e: e.memset(va2[1][:], 1.0), writes=[("va", 1)])
            hT4 = [st.enter_context(nc.sbuf_tensor(_nm(f"hT4_{j}"), [128, 8, 128], F32R)) for j in range(4)]
            rp4 = [T(st, f"rp4_{j}", [128, 64]) for j in range(4)]
            pcount = [0]

            def prep(ti, kind, i, j):
                lat = kind == "l"
                src = x_d if lat else ctx_d
                tg = ti
                X = xt[ti % 2]; xid = ("xt", ti % 2)
                s.dma(X[:], src[i * 128:(i + 1) * 128, :], writes=[xid])
                if lat:
                    R = rp4[j]; rid = ("rp", j)
                    s.dma(R[:], rope_d[i * 128:(i + 1) * 128, :], writes=[rid], queue="act")
                s.op("act", lambda e, X=X: e.activation(out=junk[:], in_=X[:], func=AF.Square, accum_out=ss[:]), reads=[xid], writes=["junk", "ss"])
                s.op("dve", lambda e: e.tensor_scalar(out=rstd[:], in0=ss[:], scalar1=1.0 / D, scalar2=1e-6, op0=ALU.mult, op1=ALU.add), reads=["ss"], writes=["rstd"])
                s.op("act", lambda e: e.sqrt(out=rstd[:], in_=rstd[:]), reads=["rstd"], writes=["rstd"])
                s.op("dve", lambda e: e.reciprocal(out=rstd[:], in_=rstd[:]), reads=["rstd"], writes=["rstd"])
                G = BV_G1 if lat else BV_CG1
                SH = BV_SH1 if lat else BV_CSH1
                s.op("dve", lambda e, X=X, G=G: e.scalar_tensor_tensor(out=h[:], in0=X[:], scalar=rstd[:, 0:1], in1=bvv(G)[:, :], op0=ALU.mult, op1=ALU.mult), reads=[xid, "rstd", "bv"], writes=["h"])
                s.op("pool", lambda e, SH=SH: e.tensor_tensor(out=h[:], in0=h[:], in1=bvv(SH)[:, :], op=ALU.add), reads=["h", "bv"], writes=["h"])
                for hb in range(2):
                    for k4 in range(4):
                        kc = hb * 4 + k4
                        s.op("pe", lambda e, kc=kc, hb=hb, k4=k4: e.transpose(out=pT[hb][:, k4 * 128:(k4 + 1) * 128], in_=h[:, kc * 128:(kc + 1) * 128], identity=ident), reads=["h", "cm"], writes=[("pT", hb)])
                    eng = "act" if hb == 0 else "dve"
                    if eng == "act":
                        s.op("act", lambda e, hb=hb: e.copy(out=hT4[j][:, hb * 4:(hb + 1) * 4, :].rearrange("p a b -> p (a b)"), in_=pT[hb][:]), reads=[("pT", hb)], writes=[("hT", j, hb)])
                    else:
                        s.op("dve", lambda e, hb=hb: e.tensor_copy(out=hT4[j][:, hb * 4:(hb + 1) * 4, :].rearrange("p a b -> p (a b)"), in_=pT[hb][:]), reads=[("pT", hb)], writes=[("hT", j, hb)])

            def proj(ti, kind, i, j, bi, W, wi):
                lat = kind == "l"; tg = ti; R = rp4[j]; rid = ("rp", j)
                c0, cw = blocks[bi]
                pi_ = pcount[0] % 3; pp = pcount[0] % 2; pcount[0] += 1
                P = pY[pi_]
                for kc in range(8):
                    s.op("pe", lambda e, kc=kc, W=W, P=P, cw=cw: e.matmul(out=P[:, 0:cw], lhsT=hT4[j][:, kc, :], rhs=W[:, kc, 0:cw], start=(kc == 0), stop=(kc == 7)),
                         reads=[("hT", j, 0), ("hT", j, 1), ("wb", wi)], writes=[("pY", pi_)])
                pid = ("pY", pi_)
                if bi == 0:
                    s.op("act", lambda e, P=P: e.activation(out=qs2[pp][:], in_=P[:], func=AF.Copy, scale=0.125), reads=[pid], writes=[("qs", pp)])
                    _rope(s, qs2[pp][:], qr2[pp][:], tmp2[pp], R, rid, 8, ("qs", pp), ("qr", pp), ("tmp", pp))
                    for hh in range(8):
                        s.op("pe", lambda e, hh=hh: e.transpose(out=pX[hh // 4][0:64, (hh % 4) * 128:(hh % 4 + 1) * 128], in_=qr2[pp][:, hh * 64:(hh + 1) * 64], identity=ident), reads=[("qr", pp), "cm"], writes=[("pX", hh // 4)])
                    s.op("act", lambda e: e.copy(out=qT2[pp][:, 0:4, :].rearrange("p a b -> p (a b)"), in_=pX[0][0:64, :]), reads=[("pX", 0)], writes=[("qT", pp)])
                    s.op("dve", lambda e: e.tensor_copy(out=qT2[pp][:, 4:8, :].rearrange("p a b -> p (a b)"), in_=pX[1][0:64, :]), reads=[("pX", 1)], writes=[("qT", pp)])
                    s.dma(QT_s[:, :, i * 128:(i + 1) * 128], qT2[pp][:], reads=[("qT", pp)], writes=["QT_s"], queue="sp")
                elif bi == 1:
                    s.op("act", lambda e, P=P: e.copy(out=kvs2[pp][:], in_=P[:, 0:256]), reads=[pid], writes=[("kvs", pp)])
                    if lat:
                        _rope(s, kvs2[pp][:, 0:128], kr2[pp][:], tmp2[pp], R, rid, 2, ("kvs", pp), ("kr", pp), ("tmp", pp))
                        ksrc, kid = kr2[pp], ("kr", pp)
                    else:
                        ksrc, kid = kvs2[pp], ("kvs", pp)
                    for hh in range(2):
                        s.op("pe", lambda e, hh=hh, ksrc=ksrc: e.transpose(out=pX[0][0:64, hh * 128:(hh + 1) * 128], in_=ksrc[:, hh * 64:(hh + 1) * 64], identity=ident), reads=[kid, "cm"], writes=[("pX", 0)])
                    s.op("act", lambda e: e.copy(out=kT2[pp][:].rearrange("p a b -> p (a b)"), in_=pX[0][0:64, 0:256]), reads=[("pX", 0)], writes=[("kT", pp)])
                    s.dma(KT_s[:, :, tg * 128:(tg + 1) * 128], kT2[pp][:], reads=[("kT", pp)], writes=["KT_s"], queue="sp")
                    s.op("pool", lambda e: e.tensor_copy(out=va2[pp][:, :, 0:64], in_=kvs2[pp][:, 128:256].rearrange("p (g d) -> p g d", g=2)), reads=[("kvs", pp)], writes=[("va", pp)])
                    s.dma(V_s[tg * 128:(tg + 1) * 128, :, :], va2[pp][:], reads=[("va", pp)], writes=["V_s"], queue="sp")
                elif bi in (2, 3, 4):
                    s.op("act", lambda e, P=P: e.copy(out=rw2[pp][:], in_=P[:]), reads=[pid], writes=[("rw", pp)])
                    for k4 in range(4):
                        s.op("pe", lambda e, k4=k4: e.transpose(out=pX[1][:, k4 * 128:(k4 + 1) * 128], in_=rw2[pp][:, k4 * 128:(k4 + 1) * 128], identity=ident), reads=[("rw", pp), "cm"], writes=[("pX", 1)])
                    s.op("dve", lambda e: e.tensor_copy(out=rT2[pp][:].rearrange("p a b -> p (a b)"), in_=pX[1][:]), reads=[("pX", 1)], writes=[("rT", pp)])
                    f0 = (bi - 2) * 512
                    s.dma(RT_s[f0:f0 + 512, tg * 128:(tg + 1) * 128].rearrange("(a p) t -> p a t", p=128), rT2[pp][:], reads=[("rT", pp)], writes=["RT_s"], queue="sp")
                elif bi == 5:
                    s.op("act", lambda e, P=P: e.activation(out=zz2[pp][:], in_=P[:], func=AF.Silu), reads=[pid], writes=[("zz", pp)])
                    s.op("pool", lambda e: e.tensor_tensor(out=zz2[pp][:].rearrange("p (h d) -> p h d", h=4), in0=zz2[pp][:].rearrange("p (h d) -> p h d", h=4), in1=gn[:].unsqueeze(1).to_broadcast([128, 4, 128]), op=ALU.mult), reads=[("zz", pp), "gn"], writes=[("zz", pp)])
                    s.dma(Z_s[i * 128:(i + 1) * 128, :], zz2[pp][:], reads=[("zz", pp)], writes=["Z_s"], queue="sp")
                elif bi == 6:
                    s.op("dve", lambda e, P=P: e.tensor_tensor(out=ab2[pp][:, 0:8], in0=P[:, 0:8], in1=abc[:, 0, :], op=ALU.add), reads=[pid, "abc"], writes=[("ab", pp)])
                    s.op("act", lambda e: e.activation(out=ab2[pp][:, 0:8], in_=ab2[pp][:, 0:8], func=AF.Exp), reads=[("ab", pp)], writes=[("ab", pp)])
                    s.op("dve", lambda e: e.tensor_scalar(out=ab2[pp][:, 0:8], in0=ab2[pp][:, 0:8], scalar1=1.0, scalar2=None, op0=ALU.add), reads=[("ab", pp)], writes=[("ab", pp)])
                    s.op("act", lambda e: e.activation(out=ab2[pp][:, 0:8], in_=ab2[pp][:, 0:8], func=AF.Ln), reads=[("ab", pp)], writes=[("ab", pp)])
                    s.op("dve", lambda e: e.tensor_tensor(out=gbo2[pp][:, 0:8], in0=ab2[pp][:, 0:8], in1=abc[:, 1, :], op=ALU.mult), reads=[("ab", pp), "abc"], writes=[("gbo", pp)])
                    s.op("act", lambda e, P=P: e.activation(out=gbo2[pp][:, 8:16], in_=P[:, 8:16], func=AF.Sigmoid), reads=[pid], writes=[("gbo", pp)])
                    s.dma(GB_s[tg * 128:(tg + 1) * 128, :], gbo2[pp][:], reads=[("gbo", pp)], writes=["GB_s"], queue="sp")
                else:
                    gi = bi - 7
                    s.op("dve", lambda e, P=P, gi=gi: e.tensor_tensor(out=gg2[pp][:], in0=P[:], in1=bg[:, gi * 512:(gi + 1) * 512], op=ALU.add), reads=[pid, "bg"], writes=[("gg", pp)])
                    s.op("act", lambda e: e.activation(out=gg2[pp][:], in_=gg2[pp][:], func=AF.Sigmoid), reads=[("gg", pp)], writes=[("gg", pp)])
                    s.dma(GT_s[i * 128:(i + 1) * 128, gi * 512:(gi + 1) * 512], gg2[pp][:], reads=[("gg", pp)], writes=["GT_s"], queue="sp")

            groups = [tiles[0:2]] + [tiles[k:k + 4] for k in range(2, len(tiles), 4)]
            tbase = 0
            for grp in groups:
                for j, (kind, i) in enumerate(grp):
                    prep(tbase + j, kind, i, j)
                need = range(11) if grp[0][0] == "l" else (1, 2, 3, 4, 6)
                for bi in need:
                    c0, cw = blocks[bi]
                    wi = wcount[0] % 3; wcount[0] += 1
                    W = wb[wi]
                    s.dma(W[:, :, 0:cw], win_d[:, c0:c0 + cw].rearrange("(kc p) n -> p kc n", p=128), writes=[("wb", wi)], queue="pool", allow_slow_non_contiguous=(cw < 128))
                    for j, (kind, i) in enumerate(grp):
                        proj(tbase + j, kind, i, j, bi, W, wi)
                tbase += len(grp)
            s.flush()
        stA.close()
        if upto <= 1:
            return nc

        with contextlib.ExitStack() as st:
            cw = T(st, "cw", [128, 12, 5])
            Rt = [T(st, f"Rt{i}", [128, 516]) for i in range(3)]
            acc = [T(st, f"acc{i}", [128, 512]) for i in range(2)]
            y = [T(st, f"y{i}", [128, 512]) for i in range(2)]
            y2 = T(st, "y2", [128, 512]); rn = T(st, "rn", [128, 512]); yn = [T(st, f"yn{i}", [128, 512]) for i in range(2)]
            tok = [T(st, f"tok{i}", [128, 4, 128]) for i in range(2)]
            pN = [PS(st, f"pN{i}", [128, 512]) for i in range(2)]
            pK = [PS(st, f"pK{i}", [128, 512]) for i in range(2)]
            for j in range(5):
                s.dma(cw[:, :, j], conv_d[j, :].rearrange("(fc p) -> p fc", p=128), writes=["cw"], allow_slow_non_contiguous=True)
            it = 0
            segs = [(0, CTX), (CTX, TALL)]
            if "small" in dbg:
                segs = [(0, CTX), (CTX, CTX + 256)]
            for (g0, g1) in segs:
                for t0 in range(g0, g1, 512):
                    n = min(512, g1 - t0)
                    for fc in range(12):
                        R = Rt[it % 3]; rid = ("Rt", it % 3); A = acc[it % 2]; aid = ("acc", it % 2); Y = y[it % 2]; yid = ("y", it % 2)
                        lo = max(t0 - 2, g0); hi = min(t0 + n + 2, g1)
                        if lo > t0 - 2 or hi < t0 + n + 2:
                            s.op("pool", lambda e, R=R: e.memset(R[:], 0.0), writes=[rid])
                        s.dma(R[:, lo - (t0 - 2):hi - (t0 - 2)], RT_s[fc * 128:(fc + 1) * 128, lo:hi], reads=["RT_s"], writes=[rid], queue=("sp", "act")[it % 2])
                        s.op("dve", lambda e, R=R, A=A, fc=fc, n=n: e.tensor_scalar(out=A[:, 0:n], in0=R[:, 0:n], scalar1=cw[:, fc, 0:1], scalar2=None, op0=ALU.mult), reads=[rid, "cw"], writes=[aid])
                        for j in range(1, 5):
                            s.op("dve", lambda e, R=R, A=A, fc=fc, n=n, j=j: e.scalar_tensor_tensor(out=A[:, 0:n], in0=R[:, j:j + n], scalar=cw[:, fc, j:j + 1], in1=A[:, 0:n], op0=ALU.mult, op1=ALU.add), reads=[rid, "cw", aid], writes=[aid])
                        s.op("act", lambda e, A=A, Y=Y, n=n: e.activation(out=Y[:, 0:n], in_=A[:, 0:n], func=AF.Silu), reads=[aid], writes=[yid])
                        src, sid = Y, yid
                        if fc < 8:
                            YN = yn[it % 2]; nid = ("yn", it % 2); P = pN[it % 2]; pid = ("pN", it % 2)
                            s.op("act", lambda e, Y=Y, n=n: e.activation(out=y2[:, 0:n], in_=Y[:, 0:n], func=AF.Square), reads=[yid], writes=["y2"])
                            s.op("pe", lambda e, P=P, n=n: e.matmul(out=P[:, 0:n], lhsT=cm[:, C_ONES, :], rhs=y2[:, 0:n], start=True, stop=True), reads=["cm", "y2"], writes=[pid])
                            s.op("dve", lambda e, P=P, n=n: e.tensor_scalar(out=rn[:, 0:n], in0=P[:, 0:n], scalar1=1e-6, scalar2=None, op0=ALU.add), reads=[pid], writes=["rn"])
                            s.op("act", lambda e, n=n: e.sqrt(out=rn[:, 0:n], in_=rn[:, 0:n]), reads=["rn"], writes=["rn"])
                            s.op("dve", lambda e, n=n: e.reciprocal(out=rn[:, 0:n], in_=rn[:, 0:n]), reads=["rn"], writes=["rn"])
                            sc = float(128 ** -0.5) if fc < 4 else 1.0
                            s.op("dve", lambda e, Y=Y, YN=YN, n=n, sc=sc: e.scalar_tensor_tensor(out=YN[:, 0:n], in0=Y[:, 0:n], scalar=sc, in1=rn[:, 0:n], op0=ALU.mult, op1=ALU.mult), reads=[yid, "rn"], writes=[nid])
                            s.dma(QK_s[fc * 128:(fc + 1) * 128, t0:t0 + n], YN[:, 0:n], reads=[nid], writes=["QK_s"], queue="pool")
                            src, sid = YN, nid
                        if fc >= 4:
                            PK = pK[it % 2]; kid = ("pK", it % 2); TK = tok[it % 2]; tid = ("tok", it % 2)
                            nsb = n // 128
                            for sb in range(nsb):
                                s.op("pe", lambda e, PK=PK, src=src, sb=sb: e.transpose(out=PK[:, sb * 128:(sb + 1) * 128], in_=src[:, sb * 128:(sb + 1) * 128], identity=ident), reads=[sid, "cm"], writes=[kid])
                            s.op("act", lambda e, PK=PK, TK=TK, n=n: e.copy(out=TK[:].rearrange("p a b -> p (a b)")[:, 0:n], in_=PK[:, 0:n]), reads=[kid], writes=[tid])
                            s.dma(KV_s[t0:t0 + n, (fc - 4) * 128:(fc - 3) * 128].rearrange("(sb p) f -> p sb f", p=128), TK[:, 0:nsb, :], reads=[tid], writes=["KV_s"], queue="pool")
                        it += 1
            s.flush()
        if upto <= 2:
            return nc

        with contextlib.ExitStack() as st:
            Sst = [T(st, f"Sst{i}", [128, 4, 128]) for i in range(2)]
            qT4 = T(st, "qT4", [128, 4, 128]); kT4 = T(st, "kT4", [128, 4, 128]); ktok = T(st, "ktok", [128, 4, 128]); vtok = T(st, "vtok", [128, 4, 128])
            gb = T(st, "gb", [128, 16]); sm = T(st, "sm", [128, 16]); ex = T(st, "ex", [128, 16]); beg = T(st, "beg", [128, 4])
            G1 = T(st, "G1", [128, 4, 128]); dl = T(st, "dl", [128, 4, 128]); du = T(st, "du", [128, 4, 128])
            Bm = [T(st, f"Bm{i}", [128, 4, 128]) for i in range(2)]; Cm = [T(st, f"Cm{i}", [128, 4, 128]) for i in range(2)]; Pm = [T(st, f"Pm{i}", [128, 4, 128]) for i in range(2)]
            aT = T(st, "aT", [128, 4, 128]); kbg = T(st, "kbg", [128, 4, 128]); vb = T(st, "vb", [128, 4, 128]); ktl = T(st, "ktl", [128, 4, 128])
            WT = T(st, "WT", [128, 4, 128]); U = T(st, "U", [128, 4, 128]); vn = T(st, "vn", [128, 4, 128]); o1 = T(st, "o1", [128, 4, 128]); ot = T(st, "ot", [128, 4, 128])
            pb = [PS(st, f"pb{i}", [128, 4, 128]) for i in range(8)]
            pA, pB_, pC, pD, pE, pF, pG, pH = pb
            pid = lambda k: ("pb", k)
            H4 = [128, 4, 128]
            bc_h = lambda ap2: ap2.unsqueeze(1).to_broadcast(H4)
            bc_l = lambda ap2: ap2.unsqueeze(2).to_broadcast(H4)
            ntl = (2 if "small" in dbg else NT)
            for dr in range(2):
                M1 = cm[:, C_M1F if dr == 0 else C_M1B, :]; NM1 = cm[:, C_NM1F if dr == 0 else C_NM1B, :]
                NB = cm[:, C_NLOW if dr == 0 else C_NUP, :]; NTm = cm[:, C_NUP if dr == 0 else C_NLOW, :]
                STR = cm[:, C_SLOW if dr == 0 else C_SUP, :]
                SS = Sst[dr]; ssid = ("Sst", dr)
                s.op("pool", lambda e, SS=SS: e.memset(SS[:], 0.0), writes=[ssid])
                order = [("c", i) for i in range(CTX // 128)] + [("l", i) for i in range(ntl)]
                if dr == 1:
                    order = [("c", i) for i in reversed(range(CTX // 128))] + [("l", i) for i in reversed(range(ntl))]
                for (kind, i) in order:
                    lat = kind == "l"
                    tg = i if not lat else CTX // 128 + i
                    tsl = slice(tg * 128, (tg + 1) * 128)
                    s.dma(qT4[:], QK_s[0:512, tsl].rearrange("(h p) t -> p h t", p=128), reads=["QK_s"], writes=["qT4"])
                    s.dma(kT4[:], QK_s[512:1024, tsl].rearrange("(h p) t -> p h t", p=128), reads=["QK_s"], writes=["kT4"], queue="act")
                    s.dma(ktok[:].rearrange("p h d -> p (h d)"), KV_s[tsl, 0:512], reads=["KV_s"], writes=["ktok"])
                    s.dma(vtok[:].rearrange("p h d -> p (h d)"), KV_s[tsl, 512:1024], reads=["KV_s"], writes=["vtok"], queue="act")
                    s.dma(gb[:], GB_s[tsl, :], reads=["GB_s"], writes=["gb"])
                    g = gb[:, dr * 4:dr * 4 + 4]; beta = gb[:, 8 + dr * 4:12 + dr * 4]
                    pAf = pA[:].rearrange("p a b -> p (a b)")
                    for k, L in enumerate((M1, cm[:, C_SAME, :], cm[:, C_SEL0, :], cm[:, C_SEL1, :])):
                        s.op("pe", lambda e, k=k, L=L, g=g: e.matmul(out=pAf[:, 4 * k:4 * k + 4], lhsT=L, rhs=g, start=True, stop=True), reads=["cm", "gb"], writes=[pid(0)])
                    s.op("dve", lambda e: e.tensor_copy(out=sm[:], in_=pAf[:, 0:16]), reads=[pid(0)], writes=["sm"])
                    s.op("dve", lambda e: e.tensor_tensor(out=sm[:, 4:8], in0=sm[:, 4:8], in1=sm[:, 0:4], op=ALU.subtract), reads=["sm"], writes=["sm"])
                    s.op("act", lambda e: e.activation(out=ex[:], in_=sm[:], func=AF.Exp), reads=["sm"], writes=["ex"])
                    s.op("dve", lambda e, beta=beta: e.tensor_tensor(out=beg[:], in0=ex[:, 0:4], in1=beta, op=ALU.mult), reads=["ex", "gb"], writes=["beg"])
                    s.op("dve", lambda e, g=g: e.tensor_tensor(out=G1[:], in0=bc_h(cm[:, C_SAME, :]), in1=bc_l(g), op=ALU.mult), reads=["cm", "gb"], writes=["G1"])
                    for hh in range(4):
                        s.op("pe", lambda e, hh=hh, M1=M1: e.matmul(out=pB_[:, hh, :], lhsT=M1, rhs=G1[:, hh, :], start=True, stop=False), reads=["cm", "G1"], writes=[pid(1)])
                        s.op("pe", lambda e, hh=hh, NM1=NM1: e.matmul(out=pB_[:, hh, :], lhsT=G1[:, hh, :], rhs=NM1, start=False, stop=True), reads=["cm", "G1"], writes=[pid(1)])
                    s.op("dve", lambda e, NB=NB: e.tensor_tensor(out=dl[:], in0=pB_[:], in1=bc_h(NB), op=ALU.add), reads=[pid(1), "cm"], writes=["dl"])
                    s.op("dve", lambda e, NTm=NTm: e.scalar_tensor_tensor(out=du[:], in0=pB_[:], scalar=-1.0, in1=bc_h(NTm), op0=ALU.mult, op1=ALU.add), reads=[pid(1), "cm"], writes=["du"])
                    s.op("act", lambda e: e.activation(out=dl[:], in_=dl[:], func=AF.Exp), reads=["dl"], writes=["dl"])
                    s.op("act", lambda e: e.activation(out=du[:], in_=du[:], func=AF.Exp), reads=["du"], writes=["du"])
                    for hh in range(4):
                        s.op("pe", lambda e, hh=hh: e.matmul(out=pC[:, hh, :], lhsT=kT4[:, hh, :], rhs=kT4[:, hh, :], start=True, stop=True), reads=["kT4"], writes=[pid(2)])
                    for hh in range(4):
                        s.op("pe", lambda e, hh=hh: e.matmul(out=pD[:, hh, :], lhsT=kT4[:, hh, :], rhs=qT4[:, hh, :], start=True, stop=True), reads=["kT4", "qT4"], writes=[pid(3)])
                    B0 = Bm[0]; C0 = Cm[0]; P0 = Pm[0]
                    s.op("dve", lambda e: e.tensor_tensor(out=B0[:], in0=pC[:], in1=dl[:], op=ALU.mult), reads=[pid(2), "dl"], writes=[("Bm", 0)])
                    s.op("pool", lambda e, STR=STR: e.tensor_tensor(out=B0[:], in0=B0[:], in1=bc_h(STR), op=ALU.mult), reads=[("Bm", 0), "cm"], writes=[("Bm", 0)])
                    s.op("pool", lambda e, beta=beta: e.tensor_tensor(out=B0[:], in0=B0[:], in1=bc_l(beta), op=ALU.mult), reads=[("Bm", 0), "gb"], writes=[("Bm", 0)])
                    s.op("dve", lambda e: e.tensor_tensor(out=aT[:], in0=pD[:], in1=du[:], op=ALU.mult), reads=[pid(3), "du"], writes=["aT"])
                    for hh in range(4):
                        s.op("pe", lambda e, hh=hh: e.transpose(out=pE[:, hh, :], in_=B0[:, hh, :], identity=ident), reads=[("Bm", 0), "cm"], writes=[pid(4)])
                    s.op("act", lambda e: e.copy(out=C0[:], in_=pE[:]), reads=[pid(4)], writes=[("Cm", 0)])
                    s.op("dve", lambda e: e.tensor_tensor(out=P0[:], in0=C0[:], in1=bc_h(ident), op=ALU.add), reads=[("Cm", 0), "cm"], writes=[("Pm", 0)])
                    cur = 0
                    for lv in range(1, 6):
                        nx = 1 - cur
                        Bc, Cc, Pc = Bm[cur], Cm[cur], Pm[cur]; Bn, Cn, Pn = Bm[nx], Cm[nx], Pm[nx]
                        for hh in range(4):
                            s.op("pe", lambda e, hh=hh, Bc=Bc, Cc=Cc: e.matmul(out=pF[:, hh, :], lhsT=Cc[:, hh, :], rhs=Bc[:, hh, :], start=True, stop=True), reads=[("Bm", cur), ("Cm", cur)], writes=[pid(5)])
                        s.op("act", lambda e, Bn=Bn: e.copy(out=Bn[:], in_=pF[:]), reads=[pid(5)], writes=[("Bm", nx)])
                        if lv < 5:
                            for hh in range(4):
                                s.op("pe", lambda e, hh=hh, Bc=Bc, Cc=Cc: e.matmul(out=pG[:, hh, :], lhsT=Bc[:, hh, :], rhs=Cc[:, hh, :], start=True, stop=True), reads=[("Bm", cur), ("Cm", cur)], writes=[pid(6)])
                            s.op("dve", lambda e, Cn=Cn: e.tensor_copy(out=Cn[:], in_=pG[:]), reads=[pid(6)], writes=[("Cm", nx)])
                        for hh in range(4):
                            s.op("pe", lambda e, hh=hh, Pc=Pc: e.matmul(out=pH[:, hh, :], lhsT=ident, rhs=Pc[:, hh, :], start=True, stop=False), reads=[("Pm", cur), "cm"], writes=[pid(7)])
                            s.op("pe", lambda e, hh=hh, Pc=Pc, Bn=Bn: e.matmul(out=pH[:, hh, :], lhsT=Bn[:, hh, :], rhs=Pc[:, hh, :], start=False, stop=True), reads=[("Pm", cur), ("Bm", nx)], writes=[pid(7)])
                        s.op("dve", lambda e, Pn=Pn: e.tensor_copy(out=Pn[:], in_=pH[:]), reads=[pid(7)], writes=[("Pm", nx)])
                        cur = nx
                    TT = Pm[cur]; ttid = ("Pm", cur)
                    s.op("pool", lambda e: e.tensor_tensor(out=kbg[:], in0=ktok[:], in1=bc_l(beg[:]), op=ALU.mult), reads=["ktok", "beg"], writes=["kbg"])
                    s.op("pool", lambda e, beta=beta: e.tensor_tensor(out=vb[:], in0=vtok[:], in1=bc_l(beta), op=ALU.mult), reads=["vtok", "gb"], writes=["vb"])
                    s.op("pool", lambda e: e.tensor_tensor(out=ktl[:], in0=ktok[:], in1=bc_l(ex[:, 4:8]), op=ALU.mult), reads=["ktok", "ex"], writes=["ktl"])
                    for hh in range(4):
                        s.op("pe", lambda e, hh=hh, TT=TT: e.matmul(out=pE[:, hh, :], lhsT=kbg[:, hh, :], rhs=TT[:, hh, :], start=True, stop=True), reads=["kbg", ttid], writes=[pid(4)])
                    s.op("act", lambda e: e.copy(out=WT[:], in_=pE[:]), reads=[pid(4)], writes=["WT"])
                    for hh in range(4):
                        s.op("pe", lambda e, hh=hh, TT=TT: e.matmul(out=pF[:, hh, :], lhsT=TT[:, hh, :], rhs=vb[:, hh, :], start=True, stop=True), reads=["vb", ttid], writes=[pid(5)])
                    s.op("dve", lambda e: e.tensor_copy(out=U[:], in_=pF[:]), reads=[pid(5)], writes=["U"])
                    for c in ((0, 1) if dr == 0 else (1, 0)):
                        pr = slice(64 * c, 64 * c + 64)
                        for hh in range(4):
                            s.op("pe", lambda e, hh=hh, SS=SS: e.matmul(out=pG[:, hh, :], lhsT=WT[:, hh, :], rhs=SS[:, hh, :], start=True, stop=True), reads=["WT", ssid], writes=[pid(6)])
                        s.op("dve", lambda e, pr=pr: e.tensor_tensor(out=vn[pr], in0=U[pr], in1=pG[pr], op=ALU.subtract), reads=["U", pid(6)], writes=["vn"])
                        for hh in range(4):
                            s.op("pe", lambda e, hh=hh, SS=SS: e.matmul(out=pH[:, hh, :], lhsT=qT4[:, hh, :], rhs=SS[:, hh, :], start=True, stop=True), reads=["qT4", ssid], writes=[pid(7)])
                        for hh in range(4):
                            s.op("pe", lambda e, hh=hh, pr=pr: e.matmul(out=pC[:, hh, :], lhsT=aT[pr, hh, :], rhs=vn[pr, hh, :], start=True, stop=True), reads=["aT", "vn"], writes=[pid(2)])
                        for hh in range(4):
                            s.op("pe", lambda e, hh=hh, pr=pr: e.matmul(out=pD[:, hh, :], lhsT=ktl[pr, hh, :], rhs=vn[pr, hh, :], start=True, stop=True), reads=["ktl", "vn"], writes=[pid(3)])
                        if lat:
                            s.op("dve", lambda e, pr=pr: e.tensor_tensor(out=o1[pr], in0=pH[pr], in1=bc_l(ex[:, 0:4])[pr], op=ALU.mult), reads=[pid(7), "ex"], writes=["o1"])
                            s.op("dve", lambda e, pr=pr: e.tensor_tensor(out=ot[pr], in0=o1[pr], in1=pC[pr], op=ALU.add), reads=["o1", pid(2)], writes=["ot"])
                        s.op("pool", lambda e, c=c, SS=SS: e.tensor_tensor(out=SS[:], in0=SS[:], in1=bc_l(ex[:, 8 + 4 * c:12 + 4 * c]), op=ALU.mult), reads=[ssid, "ex", pid(6), pid(7)], writes=[ssid])
                        s.op("dve", lambda e, SS=SS: e.tensor_tensor(out=SS[:], in0=SS[:], in1=pD[:], op=ALU.add), reads=[ssid, pid(3)], writes=[ssid])
                    if lat:
                        s.dma(OD_s[dr, i * 128:(i + 1) * 128, :], ot[:].rearrange("p h d -> p (h d)"), reads=["ot"], writes=["OD_s"], queue="pool")
            s.flush()
        if upto <= 3:
            return nc

        with contextlib.ExitStack() as st:
            kt = [T(st, f"kt{i}", [64, 2, 384]) for i in range(2)]
            vt = [T(st, f"vt{i}", [128, 3, 130]) for i in range(2)]
            ktc = T(st, "ktc", [64, 2, 256]); vtc = T(st, "vtc", [128, 2, 130])
            qt = [T(st, f"qt{i}", [64, 8, 128]) for i in range(2)]
            E = [T(st, f"E{i}", [128, 5, 512]) for i in range(2)]
            esink = T(st, "esink", [128, 8]); den = T(st, "den", [128, 8]); oa = [T(st, f"oa{i}", [128, 512]) for i in range(2)]
            pS = [PS(st, f"pS{i}", [128, 512]) for i in range(3)]
            pO = [PS(st, f"pO{i}", [128, 4, 65]) for i in range(2)]
            s.dma(ktc[:], KT_s[:, :, 0:CTX], reads=["KT_s"], writes=["ktc"])
            s.dma(vtc[:], V_s[0:CTX].rearrange("(b p) g d -> p b (g d)", p=128), reads=["V_s"], writes=["vtc"])
            s.dma(esink[:], sink_d.partition_broadcast(128), writes=["esink"])
            s.op("act", lambda e: e.activation(out=esink[:], in_=esink[:], func=AF.Exp), reads=["esink"], writes=["esink"])
            ntl = (2 if "small" in dbg else NT)
            nS = 0
            for i in range(ntl):
                lo = max(i - 1, 0); hi = min(i + 1, ntl - 1); nb = hi - lo + 1
                KT_ = kt[i % 2]; VT_ = vt[i % 2]; QT_ = qt[i % 2]; OA = oa[i % 2]
                s.dma(KT_[:, :, 0:nb * 128], KT_s[:, :, CTX + lo * 128:CTX + (hi + 1) * 128], reads=["KT_s"], writes=[("kt", i % 2)])
                s.dma(VT_[:, 0:nb, :], V_s[CTX + lo * 128:CTX + (hi + 1) * 128].rearrange("(b p) g d -> p b (g d)", p=128), reads=["V_s"], writes=[("vt", i % 2)], queue="act")
                s.dma(QT_[:], QT_s[:, :, i * 128:(i + 1) * 128], reads=["QT_s"], writes=[("qt", i % 2)])
                for g in range(2):
                    Eg = E[g]; eid = ("E", g)
                    kb = [("l", j - lo, (C_WPREV if j < i else (C_WNEXT if j > i else None))) for j in range(lo, hi + 1)] + [("c", 0, None), ("c", 1, None)]
                    for bi, (kk, bl, msk) in enumerate(kb):
                        P = pS[nS % 3]; psid = ("pS", nS % 3); nS += 1
                        lhs = KT_[:, g, bl * 128:(bl + 1) * 128] if kk == "l" else ktc[:, g, bl * 128:(bl + 1) * 128]
                        s.op("pe", lambda e, P=P, lhs=lhs, QT_=QT_, g=g: e.matmul(out=P[:].rearrange("p (h q) -> p h q", h=4), lhsT=lhs, rhs=QT_[:, 4 * g:4 * g + 4, :], start=True, stop=True),
                             reads=[("kt", i % 2), "ktc", ("qt", i % 2)], writes=[psid])
                        s.op("act", lambda e, P=P, Eg=Eg, bi=bi: e.activation(out=Eg[:, bi, :], in_=P[:], func=AF.Exp), reads=[psid], writes=[eid])
                        if msk is not None:
                            s.op("dve", lambda e, Eg=Eg, bi=bi, msk=msk: e.tensor_tensor(out=Eg[:, bi, :].rearrange("p (h q) -> p h q", h=4), in0=Eg[:, bi, :].rearrange("p (h q) -> p h q", h=4),
                                                                              in1=cm[:, msk, :].unsqueeze(1).to_broadcast([128, 4, 128]), op=ALU.mult), reads=[eid, "cm"], writes=[eid])
                    for hh in range(4):
                        for bi, (kk, bl, msk) in enumerate(kb):
                            rhs = VT_[:, bl, g * 65:(g + 1) * 65] if kk == "l" else vtc[:, bl, g * 65:(g + 1) * 65]
                            s.op("pe", lambda e, Eg=Eg, bi=bi, hh=hh, rhs=rhs, g=g, last=(bi == len(kb) - 1): e.matmul(out=pO[g][:, hh, :], lhsT=Eg[:, bi, hh * 128:(hh + 1) * 128], rhs=rhs, start=(bi == 0), stop=last),
                                 reads=[eid, ("vt", i % 2), "vtc"], writes=[("pO", g)])
                    s.op("dve", lambda e, g=g: e.tensor_tensor(out=den[:, 4 * g:4 * g + 4], in0=pO[g][:, :, 64], in1=esink[:, 4 * g:4 * g + 4], op=ALU.add), reads=[("pO", g), "esink"], writes=["den"])
                    s.op("dve", lambda e, g=g: e.reciprocal(out=den[:, 4 * g:4 * g + 4], in_=den[:, 4 * g:4 * g + 4]), reads=["den"], writes=["den"])
                    s.op("dve", lambda e, g=g, OA=OA: e.tensor_tensor(out=OA[:, g * 256:(g + 1) * 256].rearrange("p (h d) -> p h d", h=4), in0=pO[g][:, :, 0:64],
                                                              in1=den[:, 4 * g:4 * g + 4].unsqueeze(2).to_broadcast([128, 4, 64]), op=ALU.mult), reads=[("pO", g), "den"], writes=[("oa", i % 2)])
                s.dma(OA_s[i * 128:(i + 1) * 128, :], OA[:], reads=[("oa", i % 2)], writes=["OA_s"], queue="pool")
            s.flush()
        if upto <= 5:
            return nc

        UTr_s = nc.dram_tensor("UTr_s", [D, 16384], F32R, kind="Internal").ap()
        Vr_s = nc.dram_tensor("Vr_s", [16384, D], F32R, kind="Internal").ap()
        X1_s = scr("X1_s", [S, D])
        H2T_s = scr("H2T_s", [D, S])
        H2R_s = nc.dram_tensor("H2R_s", [D, S], F32R, kind="Internal").ap()
        SC_s = scr("SC_s", [S, 2048])
        KAP_s = scr("KAP_s", [S, 8])
        TR = lambda st, name, shape: st.enter_context(nc.sbuf_tensor(_nm(name), list(shape), F32R))
        B = lambda k: ("bk", k)
        ntl6 = (2 if "small" in dbg else NT)

        with contextlib.ExitStack() as st:
            cvb = [TR(st, "cvb", [128, 4096]) for _ in range(2)]
            wbr = T(st, "wbr", [128, 8, D]); wo = T(st, "wo", [128, 8, D])
            xa = [T(st, f"xa{b}", [128, D]) for b in range(2)]; yb = [T(st, f"yb{b}", [128, D]) for b in range(2)]
            tcA = [T(st, f"tcA{b}", [128, 8, 128]) for b in range(2)]; h2r = [TR(st, f"h2r{b}", [128, 8, 128]) for b in range(2)]
            gt = [T(st, f"gt{b}", [128, 2048]) for b in range(2)]
            od = [T(st, f"od{b}", [128, 2, 512]) for b in range(2)]; zt = [T(st, f"zt{b}", [128, 512]) for b in range(2)]
            oat = [T(st, f"oat{b}", [128, 512]) for b in range(2)]; o2 = [T(st, f"o2{b}", [128, 512]) for b in range(2)]
            qsb = [T(st, f"qsb{b}", [128, D]) for b in range(2)]
            ssq = [T(st, f"ssq{b}", [128, 4]) for b in range(2)]; ss = [T(st, f"ss{b}", [128, 1]) for b in range(2)]; rstd = [T(st, f"rstd{b}", [128, 1]) for b in range(2)]
            bk = [PS(st, f"bkA{i}", [128, 512]) for i in range(8)]
            cjobs = []
            for r0 in range(0, D, 128):
                for c0 in range(0, 16384, 4096):
                    cjobs.append((pu_d[r0:r0 + 128, c0:c0 + 4096], UTr_s[r0:r0 + 128, c0:c0 + 4096], "UTr_s"))
            for r0 in range(0, 16384, 512):
                cjobs.append((pv_d[r0:r0 + 512, :].rearrange("(p a) n -> p (a n)", p=128), Vr_s[r0:r0 + 512, :].rearrange("(p a) n -> p (a n)", p=128), "Vr_s"))
            cstate = [0]

            def conv_some(n):
                for _ in range(n):
                    if not cjobs:
                        return
                    src, dst, did = cjobs.pop(0)
                    k = cstate[0] % 2; cstate[0] += 1
                    s.dma(cvb[k][:], src, writes=[("cvb", k)], queue="pool")
                    s.dma(dst, cvb[k][:], reads=[("cvb", k)], writes=[did], queue="pool")
            s.dma(wbr[:, 0:4, :], wba_d.rearrange("(kc p) n -> p kc n", p=128), writes=["wbr"])
            s.dma(wbr[:, 4:8, :], wbd_d.rearrange("(kc p) n -> p kc n", p=128), writes=["wbr"], queue="act")
            s.dma(wo[:], wout_d.rearrange("(kc p) n -> p kc n", p=128), writes=["wo"])

            def tr8(b, src, sid, nkc, dst_off, dst, did, extra=None):
                pT = [bk[4 * b], bk[4 * b + 1]]
                for kc in range(nkc):
                    q_ = (dst_off + kc) // 4
                    s.op("pe", lambda e, kc=kc, q_=q_: e.transpose(out=pT[q_][:, ((dst_off + kc) % 4) * 128:((dst_off + kc) % 4 + 1) * 128], in_=src[:, kc * 128:(kc + 1) * 128], identity=ident), reads=[sid, "cm"], writes=[B(4 * b + q_)])
                for q_ in sorted(set((dst_off + kc) // 4 for kc in range(nkc))):
                    if q_ == 0:
                        s.op("act", lambda e, q_=q_: e.copy(out=dst[:, q_ * 4:(q_ + 1) * 4, :].rearrange("p a b -> p (a b)"), in_=pT[q_][:]), reads=[B(4 * b + q_)], writes=[(did, q_)])
                    else:
                        s.op("dve", lambda e, q_=q_: e.tensor_copy(out=dst[:, q_ * 4:(q_ + 1) * 4, :].rearrange("p a b -> p (a b)"), in_=pT[q_][:]), reads=[B(4 * b + q_)], writes=[(did, q_)])
                    if extra is not None:
                        d2, d2id = extra
                        if q_ == 0:
                            s.op("dve", lambda e, q_=q_: e.tensor_copy(out=d2[:, q_ * 4:(q_ + 1) * 4, :].rearrange("p a b -> p (a b)"), in_=pT[q_][:]), reads=[B(4 * b + q_)], writes=[(d2id, q_)])
                        else:
                            s.op("act", lambda e, q_=q_: e.copy(out=d2[:, q_ * 4:(q_ + 1) * 4, :].rearrange("p a b -> p (a b)"), in_=pT[q_][:]), reads=[B(4 * b + q_)], writes=[(d2id, q_)])

            def rms6(b, src, sid):
                s.op("act", lambda e: e.activation(out=qsb[b][:], in_=src[:], func=AF.Square, accum_out=ss[b][:]), reads=[sid], writes=[("qsb", b), ("ss", b)])
                s.op("dve", lambda e: e.tensor_scalar(out=rstd[b][:], in0=ss[b][:], scalar1=1.0 / D, scalar2=1e-6, op0=ALU.mult, op1=ALU.add), reads=[("ss", b)], writes=[("rstd", b)])
                s.op("act", lambda e: e.sqrt(out=rstd[b][:], in_=rstd[b][:]), reads=[("rstd", b)], writes=[("rstd", b)])
                s.op("dve", lambda e: e.reciprocal(out=rstd[b][:], in_=rstd[b][:]), reads=[("rstd", b)], writes=[("rstd", b)])

            for i in range(ntl6):
                conv_some(4)
                b = i % 2
                tsl = slice(i * 128, (i + 1) * 128)
                XA = xa[b]; YB = yb[b]; GT = gt[b]; OD = od[b]; ZT = zt[b]; OAT = oat[b]; O2 = o2[b]; QSB = qsb[b]; SSQ = ssq[b]; TC = tcA[b]
                pY = [bk[4 * b + 2], bk[4 * b + 3]]
                s.dma(XA[:], x_d[tsl, :], writes=[("xa", b)])
                s.dma(OD[:, 0, :], OD_s[0, tsl, :], reads=["OD_s"], writes=[("od", b)], queue="act")
                s.dma(OD[:, 1, :], OD_s[1, tsl, :], reads=["OD_s"], writes=[("od", b)], queue="act")
                s.dma(ZT[:], Z_s[tsl, :], reads=["Z_s"], writes=[("zt", b)])
                s.dma(OAT[:], OA_s[tsl, :], reads=["OA_s"], writes=[("oat", b)], queue="act")
                s.dma(GT[:], GT_s[tsl, :], reads=["GT_s"], writes=[("gt", b)])
                s.op("dve", lambda e, OD=OD: e.tensor_tensor(out=OD[:, 0, :], in0=OD[:, 0, :], in1=OD[:, 1, :], op=ALU.add), reads=[("od", b)], writes=[("od", b)])
                s.op("pool", lambda e, OD=OD, O2=O2: e.tensor_tensor(out=O2[:], in0=OD[:, 0, :], in1=OD[:, 0, :], op=ALU.mult), reads=[("od", b)], writes=[("o2", b)])
                s.op("dve", lambda e, O2=O2, SSQ=SSQ: e.tensor_reduce(out=SSQ[:], in_=O2[:].rearrange("p (h d) -> p h d", h=4), axis=AX.X, op=ALU.add), reads=[("o2", b)], writes=[("ssq", b)])
                s.op("dve", lambda e, SSQ=SSQ: e.tensor_scalar(out=SSQ[:], in0=SSQ[:], scalar1=1.0 / 128, scalar2=1e-6, op0=ALU.mult, op1=ALU.add), reads=[("ssq", b)], writes=[("ssq", b)])
                s.op("act", lambda e, SSQ=SSQ: e.sqrt(out=SSQ[:], in_=SSQ[:]), reads=[("ssq", b)], writes=[("ssq", b)])
                s.op("dve", lambda e, SSQ=SSQ: e.reciprocal(out=SSQ[:], in_=SSQ[:]), reads=[("ssq", b)], writes=[("ssq", b)])
                s.op("dve", lambda e, O2=O2, OD=OD, SSQ=SSQ: e.tensor_tensor(out=O2[:].rearrange("p (h d) -> p h d", h=4), in0=OD[:, 0, :].rearrange("p (h d) -> p h d", h=4), in1=SSQ[:].unsqueeze(2).to_broadcast([128, 4, 128]), op=ALU.mult), reads=[("od", b), ("ssq", b)], writes=[("o2", b)])
                s.op("pool", lambda e, O2=O2, ZT=ZT: e.tensor_tensor(out=O2[:], in0=O2[:], in1=ZT[:], op=ALU.mult), reads=[("o2", b), ("zt", b)], writes=[("o2", b)])
                tr8(b, OAT, ("oat", b), 4, 0, TC, ("tcA", b))
                tr8(b, O2, ("o2", b), 4, 4, TC, ("tcA", b))
                for half in range(2):
                    for kc in range(4):
                        s.op("pe", lambda e, half=half, kc=kc, pY=pY, TC=TC: e.matmul(out=pY[half][:], lhsT=TC[:, kc, :], rhs=wbr[:, kc, half * 512:(half + 1) * 512], start=(kc == 0), stop=(kc == 3)), reads=[(("tcA", b), 0), "wbr"], writes=[B(4 * b + 2 + half)])
                    s.op("dve", lambda e, half=half, pY=pY, YB=YB, GT=GT: e.tensor_tensor(out=YB[:, half * 512:(half + 1) * 512], in0=pY[half][:], in1=GT[:, half * 512:(half + 1) * 512], op=ALU.mult), reads=[B(4 * b + 2 + half), ("gt", b)], writes=[("yb", b)])
                for half in range(2):
                    for kc in range(4):
                        s.op("pe", lambda e, half=half, kc=kc, pY=pY, TC=TC: e.matmul(out=pY[half][:], lhsT=TC[:, 4 + kc, :], rhs=wbr[:, 4 + kc, half * 512:(half + 1) * 512], start=(kc == 0), stop=(kc == 3)), reads=[(("tcA", b), 1), "wbr"], writes=[B(4 * b + 2 + half)])
                    s.op("dve", lambda e, half=half, pY=pY, QSB=QSB, GT=GT: e.tensor_tensor(out=QSB[:, half * 512:(half + 1) * 512], in0=pY[half][:], in1=GT[:, 1024 + half * 512:1024 + (half + 1) * 512], op=ALU.mult), reads=[B(4 * b + 2 + half), ("gt", b)], writes=[("qsb", b)])
                s.op("pool", lambda e, YB=YB, QSB=QSB: e.tensor_tensor(out=YB[:], in0=YB[:], in1=QSB[:], op=ALU.add), reads=[("yb", b), ("qsb", b)], writes=[("yb", b)])
                tr8(b, YB, ("yb", b), 8, 0, TC, ("tcA", b))
                for half in range(2):
                    for kc in range(8):
                        s.op("pe", lambda e, half=half, kc=kc, pY=pY, TC=TC: e.matmul(out=pY[half][:], lhsT=TC[:, kc, :], rhs=wo[:, kc, half * 512:(half + 1) * 512], start=(kc == 0), stop=(kc == 7)), reads=[(("tcA", b), 0), (("tcA", b), 1), "wo"], writes=[B(4 * b + 2 + half)])
                    s.op("dve", lambda e, half=half, pY=pY, YB=YB: e.tensor_tensor(out=YB[:, half * 512:(half + 1) * 512], in0=pY[half][:], in1=bvv(BV_GT1)[:, half * 512:(half + 1) * 512], op=ALU.mult), reads=[B(4 * b + 2 + half), "bv"], writes=[("yb", b)])
                s.op("pool", lambda e, XA=XA, YB=YB: e.tensor_tensor(out=XA[:], in0=XA[:], in1=YB[:], op=ALU.add), reads=[("xa", b), ("yb", b)], writes=[("xa", b)])
                s.dma(X1_s[tsl, :], XA[:], reads=[("xa", b)], writes=["X1_s"], queue="act")
                rms6(b, XA, ("xa", b))
                s.op("dve", lambda e, XA=XA, YB=YB, RS=rstd[b]: e.scalar_tensor_tensor(out=YB[:], in0=XA[:], scalar=RS[:, 0:1], in1=bvv(BV_G2), op0=ALU.mult, op1=ALU.mult), reads=[("xa", b), ("rstd", b), "bv"], writes=[("yb", b)])
                s.op("pool", lambda e, YB=YB: e.tensor_tensor(out=YB[:], in0=YB[:], in1=bvv(BV_SH2), op=ALU.add), reads=[("yb", b), "bv"], writes=[("yb", b)])
                tr8(b, YB, ("yb", b), 8, 0, TC, ("tcA", b), extra=(h2r[b], ("h2r", b)))
                s.dma(H2T_s[:, tsl].rearrange("(kc p) t -> p kc t", p=128), TC[:], reads=[(("tcA", b), 0), (("tcA", b), 1)], writes=["H2T_s"])
                s.dma(H2R_s[:, tsl].rearrange("(kc p) t -> p kc t", p=128), h2r[b][:], reads=[(("h2r", b), 0), (("h2r", b), 1)], writes=["H2R_s"], queue="act")
            conv_some(10 ** 6)
            s.flush()
        if upto <= 6:
            return nc

        with contextlib.ExitStack() as st:
            wq = T(st, "wq", [128, 8, D]); keys2 = T(st, "keys2", [128, 8, 128])
            tcB = [T(st, f"tcB{b}", [128, 8, 128]) for b in range(2)]
            qsb = [T(st, f"qsbB{b}", [128, D]) for b in range(2)]; qTs = [T(st, f"qTs{b}", [128, 8, 128]) for b in range(2)]
            sc = [T(st, f"scB{b}", [128, 16, 128]) for b in range(2)]
            t16 = [T(st, f"t16{b}", [128, 8, 2, 16]) for b in range(2)]; c16 = [T(st, f"c16{b}", [128, 8, 16]) for b in range(2)]
            wk4 = T(st, "wk4", [128, 4, 256]); cand4 = T(st, "cand4", [128, 4, 256])
            thr = [T(st, f"thr{b}", [128, 8]) for b in range(2)]; negm = [T(st, f"negm{b}", [128, 8]) for b in range(2)]; Zs = [T(st, f"Zs{b}", [128, 8]) for b in range(2)]
            kap = [T(st, f"kap{b}", [128, 8]) for b in range(2)]; m1 = [T(st, f"m1{b}", [128, 8]) for b in range(2)]; th2 = [T(st, f"th2{b}", [128, 8]) for b in range(2)]
            e16 = [T(st, f"e16{b}", [128, 8, 16]) for b in range(2)]
            bk = [PS(st, f"bkB{i}", [128, 512]) for i in range(8)]
            s.dma(wq[:], pwq_d.rearrange("(kc p) n -> p kc n", p=128), writes=["wq"])
            s.dma(keys2[:], pkeys_d, writes=["keys2"])
            _cb = [int(x[4:]) for x in dbg if x.startswith("cutB")]
            cutB = _cb[0] if _cb else 99
            for i in range(ntl6):
                b = i % 2
                tsl = slice(i * 128, (i + 1) * 128)
                TC = tcB[b]; QSB = qsb[b]; QT = qTs[b]; SC = sc[b]; T16 = t16[b]; C16 = c16[b]
                pT = [bk[4 * b], bk[4 * b + 1]]; pY = [bk[4 * b + 2], bk[4 * b + 3]]
                s.dma(TC[:], H2T_s[:, tsl].rearrange("(kc p) t -> p kc t", p=128), reads=["H2T_s"], writes=[("tcB", b)], queue=("sp", "act")[b])
                for half in range(2):
                    for kc in range(8):
                        s.op("pe", lambda e, half=half, kc=kc, pY=pY, TC=TC: e.matmul(out=pY[half][:], lhsT=TC[:, kc, :], rhs=wq[:, kc, half * 512:(half + 1) * 512], start=(kc == 0), stop=(kc == 7)), reads=[("tcB", b), "wq"], writes=[B(4 * b + 2 + half)])
                    if half == 0:
                        s.op("act", lambda e, pY=pY, QSB=QSB: e.copy(out=QSB[:, 0:512], in_=pY[0][:]), reads=[B(4 * b + 2)], writes=[("qsbB", b)])
                    else:
                        s.op("dve", lambda e, pY=pY, QSB=QSB: e.tensor_copy(out=QSB[:, 512:1024], in_=pY[1][:]), reads=[B(4 * b + 3)], writes=[("qsbB", b)])
                if cutB < 2:
                    continue
                for hh in range(8):
                    s.op("pe", lambda e, hh=hh, pT=pT, QSB=QSB: e.transpose(out=pT[hh // 4][:, (hh % 4) * 128:(hh % 4 + 1) * 128], in_=QSB[:, hh * 128:(hh + 1) * 128], identity=ident), reads=[("qsbB", b), "cm"], writes=[B(4 * b + hh // 4)])
                s.op("act", lambda e, pT=pT, QT=QT: e.copy(out=QT[:, 0:4, :].rearrange("p a b -> p (a b)"), in_=pT[0][:]), reads=[B(4 * b)], writes=[("qTs", b)])
                s.op("dve", lambda e, pT=pT, QT=QT: e.tensor_copy(out=QT[:, 4:8, :].rearrange("p a b -> p (a b)"), in_=pT[1][:]), reads=[B(4 * b + 1)], writes=[("qTs", b)])
                if cutB < 3:
                    continue
                banks = [pY[0], pY[1], pT[0], pT[1]]; bids = [B(4 * b + 2), B(4 * b + 3), B(4 * b), B(4 * b + 1)]
                for p in range(2):
                    for hh in range(8):
                        bi_ = 2 * p + hh // 4
                        s.op("pe", lambda e, hh=hh, p=p, bi_=bi_, QT=QT, banks=banks: e.matmul(out=banks[bi_][:, (hh % 4) * 128:(hh % 4 + 1) * 128], lhsT=QT[64 * p:64 * p + 64, hh, :], rhs=keys2[64 * p:64 * p + 64, hh, :], start=True, stop=True), reads=[("qTs", b), "keys2"], writes=[bids[bi_]])
                SC4 = SC[:].rearrange("p (h q) k -> p h q k", q=2)
                for bi_ in range(4):
                    p, hq = bi_ // 2, bi_ % 2
                    dst = SC4[:, hq * 4:(hq + 1) * 4, p, :]
                    src = banks[bi_][:].rearrange("p (a k) -> p a k", a=4)
                    allq = [("scB", b, q4) for q4 in range(4)]
                    if bi_ % 2 == 0:
                        s.op("act", lambda e, dst=dst, src=src: e.copy(out=dst, in_=src), reads=[bids[bi_]], writes=[("scB", b, 2 * hq), ("scB", b, 2 * hq + 1)])
                    else:
                        s.op("dve", lambda e, dst=dst, src=src: e.tensor_copy(out=dst, in_=src), reads=[bids[bi_]], writes=[("scB", b, 2 * hq), ("scB", b, 2 * hq + 1)])
                if cutB < 4:
                    continue
                for hb in range(2):
                    hs = range(hb * 4, hb * 4 + 4)
                    scid = lambda hh: ("scB", b, hh // 2)
                    for hh in hs:
                        for p in range(2):
                            s.op("dve", lambda e, hh=hh, p=p, SC=SC, T16=T16: e.max(out=T16[:, hh, p, 0:8], in_=SC[:, 2 * hh + p, :]), reads=[scid(hh)], writes=[("t16", b, hh, p)])
                    for hh in hs:
                        for p in range(2):
                            s.op("dve", lambda e, hh=hh, p=p, SC=SC, T16=T16: e.match_replace(out=wk4[:, hh % 4, p * 128:(p + 1) * 128], in_to_replace=T16[:, hh, p, 0:8], in_values=SC[:, 2 * hh + p, :], imm_value=-1e30), reads=[scid(hh), ("t16", b, hh, p)], writes=[("wk4", hh % 4, p)])
                    for hh in hs:
                        for p in range(2):
                            s.op("dve", lambda e, hh=hh, p=p, T16=T16: e.max(out=T16[:, hh, p, 8:16], in_=wk4[:, hh % 4, p * 128:(p + 1) * 128]), reads=[("wk4", hh % 4, p)], writes=[("t16", b, hh, p)])
                    for hh in hs:
                        s.op("dve", lambda e, hh=hh, T16=T16: e.tensor_tensor(out=cand4[:, hh % 4, :].rearrange("p (a c) -> p a c", a=16), in0=T16[:, hh, 0, :].unsqueeze(2).to_broadcast([128, 16, 16]), in1=T16[:, hh, 1, :].unsqueeze(1).to_broadcast([128, 16, 16]), op=ALU.add), reads=[("t16", b, hh, 0), ("t16", b, hh, 1)], writes=[("cand4", hh % 4)])
                    for hh in hs:
                        s.op("dve", lambda e, hh=hh, C16=C16: e.max(out=C16[:, hh, 0:8], in_=cand4[:, hh % 4, :]), reads=[("cand4", hh % 4)], writes=[("c16", b, hh)])
                    for hh in hs:
                        s.op("dve", lambda e, hh=hh, C16=C16: e.match_replace(out=wk4[:, hh % 4, :], in_to_replace=C16[:, hh, 0:8], in_values=cand4[:, hh % 4, :], imm_value=-1e30), reads=[("cand4", hh % 4), ("c16", b, hh)], writes=[("wk4", hh % 4, 0), ("wk4", hh % 4, 1)])
                    for hh in hs:
                        s.op("dve", lambda e, hh=hh, C16=C16: e.max(out=C16[:, hh, 8:16], in_=wk4[:, hh % 4, :]), reads=[("wk4", hh % 4, 0), ("wk4", hh % 4, 1)], writes=[("c16", b, hh)])
                if cutB < 5:
                    continue
                allc = [("c16", b, hh) for hh in range(8)]; allt = [("t16", b, hh, 0) for hh in range(8)]
                THR = thr[b]; NEGM = negm[b]; M1 = m1[b]; ZS = Zs[b]; KAP = kap[b]; TH2 = th2[b]; E16 = e16[b]
                s.op("dve", lambda e, C16=C16, THR=THR: e.tensor_scalar(out=THR[:], in0=C16[:, :, 15], scalar1=-1e-4, scalar2=None, op0=ALU.add), reads=allc, writes=[("thr", b)])
                s.op("dve", lambda e, C16=C16, NEGM=NEGM: e.tensor_scalar(out=NEGM[:], in0=C16[:, :, 0], scalar1=-1.0, scalar2=None, op0=ALU.mult), reads=allc, writes=[("negm", b)])
                s.op("dve", lambda e, T16=T16, M1=M1: e.tensor_copy(out=M1[:], in_=T16[:, :, 0, 0]), reads=allt, writes=[("m1", b)])
                s.op("dve", lambda e, C16=C16, NEGM=NEGM, E16=E16: e.tensor_tensor(out=E16[:], in0=C16[:], in1=NEGM[:].unsqueeze(2).to_broadcast([128, 8, 16]), op=ALU.add), reads=allc + [("negm", b)], writes=[("e16", b)])
                s.op("act", lambda e, E16=E16: e.activation(out=E16[:], in_=E16[:], func=AF.Exp), reads=[("e16", b)], writes=[("e16", b)])
                s.op("dve", lambda e, E16=E16, ZS=ZS: e.tensor_reduce(out=ZS[:], in_=E16[:], axis=AX.X, op=ALU.add), reads=[("e16", b)], writes=[("Zs", b)])
                s.op("dve", lambda e, KAP=KAP, THR=THR, NEGM=NEGM: e.tensor_tensor(out=KAP[:], in0=THR[:], in1=NEGM[:], op=ALU.add), reads=[("thr", b), ("negm", b)], writes=[("kap", b)])
                s.op("act", lambda e, KAP=KAP: e.activation(out=KAP[:], in_=KAP[:], func=AF.Exp), reads=[("kap", b)], writes=[("kap", b)])
                s.op("dve", lambda e, ZS=ZS: e.reciprocal(out=ZS[:], in_=ZS[:]), reads=[("Zs", b)], writes=[("Zs", b)])
                s.op("dve", lambda e, KAP=KAP, ZS=ZS: e.tensor_tensor(out=KAP[:], in0=KAP[:], in1=ZS[:], op=ALU.mult), reads=[("kap", b), ("Zs", b)], writes=[("kap", b)])
                s.op("dve", lambda e, TH2=TH2, THR=THR, M1=M1: e.tensor_tensor(out=TH2[:], in0=THR[:], in1=M1[:], op=ALU.subtract), reads=[("thr", b), ("m1", b)], writes=[("th2", b)])
                sc4 = SC[:].rearrange("p (h q) k -> p h q k", q=2)
                allsc = [("scB", b, q4) for q4 in range(4)]
                s.op("dve", lambda e, sc4=sc4, M1=M1: e.tensor_tensor(out=sc4[:, :, 0, :], in0=sc4[:, :, 0, :], in1=M1[:].unsqueeze(2).to_broadcast([128, 8, 128]), op=ALU.subtract), reads=allsc + [("m1", b)], writes=allsc)
                s.op("pool", lambda e, sc4=sc4, TH2=TH2: e.tensor_tensor(out=sc4[:, :, 1, :], in0=sc4[:, :, 1, :], in1=TH2[:].unsqueeze(2).to_broadcast([128, 8, 128]), op=ALU.subtract), reads=allsc + [("th2", b)], writes=allsc)
                s.op("act", lambda e, SC=SC: e.activation(out=SC[:], in_=SC[:], func=AF.Exp), reads=allsc, writes=allsc)
                s.dma(SC_s[tsl, :], SC[:].rearrange("p a b -> p (a b)"), reads=allsc, writes=["SC_s"], queue="pool")
                s.dma(KAP_s[tsl, :], KAP[:], reads=[("kap", b)], writes=["KAP_s"], queue="pool")
            s.flush()
        if upto <= 7:
            return nc

        GI = 2
        NG = 128 // GI
        NB = 2
        with contextlib.ExitStack() as st:
            _xa = T(st, "xc", [128, D]); xa = [_xa, _xa]; yb = T(st, "ybc", [128, D]); qsb = yb
            ss = T(st, "ssc", [128, 1]); rstd = T(st, "rstdc", [128, 1])
            h2r = [[TR(st, f"h2c{k}{t}", [128, 8, 128]) for t in range(NB)] for k in range(2)]
            sc = [[T(st, f"scc{k}{t}", [128, 16, 128]) for t in range(NB)] for k in range(2)]
            kap = [[T(st, f"kapc{k}{t}", [128, 8]) for t in range(NB)] for k in range(2)]
            dg = [[TR(st, f"dgc{k}{t}", [128, 8, 128]) for t in range(NB)] for k in range(2)]
            UT = [TR(st, f"UT{i}", [128, 8, GI * 128]) for i in range(2)]
            VG = [TR(st, f"VG{i}", [128, GI, D]) for i in range(2)]
            pe_t = [T(st, f"pec{t}", [128, 8, GI * 128]) for t in range(NB)]
            Mr2 = [[TR(st, f"Mr{k}{t}", [128, 8, GI * 128]) for t in range(NB)] for k in range(2)]
            Sm2 = [TR(st, f"Sm{k}", [128, 8, GI * 128]) for k in range(2)]
            negone = T(st, "negone", [128, 1])
            s.op("pool", lambda e: e.memset(negone[:], -1.0), writes=["negone"])
            W5 = NB * GI * 128
            g1 = [T(st, f"g1c{i}", [128, W5]) for i in range(2)]; Pm_ = [T(st, f"Pmc{i}", [128, W5]) for i in range(2)]
            PT = [TR(st, f"PT{i}", [128, NB * GI, 128]) for i in range(2)]
            bk = [PS(st, f"bkC{i}", [128, 512]) for i in range(8)]
            nblk = ntl6 // NB
            ngr = (2 if "small2" in dbg else NG)
            gcount = 0

            def blk_load(blk):
                k = blk % 2
                for tau in range(NB):
                    i = blk * NB + tau
                    tsl = slice(i * 128, (i + 1) * 128)
                    s.dma(h2r[k][tau][:], H2R_s[:, tsl].rearrange("(kc p) t -> p kc t", p=128), reads=["H2R_s"], writes=[("h2c", k, tau)], queue="pool")
                    s.dma(sc[k][tau][:].rearrange("p a b -> p (a b)"), SC_s[tsl, :], reads=["SC_s"], writes=[("scc", k, tau)], queue="pool")
                    s.dma(kap[k][tau][:], KAP_s[tsl, :], reads=["KAP_s"], writes=[("kapc", k, tau)], queue="pool")
                    for hh in range(8):
                        s.op("dve", lambda e, hh=hh, tau=tau, k=k: e.tensor_scalar(out=dg[k][tau][:, hh, :], in0=ident, scalar1=kap[k][tau][:, hh:hh + 1], scalar2=None, op0=ALU.mult), reads=["cm", ("kapc", k, tau)], writes=[("dgc", k, tau)])

            blk_load(0)
            for blk in range(nblk):
              kb = blk % 2
              pU = [[bk[4 + 2 * t + hf] for hf in range(2)] for t in range(NB)]
              ub = [k % 2 for k in range(ngr + 4)]

              def g_utload(g):
                  u = g % 2; ub[g] = u
                  e0 = g * GI * 128
                  s.dma(UT[u][:], UTr_s[:, e0:e0 + GI * 128].rearrange("(kc p) n -> p kc n", p=128), reads=["UTr_s"], writes=[("UT", u)], queue="sp")

              def g_vgload(g):
                  u = g % 2
                  e0 = g * GI * 128
                  s.dma(VG[u][:], Vr_s[e0:e0 + GI * 128, :].rearrange("(a p) n -> p a n", p=128), reads=["Vr_s"], writes=[("VG", u)], queue="act")

              def g_prodmask(g):
                  mk = g % 2
                  for tau in range(NB):
                      sc4 = sc[kb][tau][:].rearrange("p (h q) k -> p h q k", q=2)
                      e1b = sc4[:, :, 0, g * GI:(g + 1) * GI].unsqueeze(3).to_broadcast([128, 8, GI, 128])
                      e2b = sc4[:, :, 1, :].unsqueeze(2).to_broadcast([128, 8, GI, 128])
                      s.op("dve", lambda e, e1b=e1b, e2b=e2b, tau=tau: e.tensor_tensor(out=pe_t[tau][:].rearrange("p h (a k) -> p h a k", a=GI), in0=e1b, in1=e2b, op=ALU.mult), reads=[("scc", kb, tau)], writes=[("pe", tau)])
                      if tau == 0:
                          s.op("dve", lambda e, mk=mk: e.scalar_tensor_tensor(out=Mr2[mk][0][:], in0=pe_t[0][:], scalar=1.0, in1=pe_t[0][:], op0=ALU.is_ge, op1=ALU.mult), reads=[("pe", 0)], writes=[("Mr", mk, 0)])
                      else:
                          s.op("act", lambda e, mk=mk: e.activation(out=Mr2[mk][1][:], in_=pe_t[1][:], func=AF.Relu, bias=negone[:, 0:1], scale=1.0), reads=[("pe", 1), "negone"], writes=[("Mr", mk, 1)])
                          s.op("act", lambda e, mk=mk: e.activation(out=Sm2[mk][:], in_=Mr2[mk][1][:].bitcast(F32), func=AF.Sign), reads=[("Mr", mk, 1)], writes=[("Sm", mk)])

              def g_act(g):
                  u = ub[g]; pR = bk[u]
                  for tau in range(NB):
                      for kc in range(8):
                          s.op("pe", lambda e, kc=kc, u=u, tau=tau, pR=pR, H=h2r[kb][tau]: e.matmul(out=pR[:, tau * GI * 128:(tau + 1) * GI * 128], lhsT=H[:, kc, :], rhs=UT[u][:, kc, :], start=(kc == 0), stop=(kc == 7)), reads=[("h2c", kb, tau), ("UT", u)], writes=[B(u)])

              def g_gelu(g):
                  u = ub[g]; pR = bk[u]
                  s.op("act", lambda e, pR=pR, u=u: e.activation(out=g1[u][:], in_=pR[:, 0:W5], func=AF.Gelu_apprx_tanh), reads=[B(u)], writes=[("g1", u)])

              def g_gd(g):
                  u = ub[g]; pG = bk[2 + u]; mk = g % 2
                  for hh in range(8):
                      s.op("pe", lambda e, hh=hh, pG=pG, DG=dg[kb][0], M=Mr2[mk][0]: e.matmul(out=pG[:, 0:GI * 128], lhsT=DG[:, hh, :], rhs=M[:, hh, :], start=(hh == 0), stop=(hh == 7)), reads=[("dgc", kb, 0), ("Mr", mk, 0)], writes=[B(2 + u)])
                  for hh in range(8):
                      s.op("pe", lambda e, hh=hh, pG=pG, DG=dg[kb][1], M=Mr2[mk][1]: e.matmul(out=pG[:, GI * 128:2 * GI * 128], lhsT=DG[:, hh, :], rhs=M[:, hh, :], start=(hh == 0), stop=False), reads=[("dgc", kb, 1), ("Mr", mk, 1)], writes=[B(2 + u)])
                  for hh in range(8):
                      s.op("pe", lambda e, hh=hh, pG=pG, DG=dg[kb][1], M=Sm2[mk]: e.matmul(out=pG[:, GI * 128:2 * GI * 128], lhsT=DG[:, hh, :], rhs=M[:, hh, :], start=False, stop=(hh == 7)), reads=[("dgc", kb, 1), ("Sm", mk)], writes=[B(2 + u)])

              def g_pm(g):
                  u = ub[g]; pG = bk[2 + u]
                  s.op("dve", lambda e, u=u, pG=pG: e.tensor_tensor(out=Pm_[u][:], in0=g1[u][:], in1=pG[:, 0:W5], op=ALU.mult), reads=[("g1", u), B(2 + u)], writes=[("Pm", u)])

              def g_tr(g):
                  u = ub[g]; pW = bk[2 + u]
                  for k in range(NB * GI):
                      s.op("pe", lambda e, k=k, u=u, pW=pW: e.transpose(out=pW[:, k * 128:(k + 1) * 128], in_=Pm_[u][:, k * 128:(k + 1) * 128], identity=ident), reads=[("Pm", u), "cm"], writes=[B(2 + u)])
                  s.op("act", lambda e, u=u, pW=pW: e.copy(out=PT[u][:].rearrange("p a b -> p (a b)"), in_=pW[:, 0:W5]), reads=[B(2 + u)], writes=[("PT", u)])

              def g_out(g):
                  u = ub[g]
                  for tau in range(NB):
                      for a in range(GI):
                          for half in range(2):
                              s.op("pe", lambda e, a=a, half=half, u=u, tau=tau, first=(g == 0 and a == 0), last=(g == ngr - 1 and a == GI - 1): e.matmul(out=pU[tau][half][:], lhsT=PT[u][:, tau * GI + a, :], rhs=VG[u][:, a, half * 512:(half + 1) * 512], start=first, stop=last),
                                   reads=[("PT", u), ("VG", u)], writes=[B(4 + 2 * tau + half)])

              ok = lambda k: 0 <= k < ngr
              for g in range(-3, ngr + 1):
                  if ok(g + 3):
                      g_utload(g + 3)
                  if ok(g - 1):
                      g_out(g - 1)
                  if ok(g + 1):
                      g_vgload(g + 1)
                      g_gd(g + 1)
                  if ok(g):
                      g_tr(g)
                  if ok(g + 2):
                      g_act(g + 2); g_gelu(g + 2)
                  if ok(g + 3):
                      g_prodmask(g + 3)
                  if ok(g + 1):
                      g_pm(g + 1)
                  if g == 4 and blk + 1 < nblk:
                      blk_load(blk + 1)
              for tau in range(NB):
                i = blk * NB + tau
                XA = xa[tau]; xid = "xc"
                s.dma(XA[:], X1_s[i * 128:(i + 1) * 128, :], reads=["X1_s"], writes=[xid], queue="pool")
                for half in range(2):
                    s.op("dve", lambda e, half=half, tau=tau: e.tensor_tensor(out=yb[:, half * 512:(half + 1) * 512], in0=pU[tau][half][:], in1=bvv(BV_GT2)[:, half * 512:(half + 1) * 512], op=ALU.mult), reads=[B(4 + 2 * tau + half), "bv"], writes=["ybc"])
                s.op("pool", lambda e, XA=XA: e.tensor_tensor(out=XA[:], in0=XA[:], in1=yb[:], op=ALU.add), reads=[xid, "ybc"], writes=[xid])
                s.op("act", lambda e, XA=XA: e.activation(out=qsb[:], in_=XA[:], func=AF.Square, accum_out=ss[:]), reads=[xid, "ybc"], writes=["ybc", "ssc"])
                s.op("dve", lambda e: e.tensor_scalar(out=rstd[:], in0=ss[:], scalar1=1.0 / D, scalar2=1e-6, op0=ALU.mult, op1=ALU.add), reads=["ssc"], writes=["rstdc"])
                s.op("act", lambda e: e.sqrt(out=rstd[:], in_=rstd[:]), reads=["rstdc"], writes=["rstdc"])
                s.op("dve", lambda e: e.reciprocal(out=rstd[:], in_=rstd[:]), reads=["rstdc"], writes=["rstdc"])
                s.op("dve", lambda e, XA=XA: e.scalar_tensor_tensor(out=yb[:], in0=XA[:], scalar=rstd[:, 0:1], in1=bvv(BV_FN), op0=ALU.mult, op1=ALU.mult), reads=[xid, "rstdc", "bv"], writes=["ybc"])
                s.dma(out_d[i * 128:(i + 1) * 128, :], yb[:], reads=["ybc"], writes=["out"], queue="pool")
            s.flush()
        return nc


def _rope(s, src, dst, tmp, R, rid, H, sid, did, tid="tmp"):
    sv = src.rearrange("p (h a b c) -> p h a b c", h=H, a=2, b=2)
    dv = dst.rearrange("p (h a b c) -> p h a b c", h=H, a=2, b=2)
    tv = tmp[:, 0:H * 32].rearrange("p (h a c) -> p h a c", h=H, a=2)
    rv = R[:].rearrange("p (a b c) -> p a b c", a=2, b=2)
    cosb = rv[:, :, 0, :].unsqueeze(1).to_broadcast([128, H, 2, 16])
    sinb = rv[:, :, 1, :].unsqueeze(1).to_broadcast([128, H, 2, 16])
    x1 = sv[:, :, :, 0, :]; x2 = sv[:, :, :, 1, :]
    o1 = dv[:, :, :, 0, :]; o2 = dv[:, :, :, 1, :]
    s.op("dve", lambda e: e.tensor_tensor(out=o1, in0=x1, in1=cosb, op=ALU.mult), reads=[sid, rid], writes=[did])
    s.op("dve", lambda e: e.tensor_tensor(out=tv, in0=x2, in1=sinb, op=ALU.mult), reads=[sid, rid], writes=[tid])
    s.op("dve", lambda e: e.tensor_tensor(out=o1, in0=o1, in1=tv, op=ALU.subtract), reads=[did, tid], writes=[did])
    s.op("dve", lambda e: e.tensor_tensor(out=o2, in0=x1, in1=sinb, op=ALU.mult), reads=[sid, rid, did], writes=[did])
    s.op("dve", lambda e: e.tensor_tensor(out=tv, in0=x2, in1=cosb, op=ALU.mult), reads=[sid, rid, did], writes=[tid])
    s.op("dve", lambda e: e.tensor_tensor(out=o2, in0=o2, in1=tv, op=ALU.add), reads=[did, tid], writes=[did])


def _host_inputs(inputs, b, consts):
    g = lambda k: np.ascontiguousarray(inputs[k], dtype=np.float32)
    m = {
        "x": g("x")[b], "c": g("c")[b], "ctx": g("ctx")[b], "c_ctx": g("c_ctx"),
        "w_ada": g("w_ada")[0], "b_ada": g("b_ada")[0], "norm_mix": g("norm_mix")[0], "norm_ffn": g("norm_ffn")[0],
        "w_in": g("w_in")[0], "b_gate": g("b_gate")[0], "attn_sink": g("attn_sink")[0], "dn_conv": g("dn_conv")[0],
        "dn_a_log_f": g("dn_a_log_f")[0], "dn_dt_bias_f": g("dn_dt_bias_f")[0], "dn_a_log_b": g("dn_a_log_b")[0], "dn_dt_bias_b": g("dn_dt_bias_b")[0],
        "dn_norm": g("dn_norm")[0], "w_br_attn": g("w_br_attn")[0], "w_br_dn": g("w_br_dn")[0], "w_out": g("w_out")[0],
        "peer_wq": g("peer_wq")[0], "final_norm": g("final_norm"),
    }
    m.update(consts)
    return {k: np.ascontiguousarray(v) for k, v in m.items()}


_SHARED = {}


def kernel(**inputs):
    consts = _consts()
    nc = build()
    keysT = np.ascontiguousarray(np.transpose(np.asarray(inputs["peer_keys"], np.float32)[0], (1, 3, 0, 2)).reshape(128, 8, 128))
    uT = np.ascontiguousarray(np.asarray(inputs["peer_u"], np.float32)[0].T)
    pv = np.ascontiguousarray(np.asarray(inputs["peer_v"], np.float32)[0])
    in_maps = []
    for b in range(8):
        m = _host_inputs(inputs, b, consts)
        m["peer_keysT"] = keysT; m["peer_uT"] = uT; m["peer_v"] = pv
        in_maps.append(m)
    res = run_bass_kernel_spmd(nc, in_maps, core_ids=list(range(8)))
    return np.stack([np.asarray(r["out"], dtype=np.float32) for r in res.results], axis=0)
```

```python
import contextlib
import numpy as np
import concourse.bass as bass
import concourse.mybir as mybir
from concourse.bass_utils import run_bass_kernel_spmd

F32 = mybir.dt.float32
F32R = mybir.dt.float32r
ALU = mybir.AluOpType
AF = mybir.ActivationFunctionType
AX = mybir.AxisListType

D = 1024
S = 8192
CTX = 256
TALL = CTX + S
NT = S // 128
IN_COLS = 4880
NEG = -30000.0


class _Ins:
    __slots__ = ("eng", "fn", "deps", "signal", "sig_no", "dma", "idx")

    def __init__(self, eng, fn, dma=None):
        self.eng = eng
        self.fn = fn
        self.deps = []
        self.signal = False
        self.sig_no = None
        self.dma = dma
        self.idx = None


class Sch:
    EPOCH = 20000
    NDMA = 24
    NEP = 16

    def __init__(self, nc, st):
        self.nc = nc
        self.engs = ("pe", "act", "dve", "pool", "sp")
        self.nep = {"pe": 12, "act": 4, "dve": 6, "pool": 3, "sp": 1}
        self.sems = {e: [st.enter_context(nc.semaphore(f"s_{e}_{i}")) for i in range(self.nep[e])] for e in self.engs}
        self.dsems = [st.enter_context(nc.semaphore(f"s_dma_{i}")) for i in range(self.NDMA)]
        self.sigc = {e: 0 for e in self.engs}
        self.dma_rr = 0
        self.dma_cnt = [0] * self.NDMA
        self.dma_last = [None] * self.NDMA
        self._reset()

    def _reset(self):
        self.q = {e: [] for e in self.engs}
        self.lastw = {}
        self.readers = {}

    def _add(self, ins, reads, writes):
        q = self.q[ins.eng]
        ins.idx = len(q)
        deps = []
        for r in reads:
            w = self.lastw.get(r)
            if w is not None:
                deps.append((w, "raw"))
        for w_ in writes:
            w = self.lastw.get(w_)
            if w is not None:
                deps.append((w, "waw"))
            for rd in self.readers.get(w_, ()):
                deps.append((rd, "war"))
        for d, kind in deps:
            if d is ins:
                continue
            if d.dma is None and ins.dma is None and d.eng == ins.eng:
                if ins.eng == "pe":
                    continue
                if kind != "raw":
                    continue
            ins.deps.append(d)
            if d.dma is None:
                d.signal = True
        for r in reads:
            self.readers.setdefault(r, []).append(ins)
        for w_ in writes:
            self.lastw[w_] = ins
            self.readers[w_] = []
        q.append(ins)
        return ins

    PSUM_NAMES = {"bk", "pm", "pT", "pY", "pX", "pN", "pK", "pb", "pS", "pO", "pQ", "pZ", "pR", "pU", "pW"}

    def op(self, eng, fn, reads=(), writes=()):
        writes = list(writes)
        if eng != "pe":
            for r in reads:
                if isinstance(r, tuple) and r[0] in self.PSUM_NAMES and r not in writes:
                    writes.append(r)
        return self._add(_Ins(eng, fn), list(reads), writes)

    def dma(self, out, in_, reads=(), writes=(), queue="sp", **kw):
        slot = self.dma_rr
        self.dma_rr = (self.dma_rr + 1) % self.NDMA
        self.dma_cnt[slot] += 1
        n = self.dma_cnt[slot]
        ins = _Ins(queue, lambda e: e.dma_start(out=out, in_=in_, **kw), dma=(slot, n))
        prev = self.dma_last[slot]
        self._add(ins, list(reads), list(writes))
        if prev is not None:
            ins.deps.append(prev)
        self.dma_last[slot] = ins
        return ins

    def flush(self):
        nc = self.nc
        for e, q in self.q.items():
            for ins in q:
                if ins.dma is None and ins.signal:
                    ins.sig_no = self.sigc[e]
                    self.sigc[e] += 1
            assert self.sigc[e] < self.EPOCH * self.nep[e], f"too many signals on {e}: {self.sigc[e]}"
        dma_final = list(self.dma_cnt)
        with nc.Block() as block:
            def run(ename):
                def body(eng):
                    seen_c = {}
                    seen_d = {}
                    for ins in self.q[ename]:
                        wc = {}
                        wd = {}
                        for d in ins.deps:
                            if d.dma is None:
                                if d.sig_no is None:
                                    continue
                                if seen_c.get(d.eng, -1) < d.sig_no:
                                    wc[d.eng] = max(wc.get(d.eng, -1), d.sig_no)
                            else:
                                s_, n = d.dma
                                if seen_d.get(s_, 0) < n:
                                    wd[s_] = max(wd.get(s_, 0), n)
                        for e2, sn in wc.items():
                            eng.wait_ge(self.sems[e2][sn // self.EPOCH], sn % self.EPOCH + 1)
                            seen_c[e2] = sn
                        for s_, n in wd.items():
                            eng.wait_ge(self.dsems[s_], 16 * n)
                            seen_d[s_] = n
                        h = ins.fn(eng)
                        if ins.dma is not None:
                            h.then_inc(self.dsems[ins.dma[0]], 16)
                        elif ins.signal:
                            h.then_inc(self.sems[ename][ins.sig_no // self.EPOCH], 1)
                    if ename == "sp":
                        for s_, n in enumerate(dma_final):
                            if n > 0:
                                eng.wait_ge(self.dsems[s_], 16 * n)
                return body

            block.sync(run("sp"))
            block.tensor(run("pe"))
            block.scalar(run("act"))
            block.vector(run("dve"))
            block.gpsimd(run("pool"))
        nc.all_engine_barrier()
        self._reset()


def _consts():
    c = {}
    ident = np.eye(128, dtype=np.float32)
    ones = np.ones((128, 128), np.float32)
    idx = np.arange(128)
    same = (idx[:, None] // 64 == idx[None, :] // 64).astype(np.float32)
    m1f = ((idx[:, None] <= idx[None, :]) * same).astype(np.float32)
    m1b = ((idx[:, None] >= idx[None, :]) * same).astype(np.float32)
    sel0 = np.zeros((128, 128), np.float32); sel0[:64, :] = 1
    sel1 = np.zeros((128, 128), np.float32); sel1[64:, :] = 1
    low_incl = ((idx[None, :] <= idx[:, None]) * same)
    up_incl = ((idx[None, :] >= idx[:, None]) * same)
    low_strict = ((idx[None, :] < idx[:, None]) * same)
    up_strict = ((idx[None, :] > idx[:, None]) * same)
    negmask = lambda m: np.where(m > 0, 0.0, NEG).astype(np.float32)
    w_prev = (idx[None, :] <= idx[:, None]).astype(np.float32)
    w_next = (idx[:, None] <= idx[None, :]).astype(np.float32)
    mats = [ident, ones, same, m1f, -m1f, m1b, -m1b, sel0, sel1,
            negmask(low_incl), negmask(up_incl), -low_strict.astype(np.float32), -up_strict.astype(np.float32),
            w_prev, w_next]
    c["cmat"] = np.ascontiguousarray(np.stack(mats, axis=1)).astype(np.float32)
    pos = np.arange(S)
    inv = (10000.0 ** (-np.arange(16, dtype=np.float32) / 16)).astype(np.float32)
    ar = (pos // 64).astype(np.float32)[:, None] * inv[None, :]
    ac = (pos % 64).astype(np.float32)[:, None] * inv[None, :]
    c["rope"] = np.concatenate([np.cos(ar), np.sin(ar), np.cos(ac), np.sin(ac)], axis=1).astype(np.float32)
    return c

(C_ID, C_ONES, C_SAME, C_M1F, C_NM1F, C_M1B, C_NM1B, C_SEL0, C_SEL1, C_NLOW, C_NUP, C_SLOW, C_SUP, C_WPREV, C_WNEXT) = range(15)


def build(upto=99, dbg=()):
    nc = bass.Bass("TRN2", target_bir_lowering=False)
    nc.dge_precook = False
    inp = lambda name, shape: nc.dram_tensor(name, list(shape), F32, kind="ExternalInput").ap()
    x_d = inp("x", [S, D]); c_d = inp("c", [D]); ctx_d = inp("ctx", [CTX, D]); cctx_d = inp("c_ctx", [D])
    wada_d = inp("w_ada", [D, 6 * D]); bada_d = inp("b_ada", [6 * D])
    nmix_d = inp("norm_mix", [D]); nffn_d = inp("norm_ffn", [D])
    win_d = inp("w_in", [D, IN_COLS]); bgate_d = inp("b_gate", [2 * D])
    sink_d = inp("attn_sink", [8]); conv_d = inp("dn_conv", [5, 1536])
    alf_d = inp("dn_a_log_f", [4]); dtf_d = inp("dn_dt_bias_f", [4]); alb_d = inp("dn_a_log_b", [4]); dtb_d = inp("dn_dt_bias_b", [4])
    dnn_d = inp("dn_norm", [128]); wba_d = inp("w_br_attn", [512, D]); wbd_d = inp("w_br_dn", [512, D]); wout_d = inp("w_out", [D, D])
    pwq_d = inp("peer_wq", [D, D]); pkeys_d = inp("peer_keysT", [128, 8, 128]); pu_d = inp("peer_uT", [D, 16384]); pv_d = inp("peer_v", [16384, D])
    fnorm_d = inp("final_norm", [D]); cmat_d = inp("cmat", [128, 15, 128]); rope_d = inp("rope", [S, 64])
    out_d = nc.dram_tensor("out", [S, D], F32, kind="ExternalOutput").ap()
    scr = lambda name, shape: nc.dram_tensor(name, list(shape), F32, kind=("ExternalOutput" if name in dbg else "Internal")).ap()
    QT_s = scr("QT_s", [64, 8, S])
    KT_s = scr("KT_s", [64, 2, TALL])
    V_s = scr("V_s", [TALL, 2, 65])
    RT_s = scr("RT_s", [1536, TALL])
    Z_s = scr("Z_s", [S, 512])
    GB_s = scr("GB_s", [TALL, 16])
    GT_s = scr("GT_s", [S, 2048])
    QK_s = scr("QK_s", [1024, TALL])
    KV_s = scr("KV_s", [TALL, 1024])
    OD_s = scr("OD_s", [2, S, 512])
    OA_s = scr("OA_s", [S, 512])
    MOD_s = scr("MOD_s", [8, D])

    with contextlib.ExitStack() as gst:
        s = Sch(nc, gst)
        _uid = [0]

        def _nm(name):
            _uid[0] += 1
            return f"{name}_u{_uid[0]}"
        T = lambda st, name, shape: st.enter_context(nc.sbuf_tensor(_nm(name), list(shape), F32))
        PS = lambda st, name, shape: st.enter_context(nc.psum_tensor(_nm(name), list(shape), F32))
        cm = T(gst, "cm", [128, 15, 128])
        s.dma(cm[:], cmat_d, writes=["cm"])
        ident = cm[:, C_ID, :]
        BV_G1, BV_SH1, BV_GT1, BV_G2, BV_SH2, BV_GT2, BV_CG1, BV_CSH1, BV_FN = range(9)
        bvB = T(gst, "bvB", [128, 5, D])
        stA = contextlib.ExitStack()
        bvA = T(stA, "bvA", [128, 4, D])
        _amap = {BV_G1: 0, BV_SH1: 1, BV_CG1: 2, BV_CSH1: 3}
        _bmap = {BV_GT1: 0, BV_G2: 1, BV_SH2: 2, BV_GT2: 3, BV_FN: 4}

        def bvv(k):
            return bvA[:, _amap[k], :] if k in _amap else bvB[:, _bmap[k], :]

        with contextlib.ExitStack() as st:
            cc = T(st, "cc", [128, 2, 8]); cs = T(st, "cs", [128, 2, 8]); lh = T(st, "lh", [128, 2, 8, 128])
            wa = [T(st, f"wa{i}", [128, 8, 512]) for i in range(2)]
            bb = T(st, "bb", [128, 6 * D]); nm = T(st, "nm", [128, 2, D])
            pm = [PS(st, f"pm{i}", [128, 512]) for i in range(2)]
            s.dma(cc[:, 0, :], c_d.rearrange("(kc p) -> p kc", p=128), writes=["cc"], allow_slow_non_contiguous=True)
            s.dma(cc[:, 1, :], cctx_d.rearrange("(kc p) -> p kc", p=128), writes=["cc"], allow_slow_non_contiguous=True)
            s.dma(bb[:], bada_d.partition_broadcast(128), writes=["bb"])
            s.dma(nm[:, 0, :], nmix_d.partition_broadcast(128), writes=["nm"])
            s.dma(nm[:, 1, :], nffn_d.partition_broadcast(128), writes=["nm"])
            s.dma(bvv(BV_FN)[:, :], fnorm_d.partition_broadcast(128), writes=["bv"])
            s.op("act", lambda e: e.activation(out=cs[:], in_=cc[:], func=AF.Silu), reads=["cc"], writes=["cs"])
            s.op("dve", lambda e: e.tensor_copy(out=lh[:], in_=cs[:].unsqueeze(3).to_broadcast([128, 2, 8, 128])), reads=["cs"], writes=["lh"])
            jobs = [(0, nb) for nb in range(12)] + [(1, nb) for nb in range(4)]
            for ji, (w, nb) in enumerate(jobs):
                wt = wa[ji % 2]; p = pm[ji % 2]
                s.dma(wt[:], wada_d[:, nb * 512:(nb + 1) * 512].rearrange("(kc p) n -> p kc n", p=128), writes=[("wa", ji % 2)], queue=("sp" if ji % 2 == 0 else "act"))
                for kc in range(8):
                    s.op("pe", lambda e, w=w, kc=kc, wt=wt, p=p: e.matmul(out=p[:], lhsT=lh[:, w, kc, :], rhs=wt[:, kc, :], start=(kc == 0), stop=(kc == 7)),
                         reads=["lh", ("wa", ji % 2)], writes=[("pm", ji % 2)])
                ch, half = nb // 2, nb % 2
                if w == 0:
                    dst = {0: BV_SH1, 1: BV_G1, 2: BV_GT1, 3: BV_SH2, 4: BV_G2, 5: BV_GT2}[ch]
                else:
                    dst = {0: BV_CSH1, 1: BV_CG1}[ch]
                o = bvv(dst)[:, half * 512:(half + 1) * 512]
                s.op("dve", lambda e, o=o, p=p, nb=nb: e.tensor_tensor(out=o, in0=p[:], in1=bb[:, nb * 512:(nb + 1) * 512], op=ALU.add),
                     reads=[("pm", ji % 2), "bb"], writes=["bv"])
            for dst, ni in ((BV_G1, 0), (BV_G2, 1), (BV_CG1, 0)):
                s.op("dve", lambda e, dst=dst, ni=ni: e.scalar_tensor_tensor(out=bvv(dst)[:, :], in0=bvv(dst)[:, :], scalar=1.0, in1=nm[:, ni, :], op0=ALU.add, op1=ALU.mult),
                     reads=["bv", "nm"], writes=["bv"])
            s.flush()
        if upto <= 0:
            stA.close()
            return nc

        blocks = [(0, 512), (512, 256), (768, 512), (1280, 512), (1792, 512), (2304, 512), (2816, 16)] + [(2832 + 512 * i, 512) for i in range(4)]
        with contextlib.ExitStack() as st:
            xt = [T(st, f"xt{i}", [128, D]) for i in range(2)]
            junk = T(st, "junk", [128, D]); ss = T(st, "ss", [128, 1]); rstd = T(st, "rstd", [128, 1])
            h = T(st, "h", [128, D]); hT = T(st, "hT", [128, 8, 128])
            wb = [st.enter_context(nc.sbuf_tensor(_nm(f"wb{i}"), [128, 8, 512], F32R)) for i in range(3)]
            rp = [T(st, f"rp{i}", [128, 64]) for i in range(2)]
            qs = T(st, "qs", [128, 512]); qr = T(st, "qr", [128, 512]); tmp = T(st, "tmp", [128, 512])
            qT = T(st, "qT", [64, 8, 128]); kvs = T(st, "kvs", [128, 256]); kr = T(st, "kr", [128, 128]); kT = T(st, "kT", [64, 2, 128])
            va = T(st, "va", [128, 2, 65]); rw = T(st, "rw", [128, 512]); rT = T(st, "rT", [128, 4, 128])
            zz = T(st, "zz", [128, 512]); gn = T(st, "gn", [128, 128]); ab = T(st, "ab", [128, 16]); abc = T(st, "abc", [128, 2, 8])
            gbo = T(st, "gbo", [128, 16]); gg = T(st, "gg", [128, 512]); bg = T(st, "bg", [128, 2048])
            pT = [PS(st, f"pT{i}", [128, 512]) for i in range(2)]
            pY = [PS(st, f"pY{i}", [128, 512]) for i in range(3)]
            pX = [PS(st, f"pX{i}", [128, 512]) for i in range(2)]
            s.dma(bg[:], bgate_d.partition_broadcast(128), writes=["bg"])
            s.dma(gn[:], dnn_d.partition_broadcast(128), writes=["gn"])
            s.dma(abc[:, 0, 0:4], dtf_d.partition_broadcast(128), writes=["abc"])
            s.dma(abc[:, 0, 4:8], dtb_d.partition_broadcast(128), writes=["abc"])
            s.dma(abc[:, 1, 0:4], alf_d.partition_broadcast(128), writes=["abc"])
            s.dma(abc[:, 1, 4:8], alb_d.partition_broadcast(128), writes=["abc"])
            s.op("act", lambda e: e.activation(out=abc[:, 1, :], in_=abc[:, 1, :], func=AF.Exp), reads=["abc"], writes=["abc"])
            s.op("dve", lambda e: e.tensor_scalar(out=abc[:, 1, :], in0=abc[:, 1, :], scalar1=-1.0, scalar2=None, op0=ALU.mult), reads=["abc"], writes=["abc"])
            s.op("pool", lambda e: e.memset(va[:], 1.0), writes=[("va", 0)])
            wcount = [0]

            def rope_ops(src, dst, H):
                sv = src.rearrange("p (h a b c) -> p h a b c", h=H, a=2, b=2)
                dv = dst.rearrange("p (h a b c) -> p h a b c", h=H, a=2, b=2)
                tv = tmp[:, 0:H * 64].rearrange("p (h a b c) -> p h a b c", h=H, a=2, b=2)
                return sv, dv, tv

            tiles = [("c", i) for i in range(CTX // 128)] + [("l", i) for i in range(NT)]
            if upto == 1 and "small" in dbg:
                tiles = tiles[:4]
            qs2 = [qs, T(st, "qsb_", [128, 512])]; qr2 = [qr, T(st, "qrb_", [128, 512])]; tmp2 = [tmp, T(st, "tmpb_", [128, 512])]
            qT2 = [qT, T(st, "qTb_", [64, 8, 128])]; kvs2 = [kvs, T(st, "kvsb_", [128, 256])]; kr2 = [kr, T(st, "krb_", [128, 128])]; kT2 = [kT, T(st, "kTb_", [64, 2, 128])]
            va2 = [va, T(st, "vab_", [128, 2, 65])]; rw2 = [rw, T(st, "rwb_", [128, 512])]; rT2 = [rT, T(st, "rTb_", [128, 4, 128])]; zz2 = [zz, T(st, "zzb_", [128, 512])]
            ab2 = [ab, T(st, "abb_", [128, 16])]; gbo2 = [gbo, T(st, "gbob_", [128, 16])]; gg2 = [gg, T(st, "ggb_", [128, 512])]
            s.op("pool", lambda e: e.memset(va2[1][:], 1.0), writes=[("va", 1)])
            hT4 = [st.enter_context(nc.sbuf_tensor(_nm(f"hT4_{j}"), [128, 8, 128], F32R)) for j in range(4)]
            rp4 = [T(st, f"rp4_{j}", [128, 64]) for j in range(4)]
            pcount = [0]

            def prep(ti, kind, i, j):
                lat = kind == "l"
                src = x_d if lat else ctx_d
                tg = ti
                X = xt[ti % 2]; xid = ("xt", ti % 2)
                s.dma(X[:], src[i * 128:(i + 1) * 128, :], writes=[xid])
                if lat:
                    R = rp4[j]; rid = ("rp", j)
                    s.dma(R[:], rope_d[i * 128:(i + 1) * 128, :], writes=[rid], queue="act")
                s.op("act", lambda e, X=X: e.activation(out=junk[:], in_=X[:], func=AF.Square, accum_out=ss[:]), reads=[xid], writes=["junk", "ss"])
                s.op("dve", lambda e: e.tensor_scalar(out=rstd[:], in0=ss[:], scalar1=1.0 / D, scalar2=1e-6, op0=ALU.mult, op1=ALU.add), reads=["ss"], writes=["rstd"])
                s.op("act", lambda e: e.sqrt(out=rstd[:], in_=rstd[:]), reads=["rstd"], writes=["rstd"])
                s.op("dve", lambda e: e.reciprocal(out=rstd[:], in_=rstd[:]), reads=["rstd"], writes=["rstd"])
                G = BV_G1 if lat else BV_CG1
                SH = BV_SH1 if lat else BV_CSH1
                s.op("dve", lambda e, X=X, G=G: e.scalar_tensor_tensor(out=h[:], in0=X[:], scalar=rstd[:, 0:1], in1=bvv(G)[:, :], op0=ALU.mult, op1=ALU.mult), reads=[xid, "rstd", "bv"], writes=["h"])
                s.op("pool", lambda e, SH=SH: e.tensor_tensor(out=h[:], in0=h[:], in1=bvv(SH)[:, :], op=ALU.add), reads=["h", "bv"], writes=["h"])
                for hb in range(2):
                    for k4 in range(4):
                        kc = hb * 4 + k4
                        s.op("pe", lambda e, kc=kc, hb=hb, k4=k4: e.transpose(out=pT[hb][:, k4 * 128:(k4 + 1) * 128], in_=h[:, kc * 128:(kc + 1) * 128], identity=ident), reads=["h", "cm"], writes=[("pT", hb)])
                    eng = "act" if hb == 0 else "dve"
                    if eng == "act":
                        s.op("act", lambda e, hb=hb: e.copy(out=hT4[j][:, hb * 4:(hb + 1) * 4, :].rearrange("p a b -> p (a b)"), in_=pT[hb][:]), reads=[("pT", hb)], writes=[("hT", j, hb)])
                    else:
                        s.op("dve", lambda e, hb=hb: e.tensor_copy(out=hT4[j][:, hb * 4:(hb + 1) * 4, :].rearrange("p a b -> p (a b)"), in_=pT[hb][:]), reads=[("pT", hb)], writes=[("hT", j, hb)])

            def proj(ti, kind, i, j, bi, W, wi):
                lat = kind == "l"; tg = ti; R = rp4[j]; rid = ("rp", j)
                c0, cw = blocks[bi]
                pi_ = pcount[0] % 3; pp = pcount[0] % 2; pcount[0] += 1
                P = pY[pi_]
                for kc in range(8):
                    s.op("pe", lambda e, kc=kc, W=W, P=P, cw=cw: e.matmul(out=P[:, 0:cw], lhsT=hT4[j][:, kc, :], rhs=W[:, kc, 0:cw], start=(kc == 0), stop=(kc == 7)),
                         reads=[("hT", j, 0), ("hT", j, 1), ("wb", wi)], writes=[("pY", pi_)])
                pid = ("pY", pi_)
                if bi == 0:
                    s.op("act", lambda e, P=P: e.activation(out=qs2[pp][:], in_=P[:], func=AF.Copy, scale=0.125), reads=[pid], writes=[("qs", pp)])
                    _rope(s, qs2[pp][:], qr2[pp][:], tmp2[pp], R, rid, 8, ("qs", pp), ("qr", pp), ("tmp", pp))
                    for hh in range(8):
                        s.op("pe", lambda e, hh=hh: e.transpose(out=pX[hh // 4][0:64, (hh % 4) * 128:(hh % 4 + 1) * 128], in_=qr2[pp][:, hh * 64:(hh + 1) * 64], identity=ident), reads=[("qr", pp), "cm"], writes=[("pX", hh // 4)])
                    s.op("act", lambda e: e.copy(out=qT2[pp][:, 0:4, :].rearrange("p a b -> p (a b)"), in_=pX[0][0:64, :]), reads=[("pX", 0)], writes=[("qT", pp)])
                    s.op("dve", lambda e: e.tensor_copy(out=qT2[pp][:, 4:8, :].rearrange("p a b -> p (a b)"), in_=pX[1][0:64, :]), reads=[("pX", 1)], writes=[("qT", pp)])
                    s.dma(QT_s[:, :, i * 128:(i + 1) * 128], qT2[pp][:], reads=[("qT", pp)], writes=["QT_s"], queue="sp")
                elif bi == 1:
                    s.op("act", lambda e, P=P: e.copy(out=kvs2[pp][:], in_=P[:, 0:256]), reads=[pid], writes=[("kvs", pp)])
                    if lat:
                        _rope(s, kvs2[pp][:, 0:128], kr2[pp][:], tmp2[pp], R, rid, 2, ("kvs", pp), ("kr", pp), ("tmp", pp))
                        ksrc, kid = kr2[pp], ("kr", pp)
                    else:
                        ksrc, kid = kvs2[pp], ("kvs", pp)
                    for hh in range(2):
                        s.op("pe", lambda e, hh=hh, ksrc=ksrc: e.transpose(out=pX[0][0:64, hh * 128:(hh + 1) * 128], in_=ksrc[:, hh * 64:(hh + 1) * 64], identity=ident), reads=[kid, "cm"], writes=[("pX", 0)])
                    s.op("act", lambda e: e.copy(out=kT2[pp][:].rearrange("p a b -> p (a b)"), in_=pX[0][0:64, 0:256]), reads=[("pX", 0)], writes=[("kT", pp)])
                    s.dma(KT_s[:, :, tg * 128:(tg + 1) * 128], kT2[pp][:], reads=[("kT", pp)], writes=["KT_s"], queue="sp")
                    s.op("pool", lambda e: e.tensor_copy(out=va2[pp][:, :, 0:64], in_=kvs2[pp][:, 128:256].rearrange("p (g d) -> p g d", g=2)), reads=[("kvs", pp)], writes=[("va", pp)])
                    s.dma(V_s[tg * 128:(tg + 1) * 128, :, :], va2[pp][:], reads=[("va", pp)], writes=["V_s"], queue="sp")
                elif bi in (2, 3, 4):
                    s.op("act", lambda e, P=P: e.copy(out=rw2[pp][:], in_=P[:]), reads=[pid], writes=[("rw", pp)])
                    for k4 in range(4):
                        s.op("pe", lambda e, k4=k4: e.transpose(out=pX[1][:, k4 * 128:(k4 + 1) * 128], in_=rw2[pp][:, k4 * 128:(k4 + 1) * 128], identity=ident), reads=[("rw", pp), "cm"], writes=[("pX", 1)])
                    s.op("dve", lambda e: e.tensor_copy(out=rT2[pp][:].rearrange("p a b -> p (a b)"), in_=pX[1][:]), reads=[("pX", 1)], writes=[("rT", pp)])
                    f0 = (bi - 2) * 512
                    s.dma(RT_s[f0:f0 + 512, tg * 128:(tg + 1) * 128].rearrange("(a p) t -> p a t", p=128), rT2[pp][:], reads=[("rT", pp)], writes=["RT_s"], queue="sp")
                elif bi == 5:
                    s.op("act", lambda e, P=P: e.activation(out=zz2[pp][:], in_=P[:], func=AF.Silu), reads=[pid], writes=[("zz", pp)])
                    s.op("pool", lambda e: e.tensor_tensor(out=zz2[pp][:].rearrange("p (h d) -> p h d", h=4), in0=zz2[pp][:].rearrange("p (h d) -> p h d", h=4), in1=gn[:].unsqueeze(1).to_broadcast([128, 4, 128]), op=ALU.mult), reads=[("zz", pp), "gn"], writes=[("zz", pp)])
                    s.dma(Z_s[i * 128:(i + 1) * 128, :], zz2[pp][:], reads=[("zz", pp)], writes=["Z_s"], queue="sp")
                elif bi == 6:
                    s.op("dve", lambda e, P=P: e.tensor_tensor(out=ab2[pp][:, 0:8], in0=P[:, 0:8], in1=abc[:, 0, :], op=ALU.add), reads=[pid, "abc"], writes=[("ab", pp)])
                    s.op("act", lambda e: e.activation(out=ab2[pp][:, 0:8], in_=ab2[pp][:, 0:8], func=AF.Exp), reads=[("ab", pp)], writes=[("ab", pp)])
                    s.op("dve", lambda e: e.tensor_scalar(out=ab2[pp][:, 0:8], in0=ab2[pp][:, 0:8], scalar1=1.0, scalar2=None, op0=ALU.add), reads=[("ab", pp)], writes=[("ab", pp)])
                    s.op("act", lambda e: e.activation(out=ab2[pp][:, 0:8], in_=ab2[pp][:, 0:8], func=AF.Ln), reads=[("ab", pp)], writes=[("ab", pp)])
                    s.op("dve", lambda e: e.tensor_tensor(out=gbo2[pp][:, 0:8], in0=ab2[pp][:, 0:8], in1=abc[:, 1, :], op=ALU.mult), reads=[("ab", pp), "abc"], writes=[("gbo", pp)])
                    s.op("act", lambda e, P=P: e.activation(out=gbo2[pp][:, 8:16], in_=P[:, 8:16], func=AF.Sigmoid), reads=[pid], writes=[("gbo", pp)])
                    s.dma(GB_s[tg * 128:(tg + 1) * 128, :], gbo2[pp][:], reads=[("gbo", pp)], writes=["GB_s"], queue="sp")
                else:
                    gi = bi - 7
                    s.op("dve", lambda e, P=P, gi=gi: e.tensor_tensor(out=gg2[pp][:], in0=P[:], in1=bg[:, gi * 512:(gi + 1) * 512], op=ALU.add), reads=[pid, "bg"], writes=[("gg", pp)])
                    s.op("act", lambda e: e.activation(out=gg2[pp][:], in_=gg2[pp][:], func=AF.Sigmoid), reads=[("gg", pp)], writes=[("gg", pp)])
                    s.dma(GT_s[i * 128:(i + 1) * 128, gi * 512:(gi + 1) * 512], gg2[pp][:], reads=[("gg", pp)], writes=["GT_s"], queue="sp")

            groups = [tiles[0:2]] + [tiles[k:k + 4] for k in range(2, len(tiles), 4)]
            tbase = 0
            for grp in groups:
                for j, (kind, i) in enumerate(grp):
                    prep(tbase + j, kind, i, j)
                need = range(11) if grp[0][0] == "l" else (1, 2, 3, 4, 6)
                for bi in need:
                    c0, cw = blocks[bi]
                    wi = wcount[0] % 3; wcount[0] += 1
                    W = wb[wi]
                    s.dma(W[:, :, 0:cw], win_d[:, c0:c0 + cw].rearrange("(kc p) n -> p kc n", p=128), writes=[("wb", wi)], queue="pool", allow_slow_non_contiguous=(cw < 128))
                    for j, (kind, i) in enumerate(grp):
                        proj(tbase + j, kind, i, j, bi, W, wi)
                tbase += len(grp)
            s.flush()
        stA.close()
        if upto <= 1:
            return nc

        with contextlib.ExitStack() as st:
            cw = T(st, "cw", [128, 12, 5])
            Rt = [T(st, f"Rt{i}", [128, 516]) for i in range(3)]
            acc = [T(st, f"acc{i}", [128, 512]) for i in range(2)]
            y = [T(st, f"y{i}", [128, 512]) for i in range(2)]
            y2 = T(st, "y2", [128, 512]); rn = T(st, "rn", [128, 512]); yn = [T(st, f"yn{i}", [128, 512]) for i in range(2)]
            tok = [T(st, f"tok{i}", [128, 4, 128]) for i in range(2)]
            pN = [PS(st, f"pN{i}", [128, 512]) for i in range(2)]
            pK = [PS(st, f"pK{i}", [128, 512]) for i in range(2)]
            for j in range(5):
                s.dma(cw[:, :, j], conv_d[j, :].rearrange("(fc p) -> p fc", p=128), writes=["cw"], allow_slow_non_contiguous=True)
            it = 0
            segs = [(0, CTX), (CTX, TALL)]
            if "small" in dbg:
                segs = [(0, CTX), (CTX, CTX + 256)]
            for (g0, g1) in segs:
                for t0 in range(g0, g1, 512):
                    n = min(512, g1 - t0)
                    for fc in range(12):
                        R = Rt[it % 3]; rid = ("Rt", it % 3); A = acc[it % 2]; aid = ("acc", it % 2); Y = y[it % 2]; yid = ("y", it % 2)
                        lo = max(t0 - 2, g0); hi = min(t0 + n + 2, g1)
                        if lo > t0 - 2 or hi < t0 + n + 2:
                            s.op("pool", lambda e, R=R: e.memset(R[:], 0.0), writes=[rid])
                        s.dma(R[:, lo - (t0 - 2):hi - (t0 - 2)], RT_s[fc * 128:(fc + 1) * 128, lo:hi], reads=["RT_s"], writes=[rid], queue=("sp", "act")[it % 2])
                        s.op("dve", lambda e, R=R, A=A, fc=fc, n=n: e.tensor_scalar(out=A[:, 0:n], in0=R[:, 0:n], scalar1=cw[:, fc, 0:1], scalar2=None, op0=ALU.mult), reads=[rid, "cw"], writes=[aid])
                        for j in range(1, 5):
                            s.op("dve", lambda e, R=R, A=A, fc=fc, n=n, j=j: e.scalar_tensor_tensor(out=A[:, 0:n], in0=R[:, j:j + n], scalar=cw[:, fc, j:j + 1], in1=A[:, 0:n], op0=ALU.mult, op1=ALU.add), reads=[rid, "cw", aid], writes=[aid])
                        s.op("act", lambda e, A=A, Y=Y, n=n: e.activation(out=Y[:, 0:n], in_=A[:, 0:n], func=AF.Silu), reads=[aid], writes=[yid])
                        src, sid = Y, yid
                        if fc < 8:
                            YN = yn[it % 2]; nid = ("yn", it % 2); P = pN[it % 2]; pid = ("pN", it % 2)
                            s.op("act", lambda e, Y=Y, n=n: e.activation(out=y2[:, 0:n], in_=Y[:, 0:n], func=AF.Square), reads=[yid], writes=["y2"])
                            s.op("pe", lambda e, P=P, n=n: e.matmul(out=P[:, 0:n], lhsT=cm[:, C_ONES, :], rhs=y2[:, 0:n], start=True, stop=True), reads=["cm", "y2"], writes=[pid])
                            s.op("dve", lambda e, P=P, n=n: e.tensor_scalar(out=rn[:, 0:n], in0=P[:, 0:n], scalar1=1e-6, scalar2=None, op0=ALU.add), reads=[pid], writes=["rn"])
                            s.op("act", lambda e, n=n: e.sqrt(out=rn[:, 0:n], in_=rn[:, 0:n]), reads=["rn"], writes=["rn"])
                            s.op("dve", lambda e, n=n: e.reciprocal(out=rn[:, 0:n], in_=rn[:, 0:n]), reads=["rn"], writes=["rn"])
                            sc = float(128 ** -0.5) if fc < 4 else 1.0
                            s.op("dve", lambda e, Y=Y, YN=YN, n=n, sc=sc: e.scalar_tensor_tensor(out=YN[:, 0:n], in0=Y[:, 0:n], scalar=sc, in1=rn[:, 0:n], op0=ALU.mult, op1=ALU.mult), reads=[yid, "rn"], writes=[nid])
                            s.dma(QK_s[fc * 128:(fc + 1) * 128, t0:t0 + n], YN[:, 0:n], reads=[nid], writes=["QK_s"], queue="pool")
                            src, sid = YN, nid
                        if fc >= 4:
                            PK = pK[it % 2]; kid = ("pK", it % 2); TK = tok[it % 2]; tid = ("tok", it % 2)
                            nsb = n // 128
                            for sb in range(nsb):
                                s.op("pe", lambda e, PK=PK, src=src, sb=sb: e.transpose(out=PK[:, sb * 128:(sb + 1) * 128], in_=src[:, sb * 128:(sb + 1) * 128], identity=ident), reads=[sid, "cm"], writes=[kid])
                            s.op("act", lambda e, PK=PK, TK=TK, n=n: e.copy(out=TK[:].rearrange("p a b -> p (a b)")[:, 0:n], in_=PK[:, 0:n]), reads=[kid], writes=[tid])
                            s.dma(KV_s[t0:t0 + n, (fc - 4) * 128:(fc - 3) * 128].rearrange("(sb p) f -> p sb f", p=128), TK[:, 0:nsb, :], reads=[tid], writes=["KV_s"], queue="pool")
                        it += 1
            s.flush()
        if upto <= 2:
            return nc

        with contextlib.ExitStack() as st:
            Sst = [T(st, f"Sst{i}", [128, 4, 128]) for i in range(2)]
            qT4 = T(st, "qT4", [128, 4, 128]); kT4 = T(st, "kT4", [128, 4, 128]); ktok = T(st, "ktok", [128, 4, 128]); vtok = T(st, "vtok", [128, 4, 128])
            gb = T(st, "gb", [128, 16]); sm = T(st, "sm", [128, 16]); ex = T(st, "ex", [128, 16]); beg = T(st, "beg", [128, 4])
            G1 = T(st, "G1", [128, 4, 128]); dl = T(st, "dl", [128, 4, 128]); du = T(st, "du", [128, 4, 128])
            Bm = [T(st, f"Bm{i}", [128, 4, 128]) for i in range(2)]; Cm = [T(st, f"Cm{i}", [128, 4, 128]) for i in range(2)]; Pm = [T(st, f"Pm{i}", [128, 4, 128]) for i in range(2)]
            aT = T(st, "aT", [128, 4, 128]); kbg = T(st, "kbg", [128, 4, 128]); vb = T(st, "vb", [128, 4, 128]); ktl = T(st, "ktl", [128, 4, 128])
            WT = T(st, "WT", [128, 4, 128]); U = T(st, "U", [128, 4, 128]); vn = T(st, "vn", [128, 4, 128]); o1 = T(st, "o1", [128, 4, 128]); ot = T(st, "ot", [128, 4, 128])
            pb = [PS(st, f"pb{i}", [128, 4, 128]) for i in range(8)]
            pA, pB_, pC, pD, pE, pF, pG, pH = pb
            pid = lambda k: ("pb", k)
            H4 = [128, 4, 128]
            bc_h = lambda ap2: ap2.unsqueeze(1).to_broadcast(H4)
            bc_l = lambda ap2: ap2.unsqueeze(2).to_broadcast(H4)
            ntl = (2 if "small" in dbg else NT)
            for dr in range(2):
                M1 = cm[:, C_M1F if dr == 0 else C_M1B, :]; NM1 = cm[:, C_NM1F if dr == 0 else C_NM1B, :]
                NB = cm[:, C_NLOW if dr == 0 else C_NUP, :]; NTm = cm[:, C_NUP if dr == 0 else C_NLOW, :]
                STR = cm[:, C_SLOW if dr == 0 else C_SUP, :]
                SS = Sst[dr]; ssid = ("Sst", dr)
                s.op("pool", lambda e, SS=SS: e.memset(SS[:], 0.0), writes=[ssid])
                order = [("c", i) for i in range(CTX // 128)] + [("l", i) for i in range(ntl)]
                if dr == 1:
                    order = [("c", i) for i in reversed(range(CTX // 128))] + [("l", i) for i in reversed(range(ntl))]
                for (kind, i) in order:
                    lat = kind == "l"
                    tg = i if not lat else CTX // 128 + i
                    tsl = slice(tg * 128, (tg + 1) * 128)
                    s.dma(qT4[:], QK_s[0:512, tsl].rearrange("(h p) t -> p h t", p=128), reads=["QK_s"], writes=["qT4"])
                    s.dma(kT4[:], QK_s[512:1024, tsl].rearrange("(h p) t -> p h t", p=128), reads=["QK_s"], writes=["kT4"], queue="act")
                    s.dma(ktok[:].rearrange("p h d -> p (h d)"), KV_s[tsl, 0:512], reads=["KV_s"], writes=["ktok"])
                    s.dma(vtok[:].rearrange("p h d -> p (h d)"), KV_s[tsl, 512:1024], reads=["KV_s"], writes=["vtok"], queue="act")
                    s.dma(gb[:], GB_s[tsl, :], reads=["GB_s"], writes=["gb"])
                    g = gb[:, dr * 4:dr * 4 + 4]; beta = gb[:, 8 + dr * 4:12 + dr * 4]
                    pAf = pA[:].rearrange("p a b -> p (a b)")
                    for k, L in enumerate((M1, cm[:, C_SAME, :], cm[:, C_SEL0, :], cm[:, C_SEL1, :])):
                        s.op("pe", lambda e, k=k, L=L, g=g: e.matmul(out=pAf[:, 4 * k:4 * k + 4], lhsT=L, rhs=g, start=True, stop=True), reads=["cm", "gb"], writes=[pid(0)])
                    s.op("dve", lambda e: e.tensor_copy(out=sm[:], in_=pAf[:, 0:16]), reads=[pid(0)], writes=["sm"])
                    s.op("dve", lambda e: e.tensor_tensor(out=sm[:, 4:8], in0=sm[:, 4:8], in1=sm[:, 0:4], op=ALU.subtract), reads=["sm"], writes=["sm"])
                    s.op("act", lambda e: e.activation(out=ex[:], in_=sm[:], func=AF.Exp), reads=["sm"], writes=["ex"])
                    s.op("dve", lambda e, beta=beta: e.tensor_tensor(out=beg[:], in0=ex[:, 0:4], in1=beta, op=ALU.mult), reads=["ex", "gb"], writes=["beg"])
                    s.op("dve", lambda e, g=g: e.tensor_tensor(out=G1[:], in0=bc_h(cm[:, C_SAME, :]), in1=bc_l(g), op=ALU.mult), reads=["cm", "gb"], writes=["G1"])
                    for hh in range(4):
                        s.op("pe", lambda e, hh=hh, M1=M1: e.matmul(out=pB_[:, hh, :], lhsT=M1, rhs=G1[:, hh, :], start=True, stop=False), reads=["cm", "G1"], writes=[pid(1)])
                        s.op("pe", lambda e, hh=hh, NM1=NM1: e.matmul(out=pB_[:, hh, :], lhsT=G1[:, hh, :], rhs=NM1, start=False, stop=True), reads=["cm", "G1"], writes=[pid(1)])
                    s.op("dve", lambda e, NB=NB: e.tensor_tensor(out=dl[:], in0=pB_[:], in1=bc_h(NB), op=ALU.add), reads=[pid(1), "cm"], writes=["dl"])
                    s.op("dve", lambda e, NTm=NTm: e.scalar_tensor_tensor(out=du[:], in0=pB_[:], scalar=-1.0, in1=bc_h(NTm), op0=ALU.mult, op1=ALU.add), reads=[pid(1), "cm"], writes=["du"])
                    s.op("act", lambda e: e.activation(out=dl[:], in_=dl[:], func=AF.Exp), reads=["dl"], writes=["dl"])
                    s.op("act", lambda e: e.activation(out=du[:], in_=du[:], func=AF.Exp), reads=["du"], writes=["du"])
                    for hh in range(4):
                        s.op("pe", lambda e, hh=hh: e.matmul(out=pC[:, hh, :], lhsT=kT4[:, hh, :], rhs=kT4[:, hh, :], start=True, stop=True), reads=["kT4"], writes=[pid(2)])
                    for hh in range(4):
                        s.op("pe", lambda e, hh=hh: e.matmul(out=pD[:, hh, :], lhsT=kT4[:, hh, :], rhs=qT4[:, hh, :], start=True, stop=True), reads=["kT4", "qT4"], writes=[pid(3)])
                    B0 = Bm[0]; C0 = Cm[0]; P0 = Pm[0]
                    s.op("dve", lambda e: e.tensor_tensor(out=B0[:], in0=pC[:], in1=dl[:], op=ALU.mult), reads=[pid(2), "dl"], writes=[("Bm", 0)])
                    s.op("pool", lambda e, STR=STR: e.tensor_tensor(out=B0[:], in0=B0[:], in1=bc_h(STR), op=ALU.mult), reads=[("Bm", 0), "cm"], writes=[("Bm", 0)])
                    s.op("pool", lambda e, beta=beta: e.tensor_tensor(out=B0[:], in0=B0[:], in1=bc_l(beta), op=ALU.mult), reads=[("Bm", 0), "gb"], writes=[("Bm", 0)])
                    s.op("dve", lambda e: e.tensor_tensor(out=aT[:], in0=pD[:], in1=du[:], op=ALU.mult), reads=[pid(3), "du"], writes=["aT"])
                    for hh in range(4):
                        s.op("pe", lambda e, hh=hh: e.transpose(out=pE[:, hh, :], in_=B0[:, hh, :], identity=ident), reads=[("Bm", 0), "cm"], writes=[pid(4)])
                    s.op("act", lambda e: e.copy(out=C0[:], in_=pE[:]), reads=[pid(4)], writes=[("Cm", 0)])
                    s.op("dve", lambda e: e.tensor_tensor(out=P0[:], in0=C0[:], in1=bc_h(ident), op=ALU.add), reads=[("Cm", 0), "cm"], writes=[("Pm", 0)])
                    cur = 0
                    for lv in range(1, 6):
                        nx = 1 - cur
                        Bc, Cc, Pc = Bm[cur], Cm[cur], Pm[cur]; Bn, Cn, Pn = Bm[nx], Cm[nx], Pm[nx]
                        for hh in range(4):
                            s.op("pe", lambda e, hh=hh, Bc=Bc, Cc=Cc: e.matmul(out=pF[:, hh, :], lhsT=Cc[:, hh, :], rhs=Bc[:, hh, :], start=True, stop=True), reads=[("Bm", cur), ("Cm", cur)], writes=[pid(5)])
                        s.op("act", lambda e, Bn=Bn: e.copy(out=Bn[:], in_=pF[:]), reads=[pid(5)], writes=[("Bm", nx)])
                        if lv < 5:
                            for hh in range(4):
                                s.op("pe", lambda e, hh=hh, Bc=Bc, Cc=Cc: e.matmul(out=pG[:, hh, :], lhsT=Bc[:, hh, :], rhs=Cc[:, hh, :], start=True, stop=True), reads=[("Bm", cur), ("Cm", cur)], writes=[pid(6)])
                            s.op("dve", lambda e, Cn=Cn: e.tensor_copy(out=Cn[:], in_=pG[:]), reads=[pid(6)], writes=[("Cm", nx)])
                        for hh in range(4):
                            s.op("pe", lambda e, hh=hh, Pc=Pc: e.matmul(out=pH[:, hh, :], lhsT=ident, rhs=Pc[:, hh, :], start=True, stop=False), reads=[("Pm", cur), "cm"], writes=[pid(7)])
                            s.op("pe", lambda e, hh=hh, Pc=Pc, Bn=Bn: e.matmul(out=pH[:, hh, :], lhsT=Bn[:, hh, :], rhs=Pc[:, hh, :], start=False, stop=True), reads=[("Pm", cur), ("Bm", nx)], writes=[pid(7)])
                        s.op("dve", lambda e, Pn=Pn: e.tensor_copy(out=Pn[:], in_=pH[:]), reads=[pid(7)], writes=[("Pm", nx)])
                        cur = nx
                    TT = Pm[cur]; ttid = ("Pm", cur)
                    s.op("pool", lambda e: e.tensor_tensor(out=kbg[:], in0=ktok[:], in1=bc_l(beg[:]), op=ALU.mult), reads=["ktok", "beg"], writes=["kbg"])
                    s.op("pool", lambda e, beta=beta: e.tensor_tensor(out=vb[:], in0=vtok[:], in1=bc_l(beta), op=ALU.mult), reads=["vtok", "gb"], writes=["vb"])
                    s.op("pool", lambda e: e.tensor_tensor(out=ktl[:], in0=ktok[:], in1=bc_l(ex[:, 4:8]), op=ALU.mult), reads=["ktok", "ex"], writes=["ktl"])
                    for hh in range(4):
                        s.op("pe", lambda e, hh=hh, TT=TT: e.matmul(out=pE[:, hh, :], lhsT=kbg[:, hh, :], rhs=TT[:, hh, :], start=True, stop=True), reads=["kbg", ttid], writes=[pid(4)])
                    s.op("act", lambda e: e.copy(out=WT[:], in_=pE[:]), reads=[pid(4)], writes=["WT"])
                    for hh in range(4):
                        s.op("pe", lambda e, hh=hh, TT=TT: e.matmul(out=pF[:, hh, :], lhsT=TT[:, hh, :], rhs=vb[:, hh, :], start=True, stop=True), reads=["vb", ttid], writes=[pid(5)])
                    s.op("dve", lambda e: e.tensor_copy(out=U[:], in_=pF[:]), reads=[pid(5)], writes=["U"])
                    for c in ((0, 1) if dr == 0 else (1, 0)):
                        pr = slice(64 * c, 64 * c + 64)
                        for hh in range(4):
                            s.op("pe", lambda e, hh=hh, SS=SS: e.matmul(out=pG[:, hh, :], lhsT=WT[:, hh, :], rhs=SS[:, hh, :], start=True, stop=True), reads=["WT", ssid], writes=[pid(6)])
                        s.op("dve", lambda e, pr=pr: e.tensor_tensor(out=vn[pr], in0=U[pr], in1=pG[pr], op=ALU.subtract), reads=["U", pid(6)], writes=["vn"])
                        for hh in range(4):
                            s.op("pe", lambda e, hh=hh, SS=SS: e.matmul(out=pH[:, hh, :], lhsT=qT4[:, hh, :], rhs=SS[:, hh, :], start=True, stop=True), reads=["qT4", ssid], writes=[pid(7)])
                        for hh in range(4):
                            s.op("pe", lambda e, hh=hh, pr=pr: e.matmul(out=pC[:, hh, :], lhsT=aT[pr, hh, :], rhs=vn[pr, hh, :], start=True, stop=True), reads=["aT", "vn"], writes=[pid(2)])
                        for hh in range(4):
                            s.op("pe", lambda e, hh=hh, pr=pr: e.matmul(out=pD[:, hh, :], lhsT=ktl[pr, hh, :], rhs=vn[pr, hh, :], start=True, stop=True), reads=["ktl", "vn"], writes=[pid(3)])
                        if lat:
                            s.op("dve", lambda e, pr=pr: e.tensor_tensor(out=o1[pr], in0=pH[pr], in1=bc_l(ex[:, 0:4])[pr], op=ALU.mult), reads=[pid(7), "ex"], writes=["o1"])
                            s.op("dve", lambda e, pr=pr: e.tensor_tensor(out=ot[pr], in0=o1[pr], in1=pC[pr], op=ALU.add), reads=["o1", pid(2)], writes=["ot"])
                        s.op("pool", lambda e, c=c, SS=SS: e.tensor_tensor(out=SS[:], in0=SS[:], in1=bc_l(ex[:, 8 + 4 * c:12 + 4 * c]), op=ALU.mult), reads=[ssid, "ex", pid(6), pid(7)], writes=[ssid])
                        s.op("dve", lambda e, SS=SS: e.tensor_tensor(out=SS[:], in0=SS[:], in1=pD[:], op=ALU.add), reads=[ssid, pid(3)], writes=[ssid])
                    if lat:
                        s.dma(OD_s[dr, i * 128:(i + 1) * 128, :], ot[:].rearrange("p h d -> p (h d)"), reads=["ot"], writes=["OD_s"], queue="pool")
            s.flush()
        if upto <= 3:
            return nc

        with contextlib.ExitStack() as st:
            kt = [T(st, f"kt{i}", [64, 2, 384]) for i in range(2)]
            vt = [T(st, f"vt{i}", [128, 3, 130]) for i in range(2)]
            ktc = T(st, "ktc", [64, 2, 256]); vtc = T(st, "vtc", [128, 2, 130])
            qt = [T(st, f"qt{i}", [64, 8, 128]) for i in range(2)]
            E = [T(st, f"E{i}", [128, 5, 512]) for i in range(2)]
            esink = T(st, "esink", [128, 8]); den = T(st, "den", [128, 8]); oa = [T(st, f"oa{i}", [128, 512]) for i in range(2)]
            pS = [PS(st, f"pS{i}", [128, 512]) for i in range(3)]
            pO = [PS(st, f"pO{i}", [128, 4, 65]) for i in range(2)]
            s.dma(ktc[:], KT_s[:, :, 0:CTX], reads=["KT_s"], writes=["ktc"])
            s.dma(vtc[:], V_s[0:CTX].rearrange("(b p) g d -> p b (g d)", p=128), reads=["V_s"], writes=["vtc"])
            s.dma(esink[:], sink_d.partition_broadcast(128), writes=["esink"])
            s.op("act", lambda e: e.activation(out=esink[:], in_=esink[:], func=AF.Exp), reads=["esink"], writes=["esink"])
            ntl = (2 if "small" in dbg else NT)
            nS = 0
            for i in range(ntl):
                lo = max(i - 1, 0); hi = min(i + 1, ntl - 1); nb = hi - lo + 1
                KT_ = kt[i % 2]; VT_ = vt[i % 2]; QT_ = qt[i % 2]; OA = oa[i % 2]
                s.dma(KT_[:, :, 0:nb * 128], KT_s[:, :, CTX + lo * 128:CTX + (hi + 1) * 128], reads=["KT_s"], writes=[("kt", i % 2)])
                s.dma(VT_[:, 0:nb, :], V_s[CTX + lo * 128:CTX + (hi + 1) * 128].rearrange("(b p) g d -> p b (g d)", p=128), reads=["V_s"], writes=[("vt", i % 2)], queue="act")
                s.dma(QT_[:], QT_s[:, :, i * 128:(i + 1) * 128], reads=["QT_s"], writes=[("qt", i % 2)])
                for g in range(2):
                    Eg = E[g]; eid = ("E", g)
                    kb = [("l", j - lo, (C_WPREV if j < i else (C_WNEXT if j > i else None))) for j in range(lo, hi + 1)] + [("c", 0, None), ("c", 1, None)]
                    for bi, (kk, bl, msk) in enumerate(kb):
                        P = pS[nS % 3]; psid = ("pS", nS % 3); nS += 1
                        lhs = KT_[:, g, bl * 128:(bl + 1) * 128] if kk == "l" else ktc[:, g, bl * 128:(bl + 1) * 128]
                        s.op("pe", lambda e, P=P, lhs=lhs, QT_=QT_, g=g: e.matmul(out=P[:].rearrange("p (h q) -> p h q", h=4), lhsT=lhs, rhs=QT_[:, 4 * g:4 * g + 4, :], start=True, stop=True),
                             reads=[("kt", i % 2), "ktc", ("qt", i % 2)], writes=[psid])
                        s.op("act", lambda e, P=P, Eg=Eg, bi=bi: e.activation(out=Eg[:, bi, :], in_=P[:], func=AF.Exp), reads=[psid], writes=[eid])
                        if msk is not None:
                            s.op("dve", lambda e, Eg=Eg, bi=bi, msk=msk: e.tensor_tensor(out=Eg[:, bi, :].rearrange("p (h q) -> p h q", h=4), in0=Eg[:, bi, :].rearrange("p (h q) -> p h q", h=4),
                                                                              in1=cm[:, msk, :].unsqueeze(1).to_broadcast([128, 4, 128]), op=ALU.mult), reads=[eid, "cm"], writes=[eid])
                    for hh in range(4):
                        for bi, (kk, bl, msk) in enumerate(kb):
                            rhs = VT_[:, bl, g * 65:(g + 1) * 65] if kk == "l" else vtc[:, bl, g * 65:(g + 1) * 65]
                            s.op("pe", lambda e, Eg=Eg, bi=bi, hh=hh, rhs=rhs, g=g, last=(bi == len(kb) - 1): e.matmul(out=pO[g][:, hh, :], lhsT=Eg[:, bi, hh * 128:(hh + 1) * 128], rhs=rhs, start=(bi == 0), stop=last),
                                 reads=[eid, ("vt", i % 2), "vtc"], writes=[("pO", g)])
                    s.op("dve", lambda e, g=g: e.tensor_tensor(out=den[:, 4 * g:4 * g + 4], in0=pO[g][:, :, 64], in1=esink[:, 4 * g:4 * g + 4], op=ALU.add), reads=[("pO", g), "esink"], writes=["den"])
                    s.op("dve", lambda e, g=g: e.reciprocal(out=den[:, 4 * g:4 * g + 4], in_=den[:, 4 * g:4 * g + 4]), reads=["den"], writes=["den"])
                    s.op("dve", lambda e, g=g, OA=OA: e.tensor_tensor(out=OA[:, g * 256:(g + 1) * 256].rearrange("p (h d) -> p h d", h=4), in0=pO[g][:, :, 0:64],
                                                              in1=den[:, 4 * g:4 * g + 4].unsqueeze(2).to_broadcast([128, 4, 64]), op=ALU.mult), reads=[("pO", g), "den"], writes=[("oa", i % 2)])
                s.dma(OA_s[i * 128:(i + 1) * 128, :], OA[:], reads=[("oa", i % 2)], writes=["OA_s"], queue="pool")
            s.flush()
        if upto <= 5:
            return nc

        UTr_s = nc.dram_tensor("UTr_s", [D, 16384], F32R, kind="Internal").ap()
        Vr_s = nc.dram_tensor("Vr_s", [16384, D], F32R, kind="Internal").ap()
        X1_s = scr("X1_s", [S, D])
        H2T_s = scr("H2T_s", [D, S])
        H2R_s = nc.dram_tensor("H2R_s", [D, S], F32R, kind="Internal").ap()
        SC_s = scr("SC_s", [S, 2048])
        KAP_s = scr("KAP_s", [S, 8])
        TR = lambda st, name, shape: st.enter_context(nc.sbuf_tensor(_nm(name), list(shape), F32R))
        B = lambda k: ("bk", k)
        ntl6 = (2 if "small" in dbg else NT)

        with contextlib.ExitStack() as st:
            cvb = [TR(st, "cvb", [128, 4096]) for _ in range(2)]
            wbr = T(st, "wbr", [128, 8, D]); wo = T(st, "wo", [128, 8, D])
            xa = [T(st, f"xa{b}", [128, D]) for b in range(2)]; yb = [T(st, f"yb{b}", [128, D]) for b in range(2)]
            tcA = [T(st, f"tcA{b}", [128, 8, 128]) for b in range(2)]; h2r = [TR(st, f"h2r{b}", [128, 8, 128]) for b in range(2)]
            gt = [T(st, f"gt{b}", [128, 2048]) for b in range(2)]
            od = [T(st, f"od{b}", [128, 2, 512]) for b in range(2)]; zt = [T(st, f"zt{b}", [128, 512]) for b in range(2)]
            oat = [T(st, f"oat{b}", [128, 512]) for b in range(2)]; o2 = [T(st, f"o2{b}", [128, 512]) for b in range(2)]
            qsb = [T(st, f"qsb{b}", [128, D]) for b in range(2)]
            ssq = [T(st, f"ssq{b}", [128, 4]) for b in range(2)]; ss = [T(st, f"ss{b}", [128, 1]) for b in range(2)]; rstd = [T(st, f"rstd{b}", [128, 1]) for b in range(2)]
            bk = [PS(st, f"bkA{i}", [128, 512]) for i in range(8)]
            cjobs = []
            for r0 in range(0, D, 128):
                for c0 in range(0, 16384, 4096):
                    cjobs.append((pu_d[r0:r0 + 128, c0:c0 + 4096], UTr_s[r0:r0 + 128, c0:c0 + 4096], "UTr_s"))
            for r0 in range(0, 16384, 512):
                cjobs.append((pv_d[r0:r0 + 512, :].rearrange("(p a) n -> p (a n)", p=128), Vr_s[r0:r0 + 512, :].rearrange("(p a) n -> p (a n)", p=128), "Vr_s"))
            cstate = [0]

            def conv_some(n):
                for _ in range(n):
                    if not cjobs:
                        return
                    src, dst, did = cjobs.pop(0)
                    k = cstate[0] % 2; cstate[0] += 1
                    s.dma(cvb[k][:], src, writes=[("cvb", k)], queue="pool")
                    s.dma(dst, cvb[k][:], reads=[("cvb", k)], writes=[did], queue=("sp", "act")[k])
            s.dma(wbr[:, 0:4, :], wba_d.rearrange("(kc p) n -> p kc n", p=128), writes=["wbr"])
            s.dma(wbr[:, 4:8, :], wbd_d.rearrange("(kc p) n -> p kc n", p=128), writes=["wbr"], queue="act")
            s.dma(wo[:], wout_d.rearrange("(kc p) n -> p kc n", p=128), writes=["wo"])

            def tr8(b, src, sid, nkc, dst_off, dst, did, extra=None):
                pT = [bk[4 * b], bk[4 * b + 1]]
                for kc in range(nkc):
                    q_ = (dst_off + kc) // 4
                    s.op("pe", lambda e, kc=kc, q_=q_: e.transpose(out=pT[q_][:, ((dst_off + kc) % 4) * 128:((dst_off + kc) % 4 + 1) * 128], in_=src[:, kc * 128:(kc + 1) * 128], identity=ident), reads=[sid, "cm"], writes=[B(4 * b + q_)])
                for q_ in sorted(set((dst_off + kc) // 4 for kc in range(nkc))):
                    if q_ == 0:
                        s.op("act", lambda e, q_=q_: e.copy(out=dst[:, q_ * 4:(q_ + 1) * 4, :].rearrange("p a b -> p (a b)"), in_=pT[q_][:]), reads=[B(4 * b + q_)], writes=[(did, q_)])
                    else:
                        s.op("dve", lambda e, q_=q_: e.tensor_copy(out=dst[:, q_ * 4:(q_ + 1) * 4, :].rearrange("p a b -> p (a b)"), in_=pT[q_][:]), reads=[B(4 * b + q_)], writes=[(did, q_)])
                    if extra is not None:
                        d2, d2id = extra
                        if q_ == 0:
                            s.op("dve", lambda e, q_=q_: e.tensor_copy(out=d2[:, q_ * 4:(q_ + 1) * 4, :].rearrange("p a b -> p (a b)"), in_=pT[q_][:]), reads=[B(4 * b + q_)], writes=[(d2id, q_)])
                        else:
                            s.op("act", lambda e, q_=q_: e.copy(out=d2[:, q_ * 4:(q_ + 1) * 4, :].rearrange("p a b -> p (a b)"), in_=pT[q_][:]), reads=[B(4 * b + q_)], writes=[(d2id, q_)])

            def rms6(b, src, sid):
                s.op("act", lambda e: e.activation(out=qsb[b][:], in_=src[:], func=AF.Square, accum_out=ss[b][:]), reads=[sid], writes=[("qsb", b), ("ss", b)])
                s.op("dve", lambda e: e.tensor_scalar(out=rstd[b][:], in0=ss[b][:], scalar1=1.0 / D, scalar2=1e-6, op0=ALU.mult, op1=ALU.add), reads=[("ss", b)], writes=[("rstd", b)])
                s.op("act", lambda e: e.sqrt(out=rstd[b][:], in_=rstd[b][:]), reads=[("rstd", b)], writes=[("rstd", b)])
                s.op("dve", lambda e: e.reciprocal(out=rstd[b][:], in_=rstd[b][:]), reads=[("rstd", b)], writes=[("rstd", b)])

            for i in range(ntl6):
                conv_some(4)
                b = i % 2
                tsl = slice(i * 128, (i + 1) * 128)
                XA = xa[b]; YB = yb[b]; GT = gt[b]; OD = od[b]; ZT = zt[b]; OAT = oat[b]; O2 = o2[b]; QSB = qsb[b]; SSQ = ssq[b]; TC = tcA[b]
                pY = [bk[4 * b + 2], bk[4 * b + 3]]
                s.dma(XA[:], x_d[tsl, :], writes=[("xa", b)])
                s.dma(OD[:, 0, :], OD_s[0, tsl, :], reads=["OD_s"], writes=[("od", b)], queue="act")
                s.dma(OD[:, 1, :], OD_s[1, tsl, :], reads=["OD_s"], writes=[("od", b)], queue="act")
                s.dma(ZT[:], Z_s[tsl, :], reads=["Z_s"], writes=[("zt", b)])
                s.dma(OAT[:], OA_s[tsl, :], reads=["OA_s"], writes=[("oat", b)], queue="act")
                s.dma(GT[:], GT_s[tsl, :], reads=["GT_s"], writes=[("gt", b)])
                s.op("dve", lambda e, OD=OD: e.tensor_tensor(out=OD[:, 0, :], in0=OD[:, 0, :], in1=OD[:, 1, :], op=ALU.add), reads=[("od", b)], writes=[("od", b)])
                s.op("dve", lambda e, OD=OD, O2=O2: e.tensor_tensor(out=O2[:], in0=OD[:, 0, :], in1=OD[:, 0, :], op=ALU.mult), reads=[("od", b)], writes=[("o2", b)])
                s.op("dve", lambda e, O2=O2, SSQ=SSQ: e.tensor_reduce(out=SSQ[:], in_=O2[:].rearrange("p (h d) -> p h d", h=4), axis=AX.X, op=ALU.add), reads=[("o2", b)], writes=[("ssq", b)])
                s.op("dve", lambda e, SSQ=SSQ: e.tensor_scalar(out=SSQ[:], in0=SSQ[:], scalar1=1.0 / 128, scalar2=1e-6, op0=ALU.mult, op1=ALU.add), reads=[("ssq", b)], writes=[("ssq", b)])
                s.op("act", lambda e, SSQ=SSQ: e.sqrt(out=SSQ[:], in_=SSQ[:]), reads=[("ssq", b)], writes=[("ssq", b)])
                s.op("dve", lambda e, SSQ=SSQ: e.reciprocal(out=SSQ[:], in_=SSQ[:]), reads=[("ssq", b)], writes=[("ssq", b)])
                s.op("dve", lambda e, O2=O2, OD=OD, SSQ=SSQ: e.tensor_tensor(out=O2[:].rearrange("p (h d) -> p h d", h=4), in0=OD[:, 0, :].rearrange("p (h d) -> p h d", h=4), in1=SSQ[:].unsqueeze(2).to_broadcast([128, 4, 128]), op=ALU.mult), reads=[("od", b), ("ssq", b)], writes=[("o2", b)])
                s.op("dve", lambda e, O2=O2, ZT=ZT: e.tensor_tensor(out=O2[:], in0=O2[:], in1=ZT[:], op=ALU.mult), reads=[("o2", b), ("zt", b)], writes=[("o2", b)])
                tr8(b, OAT, ("oat", b), 4, 0, TC, ("tcA", b))
                tr8(b, O2, ("o2", b), 4, 4, TC, ("tcA", b))
                for half in range(2):
                    for kc in range(4):
                        s.op("pe", lambda e, half=half, kc=kc, pY=pY, TC=TC: e.matmul(out=pY[half][:], lhsT=TC[:, kc, :], rhs=wbr[:, kc, half * 512:(half + 1) * 512], start=(kc == 0), stop=(kc == 3)), reads=[(("tcA", b), 0), "wbr"], writes=[B(4 * b + 2 + half)])
                    s.op("dve", lambda e, half=half, pY=pY, YB=YB, GT=GT: e.tensor_tensor(out=YB[:, half * 512:(half + 1) * 512], in0=pY[half][:], in1=GT[:, half * 512:(half + 1) * 512], op=ALU.mult), reads=[B(4 * b + 2 + half), ("gt", b)], writes=[("yb", b)])
                for half in range(2):
                    for kc in range(4):
                        s.op("pe", lambda e, half=half, kc=kc, pY=pY, TC=TC: e.matmul(out=pY[half][:], lhsT=TC[:, 4 + kc, :], rhs=wbr[:, 4 + kc, half * 512:(half + 1) * 512], start=(kc == 0), stop=(kc == 3)), reads=[(("tcA", b), 1), "wbr"], writes=[B(4 * b + 2 + half)])
                    s.op("dve", lambda e, half=half, pY=pY, QSB=QSB, GT=GT: e.tensor_tensor(out=QSB[:, half * 512:(half + 1) * 512], in0=pY[half][:], in1=GT[:, 1024 + half * 512:1024 + (half + 1) * 512], op=ALU.mult), reads=[B(4 * b + 2 + half), ("gt", b)], writes=[("qsb", b)])
                s.op("dve", lambda e, YB=YB, QSB=QSB: e.tensor_tensor(out=YB[:], in0=YB[:], in1=QSB[:], op=ALU.add), reads=[("yb", b), ("qsb", b)], writes=[("yb", b)])
                tr8(b, YB, ("yb", b), 8, 0, TC, ("tcA", b))
                for half in range(2):
                    for kc in range(8):
                        s.op("pe", lambda e, half=half, kc=kc, pY=pY, TC=TC: e.matmul(out=pY[half][:], lhsT=TC[:, kc, :], rhs=wo[:, kc, half * 512:(half + 1) * 512], start=(kc == 0), stop=(kc == 7)), reads=[(("tcA", b), 0), (("tcA", b), 1), "wo"], writes=[B(4 * b + 2 + half)])
                    s.op("dve", lambda e, half=half, pY=pY, YB=YB: e.tensor_tensor(out=YB[:, half * 512:(half + 1) * 512], in0=pY[half][:], in1=bvv(BV_GT1)[:, half * 512:(half + 1) * 512], op=ALU.mult), reads=[B(4 * b + 2 + half), "bv"], writes=[("yb", b)])
                s.op("dve", lambda e, XA=XA, YB=YB: e.tensor_tensor(out=XA[:], in0=XA[:], in1=YB[:], op=ALU.add), reads=[("xa", b), ("yb", b)], writes=[("xa", b)])
                s.dma(X1_s[tsl, :], XA[:], reads=[("xa", b)], writes=["X1_s"], queue="act")
                rms6(b, XA, ("xa", b))
                s.op("dve", lambda e, XA=XA, YB=YB, RS=rstd[b]: e.scalar_tensor_tensor(out=YB[:], in0=XA[:], scalar=RS[:, 0:1], in1=bvv(BV_G2), op0=ALU.mult, op1=ALU.mult), reads=[("xa", b), ("rstd", b), "bv"], writes=[("yb", b)])
                s.op("dve", lambda e, YB=YB: e.tensor_tensor(out=YB[:], in0=YB[:], in1=bvv(BV_SH2), op=ALU.add), reads=[("yb", b), "bv"], writes=[("yb", b)])
                tr8(b, YB, ("yb", b), 8, 0, TC, ("tcA", b), extra=(h2r[b], ("h2r", b)))
                s.dma(H2T_s[:, tsl].rearrange("(kc p) t -> p kc t", p=128), TC[:], reads=[(("tcA", b), 0), (("tcA", b), 1)], writes=["H2T_s"])
                s.dma(H2R_s[:, tsl].rearrange("(kc p) t -> p kc t", p=128), h2r[b][:], reads=[(("h2r", b), 0), (("h2r", b), 1)], writes=["H2R_s"], queue="act")
            conv_some(10 ** 6)
            s.flush()
        if upto <= 6:
            return nc

        with contextlib.ExitStack() as st:
            wq = T(st, "wq", [128, 8, D]); keys2 = T(st, "keys2", [128, 8, 128])
            tcB = [T(st, f"tcB{b}", [128, 8, 128]) for b in range(2)]
            qsb = [T(st, f"qsbB{b}", [128, D]) for b in range(2)]; qTs = [T(st, f"qTs{b}", [128, 8, 128]) for b in range(2)]
            sc = [T(st, f"scB{b}", [128, 16, 128]) for b in range(2)]
            t16 = [T(st, f"t16{b}", [128, 8, 2, 16]) for b in range(2)]; c16 = [T(st, f"c16{b}", [128, 8, 16]) for b in range(2)]
            wk4 = T(st, "wk4", [128, 4, 256]); cand4 = T(st, "cand4", [128, 4, 256])
            thr = [T(st, f"thr{b}", [128, 8]) for b in range(2)]; negm = [T(st, f"negm{b}", [128, 8]) for b in range(2)]; Zs = [T(st, f"Zs{b}", [128, 8]) for b in range(2)]
            kap = [T(st, f"kap{b}", [128, 8]) for b in range(2)]; m1 = [T(st, f"m1{b}", [128, 8]) for b in range(2)]; th2 = [T(st, f"th2{b}", [128, 8]) for b in range(2)]
            e16 = [T(st, f"e16{b}", [128, 8, 16]) for b in range(2)]
            bk = [PS(st, f"bkB{i}", [128, 512]) for i in range(8)]
            s.dma(wq[:], pwq_d.rearrange("(kc p) n -> p kc n", p=128), writes=["wq"])
            s.dma(keys2[:], pkeys_d, writes=["keys2"])
            _cb = [int(x[4:]) for x in dbg if x.startswith("cutB")]
            cutB = _cb[0] if _cb else 99
            for i in range(ntl6):
                b = i % 2
                tsl = slice(i * 128, (i + 1) * 128)
                TC = tcB[b]; QSB = qsb[b]; QT = qTs[b]; SC = sc[b]; T16 = t16[b]; C16 = c16[b]
                pT = [bk[4 * b], bk[4 * b + 1]]; pY = [bk[4 * b + 2], bk[4 * b + 3]]
                s.dma(TC[:], H2T_s[:, tsl].rearrange("(kc p) t -> p kc t", p=128), reads=["H2T_s"], writes=[("tcB", b)], queue=("sp", "act")[b])
                for half in range(2):
                    for kc in range(8):
                        s.op("pe", lambda e, half=half, kc=kc, pY=pY, TC=TC: e.matmul(out=pY[half][:], lhsT=TC[:, kc, :], rhs=wq[:, kc, half * 512:(half + 1) * 512], start=(kc == 0), stop=(kc == 7)), reads=[("tcB", b), "wq"], writes=[B(4 * b + 2 + half)])
                    if half == 0:
                        s.op("act", lambda e, pY=pY, QSB=QSB: e.copy(out=QSB[:, 0:512], in_=pY[0][:]), reads=[B(4 * b + 2)], writes=[("qsbB", b)])
                    else:
                        s.op("dve", lambda e, pY=pY, QSB=QSB: e.tensor_copy(out=QSB[:, 512:1024], in_=pY[1][:]), reads=[B(4 * b + 3)], writes=[("qsbB", b)])
                if cutB < 2:
                    continue
                for hh in range(8):
                    s.op("pe", lambda e, hh=hh, pT=pT, QSB=QSB: e.transpose(out=pT[hh // 4][:, (hh % 4) * 128:(hh % 4 + 1) * 128], in_=QSB[:, hh * 128:(hh + 1) * 128], identity=ident), reads=[("qsbB", b), "cm"], writes=[B(4 * b + hh // 4)])
                s.op("act", lambda e, pT=pT, QT=QT: e.copy(out=QT[:, 0:4, :].rearrange("p a b -> p (a b)"), in_=pT[0][:]), reads=[B(4 * b)], writes=[("qTs", b)])
                s.op("dve", lambda e, pT=pT, QT=QT: e.tensor_copy(out=QT[:, 4:8, :].rearrange("p a b -> p (a b)"), in_=pT[1][:]), reads=[B(4 * b + 1)], writes=[("qTs", b)])
                if cutB < 3:
                    continue
                banks = [pY[0], pY[1], pT[0], pT[1]]; bids = [B(4 * b + 2), B(4 * b + 3), B(4 * b), B(4 * b + 1)]
                for p in range(2):
                    for hh in range(8):
                        bi_ = 2 * p + hh // 4
                        s.op("pe", lambda e, hh=hh, p=p, bi_=bi_, QT=QT, banks=banks: e.matmul(out=banks[bi_][:, (hh % 4) * 128:(hh % 4 + 1) * 128], lhsT=QT[64 * p:64 * p + 64, hh, :], rhs=keys2[64 * p:64 * p + 64, hh, :], start=True, stop=True), reads=[("qTs", b), "keys2"], writes=[bids[bi_]])
                SC4 = SC[:].rearrange("p (h q) k -> p h q k", q=2)
                for bi_ in range(4):
                    p, hq = bi_ // 2, bi_ % 2
                    dst = SC4[:, hq * 4:(hq + 1) * 4, p, :]
                    src = banks[bi_][:].rearrange("p (a k) -> p a k", a=4)
                    allq = [("scB", b, q4) for q4 in range(4)]
                    if bi_ % 2 == 0:
                        s.op("act", lambda e, dst=dst, src=src: e.copy(out=dst, in_=src), reads=[bids[bi_]], writes=[("scB", b, 2 * hq), ("scB", b, 2 * hq + 1)])
                    else:
                        s.op("dve", lambda e, dst=dst, src=src: e.tensor_copy(out=dst, in_=src), reads=[bids[bi_]], writes=[("scB", b, 2 * hq), ("scB", b, 2 * hq + 1)])
                if cutB < 4:
                    continue
                for hb in range(2):
                    hs = range(hb * 4, hb * 4 + 4)
                    scid = lambda hh: ("scB", b, hh // 2)
                    for hh in hs:
                        for p in range(2):
                            s.op("dve", lambda e, hh=hh, p=p, SC=SC, T16=T16: e.max(out=T16[:, hh, p, 0:8], in_=SC[:, 2 * hh + p, :]), reads=[scid(hh)], writes=[("t16", b, hh, p)])
                    for hh in hs:
                        for p in range(2):
                            s.op("dve", lambda e, hh=hh, p=p, SC=SC, T16=T16: e.match_replace(out=wk4[:, hh % 4, p * 128:(p + 1) * 128], in_to_replace=T16[:, hh, p, 0:8], in_values=SC[:, 2 * hh + p, :], imm_value=-1e30), reads=[scid(hh), ("t16", b, hh, p)], writes=[("wk4", hh % 4, p)])
                    for hh in hs:
                        for p in range(2):
                            s.op("dve", lambda e, hh=hh, p=p, T16=T16: e.max(out=T16[:, hh, p, 8:16], in_=wk4[:, hh % 4, p * 128:(p + 1) * 128]), reads=[("wk4", hh % 4, p)], writes=[("t16", b, hh, p)])
                    for hh in hs:
                        s.op("dve", lambda e, hh=hh, T16=T16: e.tensor_tensor(out=cand4[:, hh % 4, :].rearrange("p (a c) -> p a c", a=16), in0=T16[:, hh, 0, :].unsqueeze(2).to_broadcast([128, 16, 16]), in1=T16[:, hh, 1, :].unsqueeze(1).to_broadcast([128, 16, 16]), op=ALU.add), reads=[("t16", b, hh, 0), ("t16", b, hh, 1)], writes=[("cand4", hh % 4)])
                    for hh in hs:
                        s.op("dve", lambda e, hh=hh, C16=C16: e.max(out=C16[:, hh, 0:8], in_=cand4[:, hh % 4, :]), reads=[("cand4", hh % 4)], writes=[("c16", b, hh)])
                    for hh in hs:
                        s.op("dve", lambda e, hh=hh, C16=C16: e.match_replace(out=wk4[:, hh % 4, :], in_to_replace=C16[:, hh, 0:8], in_values=cand4[:, hh % 4, :], imm_value=-1e30), reads=[("cand4", hh % 4), ("c16", b, hh)], writes=[("wk4", hh % 4, 0), ("wk4", hh % 4, 1)])
                    for hh in hs:
                        s.op("dve", lambda e, hh=hh, C16=C16: e.max(out=C16[:, hh, 8:16], in_=wk4[:, hh % 4, :]), reads=[("wk4", hh % 4, 0), ("wk4", hh % 4, 1)], writes=[("c16", b, hh)])
                if cutB < 5:
                    continue
                allc = [("c16", b, hh) for hh in range(8)]; allt = [("t16", b, hh, 0) for hh in range(8)]
                THR = thr[b]; NEGM = negm[b]; M1 = m1[b]; ZS = Zs[b]; KAP = kap[b]; TH2 = th2[b]; E16 = e16[b]
                s.op("dve", lambda e, C16=C16, THR=THR: e.tensor_scalar(out=THR[:], in0=C16[:, :, 15], scalar1=-1e-4, scalar2=None, op0=ALU.add), reads=allc, writes=[("thr", b)])
                s.op("dve", lambda e, C16=C16, NEGM=NEGM: e.tensor_scalar(out=NEGM[:], in0=C16[:, :, 0], scalar1=-1.0, scalar2=None, op0=ALU.mult), reads=allc, writes=[("negm", b)])
                s.op("dve", lambda e, T16=T16, M1=M1: e.tensor_copy(out=M1[:], in_=T16[:, :, 0, 0]), reads=allt, writes=[("m1", b)])
                s.op("dve", lambda e, C16=C16, NEGM=NEGM, E16=E16: e.tensor_tensor(out=E16[:], in0=C16[:], in1=NEGM[:].unsqueeze(2).to_broadcast([128, 8, 16]), op=ALU.add), reads=allc + [("negm", b)], writes=[("e16", b)])
                s.op("act", lambda e, E16=E16: e.activation(out=E16[:], in_=E16[:], func=AF.Exp), reads=[("e16", b)], writes=[("e16", b)])
                s.op("dve", lambda e, E16=E16, ZS=ZS: e.tensor_reduce(out=ZS[:], in_=E16[:], axis=AX.X, op=ALU.add), reads=[("e16", b)], writes=[("Zs", b)])
                s.op("dve", lambda e, KAP=KAP, THR=THR, NEGM=NEGM: e.tensor_tensor(out=KAP[:], in0=THR[:], in1=NEGM[:], op=ALU.add), reads=[("thr", b), ("negm", b)], writes=[("kap", b)])
                s.op("act", lambda e, KAP=KAP: e.activation(out=KAP[:], in_=KAP[:], func=AF.Exp), reads=[("kap", b)], writes=[("kap", b)])
                s.op("dve", lambda e, ZS=ZS: e.reciprocal(out=ZS[:], in_=ZS[:]), reads=[("Zs", b)], writes=[("Zs", b)])
                s.op("dve", lambda e, KAP=KAP, ZS=ZS: e.tensor_tensor(out=KAP[:], in0=KAP[:], in1=ZS[:], op=ALU.mult), reads=[("kap", b), ("Zs", b)], writes=[("kap", b)])
                s.op("dve", lambda e, TH2=TH2, THR=THR, M1=M1: e.tensor_tensor(out=TH2[:], in0=THR[:], in1=M1[:], op=ALU.subtract), reads=[("thr", b), ("m1", b)], writes=[("th2", b)])
                sc4 = SC[:].rearrange("p (h q) k -> p h q k", q=2)
                allsc = [("scB", b, q4) for q4 in range(4)]
                s.op("dve", lambda e, sc4=sc4, M1=M1: e.tensor_tensor(out=sc4[:, :, 0, :], in0=sc4[:, :, 0, :], in1=M1[:].unsqueeze(2).to_broadcast([128, 8, 128]), op=ALU.subtract), reads=allsc + [("m1", b)], writes=allsc)
                s.op("pool", lambda e, sc4=sc4, TH2=TH2: e.tensor_tensor(out=sc4[:, :, 1, :], in0=sc4[:, :, 1, :], in1=TH2[:].unsqueeze(2).to_broadcast([128, 8, 128]), op=ALU.subtract), reads=allsc + [("th2", b)], writes=allsc)
                s.op("act", lambda e, SC=SC: e.activation(out=SC[:], in_=SC[:], func=AF.Exp), reads=allsc, writes=allsc)
                s.dma(SC_s[tsl, :], SC[:].rearrange("p a b -> p (a b)"), reads=allsc, writes=["SC_s"], queue="pool")
                s.dma(KAP_s[tsl, :], KAP[:], reads=[("kap", b)], writes=["KAP_s"], queue="pool")
            s.flush()
        if upto <= 7:
            return nc

        GI = 2
        NG = 128 // GI
        NB = 2
        with contextlib.ExitStack() as st:
            _xa = T(st, "xc", [128, D]); xa = [_xa, _xa]; yb = T(st, "ybc", [128, D]); qsb = yb
            ss = T(st, "ssc", [128, 1]); rstd = T(st, "rstdc", [128, 1])
            h2r = [[TR(st, f"h2c{k}{t}", [128, 8, 128]) for t in range(NB)] for k in range(2)]
            sc = [[T(st, f"scc{k}{t}", [128, 16, 128]) for t in range(NB)] for k in range(2)]
            kap = [[T(st, f"kapc{k}{t}", [128, 8]) for t in range(NB)] for k in range(2)]
            dg = [[TR(st, f"dgc{k}{t}", [128, 8, 128]) for t in range(NB)] for k in range(2)]
            UT = [TR(st, f"UT{i}", [128, 8, GI * 128]) for i in range(2)]
            VG = [TR(st, f"VG{i}", [128, GI, D]) for i in range(2)]
            pe_t = [T(st, f"pec{t}", [128, 8, GI * 128]) for t in range(NB)]
            Mr2 = [[TR(st, f"Mr{k}{t}", [128, 8, GI * 128]) for t in range(NB)] for k in range(2)]
            Sm2 = [TR(st, f"Sm{k}", [128, 8, GI * 128]) for k in range(2)]
            negone = T(st, "negone", [128, 1])
            s.op("pool", lambda e: e.memset(negone[:], -1.0), writes=["negone"])
            W5 = NB * GI * 128
            g1 = [T(st, f"g1c{i}", [128, W5]) for i in range(2)]; Pm_ = [T(st, f"Pmc{i}", [128, W5]) for i in range(2)]
            PT = [TR(st, f"PT{i}", [128, NB * GI, 128]) for i in range(2)]
            bk = [PS(st, f"bkC{i}", [128, 512]) for i in range(8)]
            nblk = ntl6 // NB
            ngr = (2 if "small2" in dbg else NG)
            gcount = 0

            def blk_load(blk):
                k = blk % 2
                for tau in range(NB):
                    i = blk * NB + tau
                    tsl = slice(i * 128, (i + 1) * 128)
                    s.dma(h2r[k][tau][:], H2R_s[:, tsl].rearrange("(kc p) t -> p kc t", p=128), reads=["H2R_s"], writes=[("h2c", k, tau)], queue="pool")
                    s.dma(sc[k][tau][:].rearrange("p a b -> p (a b)"), SC_s[tsl, :], reads=["SC_s"], writes=[("scc", k, tau)], queue="pool")
                    s.dma(kap[k][tau][:], KAP_s[tsl, :], reads=["KAP_s"], writes=[("kapc", k, tau)], queue="pool")
                    for hh in range(8):
                        s.op("dve", lambda e, hh=hh, tau=tau, k=k: e.tensor_scalar(out=dg[k][tau][:, hh, :], in0=ident, scalar1=kap[k][tau][:, hh:hh + 1], scalar2=None, op0=ALU.mult), reads=["cm", ("kapc", k, tau)], writes=[("dgc", k, tau)])

            blk_load(0)
            for blk in range(nblk):
              kb = blk % 2
              pU = [[bk[4 + 2 * t + hf] for hf in range(2)] for t in range(NB)]
              ub = [k % 2 for k in range(ngr + 4)]

              def g_utload(g):
                  u = g % 2; ub[g] = u
                  e0 = g * GI * 128
                  s.dma(UT[u][:], UTr_s[:, e0:e0 + GI * 128].rearrange("(kc p) n -> p kc n", p=128), reads=["UTr_s"], writes=[("UT", u)], queue="sp")

              def g_vgload(g):
                  u = g % 2
                  e0 = g * GI * 128
                  s.dma(VG[u][:], Vr_s[e0:e0 + GI * 128, :].rearrange("(a p) n -> p a n", p=128), reads=["Vr_s"], writes=[("VG", u)], queue="act")

              def g_prodmask(g):
                  mk = g % 2
                  for tau in range(NB):
                      sc4 = sc[kb][tau][:].rearrange("p (h q) k -> p h q k", q=2)
                      e1b = sc4[:, :, 0, g * GI:(g + 1) * GI].unsqueeze(3).to_broadcast([128, 8, GI, 128])
                      e2b = sc4[:, :, 1, :].unsqueeze(2).to_broadcast([128, 8, GI, 128])
                      s.op("dve", lambda e, e1b=e1b, e2b=e2b, tau=tau: e.tensor_tensor(out=pe_t[tau][:].rearrange("p h (a k) -> p h a k", a=GI), in0=e1b, in1=e2b, op=ALU.mult), reads=[("scc", kb, tau)], writes=[("pe", tau)])
                      if tau == 0:
                          s.op("dve", lambda e, mk=mk: e.scalar_tensor_tensor(out=Mr2[mk][0][:], in0=pe_t[0][:], scalar=1.0, in1=pe_t[0][:], op0=ALU.is_ge, op1=ALU.mult), reads=[("pe", 0)], writes=[("Mr", mk, 0)])
                      else:
                          s.op("act", lambda e, mk=mk: e.activation(out=Mr2[mk][1][:], in_=pe_t[1][:], func=AF.Relu, bias=negone[:, 0:1], scale=1.0), reads=[("pe", 1), "negone"], writes=[("Mr", mk, 1)])
                          s.op("act", lambda e, mk=mk: e.activation(out=Sm2[mk][:], in_=Mr2[mk][1][:].bitcast(F32), func=AF.Sign), reads=[("Mr", mk, 1)], writes=[("Sm", mk)])

              def g_act(g):
                  u = ub[g]; pR = bk[u]
                  for tau in range(NB):
                      for kc in range(8):
                          s.op("pe", lambda e, kc=kc, u=u, tau=tau, pR=pR, H=h2r[kb][tau]: e.matmul(out=pR[:, tau * GI * 128:(tau + 1) * GI * 128], lhsT=H[:, kc, :], rhs=UT[u][:, kc, :], start=(kc == 0), stop=(kc == 7)), reads=[("h2c", kb, tau), ("UT", u)], writes=[B(u)])

              def g_gelu(g):
                  u = ub[g]; pR = bk[u]
                  s.op("act", lambda e, pR=pR, u=u: e.activation(out=g1[u][:], in_=pR[:, 0:W5], func=AF.Gelu_apprx_tanh), reads=[B(u)], writes=[("g1", u)])

              def g_gd(g):
                  u = ub[g]; pG = bk[2 + u]; mk = g % 2
                  for hh in range(8):
                      s.op("pe", lambda e, hh=hh, pG=pG, DG=dg[kb][0], M=Mr2[mk][0]: e.matmul(out=pG[:, 0:GI * 128], lhsT=DG[:, hh, :], rhs=M[:, hh, :], start=(hh == 0), stop=(hh == 7)), reads=[("dgc", kb, 0), ("Mr", mk, 0)], writes=[B(2 + u)])
                  for hh in range(8):
                      s.op("pe", lambda e, hh=hh, pG=pG, DG=dg[kb][1], M=Mr2[mk][1]: e.matmul(out=pG[:, GI * 128:2 * GI * 128], lhsT=DG[:, hh, :], rhs=M[:, hh, :], start=(hh == 0), stop=False), reads=[("dgc", kb, 1), ("Mr", mk, 1)], writes=[B(2 + u)])
                  for hh in range(8):
                      s.op("pe", lambda e, hh=hh, pG=pG, DG=dg[kb][1], M=Sm2[mk]: e.matmul(out=pG[:, GI * 128:2 * GI * 128], lhsT=DG[:, hh, :], rhs=M[:, hh, :], start=False, stop=(hh == 7)), reads=[("dgc", kb, 1), ("Sm", mk)], writes=[B(2 + u)])

              def g_pm(g):
                  u = ub[g]; pG = bk[2 + u]
                  s.op("dve", lambda e, u=u, pG=pG: e.tensor_tensor(out=Pm_[u][:], in0=g1[u][:], in1=pG[:, 0:W5], op=ALU.mult), reads=[("g1", u), B(2 + u)], writes=[("Pm", u)])

              def g_tr(g):
                  u = ub[g]; pW = bk[2 + u]
                  for k in range(NB * GI):
                      s.op("pe", lambda e, k=k, u=u, pW=pW: e.transpose(out=pW[:, k * 128:(k + 1) * 128], in_=Pm_[u][:, k * 128:(k + 1) * 128], identity=ident), reads=[("Pm", u), "cm"], writes=[B(2 + u)])
                  s.op("act", lambda e, u=u, pW=pW: e.copy(out=PT[u][:].rearrange("p a b -> p (a b)"), in_=pW[:, 0:W5]), reads=[B(2 + u)], writes=[("PT", u)])

              def g_out(g):
                  u = ub[g]
                  for tau in range(NB):
                      for a in range(GI):
                          for half in range(2):
                              s.op("pe", lambda e, a=a, half=half, u=u, tau=tau, first=(g == 0 and a == 0), last=(g == ngr - 1 and a == GI - 1): e.matmul(out=pU[tau][half][:], lhsT=PT[u][:, tau * GI + a, :], rhs=VG[u][:, a, half * 512:(half + 1) * 512], start=first, stop=last),
                                   reads=[("PT", u), ("VG", u)], writes=[B(4 + 2 * tau + half)])

              ok = lambda k: 0 <= k < ngr
              for g in range(-3, ngr + 1):
                  if ok(g + 3):
                      g_utload(g + 3)
                  if ok(g - 1):
                      g_out(g - 1)
                  if ok(g + 1):
                      g_vgload(g + 1)
                      g_gd(g + 1)
                  if ok(g):
                      g_tr(g)
                  if ok(g + 2):
                      g_act(g + 2); g_gelu(g + 2)
                  if ok(g + 3):
                      g_prodmask(g + 3)
                  if ok(g + 1):
                      g_pm(g + 1)
                  if g == 4 and blk + 1 < nblk:
                      blk_load(blk + 1)
              for tau in range(NB):
                i = blk * NB + tau
                XA = xa[tau]; xid = "xc"
                s.dma(XA[:], X1_s[i * 128:(i + 1) * 128, :], reads=["X1_s"], writes=[xid], queue="pool")
                for half in range(2):
                    s.op("dve", lambda e, half=half, tau=tau: e.tensor_tensor(out=yb[:, half * 512:(half + 1) * 512], in0=pU[tau][half][:], in1=bvv(BV_GT2)[:, half * 512:(half + 1) * 512], op=ALU.mult), reads=[B(4 + 2 * tau + half), "bv"], writes=["ybc"])
                s.op("pool", lambda e, XA=XA: e.tensor_tensor(out=XA[:], in0=XA[:], in1=yb[:], op=ALU.add), reads=[xid, "ybc"], writes=[xid])
                s.op("act", lambda e, XA=XA: e.activation(out=qsb[:], in_=XA[:], func=AF.Square, accum_out=ss[:]), reads=[xid, "ybc"], writes=["ybc", "ssc"])
                s.op("dve", lambda e: e.tensor_scalar(out=rstd[:], in0=ss[:], scalar1=1.0 / D, scalar2=1e-6, op0=ALU.mult, op1=ALU.add), reads=["ssc"], writes=["rstdc"])
                s.op("act", lambda e: e.sqrt(out=rstd[:], in_=rstd[:]), reads=["rstdc"], writes=["rstdc"])
                s.op("dve", lambda e: e.reciprocal(out=rstd[:], in_=rstd[:]), reads=["rstdc"], writes=["rstdc"])
                s.op("dve", lambda e, XA=XA: e.scalar_tensor_tensor(out=yb[:], in0=XA[:], scalar=rstd[:, 0:1], in1=bvv(BV_FN), op0=ALU.mult, op1=ALU.mult), reads=[xid, "rstdc", "bv"], writes=["ybc"])
                s.dma(out_d[i * 128:(i + 1) * 128, :], yb[:], reads=["ybc"], writes=["out"], queue="pool")
            s.flush()
        return nc


def _rope(s, src, dst, tmp, R, rid, H, sid, did, tid="tmp"):
    sv = src.rearrange("p (h a b c) -> p h a b c", h=H, a=2, b=2)
    dv = dst.rearrange("p (h a b c) -> p h a b c", h=H, a=2, b=2)
    tv = tmp[:, 0:H * 32].rearrange("p (h a c) -> p h a c", h=H, a=2)
    rv = R[:].rearrange("p (a b c) -> p a b c", a=2, b=2)
    cosb = rv[:, :, 0, :].unsqueeze(1).to_broadcast([128, H, 2, 16])
    sinb = rv[:, :, 1, :].unsqueeze(1).to_broadcast([128, H, 2, 16])
    x1 = sv[:, :, :, 0, :]; x2 = sv[:, :, :, 1, :]
    o1 = dv[:, :, :, 0, :]; o2 = dv[:, :, :, 1, :]
    s.op("dve", lambda e: e.tensor_tensor(out=o1, in0=x1, in1=cosb, op=ALU.mult), reads=[sid, rid], writes=[did])
    s.op("dve", lambda e: e.tensor_tensor(out=tv, in0=x2, in1=sinb, op=ALU.mult), reads=[sid, rid], writes=[tid])
    s.op("dve", lambda e: e.tensor_tensor(out=o1, in0=o1, in1=tv, op=ALU.subtract), reads=[did, tid], writes=[did])
    s.op("dve", lambda e: e.tensor_tensor(out=o2, in0=x1, in1=sinb, op=ALU.mult), reads=[sid, rid, did], writes=[did])
    s.op("dve", lambda e: e.tensor_tensor(out=tv, in0=x2, in1=cosb, op=ALU.mult), reads=[sid, rid, did], writes=[tid])
    s.op("dve", lambda e: e.tensor_tensor(out=o2, in0=o2, in1=tv, op=ALU.add), reads=[did, tid], writes=[did])


def _host_inputs(inputs, b, consts):
    g = lambda k: np.ascontiguousarray(inputs[k], dtype=np.float32)
    m = {
        "x": g("x")[b], "c": g("c")[b], "ctx": g("ctx")[b], "c_ctx": g("c_ctx"),
        "w_ada": g("w_ada")[0], "b_ada": g("b_ada")[0], "norm_mix": g("norm_mix")[0], "norm_ffn": g("norm_ffn")[0],
        "w_in": g("w_in")[0], "b_gate": g("b_gate")[0], "attn_sink": g("attn_sink")[0], "dn_conv": g("dn_conv")[0],
        "dn_a_log_f": g("dn_a_log_f")[0], "dn_dt_bias_f": g("dn_dt_bias_f")[0], "dn_a_log_b": g("dn_a_log_b")[0], "dn_dt_bias_b": g("dn_dt_bias_b")[0],
        "dn_norm": g("dn_norm")[0], "w_br_attn": g("w_br_attn")[0], "w_br_dn": g("w_br_dn")[0], "w_out": g("w_out")[0],
        "peer_wq": g("peer_wq")[0], "final_norm": g("final_norm"),
    }
    m.update(consts)
    return {k: np.ascontiguousarray(v) for k, v in m.items()}


_SHARED = {}


def kernel(**inputs):
    consts = _consts()
    nc = build()
    keysT = np.ascontiguousarray(np.transpose(np.asarray(inputs["peer_keys"], np.float32)[0], (1, 3, 0, 2)).reshape(128, 8, 128))
    uT = np.ascontiguousarray(np.asarray(inputs["peer_u"], np.float32)[0].T)
    pv = np.ascontiguousarray(np.asarray(inputs["peer_v"], np.float32)[0])
    in_maps = []
    for b in range(8):
        m = _host_inputs(inputs, b, consts)
        m["peer_keysT"] = keysT; m["peer_uT"] = uT; m["peer_v"] = pv
        in_maps.append(m)
    res = run_bass_kernel_spmd(nc, in_maps, core_ids=list(range(8)))
    return np.stack([np.asarray(r["out"], dtype=np.float32) for r in res.results], axis=0)
```

```python
import contextlib
import numpy as np
import concourse.bass as bass
import concourse.mybir as mybir
from concourse.bass_utils import run_bass_kernel_spmd

F32 = mybir.dt.float32
F32R = mybir.dt.float32r
ALU = mybir.AluOpType
AF = mybir.ActivationFunctionType
AX = mybir.AxisListType

D = 1024
S = 8192
CTX = 256
TALL = CTX + S
NT = S // 128
IN_COLS = 4880
NEG = -30000.0


class _Ins:
    __slots__ = ("eng", "fn", "deps", "signal", "sig_no", "dma", "idx")

    def __init__(self, eng, fn, dma=None):
        self.eng = eng
        self.fn = fn
        self.deps = []
        self.signal = False
        self.sig_no = None
        self.dma = dma
        self.idx = None


class Sch:
    EPOCH = 20000
    NDMA = 24
    NEP = 16

    def __init__(self, nc, st):
        self.nc = nc
        self.engs = ("pe", "act", "dve", "pool", "sp")
        self.nep = {"pe": 12, "act": 4, "dve": 6, "pool": 3, "sp": 1}
        self.sems = {e: [st.enter_context(nc.semaphore(f"s_{e}_{i}")) for i in range(self.nep[e])] for e in self.engs}
        self.dsems = [st.enter_context(nc.semaphore(f"s_dma_{i}")) for i in range(self.NDMA)]
        self.sigc = {e: 0 for e in self.engs}
        self.dma_rr = 0
        self.dma_cnt = [0] * self.NDMA
        self.dma_last = [None] * self.NDMA
        self._reset()

    def _reset(self):
        self.q = {e: [] for e in self.engs}
        self.lastw = {}
        self.readers = {}

    def _add(self, ins, reads, writes):
        q = self.q[ins.eng]
        ins.idx = len(q)
        deps = []
        for r in reads:
            w = self.lastw.get(r)
            if w is not None:
                deps.append((w, "raw"))
        for w_ in writes:
            w = self.lastw.get(w_)
            if w is not None:
                deps.append((w, "waw"))
            for rd in self.readers.get(w_, ()):
                deps.append((rd, "war"))
        for d, kind in deps:
            if d is ins:
                continue
            if d.dma is None and ins.dma is None and d.eng == ins.eng:
                if ins.eng == "pe":
                    continue
                if kind != "raw":
                    continue
            ins.deps.append(d)
            if d.dma is None:
                d.signal = True
        for r in reads:
            self.readers.setdefault(r, []).append(ins)
        for w_ in writes:
            self.lastw[w_] = ins
            self.readers[w_] = []
        q.append(ins)
        return ins

    PSUM_NAMES = {"bk", "pm", "pT", "pY", "pX", "pN", "pK", "pb", "pS", "pO", "pQ", "pZ", "pR", "pU", "pW"}

    def op(self, eng, fn, reads=(), writes=()):
        writes = list(writes)
        if eng != "pe":
            for r in reads:
                if isinstance(r, tuple) and r[0] in self.PSUM_NAMES and r not in writes:
                    writes.append(r)
        return self._add(_Ins(eng, fn), list(reads), writes)

    def dma(self, out, in_, reads=(), writes=(), queue="sp", **kw):
        slot = self.dma_rr
        self.dma_rr = (self.dma_rr + 1) % self.NDMA
        self.dma_cnt[slot] += 1
        n = self.dma_cnt[slot]
        ins = _Ins(queue, lambda e: e.dma_start(out=out, in_=in_, **kw), dma=(slot, n))
        prev = self.dma_last[slot]
        self._add(ins, list(reads), list(writes))
        if prev is not None:
            ins.deps.append(prev)
        self.dma_last[slot] = ins
        return ins

    def flush(self):
        nc = self.nc
        for e, q in self.q.items():
            for ins in q:
                if ins.dma is None and ins.signal:
                    ins.sig_no = self.sigc[e]
                    self.sigc[e] += 1
            assert self.sigc[e] < self.EPOCH * self.nep[e], f"too many signals on {e}: {self.sigc[e]}"
        dma_final = list(self.dma_cnt)
        with nc.Block() as block:
            def run(ename):
                def body(eng):
                    seen_c = {}
                    seen_d = {}
                    for ins in self.q[ename]:
                        wc = {}
                        wd = {}
                        for d in ins.deps:
                            if d.dma is None:
                                if d.sig_no is None:
                                    continue
                                if seen_c.get(d.eng, -1) < d.sig_no:
                                    wc[d.eng] = max(wc.get(d.eng, -1), d.sig_no)
                            else:
                                s_, n = d.dma
                                if seen_d.get(s_, 0) < n:
                                    wd[s_] = max(wd.get(s_, 0), n)
                        for e2, sn in wc.items():
                            eng.wait_ge(self.sems[e2][sn // self.EPOCH], sn % self.EPOCH + 1)
                            seen_c[e2] = sn
                        for s_, n in wd.items():
                            eng.wait_ge(self.dsems[s_], 16 * n)
                            seen_d[s_] = n
                        h = ins.fn(eng)
                        if ins.dma is not None:
                            h.then_inc(self.dsems[ins.dma[0]], 16)
                        elif ins.signal:
                            h.then_inc(self.sems[ename][ins.sig_no // self.EPOCH], 1)
                    if ename == "sp":
                        for s_, n in enumerate(dma_final):
                            if n > 0:
                                eng.wait_ge(self.dsems[s_], 16 * n)
                return body

            block.sync(run("sp"))
            block.tensor(run("pe"))
            block.scalar(run("act"))
            block.vector(run("dve"))
            block.gpsimd(run("pool"))
        nc.all_engine_barrier()
        self._reset()


def _consts():
    c = {}
    ident = np.eye(128, dtype=np.float32)
    ones = np.ones((128, 128), np.float32)
    idx = np.arange(128)
    same = (idx[:, None] // 64 == idx[None, :] // 64).astype(np.float32)
    m1f = ((idx[:, None] <= idx[None, :]) * same).astype(np.float32)
    m1b = ((idx[:, None] >= idx[None, :]) * same).astype(np.float32)
    sel0 = np.zeros((128, 128), np.float32); sel0[:64, :] = 1
    sel1 = np.zeros((128, 128), np.float32); sel1[64:, :] = 1
    low_incl = ((idx[None, :] <= idx[:, None]) * same)
    up_incl = ((idx[None, :] >= idx[:, None]) * same)
    low_strict = ((idx[None, :] < idx[:, None]) * same)
    up_strict = ((idx[None, :] > idx[:, None]) * same)
    negmask = lambda m: np.where(m > 0, 0.0, NEG).astype(np.float32)
    w_prev = (idx[None, :] <= idx[:, None]).astype(np.float32)
    w_next = (idx[:, None] <= idx[None, :]).astype(np.float32)
    mats = [ident, ones, same, m1f, -m1f, m1b, -m1b, sel0, sel1,
            negmask(low_incl), negmask(up_incl), -low_strict.astype(np.float32), -up_strict.astype(np.float32),
            w_prev, w_next]
    c["cmat"] = np.ascontiguousarray(np.stack(mats, axis=1)).astype(np.float32)
    pos = np.arange(S)
    inv = (10000.0 ** (-np.arange(16, dtype=np.float32) / 16)).astype(np.float32)
    ar = (pos // 64).astype(np.float32)[:, None] * inv[None, :]
    ac = (pos % 64).astype(np.float32)[:, None] * inv[None, :]
    c["rope"] = np.concatenate([np.cos(ar), np.sin(ar), np.cos(ac), np.sin(ac)], axis=1).astype(np.float32)
    return c

(C_ID, C_ONES, C_SAME, C_M1F, C_NM1F, C_M1B, C_NM1B, C_SEL0, C_SEL1, C_NLOW, C_NUP, C_SLOW, C_SUP, C_WPREV, C_WNEXT) = range(15)


def build(upto=99, dbg=()):
    nc = bass.Bass("TRN2", target_bir_lowering=False)
    nc.dge_precook = False
    inp = lambda name, shape: nc.dram_tensor(name, list(shape), F32, kind="ExternalInput").ap()
    x_d = inp("x", [S, D]); c_d = inp("c", [D]); ctx_d = inp("ctx", [CTX, D]); cctx_d = inp("c_ctx", [D])
    wada_d = inp("w_ada", [D, 6 * D]); bada_d = inp("b_ada", [6 * D])
    nmix_d = inp("norm_mix", [D]); nffn_d = inp("norm_ffn", [D])
    win_d = inp("w_in", [D, IN_COLS]); bgate_d = inp("b_gate", [2 * D])
    sink_d = inp("attn_sink", [8]); conv_d = inp("dn_conv", [5, 1536])
    alf_d = inp("dn_a_log_f", [4]); dtf_d = inp("dn_dt_bias_f", [4]); alb_d = inp("dn_a_log_b", [4]); dtb_d = inp("dn_dt_bias_b", [4])
    dnn_d = inp("dn_norm", [128]); wba_d = inp("w_br_attn", [512, D]); wbd_d = inp("w_br_dn", [512, D]); wout_d = inp("w_out", [D, D])
    pwq_d = inp("peer_wq", [D, D]); pkeys_d = inp("peer_keysT", [128, 8, 128]); pu_d = inp("peer_uT", [D, 16384]); pv_d = inp("peer_v", [16384, D])
    fnorm_d = inp("final_norm", [D]); cmat_d = inp("cmat", [128, 15, 128]); rope_d = inp("rope", [S, 64])
    out_d = nc.dram_tensor("out", [S, D], F32, kind="ExternalOutput").ap()
    scr = lambda name, shape: nc.dram_tensor(name, list(shape), F32, kind=("ExternalOutput" if name in dbg else "Internal")).ap()
    QT_s = scr("QT_s", [64, 8, S])
    KT_s = scr("KT_s", [64, 2, TALL])
    V_s = scr("V_s", [TALL, 2, 65])
    RT_s = scr("RT_s", [1536, TALL])
    Z_s = scr("Z_s", [S, 512])
    GB_s = scr("GB_s", [TALL, 16])
    GT_s = scr("GT_s", [S, 2048])
    QK_s = scr("QK_s", [1024, TALL])
    KV_s = scr("KV_s", [TALL, 1024])
    OD_s = scr("OD_s", [2, S, 512])
    OA_s = scr("OA_s", [S, 512])
    MOD_s = scr("MOD_s", [8, D])

    with contextlib.ExitStack() as gst:
        s = Sch(nc, gst)
        _uid = [0]

        def _nm(name):
            _uid[0] += 1
            return f"{name}_u{_uid[0]}"
        T = lambda st, name, shape: st.enter_context(nc.sbuf_tensor(_nm(name), list(shape), F32))
        PS = lambda st, name, shape: st.enter_context(nc.psum_tensor(_nm(name), list(shape), F32))
        cm = T(gst, "cm", [128, 15, 128])
        s.dma(cm[:], cmat_d, writes=["cm"])
        ident = cm[:, C_ID, :]
        BV_G1, BV_SH1, BV_GT1, BV_G2, BV_SH2, BV_GT2, BV_CG1, BV_CSH1, BV_FN = range(9)
        bvB = T(gst, "bvB", [128, 5, D])
        stA = contextlib.ExitStack()
        bvA = T(stA, "bvA", [128, 4, D])
        _amap = {BV_G1: 0, BV_SH1: 1, BV_CG1: 2, BV_CSH1: 3}
        _bmap = {BV_GT1: 0, BV_G2: 1, BV_SH2: 2, BV_GT2: 3, BV_FN: 4}

        def bvv(k):
            return bvA[:, _amap[k], :] if k in _amap else bvB[:, _bmap[k], :]

        with contextlib.ExitStack() as st:
            cc = T(st, "cc", [128, 2, 8]); cs = T(st, "cs", [128, 2, 8]); lh = T(st, "lh", [128, 2, 8, 128])
            wa = [T(st, f"wa{i}", [128, 8, 512]) for i in range(2)]
            bb = T(st, "bb", [128, 6 * D]); nm = T(st, "nm", [128, 2, D])
            pm = [PS(st, f"pm{i}", [128, 512]) for i in range(2)]
            s.dma(cc[:, 0, :], c_d.rearrange("(kc p) -> p kc", p=128), writes=["cc"], allow_slow_non_contiguous=True)
            s.dma(cc[:, 1, :], cctx_d.rearrange("(kc p) -> p kc", p=128), writes=["cc"], allow_slow_non_contiguous=True)
            s.dma(bb[:], bada_d.partition_broadcast(128), writes=["bb"])
            s.dma(nm[:, 0, :], nmix_d.partition_broadcast(128), writes=["nm"])
            s.dma(nm[:, 1, :], nffn_d.partition_broadcast(128), writes=["nm"])
            s.dma(bvv(BV_FN)[:, :], fnorm_d.partition_broadcast(128), writes=["bv"])
            s.op("act", lambda e: e.activation(out=cs[:], in_=cc[:], func=AF.Silu), reads=["cc"], writes=["cs"])
            s.op("dve", lambda e: e.tensor_copy(out=lh[:], in_=cs[:].unsqueeze(3).to_broadcast([128, 2, 8, 128])), reads=["cs"], writes=["lh"])
            jobs = [(0, nb) for nb in range(12)] + [(1, nb) for nb in range(4)]
            for ji, (w, nb) in enumerate(jobs):
                wt = wa[ji % 2]; p = pm[ji % 2]
                s.dma(wt[:], wada_d[:, nb * 512:(nb + 1) * 512].rearrange("(kc p) n -> p kc n", p=128), writes=[("wa", ji % 2)], queue=("sp" if ji % 2 == 0 else "act"))
                for kc in range(8):
                    s.op("pe", lambda e, w=w, kc=kc, wt=wt, p=p: e.matmul(out=p[:], lhsT=lh[:, w, kc, :], rhs=wt[:, kc, :], start=(kc == 0), stop=(kc == 7)),
                         reads=["lh", ("wa", ji % 2)], writes=[("pm", ji % 2)])
                ch, half = nb // 2, nb % 2
                if w == 0:
                    dst = {0: BV_SH1, 1: BV_G1, 2: BV_GT1, 3: BV_SH2, 4: BV_G2, 5: BV_GT2}[ch]
                else:
                    dst = {0: BV_CSH1, 1: BV_CG1}[ch]
                o = bvv(dst)[:, half * 512:(half + 1) * 512]
                s.op("dve", lambda e, o=o, p=p, nb=nb: e.tensor_tensor(out=o, in0=p[:], in1=bb[:, nb * 512:(nb + 1) * 512], op=ALU.add),
                     reads=[("pm", ji % 2), "bb"], writes=["bv"])
            for dst, ni in ((BV_G1, 0), (BV_G2, 1), (BV_CG1, 0)):
                s.op("dve", lambda e, dst=dst, ni=ni: e.scalar_tensor_tensor(out=bvv(dst)[:, :], in0=bvv(dst)[:, :], scalar=1.0, in1=nm[:, ni, :], op0=ALU.add, op1=ALU.mult),
                     reads=["bv", "nm"], writes=["bv"])
            s.flush()
        if upto <= 0:
            stA.close()
            return nc

        blocks = [(0, 512), (512, 256), (768, 512), (1280, 512), (1792, 512), (2304, 512), (2816, 16)] + [(2832 + 512 * i, 512) for i in range(4)]
        with contextlib.ExitStack() as st:
            xt = [T(st, f"xt{i}", [128, D]) for i in range(2)]
            junk = T(st, "junk", [128, D]); ss = T(st, "ss", [128, 1]); rstd = T(st, "rstd", [128, 1])
            h = T(st, "h", [128, D]); hT = T(st, "hT", [128, 8, 128])
            wb = [st.enter_context(nc.sbuf_tensor(_nm(f"wb{i}"), [128, 8, 512], F32R)) for i in range(3)]
            rp = [T(st, f"rp{i}", [128, 64]) for i in range(2)]
            qs = T(st, "qs", [128, 512]); qr = T(st, "qr", [128, 512]); tmp = T(st, "tmp", [128, 512])
            qT = T(st, "qT", [64, 8, 128]); kvs = T(st, "kvs", [128, 256]); kr = T(st, "kr", [128, 128]); kT = T(st, "kT", [64, 2, 128])
            va = T(st, "va", [128, 2, 65]); rw = T(st, "rw", [128, 512]); rT = T(st, "rT", [128, 4, 128])
            zz = T(st, "zz", [128, 512]); gn = T(st, "gn", [128, 128]); ab = T(st, "ab", [128, 16]); abc = T(st, "abc", [128, 2, 8])
            gbo = T(st, "gbo", [128, 16]); gg = T(st, "gg", [128, 512]); bg = T(st, "bg", [128, 2048])
            pT = [PS(st, f"pT{i}", [128, 512]) for i in range(2)]
            pY = [PS(st, f"pY{i}", [128, 512]) for i in range(3)]
            pX = [PS(st, f"pX{i}", [128, 512]) for i in range(2)]
            s.dma(bg[:], bgate_d.partition_broadcast(128), writes=["bg"])
            s.dma(gn[:], dnn_d.partition_broadcast(128), writes=["gn"])
            s.dma(abc[:, 0, 0:4], dtf_d.partition_broadcast(128), writes=["abc"])
            s.dma(abc[:, 0, 4:8], dtb_d.partition_broadcast(128), writes=["abc"])
            s.dma(abc[:, 1, 0:4], alf_d.partition_broadcast(128), writes=["abc"])
            s.dma(abc[:, 1, 4:8], alb_d.partition_broadcast(128), writes=["abc"])
            s.op("act", lambda e: e.activation(out=abc[:, 1, :], in_=abc[:, 1, :], func=AF.Exp), reads=["abc"], writes=["abc"])
            s.op("dve", lambda e: e.tensor_scalar(out=abc[:, 1, :], in0=abc[:, 1, :], scalar1=-1.0, scalar2=None, op0=ALU.mult), reads=["abc"], writes=["abc"])
            s.op("pool", lambda e: e.memset(va[:], 1.0), writes=[("va", 0)])
            wcount = [0]

            def rope_ops(src, dst, H):
                sv = src.rearrange("p (h a b c) -> p h a b c", h=H, a=2, b=2)
                dv = dst.rearrange("p (h a b c) -> p h a b c", h=H, a=2, b=2)
                tv = tmp[:, 0:H * 64].rearrange("p (h a b c) -> p h a b c", h=H, a=2, b=2)
                return sv, dv, tv

            tiles = [("c", i) for i in range(CTX // 128)] + [("l", i) for i in range(NT)]
            if upto == 1 and "small" in dbg:
                tiles = tiles[:4]
            qs2 = [qs, T(st, "qsb_", [128, 512])]; qr2 = [qr, T(st, "qrb_", [128, 512])]; tmp2 = [tmp, T(st, "tmpb_", [128, 512])]
            qT2 = [qT, T(st, "qTb_", [64, 8, 128])]; kvs2 = [kvs, T(st, "kvsb_", [128, 256])]; kr2 = [kr, T(st, "krb_", [128, 128])]; kT2 = [kT, T(st, "kTb_", [64, 2, 128])]
            va2 = [va, T(st, "vab_", [128, 2, 65])]; rw2 = [rw, T(st, "rwb_", [128, 512])]; rT2 = [rT, T(st, "rTb_", [128, 4, 128])]; zz2 = [zz, T(st, "zzb_", [128, 512])]
            ab2 = [ab, T(st, "abb_", [128, 16])]; gbo2 = [gbo, T(st, "gbob_", [128, 16])]; gg2 = [gg, T(st, "ggb_", [128, 512])]
            s.op("pool", lambda e: e.memset(va2[1][:], 1.0), writes=[("va", 1)])
            hT4 = [st.enter_context(nc.sbuf_tensor(_nm(f"hT4_{j}"), [128, 8, 128], F32R)) for j in range(4)]
            rp4 = [T(st, f"rp4_{j}", [128, 64]) for j in range(4)]
            pcount = [0]

            def prep(ti, kind, i, j):
                lat = kind == "l"
                src = x_d if lat else ctx_d
                tg = ti
                X = xt[ti % 2]; xid = ("xt", ti % 2)
                s.dma(X[:], src[i * 128:(i + 1) * 128, :], writes=[xid])
                if lat:
                    R = rp4[j]; rid = ("rp", j)
                    s.dma(R[:], rope_d[i * 128:(i + 1) * 128, :], writes=[rid], queue="act")
                s.op("act", lambda e, X=X: e.activation(out=junk[:], in_=X[:], func=AF.Square, accum_out=ss[:]), reads=[xid], writes=["junk", "ss"])
                s.op("dve", lambda e: e.tensor_scalar(out=rstd[:], in0=ss[:], scalar1=1.0 / D, scalar2=1e-6, op0=ALU.mult, op1=ALU.add), reads=["ss"], writes=["rstd"])
                s.op("act", lambda e: e.sqrt(out=rstd[:], in_=rstd[:]), reads=["rstd"], writes=["rstd"])
                s.op("dve", lambda e: e.reciprocal(out=rstd[:], in_=rstd[:]), reads=["rstd"], writes=["rstd"])
                G = BV_G1 if lat else BV_CG1
                SH = BV_SH1 if lat else BV_CSH1
                s.op("dve", lambda e, X=X, G=G: e.scalar_tensor_tensor(out=h[:], in0=X[:], scalar=rstd[:, 0:1], in1=bvv(G)[:, :], op0=ALU.mult, op1=ALU.mult), reads=[xid, "rstd", "bv"], writes=["h"])
                s.op("pool", lambda e, SH=SH: e.tensor_tensor(out=h[:], in0=h[:], in1=bvv(SH)[:, :], op=ALU.add), reads=["h", "bv"], writes=["h"])
                for hb in range(2):
                    for k4 in range(4):
                        kc = hb * 4 + k4
                        s.op("pe", lambda e, kc=kc, hb=hb, k4=k4: e.transpose(out=pT[hb][:, k4 * 128:(k4 + 1) * 128], in_=h[:, kc * 128:(kc + 1) * 128], identity=ident), reads=["h", "cm"], writes=[("pT", hb)])
                    eng = "act" if hb == 0 else "dve"
                    if eng == "act":
                        s.op("act", lambda e, hb=hb: e.copy(out=hT4[j][:, hb * 4:(hb + 1) * 4, :].rearrange("p a b -> p (a b)"), in_=pT[hb][:]), reads=[("pT", hb)], writes=[("hT", j, hb)])
                    else:
                        s.op("dve", lambda e, hb=hb: e.tensor_copy(out=hT4[j][:, hb * 4:(hb + 1) * 4, :].rearrange("p a b -> p (a b)"), in_=pT[hb][:]), reads=[("pT", hb)], writes=[("hT", j, hb)])

            def proj(ti, kind, i, j, bi, W, wi):
                lat = kind == "l"; tg = ti; R = rp4[j]; rid = ("rp", j)
                c0, cw = blocks[bi]
                pi_ = pcount[0] % 3; pp = pcount[0] % 2; pcount[0] += 1
                P = pY[pi_]
                for kc in range(8):
                    s.op("pe", lambda e, kc=kc, W=W, P=P, cw=cw: e.matmul(out=P[:, 0:cw], lhsT=hT4[j][:, kc, :], rhs=W[:, kc, 0:cw], start=(kc == 0), stop=(kc == 7)),
                         reads=[("hT", j, 0), ("hT", j, 1), ("wb", wi)], writes=[("pY", pi_)])
                pid = ("pY", pi_)
                if bi == 0:
                    s.op("act", lambda e, P=P: e.activation(out=qs2[pp][:], in_=P[:], func=AF.Copy, scale=0.125), reads=[pid], writes=[("qs", pp)])
                    _rope(s, qs2[pp][:], qr2[pp][:], tmp2[pp], R, rid, 8, ("qs", pp), ("qr", pp), ("tmp", pp))
                    for hh in range(8):
                        s.op("pe", lambda e, hh=hh: e.transpose(out=pX[hh // 4][0:64, (hh % 4) * 128:(hh % 4 + 1) * 128], in_=qr2[pp][:, hh * 64:(hh + 1) * 64], identity=ident), reads=[("qr", pp), "cm"], writes=[("pX", hh // 4)])
                    s.op("act", lambda e: e.copy(out=qT2[pp][:, 0:4, :].rearrange("p a b -> p (a b)"), in_=pX[0][0:64, :]), reads=[("pX", 0)], writes=[("qT", pp)])
                    s.op("dve", lambda e: e.tensor_copy(out=qT2[pp][:, 4:8, :].rearrange("p a b -> p (a b)"), in_=pX[1][0:64, :]), reads=[("pX", 1)], writes=[("qT", pp)])
                    s.dma(QT_s[:, :, i * 128:(i + 1) * 128], qT2[pp][:], reads=[("qT", pp)], writes=["QT_s"], queue="sp")
                elif bi == 1:
                    s.op("act", lambda e, P=P: e.copy(out=kvs2[pp][:], in_=P[:, 0:256]), reads=[pid], writes=[("kvs", pp)])
                    if lat:
                        _rope(s, kvs2[pp][:, 0:128], kr2[pp][:], tmp2[pp], R, rid, 2, ("kvs", pp), ("kr", pp), ("tmp", pp))
                        ksrc, kid = kr2[pp], ("kr", pp)
                    else:
                        ksrc, kid = kvs2[pp], ("kvs", pp)
                    for hh in range(2):
                        s.op("pe", lambda e, hh=hh, ksrc=ksrc: e.transpose(out=pX[0][0:64, hh * 128:(hh + 1) * 128], in_=ksrc[:, hh * 64:(hh + 1) * 64], identity=ident), reads=[kid, "cm"], writes=[("pX", 0)])
                    s.op("act", lambda e: e.copy(out=kT2[pp][:].rearrange("p a b -> p (a b)"), in_=pX[0][0:64, 0:256]), reads=[("pX", 0)], writes=[("kT", pp)])
                    s.dma(KT_s[:, :, tg * 128:(tg + 1) * 128], kT2[pp][:], reads=[("kT", pp)], writes=["KT_s"], queue="sp")
                    s.op("pool", lambda e: e.tensor_copy(out=va2[pp][:, :, 0:64], in_=kvs2[pp][:, 128:256].rearrange("p (g d) -> p g d", g=2)), reads=[("kvs", pp)], writes=[("va", pp)])
                    s.dma(V_s[tg * 128:(tg + 1) * 128, :, :], va2[pp][:], reads=[("va", pp)], writes=["V_s"], queue="sp")
                elif bi in (2, 3, 4):
                    s.op("act", lambda e, P=P: e.copy(out=rw2[pp][:], in_=P[:]), reads=[pid], writes=[("rw", pp)])
                    for k4 in range(4):
                        s.op("pe", lambda e, k4=k4: e.transpose(out=pX[1][:, k4 * 128:(k4 + 1) * 128], in_=rw2[pp][:, k4 * 128:(k4 + 1) * 128], identity=ident), reads=[("rw", pp), "cm"], writes=[("pX", 1)])
                    s.op("dve", lambda e: e.tensor_copy(out=rT2[pp][:].rearrange("p a b -> p (a b)"), in_=pX[1][:]), reads=[("pX", 1)], writes=[("rT", pp)])
                    f0 = (bi - 2) * 512
                    s.dma(RT_s[f0:f0 + 512, tg * 128:(tg + 1) * 128].rearrange("(a p) t -> p a t", p=128), rT2[pp][:], reads=[("rT", pp)], writes=["RT_s"], queue="sp")
                elif bi == 5:
                    s.op("act", lambda e, P=P: e.activation(out=zz2[pp][:], in_=P[:], func=AF.Silu), reads=[pid], writes=[("zz", pp)])
                    s.op("pool", lambda e: e.tensor_tensor(out=zz2[pp][:].rearrange("p (h d) -> p h d", h=4), in0=zz2[pp][:].rearrange("p (h d) -> p h d", h=4), in1=gn[:].unsqueeze(1).to_broadcast([128, 4, 128]), op=ALU.mult), reads=[("zz", pp), "gn"], writes=[("zz", pp)])
                    s.dma(Z_s[i * 128:(i + 1) * 128, :], zz2[pp][:], reads=[("zz", pp)], writes=["Z_s"], queue="sp")
                elif bi == 6:
                    s.op("dve", lambda e, P=P: e.tensor_tensor(out=ab2[pp][:, 0:8], in0=P[:, 0:8], in1=abc[:, 0, :], op=ALU.add), reads=[pid, "abc"], writes=[("ab", pp)])
                    s.op("act", lambda e: e.activation(out=ab2[pp][:, 0:8], in_=ab2[pp][:, 0:8], func=AF.Exp), reads=[("ab", pp)], writes=[("ab", pp)])
                    s.op("dve", lambda e: e.tensor_scalar(out=ab2[pp][:, 0:8], in0=ab2[pp][:, 0:8], scalar1=1.0, scalar2=None, op0=ALU.add), reads=[("ab", pp)], writes=[("ab", pp)])
                    s.op("act", lambda e: e.activation(out=ab2[pp][:, 0:8], in_=ab2[pp][:, 0:8], func=AF.Ln), reads=[("ab", pp)], writes=[("ab", pp)])
                    s.op("dve", lambda e: e.tensor_tensor(out=gbo2[pp][:, 0:8], in0=ab2[pp][:, 0:8], in1=abc[:, 1, :], op=ALU.mult), reads=[("ab", pp), "abc"], writes=[("gbo", pp)])
                    s.op("act", lambda e, P=P: e.activation(out=gbo2[pp][:, 8:16], in_=P[:, 8:16], func=AF.Sigmoid), reads=[pid], writes=[("gbo", pp)])
                    s.dma(GB_s[tg * 128:(tg + 1) * 128, :], gbo2[pp][:], reads=[("gbo", pp)], writes=["GB_s"], queue="sp")
                else:
                    gi = bi - 7
                    s.op("dve", lambda e, P=P, gi=gi: e.tensor_tensor(out=gg2[pp][:], in0=P[:], in1=bg[:, gi * 512:(gi + 1) * 512], op=ALU.add), reads=[pid, "bg"], writes=[("gg", pp)])
                    s.op("act", lambda e: e.activation(out=gg2[pp][:], in_=gg2[pp][:], func=AF.Sigmoid), reads=[("gg", pp)], writes=[("gg", pp)])
                    s.dma(GT_s[i * 128:(i + 1) * 128, gi * 512:(gi + 1) * 512], gg2[pp][:], reads=[("gg", pp)], writes=["GT_s"], queue="sp")

            groups = [tiles[0:2]] + [tiles[k:k + 4] for k in range(2, len(tiles), 4)]
            tbase = 0
            for grp in groups:
                for j, (kind, i) in enumerate(grp):
                    prep(tbase + j, kind, i, j)
                need = range(11) if grp[0][0] == "l" else (1, 2, 3, 4, 6)
                for bi in need:
                    c0, cw = blocks[bi]
                    wi = wcount[0] % 3; wcount[0] += 1
                    W = wb[wi]
                    s.dma(W[:, :, 0:cw], win_d[:, c0:c0 + cw].rearrange("(kc p) n -> p kc n", p=128), writes=[("wb", wi)], queue="pool", allow_slow_non_contiguous=(cw < 128))
                    for j, (kind, i) in enumerate(grp):
                        proj(tbase + j, kind, i, j, bi, W, wi)
                tbase += len(grp)
            s.flush()
        stA.close()
        if upto <= 1:
            return nc

        with contextlib.ExitStack() as st:
            cw = T(st, "cw", [128, 12, 5])
            Rt = [T(st, f"Rt{i}", [128, 516]) for i in range(3)]
            acc = [T(st, f"acc{i}", [128, 512]) for i in range(2)]
            y = [T(st, f"y{i}", [128, 512]) for i in range(2)]
            y2 = T(st, "y2", [128, 512]); rn = T(st, "rn", [128, 512]); yn = [T(st, f"yn{i}", [128, 512]) for i in range(2)]
            tok = [T(st, f"tok{i}", [128, 4, 128]) for i in range(2)]
            pN = [PS(st, f"pN{i}", [128, 512]) for i in range(2)]
            pK = [PS(st, f"pK{i}", [128, 512]) for i in range(2)]
            for j in range(5):
                s.dma(cw[:, :, j], conv_d[j, :].rearrange("(fc p) -> p fc", p=128), writes=["cw"], allow_slow_non_contiguous=True)
            it = 0
            segs = [(0, CTX), (CTX, TALL)]
            if "small" in dbg:
                segs = [(0, CTX), (CTX, CTX + 256)]
            for (g0, g1) in segs:
                for t0 in range(g0, g1, 512):
                    n = min(512, g1 - t0)
                    for fc in range(12):
                        R = Rt[it % 3]; rid = ("Rt", it % 3); A = acc[it % 2]; aid = ("acc", it % 2); Y = y[it % 2]; yid = ("y", it % 2)
                        lo = max(t0 - 2, g0); hi = min(t0 + n + 2, g1)
                        if lo > t0 - 2 or hi < t0 + n + 2:
                            s.op("pool", lambda e, R=R: e.memset(R[:], 0.0), writes=[rid])
                        s.dma(R[:, lo - (t0 - 2):hi - (t0 - 2)], RT_s[fc * 128:(fc + 1) * 128, lo:hi], reads=["RT_s"], writes=[rid], queue=("sp", "act")[it % 2])
                        s.op("dve", lambda e, R=R, A=A, fc=fc, n=n: e.tensor_scalar(out=A[:, 0:n], in0=R[:, 0:n], scalar1=cw[:, fc, 0:1], scalar2=None, op0=ALU.mult), reads=[rid, "cw"], writes=[aid])
                        for j in range(1, 5):
                            s.op("dve", lambda e, R=R, A=A, fc=fc, n=n, j=j: e.scalar_tensor_tensor(out=A[:, 0:n], in0=R[:, j:j + n], scalar=cw[:, fc, j:j + 1], in1=A[:, 0:n], op0=ALU.mult, op1=ALU.add), reads=[rid, "cw", aid], writes=[aid])
                        s.op("act", lambda e, A=A, Y=Y, n=n: e.activation(out=Y[:, 0:n], in_=A[:, 0:n], func=AF.Silu), reads=[aid], writes=[yid])
                        src, sid = Y, yid
                        if fc < 8:
                            YN = yn[it % 2]; nid = ("yn", it % 2); P = pN[it % 2]; pid = ("pN", it % 2)
                            s.op("act", lambda e, Y=Y, n=n: e.activation(out=y2[:, 0:n], in_=Y[:, 0:n], func=AF.Square), reads=[yid], writes=["y2"])
                            s.op("pe", lambda e, P=P, n=n: e.matmul(out=P[:, 0:n], lhsT=cm[:, C_ONES, :], rhs=y2[:, 0:n], start=True, stop=True), reads=["cm", "y2"], writes=[pid])
                            s.op("dve", lambda e, P=P, n=n: e.tensor_scalar(out=rn[:, 0:n], in0=P[:, 0:n], scalar1=1e-6, scalar2=None, op0=ALU.add), reads=[pid], writes=["rn"])
                            s.op("act", lambda e, n=n: e.sqrt(out=rn[:, 0:n], in_=rn[:, 0:n]), reads=["rn"], writes=["rn"])
                            s.op("dve", lambda e, n=n: e.reciprocal(out=rn[:, 0:n], in_=rn[:, 0:n]), reads=["rn"], writes=["rn"])
                            sc = float(128 ** -0.5) if fc < 4 else 1.0
                            s.op("dve", lambda e, Y=Y, YN=YN, n=n, sc=sc: e.scalar_tensor_tensor(out=YN[:, 0:n], in0=Y[:, 0:n], scalar=sc, in1=rn[:, 0:n], op0=ALU.mult, op1=ALU.mult), reads=[yid, "rn"], writes=[nid])
                            s.dma(QK_s[fc * 128:(fc + 1) * 128, t0:t0 + n], YN[:, 0:n], reads=[nid], writes=["QK_s"], queue="pool")
                            src, sid = YN, nid
                        if fc >= 4:
                            PK = pK[it % 2]; kid = ("pK", it % 2); TK = tok[it % 2]; tid = ("tok", it % 2)
                            nsb = n // 128
                            for sb in range(nsb):
                                s.op("pe", lambda e, PK=PK, src=src, sb=sb: e.transpose(out=PK[:, sb * 128:(sb + 1) * 128], in_=src[:, sb * 128:(sb + 1) * 128], identity=ident), reads=[sid, "cm"], writes=[kid])
                            s.op("act", lambda e, PK=PK, TK=TK, n=n: e.copy(out=TK[:].rearrange("p a b -> p (a b)")[:, 0:n], in_=PK[:, 0:n]), reads=[kid], writes=[tid])
                            s.dma(KV_s[t0:t0 + n, (fc - 4) * 128:(fc - 3) * 128].rearrange("(sb p) f -> p sb f", p=128), TK[:, 0:nsb, :], reads=[tid], writes=["KV_s"], queue="pool")
                        it += 1
            s.flush()
        if upto <= 2:
            return nc

        with contextlib.ExitStack() as st:
            Sst = [T(st, f"Sst{i}", [128, 4, 128]) for i in range(2)]
            qT4 = T(st, "qT4", [128, 4, 128]); kT4 = T(st, "kT4", [128, 4, 128]); ktok = T(st, "ktok", [128, 4, 128]); vtok = T(st, "vtok", [128, 4, 128])
            gb = T(st, "gb", [128, 16]); sm = T(st, "sm", [128, 16]); ex = T(st, "ex", [128, 16]); beg = T(st, "beg", [128, 4])
            G1 = T(st, "G1", [128, 4, 128]); dl = T(st, "dl", [128, 4, 128]); du = T(st, "du", [128, 4, 128])
            Bm = [T(st, f"Bm{i}", [128, 4, 128]) for i in range(2)]; Cm = [T(st, f"Cm{i}", [128, 4, 128]) for i in range(2)]; Pm = [T(st, f"Pm{i}", [128, 4, 128]) for i in range(2)]
            aT = T(st, "aT", [128, 4, 128]); kbg = T(st, "kbg", [128, 4, 128]); vb = T(st, "vb", [128, 4, 128]); ktl = T(st, "ktl", [128, 4, 128])
            WT = T(st, "WT", [128, 4, 128]); U = T(st, "U", [128, 4, 128]); vn = T(st, "vn", [128, 4, 128]); o1 = T(st, "o1", [128, 4, 128]); ot = T(st, "ot", [128, 4, 128])
            pb = [PS(st, f"pb{i}", [128, 4, 128]) for i in range(8)]
            pA, pB_, pC, pD, pE, pF, pG, pH = pb
            pid = lambda k: ("pb", k)
            H4 = [128, 4, 128]
            bc_h = lambda ap2: ap2.unsqueeze(1).to_broadcast(H4)
            bc_l = lambda ap2: ap2.unsqueeze(2).to_broadcast(H4)
            ntl = (2 if "small" in dbg else NT)
            for dr in range(2):
                M1 = cm[:, C_M1F if dr == 0 else C_M1B, :]; NM1 = cm[:, C_NM1F if dr == 0 else C_NM1B, :]
                NB = cm[:, C_NLOW if dr == 0 else C_NUP, :]; NTm = cm[:, C_NUP if dr == 0 else C_NLOW, :]
                STR = cm[:, C_SLOW if dr == 0 else C_SUP, :]
                SS = Sst[dr]; ssid = ("Sst", dr)
                s.op("pool", lambda e, SS=SS: e.memset(SS[:], 0.0), writes=[ssid])
                order = [("c", i) for i in range(CTX // 128)] + [("l", i) for i in range(ntl)]
                if dr == 1:
                    order = [("c", i) for i in reversed(range(CTX // 128))] + [("l", i) for i in reversed(range(ntl))]
                for (kind, i) in order:
                    lat = kind == "l"
                    tg = i if not lat else CTX // 128 + i
                    tsl = slice(tg * 128, (tg + 1) * 128)
                    s.dma(qT4[:], QK_s[0:512, tsl].rearrange("(h p) t -> p h t", p=128), reads=["QK_s"], writes=["qT4"])
                    s.dma(kT4[:], QK_s[512:1024, tsl].rearrange("(h p) t -> p h t", p=128), reads=["QK_s"], writes=["kT4"], queue="act")
                    s.dma(ktok[:].rearrange("p h d -> p (h d)"), KV_s[tsl, 0:512], reads=["KV_s"], writes=["ktok"])
                    s.dma(vtok[:].rearrange("p h d -> p (h d)"), KV_s[tsl, 512:1024], reads=["KV_s"], writes=["vtok"], queue="act")
                    s.dma(gb[:], GB_s[tsl, :], reads=["GB_s"], writes=["gb"])
                    g = gb[:, dr * 4:dr * 4 + 4]; beta = gb[:, 8 + dr * 4:12 + dr * 4]
                    pAf = pA[:].rearrange("p a b -> p (a b)")
                    for k, L in enumerate((M1, cm[:, C_SAME, :], cm[:, C_SEL0, :], cm[:, C_SEL1, :])):
                        s.op("pe", lambda e, k=k, L=L, g=g: e.matmul(out=pAf[:, 4 * k:4 * k + 4], lhsT=L, rhs=g, start=True, stop=True), reads=["cm", "gb"], writes=[pid(0)])
                    s.op("dve", lambda e: e.tensor_copy(out=sm[:], in_=pAf[:, 0:16]), reads=[pid(0)], writes=["sm"])
                    s.op("dve", lambda e: e.tensor_tensor(out=sm[:, 4:8], in0=sm[:, 4:8], in1=sm[:, 0:4], op=ALU.subtract), reads=["sm"], writes=["sm"])
                    s.op("act", lambda e: e.activation(out=ex[:], in_=sm[:], func=AF.Exp), reads=["sm"], writes=["ex"])
                    s.op("dve", lambda e, beta=beta: e.tensor_tensor(out=beg[:], in0=ex[:, 0:4], in1=beta, op=ALU.mult), reads=["ex", "gb"], writes=["beg"])
                    s.op("dve", lambda e, g=g: e.tensor_tensor(out=G1[:], in0=bc_h(cm[:, C_SAME, :]), in1=bc_l(g), op=ALU.mult), reads=["cm", "gb"], writes=["G1"])
                    for hh in range(4):
                        s.op("pe", lambda e, hh=hh, M1=M1: e.matmul(out=pB_[:, hh, :], lhsT=M1, rhs=G1[:, hh, :], start=True, stop=False), reads=["cm", "G1"], writes=[pid(1)])
                        s.op("pe", lambda e, hh=hh, NM1=NM1: e.matmul(out=pB_[:, hh, :], lhsT=G1[:, hh, :], rhs=NM1, start=False, stop=True), reads=["cm", "G1"], writes=[pid(1)])
                    s.op("dve", lambda e, NB=NB: e.tensor_tensor(out=dl[:], in0=pB_[:], in1=bc_h(NB), op=ALU.add), reads=[pid(1), "cm"], writes=["dl"])
                    s.op("dve", lambda e, NTm=NTm: e.scalar_tensor_tensor(out=du[:], in0=pB_[:], scalar=-1.0, in1=bc_h(NTm), op0=ALU.mult, op1=ALU.add), reads=[pid(1), "cm"], writes=["du"])
                    s.op("act", lambda e: e.activation(out=dl[:], in_=dl[:], func=AF.Exp), reads=["dl"], writes=["dl"])
                    s.op("act", lambda e: e.activation(out=du[:], in_=du[:], func=AF.Exp), reads=["du"], writes=["du"])
                    for hh in range(4):
                        s.op("pe", lambda e, hh=hh: e.matmul(out=pC[:, hh, :], lhsT=kT4[:, hh, :], rhs=kT4[:, hh, :], start=True, stop=True), reads=["kT4"], writes=[pid(2)])
                    for hh in range(4):
                        s.op("pe", lambda e, hh=hh: e.matmul(out=pD[:, hh, :], lhsT=kT4[:, hh, :], rhs=qT4[:, hh, :], start=True, stop=True), reads=["kT4", "qT4"], writes=[pid(3)])
                    B0 = Bm[0]; C0 = Cm[0]; P0 = Pm[0]
                    s.op("dve", lambda e: e.tensor_tensor(out=B0[:], in0=pC[:], in1=dl[:], op=ALU.mult), reads=[pid(2), "dl"], writes=[("Bm", 0)])
                    s.op("pool", lambda e, STR=STR: e.tensor_tensor(out=B0[:], in0=B0[:], in1=bc_h(STR), op=ALU.mult), reads=[("Bm", 0), "cm"], writes=[("Bm", 0)])
                    s.op("pool", lambda e, beta=beta: e.tensor_tensor(out=B0[:], in0=B0[:], in1=bc_l(beta), op=ALU.mult), reads=[("Bm", 0), "gb"], writes=[("Bm", 0)])
                    s.op("dve", lambda e: e.tensor_tensor(out=aT[:], in0=pD[:], in1=du[:], op=ALU.mult), reads=[pid(3), "du"], writes=["aT"])
                    for hh in range(4):
                        s.op("pe", lambda e, hh=hh: e.transpose(out=pE[:, hh, :], in_=B0[:, hh, :], identity=ident), reads=[("Bm", 0), "cm"], writes=[pid(4)])
                    s.op("act", lambda e: e.copy(out=C0[:], in_=pE[:]), reads=[pid(4)], writes=[("Cm", 0)])
                    s.op("dve", lambda e: e.tensor_tensor(out=P0[:], in0=C0[:], in1=bc_h(ident), op=ALU.add), reads=[("Cm", 0), "cm"], writes=[("Pm", 0)])
                    cur = 0
                    for lv in range(1, 6):
                        nx = 1 - cur
                        Bc, Cc, Pc = Bm[cur], Cm[cur], Pm[cur]; Bn, Cn, Pn = Bm[nx], Cm[nx], Pm[nx]
                        for hh in range(4):
                            s.op("pe", lambda e, hh=hh, Bc=Bc, Cc=Cc: e.matmul(out=pF[:, hh, :], lhsT=Cc[:, hh, :], rhs=Bc[:, hh, :], start=True, stop=True), reads=[("Bm", cur), ("Cm", cur)], writes=[pid(5)])
                        s.op("act", lambda e, Bn=Bn: e.copy(out=Bn[:], in_=pF[:]), reads=[pid(5)], writes=[("Bm", nx)])
                        if lv < 5:
                            for hh in range(4):
                                s.op("pe", lambda e, hh=hh, Bc=Bc, Cc=Cc: e.matmul(out=pG[:, hh, :], lhsT=Bc[:, hh, :], rhs=Cc[:, hh, :], start=True, stop=True), reads=[("Bm", cur), ("Cm", cur)], writes=[pid(6)])
                            s.op("dve", lambda e, Cn=Cn: e.tensor_copy(out=Cn[:], in_=pG[:]), reads=[pid(6)], writes=[("Cm", nx)])
                        for hh in range(4):
                            s.op("pe", lambda e, hh=hh, Pc=Pc: e.matmul(out=pH[:, hh, :], lhsT=ident, rhs=Pc[:, hh, :], start=True, stop=False), reads=[("Pm", cur), "cm"], writes=[pid(7)])
                            s.op("pe", lambda e, hh=hh, Pc=Pc, Bn=Bn: e.matmul(out=pH[:, hh, :], lhsT=Bn[:, hh, :], rhs=Pc[:, hh, :], start=False, stop=True), reads=[("Pm", cur), ("Bm", nx)], writes=[pid(7)])
                        s.op("dve", lambda e, Pn=Pn: e.tensor_copy(out=Pn[:], in_=pH[:]), reads=[pid(7)], writes=[("Pm", nx)])
                        cur = nx
                    TT = Pm[cur]; ttid = ("Pm", cur)
                    s.op("pool", lambda e: e.tensor_tensor(out=kbg[:], in0=ktok[:], in1=bc_l(beg[:]), op=ALU.mult), reads=["ktok", "beg"], writes=["kbg"])
                    s.op("pool", lambda e, beta=beta: e.tensor_tensor(out=vb[:], in0=vtok[:], in1=bc_l(beta), op=ALU.mult), reads=["vtok", "gb"], writes=["vb"])
                    s.op("pool", lambda e: e.tensor_tensor(out=ktl[:], in0=ktok[:], in1=bc_l(ex[:, 4:8]), op=ALU.mult), reads=["ktok", "ex"], writes=["ktl"])
                    for hh in range(4):
                        s.op("pe", lambda e, hh=hh, TT=TT: e.matmul(out=pE[:, hh, :], lhsT=kbg[:, hh, :], rhs=TT[:, hh, :], start=True, stop=True), reads=["kbg", ttid], writes=[pid(4)])
                    s.op("act", lambda e: e.copy(out=WT[:], in_=pE[:]), reads=[pid(4)], writes=["WT"])
                    for hh in range(4):
                        s.op("pe", lambda e, hh=hh, TT=TT: e.matmul(out=pF[:, hh, :], lhsT=TT[:, hh, :], rhs=vb[:, hh, :], start=True, stop=True), reads=["vb", ttid], writes=[pid(5)])
                    s.op("dve", lambda e: e.tensor_copy(out=U[:], in_=pF[:]), reads=[pid(5)], writes=["U"])
                    for c in ((0, 1) if dr == 0 else (1, 0)):
                        pr = slice(64 * c, 64 * c + 64)
                        for hh in range(4):
                            s.op("pe", lambda e, hh=hh, SS=SS: e.matmul(out=pG[:, hh, :], lhsT=WT[:, hh, :], rhs=SS[:, hh, :], start=True, stop=True), reads=["WT", ssid], writes=[pid(6)])
                        s.op("dve", lambda e, pr=pr: e.tensor_tensor(out=vn[pr], in0=U[pr], in1=pG[pr], op=ALU.subtract), reads=["U", pid(6)], writes=["vn"])
                        for hh in range(4):
                            s.op("pe", lambda e, hh=hh, SS=SS: e.matmul(out=pH[:, hh, :], lhsT=qT4[:, hh, :], rhs=SS[:, hh, :], start=True, stop=True), reads=["qT4", ssid], writes=[pid(7)])
                        for hh in range(4):
                            s.op("pe", lambda e, hh=hh, pr=pr: e.matmul(out=pC[:, hh, :], lhsT=aT[pr, hh, :], rhs=vn[pr, hh, :], start=True, stop=True), reads=["aT", "vn"], writes=[pid(2)])
                        for hh in range(4):
                            s.op("pe", lambda e, hh=hh, pr=pr: e.matmul(out=pD[:, hh, :], lhsT=ktl[pr, hh, :], rhs=vn[pr, hh, :], start=True, stop=True), reads=["ktl", "vn"], writes=[pid(3)])
                        if lat:
                            s.op("dve", lambda e, pr=pr: e.tensor_tensor(out=o1[pr], in0=pH[pr], in1=bc_l(ex[:, 0:4])[pr], op=ALU.mult), reads=[pid(7), "ex"], writes=["o1"])
                            s.op("dve", lambda e, pr=pr: e.tensor_tensor(out=ot[pr], in0=o1[pr], in1=pC[pr], op=ALU.add), reads=["o1", pid(2)], writes=["ot"])
                        s.op("pool", lambda e, c=c, SS=SS: e.tensor_tensor(out=SS[:], in0=SS[:], in1=bc_l(ex[:, 8 + 4 * c:12 + 4 * c]), op=ALU.mult), reads=[ssid, "ex", pid(6), pid(7)], writes=[ssid])
                        s.op("dve", lambda e, SS=SS: e.tensor_tensor(out=SS[:], in0=SS[:], in1=pD[:], op=ALU.add), reads=[ssid, pid(3)], writes=[ssid])
                    if lat:
                        s.dma(OD_s[dr, i * 128:(i + 1) * 128, :], ot[:].rearrange("p h d -> p (h d)"), reads=["ot"], writes=["OD_s"], queue="pool")
            s.flush()
        if upto <= 3:
            return nc

        with contextlib.ExitStack() as st:
            kt = [T(st, f"kt{i}", [64, 2, 384]) for i in range(2)]
            vt = [T(st, f"vt{i}", [128, 3, 130]) for i in range(2)]
            ktc = T(st, "ktc", [64, 2, 256]); vtc = T(st, "vtc", [128, 2, 130])
            qt = [T(st, f"qt{i}", [64, 8, 128]) for i in range(2)]
            E = [T(st, f"E{i}", [128, 5, 512]) for i in range(2)]
            esink = T(st, "esink", [128, 8]); den = T(st, "den", [128, 8]); oa = [T(st, f"oa{i}", [128, 512]) for i in range(2)]
            pS = [PS(st, f"pS{i}", [128, 512]) for i in range(3)]
            pO = [PS(st, f"pO{i}", [128, 4, 65]) for i in range(2)]
            s.dma(ktc[:], KT_s[:, :, 0:CTX], reads=["KT_s"], writes=["ktc"])
            s.dma(vtc[:], V_s[0:CTX].rearrange("(b p) g d -> p b (g d)", p=128), reads=["V_s"], writes=["vtc"])
            s.dma(esink[:], sink_d.partition_broadcast(128), writes=["esink"])
            s.op("act", lambda e: e.activation(out=esink[:], in_=esink[:], func=AF.Exp), reads=["esink"], writes=["esink"])
            ntl = (2 if "small" in dbg else NT)
            nS = 0
            for i in range(ntl):
                lo = max(i - 1, 0); hi = min(i + 1, ntl - 1); nb = hi - lo + 1
                KT_ = kt[i % 2]; VT_ = vt[i % 2]; QT_ = qt[i % 2]; OA = oa[i % 2]
                s.dma(KT_[:, :, 0:nb * 128], KT_s[:, :, CTX + lo * 128:CTX + (hi + 1) * 128], reads=["KT_s"], writes=[("kt", i % 2)])
                s.dma(VT_[:, 0:nb, :], V_s[CTX + lo * 128:CTX + (hi + 1) * 128].rearrange("(b p) g d -> p b (g d)", p=128), reads=["V_s"], writes=[("vt", i % 2)], queue="act")
                s.dma(QT_[:], QT_s[:, :, i * 128:(i + 1) * 128], reads=["QT_s"], writes=[("qt", i % 2)])
                for g in range(2):
                    Eg = E[g]; eid = ("E", g)
                    kb = [("l", j - lo, (C_WPREV if j < i else (C_WNEXT if j > i else None))) for j in range(lo, hi + 1)] + [("c", 0, None), ("c", 1, None)]
                    for bi, (kk, bl, msk) in enumerate(kb):
                        P = pS[nS % 3]; psid = ("pS", nS % 3); nS += 1
                        lhs = KT_[:, g, bl * 128:(bl + 1) * 128] if kk == "l" else ktc[:, g, bl * 128:(bl + 1) * 128]
                        s.op("pe", lambda e, P=P, lhs=lhs, QT_=QT_, g=g: e.matmul(out=P[:].rearrange("p (h q) -> p h q", h=4), lhsT=lhs, rhs=QT_[:, 4 * g:4 * g + 4, :], start=True, stop=True),
                             reads=[("kt", i % 2), "ktc", ("qt", i % 2)], writes=[psid])
                        s.op("act", lambda e, P=P, Eg=Eg, bi=bi: e.activation(out=Eg[:, bi, :], in_=P[:], func=AF.Exp), reads=[psid], writes=[eid])
                        if msk is not None:
                            s.op("dve", lambda e, Eg=Eg, bi=bi, msk=msk: e.tensor_tensor(out=Eg[:, bi, :].rearrange("p (h q) -> p h q", h=4), in0=Eg[:, bi, :].rearrange("p (h q) -> p h q", h=4),
                                                                              in1=cm[:, msk, :].unsqueeze(1).to_broadcast([128, 4, 128]), op=ALU.mult), reads=[eid, "cm"], writes=[eid])
                    for hh in range(4):
                        for bi, (kk, bl, msk) in enumerate(kb):
                            rhs = VT_[:, bl, g * 65:(g + 1) * 65] if kk == "l" else vtc[:, bl, g * 65:(g + 1) * 65]
                            s.op("pe", lambda e, Eg=Eg, bi=bi, hh=hh, rhs=rhs, g=g, last=(bi == len(kb) - 1): e.matmul(out=pO[g][:, hh, :], lhsT=Eg[:, bi, hh * 128:(hh + 1) * 128], rhs=rhs, start=(bi == 0), stop=last),
                                 reads=[eid, ("vt", i % 2), "vtc"], writes=[("pO", g)])
                    s.op("dve", lambda e, g=g: e.tensor_tensor(out=den[:, 4 * g:4 * g + 4], in0=pO[g][:, :, 64], in1=esink[:, 4 * g:4 * g + 4], op=ALU.add), reads=[("pO", g), "esink"], writes=["den"])
                    s.op("dve", lambda e, g=g: e.reciprocal(out=den[:, 4 * g:4 * g + 4], in_=den[:, 4 * g:4 * g + 4]), reads=["den"], writes=["den"])
                    s.op("dve", lambda e, g=g, OA=OA: e.tensor_tensor(out=OA[:, g * 256:(g + 1) * 256].rearrange("p (h d) -> p h d", h=4), in0=pO[g][:, :, 0:64],
                                                              in1=den[:, 4 * g:4 * g + 4].unsqueeze(2).to_broadcast([128, 4, 64]), op=ALU.mult), reads=[("pO", g), "den"], writes=[("oa", i % 2)])
                s.dma(OA_s[i * 128:(i + 1) * 128, :], OA[:], reads=[("oa", i % 2)], writes=["OA_s"], queue="pool")
            s.flush()
        if upto <= 5:
            return nc

        UTr_s = nc.dram_tensor("UTr_s", [64, 128, 8, 256], F32R, kind="Internal").ap()
        Vr_s = nc.dram_tensor("Vr_s", [16384, D], F32R, kind="Internal").ap()
        X1_s = scr("X1_s", [S, D])
        H2T_s = scr("H2T_s", [D, S])
        H2R_s = nc.dram_tensor("H2R_s", [D, S], F32R, kind="Internal").ap()
        SC_s = scr("SC_s", [S, 2048])
        KAP_s = scr("KAP_s", [S, 8])
        TR = lambda st, name, shape: st.enter_context(nc.sbuf_tensor(_nm(name), list(shape), F32R))
        B = lambda k: ("bk", k)
        ntl6 = (2 if "small" in dbg else NT)

        with contextlib.ExitStack() as st:
            cvb = [TR(st, "cvb", [128, 4096]) for _ in range(2)]
            wbr = T(st, "wbr", [128, 8, D]); wo = T(st, "wo", [128, 8, D])
            xa = [T(st, f"xa{b}", [128, D]) for b in range(2)]; yb = [T(st, f"yb{b}", [128, D]) for b in range(2)]
            tcA = [T(st, f"tcA{b}", [128, 8, 128]) for b in range(2)]; h2r = [TR(st, f"h2r{b}", [128, 8, 128]) for b in range(2)]
            gt = [T(st, f"gt{b}", [128, 2048]) for b in range(2)]
            od = [T(st, f"od{b}", [128, 2, 512]) for b in range(2)]; zt = [T(st, f"zt{b}", [128, 512]) for b in range(2)]
            oat = [T(st, f"oat{b}", [128, 512]) for b in range(2)]; o2 = [T(st, f"o2{b}", [128, 512]) for b in range(2)]
            qsb = [T(st, f"qsb{b}", [128, D]) for b in range(2)]
            ssq = [T(st, f"ssq{b}", [128, 4]) for b in range(2)]; ss = [T(st, f"ss{b}", [128, 1]) for b in range(2)]; rstd = [T(st, f"rstd{b}", [128, 1]) for b in range(2)]
            bk = [PS(st, f"bkA{i}", [128, 512]) for i in range(8)]
            cjobs = []
            for r0 in range(0, D, 128):
                for c0 in range(0, 16384, 4096):
                    cjobs.append((pu_d[r0:r0 + 128, c0:c0 + 4096], UTr_s[c0 // 256:c0 // 256 + 16, :, r0 // 128, :].rearrange("g p n -> p g n"), "UTr_s"))
            for r0 in range(0, 16384, 512):
                cjobs.append((pv_d[r0:r0 + 512, :].rearrange("(p a) n -> p (a n)", p=128), Vr_s[r0:r0 + 512, :].rearrange("(p a) n -> p (a n)", p=128), "Vr_s"))
            cstate = [0]

            def conv_some(n):
                for _ in range(n):
                    if not cjobs:
                        return
                    src, dst, did = cjobs.pop(0)
                    k = cstate[0] % 2; cstate[0] += 1
                    s.dma(cvb[k][:], src, writes=[("cvb", k)], queue="pool")
                    s.dma(dst, (cvb[k][:].rearrange("p (g n) -> p g n", n=256) if did == "UTr_s" else cvb[k][:]), reads=[("cvb", k)], writes=[did], queue="pool")
            s.dma(wbr[:, 0:4, :], wba_d.rearrange("(kc p) n -> p kc n", p=128), writes=["wbr"])
            s.dma(wbr[:, 4:8, :], wbd_d.rearrange("(kc p) n -> p kc n", p=128), writes=["wbr"], queue="act")
            s.dma(wo[:], wout_d.rearrange("(kc p) n -> p kc n", p=128), writes=["wo"])

            def tr8(b, src, sid, nkc, dst_off, dst, did, extra=None):
                pT = [bk[4 * b], bk[4 * b + 1]]
                for kc in range(nkc):
                    q_ = (dst_off + kc) // 4
                    s.op("pe", lambda e, kc=kc, q_=q_: e.transpose(out=pT[q_][:, ((dst_off + kc) % 4) * 128:((dst_off + kc) % 4 + 1) * 128], in_=src[:, kc * 128:(kc + 1) * 128], identity=ident), reads=[sid, "cm"], writes=[B(4 * b + q_)])
                for q_ in sorted(set((dst_off + kc) // 4 for kc in range(nkc))):
                    if q_ == 0:
                        s.op("act", lambda e, q_=q_: e.copy(out=dst[:, q_ * 4:(q_ + 1) * 4, :].rearrange("p a b -> p (a b)"), in_=pT[q_][:]), reads=[B(4 * b + q_)], writes=[(did, q_)])
                    else:
                        s.op("dve", lambda e, q_=q_: e.tensor_copy(out=dst[:, q_ * 4:(q_ + 1) * 4, :].rearrange("p a b -> p (a b)"), in_=pT[q_][:]), reads=[B(4 * b + q_)], writes=[(did, q_)])
                    if extra is not None:
                        d2, d2id = extra
                        if q_ == 0:
                            s.op("dve", lambda e, q_=q_: e.tensor_copy(out=d2[:, q_ * 4:(q_ + 1) * 4, :].rearrange("p a b -> p (a b)"), in_=pT[q_][:]), reads=[B(4 * b + q_)], writes=[(d2id, q_)])
                        else:
                            s.op("act", lambda e, q_=q_: e.copy(out=d2[:, q_ * 4:(q_ + 1) * 4, :].rearrange("p a b -> p (a b)"), in_=pT[q_][:]), reads=[B(4 * b + q_)], writes=[(d2id, q_)])

            def rms6(b, src, sid):
                s.op("act", lambda e: e.activation(out=qsb[b][:], in_=src[:], func=AF.Square, accum_out=ss[b][:]), reads=[sid], writes=[("qsb", b), ("ss", b)])
                s.op("dve", lambda e: e.tensor_scalar(out=rstd[b][:], in0=ss[b][:], scalar1=1.0 / D, scalar2=1e-6, op0=ALU.mult, op1=ALU.add), reads=[("ss", b)], writes=[("rstd", b)])
                s.op("act", lambda e: e.sqrt(out=rstd[b][:], in_=rstd[b][:]), reads=[("rstd", b)], writes=[("rstd", b)])
                s.op("dve", lambda e: e.reciprocal(out=rstd[b][:], in_=rstd[b][:]), reads=[("rstd", b)], writes=[("rstd", b)])

            for i in range(ntl6):
                conv_some(4)
                b = i % 2
                tsl = slice(i * 128, (i + 1) * 128)
                XA = xa[b]; YB = yb[b]; GT = gt[b]; OD = od[b]; ZT = zt[b]; OAT = oat[b]; O2 = o2[b]; QSB = qsb[b]; SSQ = ssq[b]; TC = tcA[b]
                pY = [bk[4 * b + 2], bk[4 * b + 3]]
                s.dma(XA[:], x_d[tsl, :], writes=[("xa", b)])
                s.dma(OD[:, 0, :], OD_s[0, tsl, :], reads=["OD_s"], writes=[("od", b)], queue="act")
                s.dma(OD[:, 1, :], OD_s[1, tsl, :], reads=["OD_s"], writes=[("od", b)], queue="act")
                s.dma(ZT[:], Z_s[tsl, :], reads=["Z_s"], writes=[("zt", b)])
                s.dma(OAT[:], OA_s[tsl, :], reads=["OA_s"], writes=[("oat", b)], queue="act")
                s.dma(GT[:], GT_s[tsl, :], reads=["GT_s"], writes=[("gt", b)])
                s.op("dve", lambda e, OD=OD: e.tensor_tensor(out=OD[:, 0, :], in0=OD[:, 0, :], in1=OD[:, 1, :], op=ALU.add), reads=[("od", b)], writes=[("od", b)])
                s.op("pool", lambda e, OD=OD, O2=O2: e.tensor_tensor(out=O2[:], in0=OD[:, 0, :], in1=OD[:, 0, :], op=ALU.mult), reads=[("od", b)], writes=[("o2", b)])
                s.op("dve", lambda e, O2=O2, SSQ=SSQ: e.tensor_reduce(out=SSQ[:], in_=O2[:].rearrange("p (h d) -> p h d", h=4), axis=AX.X, op=ALU.add), reads=[("o2", b)], writes=[("ssq", b)])
                s.op("dve", lambda e, SSQ=SSQ: e.tensor_scalar(out=SSQ[:], in0=SSQ[:], scalar1=1.0 / 128, scalar2=1e-6, op0=ALU.mult, op1=ALU.add), reads=[("ssq", b)], writes=[("ssq", b)])
                s.op("act", lambda e, SSQ=SSQ: e.sqrt(out=SSQ[:], in_=SSQ[:]), reads=[("ssq", b)], writes=[("ssq", b)])
                s.op("dve", lambda e, SSQ=SSQ: e.reciprocal(out=SSQ[:], in_=SSQ[:]), reads=[("ssq", b)], writes=[("ssq", b)])
                s.op("dve", lambda e, O2=O2, OD=OD, SSQ=SSQ: e.tensor_tensor(out=O2[:].rearrange("p (h d) -> p h d", h=4), in0=OD[:, 0, :].rearrange("p (h d) -> p h d", h=4), in1=SSQ[:].unsqueeze(2).to_broadcast([128, 4, 128]), op=ALU.mult), reads=[("od", b), ("ssq", b)], writes=[("o2", b)])
                s.op("pool", lambda e, O2=O2, ZT=ZT: e.tensor_tensor(out=O2[:], in0=O2[:], in1=ZT[:], op=ALU.mult), reads=[("o2", b), ("zt", b)], writes=[("o2", b)])
                tr8(b, OAT, ("oat", b), 4, 0, TC, ("tcA", b))
                tr8(b, O2, ("o2", b), 4, 4, TC, ("tcA", b))
                for half in range(2):
                    for kc in range(4):
                        s.op("pe", lambda e, half=half, kc=kc, pY=pY, TC=TC: e.matmul(out=pY[half][:], lhsT=TC[:, kc, :], rhs=wbr[:, kc, half * 512:(half + 1) * 512], start=(kc == 0), stop=(kc == 3)), reads=[(("tcA", b), 0), "wbr"], writes=[B(4 * b + 2 + half)])
                    s.op("dve", lambda e, half=half, pY=pY, YB=YB, GT=GT: e.tensor_tensor(out=YB[:, half * 512:(half + 1) * 512], in0=pY[half][:], in1=GT[:, half * 512:(half + 1) * 512], op=ALU.mult), reads=[B(4 * b + 2 + half), ("gt", b)], writes=[("yb", b)])
                for half in range(2):
                    for kc in range(4):
                        s.op("pe", lambda e, half=half, kc=kc, pY=pY, TC=TC: e.matmul(out=pY[half][:], lhsT=TC[:, 4 + kc, :], rhs=wbr[:, 4 + kc, half * 512:(half + 1) * 512], start=(kc == 0), stop=(kc == 3)), reads=[(("tcA", b), 1), "wbr"], writes=[B(4 * b + 2 + half)])
                    s.op("dve", lambda e, half=half, pY=pY, QSB=QSB, GT=GT: e.tensor_tensor(out=QSB[:, half * 512:(half + 1) * 512], in0=pY[half][:], in1=GT[:, 1024 + half * 512:1024 + (half + 1) * 512], op=ALU.mult), reads=[B(4 * b + 2 + half), ("gt", b)], writes=[("qsb", b)])
                s.op("pool", lambda e, YB=YB, QSB=QSB: e.tensor_tensor(out=YB[:], in0=YB[:], in1=QSB[:], op=ALU.add), reads=[("yb", b), ("qsb", b)], writes=[("yb", b)])
                tr8(b, YB, ("yb", b), 8, 0, TC, ("tcA", b))
                for half in range(2):
                    for kc in range(8):
                        s.op("pe", lambda e, half=half, kc=kc, pY=pY, TC=TC: e.matmul(out=pY[half][:], lhsT=TC[:, kc, :], rhs=wo[:, kc, half * 512:(half + 1) * 512], start=(kc == 0), stop=(kc == 7)), reads=[(("tcA", b), 0), (("tcA", b), 1), "wo"], writes=[B(4 * b + 2 + half)])
                    s.op("dve", lambda e, half=half, pY=pY, YB=YB: e.tensor_tensor(out=YB[:, half * 512:(half + 1) * 512], in0=pY[half][:], in1=bvv(BV_GT1)[:, half * 512:(half + 1) * 512], op=ALU.mult), reads=[B(4 * b + 2 + half), "bv"], writes=[("yb", b)])
                s.op("pool", lambda e, XA=XA, YB=YB: e.tensor_tensor(out=XA[:], in0=XA[:], in1=YB[:], op=ALU.add), reads=[("xa", b), ("yb", b)], writes=[("xa", b)])
                s.dma(X1_s[tsl, :], XA[:], reads=[("xa", b)], writes=["X1_s"], queue="act")
                rms6(b, XA, ("xa", b))
                s.op("dve", lambda e, XA=XA, YB=YB, RS=rstd[b]: e.scalar_tensor_tensor(out=YB[:], in0=XA[:], scalar=RS[:, 0:1], in1=bvv(BV_G2), op0=ALU.mult, op1=ALU.mult), reads=[("xa", b), ("rstd", b), "bv"], writes=[("yb", b)])
                s.op("pool", lambda e, YB=YB: e.tensor_tensor(out=YB[:], in0=YB[:], in1=bvv(BV_SH2), op=ALU.add), reads=[("yb", b), "bv"], writes=[("yb", b)])
                tr8(b, YB, ("yb", b), 8, 0, TC, ("tcA", b), extra=(h2r[b], ("h2r", b)))
                s.dma(H2T_s[:, tsl].rearrange("(kc p) t -> p kc t", p=128), TC[:], reads=[(("tcA", b), 0), (("tcA", b), 1)], writes=["H2T_s"])
                s.dma(H2R_s[:, tsl].rearrange("(kc p) t -> p kc t", p=128), h2r[b][:], reads=[(("h2r", b), 0), (("h2r", b), 1)], writes=["H2R_s"], queue="act")
            conv_some(10 ** 6)
            s.flush()
        if upto <= 6:
            return nc

        with contextlib.ExitStack() as st:
            wq = T(st, "wq", [128, 8, D]); keys2 = T(st, "keys2", [128, 8, 128])
            tcB = [T(st, f"tcB{b}", [128, 8, 128]) for b in range(2)]
            qsb = [T(st, f"qsbB{b}", [128, D]) for b in range(2)]; qTs = [T(st, f"qTs{b}", [128, 8, 128]) for b in range(2)]
            sc = [T(st, f"scB{b}", [128, 16, 128]) for b in range(2)]
            t16 = [T(st, f"t16{b}", [128, 8, 2, 16]) for b in range(2)]; c16 = [T(st, f"c16{b}", [128, 8, 16]) for b in range(2)]
            wk4 = T(st, "wk4", [128, 4, 256]); cand4 = T(st, "cand4", [128, 4, 256])
            thr = [T(st, f"thr{b}", [128, 8]) for b in range(2)]; negm = [T(st, f"negm{b}", [128, 8]) for b in range(2)]; Zs = [T(st, f"Zs{b}", [128, 8]) for b in range(2)]
            kap = [T(st, f"kap{b}", [128, 8]) for b in range(2)]; m1 = [T(st, f"m1{b}", [128, 8]) for b in range(2)]; th2 = [T(st, f"th2{b}", [128, 8]) for b in range(2)]
            e16 = [T(st, f"e16{b}", [128, 8, 16]) for b in range(2)]
            bk = [PS(st, f"bkB{i}", [128, 512]) for i in range(8)]
            s.dma(wq[:], pwq_d.rearrange("(kc p) n -> p kc n", p=128), writes=["wq"])
            s.dma(keys2[:], pkeys_d, writes=["keys2"])
            _cb = [int(x[4:]) for x in dbg if x.startswith("cutB")]
            cutB = _cb[0] if _cb else 99
            for i in range(ntl6):
                b = i % 2
                tsl = slice(i * 128, (i + 1) * 128)
                TC = tcB[b]; QSB = qsb[b]; QT = qTs[b]; SC = sc[b]; T16 = t16[b]; C16 = c16[b]
                pT = [bk[4 * b], bk[4 * b + 1]]; pY = [bk[4 * b + 2], bk[4 * b + 3]]
                s.dma(TC[:], H2T_s[:, tsl].rearrange("(kc p) t -> p kc t", p=128), reads=["H2T_s"], writes=[("tcB", b)], queue=("sp", "act")[b])
                for half in range(2):
                    for kc in range(8):
                        s.op("pe", lambda e, half=half, kc=kc, pY=pY, TC=TC: e.matmul(out=pY[half][:], lhsT=TC[:, kc, :], rhs=wq[:, kc, half * 512:(half + 1) * 512], start=(kc == 0), stop=(kc == 7)), reads=[("tcB", b), "wq"], writes=[B(4 * b + 2 + half)])
                    if half == 0:
                        s.op("act", lambda e, pY=pY, QSB=QSB: e.copy(out=QSB[:, 0:512], in_=pY[0][:]), reads=[B(4 * b + 2)], writes=[("qsbB", b)])
                    else:
                        s.op("dve", lambda e, pY=pY, QSB=QSB: e.tensor_copy(out=QSB[:, 512:1024], in_=pY[1][:]), reads=[B(4 * b + 3)], writes=[("qsbB", b)])
                if cutB < 2:
                    continue
                for hh in range(8):
                    s.op("pe", lambda e, hh=hh, pT=pT, QSB=QSB: e.transpose(out=pT[hh // 4][:, (hh % 4) * 128:(hh % 4 + 1) * 128], in_=QSB[:, hh * 128:(hh + 1) * 128], identity=ident), reads=[("qsbB", b), "cm"], writes=[B(4 * b + hh // 4)])
                s.op("act", lambda e, pT=pT, QT=QT: e.copy(out=QT[:, 0:4, :].rearrange("p a b -> p (a b)"), in_=pT[0][:]), reads=[B(4 * b)], writes=[("qTs", b)])
                s.op("dve", lambda e, pT=pT, QT=QT: e.tensor_copy(out=QT[:, 4:8, :].rearrange("p a b -> p (a b)"), in_=pT[1][:]), reads=[B(4 * b + 1)], writes=[("qTs", b)])
                if cutB < 3:
                    continue
                banks = [pY[0], pY[1], pT[0], pT[1]]; bids = [B(4 * b + 2), B(4 * b + 3), B(4 * b), B(4 * b + 1)]
                for p in range(2):
                    for hh in range(8):
                        bi_ = 2 * p + hh // 4
                        s.op("pe", lambda e, hh=hh, p=p, bi_=bi_, QT=QT, banks=banks: e.matmul(out=banks[bi_][:, (hh % 4) * 128:(hh % 4 + 1) * 128], lhsT=QT[64 * p:64 * p + 64, hh, :], rhs=keys2[64 * p:64 * p + 64, hh, :], start=True, stop=True), reads=[("qTs", b), "keys2"], writes=[bids[bi_]])
                SC4 = SC[:].rearrange("p (h q) k -> p h q k", q=2)
                for bi_ in range(4):
                    p, hq = bi_ // 2, bi_ % 2
                    dst = SC4[:, hq * 4:(hq + 1) * 4, p, :]
                    src = banks[bi_][:].rearrange("p (a k) -> p a k", a=4)
                    allq = [("scB", b, q4) for q4 in range(4)]
                    if bi_ % 2 == 0:
                        s.op("act", lambda e, dst=dst, src=src: e.copy(out=dst, in_=src), reads=[bids[bi_]], writes=[("scB", b, 2 * hq), ("scB", b, 2 * hq + 1)])
                    else:
                        s.op("dve", lambda e, dst=dst, src=src: e.tensor_copy(out=dst, in_=src), reads=[bids[bi_]], writes=[("scB", b, 2 * hq), ("scB", b, 2 * hq + 1)])
                if cutB < 4:
                    continue
                for hb in range(2):
                    hs = range(hb * 4, hb * 4 + 4)
                    scid = lambda hh: ("scB", b, hh // 2)
                    for hh in hs:
                        for p in range(2):
                            s.op("dve", lambda e, hh=hh, p=p, SC=SC, T16=T16: e.max(out=T16[:, hh, p, 0:8], in_=SC[:, 2 * hh + p, :]), reads=[scid(hh)], writes=[("t16", b, hh, p)])
                    for hh in hs:
                        for p in range(2):
                            s.op("dve", lambda e, hh=hh, p=p, SC=SC, T16=T16: e.match_replace(out=wk4[:, hh % 4, p * 128:(p + 1) * 128], in_to_replace=T16[:, hh, p, 0:8], in_values=SC[:, 2 * hh + p, :], imm_value=-1e30), reads=[scid(hh), ("t16", b, hh, p)], writes=[("wk4", hh % 4, p)])
                    for hh in hs:
                        for p in range(2):
                            s.op("dve", lambda e, hh=hh, p=p, T16=T16: e.max(out=T16[:, hh, p, 8:16], in_=wk4[:, hh % 4, p * 128:(p + 1) * 128]), reads=[("wk4", hh % 4, p)], writes=[("t16", b, hh, p)])
                    for hh in hs:
                        s.op("dve", lambda e, hh=hh, T16=T16: e.tensor_tensor(out=cand4[:, hh % 4, :].rearrange("p (a c) -> p a c", a=16), in0=T16[:, hh, 0, :].unsqueeze(2).to_broadcast([128, 16, 16]), in1=T16[:, hh, 1, :].unsqueeze(1).to_broadcast([128, 16, 16]), op=ALU.add), reads=[("t16", b, hh, 0), ("t16", b, hh, 1)], writes=[("cand4", hh % 4)])
                    for hh in hs:
                        s.op("dve", lambda e, hh=hh, C16=C16: e.max(out=C16[:, hh, 0:8], in_=cand4[:, hh % 4, :]), reads=[("cand4", hh % 4)], writes=[("c16", b, hh)])
                    for hh in hs:
                        s.op("dve", lambda e, hh=hh, C16=C16: e.match_replace(out=wk4[:, hh % 4, :], in_to_replace=C16[:, hh, 0:8], in_values=cand4[:, hh % 4, :], imm_value=-1e30), reads=[("cand4", hh % 4), ("c16", b, hh)], writes=[("wk4", hh % 4, 0), ("wk4", hh % 4, 1)])
                    for hh in hs:
                        s.op("dve", lambda e, hh=hh, C16=C16: e.max(out=C16[:, hh, 8:16], in_=wk4[:, hh % 4, :]), reads=[("wk4", hh % 4, 0), ("wk4", hh % 4, 1)], writes=[("c16", b, hh)])
                if cutB < 5:
                    continue
                allc = [("c16", b, hh) for hh in range(8)]; allt = [("t16", b, hh, 0) for hh in range(8)]
                THR = thr[b]; NEGM = negm[b]; M1 = m1[b]; ZS = Zs[b]; KAP = kap[b]; TH2 = th2[b]; E16 = e16[b]
                s.op("dve", lambda e, C16=C16, THR=THR: e.tensor_scalar(out=THR[:], in0=C16[:, :, 15], scalar1=-1e-4, scalar2=None, op0=ALU.add), reads=allc, writes=[("thr", b)])
                s.op("dve", lambda e, C16=C16, NEGM=NEGM: e.tensor_scalar(out=NEGM[:], in0=C16[:, :, 0], scalar1=-1.0, scalar2=None, op0=ALU.mult), reads=allc, writes=[("negm", b)])
                s.op("dve", lambda e, T16=T16, M1=M1: e.tensor_copy(out=M1[:], in_=T16[:, :, 0, 0]), reads=allt, writes=[("m1", b)])
                s.op("dve", lambda e, C16=C16, NEGM=NEGM, E16=E16: e.tensor_tensor(out=E16[:], in0=C16[:], in1=NEGM[:].unsqueeze(2).to_broadcast([128, 8, 16]), op=ALU.add), reads=allc + [("negm", b)], writes=[("e16", b)])
                s.op("act", lambda e, E16=E16: e.activation(out=E16[:], in_=E16[:], func=AF.Exp), reads=[("e16", b)], writes=[("e16", b)])
                s.op("dve", lambda e, E16=E16, ZS=ZS: e.tensor_reduce(out=ZS[:], in_=E16[:], axis=AX.X, op=ALU.add), reads=[("e16", b)], writes=[("Zs", b)])
                s.op("dve", lambda e, KAP=KAP, THR=THR, NEGM=NEGM: e.tensor_tensor(out=KAP[:], in0=THR[:], in1=NEGM[:], op=ALU.add), reads=[("thr", b), ("negm", b)], writes=[("kap", b)])
                s.op("act", lambda e, KAP=KAP: e.activation(out=KAP[:], in_=KAP[:], func=AF.Exp), reads=[("kap", b)], writes=[("kap", b)])
                s.op("dve", lambda e, ZS=ZS: e.reciprocal(out=ZS[:], in_=ZS[:]), reads=[("Zs", b)], writes=[("Zs", b)])
                s.op("dve", lambda e, KAP=KAP, ZS=ZS: e.tensor_tensor(out=KAP[:], in0=KAP[:], in1=ZS[:], op=ALU.mult), reads=[("kap", b), ("Zs", b)], writes=[("kap", b)])
                s.op("dve", lambda e, TH2=TH2, THR=THR, M1=M1: e.tensor_tensor(out=TH2[:], in0=THR[:], in1=M1[:], op=ALU.subtract), reads=[("thr", b), ("m1", b)], writes=[("th2", b)])
                sc4 = SC[:].rearrange("p (h q) k -> p h q k", q=2)
                allsc = [("scB", b, q4) for q4 in range(4)]
                s.op("dve", lambda e, sc4=sc4, M1=M1: e.tensor_tensor(out=sc4[:, :, 0, :], in0=sc4[:, :, 0, :], in1=M1[:].unsqueeze(2).to_broadcast([128, 8, 128]), op=ALU.subtract), reads=allsc + [("m1", b)], writes=allsc)
                s.op("pool", lambda e, sc4=sc4, TH2=TH2: e.tensor_tensor(out=sc4[:, :, 1, :], in0=sc4[:, :, 1, :], in1=TH2[:].unsqueeze(2).to_broadcast([128, 8, 128]), op=ALU.subtract), reads=allsc + [("th2", b)], writes=allsc)
                s.op("act", lambda e, SC=SC: e.activation(out=SC[:], in_=SC[:], func=AF.Exp), reads=allsc, writes=allsc)
                s.dma(SC_s[tsl, :], SC[:].rearrange("p a b -> p (a b)"), reads=allsc, writes=["SC_s"], queue="pool")
                s.dma(KAP_s[tsl, :], KAP[:], reads=[("kap", b)], writes=["KAP_s"], queue="pool")
            s.flush()
        if upto <= 7:
            return nc

        GI = 2
        NG = 128 // GI
        NB = 2
        with contextlib.ExitStack() as st:
            _xa = T(st, "xc", [128, D]); xa = [_xa, _xa]; yb = T(st, "ybc", [128, D]); qsb = yb
            ss = T(st, "ssc", [128, 1]); rstd = T(st, "rstdc", [128, 1])
            h2r = [[TR(st, f"h2c{k}{t}", [128, 8, 128]) for t in range(NB)] for k in range(2)]
            sc = [[T(st, f"scc{k}{t}", [128, 16, 128]) for t in range(NB)] for k in range(2)]
            kap = [[T(st, f"kapc{k}{t}", [128, 8]) for t in range(NB)] for k in range(2)]
            dg = [[TR(st, f"dgc{k}{t}", [128, 8, 128]) for t in range(NB)] for k in range(2)]
            UT = [TR(st, f"UT{i}", [128, 8, GI * 128]) for i in range(2)]
            VG = [TR(st, f"VG{i}", [128, GI, D]) for i in range(2)]
            pe_t = [T(st, f"pec{t}", [128, 8, GI * 128]) for t in range(NB)]
            Mr2 = [[TR(st, f"Mr{k}{t}", [128, 8, GI * 128]) for t in range(NB)] for k in range(2)]
            Sm2 = [TR(st, f"Sm{k}", [128, 8, GI * 128]) for k in range(2)]
            negone = T(st, "negone", [128, 1])
            s.op("pool", lambda e: e.memset(negone[:], -1.0), writes=["negone"])
            W5 = NB * GI * 128
            g1 = [T(st, f"g1c{i}", [128, W5]) for i in range(2)]; Pm_ = [T(st, f"Pmc{i}", [128, W5]) for i in range(2)]
            PT = [TR(st, f"PT{i}", [128, NB * GI, 128]) for i in range(2)]
            bk = [PS(st, f"bkC{i}", [128, 512]) for i in range(8)]
            nblk = ntl6 // NB
            ngr = (2 if "small2" in dbg else NG)
            gcount = 0

            def blk_load(blk):
                k = blk % 2
                for tau in range(NB):
                    i = blk * NB + tau
                    tsl = slice(i * 128, (i + 1) * 128)
                    s.dma(h2r[k][tau][:], H2R_s[:, tsl].rearrange("(kc p) t -> p kc t", p=128), reads=["H2R_s"], writes=[("h2c", k, tau)], queue="pool")
                    s.dma(sc[k][tau][:].rearrange("p a b -> p (a b)"), SC_s[tsl, :], reads=["SC_s"], writes=[("scc", k, tau)], queue="pool")
                    s.dma(kap[k][tau][:], KAP_s[tsl, :], reads=["KAP_s"], writes=[("kapc", k, tau)], queue="pool")
                    for hh in range(8):
                        s.op("dve", lambda e, hh=hh, tau=tau, k=k: e.tensor_scalar(out=dg[k][tau][:, hh, :], in0=ident, scalar1=kap[k][tau][:, hh:hh + 1], scalar2=None, op0=ALU.mult), reads=["cm", ("kapc", k, tau)], writes=[("dgc", k, tau)])

            blk_load(0)
            for blk in range(nblk):
              kb = blk % 2
              pU = [[bk[4 + 2 * t + hf] for hf in range(2)] for t in range(NB)]
              ub = [k % 2 for k in range(ngr + 4)]

              def g_utload(g):
                  u = g % 2; ub[g] = u
                  e0 = g * GI * 128
                  s.dma(UT[u][:], UTr_s[g], reads=["UTr_s"], writes=[("UT", u)], queue="sp")

              def g_vgload(g):
                  u = g % 2
                  e0 = g * GI * 128
                  s.dma(VG[u][:], Vr_s[e0:e0 + GI * 128, :].rearrange("(a p) n -> p a n", p=128), reads=["Vr_s"], writes=[("VG", u)], queue="act")

              def g_prodmask(g):
                  mk = g % 2
                  for tau in range(NB):
                      sc4 = sc[kb][tau][:].rearrange("p (h q) k -> p h q k", q=2)
                      e1b = sc4[:, :, 0, g * GI:(g + 1) * GI].unsqueeze(3).to_broadcast([128, 8, GI, 128])
                      e2b = sc4[:, :, 1, :].unsqueeze(2).to_broadcast([128, 8, GI, 128])
                      s.op("dve", lambda e, e1b=e1b, e2b=e2b, tau=tau: e.tensor_tensor(out=pe_t[tau][:].rearrange("p h (a k) -> p h a k", a=GI), in0=e1b, in1=e2b, op=ALU.mult), reads=[("scc", kb, tau)], writes=[("pe", tau)])
                      if tau == 0:
                          s.op("dve", lambda e, mk=mk: e.scalar_tensor_tensor(out=Mr2[mk][0][:], in0=pe_t[0][:], scalar=1.0, in1=pe_t[0][:], op0=ALU.is_ge, op1=ALU.mult), reads=[("pe", 0)], writes=[("Mr", mk, 0)])
                      else:
                          s.op("act", lambda e, mk=mk: e.activation(out=Mr2[mk][1][:], in_=pe_t[1][:], func=AF.Relu, bias=negone[:, 0:1], scale=1.0), reads=[("pe", 1), "negone"], writes=[("Mr", mk, 1)])
                          s.op("act", lambda e, mk=mk: e.activation(out=Sm2[mk][:], in_=Mr2[mk][1][:].bitcast(F32), func=AF.Sign), reads=[("Mr", mk, 1)], writes=[("Sm", mk)])

              def g_act(g):
                  u = ub[g]; pR = bk[u]
                  for tau in range(NB):
                      for kc in range(8):
                          s.op("pe", lambda e, kc=kc, u=u, tau=tau, pR=pR, H=h2r[kb][tau]: e.matmul(out=pR[:, tau * GI * 128:(tau + 1) * GI * 128], lhsT=H[:, kc, :], rhs=UT[u][:, kc, :], start=(kc == 0), stop=(kc == 7)), reads=[("h2c", kb, tau), ("UT", u)], writes=[B(u)])

              def g_gelu(g):
                  u = ub[g]; pR = bk[u]
                  s.op("act", lambda e, pR=pR, u=u: e.activation(out=g1[u][:], in_=pR[:, 0:W5], func=AF.Gelu_apprx_tanh), reads=[B(u)], writes=[("g1", u)])

              def g_gd(g):
                  u = ub[g]; pG = bk[2 + u]; mk = g % 2
                  for hh in range(8):
                      s.op("pe", lambda e, hh=hh, pG=pG, DG=dg[kb][0], M=Mr2[mk][0]: e.matmul(out=pG[:, 0:GI * 128], lhsT=DG[:, hh, :], rhs=M[:, hh, :], start=(hh == 0), stop=(hh == 7)), reads=[("dgc", kb, 0), ("Mr", mk, 0)], writes=[B(2 + u)])
                  for hh in range(8):
                      s.op("pe", lambda e, hh=hh, pG=pG, DG=dg[kb][1], M=Mr2[mk][1]: e.matmul(out=pG[:, GI * 128:2 * GI * 128], lhsT=DG[:, hh, :], rhs=M[:, hh, :], start=(hh == 0), stop=False), reads=[("dgc", kb, 1), ("Mr", mk, 1)], writes=[B(2 + u)])
                  for hh in range(8):
                      s.op("pe", lambda e, hh=hh, pG=pG, DG=dg[kb][1], M=Sm2[mk]: e.matmul(out=pG[:, GI * 128:2 * GI * 128], lhsT=DG[:, hh, :], rhs=M[:, hh, :], start=False, stop=(hh == 7)), reads=[("dgc", kb, 1), ("Sm", mk)], writes=[B(2 + u)])

              def g_pm(g):
                  u = ub[g]; pG = bk[2 + u]
                  s.op("dve", lambda e, u=u, pG=pG: e.tensor_tensor(out=Pm_[u][:], in0=g1[u][:], in1=pG[:, 0:W5], op=ALU.mult), reads=[("g1", u), B(2 + u)], writes=[("Pm", u)])

              def g_tr(g):
                  u = ub[g]; pW = bk[2 + u]
                  for k in range(NB * GI):
                      s.op("pe", lambda e, k=k, u=u, pW=pW: e.transpose(out=pW[:, k * 128:(k + 1) * 128], in_=Pm_[u][:, k * 128:(k + 1) * 128], identity=ident), reads=[("Pm", u), "cm"], writes=[B(2 + u)])
                  s.op("act", lambda e, u=u, pW=pW: e.copy(out=PT[u][:].rearrange("p a b -> p (a b)"), in_=pW[:, 0:W5]), reads=[B(2 + u)], writes=[("PT", u)])

              def g_out(g):
                  u = ub[g]
                  for tau in range(NB):
                      for a in range(GI):
                          for half in range(2):
                              s.op("pe", lambda e, a=a, half=half, u=u, tau=tau, first=(g == 0 and a == 0), last=(g == ngr - 1 and a == GI - 1): e.matmul(out=pU[tau][half][:], lhsT=PT[u][:, tau * GI + a, :], rhs=VG[u][:, a, half * 512:(half + 1) * 512], start=first, stop=last),
                                   reads=[("PT", u), ("VG", u)], writes=[B(4 + 2 * tau + half)])

              ok = lambda k: 0 <= k < ngr
              for g in range(-3, ngr + 1):
                  if ok(g + 3):
                      g_utload(g + 3)
                  if ok(g - 1):
                      g_out(g - 1)
                  if ok(g + 1):
                      g_vgload(g + 1)
                      g_gd(g + 1)
                  if ok(g):
                      g_tr(g)
                  if ok(g + 2):
                      g_act(g + 2); g_gelu(g + 2)
                  if ok(g + 3):
                      g_prodmask(g + 3)
                  if ok(g + 1):
                      g_pm(g + 1)
                  if g == 4 and blk + 1 < nblk:
                      blk_load(blk + 1)
              for tau in range(NB):
                i = blk * NB + tau
                XA = xa[tau]; xid = "xc"
                s.dma(XA[:], X1_s[i * 128:(i + 1) * 128, :], reads=["X1_s"], writes=[xid], queue="pool")
                for half in range(2):
                    s.op("dve", lambda e, half=half, tau=tau: e.tensor_tensor(out=yb[:, half * 512:(half + 1) * 512], in0=pU[tau][half][:], in1=bvv(BV_GT2)[:, half * 512:(half + 1) * 512], op=ALU.mult), reads=[B(4 + 2 * tau + half), "bv"], writes=["ybc"])
                s.op("pool", lambda e, XA=XA: e.tensor_tensor(out=XA[:], in0=XA[:], in1=yb[:], op=ALU.add), reads=[xid, "ybc"], writes=[xid])
                s.op("act", lambda e, XA=XA: e.activation(out=qsb[:], in_=XA[:], func=AF.Square, accum_out=ss[:]), reads=[xid, "ybc"], writes=["ybc", "ssc"])
                s.op("dve", lambda e: e.tensor_scalar(out=rstd[:], in0=ss[:], scalar1=1.0 / D, scalar2=1e-6, op0=ALU.mult, op1=ALU.add), reads=["ssc"], writes=["rstdc"])
                s.op("act", lambda e: e.sqrt(out=rstd[:], in_=rstd[:]), reads=["rstdc"], writes=["rstdc"])
                s.op("dve", lambda e: e.reciprocal(out=rstd[:], in_=rstd[:]), reads=["rstdc"], writes=["rstdc"])
                s.op("dve", lambda e, XA=XA: e.scalar_tensor_tensor(out=yb[:], in0=XA[:], scalar=rstd[:, 0:1], in1=bvv(BV_FN), op0=ALU.mult, op1=ALU.mult), reads=[xid, "rstdc", "bv"], writes=["ybc"])
                s.dma(out_d[i * 128:(i + 1) * 128, :], yb[:], reads=["ybc"], writes=["out"], queue="pool")
            s.flush()
        return nc


def _rope(s, src, dst, tmp, R, rid, H, sid, did, tid="tmp"):
    sv = src.rearrange("p (h a b c) -> p h a b c", h=H, a=2, b=2)
    dv = dst.rearrange("p (h a b c) -> p h a b c", h=H, a=2, b=2)
    tv = tmp[:, 0:H * 32].rearrange("p (h a c) -> p h a c", h=H, a=2)
    rv = R[:].rearrange("p (a b c) -> p a b c", a=2, b=2)
    cosb = rv[:, :, 0, :].unsqueeze(1).to_broadcast([128, H, 2, 16])
    sinb = rv[:, :, 1, :].unsqueeze(1).to_broadcast([128, H, 2, 16])
    x1 = sv[:, :, :, 0, :]; x2 = sv[:, :, :, 1, :]
    o1 = dv[:, :, :, 0, :]; o2 = dv[:, :, :, 1, :]
    s.op("dve", lambda e: e.tensor_tensor(out=o1, in0=x1, in1=cosb, op=ALU.mult), reads=[sid, rid], writes=[did])
    s.op("dve", lambda e: e.tensor_tensor(out=tv, in0=x2, in1=sinb, op=ALU.mult), reads=[sid, rid], writes=[tid])
    s.op("dve", lambda e: e.tensor_tensor(out=o1, in0=o1, in1=tv, op=ALU.subtract), reads=[did, tid], writes=[did])
    s.op("dve", lambda e: e.tensor_tensor(out=o2, in0=x1, in1=sinb, op=ALU.mult), reads=[sid, rid, did], writes=[did])
    s.op("dve", lambda e: e.tensor_tensor(out=tv, in0=x2, in1=cosb, op=ALU.mult), reads=[sid, rid, did], writes=[tid])
    s.op("dve", lambda e: e.tensor_tensor(out=o2, in0=o2, in1=tv, op=ALU.add), reads=[did, tid], writes=[did])


def _host_inputs(inputs, b, consts):
    g = lambda k: np.ascontiguousarray(inputs[k], dtype=np.float32)
    m = {
        "x": g("x")[b], "c": g("c")[b], "ctx": g("ctx")[b], "c_ctx": g("c_ctx"),
        "w_ada": g("w_ada")[0], "b_ada": g("b_ada")[0], "norm_mix": g("norm_mix")[0], "norm_ffn": g("norm_ffn")[0],
        "w_in": g("w_in")[0], "b_gate": g("b_gate")[0], "attn_sink": g("attn_sink")[0], "dn_conv": g("dn_conv")[0],
        "dn_a_log_f": g("dn_a_log_f")[0], "dn_dt_bias_f": g("dn_dt_bias_f")[0], "dn_a_log_b": g("dn_a_log_b")[0], "dn_dt_bias_b": g("dn_dt_bias_b")[0],
        "dn_norm": g("dn_norm")[0], "w_br_attn": g("w_br_attn")[0], "w_br_dn": g("w_br_dn")[0], "w_out": g("w_out")[0],
        "peer_wq": g("peer_wq")[0], "final_norm": g("final_norm"),
    }
    m.update(consts)
    return {k: np.ascontiguousarray(v) for k, v in m.items()}


_SHARED = {}


def kernel(**inputs):
    consts = _consts()
    nc = build()
    keysT = np.ascontiguousarray(np.transpose(np.asarray(inputs["peer_keys"], np.float32)[0], (1, 3, 0, 2)).reshape(128, 8, 128))
    uT = np.ascontiguousarray(np.asarray(inputs["peer_u"], np.float32)[0].T)
    pv = np.ascontiguousarray(np.asarray(inputs["peer_v"], np.float32)[0])
    in_maps = []
    for b in range(8):
        m = _host_inputs(inputs, b, consts)
        m["peer_keysT"] = keysT; m["peer_uT"] = uT; m["peer_v"] = pv
        in_maps.append(m)
    res = run_bass_kernel_spmd(nc, in_maps, core_ids=list(range(8)))
    return np.stack([np.asarray(r["out"], dtype=np.float32) for r in res.results], axis=0)
```
